# Optimizing a Trainium2 kernel written in Bass

```python
import math
import jax, jax.numpy as jnp
from jax import lax
import numpy as np

D_MODEL = 1024
BATCH = 8
SEQ = 2048
DEPTH = 2

CHUNK = 64

SSD_D_INNER = D_MODEL
SSD_HEADDIM = 64
SSD_HEADS = SSD_D_INNER // SSD_HEADDIM
SSD_GROUPS = 2
SSD_STATE = 128
SSD_CONV = 4
SSD_XBC = SSD_D_INNER + 2 * SSD_GROUPS * SSD_STATE

RWKV_DIM = D_MODEL
RWKV_HEAD = 64
RWKV_HEADS = RWKV_DIM // RWKV_HEAD
RWKV_W_LORA = 64
RWKV_A_LORA = 64
RWKV_G_LORA = 128
RWKV_COLS = 3 * RWKV_DIM + RWKV_W_LORA + RWKV_A_LORA + RWKV_G_LORA
RWKV_GN_EPS = 64e-5

HGRN_DIM = D_MODEL
HGRN_EXPAND = 128
HGRN_HEADS = HGRN_DIM // HGRN_EXPAND
HGRN_VDIM = HGRN_DIM // HGRN_HEADS

IN_SIZES = (SSD_D_INNER, SSD_XBC, SSD_HEADS, RWKV_COLS, 4 * HGRN_DIM, 3 * D_MODEL)
N_IN = sum(IN_SIZES)

N_EXPERTS = 16
N_GROUPS = 4
EXPERTS_PER_GROUP = N_EXPERTS // N_GROUPS
TOPK_GROUPS = 1
TOP_K = 2
D_EXPERT = 512

DN_ALPHA = (2 * DEPTH) ** 0.25
DN_BETA = (8 * DEPTH) ** -0.25
LN_EPS = 1e-5
RMS_EPS = 1e-6

kernel_name = "hybrid_ssd_rwkv7_hgrn2_moe_deepnorm"


def _split(x, sizes):
    offs, acc = [], 0
    for s in sizes[:-1]:
        acc += s
        offs.append(acc)
    return jnp.split(x, offs, axis=-1)


def _layernorm(x, g, b):
    xf = x.astype(jnp.float32)
    mu = jnp.mean(xf, -1, keepdims=True)
    var = jnp.mean(jnp.square(xf - mu), -1, keepdims=True)
    return ((xf - mu) * lax.rsqrt(var + LN_EPS) * g + b).astype(x.dtype)


def _rmsnorm(x, w):
    xf = x.astype(jnp.float32)
    return xf * lax.rsqrt(jnp.mean(xf * xf, -1, keepdims=True) + RMS_EPS) * w


def _causal_dwconv(x, w, b):
    k, c = w.shape
    y = lax.conv_general_dilated(x, w[:, None, :], window_strides=(1,), padding=[(k - 1, 0)],
                                 dimension_numbers=("NWC", "WIO", "NWC"), feature_group_count=c)
    return y + b


def _chunk_scan(states, decay):
    def step(s, inp):
        st, dec = inp
        return s * dec + st, s
    s0 = jnp.zeros_like(states[:, 0])
    _, s_in = lax.scan(step, s0, (jnp.moveaxis(states, 1, 0), jnp.moveaxis(decay, 1, 0)))
    return jnp.moveaxis(s_in, 0, 1)


def _ssd(z, xbc, dt_raw, conv_w, conv_b, dt_bias, a_log, d_skip, norm_w):
    f32 = jnp.float32
    b, L, _ = z.shape
    nc = L // CHUNK
    hg = SSD_HEADS // SSD_GROUPS
    xbc = jax.nn.silu(_causal_dwconv(xbc, conv_w, conv_b))
    xs, bm, cm = _split(xbc, (SSD_D_INNER, SSD_GROUPS * SSD_STATE, SSD_GROUPS * SSD_STATE))
    xs = xs.reshape(b, nc, CHUNK, SSD_GROUPS, hg, SSD_HEADDIM).astype(f32)
    bm = bm.reshape(b, nc, CHUNK, SSD_GROUPS, SSD_STATE).astype(f32)
    cm = cm.reshape(b, nc, CHUNK, SSD_GROUPS, SSD_STATE).astype(f32)
    dt = jax.nn.softplus(dt_raw.astype(f32) + dt_bias)
    a = -jnp.exp(a_log.astype(f32))
    da = (dt * a).reshape(b, nc, CHUNK, SSD_GROUPS, hg)
    dt = dt.reshape(b, nc, CHUNK, SSD_GROUPS, hg)
    acs = jnp.cumsum(da, axis=2)
    causal = jnp.tril(jnp.ones((CHUNK, CHUNK), bool))
    seg = acs[:, :, :, None] - acs[:, :, None, :]
    decay = jnp.exp(jnp.where(causal[:, :, None, None], seg, -jnp.inf))
    cb = jnp.einsum('bclgn,bcsgn->bclsg', cm, bm)
    wts = cb[..., None] * decay * dt[:, :, None]
    y_diag = jnp.einsum('bclsgh,bcsghp->bclghp', wts, xs)
    to_end = jnp.exp(acs[:, :, -1:] - acs) * dt
    states = jnp.einsum('bclgn,bclgh,bclghp->bcghpn', bm, to_end, xs)
    s_in = _chunk_scan(states, jnp.exp(acs[:, :, -1])[..., None, None])
    y_off = jnp.einsum('bclgn,bcghpn,bclgh->bclghp', cm, s_in, jnp.exp(acs))
    y = y_diag + y_off + xs * d_skip.astype(f32).reshape(SSD_GROUPS, hg)[:, :, None]
    y = y.reshape(b, L, SSD_D_INNER) * jax.nn.silu(z.astype(f32))
    y = _rmsnorm(y.reshape(b, L, SSD_GROUPS, -1), norm_w.reshape(SSD_GROUPS, -1))
    return y.reshape(b, L, SSD_D_INNER).astype(z.dtype)


def _rwkv7(feat, mu, w0, w2, a0, a2, g2, k_k, k_a, r_k, ln_w, ln_b):
    f32 = jnp.float32
    b, L, _ = feat.shape
    prev = jnp.pad(feat, ((0, 0), (1, 0), (0, 0)))[:, :-1]
    feat = feat + (prev - feat) * mu
    r, k, v, xw, xa, xg = _split(feat, (RWKV_DIM, RWKV_DIM, RWKV_DIM, RWKV_W_LORA, RWKV_A_LORA, RWKV_G_LORA))
    w = -jax.nn.softplus(-(w0 + jnp.tanh(xw) @ w2)) - 0.5
    decay = jnp.exp(-jnp.exp(w.astype(f32)))
    a = jax.nn.sigmoid((a0 + xa @ a2).astype(f32))
    g = jax.nn.sigmoid(xg) @ g2
    hs = lambda t: t.astype(f32).reshape(b, L, RWKV_HEADS, RWKV_HEAD)
    kk = hs(k * k_k)
    kk = kk / jnp.maximum(jnp.sqrt(jnp.sum(kk * kk, -1, keepdims=True)), 1e-12)
    k = k.astype(f32) * (1.0 + (a - 1.0) * k_a)
    r, k, v, decay, a = hs(r), hs(k), hs(v), hs(decay), hs(a)
    a_vec, b_vec = -kk, kk * a

    def step(s, inp):
        r_t, w_t, k_t, v_t, a_t, b_t = inp
        sa = jnp.einsum('bhvk,bhk->bhv', s, a_t)
        s = s * w_t[:, :, None, :] + sa[..., None] * b_t[:, :, None, :] + v_t[..., None] * k_t[:, :, None, :]
        return s, jnp.einsum('bhvk,bhk->bhv', s, r_t)

    tm = lambda t: jnp.moveaxis(t, 1, 0)
    s0 = jnp.zeros((b, RWKV_HEADS, RWKV_HEAD, RWKV_HEAD), f32)
    _, y = lax.scan(step, s0, (tm(r), tm(decay), tm(k), tm(v), tm(a_vec), tm(b_vec)))
    y = jnp.moveaxis(y, 0, 1)
    mean = jnp.mean(y, -1, keepdims=True)
    var = jnp.mean(jnp.square(y - mean), -1, keepdims=True)
    y = ((y - mean) * lax.rsqrt(var + RWKV_GN_EPS)).reshape(b, L, RWKV_DIM) * ln_w + ln_b
    bonus = jnp.sum(r * k * r_k.astype(f32), -1, keepdims=True) * v
    y = (y + bonus.reshape(b, L, RWKV_DIM)) * g
    return y.astype(feat.dtype)


def _hgrn2(feat, lb, norm_w):
    f32 = jnp.float32
    b, L, _ = feat.shape
    nc = L // CHUNK
    q, f, i, g = jnp.split(feat, 4, axis=-1)
    q = jax.nn.silu(q.astype(f32))
    forget = lb + (1.0 - lb) * jax.nn.sigmoid(f.astype(f32))
    k = 1.0 - forget
    rs = lambda t, d: t.reshape(b, nc, CHUNK, HGRN_HEADS, d)
    q, k = rs(q, HGRN_EXPAND), rs(k, HGRN_EXPAND)
    logf = rs(jnp.log(forget), HGRN_EXPAND)
    v = rs(i.astype(f32), HGRN_VDIM)
    bc = jnp.cumsum(logf, axis=2)
    ref = bc[:, :, CHUNK // 2:CHUNK // 2 + 1]
    att = jnp.einsum('bclhd,bcshd->bchls', q * jnp.exp(bc - ref), k * jnp.exp(ref - bc))
    causal = jnp.tril(jnp.ones((CHUNK, CHUNK), bool))
    att = jnp.where(causal, att, 0.0)
    o_intra = jnp.einsum('bchls,bcshe->bclhe', att, v)
    states = jnp.einsum('bclhd,bclhe->bchde', k * jnp.exp(bc[:, :, -1:] - bc), v)
    s_in = _chunk_scan(states, jnp.exp(bc[:, :, -1])[..., None])
    o_inter = jnp.einsum('bclhd,bchde->bclhe', q * jnp.exp(bc), s_in)
    o = _rmsnorm(o_intra + o_inter, norm_w).reshape(b, L, HGRN_DIM)
    o = o * jax.nn.sigmoid(g.astype(f32))
    return o.astype(feat.dtype)


def _moe(x, router_w, router_bias, w_gate, w_up, w_down):
    f32 = jnp.float32
    b, L, d = x.shape
    t = x.reshape(-1, d)
    probs = jax.nn.softmax((t @ router_w).astype(f32), axis=-1)
    sel = probs + router_bias.astype(f32)
    grp = sel.reshape(-1, N_GROUPS, EXPERTS_PER_GROUP)
    grp_score = jnp.sum(lax.top_k(grp, TOP_K)[0], -1)
    _, gidx = lax.top_k(grp_score, TOPK_GROUPS)
    gmask = jnp.sum(jax.nn.one_hot(gidx, N_GROUPS, dtype=f32), -2) > 0
    masked = jnp.where(jnp.repeat(gmask, EXPERTS_PER_GROUP, axis=-1), sel, -jnp.inf)
    _, eidx = lax.top_k(masked, TOP_K)
    w_sel = jnp.take_along_axis(probs, eidx, -1)
    w_sel = w_sel / jnp.sum(w_sel, -1, keepdims=True)
    gates = jnp.sum(jax.nn.one_hot(eidx, N_EXPERTS, dtype=f32) * w_sel[..., None], -2)
    out = jnp.zeros(t.shape, f32)
    for e in range(N_EXPERTS):
        h = jax.nn.silu(t @ w_gate[e]) * (t @ w_up[e])
        out = out + gates[:, e:e + 1] * (h @ w_down[e]).astype(f32)
    return out.reshape(b, L, d).astype(x.dtype)


def setup_inputs(seed: int = 0) -> dict:
    key = jax.random.key(seed)
    ks = iter(jax.random.split(key, 48))
    f32 = jnp.float32

    def nrm(shape, scale):
        return jax.random.normal(next(ks), shape, f32) * scale

    def unif(shape, lo, hi):
        return jax.random.uniform(next(ks), shape, f32, lo, hi)

    nl = DEPTH
    dt0 = jnp.exp(unif((nl, SSD_HEADS), math.log(1e-3), math.log(1e-1)))
    return {
        "x": nrm((BATCH, SEQ, D_MODEL), 1.0),
        "ln_in_g": 1.0 + nrm((D_MODEL,), 0.02),
        "ln_in_b": nrm((D_MODEL,), 0.02),
        "w_in": nrm((nl, D_MODEL, N_IN), D_MODEL ** -0.5),
        "ssd_conv_w": nrm((nl, SSD_CONV, SSD_XBC), SSD_CONV ** -0.5),
        "ssd_conv_b": nrm((nl, SSD_XBC), 0.02),
        "ssd_dt_bias": dt0 + jnp.log(-jnp.expm1(-dt0)),
        "ssd_a_log": jnp.log(unif((nl, SSD_HEADS), 1.0, 16.0)),
        "ssd_d": 1.0 + nrm((nl, SSD_HEADS), 0.1),
        "ssd_norm_w": 1.0 + nrm((nl, SSD_D_INNER), 0.02),
        "rwkv_mu": unif((nl, RWKV_COLS), 0.0, 1.0),
        "rwkv_w0": unif((nl, RWKV_DIM), -6.0, -1.0),
        "rwkv_w2": nrm((nl, RWKV_W_LORA, RWKV_DIM), 0.1 * RWKV_W_LORA ** -0.5),
        "rwkv_a0": nrm((nl, RWKV_DIM), 0.1),
        "rwkv_a2": nrm((nl, RWKV_A_LORA, RWKV_DIM), 0.1 * RWKV_A_LORA ** -0.5),
        "rwkv_g2": nrm((nl, RWKV_G_LORA, RWKV_DIM), RWKV_G_LORA ** -0.5),
        "rwkv_k_k": 0.85 + nrm((nl, RWKV_DIM), 0.02),
        "rwkv_k_a": 1.0 + nrm((nl, RWKV_DIM), 0.02),
        "rwkv_r_k": nrm((nl, RWKV_HEADS, RWKV_HEAD), 0.1),
        "rwkv_ln_w": 1.0 + nrm((nl, RWKV_DIM), 0.02),
        "rwkv_ln_b": nrm((nl, RWKV_DIM), 0.02),
        "hgrn_lb": nrm((nl, HGRN_DIM), 0.5),
        "hgrn_norm_w": 1.0 + nrm((nl, HGRN_VDIM), 0.02),
        "w_br_ssd": nrm((nl, SSD_D_INNER, D_MODEL), DN_BETA * SSD_D_INNER ** -0.5),
        "w_br_rwkv": nrm((nl, RWKV_DIM, D_MODEL), DN_BETA * RWKV_DIM ** -0.5),
        "w_br_hgrn": nrm((nl, HGRN_DIM, D_MODEL), DN_BETA * HGRN_DIM ** -0.5),
        "w_out": nrm((nl, D_MODEL, D_MODEL), DN_BETA * D_MODEL ** -0.5),
        "ln1_g": 1.0 + nrm((nl, D_MODEL), 0.02),
        "ln1_b": nrm((nl, D_MODEL), 0.02),
        "router_w": nrm((D_MODEL, N_EXPERTS), D_MODEL ** -0.5),
        "router_bias": nrm((N_EXPERTS,), 0.01),
        "exp_w_gate": nrm((nl, N_EXPERTS, D_MODEL, D_EXPERT), DN_BETA * D_MODEL ** -0.5),
        "exp_w_up": nrm((nl, N_EXPERTS, D_MODEL, D_EXPERT), DN_BETA * D_MODEL ** -0.5),
        "exp_w_down": nrm((nl, N_EXPERTS, D_EXPERT, D_MODEL), DN_BETA * D_EXPERT ** -0.5),
        "ln2_g": 1.0 + nrm((nl, D_MODEL), 0.02),
        "ln2_b": nrm((nl, D_MODEL), 0.02),
    }


def reference(x, ln_in_g, ln_in_b, w_in, ssd_conv_w, ssd_conv_b, ssd_dt_bias, ssd_a_log, ssd_d, ssd_norm_w,
              rwkv_mu, rwkv_w0, rwkv_w2, rwkv_a0, rwkv_a2, rwkv_g2, rwkv_k_k, rwkv_k_a, rwkv_r_k, rwkv_ln_w,
              rwkv_ln_b, hgrn_lb, hgrn_norm_w, w_br_ssd, w_br_rwkv, w_br_hgrn, w_out, ln1_g, ln1_b, router_w,
              router_bias, exp_w_gate, exp_w_up, exp_w_down, ln2_g, ln2_b):
    h = _layernorm(x, ln_in_g, ln_in_b)
    lsm = jax.nn.softmax(hgrn_lb.astype(jnp.float32), axis=0)
    lower_bounds = jnp.cumsum(lsm, axis=0) - lsm[0]
    for l in range(DEPTH):
        feats = h @ w_in[l]
        z, xbc, dt_raw, f_rwkv, f_hgrn, gates = _split(feats, IN_SIZES)
        y_a = _ssd(z, xbc, dt_raw, ssd_conv_w[l], ssd_conv_b[l], ssd_dt_bias[l], ssd_a_log[l], ssd_d[l],
                   ssd_norm_w[l])
        y_b = _rwkv7(f_rwkv, rwkv_mu[l], rwkv_w0[l], rwkv_w2[l], rwkv_a0[l], rwkv_a2[l], rwkv_g2[l],
                     rwkv_k_k[l], rwkv_k_a[l], rwkv_r_k[l], rwkv_ln_w[l], rwkv_ln_b[l])
        y_c = _hgrn2(f_hgrn, lower_bounds[l], hgrn_norm_w[l])
        g_a, g_b, g_c = jnp.split(gates, 3, axis=-1)
        merged = (jax.nn.sigmoid(g_a) * (y_a @ w_br_ssd[l])
                  + jax.nn.sigmoid(g_b) * (y_b @ w_br_rwkv[l])
                  + jax.nn.sigmoid(g_c) * (y_c @ w_br_hgrn[l]))
        h = _layernorm(DN_ALPHA * h + merged @ w_out[l], ln1_g[l], ln1_b[l])
        moe = _moe(h, router_w, router_bias, exp_w_gate[l], exp_w_up[l], exp_w_down[l])
        h = _layernorm(DN_ALPHA * h + moe, ln2_g[l], ln2_b[l])
    return h
```

```python
import contextlib
import os
import numpy as np
CUT = int(os.environ.get('CUT', '99'))
HC = int(os.environ.get('HC', '99'))
HL = int(os.environ.get('HL', '99'))
import concourse.bass as bass
import concourse.mybir as mybir
from concourse.bass_utils import run_bass_kernel_spmd

F32 = mybir.dt.float32
BF16 = mybir.dt.bfloat16
AF = mybir.ActivationFunctionType
ALU = mybir.AluOpType
AX = mybir.AxisListType

D = 1024
T = 2048
NT = T // 128
KT = D // 128
DEPTH = 2
NE = 16
DEXP = 512
N_IN = 13072
ALPHA = (2 * DEPTH) ** 0.25
LN_EPS = 1e-5
RMS_EPS = 1e-6
GN_EPS = 64e-5
OFF_Z = 0
OFF_XBC = 1024
OFF_DT = 2560
OFF_RWKV = 2576
OFF_HGRN = OFF_RWKV + 3328
OFF_GATES = OFF_HGRN + 4096

PARAM_SHAPES = {
    "ln_in_g": [1024], "ln_in_b": [1024], "w_in": [2, 1024, 13072],
    "ssd_conv_w": [2, 4, 1536], "ssd_conv_b": [2, 1536], "ssd_dt_bias": [2, 16],
    "ssd_a_log": [2, 16], "ssd_d": [2, 16], "ssd_norm_w": [2, 1024],
    "rwkv_mu": [2, 3328], "rwkv_w0": [2, 1024], "rwkv_w2": [2, 64, 1024],
    "rwkv_a0": [2, 1024], "rwkv_a2": [2, 64, 1024], "rwkv_g2": [2, 128, 1024],
    "rwkv_k_k": [2, 1024], "rwkv_k_a": [2, 1024], "rwkv_r_k": [2, 16, 64],
    "rwkv_ln_w": [2, 1024], "rwkv_ln_b": [2, 1024], "hgrn_lb": [2, 1024],
    "hgrn_norm_w": [2, 128], "w_br_ssd": [2, 1024, 1024], "w_br_rwkv": [2, 1024, 1024],
    "w_br_hgrn": [2, 1024, 1024], "w_out": [2, 1024, 1024], "ln1_g": [2, 1024],
    "ln1_b": [2, 1024], "router_w": [1024, 16], "router_bias": [16],
    "exp_w_gate": [2, 16, 1024, 512], "exp_w_up": [2, 16, 1024, 512],
    "exp_w_down": [2, 16, 512, 1024], "ln2_g": [2, 1024], "ln2_b": [2, 1024],
}


class Sched:
    ENG = ["pe", "act", "dve", "pool", "sp"]

    def __init__(self, nc, es, n_dma=32, n_pdma=24):
        self.nc = nc
        self.e = {"pe": nc.tensor, "act": nc.scalar, "dve": nc.vector, "pool": nc.gpsimd, "sp": nc.sync}
        self.sem = {k: es.enter_context(nc.semaphore("sem_" + k)) for k in self.ENG}
        self.cnt = {k: 0 for k in self.ENG}
        self.dsem = [es.enter_context(nc.semaphore("dsem%d" % i)) for i in range(n_dma)]
        self.dtot = [0] * n_dma
        self.drr = 0
        self.psem = [es.enter_context(nc.semaphore("psem%d" % i)) for i in range(n_pdma)]
        self.pused = [False] * n_pdma
        self.pwaiters = [[] for _ in range(n_pdma)]
        self.pclr = [None] * n_pdma
        self.prr = 0
        self.msem = {k: es.enter_context(nc.semaphore("msem_" + k)) for k in self.ENG}
        self.mcnt = {k: 0 for k in self.ENG}
        self.seen = {k: {} for k in self.ENG}
        self.lastw = {}
        self.readers = {}
        self.nwait = 0

    def _semh(self, sk):
        if isinstance(sk, str):
            return self.sem[sk]
        if sk[0] == "m":
            return self.msem[sk[1]]
        return self.dsem[sk[1]] if sk[0] == "d" else self.psem[sk[1]]

    def _wait(self, e, tag):
        sk, val = tag
        if val <= 0 or self.seen[e].get(sk, 0) >= val:
            return
        if not isinstance(sk, str) and sk[0] == "p" and self.pclr[sk[1]] is not None and e != "pool":
            self._wait(e, self.pclr[sk[1]])
        self.e[e].wait_ge(self._semh(sk), val)
        self.seen[e][sk] = val
        self.nwait += 1
        if not isinstance(sk, str) and sk[0] == "p":
            self.pwaiters[sk[1]].append(self._marker(e))

    def _marker(self, e):
        self.e[e].sem_inc(self.msem[e], 1)
        self.mcnt[e] += 1
        return (("m", e), self.mcnt[e])

    def _deps(self, e, r, w):
        for k in r:
            t = self.lastw.get(k)
            if t is not None:
                self._wait(e, t)
        for k in w:
            t = self.lastw.get(k)
            if t is not None and (t[0] != e or e != "pe"):
                self._wait(e, t)
            for sk, val in self.readers.get(k, {}).items():
                if sk != e or e != "pe":
                    self._wait(e, (sk, val))

    def _record(self, tag, r, w):
        for k in r:
            d = self.readers.setdefault(k, {})
            if d.get(tag[0], 0) < tag[1]:
                d[tag[0]] = tag[1]
        for k in w:
            self.lastw[k] = tag
            self.readers[k] = {}

    def op(self, e, fn, r=(), w=()):
        self._deps(e, r, w)
        ins = fn()
        self.cnt[e] += 1
        ins.then_inc(self.sem[e], 1)
        if os.environ.get("OPLOG"):
            self.oplog = getattr(self, "oplog", {})
            self.oplog[(e, self.cnt[e])] = fn.__code__.co_firstlineno
        self._record((e, self.cnt[e]), r, w)

    def _dma_sw(self, out, in_, r, w):
        q = "pool"
        self._deps(q, r, w)
        i = self.prr
        self.prr = (self.prr + 1) % len(self.psem)
        sk = ("p", i)
        if self.pused[i]:
            self._wait(q, (sk, 16))
            for e in self.ENG:
                if e != q:
                    self._wait(e, (sk, 16))
            for tg in self.pwaiters[i]:
                if tg[0][1] != q:
                    self._wait(q, tg)
            self.e[q].sem_clear(self.psem[i])
            tclr = self._marker(q)
            self.pclr[i] = tclr
            for k, t in list(self.lastw.items()):
                if t[0] == sk:
                    self.lastw[k] = tclr
            for k, d in self.readers.items():
                if sk in d:
                    d.pop(sk)
                    d[tclr[0]] = tclr[1]
            for e in self.ENG:
                self.seen[e].pop(sk, None)
            self.pwaiters[i] = []
        ins = self.e[q].dma_start(out=out, in_=in_)
        ins.then_inc(self.psem[i], 16)
        self.pused[i] = True
        self._record((sk, 16), r, w)

    def dma(self, q, out, in_, r=(), w=()):
        if q == "pool" and os.environ.get("PSEM_CLEAR"):
            return self._dma_sw(out, in_, r, w)
        self._deps(q, r, w)
        i = self.drr
        self.drr = (self.drr + 1) % len(self.dsem)
        self._wait(q, (("d", i), self.dtot[i]))
        with self.nc.allow_non_contiguous_dma(reason="small per-feature parameter columns"):
            ins = self.e[q].dma_start(out=out, in_=in_)
        self.dtot[i] += 16
        ins.then_inc(self.dsem[i], 16)
        self._record((("d", i), self.dtot[i]), r, w)

    def barrier(self):
        for e in self.ENG:
            for o in self.ENG:
                if o != e:
                    self._wait(e, (o, self.cnt[o]))
            for i in range(len(self.dsem)):
                self._wait(e, (("d", i), self.dtot[i]))
            for i in range(len(self.psem)):
                if self.pused[i]:
                    self._wait(e, (("p", i), 16))

    def finish(self):
        for i in range(len(self.dsem)):
            self._wait("sp", (("d", i), self.dtot[i]))
        for i in range(len(self.psem)):
            if self.pused[i]:
                self._wait("sp", (("p", i), 16))
        for o in self.ENG:
            if o != "sp":
                self._wait("sp", (o, self.cnt[o]))


class Builder:
    def __init__(self, debug=None, stages=("pre", "hgrn", "ssd", "rwkv", "merge", "moe"), depth=DEPTH, pre_router=False):
        self.pre_router = pre_router
        self.debug = debug or {}
        self.stages = stages
        self.depth = depth
        self.nc = bass.Bass("TRN2", target_bir_lowering=False)
        nc = self.nc
        self.x = nc.dram_tensor("x", [T, D], F32, kind="ExternalInput").ap()
        self.P = {k: nc.dram_tensor(k, s, F32, kind="ExternalInput").ap() for k, s in PARAM_SHAPES.items()}
        self.out = nc.dram_tensor("out", [T, D], F32, kind="ExternalOutput").ap()
        self.h_dram = nc.dram_tensor("h_scr", [T, D], F32, kind="Internal").ap()
        self.yT_dram = [nc.dram_tensor("yT_scr%d" % i, [D, T], BF16, kind="Internal").ap() for i in range(3)]
        self.dbg_out = {}
        for name, (shape, dt) in self.debug.items():
            self.dbg_out[name] = nc.dram_tensor("dbg_" + name, shape, dt, kind="ExternalOutput").ap()
        self.uid = 0

    def sb(self, es, name, shape, dt):
        self.uid += 1
        return es.enter_context(self.nc.sbuf_tensor("%s_%d" % (name, self.uid), shape, dt))

    def psum(self):
        i = self.ps_rr
        self.ps_rr = (self.ps_rr + 1) % 8
        return self.ps[i], ("ps", i)

    def build(self):
        nc = self.nc
        with contextlib.ExitStack() as es:
            self.S = Sched(nc, es)
            S = self.S
            self.ps = [es.enter_context(nc.psum_tensor("psb%d" % i, [128, 512], F32)) for i in range(8)]
            self.ps_rr = 0
            self.ident32 = self.sb(es, "ident32", [128, 128], F32)
            self.identbf = self.sb(es, "identbf", [128, 128], BF16)
            self.zeros = self.sb(es, "zeros", [128, 128], F32)
            self.ones = self.sb(es, "ones", [128, 128], F32)
            self.onesbf = self.sb(es, "onesbf", [128, 128], BF16)
            self.epsc = self.sb(es, "epsc", [128, 4], F32)
            S.op("pool", lambda: nc.gpsimd.memset(self.zeros[:], 0.0), w=["zeros"])
            S.op("pool", lambda: nc.gpsimd.memset(self.ones[:], 1.0), w=["ones"])
            S.op("pool", lambda: nc.gpsimd.memset(self.onesbf[:], 1.0), w=["onesbf"])
            S.op("pool", lambda: nc.gpsimd.memset(self.epsc[:, 0:1], LN_EPS), w=["epsc"])
            S.op("pool", lambda: nc.gpsimd.memset(self.epsc[:, 1:2], RMS_EPS), w=["epsc"])
            S.op("pool", lambda: nc.gpsimd.memset(self.epsc[:, 2:3], GN_EPS), w=["epsc"])
            S.op("pool", lambda: nc.gpsimd.memset(self.epsc[:, 3:4], 1.0), w=["epsc"])
            S.op("pool", lambda: nc.gpsimd.affine_select(
                out=self.ident32[:], in_=self.zeros[:], pattern=[[1, 128]], compare_op=ALU.not_equal,
                fill=1.0, base=0, channel_multiplier=-1), r=["zeros"], w=["ident32"])
            S.op("pool", lambda: nc.gpsimd.tensor_copy(out=self.identbf[:], in_=self.ident32[:]),
                 r=["ident32"], w=["identbf"])
            self.hT = self.sb(es, "hT", [128, KT, T], BF16)
            self.gates = self.sb(es, "gates", [128, NT, NE], F32)
            self.logits = self.sb(es, "logits", [128, NT, NE], F32)
            self.rw32 = self.sb(es, "rw32", [128, KT, NE], F32)
            self.rbias = self.sb(es, "rbias", [128, NE], F32)
            S.dma("sp", self.rw32[:], self.P["router_w"].rearrange("(kt p) e -> p kt e", p=128), w=["rw32"])
            S.dma("sp", self.rbias[:], self.P["router_bias"].partition_broadcast(128), w=["rbias"])

            with contextlib.ExitStack() as st:
                gbc = self.sb(st, "gbc", [128, D], F32)
                bbc = self.sb(st, "bbc", [128, D], F32)
                S.dma("sp", gbc[:], self.P["ln_in_g"].partition_broadcast(128), w=["gbc"])
                S.dma("sp", bbc[:], self.P["ln_in_b"].partition_broadcast(128), w=["bbc"])
                lnw = self.ln_alloc(st)
                for tt in range(NT):
                    xin, kx = self.ln_xin(lnw, tt)
                    S.dma("sp", xin[:], self.x[tt * 128:(tt + 1) * 128, :], w=[kx])
                    self.ln_tile(lnw, tt, gbc, bbc, self.h_dram, router=self.pre_router, extra=self.dbg_out.get("h0"))
                S.barrier()
            self.dbg_dump("hT", lambda o: S.dma("sp", o, self.hT[:], r=[("hT", t) for t in range(NT)]))
            self.dbg_dump("logits", lambda o: S.dma("sp", o, self.logits[:], r=["logits"]))

            for l in range(self.depth):
                self.layer(l)
            S.finish()
        return nc

    def dbg_dump(self, name, fn):
        if name in self.dbg_out:
            fn(self.dbg_out[name])

    def ln_alloc(self, st):
        w = {}
        w["xin"] = [self.sb(st, "xin", [128, D], F32) for _ in range(2)]
        w["hh"] = [self.sb(st, "hh", [128, D], F32) for _ in range(2)]
        w["bst"] = [self.sb(st, "bst", [128, 2, 6], F32) for _ in range(2)]
        w["mv"] = [self.sb(st, "mv", [128, 4], F32) for _ in range(2)]
        w["h32"] = [self.sb(st, "h32", [128, KT, 128], F32) for _ in range(2)]
        w["id"] = self.uid
        return w

    def ln_xin(self, w, tt):
        return w["xin"][tt % 2], ("xin", w["id"], tt % 2)

    def ln_tile(self, w, tt, gbc, bbc, dst_dram, router, extra=None):
        nc, S = self.nc, self.S
        s = tt % 2
        wid = w["id"]
        xin, kx = w["xin"][s], ("xin", wid, s)
        hh, kh = w["hh"][s], ("hh", wid, s)
        bst, kb = w["bst"][s], ("bst", wid, s)
        mv, km = w["mv"][s], ("mv", wid, s)
        h32, k32 = w["h32"][s], ("h32", wid, s)
        for c in range(2):
            S.op("dve", lambda c=c: nc.vector.bn_stats(out=bst[:, c, :], in_=xin[:, c * 512:(c + 1) * 512]),
                 r=[kx], w=[kb])
        S.op("dve", lambda: nc.vector.bn_aggr(out=mv[:, 0:2], in_=bst[:].rearrange("p a b -> p (a b)")),
             r=[kb], w=[km])
        if CUT < 2:
            return
        S.op("act", lambda: nc.scalar.activation(out=mv[:, 2:3], in_=mv[:, 1:2], func=AF.Sqrt,
                                                 bias=self.epsc[:, 0:1], scale=1.0), r=[km, "epsc"], w=[km])
        S.op("dve", lambda: nc.vector.reciprocal(out=mv[:, 3:4], in_=mv[:, 2:3]), r=[km], w=[km])
        S.op("dve", lambda: nc.vector.tensor_scalar(out=xin[:], in0=xin[:], scalar1=mv[:, 0:1], scalar2=mv[:, 3:4],
                                                    op0=ALU.subtract, op1=ALU.mult), r=[kx, km], w=[kx])
        if CUT < 3:
            return
        S.op("pool", lambda: nc.gpsimd.tensor_tensor(out=hh[:], in0=xin[:], in1=gbc[:], op=ALU.mult),
             r=[kx, "gbc"], w=[kh])
        S.op("pool", lambda: nc.gpsimd.tensor_tensor(out=hh[:], in0=hh[:], in1=bbc[:], op=ALU.add),
             r=[kh, "bbc"], w=[kh])
        S.dma("sp", dst_dram[tt * 128:(tt + 1) * 128, :], hh[:], r=[kh], w=[("hd", tt)])
        if extra is not None:
            S.dma("sp", extra[tt * 128:(tt + 1) * 128, :], hh[:], r=[kh], w=[("hdx", tt)])
        if CUT < 4:
            return
        for half in range(2):
            pb, kp = self.psum()
            for j in range(4):
                kt = half * 4 + j
                S.op("pe", lambda j=j, kt=kt: nc.tensor.transpose(
                    out=pb[:, j * 128:(j + 1) * 128], in_=hh[:, kt * 128:(kt + 1) * 128], identity=self.ident32[:]),
                    r=[kh, "ident32"], w=[kp])
            if os.environ.get("EVAC", "act") == "act":
                S.op("act", lambda half=half, pb=pb: nc.scalar.activation(
                    out=self.hT[:, half * 4:(half + 1) * 4, tt * 128:(tt + 1) * 128],
                    in_=pb[:].rearrange("p (a b) -> p a b", a=4), func=AF.Copy), r=[kp], w=[("hT", tt), kp])
            else:
                S.op("dve", lambda half=half, pb=pb: nc.vector.tensor_copy(
                    out=self.hT[:, half * 4:(half + 1) * 4, tt * 128:(tt + 1) * 128],
                    in_=pb[:].rearrange("p (a b) -> p a b", a=4)), r=[kp], w=[("hT", tt), kp])
            if router:
                S.op("dve", lambda half=half, pb=pb: nc.vector.tensor_copy(
                    out=h32[:, half * 4:(half + 1) * 4, :], in_=pb[:].rearrange("p (a b) -> p a b", a=4)),
                    r=[kp], w=[k32, kp])
        if router and CUT >= 5:
            pb, kp = self.psum()
            for kt in range(KT):
                S.op("pe", lambda kt=kt: nc.tensor.matmul(pb[:, 0:NE], lhsT=h32[:, kt, :], rhs=self.rw32[:, kt, :],
                                                          start=(kt == 0), stop=(kt == KT - 1)),
                     r=[k32, "rw32"], w=[kp])
            S.op("dve", lambda: nc.vector.tensor_copy(out=self.logits[:, tt, :], in_=pb[:, 0:NE]),
                 r=[kp], w=["logits"])

    def router(self, st):
        nc, S = self.nc, self.S
        V = nc.vector
        L = self.logits
        t1 = self.sb(st, "rt1", [128, NT, NE], F32)
        probs = self.sb(st, "probs", [128, NT, NE], F32)
        sel = self.sb(st, "sel", [128, NT, NE], F32)
        p6 = self.sb(st, "p6", [128, NT, 4, 6], F32)
        gs = self.sb(st, "gs", [128, NT, 4], F32)
        gm = self.sb(st, "gm", [128, NT, 4], F32)
        gt = self.sb(st, "gt", [128, NT, 4], F32)
        red = self.sb(st, "red", [128, NT], F32)
        red2 = self.sb(st, "red2", [128, NT], F32)
        msk = self.sb(st, "msk", [128, NT, NE], F32)
        eq = self.sb(st, "eq", [128, NT, NE], F32)
        BIG = 1.0e9

        def bc(a):
            return a[:].unsqueeze(2).to_broadcast([128, NT, NE])

        S.op("dve", lambda: V.tensor_reduce(out=red[:], in_=L[:], axis=AX.X, op=ALU.max), r=["logits"], w=["red"])
        S.op("dve", lambda: V.tensor_tensor(out=t1[:], in0=L[:], in1=bc(red), op=ALU.subtract),
             r=["logits", "red"], w=["rt1"])
        S.op("act", lambda: nc.scalar.activation(out=t1[:], in_=t1[:], func=AF.Exp), r=["rt1"], w=["rt1"])
        S.op("dve", lambda: V.tensor_reduce(out=red[:], in_=t1[:], axis=AX.X, op=ALU.add), r=["rt1"], w=["red"])
        S.op("dve", lambda: V.reciprocal(out=red[:], in_=red[:]), r=["red"], w=["red"])
        S.op("dve", lambda: V.tensor_tensor(out=probs[:], in0=t1[:], in1=bc(red), op=ALU.mult),
             r=["rt1", "red"], w=["probs"])
        S.op("dve", lambda: V.tensor_tensor(out=sel[:], in0=probs[:],
                                            in1=self.rbias[:].unsqueeze(1).to_broadcast([128, NT, NE]), op=ALU.add),
             r=["probs", "rbias"], w=["sel"])
        s4 = sel[:].rearrange("p t (g e) -> p t g e", g=4)
        S.op("dve", lambda: V.tensor_tensor(out=p6[:, :, :, 0:3], in0=s4[:, :, :, 0:3], in1=s4[:, :, :, 1:4],
                                            op=ALU.add), r=["sel"], w=["p6"])
        S.op("dve", lambda: V.tensor_tensor(out=p6[:, :, :, 3:5], in0=s4[:, :, :, 0:2], in1=s4[:, :, :, 2:4],
                                            op=ALU.add), r=["sel"], w=["p6"])
        S.op("dve", lambda: V.tensor_tensor(out=p6[:, :, :, 5:6], in0=s4[:, :, :, 0:1], in1=s4[:, :, :, 3:4],
                                            op=ALU.add), r=["sel"], w=["p6"])
        S.op("dve", lambda: V.tensor_reduce(out=gs[:], in_=p6[:], axis=AX.X, op=ALU.max), r=["p6"], w=["gs"])
        S.op("dve", lambda: V.tensor_reduce(out=red[:], in_=gs[:], axis=AX.X, op=ALU.max), r=["gs"], w=["red"])
        S.op("dve", lambda: V.tensor_tensor(out=gm[:], in0=gs[:], in1=red[:].unsqueeze(2).to_broadcast([128, NT, 4]),
                                            op=ALU.is_ge), r=["gs", "red"], w=["gm"])
        S.op("dve", lambda: V.tensor_scalar(out=gt[:], in0=gm[:], scalar1=BIG, scalar2=-BIG, op0=ALU.mult,
                                            op1=ALU.add), r=["gm"], w=["gt"])
        m4 = msk[:].rearrange("p t (g e) -> p t g e", g=4)
        S.op("dve", lambda: V.tensor_tensor(out=m4, in0=s4, in1=gm[:].unsqueeze(3).to_broadcast([128, NT, 4, 4]),
                                            op=ALU.mult), r=["sel", "gm"], w=["msk"])
        S.op("dve", lambda: V.tensor_tensor(out=m4, in0=m4, in1=gt[:].unsqueeze(3).to_broadcast([128, NT, 4, 4]),
                                            op=ALU.add), r=["msk", "gt"], w=["msk"])
        S.op("dve", lambda: V.tensor_reduce(out=red[:], in_=msk[:], axis=AX.X, op=ALU.max), r=["msk"], w=["red"])
        S.op("dve", lambda: V.tensor_tensor(out=eq[:], in0=msk[:], in1=bc(red), op=ALU.is_equal),
             r=["msk", "red"], w=["eq"])
        S.op("dve", lambda: V.scalar_tensor_tensor(out=eq[:], in0=eq[:], scalar=-BIG, in1=msk[:], op0=ALU.mult,
                                                   op1=ALU.add), r=["eq", "msk"], w=["eq"])
        S.op("dve", lambda: V.tensor_reduce(out=red2[:], in_=eq[:], axis=AX.X, op=ALU.max), r=["eq"], w=["red2"])
        S.op("dve", lambda: V.tensor_tensor(out=eq[:], in0=msk[:], in1=bc(red2), op=ALU.is_ge),
             r=["msk", "red2"], w=["eq"])
        S.op("dve", lambda: V.tensor_tensor(out=eq[:], in0=eq[:], in1=probs[:], op=ALU.mult),
             r=["eq", "probs"], w=["eq"])
        S.op("dve", lambda: V.tensor_reduce(out=red[:], in_=eq[:], axis=AX.X, op=ALU.add), r=["eq"], w=["red"])
        S.op("dve", lambda: V.reciprocal(out=red[:], in_=red[:]), r=["red"], w=["red"])
        S.op("dve", lambda: V.tensor_tensor(out=self.gates[:], in0=eq[:], in1=bc(red), op=ALU.mult),
             r=["eq", "red"], w=["gates"])

    def moe(self, l, last):
        nc, S = self.nc, self.S
        wg_d, wu_d, wd_d = self.P["exp_w_gate"], self.P["exp_w_up"], self.P["exp_w_down"]
        with contextlib.ExitStack() as st:
            self.router(st)
            self.dbg_dump("gates%d" % l, lambda o: S.dma("sp", o, self.gates[:], r=["gates"]))
            acc = self.sb(st, "acc", [128, NT, D], F32)
            wg = [self.sb(st, "wg", [128, KT, DEXP], BF16) for _ in range(2)]
            wu = [self.sb(st, "wu", [128, KT, DEXP], BF16) for _ in range(2)]
            wd = [self.sb(st, "wd", [128, 4, D], BF16) for _ in range(2)]
            hg = [self.sb(st, "hg", [128, 4, 512], BF16) for _ in range(2)]
            sg = [self.sb(st, "sg", [128, 512], BF16) for _ in range(2)]
            gbc = self.sb(st, "gbc2", [128, D], F32)
            bbc = self.sb(st, "bbc2", [128, D], F32)
            S.dma("sp", gbc[:], self.P["ln2_g"][l].partition_broadcast(128), w=["gbc"])
            S.dma("sp", bbc[:], self.P["ln2_b"][l].partition_broadcast(128), w=["bbc"])

            def load_w(e):
                b = e % 2
                S.dma("pool", wg[b][:], wg_d[l, e].rearrange("(kt p) n -> p kt n", p=128), w=[("wg", b)])
                S.dma("pool", wu[b][:], wu_d[l, e].rearrange("(kt p) n -> p kt n", p=128), w=[("wu", b)])
                S.dma("pool", wd[b][:], wd_d[l, e].rearrange("(kt p) n -> p kt n", p=128), w=[("wd", b)])

            items = [(e, q) for e in range(int(os.environ.get('ME', NE))) for q in range(4)]
            sgi = [0]

            def G(i):
                e, q = items[i]
                b = e % 2
                hb = i % 2
                for dt_ in range(4):
                    pa, ka = self.psum()
                    pu, ku = self.psum()
                    for kt in range(KT):
                        S.op("pe", lambda kt=kt, pa=pa: nc.tensor.matmul(
                            pa[:], lhsT=wg[b][:, kt, dt_ * 128:(dt_ + 1) * 128], rhs=self.hT[:, kt, q * 512:(q + 1) * 512],
                            start=(kt == 0), stop=(kt == KT - 1)),
                            r=[("wg", b)] + [("hT", q * 4 + j) for j in range(4)], w=[ka])
                    for kt in range(KT):
                        S.op("pe", lambda kt=kt, pu=pu: nc.tensor.matmul(
                            pu[:], lhsT=wu[b][:, kt, dt_ * 128:(dt_ + 1) * 128], rhs=self.hT[:, kt, q * 512:(q + 1) * 512],
                            start=(kt == 0), stop=(kt == KT - 1)),
                            r=[("wu", b)] + [("hT", q * 4 + j) for j in range(4)], w=[ku])
                    si = sgi[0] % 2
                    sgi[0] += 1
                    S.op("act", lambda pa=pa, si=si: nc.scalar.activation(out=sg[si][:], in_=pa[:], func=AF.Silu),
                         r=[ka], w=[("sg", si)])
                    S.op("dve", lambda pu=pu, si=si: nc.vector.tensor_tensor(
                        out=hg[hb][:, dt_, :], in0=pu[:], in1=sg[si][:], op=ALU.mult),
                        r=[ku, ("sg", si)], w=[("hg", hb)])

            def Dn(i):
                e, q = items[i]
                b = e % 2
                hb = i % 2
                for j in range(4):
                    tt = q * 4 + j
                    for half in range(2):
                        pc, kc = self.psum()
                        for dt_ in range(4):
                            S.op("pe", lambda dt_=dt_, pc=pc: nc.tensor.matmul(
                                pc[:], lhsT=hg[hb][:, dt_, j * 128:(j + 1) * 128],
                                rhs=wd[b][:, dt_, half * 512:(half + 1) * 512], start=(dt_ == 0), stop=(dt_ == 3)),
                                r=[("hg", hb), ("wd", b)], w=[kc])
                        dst = acc[:, tt, half * 512:(half + 1) * 512]
                        if e == 0:
                            S.op("dve", lambda pc=pc, dst=dst: nc.vector.tensor_scalar(
                                out=dst, in0=pc[:], scalar1=self.gates[:, tt, e:e + 1], scalar2=None, op0=ALU.mult),
                                r=[kc, "gates"], w=[("acc", tt)])
                        else:
                            S.op("dve", lambda pc=pc, dst=dst: nc.vector.scalar_tensor_tensor(
                                out=dst, in0=pc[:], scalar=self.gates[:, tt, e:e + 1], in1=dst, op0=ALU.mult,
                                op1=ALU.add), r=[kc, "gates", ("acc", tt)], w=[("acc", tt)])

            load_w(0)
            for i in range(len(items)):
                e, q = items[i]
                G(i)
                if i >= 1:
                    Dn(i - 1)
                if q == 0 and e + 1 < int(os.environ.get('ME', NE)):
                    load_w(e + 1)
            Dn(len(items) - 1)
            self.dbg_dump("moe%d" % l, lambda o: S.dma("sp", o.rearrange("(t p) d -> p t d", p=128), acc[:],
                                                       r=[("acc", t) for t in range(NT)]))
            lnw = self.ln_alloc(st)
            h1 = [self.sb(st, "h1t", [128, D], F32) for _ in range(2)]
            dst = self.out if last else self.h_dram
            for tt in range(NT):
                s = tt % 2
                S.dma("sp", h1[s][:], self.h_dram[tt * 128:(tt + 1) * 128, :], r=[("hd", tt)], w=[("h1t", s)])
                xin, kx = self.ln_xin(lnw, tt)
                S.op("dve", lambda s=s, xin=xin: nc.vector.scalar_tensor_tensor(
                    out=xin[:], in0=h1[s][:], scalar=ALPHA, in1=acc[:, tt, :], op0=ALU.mult, op1=ALU.add),
                    r=[("h1t", s), ("acc", tt)], w=[kx])
                self.ln_tile(lnw, tt, gbc, bbc, dst, router=False)
            S.barrier()


    def hgrn(self, l):
        nc, S = self.nc, self.S
        V, A, G, PE = nc.vector, nc.scalar, nc.gpsimd, nc.tensor
        Wl = self.P["w_in"][l]
        ydst = self.yT_dram[2]
        with contextlib.ExitStack() as st:
            mask2 = self.sb(st, "mask2", [128, 128], F32)
            rm = self.sb(st, "rm", [128, T], F32)
            nw = self.sb(st, "nw", [128, 1], F32)
            lbt = self.sb(st, "lbt", [128, 8, 2], F32)
            lbv = self.sb(st, "lbv", [128, 8], F32)
            oml = self.sb(st, "oml", [128, 8], F32)
            S.op("pool", lambda: G.affine_select(out=mask2[:], in_=self.ones[:], pattern=[[1, 128]],
                                                 compare_op=ALU.is_ge, fill=0.0, base=0, channel_multiplier=-1),
                 r=["ones"], w=["mask2"])
            S.op("pool", lambda: G.memset(mask2[0:64, 64:128], 0.0), w=["mask2"])
            S.op("pool", lambda: G.memset(rm[:], 1.0), w=["rm"])
            S.op("pool", lambda: G.memset(rm[:].rearrange("p (c j) -> p c j", j=64)[:, :, 0:1], 0.0), w=["rm"])
            S.dma("sp", nw[:], self.P["hgrn_norm_w"][l].rearrange("(p o) -> p o", o=1), w=["nw"])
            if l == 0:
                S.op("pool", lambda: G.memset(lbv[:], 0.0), w=["lbv"])
                S.op("pool", lambda: G.memset(oml[:], 1.0), w=["oml"])
            else:
                for j in range(2):
                    S.dma("sp", lbt[:, :, j:j + 1],
                          self.P["hgrn_lb"][j].rearrange("(h p o) -> p h o", p=128, o=1), w=["lbt"])
                S.op("dve", lambda: V.tensor_tensor(out=lbv[:], in0=lbt[:, :, 1], in1=lbt[:, :, 0], op=ALU.subtract),
                     r=["lbt"], w=["lbv"])
                S.op("act", lambda: A.activation(out=lbv[:], in_=lbv[:], func=AF.Sigmoid), r=["lbv"], w=["lbv"])
                S.op("dve", lambda: V.tensor_scalar(out=oml[:], in0=lbv[:], scalar1=-1.0, scalar2=1.0, op0=ALU.mult,
                                                    op1=ALU.add), r=["lbv"], w=["oml"])
            w4 = [self.sb(st, "w4", [128, 4, KT, 128], BF16) for _ in range(2)]
            qs = self.sb(st, "qs", [128, T], F32)
            fs = self.sb(st, "fs", [128, T], F32)
            lf = self.sb(st, "lf", [128, T], F32)
            bc = self.sb(st, "bc", [128, T], F32)
            eb = self.sb(st, "eb", [128, T], F32)
            enb = self.sb(st, "enb", [128, T], F32)
            qb = self.sb(st, "qb", [128, T], BF16)
            kb = self.sb(st, "kb", [128, T], BF16)
            gs = self.sb(st, "gs", [128, T], BF16)
            yt = self.sb(st, "yt", [128, T], BF16)
            v = self.sb(st, "v", [128, NT, 128], BF16)
            kbt = self.sb(st, "kbt", [128, NT, 128], BF16)
            kbtB = self.sb(st, "kbtB", [128, NT, 128], BF16)
            mAB = self.sb(st, "mAB", [128, 2], F32)
            S.op("pool", lambda: G.memset(mAB[0:64, 0:1], 1.0), w=["mAB"])
            S.op("pool", lambda: G.memset(mAB[64:128, 0:1], 0.0), w=["mAB"])
            S.op("pool", lambda: G.memset(mAB[0:64, 1:2], 0.0), w=["mAB"])
            S.op("pool", lambda: G.memset(mAB[64:128, 1:2], 1.0), w=["mAB"])
            S32 = self.sb(st, "S32", [128, 128], F32)
            Sbf = [self.sb(st, "Sbf", [128, 128], BF16) for _ in range(4)]
            attm = [self.sb(st, "attm", [128, 128], BF16) for _ in range(2)]
            osb = [self.sb(st, "osb", [128, 128], F32) for _ in range(2)]
            osq = [self.sb(st, "osq", [128, 128], BF16) for _ in range(2)]
            sd = [self.sb(st, "sd", [128, 128], F32) for _ in range(2)]
            hTk = [("hT", t) for t in range(NT)]

            def load_w(h):
                b = h % 2
                for j in range(4):
                    c0 = OFF_HGRN + j * 1024 + h * 128
                    S.dma("pool", w4[b][:, j], Wl[:, c0:c0 + 128].rearrange("(kt p) n -> p kt n", p=128),
                          w=[("w4", b)])

            if HC >= 2:
                load_w(0)
            for h in range(8 if HC >= 6 else (1 if HC >= 2 else 0)):
                b = h % 2
                if h + 1 < 8 and HC >= 6:
                    load_w(h + 1)
                for (j, func, dst, kd) in ((0, AF.Silu, qs, "qs"), (1, AF.Sigmoid, fs, "fs"), (3, AF.Sigmoid, gs, "gs")):
                    for tq in range(4):
                        pb, kp = self.psum()
                        for kt in range(KT):
                            S.op("pe", lambda kt=kt, pb=pb, j=j, tq=tq: PE.matmul(
                                pb[:], lhsT=w4[b][:, j, kt, :], rhs=self.hT[:, kt, tq * 512:(tq + 1) * 512],
                                start=(kt == 0), stop=(kt == KT - 1)), r=[("w4", b)] + hTk[tq * 4:tq * 4 + 4], w=[kp])
                        S.op("act", lambda pb=pb, dst=dst, func=func, tq=tq: A.activation(
                            out=dst[:, tq * 512:(tq + 1) * 512], in_=pb[:], func=func), r=[kp], w=[kd, kp])
                for t4 in range(4):
                    pb, kp = self.psum()
                    for j4 in range(4):
                        tt = t4 * 4 + j4
                        for kt in range(KT):
                            S.op("pe", lambda kt=kt, pb=pb, j4=j4, tt=tt: PE.matmul(
                                pb[:, j4 * 128:(j4 + 1) * 128], lhsT=self.hT[:, kt, tt * 128:(tt + 1) * 128],
                                rhs=w4[b][:, 2, kt, :], start=(kt == 0), stop=(kt == KT - 1)),
                                r=[("w4", b), ("hT", tt)], w=[kp])
                    S.op("dve", lambda pb=pb, t4=t4: V.tensor_copy(
                        out=v[:, t4 * 4:(t4 + 1) * 4, :], in_=pb[:].rearrange("p (a b) -> p a b", a=4)),
                        r=[kp], w=["v", kp])
                if HC < 3:
                    continue
                S.op("dve", lambda: V.tensor_scalar(out=fs[:], in0=fs[:], scalar1=oml[:, h:h + 1], scalar2=lbv[:, h:h + 1],
                                                    op0=ALU.mult, op1=ALU.add), r=["fs", "oml", "lbv"], w=["fs"])
                S.op("act", lambda: A.activation(out=lf[:], in_=fs[:], func=AF.Ln), r=["fs"], w=["lf"])
                S.op("dve", lambda: V.tensor_tensor_scan(out=bc[:], data0=rm[:], data1=lf[:], initial=0.0,
                                                         op0=ALU.mult, op1=ALU.add), r=["rm", "lf"], w=["bc"])
                S.op("act", lambda: A.activation(out=eb[:], in_=bc[:], func=AF.Exp), r=["bc"], w=["eb"])
                S.op("act", lambda: A.activation(out=enb[:], in_=bc[:], func=AF.Exp, scale=-1.0), r=["bc"], w=["enb"])
                S.op("pool", lambda: G.tensor_scalar(out=fs[:], in0=fs[:], scalar1=-1.0, scalar2=1.0, op0=ALU.mult,
                                                     op1=ALU.add), r=["fs", "lf"], w=["fs"])
                S.op("pool", lambda: G.tensor_tensor(out=qb[:], in0=qs[:], in1=eb[:], op=ALU.mult),
                     r=["qs", "eb"], w=["qb"])
                S.op("dve", lambda: V.tensor_tensor(out=kb[:], in0=fs[:], in1=enb[:], op=ALU.mult),
                     r=["fs", "enb"], w=["kb"])
                if HC < 4:
                    continue
                for t4 in range(4):
                    pb, kp = self.psum()
                    pbv = pb[:].bitcast(BF16)
                    for j4 in range(4):
                        tt = t4 * 4 + j4
                        S.op("pe", lambda pbv=pbv, j4=j4, tt=tt: PE.transpose(
                            out=pbv[:, j4 * 128:(j4 + 1) * 128], in_=kb[:, tt * 128:(tt + 1) * 128],
                            identity=self.identbf[:]), r=["kb", "identbf"], w=[kp])
                    S.op("dve", lambda pbv=pbv, t4=t4: V.tensor_scalar(
                        out=kbt[:, t4 * 4:(t4 + 1) * 4, :], in0=pbv[:, 0:512].rearrange("p (a b) -> p a b", a=4),
                        scalar1=mAB[:, 0:1], scalar2=None, op0=ALU.mult), r=[kp, "mAB"], w=["kbt", kp])
                    S.op("dve", lambda pbv=pbv, t4=t4: V.tensor_scalar(
                        out=kbtB[:, t4 * 4:(t4 + 1) * 4, :], in0=pbv[:, 0:512].rearrange("p (a b) -> p a b", a=4),
                        scalar1=mAB[:, 1:2], scalar2=None, op0=ALU.mult), r=[kp, "mAB"], w=["kbtB", kp])
                if HC < 5:
                    continue
                S.op("pool", lambda: G.memset(S32[:], 0.0), w=["S32"])
                S.op("pool", lambda: G.memset(Sbf[0][:], 0.0), w=[("Sbf", 0)])
                for tt in range(NT):
                    cA, cB = 2 * tt, 2 * tt + 1
                    tsl = slice(tt * 128, (tt + 1) * 128)
                    i2 = tt % 2
                    pa, kpa = self.psum()
                    S.op("pe", lambda pa=pa: PE.matmul(pa[:, 0:128], lhsT=kb[:, tsl], rhs=qb[:, tsl], start=True, stop=True),
                         r=["kb", "qb"], w=[kpa])
                    S.op("dve", lambda pa=pa: V.tensor_tensor(out=attm[i2][:], in0=pa[:, 0:128], in1=mask2[:], op=ALU.mult),
                         r=[kpa, "mask2"], w=[("attm", i2), kpa])
                    if HL < 2:
                        continue
                    pr, kpr = self.psum()
                    S.op("pe", lambda pr=pr: PE.matmul(pr[:, 0:128], lhsT=kbt[:, tt, :], rhs=v[:, tt, :],
                                                       start=True, stop=True), r=["kbt", "v"], w=[kpr])
                    S.op("pe", lambda pr=pr: PE.matmul(pr[:, 128:256], lhsT=kbtB[:, tt, :], rhs=v[:, tt, :],
                                                       start=True, stop=True), r=["kbtB", "v"], w=[kpr])
                    for (ci, off) in ((cA, 0), (cB, 128)):
                        S.op("dve", lambda pr=pr, off=off: V.tensor_tensor(
                            out=S32[:], in0=pr[:, off:off + 128], in1=S32[:], op=ALU.add), r=[kpr, "S32"], w=["S32", kpr])
                        S.op("dve", lambda ci=ci: V.tensor_scalar(
                            out=S32[:], in0=S32[:], scalar1=eb[:, ci * 64 + 63:ci * 64 + 64], scalar2=None, op0=ALU.mult),
                            r=["S32", "eb"], w=["S32"])
                        S.op("pool", lambda ci=ci: G.tensor_copy(out=Sbf[(ci + 1) % 4][:], in_=S32[:]),
                             r=["S32"], w=[("Sbf", (ci + 1) % 4)])
                    if HL < 3:
                        continue
                    po, kpo = self.psum()
                    S.op("pe", lambda po=po: PE.matmul(po[:, 0:128], lhsT=v[:, tt, :], rhs=attm[i2][:], start=True, stop=False),
                         r=["v", ("attm", i2)], w=[kpo])
                    S.op("pe", lambda po=po: PE.matmul(po[:, 0:64], lhsT=Sbf[cA % 4][:], rhs=qb[:, tt * 128:tt * 128 + 64],
                                                       start=False, stop=False), r=[("Sbf", cA % 4), "qb"], w=[kpo])
                    S.op("pe", lambda po=po: PE.matmul(po[:, 64:128], lhsT=Sbf[cB % 4][:],
                                                       rhs=qb[:, tt * 128 + 64:(tt + 1) * 128], start=False, stop=True),
                         r=[("Sbf", cB % 4), "qb"], w=[kpo])
                    if HL < 4:
                        continue
                    S.op("act", lambda po=po: A.activation(out=osb[i2][:], in_=po[:, 0:128], func=AF.Copy),
                         r=[kpo], w=[("osb", i2), kpo])
                    S.op("act", lambda po=po: A.activation(out=osq[i2][:], in_=po[:, 0:128], func=AF.Square),
                         r=[kpo], w=[("osq", i2), kpo])
                    if HL < 5:
                        continue
                    pss, kps = self.psum()
                    S.op("pe", lambda pss=pss: PE.matmul(pss[:, 0:128], lhsT=self.onesbf[:], rhs=osq[i2][:], start=True,
                                                         stop=True), r=["onesbf", ("osq", i2)], w=[kps])
                    S.op("act", lambda pss=pss: A.activation(out=sd[i2][:], in_=pss[:, 0:128], func=AF.Sqrt,
                                                             bias=self.epsc[:, 1:2], scale=1.0 / 128.0),
                         r=[kps, "epsc"], w=[("sd", i2), kps])
                    S.op("dve", lambda: V.reciprocal(out=sd[i2][:], in_=sd[i2][:]), r=[("sd", i2)], w=[("sd", i2)])
                    S.op("dve", lambda: V.scalar_tensor_tensor(out=osb[i2][:], in0=osb[i2][:], scalar=nw[:, 0:1],
                                                               in1=sd[i2][:], op0=ALU.mult, op1=ALU.mult),
                         r=[("osb", i2), ("sd", i2), "nw"], w=[("osb", i2)])
                    S.op("dve", lambda: V.tensor_tensor(out=yt[:, tsl], in0=osb[i2][:], in1=gs[:, tsl], op=ALU.mult),
                         r=[("osb", i2), "gs"], w=["yt"])
                S.dma("sp", ydst[h * 128:(h + 1) * 128, :], yt[:], r=["yt"], w=[("yT2", h)])
            S.barrier()


    def ssd(self, l):
        nc, S = self.nc, self.S
        V, A, G, PE = nc.vector, nc.scalar, nc.gpsimd, nc.tensor
        Wl = self.P["w_in"][l]
        ydst = self.yT_dram[0]
        NEG = -30000.0
        hTk = [("hT", t) for t in range(NT)]
        with contextlib.ExitStack() as st:
            tri2 = self.sb(st, "tri2", [128, 128], F32)
            same2 = self.sb(st, "same2", [128, 128], F32)
            indA = self.sb(st, "indA", [128, 128], F32)
            indB = self.sb(st, "indB", [128, 128], F32)
            mAB = self.sb(st, "mABs", [128, 2], F32)
            negmask = self.sb(st, "negmask", [128, 8, 128], F32)
            bd1 = self.sb(st, "bd", [16, 8, 128], F32)
            bd = [bd1, bd1]
            S.op("pool", lambda: G.affine_select(out=tri2[:], in_=self.ones[:], pattern=[[1, 128]], compare_op=ALU.is_ge,
                                                 fill=0.0, base=0, channel_multiplier=-1), r=["ones"], w=["tri2"])
            S.op("pool", lambda: G.memset(tri2[0:64, 64:128], 0.0), w=["tri2"])
            S.op("pool", lambda: G.memset(same2[:], 0.0), w=["same2"])
            S.op("pool", lambda: G.memset(same2[0:64, 0:64], 1.0), w=["same2"])
            S.op("pool", lambda: G.memset(same2[64:128, 64:128], 1.0), w=["same2"])
            S.op("pool", lambda: G.memset(indA[0:64, :], 1.0), w=["indA"])
            S.op("pool", lambda: G.memset(indA[64:128, :], 0.0), w=["indA"])
            S.op("pool", lambda: G.memset(indB[0:64, :], 0.0), w=["indB"])
            S.op("pool", lambda: G.memset(indB[64:128, :], 1.0), w=["indB"])
            S.op("pool", lambda: G.memset(mAB[0:64, 0:1], 1.0), w=["mAB"])
            S.op("pool", lambda: G.memset(mAB[64:128, 0:1], 0.0), w=["mAB"])
            S.op("pool", lambda: G.memset(mAB[0:64, 1:2], 0.0), w=["mAB"])
            S.op("pool", lambda: G.memset(mAB[64:128, 1:2], 1.0), w=["mAB"])
            S.op("pool", lambda: G.memset(negmask[:], 0.0), w=["negmask"])
            S.op("pool", lambda: G.affine_select(out=negmask[:], in_=negmask[:], pattern=[[0, 8], [1, 128]],
                                                 compare_op=ALU.is_ge, fill=NEG, base=0, channel_multiplier=-1),
                 r=["negmask"], w=["negmask"])
            S.op("pool", lambda: G.memset(negmask[0:64, :, 64:128], NEG), w=["negmask"])
            dtb = self.sb(st, "dtb", [128, 16], F32)
            alog = self.sb(st, "alog", [128, 16], F32)
            dsk = self.sb(st, "dsk", [128, 16], F32)
            nwbc = self.sb(st, "nwbc", [128, D], F32)
            S.dma("sp", dtb[:], self.P["ssd_dt_bias"][l].partition_broadcast(128), w=["dtb"])
            S.dma("sp", alog[:], self.P["ssd_a_log"][l].partition_broadcast(128), w=["alog"])
            S.dma("sp", dsk[:], self.P["ssd_d"][l].partition_broadcast(128), w=["dsk"])
            S.dma("sp", nwbc[:], self.P["ssd_norm_w"][l].partition_broadcast(128), w=["nwbc"])
            S.op("act", lambda: A.activation(out=alog[:], in_=alog[:], func=AF.Exp), r=["alog"], w=["alog"])
            S.op("dve", lambda: V.tensor_scalar(out=alog[:], in0=alog[:], scalar1=-1.0, scalar2=None, op0=ALU.mult),
                 r=["alog"], w=["alog"])
            wdt = self.sb(st, "wdt", [128, KT, 16], BF16)
            S.dma("pool", wdt[:], Wl[:, OFF_DT:OFF_DT + 16].rearrange("(kt p) n -> p kt n", p=128), w=["wdt"])
            dt = self.sb(st, "dt", [128, NT, 16], F32)
            da = self.sb(st, "da", [128, NT, 16], F32)
            cum4 = self.sb(st, "cum4", [128, NT, 4, 16], F32)
            eacs = self.sb(st, "eacs", [128, NT, 16], F32)
            eend = self.sb(st, "eend", [128, NT, 16], F32)
            edec = self.sb(st, "edec", [128, NT, 2, 16], F32)
            acsTt = [self.sb(st, "acsTt", [16, 128], F32) for _ in range(2)]
            nacsTt = [self.sb(st, "nacsTt", [16, 128], F32) for _ in range(2)]
            pb, kp = self.psum()
            for tt in range(NT):
                for kt in range(KT):
                    S.op("pe", lambda kt=kt, tt=tt: PE.matmul(pb[:, tt * 16:(tt + 1) * 16], lhsT=self.hT[:, kt, tt * 128:(tt + 1) * 128],
                                                              rhs=wdt[:, kt, :], start=(kt == 0), stop=(kt == KT - 1)),
                         r=["wdt", ("hT", tt)], w=[kp])
            S.op("dve", lambda: V.tensor_tensor(out=dt[:], in0=pb[:, 0:256].rearrange("p (t h) -> p t h", h=16),
                                                in1=dtb[:].unsqueeze(1).to_broadcast([128, NT, 16]), op=ALU.add),
                 r=[kp, "dtb"], w=["dt", kp])
            S.op("act", lambda: A.activation(out=dt[:], in_=dt[:], func=AF.Exp), r=["dt"], w=["dt"])
            S.op("act", lambda: A.activation(out=dt[:], in_=dt[:], func=AF.Ln, bias=self.epsc[:, 3:4], scale=1.0),
                 r=["dt", "epsc"], w=["dt"])
            S.op("dve", lambda: V.tensor_tensor(out=da[:], in0=dt[:], in1=alog[:].unsqueeze(1).to_broadcast([128, NT, 16]),
                                                op=ALU.mult), r=["dt", "alog"], w=["da"])
            for half in range(2):
                pb, kp = self.psum()
                for j in range(8):
                    tt = half * 8 + j
                    for qi, L in enumerate((tri2, same2, indA, indB)):
                        S.op("pe", lambda j=j, qi=qi, L=L, tt=tt, pb=pb: PE.matmul(
                            pb[:, j * 64 + qi * 16:j * 64 + (qi + 1) * 16], lhsT=L[:], rhs=da[:, tt, :], start=True, stop=True),
                            r=["da", "tri2", "same2", "indA", "indB"], w=[kp])
                S.op("dve", lambda pb=pb, half=half: V.tensor_copy(
                    out=cum4[:, half * 8:(half + 1) * 8].rearrange("p t q h -> p (t q h)"), in_=pb[:]),
                    r=[kp], w=["cum4", kp])
            S.op("act", lambda: A.activation(out=eacs[:], in_=cum4[:, :, 0, :], func=AF.Exp), r=["cum4"], w=["eacs"])
            S.op("dve", lambda: V.tensor_tensor(out=eend[:], in0=cum4[:, :, 1, :], in1=cum4[:, :, 0, :], op=ALU.subtract),
                 r=["cum4"], w=["eend"])
            S.op("act", lambda: A.activation(out=eend[:], in_=eend[:], func=AF.Exp), r=["eend"], w=["eend"])
            S.op("act", lambda: A.activation(out=edec[:], in_=cum4[:, :, 2:4, :], func=AF.Exp), r=["cum4"], w=["edec"])
            wx = self.sb(st, "wx", [128, KT, 768], BF16)
            wz = self.sb(st, "wz", [128, KT, 512], BF16)
            cw = self.sb(st, "cw", [128, 6, 4], F32)
            cbi = self.sb(st, "cbi", [128, 6], F32)
            xp1 = self.sb(st, "xp", [128, T + 3], F32)
            xp = [xp1, xp1]
            fTa = [self.sb(st, "fT", [128, T], BF16) for _ in range(4)]
            fT = [fTa[0], fTa[1], fTa[0], fTa[1], fTa[2], fTa[3]]
            fk = [("fT", 0), ("fT", 1), ("fT", 0), ("fT", 1), ("fT", 2), ("fT", 3)]
            cmTA = self.sb(st, "cmTA", [128, T], BF16)
            cmTB = self.sb(st, "cmTB", [128, T], BF16)
            xs = self.sb(st, "xs", [128, NT, 512], BF16)
            xdtt = [self.sb(st, "xdtt", [128, 512], BF16) for _ in range(2)]
            xendt = [self.sb(st, "xendt", [128, 512], BF16) for _ in range(2)]
            bmA = self.sb(st, "bmA", [128, NT, 128], BF16)
            bmB = self.sb(st, "bmB", [128, NT, 128], BF16)
            yTg = self.sb(st, "yTg", [128, 4, T], BF16)
            S32 = self.sb(st, "S32s", [128, 512], F32)
            Sbf = [self.sb(st, "Sbfs", [128, 512], BF16) for _ in range(4)]
            cbs = [self.sb(st, "cbs", [128, 128], BF16) for _ in range(2)]
            Dx = [self.sb(st, "Dx", [16, 8, 128], F32) for _ in range(2)]
            Es = [self.sb(st, "Es", [128, 8, 128], BF16) for _ in range(2)]
            wT = [self.sb(st, "wT", [128, 8, 128], BF16) for _ in range(2)]
            t1 = [self.sb(st, "t1", [128, 512], F32) for _ in range(2)]
            t2 = [self.sb(st, "t2", [128, 512], F32) for _ in range(2)]
            zs = [self.sb(st, "zs", [128, 512], BF16) for _ in range(2)]
            ytm = [self.sb(st, "ytm", [128, 512], BF16) for _ in range(2)]
            ss = [self.sb(st, "ss", [128, 2], F32) for _ in range(2)]
            S.op("pool", lambda: G.memset(xp[0][:, 0:3], 0.0), w=[("xp", 0)])
            for g in range(2):
                S.op("pool", lambda g=g: G.memset(bd[g][:], 1.0), r=[("bd", 0), ("bd", 1)], w=[("bd", 0), ("bd", 1)])
                S.op("pool", lambda g=g: G.affine_select(out=bd[g][:], in_=bd[g][:], pattern=[[1, 8], [0, 128]],
                                                         compare_op=ALU.is_equal, fill=0.0, base=8 * g,
                                                         channel_multiplier=-1), r=[("bd", 0), ("bd", 1)], w=[("bd", 0), ("bd", 1)])
                choff = [g * 512 + i * 128 for i in range(4)] + [1024 + g * 128, 1280 + g * 128]
                S.dma("pool", wx[:, :, 0:512], Wl[:, OFF_XBC + g * 512:OFF_XBC + (g + 1) * 512].rearrange("(kt p) n -> p kt n", p=128), w=["wx"])
                S.dma("pool", wx[:, :, 512:640], Wl[:, OFF_XBC + 1024 + g * 128:OFF_XBC + 1024 + (g + 1) * 128].rearrange("(kt p) n -> p kt n", p=128), w=["wx"])
                S.dma("pool", wx[:, :, 640:768], Wl[:, OFF_XBC + 1280 + g * 128:OFF_XBC + 1280 + (g + 1) * 128].rearrange("(kt p) n -> p kt n", p=128), w=["wx"])
                S.dma("pool", wz[:], Wl[:, OFF_Z + g * 512:OFF_Z + (g + 1) * 512].rearrange("(kt p) n -> p kt n", p=128), w=["wz"])
                for ci in range(6):
                    for j in range(4):
                        S.dma("sp", cw[:, ci, j:j + 1], self.P["ssd_conv_w"][l, j, choff[ci]:choff[ci] + 128].rearrange("(p o) -> p o", o=1), w=["cw"])
                    S.dma("sp", cbi[:, ci:ci + 1], self.P["ssd_conv_b"][l, choff[ci]:choff[ci] + 128].rearrange("(p o) -> p o", o=1), w=["cbi"])
                for ci in range(6):
                    xb = xp[0]
                    kx = ("xp", 0)
                    for tq in range(4):
                        pb, kp = self.psum()
                        for kt in range(KT):
                            S.op("pe", lambda kt=kt, pb=pb, ci=ci, tq=tq: PE.matmul(
                                pb[:], lhsT=wx[:, kt, ci * 128:(ci + 1) * 128], rhs=self.hT[:, kt, tq * 512:(tq + 1) * 512],
                                start=(kt == 0), stop=(kt == KT - 1)), r=["wx"] + hTk[tq * 4:tq * 4 + 4], w=[kp])
                        S.op("act", lambda pb=pb, xb=xb, tq=tq: A.activation(out=xb[:, 3 + tq * 512:3 + (tq + 1) * 512], in_=pb[:],
                                                                            func=AF.Copy), r=[kp], w=[kx, kp])
                    acc = t1[0] if False else None
                    cacc = self.sb(st, "cacc", [128, T], F32) if (g == 0 and ci == 0) else self._cacc
                    self._cacc = cacc
                    S.op("dve", lambda xb=xb, ci=ci, cacc=cacc: V.tensor_scalar(
                        out=cacc[:], in0=xb[:, 3:3 + T], scalar1=cw[:, ci, 3:4], scalar2=cbi[:, ci:ci + 1], op0=ALU.mult,
                        op1=ALU.add), r=[kx, "cw", "cbi"], w=["cacc"])
                    for j in range(3):
                        S.op("dve", lambda xb=xb, ci=ci, j=j, cacc=cacc: V.scalar_tensor_tensor(
                            out=cacc[:], in0=xb[:, j:j + T], scalar=cw[:, ci, j:j + 1], in1=cacc[:], op0=ALU.mult, op1=ALU.add),
                            r=[kx, "cw", "cacc"], w=["cacc"])
                    S.op("act", lambda ci=ci, cacc=cacc: A.activation(out=fT[ci][:], in_=cacc[:], func=AF.Silu),
                         r=["cacc"], w=[fk[ci]])
                    if ci < 5:
                        for t4 in range(4):
                            pb, kp = self.psum()
                            pbv = pb[:].bitcast(BF16)
                            for j in range(4):
                                tt = t4 * 4 + j
                                S.op("pe", lambda j=j, tt=tt, pbv=pbv, ci=ci: PE.transpose(
                                    out=pbv[:, j * 128:(j + 1) * 128], in_=fT[ci][:, tt * 128:(tt + 1) * 128],
                                    identity=self.identbf[:]), r=[fk[ci], "identbf"], w=[kp])
                            src = pbv[:, 0:512].rearrange("p (a b) -> p a b", a=4)
                            if ci < 4:
                                S.op("act", lambda t4=t4, src=src, ci=ci: A.activation(
                                    out=xs[:, t4 * 4:(t4 + 1) * 4, ci * 128:(ci + 1) * 128], in_=src, func=AF.Copy),
                                    r=[kp], w=["xs", kp])
                            else:
                                S.op("dve", lambda t4=t4, src=src: V.tensor_scalar(
                                    out=bmA[:, t4 * 4:(t4 + 1) * 4, :], in0=src, scalar1=mAB[:, 0:1], scalar2=None, op0=ALU.mult),
                                    r=[kp, "mAB"], w=["bmA", kp])
                                S.op("dve", lambda t4=t4, src=src: V.tensor_scalar(
                                    out=bmB[:, t4 * 4:(t4 + 1) * 4, :], in0=src, scalar1=mAB[:, 1:2], scalar2=None, op0=ALU.mult),
                                    r=[kp, "mAB"], w=["bmB", kp])
                bmT, cmT = fT[4], fT[5]
                cv = cmT[:].rearrange("p (t c j) -> p t c j", c=2, j=64)
                cva = cmTA[:].rearrange("p (t c j) -> p t c j", c=2, j=64)
                cvb = cmTB[:].rearrange("p (t c j) -> p t c j", c=2, j=64)
                S.op("pool", lambda: G.tensor_copy(out=cva[:, :, 0, :], in_=cv[:, :, 0, :]), r=[("fT", 3)], w=["cmTA"])
                S.op("pool", lambda: G.memset(cva[:, :, 1, :], 0.0), w=["cmTA"])
                S.op("pool", lambda: G.tensor_copy(out=cvb[:, :, 1, :], in_=cv[:, :, 1, :]), r=[("fT", 3)], w=["cmTB"])
                S.op("pool", lambda: G.memset(cvb[:, :, 0, :], 0.0), w=["cmTB"])
                hs = slice(g * 8, (g + 1) * 8)
                S.op("pool", lambda: G.memset(S32[:], 0.0), w=["S32"])
                S.op("pool", lambda: G.memset(Sbf[0][:], 0.0), w=[("Sbf", 0)])
                for tt in range(NT):
                    i2 = tt % 2
                    tsl = slice(tt * 128, (tt + 1) * 128)
                    cA, cB = 2 * tt, 2 * tt + 1
                    pc, kpc = self.psum()
                    S.op("pe", lambda pc=pc: PE.matmul(pc[:, 0:128], lhsT=bmT[:, tsl], rhs=cmT[:, tsl], start=True, stop=True),
                         r=[("fT", 2), ("fT", 3)], w=[kpc])
                    S.op("act", lambda pc=pc: A.activation(out=cbs[i2][:], in_=pc[:, 0:128], func=AF.Copy),
                         r=[kpc], w=[("cbs", i2), kpc])
                    pq, kpq = self.psum()
                    S.op("pe", lambda pq=pq: PE.matmul(pq[0:16, 0:128], lhsT=da[:, tt, :], rhs=tri2[:], start=True, stop=True),
                         r=["da", "tri2"], w=[kpq])
                    S.op("dve", lambda pq=pq: V.tensor_copy(out=acsTt[i2][:], in_=pq[0:16, 0:128]), r=[kpq], w=[("acsTt", i2), kpq])
                    S.op("dve", lambda pq=pq: V.tensor_scalar(out=nacsTt[i2][:], in0=pq[0:16, 0:128], scalar1=-1.0, scalar2=None,
                                                              op0=ALU.mult), r=[kpq], w=[("nacsTt", i2), kpq])
                    S.op("pool", lambda: G.tensor_tensor(out=Dx[i2][:], in0=bd[g][:],
                                                         in1=acsTt[i2][:].unsqueeze(1).to_broadcast([16, 8, 128]), op=ALU.mult),
                         r=[("bd", g), ("acsTt", i2)], w=[("Dx", i2)])
                    for hh in range(2):
                        pe_, kpe = self.psum()
                        csl = slice(hh * 512, (hh + 1) * 512)
                        S.op("pe", lambda pe_=pe_, csl=csl: PE.matmul(
                            pe_[:], lhsT=self.ones[0:16, :], rhs=Dx[i2][:].rearrange("p h l -> p (h l)")[:, csl],
                            start=True, stop=False), r=["ones", ("Dx", i2)], w=[kpe])
                        S.op("pe", lambda pe_=pe_, csl=csl: PE.matmul(
                            pe_[:], lhsT=nacsTt[i2][:], rhs=bd[g][:].rearrange("p h l -> p (h l)")[:, csl],
                            start=False, stop=False), r=[("nacsTt", i2), ("bd", g)], w=[kpe])
                        S.op("pe", lambda pe_=pe_, csl=csl: PE.matmul(
                            pe_[:], lhsT=self.ident32[:], rhs=negmask[:].rearrange("p h l -> p (h l)")[:, csl],
                            start=False, stop=True), r=["ident32", "negmask"], w=[kpe])
                        S.op("act", lambda pe_=pe_, hh=hh: A.activation(
                            out=Es[i2][:, hh * 4:(hh + 1) * 4, :], in_=pe_[:].rearrange("p (h l) -> p h l", h=4), func=AF.Exp),
                            r=[kpe], w=[("Es", i2), kpe])
                    S.op("dve", lambda: V.tensor_tensor(out=wT[i2][:], in0=Es[i2][:],
                                                        in1=cbs[i2][:].unsqueeze(1).to_broadcast([128, 8, 128]), op=ALU.mult),
                         r=[("Es", i2), ("cbs", i2)], w=[("wT", i2)])
                    pr, kpr = self.psum()
                    pr2, kpr2 = self.psum()
                    S.op("pool", lambda: G.tensor_tensor(out=xdtt[i2][:].rearrange("p (h c) -> p h c", c=64),
                                                         in0=xs[:, tt, :].rearrange("p (h c) -> p h c", c=64),
                                                         in1=dt[:, tt, hs].unsqueeze(2).to_broadcast([128, 8, 64]), op=ALU.mult),
                         r=["xs", "dt"], w=[("xdtt", i2)])
                    S.op("pool", lambda: G.tensor_tensor(out=xendt[i2][:].rearrange("p (h c) -> p h c", c=64),
                                                         in0=xdtt[i2][:].rearrange("p (h c) -> p h c", c=64),
                                                         in1=eend[:, tt, hs].unsqueeze(2).to_broadcast([128, 8, 64]), op=ALU.mult),
                         r=[("xdtt", i2), "eend"], w=[("xendt", i2)])
                    S.op("pe", lambda pr=pr: PE.matmul(pr[:], lhsT=bmA[:, tt, :], rhs=xendt[i2][:], start=True, stop=True),
                         r=["bmA", ("xendt", i2)], w=[kpr])
                    S.op("pe", lambda pr2=pr2: PE.matmul(pr2[:], lhsT=bmB[:, tt, :], rhs=xendt[i2][:], start=True, stop=True),
                         r=["bmB", ("xendt", i2)], w=[kpr2])
                    for (ci_, prx, kprx, cc) in ((cA, pr, kpr, 0), (cB, pr2, kpr2, 1)):
                        S.op("dve", lambda cc=cc: V.tensor_tensor(
                            out=S32[:].rearrange("p (h c) -> p h c", c=64), in0=S32[:].rearrange("p (h c) -> p h c", c=64),
                            in1=edec[:, tt, cc, hs].unsqueeze(2).to_broadcast([128, 8, 64]), op=ALU.mult),
                            r=["S32", "edec"], w=["S32"])
                        S.op("dve", lambda prx=prx: V.tensor_tensor(out=S32[:], in0=prx[:], in1=S32[:], op=ALU.add),
                             r=[kprx, "S32"], w=["S32", kprx])
                        S.op("pool", lambda ci_=ci_: G.tensor_copy(out=Sbf[(ci_ + 1) % 4][:], in_=S32[:]),
                             r=["S32"], w=[("Sbf", (ci_ + 1) % 4)])
                    py, kpy = self.psum()
                    for hh in range(8):
                        S.op("pe", lambda hh=hh, py=py: PE.matmul(py[:, hh * 64:(hh + 1) * 64], lhsT=wT[i2][:, hh, :],
                                                                  rhs=xdtt[i2][:, hh * 64:(hh + 1) * 64], start=True, stop=True),
                             r=[("wT", i2), ("xdtt", i2)], w=[kpy])
                    po, kpo = self.psum()
                    S.op("pe", lambda po=po: PE.matmul(po[:], lhsT=cmTA[:, tsl], rhs=Sbf[cA % 4][:], start=True, stop=False),
                         r=["cmTA", ("Sbf", cA % 4)], w=[kpo])
                    S.op("pe", lambda po=po: PE.matmul(po[:], lhsT=cmTB[:, tsl], rhs=Sbf[cB % 4][:], start=False, stop=True),
                         r=["cmTB", ("Sbf", cB % 4)], w=[kpo])
                    pz, kpz = self.psum()
                    for kt in range(KT):
                        S.op("pe", lambda kt=kt, pz=pz: PE.matmul(pz[:], lhsT=self.hT[:, kt, tsl], rhs=wz[:, kt, :],
                                                                  start=(kt == 0), stop=(kt == KT - 1)), r=["wz", ("hT", tt)], w=[kpz])
                    S.op("act", lambda pz=pz: A.activation(out=zs[i2][:], in_=pz[:], func=AF.Silu), r=[kpz], w=[("zs", i2), kpz])
                    S.op("dve", lambda po=po: V.tensor_tensor(
                        out=t1[i2][:].rearrange("p (h c) -> p h c", c=64), in0=po[:].rearrange("p (h c) -> p h c", c=64),
                        in1=eacs[:, tt, hs].unsqueeze(2).to_broadcast([128, 8, 64]), op=ALU.mult),
                        r=[kpo, "eacs"], w=[("t1", i2), kpo])
                    S.op("dve", lambda py=py: V.tensor_tensor(out=t1[i2][:], in0=py[:], in1=t1[i2][:], op=ALU.add),
                         r=[kpy, ("t1", i2)], w=[("t1", i2), kpy])
                    S.op("pool", lambda: G.tensor_tensor(
                        out=t2[i2][:].rearrange("p (h c) -> p h c", c=64), in0=xs[:, tt, :].rearrange("p (h c) -> p h c", c=64),
                        in1=dsk[:, hs].unsqueeze(2).to_broadcast([128, 8, 64]), op=ALU.mult), r=["xs", "dsk"], w=[("t2", i2)])
                    S.op("pool", lambda: G.tensor_tensor(out=t2[i2][:], in0=t2[i2][:], in1=t1[i2][:], op=ALU.add),
                         r=[("t2", i2), ("t1", i2)], w=[("t2", i2)])
                    S.op("pool", lambda: G.tensor_tensor(out=t2[i2][:], in0=t2[i2][:], in1=zs[i2][:], op=ALU.mult),
                         r=[("t2", i2), ("zs", i2)], w=[("t2", i2)])
                    S.op("act", lambda: A.activation(out=t1[i2][:], in_=t2[i2][:], func=AF.Square, accum_out=ss[i2][:, 0:1]),
                         r=[("t2", i2)], w=[("t1", i2), ("ss", i2)])
                    S.op("act", lambda: A.activation(out=ss[i2][:, 1:2], in_=ss[i2][:, 0:1], func=AF.Sqrt, bias=self.epsc[:, 1:2],
                                                     scale=1.0 / 512.0), r=[("ss", i2), "epsc"], w=[("ss", i2)])
                    S.op("dve", lambda: V.reciprocal(out=ss[i2][:, 1:2], in_=ss[i2][:, 1:2]), r=[("ss", i2)], w=[("ss", i2)])
                    S.op("dve", lambda: V.scalar_tensor_tensor(out=ytm[i2][:], in0=t2[i2][:], scalar=ss[i2][:, 1:2],
                                                               in1=nwbc[:, g * 512:(g + 1) * 512], op0=ALU.mult, op1=ALU.mult),
                         r=[("t2", i2), ("ss", i2), "nwbc"], w=[("ytm", i2)])
                    pt, kpt = self.psum()
                    ptv = pt[:].bitcast(BF16)
                    for i in range(4):
                        S.op("pe", lambda i=i, ptv=ptv: PE.transpose(out=ptv[:, i * 128:(i + 1) * 128],
                                                                     in_=ytm[i2][:, i * 128:(i + 1) * 128], identity=self.identbf[:]),
                             r=[("ytm", i2), "identbf"], w=[kpt])
                    S.op("act", lambda ptv=ptv: A.activation(out=yTg[:, :, tsl], in_=ptv[:, 0:512].rearrange("p (a b) -> p a b", a=4),
                                                             func=AF.Copy), r=[kpt], w=["yTg", kpt])
                for i in range(4):
                    S.dma("sp", ydst[g * 512 + i * 128:g * 512 + (i + 1) * 128, :], yTg[:, i, :], r=["yTg"], w=[("yT0", g * 4 + i)])
            S.barrier()


    def rwkv(self, l):
        nc, S = self.nc, self.S
        V, A, G, PE = nc.vector, nc.scalar, nc.gpsimd, nc.tensor
        Wl = self.P["w_in"][l]
        ydst = self.yT_dram[1]
        hTk = [("hT", t) for t in range(NT)]
        P_ = self.P

        def mm(out, lhsT, rhs, start, stop, r, w):
            S.op("pe", lambda: PE.matmul(out, lhsT=lhsT, rhs=rhs, start=start, stop=stop), r=r, w=w)

        with contextlib.ExitStack() as st:
            mask4 = self.sb(st, "mask4", [128, 4, 128], F32)
            maskL = self.sb(st, "maskL", [128, 2, 128], F32)
            bdm = self.sb(st, "bdm", [128, 128], F32)
            mEO = self.sb(st, "mEO", [128, 2], F32)
            hsel = self.sb(st, "hsel", [128, 2], F32)
            rm = self.sb(st, "rm128", [128, T], BF16)
            c05 = self.sb(st, "c05", [128, 1], F32)
            for j in range(4):
                S.op("pool", lambda j=j: G.affine_select(out=mask4[:, j, :], in_=self.ones[:], pattern=[[1, 128]],
                                                         compare_op=(ALU.is_gt if j % 2 == 0 else ALU.is_ge), fill=0.0,
                                                         base=0, channel_multiplier=-1), r=["ones"], w=["mask4"])
            for j in range(2):
                S.op("pool", lambda j=j: G.affine_select(out=maskL[:, j, :], in_=self.ones[:], pattern=[[-1, 128]],
                                                         compare_op=ALU.is_gt, fill=0.0, base=0, channel_multiplier=1),
                     r=["ones"], w=["maskL"])
            S.op("pool", lambda: G.memset(bdm[:], 0.0), w=["bdm"])
            S.op("pool", lambda: G.memset(bdm[0:64, 0:64], 1.0), w=["bdm"])
            S.op("pool", lambda: G.memset(bdm[64:128, 64:128], 1.0), w=["bdm"])
            for (t_, nm) in ((mEO, "mEO"), (hsel, "hsel")):
                S.op("pool", lambda t_=t_: G.memset(t_[0:64, 0:1], 1.0), w=[nm])
                S.op("pool", lambda t_=t_: G.memset(t_[64:128, 0:1], 0.0), w=[nm])
                S.op("pool", lambda t_=t_: G.memset(t_[0:64, 1:2], 0.0), w=[nm])
                S.op("pool", lambda t_=t_: G.memset(t_[64:128, 1:2], 1.0), w=[nm])
            S.op("pool", lambda: G.memset(rm[:], 1.0), w=["rm"])
            S.op("pool", lambda: G.memset(rm[:].rearrange("p (c j) -> p c j", j=128)[:, :, 0:1], 0.0), w=["rm"])
            S.op("pool", lambda: G.memset(c05[:], -0.5), w=["c05"])
            pc = {}
            for nm, src in (("mu_r", P_["rwkv_mu"][l, 0:1024]), ("mu_k", P_["rwkv_mu"][l, 1024:2048]),
                            ("mu_v", P_["rwkv_mu"][l, 2048:3072]), ("w0", P_["rwkv_w0"][l]), ("a0", P_["rwkv_a0"][l]),
                            ("k_k", P_["rwkv_k_k"][l]), ("k_a", P_["rwkv_k_a"][l]),
                            ("r_k", P_["rwkv_r_k"][l].rearrange("h k -> (h k)"))):
                t_ = self.sb(st, "pc_" + nm, [128, 8, 1], F32)
                S.dma("sp", t_[:], src.rearrange("(q p o) -> p q o", p=128, o=1), w=["pc_" + nm])
                pc[nm] = t_
            mul = self.sb(st, "mul", [128, 3], F32)
            S.dma("sp", mul[0:64, 0:1], P_["rwkv_mu"][l, 3072:3136].rearrange("(p o) -> p o", o=1), w=["mul"])
            S.dma("sp", mul[0:64, 1:2], P_["rwkv_mu"][l, 3136:3200].rearrange("(p o) -> p o", o=1), w=["mul"])
            S.dma("sp", mul[:, 2:3], P_["rwkv_mu"][l, 3200:3328].rearrange("(p o) -> p o", o=1), w=["mul"])
            nw0 = self.sb(st, "nw0", [128, 8, 1], F32)
            omka = self.sb(st, "omka", [128, 8, 1], F32)
            S.op("dve", lambda: V.tensor_scalar(out=nw0[:], in0=pc["w0"][:], scalar1=-1.0, scalar2=None, op0=ALU.mult),
                 r=["pc_w0"], w=["nw0"])
            S.op("dve", lambda: V.tensor_scalar(out=omka[:], in0=pc["k_a"][:], scalar1=-1.0, scalar2=1.0, op0=ALU.mult,
                                                op1=ALU.add), r=["pc_k_a"], w=["omka"])
            wl = self.sb(st, "wl", [128, KT, 256], BF16)
            w2 = self.sb(st, "w2", [64, D], BF16)
            a2 = self.sb(st, "a2", [64, D], BF16)
            g2 = self.sb(st, "g2", [128, D], BF16)
            S.dma("pool", wl[:], Wl[:, OFF_RWKV + 3072:OFF_RWKV + 3328].rearrange("(kt p) n -> p kt n", p=128), w=["wl"])
            S.dma("pool", w2[:], P_["rwkv_w2"][l], w=["w2"])
            S.dma("pool", a2[:], P_["rwkv_a2"][l], w=["a2"])
            S.dma("pool", g2[:], P_["rwkv_g2"][l], w=["g2"])
            txw = self.sb(st, "txw", [64, T], BF16)
            xaT = self.sb(st, "xaT", [64, T], BF16)
            sgT = self.sb(st, "sgT", [128, T], BF16)
            xraw = self.sb(st, "xraw", [128, T + 1], F32)
            F = [self.sb(st, "F%d" % i, [128, T], F32) for i in range(5)]
            S.op("pool", lambda: G.memset(xraw[:, 0:1], 0.0), w=["xraw"])

            def proj_shift(wt, c0, m, mucol, dst, dkey, func=None, rows=128):
                for tq in range(4):
                    pb, kp = self.psum()
                    for kt in range(KT):
                        mm(pb[0:rows, :], wt[:, kt, c0:c0 + m], self.hT[:, kt, tq * 512:(tq + 1) * 512], kt == 0, kt == KT - 1,
                           [wt_key] + hTk[tq * 4:tq * 4 + 4], [kp])
                    S.op("act", lambda pb=pb, tq=tq: A.activation(out=xraw[0:rows, 1 + tq * 512:1 + (tq + 1) * 512],
                                                                  in_=pb[0:rows, :], func=AF.Copy), r=[kp], w=["xraw", kp])
                S.op("dve", lambda: V.tensor_tensor(out=F[4][0:rows, :], in0=xraw[0:rows, 0:T], in1=xraw[0:rows, 1:T + 1],
                                                    op=ALU.subtract), r=["xraw"], w=["F4"])
                if func is None:
                    S.op("dve", lambda: V.scalar_tensor_tensor(out=dst[0:rows, :], in0=F[4][0:rows, :], scalar=mucol,
                                                               in1=xraw[0:rows, 1:T + 1], op0=ALU.mult, op1=ALU.add),
                         r=["F4", "xraw", "mul"] + list(pc_keys), w=[dkey])
                else:
                    S.op("dve", lambda: V.scalar_tensor_tensor(out=F[4][0:rows, :], in0=F[4][0:rows, :], scalar=mucol,
                                                               in1=xraw[0:rows, 1:T + 1], op0=ALU.mult, op1=ALU.add),
                         r=["F4", "xraw", "mul"] + list(pc_keys), w=["F4"])
                    S.op("act", lambda: A.activation(out=dst[0:rows, :], in_=F[4][0:rows, :], func=func), r=["F4"], w=[dkey])

            pc_keys = ["pc_mu_r", "pc_mu_k", "pc_mu_v"]
            wt_key = "wl"
            proj_shift(wl, 0, 64, mul[0:64, 0:1], txw, "txw", AF.Tanh, rows=64)
            proj_shift(wl, 64, 64, mul[0:64, 1:2], xaT, "xaT", AF.Copy, rows=64)
            proj_shift(wl, 128, 128, mul[:, 2:3], sgT, "sgT", AF.Sigmoid, rows=128)
            wrkv1 = self.sb(st, "wrkv", [128, KT, 384], BF16)
            wrkv = [wrkv1, wrkv1]
            Pp = self.sb(st, "Pp", [128, T], BF16)
            Pc = self.sb(st, "Pc", [128, T], BF16)
            Pend = self.sb(st, "Pend", [128, NT], F32)
            iP = self.sb(st, "iP", [128, T], BF16)
            aT = self.sb(st, "aT", [128, T], BF16)
            rT = self.sb(st, "rT", [128, T], BF16)
            bT = self.sb(st, "bT", [128, T], BF16)
            kT = self.sb(st, "kT", [128, T], BF16)
            vT = self.sb(st, "vT", [128, T], BF16)
            AR = [self.sb(st, "AR", [128, NT, 2, 128], BF16) for _ in range(2)]
            Vtm = self.sb(st, "Vtm", [128, NT, 128], BF16)
            Btm = self.sb(st, "Btm", [128, NT, 128], BF16)
            Ktm = self.sb(st, "Ktm", [128, NT, 128], BF16)
            bon = self.sb(st, "bon", [128, NT, 2], F32)
            st32 = self.sb(st, "st32", [128, 2 * NT, 4], F32)
            lnw = self.sb(st, "lnwb", [128, 128], F32)
            lnb = self.sb(st, "lnbb", [128, 128], F32)
            Z32 = self.sb(st, "Z32", [128, 128], F32)
            Zt = self.sb(st, "Zt", [128, 128], F32)
            Zbf = [self.sb(st, "Zbf", [128, 128], BF16) for _ in range(2)]
            Wsb = [self.sb(st, "Wsb", [128, 128], BF16) for _ in range(2)]
            Usb = [self.sb(st, "Usb", [128, 128], BF16) for _ in range(2)]
            NS = 4
            abrb = [self.sb(st, "abrb", [128, 4, 128], BF16) for _ in range(NS)]
            akrk = [self.sb(st, "akrk", [128, 4, 128], BF16) for _ in range(NS)]
            L0 = [self.sb(st, "L0", [128, 2, 128], BF16) for _ in range(NS)]
            XX = [[self.sb(st, "XX", [128, 4, 128], BF16) for _ in range(NS)] for _ in range(2)]
            Tt = [self.sb(st, "Tt", [128, 2, 128], BF16) for _ in range(NS)]

            def load_w(p):
                b = 0
                for j in range(3):
                    c0 = OFF_RWKV + j * 1024 + p * 128
                    S.dma("pool", wrkv[b][:, :, j * 128:(j + 1) * 128], Wl[:, c0:c0 + 128].rearrange("(kt p) n -> p kt n", p=128),
                          w=[("wrkv", b)])

            for p in range(8):
                b = 0
                load_w(p)
                fs_ = slice(p * 128, (p + 1) * 128)
                S.dma("sp", lnw[:], P_["rwkv_ln_w"][l, fs_].partition_broadcast(128), w=["lnw"])
                S.dma("sp", lnb[:], P_["rwkv_ln_b"][l, fs_].partition_broadcast(128), w=["lnb"])
                wt_key = ("wrkv", b)
                proj_shift(wrkv[b], 128, 128, pc["mu_k"][:, p, :], F[0], "F0")
                proj_shift(wrkv[b], 0, 128, pc["mu_r"][:, p, :], F[1], "F1")
                proj_shift(wrkv[b], 256, 128, pc["mu_v"][:, p, :], vT, "vT", AF.Copy)
                for tq in range(4):
                    pb, kp = self.psum()
                    qs_ = slice(tq * 512, (tq + 1) * 512)
                    mm(pb[:], w2[:, fs_], txw[:, qs_], True, True, ["w2", "txw"], [kp])
                    S.op("act", lambda pb=pb: A.activation(out=F[2][:, qs_], in_=pb[:], func=AF.Exp, bias=nw0[:, p, :], scale=-1.0),
                         r=[kp, "nw0"], w=["F2", kp])
                S.op("act", lambda: A.activation(out=F[2][:], in_=F[2][:], func=AF.Ln, bias=self.epsc[:, 3:4], scale=1.0),
                     r=["F2", "epsc"], w=["F2"])
                S.op("act", lambda: A.activation(out=F[2][:], in_=F[2][:], func=AF.Exp, bias=c05[:, 0:1], scale=-1.0),
                     r=["F2", "c05"], w=["F2"])
                S.op("dve", lambda: V.tensor_tensor_scan(out=F[3][:], data0=rm[:], data1=F[2][:], initial=0.0, op0=ALU.mult,
                                                         op1=ALU.add), r=["rm", "F2"], w=["F3"])
                S.op("dve", lambda: V.tensor_tensor(out=F[2][:], in0=F[3][:], in1=F[2][:], op=ALU.subtract),
                     r=["F2", "F3"], w=["F2"])
                S.op("act", lambda: A.activation(out=Pp[:], in_=F[2][:], func=AF.Exp, scale=-1.0), r=["F2"], w=["Pp"])
                S.op("act", lambda: A.activation(out=Pc[:], in_=F[3][:], func=AF.Exp, scale=-1.0), r=["F3"], w=["Pc"])
                S.op("act", lambda: A.activation(out=iP[:], in_=F[3][:], func=AF.Exp), r=["F3"], w=["iP"])
                S.op("act", lambda: A.activation(out=Pend[:], in_=F[3][:].rearrange("p (t j) -> p t j", j=128)[:, :, 127],
                                                 func=AF.Exp, scale=-1.0), r=["F3"], w=["Pend"])
                for tq in range(4):
                    pb, kp = self.psum()
                    qs_ = slice(tq * 512, (tq + 1) * 512)
                    mm(pb[:], a2[:, fs_], xaT[:, qs_], True, True, ["a2", "xaT"], [kp])
                    S.op("act", lambda pb=pb: A.activation(out=F[2][:, qs_], in_=pb[:], func=AF.Sigmoid, bias=pc["a0"][:, p, :],
                                                           scale=1.0), r=[kp, "pc_a0"], w=["F2", kp])
                S.op("pool", lambda: G.tensor_scalar(out=F[3][:], in0=F[0][:], scalar1=pc["k_k"][:, p, :], scalar2=None,
                                                     op0=ALU.mult), r=["F0", "pc_k_k"], w=["F3"])
                S.op("act", lambda: A.activation(out=F[4][:], in_=F[3][:], func=AF.Square), r=["F3"], w=["F4"])
                for tq in range(4):
                    pb, kp = self.psum()
                    qs_ = slice(tq * 512, (tq + 1) * 512)
                    mm(pb[:], bdm[:], F[4][:, qs_], True, True, ["bdm", "F4"], [kp])
                    S.op("act", lambda pb=pb: A.activation(out=F[4][:, qs_], in_=pb[:], func=AF.Sqrt), r=[kp], w=["F4", kp])
                S.op("dve", lambda: V.tensor_scalar(out=F[4][:], in0=F[4][:], scalar1=1e-12, scalar2=None, op0=ALU.max),
                     r=["F4"], w=["F4"])
                S.op("dve", lambda: V.reciprocal(out=F[4][:], in_=F[4][:]), r=["F4"], w=["F4"])
                S.op("dve", lambda: V.tensor_tensor(out=F[3][:], in0=F[3][:], in1=F[4][:], op=ALU.mult), r=["F3", "F4"], w=["F3"])
                S.op("dve", lambda: V.scalar_tensor_tensor(out=aT[:], in0=F[3][:], scalar=-1.0, in1=Pp[:], op0=ALU.mult,
                                                           op1=ALU.mult), r=["F3", "Pp"], w=["aT"])
                S.op("pool", lambda: G.tensor_tensor(out=F[4][:], in0=F[3][:], in1=F[2][:], op=ALU.mult), r=["F3", "F2"], w=["F4"])
                S.op("pool", lambda: G.tensor_tensor(out=bT[:], in0=F[4][:], in1=iP[:], op=ALU.mult), r=["F4", "iP"], w=["bT"])
                S.op("dve", lambda: V.tensor_scalar(out=F[2][:], in0=F[2][:], scalar1=pc["k_a"][:, p, :], scalar2=omka[:, p, :],
                                                    op0=ALU.mult, op1=ALU.add), r=["F2", "pc_k_a", "omka"], w=["F2"])
                S.op("dve", lambda: V.tensor_tensor(out=F[0][:], in0=F[0][:], in1=F[2][:], op=ALU.mult), r=["F0", "F2"], w=["F0"])
                S.op("pool", lambda: G.tensor_tensor(out=kT[:], in0=F[0][:], in1=iP[:], op=ALU.mult), r=["F0", "iP"], w=["kT"])
                S.op("dve", lambda: V.tensor_tensor(out=rT[:], in0=F[1][:], in1=Pc[:], op=ALU.mult), r=["F1", "Pc"], w=["rT"])
                S.op("dve", lambda: V.scalar_tensor_tensor(out=F[1][:], in0=F[1][:], scalar=pc["r_k"][:, p, :], in1=F[0][:],
                                                           op0=ALU.mult, op1=ALU.mult), r=["F1", "F0", "pc_r_k"], w=["F1"])
                for h in range(2):
                    S.op("pool", lambda h=h: G.tensor_scalar(out=AR[h][:, :, 0, :], in0=aT[:].rearrange("p (t j) -> p t j", j=128),
                                                             scalar1=mEO[:, h:h + 1], scalar2=None, op0=ALU.mult),
                         r=["aT", "mEO"], w=[("AR", h)])
                    S.op("pool", lambda h=h: G.tensor_scalar(out=AR[h][:, :, 1, :], in0=rT[:].rearrange("p (t j) -> p t j", j=128),
                                                             scalar1=mEO[:, h:h + 1], scalar2=None, op0=ALU.mult),
                         r=["rT", "mEO"], w=[("AR", h)])
                for (src, skey, dst, dkey) in ((vT, "vT", Vtm, "Vtm"), (bT, "bT", Btm, "Btm"), (kT, "kT", Ktm, "Ktm")):
                    for t4 in range(4):
                        pb, kp = self.psum()
                        pbv = pb[:].bitcast(BF16)
                        for j in range(4):
                            tt = t4 * 4 + j
                            S.op("pe", lambda j=j, tt=tt, pbv=pbv, src=src: PE.transpose(
                                out=pbv[:, j * 128:(j + 1) * 128], in_=src[:, tt * 128:(tt + 1) * 128], identity=self.identbf[:]),
                                r=[skey, "identbf"], w=[kp])
                        S.op("act", lambda t4=t4, pbv=pbv, dst=dst: A.activation(
                            out=dst[:, t4 * 4:(t4 + 1) * 4, :], in_=pbv[:, 0:512].rearrange("p (a b) -> p a b", a=4), func=AF.Copy),
                            r=[kp], w=[dkey, kp])
                pb, kp = self.psum()
                for tt in range(NT):
                    mm(pb[:, tt * 2:(tt + 1) * 2], F[1][:, tt * 128:(tt + 1) * 128], hsel[:], True, True, ["F1", "hsel"], [kp])
                S.op("dve", lambda pb=pb: V.tensor_copy(out=bon[:].rearrange("p t h -> p (t h)"), in_=pb[:, 0:2 * NT]),
                     r=[kp], w=["bon", kp])
                ytm = F[2][:].rearrange("p (t j) -> p t j", j=128)
                ysq = F[3][:].rearrange("p (t j) -> p t j", j=128)

                def inv_group(gi):
                    tiles = range(gi * 4, gi * 4 + 4)
                    for tt in tiles:
                        sl = tt % NS
                        tsl = slice(tt * 128, (tt + 1) * 128)
                        p1, k1 = self.psum()
                        p2, k2 = self.psum()
                        p3, k3 = self.psum()
                        for h in range(2):
                            rhs = AR[h][:, tt].rearrange("p a j -> p (a j)")
                            mm(p1[:, h * 256:(h + 1) * 256], bT[:, tsl], rhs, True, True, ["bT", ("AR", h)], [k1])
                            mm(p2[:, h * 256:(h + 1) * 256], kT[:, tsl], rhs, True, True, ["kT", ("AR", h)], [k2])
                            mm(p3[:, h * 128:(h + 1) * 128], AR[h][:, tt, 0, :], bT[:, tsl], True, True, [("AR", h), "bT"], [k3])
                        S.op("dve", lambda p1=p1, sl=sl: V.tensor_tensor(out=abrb[sl][:].rearrange("p a j -> p (a j)"), in0=p1[:],
                                                                        in1=mask4[:].rearrange("p a j -> p (a j)"), op=ALU.mult),
                             r=[k1, "mask4"], w=[("abrb", sl), k1])
                        S.op("dve", lambda p2=p2, sl=sl: V.tensor_tensor(out=akrk[sl][:].rearrange("p a j -> p (a j)"), in0=p2[:],
                                                                        in1=mask4[:].rearrange("p a j -> p (a j)"), op=ALU.mult),
                             r=[k2, "mask4"], w=[("akrk", sl), k2])
                        S.op("dve", lambda p3=p3, sl=sl: V.tensor_tensor(out=L0[sl][:].rearrange("p a j -> p (a j)"), in0=p3[:, 0:256],
                                                                        in1=maskL[:].rearrange("p a j -> p (a j)"), op=ALU.mult),
                             r=[k3, "maskL"], w=[("L0", sl), k3])
                        for h in range(2):
                            S.op("pool", lambda h=h, sl=sl: G.tensor_tensor(out=Tt[sl][:, h, :], in0=abrb[sl][:, 2 * h, :],
                                                                            in1=self.identbf[:], op=ALU.add),
                                 r=[("abrb", sl), "identbf"], w=[("Tt", sl)])

                    def Xk(k, sl, h):
                        return (L0[sl][:, h, :], ("L0", sl)) if k == 0 else (XX[k % 2][sl][:, 2 * h, :], ("XX", k % 2, sl))

                    def Xtk(k, sl, h):
                        return (abrb[sl][:, 2 * h, :], ("abrb", sl)) if k == 0 else (XX[k % 2][sl][:, 2 * h + 1, :], ("XX", k % 2, sl))

                    for k in range(6):
                        for tt in tiles:
                            sl = tt % NS
                            pb, kp = self.psum()
                            for h in range(2):
                                x, kx = Xk(k, sl, h)
                                xt, kxt = Xtk(k, sl, h)
                                mm(pb[:, (2 * h) * 128:(2 * h + 1) * 128], xt, x, True, True, [kx, kxt], [kp])
                                if k < 5:
                                    mm(pb[:, (2 * h + 1) * 128:(2 * h + 2) * 128], x, xt, True, True, [kx, kxt], [kp])
                            kn = ("XX", (k + 1) % 2, sl)
                            if k < 5:
                                S.op("act", lambda pb=pb, sl=sl, k=k: A.activation(
                                    out=XX[(k + 1) % 2][sl][:].rearrange("p a j -> p (a j)"), in_=pb[:], func=AF.Copy),
                                    r=[kp], w=[kn, kp])
                            else:
                                S.op("act", lambda pb=pb, sl=sl, k=k: A.activation(
                                    out=XX[(k + 1) % 2][sl][:, 0:4:2, :], in_=pb[:].rearrange("p (a j) -> p a j", j=128)[:, 0:4:2, :],
                                    func=AF.Copy), r=[kp], w=[kn, kp])
                        for t2 in range(2):
                            pb, kp = self.psum()
                            for j in range(2):
                                sl = (gi * 4 + t2 * 2 + j) % NS
                                for h in range(2):
                                    x1, kx1 = Xk(k + 1, sl, h)
                                    mm(pb[:, (j * 2 + h) * 128:(j * 2 + h + 1) * 128], x1, Tt[sl][:, h, :], True, True,
                                       [kx1, ("Tt", sl)], [kp])
                            for j in range(2):
                                sl = (gi * 4 + t2 * 2 + j) % NS
                                S.op("dve", lambda pb=pb, sl=sl, j=j: V.tensor_tensor(
                                    out=Tt[sl][:].rearrange("p a j -> p (a j)"), in0=pb[:, j * 256:(j + 1) * 256],
                                    in1=Tt[sl][:].rearrange("p a j -> p (a j)"), op=ALU.add), r=[kp, ("Tt", sl)], w=[("Tt", sl), kp])

                def chain_group(gi):
                    for tt in range(gi * 4, gi * 4 + 4):
                        sl = tt % NS
                        i2 = tt % 2
                        tsl = slice(tt * 128, (tt + 1) * 128)
                        zb, kz = Zbf[i2], ("Zbf", i2)
                        pw, kpw = self.psum()
                        mm(pw[:, 0:128], aT[:, tsl], zb[:], True, False, ["aT", kz], [kpw])
                        for h in range(2):
                            mm(pw[:, h * 64:(h + 1) * 64], akrk[sl][:, 2 * h, :], Vtm[:, tt, h * 64:(h + 1) * 64], False, h == 1,
                               [("akrk", sl), "Vtm"], [kpw])
                        S.op("act", lambda pw=pw: A.activation(out=Wsb[i2][:], in_=pw[:, 0:128], func=AF.Copy),
                             r=[kpw], w=[("Wsb", i2), kpw])
                        pu, kpu = self.psum()
                        for h in range(2):
                            mm(pu[:, h * 64:(h + 1) * 64], Tt[sl][:, h, :], Wsb[i2][:, h * 64:(h + 1) * 64], True, True,
                               [("Tt", sl), ("Wsb", i2)], [kpu])
                        S.op("act", lambda pu=pu: A.activation(out=Usb[i2][:], in_=pu[:, 0:128], func=AF.Copy),
                             r=[kpu], w=[("Usb", i2), kpu])
                        py, kpy = self.psum()
                        mm(py[:, 0:128], rT[:, tsl], zb[:], True, False, ["rT", kz], [kpy])
                        for h in range(2):
                            mm(py[:, h * 64:(h + 1) * 64], abrb[sl][:, 2 * h + 1, :], Usb[i2][:, h * 64:(h + 1) * 64], False, False,
                               [("abrb", sl), ("Usb", i2)], [kpy])
                            mm(py[:, h * 64:(h + 1) * 64], akrk[sl][:, 2 * h + 1, :], Vtm[:, tt, h * 64:(h + 1) * 64], False, h == 1,
                               [("akrk", sl), "Vtm"], [kpy])
                        S.op("act", lambda py=py: A.activation(out=ytm[:, tt, :], in_=py[:, 0:128], func=AF.Copy),
                             r=[kpy], w=["F2", kpy])
                        pz, kpz = self.psum()
                        mm(pz[:, 0:128], Btm[:, tt, :], Usb[i2][:], True, False, ["Btm", ("Usb", i2)], [kpz])
                        mm(pz[:, 0:128], Ktm[:, tt, :], Vtm[:, tt, :], False, True, ["Ktm", "Vtm"], [kpz])
                        S.op("dve", lambda pz=pz: V.tensor_tensor(out=Zt[:], in0=pz[:, 0:128], in1=bdm[:], op=ALU.mult),
                             r=[kpz, "bdm"], w=["Zt", kpz])
                        S.op("dve", lambda: V.tensor_tensor(out=Zt[:], in0=Zt[:], in1=Z32[:], op=ALU.add), r=["Zt", "Z32"], w=["Zt"])
                        S.op("dve", lambda: V.tensor_scalar(out=Z32[:], in0=Zt[:], scalar1=Pend[:, tt:tt + 1],
                                                            scalar2=None, op0=ALU.mult), r=["Zt", "Pend"], w=["Z32"])
                        S.op("pool", lambda: G.tensor_copy(out=Zbf[(tt + 1) % 2][:], in_=Z32[:]), r=["Z32"], w=[("Zbf", (tt + 1) % 2)])

                S.op("pool", lambda: G.memset(Z32[:], 0.0), w=["Z32"])
                S.op("pool", lambda: G.memset(Zbf[0][:], 0.0), w=[("Zbf", 0)])
                for gi in range(4):
                    inv_group(gi)
                    chain_group(gi)
                y16, yTp = Btm, kT
                y3 = ytm.rearrange("p t (h c) -> p (t h) c", c=64)
                q3 = ysq.rearrange("p t (h c) -> p (t h) c", c=64)
                S.op("act", lambda: A.activation(out=F[3][:], in_=F[2][:], func=AF.Square), r=["F2"], w=["F3"])
                S.op("dve", lambda: V.tensor_reduce(out=st32[:, :, 0], in_=y3, axis=AX.X, op=ALU.add), r=["F2"], w=["st32"])
                S.op("dve", lambda: V.tensor_reduce(out=st32[:, :, 1], in_=q3, axis=AX.X, op=ALU.add), r=["F3"], w=["st32"])
                S.op("dve", lambda: V.tensor_scalar(out=st32[:, :, 0], in0=st32[:, :, 0], scalar1=1.0 / 64.0, scalar2=None,
                                                    op0=ALU.mult), r=["st32"], w=["st32"])
                S.op("dve", lambda: V.tensor_tensor(out=st32[:, :, 2], in0=st32[:, :, 0], in1=st32[:, :, 0], op=ALU.mult),
                     r=["st32"], w=["st32"])
                S.op("dve", lambda: V.scalar_tensor_tensor(out=st32[:, :, 1], in0=st32[:, :, 1], scalar=1.0 / 64.0, in1=st32[:, :, 2],
                                                           op0=ALU.mult, op1=ALU.subtract), r=["st32"], w=["st32"])
                S.op("act", lambda: A.activation(out=st32[:, :, 1], in_=st32[:, :, 1], func=AF.Sqrt, bias=self.epsc[:, 2:3], scale=1.0),
                     r=["st32", "epsc"], w=["st32"])
                S.op("dve", lambda: V.reciprocal(out=st32[:, :, 1], in_=st32[:, :, 1]), r=["st32"], w=["st32"])
                S.op("dve", lambda: V.tensor_tensor(out=y3, in0=y3, in1=st32[:, :, 0:1].to_broadcast([128, 2 * NT, 64]),
                                                    op=ALU.subtract), r=["F2", "st32"], w=["F2"])
                S.op("dve", lambda: V.tensor_tensor(out=y3, in0=y3, in1=st32[:, :, 1:2].to_broadcast([128, 2 * NT, 64]),
                                                    op=ALU.mult), r=["F2", "st32"], w=["F2"])
                S.op("pool", lambda: G.tensor_tensor(out=ytm, in0=ytm, in1=lnw[:].unsqueeze(1).to_broadcast([128, NT, 128]),
                                                     op=ALU.mult), r=["F2", "lnw"], w=["F2"])
                S.op("pool", lambda: G.tensor_tensor(out=ytm, in0=ytm, in1=lnb[:].unsqueeze(1).to_broadcast([128, NT, 128]),
                                                     op=ALU.add), r=["F2", "lnb"], w=["F2"])
                S.op("dve", lambda: V.tensor_tensor(out=q3, in0=Vtm[:].rearrange("p t (h c) -> p (t h) c", c=64),
                                                    in1=bon[:].rearrange("p t h -> p (t h)").unsqueeze(2).to_broadcast([128, 2 * NT, 64]),
                                                    op=ALU.mult), r=["Vtm", "bon", "F3"], w=["F3"])
                S.op("dve", lambda: V.tensor_tensor(out=F[2][:], in0=F[2][:], in1=F[3][:], op=ALU.add), r=["F2", "F3"], w=["F2"])
                for t4 in range(4):
                    pb, kp = self.psum()
                    for j in range(4):
                        tt = t4 * 4 + j
                        mm(pb[:, j * 128:(j + 1) * 128], sgT[:, tt * 128:(tt + 1) * 128], g2[:, fs_], True, True, ["sgT", "g2"], [kp])
                    S.op("dve", lambda pb=pb, t4=t4: V.tensor_tensor(
                        out=y16[:, t4 * 4:(t4 + 1) * 4, :], in0=pb[:].rearrange("p (a j) -> p a j", j=128),
                        in1=ytm[:, t4 * 4:(t4 + 1) * 4, :], op=ALU.mult), r=[kp, "F2"], w=["Btm", kp])
                for t4 in range(4):
                    pb, kp = self.psum()
                    pbv = pb[:].bitcast(BF16)
                    for j in range(4):
                        tt = t4 * 4 + j
                        S.op("pe", lambda j=j, tt=tt, pbv=pbv: PE.transpose(out=pbv[:, j * 128:(j + 1) * 128], in_=y16[:, tt, :],
                                                                           identity=self.identbf[:]), r=["Btm", "identbf"], w=[kp])
                    S.op("act", lambda t4=t4, pbv=pbv: A.activation(out=yTp[:, t4 * 512:(t4 + 1) * 512], in_=pbv[:, 0:512], func=AF.Copy),
                         r=[kp], w=["kT", kp])
                S.dma("sp", ydst[fs_, :], yTp[:], r=["kT"], w=[("yT1", p)])
            S.barrier()


    def merge(self, l):
        nc, S = self.nc, self.S
        V, A, G, PE = nc.vector, nc.scalar, nc.gpsimd, nc.tensor
        Wl = self.P["w_in"][l]
        brw = [self.P["w_br_ssd"][l], self.P["w_br_rwkv"][l], self.P["w_br_hgrn"][l]]
        hTk = [("hT", t) for t in range(NT)]
        with contextlib.ExitStack() as st:
            mT = self.sb(st, "mT", [128, KT, T], F32)
            wbr = self.sb(st, "wbr", [128, KT, D], BF16)
            wgt = self.sb(st, "wgt", [128, KT, D], BF16)
            yq = [self.sb(st, "yq", [128, KT, 512], BF16) for _ in range(2)]
            sg = [self.sb(st, "sgm", [128, 512], BF16) for _ in range(2)]
            tmp = [self.sb(st, "tmpm", [128, 512], F32) for _ in range(2)]
            cnt = 0
            for i in range(3):
                S.dma("pool", wbr[:], brw[i].rearrange("(kt p) n -> p kt n", p=128), w=["wbr"])
                c0 = OFF_GATES + i * 1024
                S.dma("pool", wgt[:], Wl[:, c0:c0 + 1024].rearrange("(kt p) n -> p kt n", p=128), w=["wgt"])
                for q in range(4):
                    qs_ = slice(q * 512, (q + 1) * 512)
                    yb = (i * 4 + q) % 2
                    S.dma("sp", yq[yb][:], self.yT_dram[i][:, qs_].rearrange("(kt p) n -> p kt n", p=128),
                          r=[("yT%d" % i, k) for k in range(8)], w=[("yq", yb)])
                    for ot in range(KT):
                        os_ = slice(ot * 128, (ot + 1) * 128)
                        pg, kg = self.psum()
                        pb, kb = self.psum()
                        for kt in range(KT):
                            S.op("pe", lambda kt=kt, pg=pg: PE.matmul(pg[:], lhsT=wgt[:, kt, os_], rhs=self.hT[:, kt, qs_],
                                                                      start=(kt == 0), stop=(kt == KT - 1)),
                                 r=["wgt"] + hTk[q * 4:q * 4 + 4], w=[kg])
                        for kt in range(KT):
                            S.op("pe", lambda kt=kt, pb=pb: PE.matmul(pb[:], lhsT=wbr[:, kt, os_], rhs=yq[yb][:, kt, :],
                                                                      start=(kt == 0), stop=(kt == KT - 1)),
                                 r=["wbr", ("yq", yb)], w=[kb])
                        c2 = cnt % 2
                        cnt += 1
                        S.op("act", lambda pg=pg, c2=c2: A.activation(out=sg[c2][:], in_=pg[:], func=AF.Sigmoid),
                             r=[kg], w=[("sgm", c2), kg])
                        if i == 0:
                            S.op("dve", lambda pb=pb, c2=c2: V.tensor_tensor(out=mT[:, ot, qs_], in0=pb[:], in1=sg[c2][:], op=ALU.mult),
                                 r=[kb, ("sgm", c2)], w=[("mT", q), kb])
                        else:
                            S.op("dve", lambda pb=pb, c2=c2: V.tensor_tensor(out=tmp[c2][:], in0=pb[:], in1=sg[c2][:], op=ALU.mult),
                                 r=[kb, ("sgm", c2)], w=[("tmpm", c2), kb])
                            S.op("pool", lambda c2=c2: G.tensor_tensor(out=mT[:, ot, qs_], in0=mT[:, ot, qs_], in1=tmp[c2][:],
                                                                       op=ALU.add), r=[("tmpm", c2), ("mT", q)], w=[("mT", q)])
            self.dbg_dump("merged%d" % l, lambda o: S.dma("sp", o.rearrange("(kt p) n -> p kt n", p=128), mT[:],
                                                          r=[("mT", q) for q in range(4)]))
            wo = wbr
            S.dma("pool", wo[:], self.P["w_out"][l].rearrange("(kt p) n -> p kt n", p=128), w=["wbr"])
            gbc = self.sb(st, "gbc1", [128, D], F32)
            bbc = self.sb(st, "bbc1", [128, D], F32)
            S.dma("sp", gbc[:], self.P["ln1_g"][l].partition_broadcast(128), w=["gbc"])
            S.dma("sp", bbc[:], self.P["ln1_b"][l].partition_broadcast(128), w=["bbc"])
            lnw = self.ln_alloc(st)
            h1 = [self.sb(st, "h1m", [128, D], F32) for _ in range(2)]
            mbf = [self.sb(st, "mbf", [128, KT, 128], BF16) for _ in range(2)]
            for tt in range(NT):
                s2 = tt % 2
                q = tt // 4
                tsl = slice(tt * 128, (tt + 1) * 128)
                S.op("act", lambda: A.activation(out=mbf[s2][:], in_=mT[:, :, tsl], func=AF.Copy), r=[("mT", q)], w=[("mbf", s2)])
                S.dma("sp", h1[s2][:], self.h_dram[tsl, :], r=[("hd", tt)], w=[("h1m", s2)])
                xin, kx = self.ln_xin(lnw, tt)
                for half in range(2):
                    po, ko = self.psum()
                    for kt in range(KT):
                        S.op("pe", lambda kt=kt, po=po: PE.matmul(po[:], lhsT=mbf[s2][:, kt, :], rhs=wo[:, kt, half * 512:(half + 1) * 512],
                                                                  start=(kt == 0), stop=(kt == KT - 1)), r=[("mbf", s2), "wbr"], w=[ko])
                    S.op("dve", lambda po=po, half=half: V.scalar_tensor_tensor(
                        out=xin[:, half * 512:(half + 1) * 512], in0=h1[s2][:, half * 512:(half + 1) * 512], scalar=ALPHA, in1=po[:],
                        op0=ALU.mult, op1=ALU.add), r=[ko, ("h1m", s2)], w=[kx, ko])
                self.ln_tile(lnw, tt, gbc, bbc, self.h_dram, router=True, extra=self.dbg_out.get("h1_%d" % l))
            S.barrier()

    def layer(self, l):
        S = self.S
        if "ssd" in self.stages:
            self.ssd(l)
            self.dbg_dump("ya%d" % l, lambda o: S.dma("sp", o, self.yT_dram[0], r=[("yT0", h) for h in range(8)]))
        if "rwkv" in self.stages:
            self.rwkv(l)
            self.dbg_dump("yb%d" % l, lambda o: S.dma("sp", o, self.yT_dram[1], r=[("yT1", h) for h in range(8)]))
        if "hgrn" in self.stages:
            self.hgrn(l)
            self.dbg_dump("yc%d" % l, lambda o: S.dma("sp", o, self.yT_dram[2], r=[("yT2", h) for h in range(8)]))
        if "merge" in self.stages:
            self.merge(l)
        if "moe" in self.stages:
            self.moe(l, last=(l == self.depth - 1))


_NC_CACHE = {}


def _get_nc():
    if "nc" not in _NC_CACHE:
        _NC_CACHE["nc"] = Builder().build()
    return _NC_CACHE["nc"]


def kernel(**inputs):
    nc = _get_nc()
    x = np.ascontiguousarray(inputs["x"], dtype=np.float32)
    base = {k: np.ascontiguousarray(inputs[k], dtype=np.float32) for k in PARAM_SHAPES}
    in_maps = []
    for c in range(8):
        m = dict(base)
        m["x"] = x[c]
        in_maps.append(m)
    res = run_bass_kernel_spmd(nc, in_maps, core_ids=list(range(8)))
    return np.stack([res.results[c]["out"] for c in range(8)], axis=0)
```

```python
import contextlib
import os
import numpy as np
CUT = int(os.environ.get('CUT', '99'))
HC = int(os.environ.get('HC', '99'))
HL = int(os.environ.get('HL', '99'))
import concourse.bass as bass
import concourse.mybir as mybir
from concourse.bass_utils import run_bass_kernel_spmd

F32 = mybir.dt.float32
BF16 = mybir.dt.bfloat16
AF = mybir.ActivationFunctionType
ALU = mybir.AluOpType
AX = mybir.AxisListType

D = 1024
T = 2048
NT = T // 128
KT = D // 128
DEPTH = 2
NE = 16
DEXP = 512
N_IN = 13072
ALPHA = (2 * DEPTH) ** 0.25
LN_EPS = 1e-5
RMS_EPS = 1e-6
GN_EPS = 64e-5
OFF_Z = 0
OFF_XBC = 1024
OFF_DT = 2560
OFF_RWKV = 2576
OFF_HGRN = OFF_RWKV + 3328
OFF_GATES = OFF_HGRN + 4096

PARAM_SHAPES = {
    "ln_in_g": [1024], "ln_in_b": [1024], "w_in": [2, 1024, 13072],
    "ssd_conv_w": [2, 4, 1536], "ssd_conv_b": [2, 1536], "ssd_dt_bias": [2, 16],
    "ssd_a_log": [2, 16], "ssd_d": [2, 16], "ssd_norm_w": [2, 1024],
    "rwkv_mu": [2, 3328], "rwkv_w0": [2, 1024], "rwkv_w2": [2, 64, 1024],
    "rwkv_a0": [2, 1024], "rwkv_a2": [2, 64, 1024], "rwkv_g2": [2, 128, 1024],
    "rwkv_k_k": [2, 1024], "rwkv_k_a": [2, 1024], "rwkv_r_k": [2, 16, 64],
    "rwkv_ln_w": [2, 1024], "rwkv_ln_b": [2, 1024], "hgrn_lb": [2, 1024],
    "hgrn_norm_w": [2, 128], "w_br_ssd": [2, 1024, 1024], "w_br_rwkv": [2, 1024, 1024],
    "w_br_hgrn": [2, 1024, 1024], "w_out": [2, 1024, 1024], "ln1_g": [2, 1024],
    "ln1_b": [2, 1024], "router_w": [1024, 16], "router_bias": [16],
    "exp_w_gate": [2, 16, 1024, 512], "exp_w_up": [2, 16, 1024, 512],
    "exp_w_down": [2, 16, 512, 1024], "ln2_g": [2, 1024], "ln2_b": [2, 1024],
}


class Sched:
    ENG = ["pe", "act", "dve", "pool", "sp"]

    def __init__(self, nc, es, n_dma=32, n_pdma=24):
        self.nc = nc
        self.e = {"pe": nc.tensor, "act": nc.scalar, "dve": nc.vector, "pool": nc.gpsimd, "sp": nc.sync}
        self.sem = {k: es.enter_context(nc.semaphore("sem_" + k)) for k in self.ENG}
        self.cnt = {k: 0 for k in self.ENG}
        self.dsem = [es.enter_context(nc.semaphore("dsem%d" % i)) for i in range(n_dma)]
        self.dtot = [0] * n_dma
        self.drr = 0
        self.psem = [es.enter_context(nc.semaphore("psem%d" % i)) for i in range(n_pdma)]
        self.pused = [False] * n_pdma
        self.pwaiters = [[] for _ in range(n_pdma)]
        self.pclr = [None] * n_pdma
        self.prr = 0
        self.msem = {k: es.enter_context(nc.semaphore("msem_" + k)) for k in self.ENG}
        self.mcnt = {k: 0 for k in self.ENG}
        self.seen = {k: {} for k in self.ENG}
        self.lastw = {}
        self.readers = {}
        self.nwait = 0

    def _semh(self, sk):
        if isinstance(sk, str):
            return self.sem[sk]
        if sk[0] == "m":
            return self.msem[sk[1]]
        return self.dsem[sk[1]] if sk[0] == "d" else self.psem[sk[1]]

    def _wait(self, e, tag):
        sk, val = tag
        if val <= 0 or self.seen[e].get(sk, 0) >= val:
            return
        if not isinstance(sk, str) and sk[0] == "p" and self.pclr[sk[1]] is not None and e != "pool":
            self._wait(e, self.pclr[sk[1]])
        self.e[e].wait_ge(self._semh(sk), val)
        self.seen[e][sk] = val
        self.nwait += 1
        if not isinstance(sk, str) and sk[0] == "p":
            self.pwaiters[sk[1]].append(self._marker(e))

    def _marker(self, e):
        self.e[e].sem_inc(self.msem[e], 1)
        self.mcnt[e] += 1
        return (("m", e), self.mcnt[e])

    def _deps(self, e, r, w):
        for k in r:
            t = self.lastw.get(k)
            if t is not None:
                self._wait(e, t)
        for k in w:
            t = self.lastw.get(k)
            if t is not None and (t[0] != e or e != "pe"):
                self._wait(e, t)
            for sk, val in self.readers.get(k, {}).items():
                if sk != e or e != "pe":
                    self._wait(e, (sk, val))

    def _record(self, tag, r, w):
        for k in r:
            d = self.readers.setdefault(k, {})
            if d.get(tag[0], 0) < tag[1]:
                d[tag[0]] = tag[1]
        for k in w:
            self.lastw[k] = tag
            self.readers[k] = {}

    def op(self, e, fn, r=(), w=()):
        self._deps(e, r, w)
        ins = fn()
        self.cnt[e] += 1
        ins.then_inc(self.sem[e], 1)
        if os.environ.get("OPLOG"):
            self.oplog = getattr(self, "oplog", {})
            self.oplog[(e, self.cnt[e])] = fn.__code__.co_firstlineno
        self._record((e, self.cnt[e]), r, w)

    def _dma_sw(self, out, in_, r, w):
        q = "pool"
        self._deps(q, r, w)
        i = self.prr
        self.prr = (self.prr + 1) % len(self.psem)
        sk = ("p", i)
        if self.pused[i]:
            self._wait(q, (sk, 16))
            for e in self.ENG:
                if e != q:
                    self._wait(e, (sk, 16))
            for tg in self.pwaiters[i]:
                if tg[0][1] != q:
                    self._wait(q, tg)
            self.e[q].sem_clear(self.psem[i])
            tclr = self._marker(q)
            self.pclr[i] = tclr
            for k, t in list(self.lastw.items()):
                if t[0] == sk:
                    self.lastw[k] = tclr
            for k, d in self.readers.items():
                if sk in d:
                    d.pop(sk)
                    d[tclr[0]] = tclr[1]
            for e in self.ENG:
                self.seen[e].pop(sk, None)
            self.pwaiters[i] = []
        ins = self.e[q].dma_start(out=out, in_=in_)
        ins.then_inc(self.psem[i], 16)
        self.pused[i] = True
        self._record((sk, 16), r, w)

    def dma(self, q, out, in_, r=(), w=()):
        if q == "pool" and os.environ.get("PSEM_CLEAR"):
            return self._dma_sw(out, in_, r, w)
        self._deps(q, r, w)
        i = self.drr
        self.drr = (self.drr + 1) % len(self.dsem)
        self._wait(q, (("d", i), self.dtot[i]))
        with self.nc.allow_non_contiguous_dma(reason="small per-feature parameter columns"):
            ins = self.e[q].dma_start(out=out, in_=in_)
        self.dtot[i] += 16
        ins.then_inc(self.dsem[i], 16)
        self._record((("d", i), self.dtot[i]), r, w)

    def barrier(self):
        for e in self.ENG:
            for o in self.ENG:
                if o != e:
                    self._wait(e, (o, self.cnt[o]))
            for i in range(len(self.dsem)):
                self._wait(e, (("d", i), self.dtot[i]))
            for i in range(len(self.psem)):
                if self.pused[i]:
                    self._wait(e, (("p", i), 16))

    def finish(self):
        for i in range(len(self.dsem)):
            self._wait("sp", (("d", i), self.dtot[i]))
        for i in range(len(self.psem)):
            if self.pused[i]:
                self._wait("sp", (("p", i), 16))
        for o in self.ENG:
            if o != "sp":
                self._wait("sp", (o, self.cnt[o]))


class Builder:
    def __init__(self, debug=None, stages=("pre", "hgrn", "ssd", "rwkv", "merge", "moe"), depth=DEPTH, pre_router=False):
        self.pre_router = pre_router
        self.debug = debug or {}
        self.stages = stages
        self.depth = depth
        self.nc = bass.Bass("TRN2", target_bir_lowering=False)
        nc = self.nc
        self.x = nc.dram_tensor("x", [T, D], F32, kind="ExternalInput").ap()
        self.P = {k: nc.dram_tensor(k, s, F32, kind="ExternalInput").ap() for k, s in PARAM_SHAPES.items()}
        self.out = nc.dram_tensor("out", [T, D], F32, kind="ExternalOutput").ap()
        self.h_dram = nc.dram_tensor("h_scr", [T, D], F32, kind="Internal").ap()
        self.yT_dram = [nc.dram_tensor("yT_scr%d" % i, [D, T], BF16, kind="Internal").ap() for i in range(3)]
        self.dbg_out = {}
        for name, (shape, dt) in self.debug.items():
            self.dbg_out[name] = nc.dram_tensor("dbg_" + name, shape, dt, kind="ExternalOutput").ap()
        self.uid = 0

    def sb(self, es, name, shape, dt):
        self.uid += 1
        return es.enter_context(self.nc.sbuf_tensor("%s_%d" % (name, self.uid), shape, dt))

    def psum(self):
        i = self.ps_rr
        self.ps_rr = (self.ps_rr + 1) % 8
        return self.ps[i], ("ps", i)

    def build(self):
        nc = self.nc
        with contextlib.ExitStack() as es:
            self.S = Sched(nc, es)
            S = self.S
            self.ps = [es.enter_context(nc.psum_tensor("psb%d" % i, [128, 512], F32)) for i in range(8)]
            self.ps_rr = 0
            self.ident32 = self.sb(es, "ident32", [128, 128], F32)
            self.identbf = self.sb(es, "identbf", [128, 128], BF16)
            self.zeros = self.sb(es, "zeros", [128, 128], F32)
            self.ones = self.sb(es, "ones", [128, 128], F32)
            self.onesbf = self.sb(es, "onesbf", [128, 128], BF16)
            self.epsc = self.sb(es, "epsc", [128, 4], F32)
            S.op("pool", lambda: nc.gpsimd.memset(self.zeros[:], 0.0), w=["zeros"])
            S.op("pool", lambda: nc.gpsimd.memset(self.ones[:], 1.0), w=["ones"])
            S.op("pool", lambda: nc.gpsimd.memset(self.onesbf[:], 1.0), w=["onesbf"])
            S.op("pool", lambda: nc.gpsimd.memset(self.epsc[:, 0:1], LN_EPS), w=["epsc"])
            S.op("pool", lambda: nc.gpsimd.memset(self.epsc[:, 1:2], RMS_EPS), w=["epsc"])
            S.op("pool", lambda: nc.gpsimd.memset(self.epsc[:, 2:3], GN_EPS), w=["epsc"])
            S.op("pool", lambda: nc.gpsimd.memset(self.epsc[:, 3:4], 1.0), w=["epsc"])
            S.op("pool", lambda: nc.gpsimd.affine_select(
                out=self.ident32[:], in_=self.zeros[:], pattern=[[1, 128]], compare_op=ALU.not_equal,
                fill=1.0, base=0, channel_multiplier=-1), r=["zeros"], w=["ident32"])
            S.op("pool", lambda: nc.gpsimd.tensor_copy(out=self.identbf[:], in_=self.ident32[:]),
                 r=["ident32"], w=["identbf"])
            self.hT = self.sb(es, "hT", [128, KT, T], BF16)
            self.gates = self.sb(es, "gates", [128, NT, NE], F32)
            self.logits = self.sb(es, "logits", [128, NT, NE], F32)
            self.rw32 = self.sb(es, "rw32", [128, KT, NE], F32)
            self.rbias = self.sb(es, "rbias", [128, NE], F32)
            S.dma("sp", self.rw32[:], self.P["router_w"].rearrange("(kt p) e -> p kt e", p=128), w=["rw32"])
            S.dma("sp", self.rbias[:], self.P["router_bias"].partition_broadcast(128), w=["rbias"])

            with contextlib.ExitStack() as st:
                gbc = self.sb(st, "gbc", [128, D], F32)
                bbc = self.sb(st, "bbc", [128, D], F32)
                S.dma("sp", gbc[:], self.P["ln_in_g"].partition_broadcast(128), w=["gbc"])
                S.dma("sp", bbc[:], self.P["ln_in_b"].partition_broadcast(128), w=["bbc"])
                lnw = self.ln_alloc(st)
                for tt in range(NT):
                    xin, kx = self.ln_xin(lnw, tt)
                    S.dma("sp", xin[:], self.x[tt * 128:(tt + 1) * 128, :], w=[kx])
                    self.ln_tile(lnw, tt, gbc, bbc, self.h_dram, router=self.pre_router, extra=self.dbg_out.get("h0"))
                S.barrier()
            self.dbg_dump("hT", lambda o: S.dma("sp", o, self.hT[:], r=[("hT", t) for t in range(NT)]))
            self.dbg_dump("logits", lambda o: S.dma("sp", o, self.logits[:], r=["logits"]))

            for l in range(self.depth):
                self.layer(l)
            S.finish()
        return nc

    def dbg_dump(self, name, fn):
        if name in self.dbg_out:
            fn(self.dbg_out[name])

    def ln_alloc(self, st):
        w = {}
        w["xin"] = [self.sb(st, "xin", [128, D], F32) for _ in range(2)]
        w["hh"] = [self.sb(st, "hh", [128, D], F32) for _ in range(2)]
        w["bst"] = [self.sb(st, "bst", [128, 2, 6], F32) for _ in range(2)]
        w["mv"] = [self.sb(st, "mv", [128, 4], F32) for _ in range(2)]
        w["h32"] = [self.sb(st, "h32", [128, KT, 128], F32) for _ in range(2)]
        w["id"] = self.uid
        return w

    def ln_xin(self, w, tt):
        return w["xin"][tt % 2], ("xin", w["id"], tt % 2)

    def ln_tile(self, w, tt, gbc, bbc, dst_dram, router, extra=None):
        nc, S = self.nc, self.S
        s = tt % 2
        wid = w["id"]
        xin, kx = w["xin"][s], ("xin", wid, s)
        hh, kh = w["hh"][s], ("hh", wid, s)
        bst, kb = w["bst"][s], ("bst", wid, s)
        mv, km = w["mv"][s], ("mv", wid, s)
        h32, k32 = w["h32"][s], ("h32", wid, s)
        for c in range(2):
            S.op("dve", lambda c=c: nc.vector.bn_stats(out=bst[:, c, :], in_=xin[:, c * 512:(c + 1) * 512]),
                 r=[kx], w=[kb])
        S.op("dve", lambda: nc.vector.bn_aggr(out=mv[:, 0:2], in_=bst[:].rearrange("p a b -> p (a b)")),
             r=[kb], w=[km])
        if CUT < 2:
            return
        S.op("act", lambda: nc.scalar.activation(out=mv[:, 2:3], in_=mv[:, 1:2], func=AF.Sqrt,
                                                 bias=self.epsc[:, 0:1], scale=1.0), r=[km, "epsc"], w=[km])
        S.op("dve", lambda: nc.vector.reciprocal(out=mv[:, 3:4], in_=mv[:, 2:3]), r=[km], w=[km])
        S.op("dve", lambda: nc.vector.tensor_scalar(out=xin[:], in0=xin[:], scalar1=mv[:, 0:1], scalar2=mv[:, 3:4],
                                                    op0=ALU.subtract, op1=ALU.mult), r=[kx, km], w=[kx])
        if CUT < 3:
            return
        S.op("pool", lambda: nc.gpsimd.tensor_tensor(out=hh[:], in0=xin[:], in1=gbc[:], op=ALU.mult),
             r=[kx, "gbc"], w=[kh])
        S.op("pool", lambda: nc.gpsimd.tensor_tensor(out=hh[:], in0=hh[:], in1=bbc[:], op=ALU.add),
             r=[kh, "bbc"], w=[kh])
        S.dma("sp", dst_dram[tt * 128:(tt + 1) * 128, :], hh[:], r=[kh], w=[("hd", tt)])
        if extra is not None:
            S.dma("sp", extra[tt * 128:(tt + 1) * 128, :], hh[:], r=[kh], w=[("hdx", tt)])
        if CUT < 4:
            return
        for half in range(2):
            pb, kp = self.psum()
            for j in range(4):
                kt = half * 4 + j
                S.op("pe", lambda j=j, kt=kt: nc.tensor.transpose(
                    out=pb[:, j * 128:(j + 1) * 128], in_=hh[:, kt * 128:(kt + 1) * 128], identity=self.ident32[:]),
                    r=[kh, "ident32"], w=[kp])
            if os.environ.get("EVAC", "act") == "act":
                S.op("act", lambda half=half, pb=pb: nc.scalar.activation(
                    out=self.hT[:, half * 4:(half + 1) * 4, tt * 128:(tt + 1) * 128],
                    in_=pb[:].rearrange("p (a b) -> p a b", a=4), func=AF.Copy), r=[kp], w=[("hT", tt), kp])
            else:
                S.op("dve", lambda half=half, pb=pb: nc.vector.tensor_copy(
                    out=self.hT[:, half * 4:(half + 1) * 4, tt * 128:(tt + 1) * 128],
                    in_=pb[:].rearrange("p (a b) -> p a b", a=4)), r=[kp], w=[("hT", tt), kp])
            if router:
                S.op("dve", lambda half=half, pb=pb: nc.vector.tensor_copy(
                    out=h32[:, half * 4:(half + 1) * 4, :], in_=pb[:].rearrange("p (a b) -> p a b", a=4)),
                    r=[kp], w=[k32, kp])
        if router and CUT >= 5:
            pb, kp = self.psum()
            for kt in range(KT):
                S.op("pe", lambda kt=kt: nc.tensor.matmul(pb[:, 0:NE], lhsT=h32[:, kt, :], rhs=self.rw32[:, kt, :],
                                                          start=(kt == 0), stop=(kt == KT - 1)),
                     r=[k32, "rw32"], w=[kp])
            S.op("dve", lambda: nc.vector.tensor_copy(out=self.logits[:, tt, :], in_=pb[:, 0:NE]),
                 r=[kp], w=["logits"])

    def router(self, st):
        nc, S = self.nc, self.S
        V = nc.vector
        L = self.logits
        t1 = self.sb(st, "rt1", [128, NT, NE], F32)
        probs = self.sb(st, "probs", [128, NT, NE], F32)
        sel = self.sb(st, "sel", [128, NT, NE], F32)
        p6 = self.sb(st, "p6", [128, NT, 4, 6], F32)
        gs = self.sb(st, "gs", [128, NT, 4], F32)
        gm = self.sb(st, "gm", [128, NT, 4], F32)
        gt = self.sb(st, "gt", [128, NT, 4], F32)
        red = self.sb(st, "red", [128, NT], F32)
        red2 = self.sb(st, "red2", [128, NT], F32)
        msk = self.sb(st, "msk", [128, NT, NE], F32)
        eq = self.sb(st, "eq", [128, NT, NE], F32)
        BIG = 1.0e9

        def bc(a):
            return a[:].unsqueeze(2).to_broadcast([128, NT, NE])

        S.op("dve", lambda: V.tensor_reduce(out=red[:], in_=L[:], axis=AX.X, op=ALU.max), r=["logits"], w=["red"])
        S.op("dve", lambda: V.tensor_tensor(out=t1[:], in0=L[:], in1=bc(red), op=ALU.subtract),
             r=["logits", "red"], w=["rt1"])
        S.op("act", lambda: nc.scalar.activation(out=t1[:], in_=t1[:], func=AF.Exp), r=["rt1"], w=["rt1"])
        S.op("dve", lambda: V.tensor_reduce(out=red[:], in_=t1[:], axis=AX.X, op=ALU.add), r=["rt1"], w=["red"])
        S.op("dve", lambda: V.reciprocal(out=red[:], in_=red[:]), r=["red"], w=["red"])
        S.op("dve", lambda: V.tensor_tensor(out=probs[:], in0=t1[:], in1=bc(red), op=ALU.mult),
             r=["rt1", "red"], w=["probs"])
        S.op("dve", lambda: V.tensor_tensor(out=sel[:], in0=probs[:],
                                            in1=self.rbias[:].unsqueeze(1).to_broadcast([128, NT, NE]), op=ALU.add),
             r=["probs", "rbias"], w=["sel"])
        s4 = sel[:].rearrange("p t (g e) -> p t g e", g=4)
        S.op("dve", lambda: V.tensor_tensor(out=p6[:, :, :, 0:3], in0=s4[:, :, :, 0:3], in1=s4[:, :, :, 1:4],
                                            op=ALU.add), r=["sel"], w=["p6"])
        S.op("dve", lambda: V.tensor_tensor(out=p6[:, :, :, 3:5], in0=s4[:, :, :, 0:2], in1=s4[:, :, :, 2:4],
                                            op=ALU.add), r=["sel"], w=["p6"])
        S.op("dve", lambda: V.tensor_tensor(out=p6[:, :, :, 5:6], in0=s4[:, :, :, 0:1], in1=s4[:, :, :, 3:4],
                                            op=ALU.add), r=["sel"], w=["p6"])
        S.op("dve", lambda: V.tensor_reduce(out=gs[:], in_=p6[:], axis=AX.X, op=ALU.max), r=["p6"], w=["gs"])
        S.op("dve", lambda: V.tensor_reduce(out=red[:], in_=gs[:], axis=AX.X, op=ALU.max), r=["gs"], w=["red"])
        S.op("dve", lambda: V.tensor_tensor(out=gm[:], in0=gs[:], in1=red[:].unsqueeze(2).to_broadcast([128, NT, 4]),
                                            op=ALU.is_ge), r=["gs", "red"], w=["gm"])
        S.op("dve", lambda: V.tensor_scalar(out=gt[:], in0=gm[:], scalar1=BIG, scalar2=-BIG, op0=ALU.mult,
                                            op1=ALU.add), r=["gm"], w=["gt"])
        m4 = msk[:].rearrange("p t (g e) -> p t g e", g=4)
        S.op("dve", lambda: V.tensor_tensor(out=m4, in0=s4, in1=gm[:].unsqueeze(3).to_broadcast([128, NT, 4, 4]),
                                            op=ALU.mult), r=["sel", "gm"], w=["msk"])
        S.op("dve", lambda: V.tensor_tensor(out=m4, in0=m4, in1=gt[:].unsqueeze(3).to_broadcast([128, NT, 4, 4]),
                                            op=ALU.add), r=["msk", "gt"], w=["msk"])
        S.op("dve", lambda: V.tensor_reduce(out=red[:], in_=msk[:], axis=AX.X, op=ALU.max), r=["msk"], w=["red"])
        S.op("dve", lambda: V.tensor_tensor(out=eq[:], in0=msk[:], in1=bc(red), op=ALU.is_equal),
             r=["msk", "red"], w=["eq"])
        S.op("dve", lambda: V.scalar_tensor_tensor(out=eq[:], in0=eq[:], scalar=-BIG, in1=msk[:], op0=ALU.mult,
                                                   op1=ALU.add), r=["eq", "msk"], w=["eq"])
        S.op("dve", lambda: V.tensor_reduce(out=red2[:], in_=eq[:], axis=AX.X, op=ALU.max), r=["eq"], w=["red2"])
        S.op("dve", lambda: V.tensor_tensor(out=eq[:], in0=msk[:], in1=bc(red2), op=ALU.is_ge),
             r=["msk", "red2"], w=["eq"])
        S.op("dve", lambda: V.tensor_tensor(out=eq[:], in0=eq[:], in1=probs[:], op=ALU.mult),
             r=["eq", "probs"], w=["eq"])
        S.op("dve", lambda: V.tensor_reduce(out=red[:], in_=eq[:], axis=AX.X, op=ALU.add), r=["eq"], w=["red"])
        S.op("dve", lambda: V.reciprocal(out=red[:], in_=red[:]), r=["red"], w=["red"])
        S.op("dve", lambda: V.tensor_tensor(out=self.gates[:], in0=eq[:], in1=bc(red), op=ALU.mult),
             r=["eq", "red"], w=["gates"])

    def moe(self, l, last):
        nc, S = self.nc, self.S
        wg_d, wu_d, wd_d = self.P["exp_w_gate"], self.P["exp_w_up"], self.P["exp_w_down"]
        with contextlib.ExitStack() as st:
            self.router(st)
            self.dbg_dump("gates%d" % l, lambda o: S.dma("sp", o, self.gates[:], r=["gates"]))
            acc = self.sb(st, "acc", [128, NT, D], F32)
            wg = [self.sb(st, "wg", [128, KT, DEXP], BF16) for _ in range(2)]
            wu = [self.sb(st, "wu", [128, KT, DEXP], BF16) for _ in range(2)]
            wd = [self.sb(st, "wd", [128, 4, D], BF16) for _ in range(2)]
            hg = [self.sb(st, "hg", [128, 4, 512], BF16) for _ in range(2)]
            sg = [self.sb(st, "sg", [128, 512], BF16) for _ in range(2)]
            gbc = self.sb(st, "gbc2", [128, D], F32)
            bbc = self.sb(st, "bbc2", [128, D], F32)
            S.dma("sp", gbc[:], self.P["ln2_g"][l].partition_broadcast(128), w=["gbc"])
            S.dma("sp", bbc[:], self.P["ln2_b"][l].partition_broadcast(128), w=["bbc"])

            def load_w(e):
                b = e % 2
                S.dma("pool", wg[b][:], wg_d[l, e].rearrange("(kt p) n -> p kt n", p=128), w=[("wg", b)])
                S.dma("pool", wu[b][:], wu_d[l, e].rearrange("(kt p) n -> p kt n", p=128), w=[("wu", b)])
                S.dma("pool", wd[b][:], wd_d[l, e].rearrange("(kt p) n -> p kt n", p=128), w=[("wd", b)])

            items = [(e, q) for e in range(int(os.environ.get('ME', NE))) for q in range(4)]
            sgi = [0]

            def G(i):
                e, q = items[i]
                b = e % 2
                hb = i % 2
                for dt_ in range(4):
                    pa, ka = self.psum()
                    pu, ku = self.psum()
                    for kt in range(KT):
                        S.op("pe", lambda kt=kt, pa=pa: nc.tensor.matmul(
                            pa[:], lhsT=wg[b][:, kt, dt_ * 128:(dt_ + 1) * 128], rhs=self.hT[:, kt, q * 512:(q + 1) * 512],
                            start=(kt == 0), stop=(kt == KT - 1)),
                            r=[("wg", b)] + [("hT", q * 4 + j) for j in range(4)], w=[ka])
                    for kt in range(KT):
                        S.op("pe", lambda kt=kt, pu=pu: nc.tensor.matmul(
                            pu[:], lhsT=wu[b][:, kt, dt_ * 128:(dt_ + 1) * 128], rhs=self.hT[:, kt, q * 512:(q + 1) * 512],
                            start=(kt == 0), stop=(kt == KT - 1)),
                            r=[("wu", b)] + [("hT", q * 4 + j) for j in range(4)], w=[ku])
                    si = sgi[0] % 2
                    sgi[0] += 1
                    S.op("act", lambda pa=pa, si=si: nc.scalar.activation(out=sg[si][:], in_=pa[:], func=AF.Silu),
                         r=[ka], w=[("sg", si)])
                    S.op("dve", lambda pu=pu, si=si: nc.vector.tensor_tensor(
                        out=hg[hb][:, dt_, :], in0=pu[:], in1=sg[si][:], op=ALU.mult),
                        r=[ku, ("sg", si)], w=[("hg", hb)])

            def Dn(i):
                e, q = items[i]
                b = e % 2
                hb = i % 2
                for j in range(4):
                    tt = q * 4 + j
                    for half in range(2):
                        pc, kc = self.psum()
                        for dt_ in range(4):
                            S.op("pe", lambda dt_=dt_, pc=pc: nc.tensor.matmul(
                                pc[:], lhsT=hg[hb][:, dt_, j * 128:(j + 1) * 128],
                                rhs=wd[b][:, dt_, half * 512:(half + 1) * 512], start=(dt_ == 0), stop=(dt_ == 3)),
                                r=[("hg", hb), ("wd", b)], w=[kc])
                        dst = acc[:, tt, half * 512:(half + 1) * 512]
                        if e == 0:
                            S.op("dve", lambda pc=pc, dst=dst: nc.vector.tensor_scalar(
                                out=dst, in0=pc[:], scalar1=self.gates[:, tt, e:e + 1], scalar2=None, op0=ALU.mult),
                                r=[kc, "gates"], w=[("acc", tt)])
                        else:
                            S.op("dve", lambda pc=pc, dst=dst: nc.vector.scalar_tensor_tensor(
                                out=dst, in0=pc[:], scalar=self.gates[:, tt, e:e + 1], in1=dst, op0=ALU.mult,
                                op1=ALU.add), r=[kc, "gates", ("acc", tt)], w=[("acc", tt)])

            load_w(0)
            for i in range(len(items)):
                e, q = items[i]
                G(i)
                if i >= 1:
                    Dn(i - 1)
                if q == 0 and e + 1 < int(os.environ.get('ME', NE)):
                    load_w(e + 1)
            Dn(len(items) - 1)
            self.dbg_dump("moe%d" % l, lambda o: S.dma("sp", o.rearrange("(t p) d -> p t d", p=128), acc[:],
                                                       r=[("acc", t) for t in range(NT)]))
            lnw = self.ln_alloc(st)
            h1 = [self.sb(st, "h1t", [128, D], F32) for _ in range(2)]
            dst = self.out if last else self.h_dram
            for tt in range(NT):
                s = tt % 2
                S.dma("sp", h1[s][:], self.h_dram[tt * 128:(tt + 1) * 128, :], r=[("hd", tt)], w=[("h1t", s)])
                xin, kx = self.ln_xin(lnw, tt)
                S.op("dve", lambda s=s, xin=xin: nc.vector.scalar_tensor_tensor(
                    out=xin[:], in0=h1[s][:], scalar=ALPHA, in1=acc[:, tt, :], op0=ALU.mult, op1=ALU.add),
                    r=[("h1t", s), ("acc", tt)], w=[kx])
                self.ln_tile(lnw, tt, gbc, bbc, dst, router=False)
            S.barrier()


    def hgrn(self, l):
        nc, S = self.nc, self.S
        V, A, G, PE = nc.vector, nc.scalar, nc.gpsimd, nc.tensor
        Wl = self.P["w_in"][l]
        ydst = self.yT_dram[2]
        with contextlib.ExitStack() as st:
            mask2 = self.sb(st, "mask2", [128, 128], F32)
            rm = self.sb(st, "rm", [128, T], F32)
            nw = self.sb(st, "nw", [128, 1], F32)
            lbt = self.sb(st, "lbt", [128, 8, 2], F32)
            lbv = self.sb(st, "lbv", [128, 8], F32)
            oml = self.sb(st, "oml", [128, 8], F32)
            S.op("pool", lambda: G.affine_select(out=mask2[:], in_=self.ones[:], pattern=[[1, 128]],
                                                 compare_op=ALU.is_ge, fill=0.0, base=0, channel_multiplier=-1),
                 r=["ones"], w=["mask2"])
            S.op("pool", lambda: G.memset(mask2[0:64, 64:128], 0.0), w=["mask2"])
            S.op("pool", lambda: G.memset(rm[:], 1.0), w=["rm"])
            S.op("pool", lambda: G.memset(rm[:].rearrange("p (c j) -> p c j", j=64)[:, :, 0:1], 0.0), w=["rm"])
            S.dma("sp", nw[:], self.P["hgrn_norm_w"][l].rearrange("(p o) -> p o", o=1), w=["nw"])
            if l == 0:
                S.op("pool", lambda: G.memset(lbv[:], 0.0), w=["lbv"])
                S.op("pool", lambda: G.memset(oml[:], 1.0), w=["oml"])
            else:
                for j in range(2):
                    S.dma("sp", lbt[:, :, j:j + 1],
                          self.P["hgrn_lb"][j].rearrange("(h p o) -> p h o", p=128, o=1), w=["lbt"])
                S.op("dve", lambda: V.tensor_tensor(out=lbv[:], in0=lbt[:, :, 1], in1=lbt[:, :, 0], op=ALU.subtract),
                     r=["lbt"], w=["lbv"])
                S.op("act", lambda: A.activation(out=lbv[:], in_=lbv[:], func=AF.Sigmoid), r=["lbv"], w=["lbv"])
                S.op("dve", lambda: V.tensor_scalar(out=oml[:], in0=lbv[:], scalar1=-1.0, scalar2=1.0, op0=ALU.mult,
                                                    op1=ALU.add), r=["lbv"], w=["oml"])
            w4 = [self.sb(st, "w4", [128, 4, KT, 128], BF16) for _ in range(2)]
            qs = self.sb(st, "qs", [128, T], F32)
            fs = self.sb(st, "fs", [128, T], F32)
            lf = self.sb(st, "lf", [128, T], F32)
            bc = self.sb(st, "bc", [128, T], F32)
            eb = self.sb(st, "eb", [128, T], F32)
            enb = self.sb(st, "enb", [128, T], F32)
            qb = self.sb(st, "qb", [128, T], BF16)
            kb = self.sb(st, "kb", [128, T], BF16)
            gs = self.sb(st, "gs", [128, T], BF16)
            yt = self.sb(st, "yt", [128, T], BF16)
            v = self.sb(st, "v", [128, NT, 128], BF16)
            kbt = self.sb(st, "kbt", [128, NT, 128], BF16)
            kbtB = self.sb(st, "kbtB", [128, NT, 128], BF16)
            mAB = self.sb(st, "mAB", [128, 2], F32)
            S.op("pool", lambda: G.memset(mAB[0:64, 0:1], 1.0), w=["mAB"])
            S.op("pool", lambda: G.memset(mAB[64:128, 0:1], 0.0), w=["mAB"])
            S.op("pool", lambda: G.memset(mAB[0:64, 1:2], 0.0), w=["mAB"])
            S.op("pool", lambda: G.memset(mAB[64:128, 1:2], 1.0), w=["mAB"])
            S32 = self.sb(st, "S32", [128, 128], F32)
            Sbf = [self.sb(st, "Sbf", [128, 128], BF16) for _ in range(4)]
            attm = [self.sb(st, "attm", [128, 128], BF16) for _ in range(2)]
            osb = [self.sb(st, "osb", [128, 128], F32) for _ in range(2)]
            osq = [self.sb(st, "osq", [128, 128], BF16) for _ in range(2)]
            sd = [self.sb(st, "sd", [128, 128], F32) for _ in range(2)]
            hTk = [("hT", t) for t in range(NT)]

            def load_w(h):
                b = h % 2
                for j in range(4):
                    c0 = OFF_HGRN + j * 1024 + h * 128
                    S.dma("pool", w4[b][:, j], Wl[:, c0:c0 + 128].rearrange("(kt p) n -> p kt n", p=128),
                          w=[("w4", b)])

            if HC >= 2:
                load_w(0)
            for h in range(8 if HC >= 6 else (1 if HC >= 2 else 0)):
                b = h % 2
                if h + 1 < 8 and HC >= 6:
                    load_w(h + 1)
                for (j, func, dst, kd) in ((0, AF.Silu, qs, "qs"), (1, AF.Sigmoid, fs, "fs"), (3, AF.Sigmoid, gs, "gs")):
                    for tq in range(4):
                        pb, kp = self.psum()
                        for kt in range(KT):
                            S.op("pe", lambda kt=kt, pb=pb, j=j, tq=tq: PE.matmul(
                                pb[:], lhsT=w4[b][:, j, kt, :], rhs=self.hT[:, kt, tq * 512:(tq + 1) * 512],
                                start=(kt == 0), stop=(kt == KT - 1)), r=[("w4", b)] + hTk[tq * 4:tq * 4 + 4], w=[kp])
                        S.op("act", lambda pb=pb, dst=dst, func=func, tq=tq: A.activation(
                            out=dst[:, tq * 512:(tq + 1) * 512], in_=pb[:], func=func), r=[kp], w=[kd, kp])
                for t4 in range(4):
                    pb, kp = self.psum()
                    for j4 in range(4):
                        tt = t4 * 4 + j4
                        for kt in range(KT):
                            S.op("pe", lambda kt=kt, pb=pb, j4=j4, tt=tt: PE.matmul(
                                pb[:, j4 * 128:(j4 + 1) * 128], lhsT=self.hT[:, kt, tt * 128:(tt + 1) * 128],
                                rhs=w4[b][:, 2, kt, :], start=(kt == 0), stop=(kt == KT - 1)),
                                r=[("w4", b), ("hT", tt)], w=[kp])
                    S.op("dve", lambda pb=pb, t4=t4: V.tensor_copy(
                        out=v[:, t4 * 4:(t4 + 1) * 4, :], in_=pb[:].rearrange("p (a b) -> p a b", a=4)),
                        r=[kp], w=["v", kp])
                if HC < 3:
                    continue
                S.op("dve", lambda: V.tensor_scalar(out=fs[:], in0=fs[:], scalar1=oml[:, h:h + 1], scalar2=lbv[:, h:h + 1],
                                                    op0=ALU.mult, op1=ALU.add), r=["fs", "oml", "lbv"], w=["fs"])
                S.op("act", lambda: A.activation(out=lf[:], in_=fs[:], func=AF.Ln), r=["fs"], w=["lf"])
                S.op("dve", lambda: V.tensor_tensor_scan(out=bc[:], data0=rm[:], data1=lf[:], initial=0.0,
                                                         op0=ALU.mult, op1=ALU.add), r=["rm", "lf"], w=["bc"])
                S.op("act", lambda: A.activation(out=eb[:], in_=bc[:], func=AF.Exp), r=["bc"], w=["eb"])
                S.op("act", lambda: A.activation(out=enb[:], in_=bc[:], func=AF.Exp, scale=-1.0), r=["bc"], w=["enb"])
                S.op("dve", lambda: V.tensor_scalar(out=fs[:], in0=fs[:], scalar1=-1.0, scalar2=1.0, op0=ALU.mult,
                                                    op1=ALU.add), r=["fs", "lf"], w=["fs"])
                S.op("pool", lambda: G.tensor_tensor(out=qb[:], in0=qs[:], in1=eb[:], op=ALU.mult),
                     r=["qs", "eb"], w=["qb"])
                S.op("dve", lambda: V.tensor_tensor(out=kb[:], in0=fs[:], in1=enb[:], op=ALU.mult),
                     r=["fs", "enb"], w=["kb"])
                if HC < 4:
                    continue
                for t4 in range(4):
                    pb, kp = self.psum()
                    pbv = pb[:].bitcast(BF16)
                    for j4 in range(4):
                        tt = t4 * 4 + j4
                        S.op("pe", lambda pbv=pbv, j4=j4, tt=tt: PE.transpose(
                            out=pbv[:, j4 * 128:(j4 + 1) * 128], in_=kb[:, tt * 128:(tt + 1) * 128],
                            identity=self.identbf[:]), r=["kb", "identbf"], w=[kp])
                    S.op("dve", lambda pbv=pbv, t4=t4: V.tensor_scalar(
                        out=kbt[:, t4 * 4:(t4 + 1) * 4, :], in0=pbv[:, 0:512].rearrange("p (a b) -> p a b", a=4),
                        scalar1=mAB[:, 0:1], scalar2=None, op0=ALU.mult), r=[kp, "mAB"], w=["kbt", kp])
                    S.op("dve", lambda pbv=pbv, t4=t4: V.tensor_scalar(
                        out=kbtB[:, t4 * 4:(t4 + 1) * 4, :], in0=pbv[:, 0:512].rearrange("p (a b) -> p a b", a=4),
                        scalar1=mAB[:, 1:2], scalar2=None, op0=ALU.mult), r=[kp, "mAB"], w=["kbtB", kp])
                if HC < 5:
                    continue
                S.op("pool", lambda: G.memset(S32[:], 0.0), w=["S32"])
                S.op("pool", lambda: G.memset(Sbf[0][:], 0.0), w=[("Sbf", 0)])
                for tt in range(NT):
                    cA, cB = 2 * tt, 2 * tt + 1
                    tsl = slice(tt * 128, (tt + 1) * 128)
                    i2 = tt % 2
                    pa, kpa = self.psum()
                    S.op("pe", lambda pa=pa: PE.matmul(pa[:, 0:128], lhsT=kb[:, tsl], rhs=qb[:, tsl], start=True, stop=True),
                         r=["kb", "qb"], w=[kpa])
                    S.op("dve", lambda pa=pa: V.tensor_tensor(out=attm[i2][:], in0=pa[:, 0:128], in1=mask2[:], op=ALU.mult),
                         r=[kpa, "mask2"], w=[("attm", i2), kpa])
                    if HL < 2:
                        continue
                    pr, kpr = self.psum()
                    S.op("pe", lambda pr=pr: PE.matmul(pr[:, 0:128], lhsT=kbt[:, tt, :], rhs=v[:, tt, :],
                                                       start=True, stop=True), r=["kbt", "v"], w=[kpr])
                    S.op("pe", lambda pr=pr: PE.matmul(pr[:, 128:256], lhsT=kbtB[:, tt, :], rhs=v[:, tt, :],
                                                       start=True, stop=True), r=["kbtB", "v"], w=[kpr])
                    for (ci, off) in ((cA, 0), (cB, 128)):
                        S.op("dve", lambda pr=pr, off=off: V.tensor_tensor(
                            out=S32[:], in0=pr[:, off:off + 128], in1=S32[:], op=ALU.add), r=[kpr, "S32"], w=["S32", kpr])
                        S.op("dve", lambda ci=ci: V.tensor_scalar(
                            out=S32[:], in0=S32[:], scalar1=eb[:, ci * 64 + 63:ci * 64 + 64], scalar2=None, op0=ALU.mult),
                            r=["S32", "eb"], w=["S32"])
                        S.op("pool", lambda ci=ci: G.tensor_copy(out=Sbf[(ci + 1) % 4][:], in_=S32[:]),
                             r=["S32"], w=[("Sbf", (ci + 1) % 4)])
                    if HL < 3:
                        continue
                    po, kpo = self.psum()
                    S.op("pe", lambda po=po: PE.matmul(po[:, 0:128], lhsT=v[:, tt, :], rhs=attm[i2][:], start=True, stop=False),
                         r=["v", ("attm", i2)], w=[kpo])
                    S.op("pe", lambda po=po: PE.matmul(po[:, 0:64], lhsT=Sbf[cA % 4][:], rhs=qb[:, tt * 128:tt * 128 + 64],
                                                       start=False, stop=False), r=[("Sbf", cA % 4), "qb"], w=[kpo])
                    S.op("pe", lambda po=po: PE.matmul(po[:, 64:128], lhsT=Sbf[cB % 4][:],
                                                       rhs=qb[:, tt * 128 + 64:(tt + 1) * 128], start=False, stop=True),
                         r=[("Sbf", cB % 4), "qb"], w=[kpo])
                    if HL < 4:
                        continue
                    S.op("act", lambda po=po: A.activation(out=osb[i2][:], in_=po[:, 0:128], func=AF.Copy),
                         r=[kpo], w=[("osb", i2), kpo])
                    S.op("act", lambda po=po: A.activation(out=osq[i2][:], in_=po[:, 0:128], func=AF.Square),
                         r=[kpo], w=[("osq", i2), kpo])
                    if HL < 5:
                        continue
                    pss, kps = self.psum()
                    S.op("pe", lambda pss=pss: PE.matmul(pss[:, 0:128], lhsT=self.onesbf[:], rhs=osq[i2][:], start=True,
                                                         stop=True), r=["onesbf", ("osq", i2)], w=[kps])
                    S.op("act", lambda pss=pss: A.activation(out=sd[i2][:], in_=pss[:, 0:128], func=AF.Sqrt,
                                                             bias=self.epsc[:, 1:2], scale=1.0 / 128.0),
                         r=[kps, "epsc"], w=[("sd", i2), kps])
                    S.op("dve", lambda: V.reciprocal(out=sd[i2][:], in_=sd[i2][:]), r=[("sd", i2)], w=[("sd", i2)])
                    S.op("dve", lambda: V.scalar_tensor_tensor(out=osb[i2][:], in0=osb[i2][:], scalar=nw[:, 0:1],
                                                               in1=sd[i2][:], op0=ALU.mult, op1=ALU.mult),
                         r=[("osb", i2), ("sd", i2), "nw"], w=[("osb", i2)])
                    S.op("dve", lambda: V.tensor_tensor(out=yt[:, tsl], in0=osb[i2][:], in1=gs[:, tsl], op=ALU.mult),
                         r=[("osb", i2), "gs"], w=["yt"])
                S.dma("sp", ydst[h * 128:(h + 1) * 128, :], yt[:], r=["yt"], w=[("yT2", h)])
            S.barrier()


    def ssd(self, l):
        nc, S = self.nc, self.S
        V, A, G, PE = nc.vector, nc.scalar, nc.gpsimd, nc.tensor
        Wl = self.P["w_in"][l]
        ydst = self.yT_dram[0]
        NEG = -30000.0
        hTk = [("hT", t) for t in range(NT)]
        with contextlib.ExitStack() as st:
            tri2 = self.sb(st, "tri2", [128, 128], F32)
            same2 = self.sb(st, "same2", [128, 128], F32)
            indA = self.sb(st, "indA", [128, 128], F32)
            indB = self.sb(st, "indB", [128, 128], F32)
            mAB = self.sb(st, "mABs", [128, 2], F32)
            negmask = self.sb(st, "negmask", [128, 8, 128], F32)
            bd1 = self.sb(st, "bd", [16, 8, 128], F32)
            bd = [bd1, bd1]
            S.op("pool", lambda: G.affine_select(out=tri2[:], in_=self.ones[:], pattern=[[1, 128]], compare_op=ALU.is_ge,
                                                 fill=0.0, base=0, channel_multiplier=-1), r=["ones"], w=["tri2"])
            S.op("pool", lambda: G.memset(tri2[0:64, 64:128], 0.0), w=["tri2"])
            S.op("pool", lambda: G.memset(same2[:], 0.0), w=["same2"])
            S.op("pool", lambda: G.memset(same2[0:64, 0:64], 1.0), w=["same2"])
            S.op("pool", lambda: G.memset(same2[64:128, 64:128], 1.0), w=["same2"])
            S.op("pool", lambda: G.memset(indA[0:64, :], 1.0), w=["indA"])
            S.op("pool", lambda: G.memset(indA[64:128, :], 0.0), w=["indA"])
            S.op("pool", lambda: G.memset(indB[0:64, :], 0.0), w=["indB"])
            S.op("pool", lambda: G.memset(indB[64:128, :], 1.0), w=["indB"])
            S.op("pool", lambda: G.memset(mAB[0:64, 0:1], 1.0), w=["mAB"])
            S.op("pool", lambda: G.memset(mAB[64:128, 0:1], 0.0), w=["mAB"])
            S.op("pool", lambda: G.memset(mAB[0:64, 1:2], 0.0), w=["mAB"])
            S.op("pool", lambda: G.memset(mAB[64:128, 1:2], 1.0), w=["mAB"])
            S.op("pool", lambda: G.memset(negmask[:], 0.0), w=["negmask"])
            S.op("pool", lambda: G.affine_select(out=negmask[:], in_=negmask[:], pattern=[[0, 8], [1, 128]],
                                                 compare_op=ALU.is_ge, fill=NEG, base=0, channel_multiplier=-1),
                 r=["negmask"], w=["negmask"])
            S.op("pool", lambda: G.memset(negmask[0:64, :, 64:128], NEG), w=["negmask"])
            dtb = self.sb(st, "dtb", [128, 16], F32)
            alog = self.sb(st, "alog", [128, 16], F32)
            dsk = self.sb(st, "dsk", [128, 16], F32)
            nwbc = self.sb(st, "nwbc", [128, D], F32)
            S.dma("sp", dtb[:], self.P["ssd_dt_bias"][l].partition_broadcast(128), w=["dtb"])
            S.dma("sp", alog[:], self.P["ssd_a_log"][l].partition_broadcast(128), w=["alog"])
            S.dma("sp", dsk[:], self.P["ssd_d"][l].partition_broadcast(128), w=["dsk"])
            S.dma("sp", nwbc[:], self.P["ssd_norm_w"][l].partition_broadcast(128), w=["nwbc"])
            S.op("act", lambda: A.activation(out=alog[:], in_=alog[:], func=AF.Exp), r=["alog"], w=["alog"])
            S.op("dve", lambda: V.tensor_scalar(out=alog[:], in0=alog[:], scalar1=-1.0, scalar2=None, op0=ALU.mult),
                 r=["alog"], w=["alog"])
            wdt = self.sb(st, "wdt", [128, KT, 16], BF16)
            S.dma("pool", wdt[:], Wl[:, OFF_DT:OFF_DT + 16].rearrange("(kt p) n -> p kt n", p=128), w=["wdt"])
            dt = self.sb(st, "dt", [128, NT, 16], F32)
            da = self.sb(st, "da", [128, NT, 16], F32)
            cum4 = self.sb(st, "cum4", [128, NT, 4, 16], F32)
            eacs = self.sb(st, "eacs", [128, NT, 16], F32)
            eend = self.sb(st, "eend", [128, NT, 16], F32)
            edec = self.sb(st, "edec", [128, NT, 2, 16], F32)
            acsTt = [self.sb(st, "acsTt", [16, 128], F32) for _ in range(2)]
            nacsTt = [self.sb(st, "nacsTt", [16, 128], F32) for _ in range(2)]
            pb, kp = self.psum()
            for tt in range(NT):
                for kt in range(KT):
                    S.op("pe", lambda kt=kt, tt=tt: PE.matmul(pb[:, tt * 16:(tt + 1) * 16], lhsT=self.hT[:, kt, tt * 128:(tt + 1) * 128],
                                                              rhs=wdt[:, kt, :], start=(kt == 0), stop=(kt == KT - 1)),
                         r=["wdt", ("hT", tt)], w=[kp])
            S.op("dve", lambda: V.tensor_tensor(out=dt[:], in0=pb[:, 0:256].rearrange("p (t h) -> p t h", h=16),
                                                in1=dtb[:].unsqueeze(1).to_broadcast([128, NT, 16]), op=ALU.add),
                 r=[kp, "dtb"], w=["dt", kp])
            S.op("act", lambda: A.activation(out=dt[:], in_=dt[:], func=AF.Exp), r=["dt"], w=["dt"])
            S.op("act", lambda: A.activation(out=dt[:], in_=dt[:], func=AF.Ln, bias=self.epsc[:, 3:4], scale=1.0),
                 r=["dt", "epsc"], w=["dt"])
            S.op("dve", lambda: V.tensor_tensor(out=da[:], in0=dt[:], in1=alog[:].unsqueeze(1).to_broadcast([128, NT, 16]),
                                                op=ALU.mult), r=["dt", "alog"], w=["da"])
            for half in range(2):
                pb, kp = self.psum()
                for j in range(8):
                    tt = half * 8 + j
                    for qi, L in enumerate((tri2, same2, indA, indB)):
                        S.op("pe", lambda j=j, qi=qi, L=L, tt=tt, pb=pb: PE.matmul(
                            pb[:, j * 64 + qi * 16:j * 64 + (qi + 1) * 16], lhsT=L[:], rhs=da[:, tt, :], start=True, stop=True),
                            r=["da", "tri2", "same2", "indA", "indB"], w=[kp])
                S.op("dve", lambda pb=pb, half=half: V.tensor_copy(
                    out=cum4[:, half * 8:(half + 1) * 8].rearrange("p t q h -> p (t q h)"), in_=pb[:]),
                    r=[kp], w=["cum4", kp])
            S.op("act", lambda: A.activation(out=eacs[:], in_=cum4[:, :, 0, :], func=AF.Exp), r=["cum4"], w=["eacs"])
            S.op("dve", lambda: V.tensor_tensor(out=eend[:], in0=cum4[:, :, 1, :], in1=cum4[:, :, 0, :], op=ALU.subtract),
                 r=["cum4"], w=["eend"])
            S.op("act", lambda: A.activation(out=eend[:], in_=eend[:], func=AF.Exp), r=["eend"], w=["eend"])
            S.op("act", lambda: A.activation(out=edec[:], in_=cum4[:, :, 2:4, :], func=AF.Exp), r=["cum4"], w=["edec"])
            wx = self.sb(st, "wx", [128, KT, 768], BF16)
            wz = self.sb(st, "wz", [128, KT, 512], BF16)
            cw = self.sb(st, "cw", [128, 6, 4], F32)
            cbi = self.sb(st, "cbi", [128, 6], F32)
            xp1 = self.sb(st, "xp", [128, T + 3], F32)
            xp = [xp1, xp1]
            fTa = [self.sb(st, "fT", [128, T], BF16) for _ in range(4)]
            fT = [fTa[0], fTa[1], fTa[0], fTa[1], fTa[2], fTa[3]]
            fk = [("fT", 0), ("fT", 1), ("fT", 0), ("fT", 1), ("fT", 2), ("fT", 3)]
            cmTA = self.sb(st, "cmTA", [128, T], BF16)
            cmTB = self.sb(st, "cmTB", [128, T], BF16)
            xs = self.sb(st, "xs", [128, NT, 512], BF16)
            xdtt = [self.sb(st, "xdtt", [128, 512], BF16) for _ in range(2)]
            xendt = [self.sb(st, "xendt", [128, 512], BF16) for _ in range(2)]
            bmA = self.sb(st, "bmA", [128, NT, 128], BF16)
            bmB = self.sb(st, "bmB", [128, NT, 128], BF16)
            yTg = self.sb(st, "yTg", [128, 4, T], BF16)
            S32 = self.sb(st, "S32s", [128, 512], F32)
            Sbf = [self.sb(st, "Sbfs", [128, 512], BF16) for _ in range(4)]
            cbs = [self.sb(st, "cbs", [128, 128], BF16) for _ in range(2)]
            Dx = [self.sb(st, "Dx", [16, 8, 128], F32) for _ in range(2)]
            Es = [self.sb(st, "Es", [128, 8, 128], BF16) for _ in range(2)]
            wT = [self.sb(st, "wT", [128, 8, 128], BF16) for _ in range(2)]
            t1 = [self.sb(st, "t1", [128, 512], F32) for _ in range(2)]
            t2 = [self.sb(st, "t2", [128, 512], F32) for _ in range(2)]
            zs = [self.sb(st, "zs", [128, 512], BF16) for _ in range(2)]
            ytm = [self.sb(st, "ytm", [128, 512], BF16) for _ in range(2)]
            ss = [self.sb(st, "ss", [128, 2], F32) for _ in range(2)]
            S.op("pool", lambda: G.memset(xp[0][:, 0:3], 0.0), w=[("xp", 0)])
            for g in range(2):
                S.op("pool", lambda g=g: G.memset(bd[g][:], 1.0), r=[("bd", 0), ("bd", 1)], w=[("bd", 0), ("bd", 1)])
                S.op("pool", lambda g=g: G.affine_select(out=bd[g][:], in_=bd[g][:], pattern=[[1, 8], [0, 128]],
                                                         compare_op=ALU.is_equal, fill=0.0, base=8 * g,
                                                         channel_multiplier=-1), r=[("bd", 0), ("bd", 1)], w=[("bd", 0), ("bd", 1)])
                choff = [g * 512 + i * 128 for i in range(4)] + [1024 + g * 128, 1280 + g * 128]
                S.dma("pool", wx[:, :, 0:512], Wl[:, OFF_XBC + g * 512:OFF_XBC + (g + 1) * 512].rearrange("(kt p) n -> p kt n", p=128), w=["wx"])
                S.dma("pool", wx[:, :, 512:640], Wl[:, OFF_XBC + 1024 + g * 128:OFF_XBC + 1024 + (g + 1) * 128].rearrange("(kt p) n -> p kt n", p=128), w=["wx"])
                S.dma("pool", wx[:, :, 640:768], Wl[:, OFF_XBC + 1280 + g * 128:OFF_XBC + 1280 + (g + 1) * 128].rearrange("(kt p) n -> p kt n", p=128), w=["wx"])
                S.dma("pool", wz[:], Wl[:, OFF_Z + g * 512:OFF_Z + (g + 1) * 512].rearrange("(kt p) n -> p kt n", p=128), w=["wz"])
                for ci in range(6):
                    for j in range(4):
                        S.dma("sp", cw[:, ci, j:j + 1], self.P["ssd_conv_w"][l, j, choff[ci]:choff[ci] + 128].rearrange("(p o) -> p o", o=1), w=["cw"])
                    S.dma("sp", cbi[:, ci:ci + 1], self.P["ssd_conv_b"][l, choff[ci]:choff[ci] + 128].rearrange("(p o) -> p o", o=1), w=["cbi"])
                for ci in range(6):
                    xb = xp[0]
                    kx = ("xp", 0)
                    for tq in range(4):
                        pb, kp = self.psum()
                        for kt in range(KT):
                            S.op("pe", lambda kt=kt, pb=pb, ci=ci, tq=tq: PE.matmul(
                                pb[:], lhsT=wx[:, kt, ci * 128:(ci + 1) * 128], rhs=self.hT[:, kt, tq * 512:(tq + 1) * 512],
                                start=(kt == 0), stop=(kt == KT - 1)), r=["wx"] + hTk[tq * 4:tq * 4 + 4], w=[kp])
                        S.op("act", lambda pb=pb, xb=xb, tq=tq: A.activation(out=xb[:, 3 + tq * 512:3 + (tq + 1) * 512], in_=pb[:],
                                                                            func=AF.Copy), r=[kp], w=[kx, kp])
                    acc = t1[0] if False else None
                    cacc = self.sb(st, "cacc", [128, T], F32) if (g == 0 and ci == 0) else self._cacc
                    self._cacc = cacc
                    S.op("dve", lambda xb=xb, ci=ci, cacc=cacc: V.tensor_scalar(
                        out=cacc[:], in0=xb[:, 3:3 + T], scalar1=cw[:, ci, 3:4], scalar2=cbi[:, ci:ci + 1], op0=ALU.mult,
                        op1=ALU.add), r=[kx, "cw", "cbi"], w=["cacc"])
                    for j in range(3):
                        S.op("dve", lambda xb=xb, ci=ci, j=j, cacc=cacc: V.scalar_tensor_tensor(
                            out=cacc[:], in0=xb[:, j:j + T], scalar=cw[:, ci, j:j + 1], in1=cacc[:], op0=ALU.mult, op1=ALU.add),
                            r=[kx, "cw", "cacc"], w=["cacc"])
                    S.op("act", lambda ci=ci, cacc=cacc: A.activation(out=fT[ci][:], in_=cacc[:], func=AF.Silu),
                         r=["cacc"], w=[fk[ci]])
                    if ci < 5:
                        for t4 in range(4):
                            pb, kp = self.psum()
                            pbv = pb[:].bitcast(BF16)
                            for j in range(4):
                                tt = t4 * 4 + j
                                S.op("pe", lambda j=j, tt=tt, pbv=pbv, ci=ci: PE.transpose(
                                    out=pbv[:, j * 128:(j + 1) * 128], in_=fT[ci][:, tt * 128:(tt + 1) * 128],
                                    identity=self.identbf[:]), r=[fk[ci], "identbf"], w=[kp])
                            src = pbv[:, 0:512].rearrange("p (a b) -> p a b", a=4)
                            if ci < 4:
                                S.op("act", lambda t4=t4, src=src, ci=ci: A.activation(
                                    out=xs[:, t4 * 4:(t4 + 1) * 4, ci * 128:(ci + 1) * 128], in_=src, func=AF.Copy),
                                    r=[kp], w=["xs", kp])
                            else:
                                S.op("dve", lambda t4=t4, src=src: V.tensor_scalar(
                                    out=bmA[:, t4 * 4:(t4 + 1) * 4, :], in0=src, scalar1=mAB[:, 0:1], scalar2=None, op0=ALU.mult),
                                    r=[kp, "mAB"], w=["bmA", kp])
                                S.op("dve", lambda t4=t4, src=src: V.tensor_scalar(
                                    out=bmB[:, t4 * 4:(t4 + 1) * 4, :], in0=src, scalar1=mAB[:, 1:2], scalar2=None, op0=ALU.mult),
                                    r=[kp, "mAB"], w=["bmB", kp])
                bmT, cmT = fT[4], fT[5]
                cv = cmT[:].rearrange("p (t c j) -> p t c j", c=2, j=64)
                cva = cmTA[:].rearrange("p (t c j) -> p t c j", c=2, j=64)
                cvb = cmTB[:].rearrange("p (t c j) -> p t c j", c=2, j=64)
                S.op("pool", lambda: G.tensor_copy(out=cva[:, :, 0, :], in_=cv[:, :, 0, :]), r=[("fT", 3)], w=["cmTA"])
                S.op("pool", lambda: G.memset(cva[:, :, 1, :], 0.0), w=["cmTA"])
                S.op("pool", lambda: G.tensor_copy(out=cvb[:, :, 1, :], in_=cv[:, :, 1, :]), r=[("fT", 3)], w=["cmTB"])
                S.op("pool", lambda: G.memset(cvb[:, :, 0, :], 0.0), w=["cmTB"])
                hs = slice(g * 8, (g + 1) * 8)
                S.op("pool", lambda: G.memset(S32[:], 0.0), w=["S32"])
                S.op("pool", lambda: G.memset(Sbf[0][:], 0.0), w=[("Sbf", 0)])
                for tt in range(NT):
                    i2 = tt % 2
                    tsl = slice(tt * 128, (tt + 1) * 128)
                    cA, cB = 2 * tt, 2 * tt + 1
                    pc, kpc = self.psum()
                    S.op("pe", lambda pc=pc: PE.matmul(pc[:, 0:128], lhsT=bmT[:, tsl], rhs=cmT[:, tsl], start=True, stop=True),
                         r=[("fT", 2), ("fT", 3)], w=[kpc])
                    S.op("act", lambda pc=pc: A.activation(out=cbs[i2][:], in_=pc[:, 0:128], func=AF.Copy),
                         r=[kpc], w=[("cbs", i2), kpc])
                    pq, kpq = self.psum()
                    S.op("pe", lambda pq=pq: PE.matmul(pq[0:16, 0:128], lhsT=da[:, tt, :], rhs=tri2[:], start=True, stop=True),
                         r=["da", "tri2"], w=[kpq])
                    S.op("dve", lambda pq=pq: V.tensor_copy(out=acsTt[i2][:], in_=pq[0:16, 0:128]), r=[kpq], w=[("acsTt", i2), kpq])
                    S.op("dve", lambda pq=pq: V.tensor_scalar(out=nacsTt[i2][:], in0=pq[0:16, 0:128], scalar1=-1.0, scalar2=None,
                                                              op0=ALU.mult), r=[kpq], w=[("nacsTt", i2), kpq])
                    S.op("pool", lambda: G.tensor_tensor(out=Dx[i2][:], in0=bd[g][:],
                                                         in1=acsTt[i2][:].unsqueeze(1).to_broadcast([16, 8, 128]), op=ALU.mult),
                         r=[("bd", g), ("acsTt", i2)], w=[("Dx", i2)])
                    for hh in range(2):
                        pe_, kpe = self.psum()
                        csl = slice(hh * 512, (hh + 1) * 512)
                        S.op("pe", lambda pe_=pe_, csl=csl: PE.matmul(
                            pe_[:], lhsT=self.ones[0:16, :], rhs=Dx[i2][:].rearrange("p h l -> p (h l)")[:, csl],
                            start=True, stop=False), r=["ones", ("Dx", i2)], w=[kpe])
                        S.op("pe", lambda pe_=pe_, csl=csl: PE.matmul(
                            pe_[:], lhsT=nacsTt[i2][:], rhs=bd[g][:].rearrange("p h l -> p (h l)")[:, csl],
                            start=False, stop=False), r=[("nacsTt", i2), ("bd", g)], w=[kpe])
                        S.op("pe", lambda pe_=pe_, csl=csl: PE.matmul(
                            pe_[:], lhsT=self.ident32[:], rhs=negmask[:].rearrange("p h l -> p (h l)")[:, csl],
                            start=False, stop=True), r=["ident32", "negmask"], w=[kpe])
                        S.op("act", lambda pe_=pe_, hh=hh: A.activation(
                            out=Es[i2][:, hh * 4:(hh + 1) * 4, :], in_=pe_[:].rearrange("p (h l) -> p h l", h=4), func=AF.Exp),
                            r=[kpe], w=[("Es", i2), kpe])
                    S.op("dve", lambda: V.tensor_tensor(out=wT[i2][:], in0=Es[i2][:],
                                                        in1=cbs[i2][:].unsqueeze(1).to_broadcast([128, 8, 128]), op=ALU.mult),
                         r=[("Es", i2), ("cbs", i2)], w=[("wT", i2)])
                    pr, kpr = self.psum()
                    pr2, kpr2 = self.psum()
                    S.op("pool", lambda: G.tensor_tensor(out=xdtt[i2][:].rearrange("p (h c) -> p h c", c=64),
                                                         in0=xs[:, tt, :].rearrange("p (h c) -> p h c", c=64),
                                                         in1=dt[:, tt, hs].unsqueeze(2).to_broadcast([128, 8, 64]), op=ALU.mult),
                         r=["xs", "dt"], w=[("xdtt", i2)])
                    S.op("pool", lambda: G.tensor_tensor(out=xendt[i2][:].rearrange("p (h c) -> p h c", c=64),
                                                         in0=xdtt[i2][:].rearrange("p (h c) -> p h c", c=64),
                                                         in1=eend[:, tt, hs].unsqueeze(2).to_broadcast([128, 8, 64]), op=ALU.mult),
                         r=[("xdtt", i2), "eend"], w=[("xendt", i2)])
                    S.op("pe", lambda pr=pr: PE.matmul(pr[:], lhsT=bmA[:, tt, :], rhs=xendt[i2][:], start=True, stop=True),
                         r=["bmA", ("xendt", i2)], w=[kpr])
                    S.op("pe", lambda pr2=pr2: PE.matmul(pr2[:], lhsT=bmB[:, tt, :], rhs=xendt[i2][:], start=True, stop=True),
                         r=["bmB", ("xendt", i2)], w=[kpr2])
                    for (ci_, prx, kprx, cc) in ((cA, pr, kpr, 0), (cB, pr2, kpr2, 1)):
                        S.op("dve", lambda cc=cc: V.tensor_tensor(
                            out=S32[:].rearrange("p (h c) -> p h c", c=64), in0=S32[:].rearrange("p (h c) -> p h c", c=64),
                            in1=edec[:, tt, cc, hs].unsqueeze(2).to_broadcast([128, 8, 64]), op=ALU.mult),
                            r=["S32", "edec"], w=["S32"])
                        S.op("dve", lambda prx=prx: V.tensor_tensor(out=S32[:], in0=prx[:], in1=S32[:], op=ALU.add),
                             r=[kprx, "S32"], w=["S32", kprx])
                        S.op("pool", lambda ci_=ci_: G.tensor_copy(out=Sbf[(ci_ + 1) % 4][:], in_=S32[:]),
                             r=["S32"], w=[("Sbf", (ci_ + 1) % 4)])
                    py, kpy = self.psum()
                    for hh in range(8):
                        S.op("pe", lambda hh=hh, py=py: PE.matmul(py[:, hh * 64:(hh + 1) * 64], lhsT=wT[i2][:, hh, :],
                                                                  rhs=xdtt[i2][:, hh * 64:(hh + 1) * 64], start=True, stop=True),
                             r=[("wT", i2), ("xdtt", i2)], w=[kpy])
                    po, kpo = self.psum()
                    S.op("pe", lambda po=po: PE.matmul(po[:], lhsT=cmTA[:, tsl], rhs=Sbf[cA % 4][:], start=True, stop=False),
                         r=["cmTA", ("Sbf", cA % 4)], w=[kpo])
                    S.op("pe", lambda po=po: PE.matmul(po[:], lhsT=cmTB[:, tsl], rhs=Sbf[cB % 4][:], start=False, stop=True),
                         r=["cmTB", ("Sbf", cB % 4)], w=[kpo])
                    pz, kpz = self.psum()
                    for kt in range(KT):
                        S.op("pe", lambda kt=kt, pz=pz: PE.matmul(pz[:], lhsT=self.hT[:, kt, tsl], rhs=wz[:, kt, :],
                                                                  start=(kt == 0), stop=(kt == KT - 1)), r=["wz", ("hT", tt)], w=[kpz])
                    S.op("act", lambda pz=pz: A.activation(out=zs[i2][:], in_=pz[:], func=AF.Silu), r=[kpz], w=[("zs", i2), kpz])
                    S.op("dve", lambda po=po: V.tensor_tensor(
                        out=t1[i2][:].rearrange("p (h c) -> p h c", c=64), in0=po[:].rearrange("p (h c) -> p h c", c=64),
                        in1=eacs[:, tt, hs].unsqueeze(2).to_broadcast([128, 8, 64]), op=ALU.mult),
                        r=[kpo, "eacs"], w=[("t1", i2), kpo])
                    S.op("dve", lambda py=py: V.tensor_tensor(out=t1[i2][:], in0=py[:], in1=t1[i2][:], op=ALU.add),
                         r=[kpy, ("t1", i2)], w=[("t1", i2), kpy])
                    S.op("pool", lambda: G.tensor_tensor(
                        out=t2[i2][:].rearrange("p (h c) -> p h c", c=64), in0=xs[:, tt, :].rearrange("p (h c) -> p h c", c=64),
                        in1=dsk[:, hs].unsqueeze(2).to_broadcast([128, 8, 64]), op=ALU.mult), r=["xs", "dsk"], w=[("t2", i2)])
                    S.op("pool", lambda: G.tensor_tensor(out=t2[i2][:], in0=t2[i2][:], in1=t1[i2][:], op=ALU.add),
                         r=[("t2", i2), ("t1", i2)], w=[("t2", i2)])
                    S.op("pool", lambda: G.tensor_tensor(out=t2[i2][:], in0=t2[i2][:], in1=zs[i2][:], op=ALU.mult),
                         r=[("t2", i2), ("zs", i2)], w=[("t2", i2)])
                    S.op("act", lambda: A.activation(out=t1[i2][:], in_=t2[i2][:], func=AF.Square, accum_out=ss[i2][:, 0:1]),
                         r=[("t2", i2)], w=[("t1", i2), ("ss", i2)])
                    S.op("act", lambda: A.activation(out=ss[i2][:, 1:2], in_=ss[i2][:, 0:1], func=AF.Sqrt, bias=self.epsc[:, 1:2],
                                                     scale=1.0 / 512.0), r=[("ss", i2), "epsc"], w=[("ss", i2)])
                    S.op("dve", lambda: V.reciprocal(out=ss[i2][:, 1:2], in_=ss[i2][:, 1:2]), r=[("ss", i2)], w=[("ss", i2)])
                    S.op("dve", lambda: V.scalar_tensor_tensor(out=ytm[i2][:], in0=t2[i2][:], scalar=ss[i2][:, 1:2],
                                                               in1=nwbc[:, g * 512:(g + 1) * 512], op0=ALU.mult, op1=ALU.mult),
                         r=[("t2", i2), ("ss", i2), "nwbc"], w=[("ytm", i2)])
                    pt, kpt = self.psum()
                    ptv = pt[:].bitcast(BF16)
                    for i in range(4):
                        S.op("pe", lambda i=i, ptv=ptv: PE.transpose(out=ptv[:, i * 128:(i + 1) * 128],
                                                                     in_=ytm[i2][:, i * 128:(i + 1) * 128], identity=self.identbf[:]),
                             r=[("ytm", i2), "identbf"], w=[kpt])
                    S.op("act", lambda ptv=ptv: A.activation(out=yTg[:, :, tsl], in_=ptv[:, 0:512].rearrange("p (a b) -> p a b", a=4),
                                                             func=AF.Copy), r=[kpt], w=["yTg", kpt])
                for i in range(4):
                    S.dma("sp", ydst[g * 512 + i * 128:g * 512 + (i + 1) * 128, :], yTg[:, i, :], r=["yTg"], w=[("yT0", g * 4 + i)])
            S.barrier()


    def rwkv(self, l):
        nc, S = self.nc, self.S
        V, A, G, PE = nc.vector, nc.scalar, nc.gpsimd, nc.tensor
        Wl = self.P["w_in"][l]
        ydst = self.yT_dram[1]
        hTk = [("hT", t) for t in range(NT)]
        P_ = self.P

        def mm(out, lhsT, rhs, start, stop, r, w):
            S.op("pe", lambda: PE.matmul(out, lhsT=lhsT, rhs=rhs, start=start, stop=stop), r=r, w=w)

        with contextlib.ExitStack() as st:
            mask4 = self.sb(st, "mask4", [128, 4, 128], F32)
            maskL = self.sb(st, "maskL", [128, 2, 128], F32)
            bdm = self.sb(st, "bdm", [128, 128], F32)
            mEO = self.sb(st, "mEO", [128, 2], F32)
            hsel = self.sb(st, "hsel", [128, 2], F32)
            rm = self.sb(st, "rm128", [128, T], BF16)
            c05 = self.sb(st, "c05", [128, 1], F32)
            for j in range(4):
                S.op("pool", lambda j=j: G.affine_select(out=mask4[:, j, :], in_=self.ones[:], pattern=[[1, 128]],
                                                         compare_op=(ALU.is_gt if j % 2 == 0 else ALU.is_ge), fill=0.0,
                                                         base=0, channel_multiplier=-1), r=["ones"], w=["mask4"])
            for j in range(2):
                S.op("pool", lambda j=j: G.affine_select(out=maskL[:, j, :], in_=self.ones[:], pattern=[[-1, 128]],
                                                         compare_op=ALU.is_gt, fill=0.0, base=0, channel_multiplier=1),
                     r=["ones"], w=["maskL"])
            S.op("pool", lambda: G.memset(bdm[:], 0.0), w=["bdm"])
            S.op("pool", lambda: G.memset(bdm[0:64, 0:64], 1.0), w=["bdm"])
            S.op("pool", lambda: G.memset(bdm[64:128, 64:128], 1.0), w=["bdm"])
            for (t_, nm) in ((mEO, "mEO"), (hsel, "hsel")):
                S.op("pool", lambda t_=t_: G.memset(t_[0:64, 0:1], 1.0), w=[nm])
                S.op("pool", lambda t_=t_: G.memset(t_[64:128, 0:1], 0.0), w=[nm])
                S.op("pool", lambda t_=t_: G.memset(t_[0:64, 1:2], 0.0), w=[nm])
                S.op("pool", lambda t_=t_: G.memset(t_[64:128, 1:2], 1.0), w=[nm])
            S.op("pool", lambda: G.memset(rm[:], 1.0), w=["rm"])
            S.op("pool", lambda: G.memset(rm[:].rearrange("p (c j) -> p c j", j=128)[:, :, 0:1], 0.0), w=["rm"])
            S.op("pool", lambda: G.memset(c05[:], -0.5), w=["c05"])
            pc = {}
            for nm, src in (("mu_r", P_["rwkv_mu"][l, 0:1024]), ("mu_k", P_["rwkv_mu"][l, 1024:2048]),
                            ("mu_v", P_["rwkv_mu"][l, 2048:3072]), ("w0", P_["rwkv_w0"][l]), ("a0", P_["rwkv_a0"][l]),
                            ("k_k", P_["rwkv_k_k"][l]), ("k_a", P_["rwkv_k_a"][l]),
                            ("r_k", P_["rwkv_r_k"][l].rearrange("h k -> (h k)"))):
                t_ = self.sb(st, "pc_" + nm, [128, 8, 1], F32)
                S.dma("sp", t_[:], src.rearrange("(q p o) -> p q o", p=128, o=1), w=["pc_" + nm])
                pc[nm] = t_
            mul = self.sb(st, "mul", [128, 3], F32)
            S.dma("sp", mul[0:64, 0:1], P_["rwkv_mu"][l, 3072:3136].rearrange("(p o) -> p o", o=1), w=["mul"])
            S.dma("sp", mul[0:64, 1:2], P_["rwkv_mu"][l, 3136:3200].rearrange("(p o) -> p o", o=1), w=["mul"])
            S.dma("sp", mul[:, 2:3], P_["rwkv_mu"][l, 3200:3328].rearrange("(p o) -> p o", o=1), w=["mul"])
            nw0 = self.sb(st, "nw0", [128, 8, 1], F32)
            omka = self.sb(st, "omka", [128, 8, 1], F32)
            S.op("dve", lambda: V.tensor_scalar(out=nw0[:], in0=pc["w0"][:], scalar1=-1.0, scalar2=None, op0=ALU.mult),
                 r=["pc_w0"], w=["nw0"])
            S.op("dve", lambda: V.tensor_scalar(out=omka[:], in0=pc["k_a"][:], scalar1=-1.0, scalar2=1.0, op0=ALU.mult,
                                                op1=ALU.add), r=["pc_k_a"], w=["omka"])
            wl = self.sb(st, "wl", [128, KT, 256], BF16)
            w2 = self.sb(st, "w2", [64, D], BF16)
            a2 = self.sb(st, "a2", [64, D], BF16)
            g2 = self.sb(st, "g2", [128, D], BF16)
            S.dma("pool", wl[:], Wl[:, OFF_RWKV + 3072:OFF_RWKV + 3328].rearrange("(kt p) n -> p kt n", p=128), w=["wl"])
            S.dma("pool", w2[:], P_["rwkv_w2"][l], w=["w2"])
            S.dma("pool", a2[:], P_["rwkv_a2"][l], w=["a2"])
            S.dma("pool", g2[:], P_["rwkv_g2"][l], w=["g2"])
            txw = self.sb(st, "txw", [64, T], BF16)
            xaT = self.sb(st, "xaT", [64, T], BF16)
            sgT = self.sb(st, "sgT", [128, T], BF16)
            xraw = self.sb(st, "xraw", [128, T + 1], F32)
            F = [self.sb(st, "F%d" % i, [128, T], F32) for i in range(5)]
            S.op("pool", lambda: G.memset(xraw[:, 0:1], 0.0), w=["xraw"])

            def proj_shift(wt, c0, m, mucol, dst, dkey, func=None, rows=128):
                for tq in range(4):
                    pb, kp = self.psum()
                    for kt in range(KT):
                        mm(pb[0:rows, :], wt[:, kt, c0:c0 + m], self.hT[:, kt, tq * 512:(tq + 1) * 512], kt == 0, kt == KT - 1,
                           [wt_key] + hTk[tq * 4:tq * 4 + 4], [kp])
                    S.op("act", lambda pb=pb, tq=tq: A.activation(out=xraw[0:rows, 1 + tq * 512:1 + (tq + 1) * 512],
                                                                  in_=pb[0:rows, :], func=AF.Copy), r=[kp], w=["xraw", kp])
                S.op("dve", lambda: V.tensor_tensor(out=F[4][0:rows, :], in0=xraw[0:rows, 0:T], in1=xraw[0:rows, 1:T + 1],
                                                    op=ALU.subtract), r=["xraw"], w=["F4"])
                if func is None:
                    S.op("dve", lambda: V.scalar_tensor_tensor(out=dst[0:rows, :], in0=F[4][0:rows, :], scalar=mucol,
                                                               in1=xraw[0:rows, 1:T + 1], op0=ALU.mult, op1=ALU.add),
                         r=["F4", "xraw", "mul"] + list(pc_keys), w=[dkey])
                else:
                    S.op("dve", lambda: V.scalar_tensor_tensor(out=F[4][0:rows, :], in0=F[4][0:rows, :], scalar=mucol,
                                                               in1=xraw[0:rows, 1:T + 1], op0=ALU.mult, op1=ALU.add),
                         r=["F4", "xraw", "mul"] + list(pc_keys), w=["F4"])
                    S.op("act", lambda: A.activation(out=dst[0:rows, :], in_=F[4][0:rows, :], func=func), r=["F4"], w=[dkey])

            pc_keys = ["pc_mu_r", "pc_mu_k", "pc_mu_v"]
            wt_key = "wl"
            proj_shift(wl, 0, 64, mul[0:64, 0:1], txw, "txw", AF.Tanh, rows=64)
            proj_shift(wl, 64, 64, mul[0:64, 1:2], xaT, "xaT", AF.Copy, rows=64)
            proj_shift(wl, 128, 128, mul[:, 2:3], sgT, "sgT", AF.Sigmoid, rows=128)
            wrkv1 = self.sb(st, "wrkv", [128, KT, 384], BF16)
            wrkv = [wrkv1, wrkv1]
            Pp = self.sb(st, "Pp", [128, T], BF16)
            Pc = self.sb(st, "Pc", [128, T], BF16)
            Pend = self.sb(st, "Pend", [128, NT], F32)
            iP = self.sb(st, "iP", [128, T], BF16)
            bT = self.sb(st, "bT", [128, T], BF16)
            kT = self.sb(st, "kT", [128, T], BF16)
            vT = self.sb(st, "vT", [128, T], BF16)
            AR = [self.sb(st, "AR", [128, NT, 2, 128], BF16) for _ in range(2)]
            Vtm = self.sb(st, "Vtm", [128, NT, 128], BF16)
            Btm = self.sb(st, "Btm", [128, NT, 128], BF16)
            aT = Vtm[:].rearrange("p t j -> p (t j)")
            rT = Btm[:].rearrange("p t j -> p (t j)")
            Ktm = self.sb(st, "Ktm", [128, NT, 128], BF16)
            bon = self.sb(st, "bon", [128, NT, 2], F32)
            st32 = self.sb(st, "st32", [128, 2 * NT, 4], F32)
            lnw = self.sb(st, "lnwb", [128, 128], F32)
            lnb = self.sb(st, "lnbb", [128, 128], F32)
            Z32 = self.sb(st, "Z32", [128, 128], F32)
            Zt = self.sb(st, "Zt", [128, 128], F32)
            Zbf = [self.sb(st, "Zbf", [128, 128], BF16) for _ in range(2)]
            Wsb = [self.sb(st, "Wsb", [128, 128], BF16) for _ in range(2)]
            Usb = [self.sb(st, "Usb", [128, 128], BF16) for _ in range(2)]
            NS = int(os.environ.get('RWKV_NS', '4'))
            abrb = [self.sb(st, "abrb", [128, 4, 128], BF16) for _ in range(NS)]
            akrk = [self.sb(st, "akrk", [128, 4, 128], BF16) for _ in range(NS)]
            L0 = [self.sb(st, "L0", [128, 2, 128], BF16) for _ in range(4)]
            XX = [[self.sb(st, "XX", [128, 4, 128], BF16) for _ in range(4)] for _ in range(2)]
            Tt = [self.sb(st, "Tt", [128, 2, 128], BF16) for _ in range(NS)]

            def load_w(p):
                b = 0
                for j in range(3):
                    c0 = OFF_RWKV + j * 1024 + p * 128
                    S.dma("pool", wrkv[b][:, :, j * 128:(j + 1) * 128], Wl[:, c0:c0 + 128].rearrange("(kt p) n -> p kt n", p=128),
                          w=[("wrkv", b)])

            for p in range(8):
                b = 0
                load_w(p)
                fs_ = slice(p * 128, (p + 1) * 128)
                S.dma("sp", lnw[:], P_["rwkv_ln_w"][l, fs_].partition_broadcast(128), w=["lnw"])
                S.dma("sp", lnb[:], P_["rwkv_ln_b"][l, fs_].partition_broadcast(128), w=["lnb"])
                wt_key = ("wrkv", b)
                proj_shift(wrkv[b], 128, 128, pc["mu_k"][:, p, :], F[0], "F0")
                proj_shift(wrkv[b], 0, 128, pc["mu_r"][:, p, :], F[1], "F1")
                proj_shift(wrkv[b], 256, 128, pc["mu_v"][:, p, :], vT, "vT", AF.Copy)
                for tq in range(4):
                    pb, kp = self.psum()
                    qs_ = slice(tq * 512, (tq + 1) * 512)
                    mm(pb[:], w2[:, fs_], txw[:, qs_], True, True, ["w2", "txw"], [kp])
                    S.op("act", lambda pb=pb: A.activation(out=F[2][:, qs_], in_=pb[:], func=AF.Exp, bias=nw0[:, p, :], scale=-1.0),
                         r=[kp, "nw0"], w=["F2", kp])
                S.op("act", lambda: A.activation(out=F[2][:], in_=F[2][:], func=AF.Ln, bias=self.epsc[:, 3:4], scale=1.0),
                     r=["F2", "epsc"], w=["F2"])
                S.op("act", lambda: A.activation(out=F[2][:], in_=F[2][:], func=AF.Exp, bias=c05[:, 0:1], scale=-1.0),
                     r=["F2", "c05"], w=["F2"])
                S.op("dve", lambda: V.tensor_tensor_scan(out=F[3][:], data0=rm[:], data1=F[2][:], initial=0.0, op0=ALU.mult,
                                                         op1=ALU.add), r=["rm", "F2"], w=["F3"])
                S.op("dve", lambda: V.tensor_tensor(out=F[2][:], in0=F[3][:], in1=F[2][:], op=ALU.subtract),
                     r=["F2", "F3"], w=["F2"])
                S.op("act", lambda: A.activation(out=Pp[:], in_=F[2][:], func=AF.Exp, scale=-1.0), r=["F2"], w=["Pp"])
                S.op("act", lambda: A.activation(out=Pc[:], in_=F[3][:], func=AF.Exp, scale=-1.0), r=["F3"], w=["Pc"])
                S.op("act", lambda: A.activation(out=iP[:], in_=F[3][:], func=AF.Exp), r=["F3"], w=["iP"])
                S.op("act", lambda: A.activation(out=Pend[:], in_=F[3][:].rearrange("p (t j) -> p t j", j=128)[:, :, 127],
                                                 func=AF.Exp, scale=-1.0), r=["F3"], w=["Pend"])
                for tq in range(4):
                    pb, kp = self.psum()
                    qs_ = slice(tq * 512, (tq + 1) * 512)
                    mm(pb[:], a2[:, fs_], xaT[:, qs_], True, True, ["a2", "xaT"], [kp])
                    S.op("act", lambda pb=pb: A.activation(out=F[2][:, qs_], in_=pb[:], func=AF.Sigmoid, bias=pc["a0"][:, p, :],
                                                           scale=1.0), r=[kp, "pc_a0"], w=["F2", kp])
                S.op("dve", lambda: V.tensor_scalar(out=F[3][:], in0=F[0][:], scalar1=pc["k_k"][:, p, :], scalar2=None,
                                                    op0=ALU.mult), r=["F0", "pc_k_k"], w=["F3"])
                S.op("act", lambda: A.activation(out=F[4][:], in_=F[3][:], func=AF.Square), r=["F3"], w=["F4"])
                for tq in range(4):
                    pb, kp = self.psum()
                    qs_ = slice(tq * 512, (tq + 1) * 512)
                    mm(pb[:], bdm[:], F[4][:, qs_], True, True, ["bdm", "F4"], [kp])
                    S.op("act", lambda pb=pb: A.activation(out=F[4][:, qs_], in_=pb[:], func=AF.Sqrt), r=[kp], w=["F4", kp])
                S.op("dve", lambda: V.tensor_scalar(out=F[4][:], in0=F[4][:], scalar1=1e-12, scalar2=None, op0=ALU.max),
                     r=["F4"], w=["F4"])
                S.op("dve", lambda: V.reciprocal(out=F[4][:], in_=F[4][:]), r=["F4"], w=["F4"])
                S.op("dve", lambda: V.tensor_tensor(out=F[3][:], in0=F[3][:], in1=F[4][:], op=ALU.mult), r=["F3", "F4"], w=["F3"])
                S.op("dve", lambda: V.scalar_tensor_tensor(out=aT, in0=F[3][:], scalar=-1.0, in1=Pp[:], op0=ALU.mult,
                                                           op1=ALU.mult), r=["F3", "Pp"], w=["Vtm"])
                S.op("pool", lambda: G.tensor_tensor(out=F[4][:], in0=F[3][:], in1=F[2][:], op=ALU.mult), r=["F3", "F2"], w=["F4"])
                S.op("pool", lambda: G.tensor_tensor(out=bT[:], in0=F[4][:], in1=iP[:], op=ALU.mult), r=["F4", "iP"], w=["bT"])
                S.op("dve", lambda: V.tensor_scalar(out=F[2][:], in0=F[2][:], scalar1=pc["k_a"][:, p, :], scalar2=omka[:, p, :],
                                                    op0=ALU.mult, op1=ALU.add), r=["F2", "pc_k_a", "omka"], w=["F2"])
                S.op("dve", lambda: V.tensor_tensor(out=F[0][:], in0=F[0][:], in1=F[2][:], op=ALU.mult), r=["F0", "F2"], w=["F0"])
                S.op("pool", lambda: G.tensor_tensor(out=kT[:], in0=F[0][:], in1=iP[:], op=ALU.mult), r=["F0", "iP"], w=["kT"])
                S.op("dve", lambda: V.tensor_tensor(out=rT, in0=F[1][:], in1=Pc[:], op=ALU.mult), r=["F1", "Pc"], w=["Btm"])
                S.op("dve", lambda: V.scalar_tensor_tensor(out=F[1][:], in0=F[1][:], scalar=pc["r_k"][:, p, :], in1=F[0][:],
                                                           op0=ALU.mult, op1=ALU.mult), r=["F1", "F0", "pc_r_k"], w=["F1"])
                for h in range(2):
                    S.op("act", lambda h=h: A.activation(out=AR[h][:, :, 0, :], in_=Vtm[:], func=AF.Identity,
                                                         scale=mEO[:, h:h + 1]), r=["Vtm", "mEO"], w=[("AR", h)])
                    S.op("dve", lambda h=h: V.tensor_scalar(out=AR[h][:, :, 1, :], in0=Btm[:],
                                                            scalar1=mEO[:, h:h + 1], scalar2=None, op0=ALU.mult),
                         r=["Btm", "mEO"], w=[("AR", h)])
                for (src, skey, dst, dkey) in ((vT, "vT", Vtm, "Vtm"), (bT, "bT", Btm, "Btm"), (kT, "kT", Ktm, "Ktm")):
                    for t4 in range(4):
                        pb, kp = self.psum()
                        pbv = pb[:].bitcast(BF16)
                        for j in range(4):
                            tt = t4 * 4 + j
                            S.op("pe", lambda j=j, tt=tt, pbv=pbv, src=src: PE.transpose(
                                out=pbv[:, j * 128:(j + 1) * 128], in_=src[:, tt * 128:(tt + 1) * 128], identity=self.identbf[:]),
                                r=[skey, "identbf"], w=[kp])
                        S.op("act", lambda t4=t4, pbv=pbv, dst=dst: A.activation(
                            out=dst[:, t4 * 4:(t4 + 1) * 4, :], in_=pbv[:, 0:512].rearrange("p (a b) -> p a b", a=4), func=AF.Copy),
                            r=[kp], w=[dkey, kp])
                pb, kp = self.psum()
                for tt in range(NT):
                    mm(pb[:, tt * 2:(tt + 1) * 2], F[1][:, tt * 128:(tt + 1) * 128], hsel[:], True, True, ["F1", "hsel"], [kp])
                S.op("dve", lambda pb=pb: V.tensor_copy(out=bon[:].rearrange("p t h -> p (t h)"), in_=pb[:, 0:2 * NT]),
                     r=[kp], w=["bon", kp])
                ytm = F[2][:].rearrange("p (t j) -> p t j", j=128)
                ysq = F[3][:].rearrange("p (t j) -> p t j", j=128)

                def inv_group(gi):
                    tiles = range(gi * 4, gi * 4 + 4)
                    for tt in tiles:
                        sl = tt % NS
                        tsl = slice(tt * 128, (tt + 1) * 128)
                        p1, k1 = self.psum()
                        p2, k2 = self.psum()
                        p3, k3 = self.psum()
                        for h in range(2):
                            rhs = AR[h][:, tt].rearrange("p a j -> p (a j)")
                            mm(p1[:, h * 256:(h + 1) * 256], bT[:, tsl], rhs, True, True, ["bT", ("AR", h)], [k1])
                            mm(p2[:, h * 256:(h + 1) * 256], kT[:, tsl], rhs, True, True, ["kT", ("AR", h)], [k2])
                            mm(p3[:, h * 128:(h + 1) * 128], AR[h][:, tt, 0, :], bT[:, tsl], True, True, [("AR", h), "bT"], [k3])
                        S.op("dve", lambda p1=p1, sl=sl: V.tensor_tensor(out=abrb[sl][:].rearrange("p a j -> p (a j)"), in0=p1[:],
                                                                        in1=mask4[:].rearrange("p a j -> p (a j)"), op=ALU.mult),
                             r=[k1, "mask4"], w=[("abrb", sl), k1])
                        S.op("dve", lambda p2=p2, sl=sl: V.tensor_tensor(out=akrk[sl][:].rearrange("p a j -> p (a j)"), in0=p2[:],
                                                                        in1=mask4[:].rearrange("p a j -> p (a j)"), op=ALU.mult),
                             r=[k2, "mask4"], w=[("akrk", sl), k2])
                        S.op("dve", lambda p3=p3, sl=sl: V.tensor_tensor(out=L0[sl % 4][:].rearrange("p a j -> p (a j)"), in0=p3[:, 0:256],
                                                                        in1=maskL[:].rearrange("p a j -> p (a j)"), op=ALU.mult),
                             r=[k3, "maskL"], w=[("L0", sl % 4), k3])
                        for h in range(2):
                            S.op("pool", lambda h=h, sl=sl: G.tensor_tensor(out=Tt[sl][:, h, :], in0=abrb[sl][:, 2 * h, :],
                                                                            in1=self.identbf[:], op=ALU.add),
                                 r=[("abrb", sl), "identbf"], w=[("Tt", sl)])

                    def Xk(k, sl, h):
                        return (L0[sl % 4][:, h, :], ("L0", sl % 4)) if k == 0 else (XX[k % 2][sl % 4][:, 2 * h, :], ("XX", k % 2, sl % 4))

                    def Xtk(k, sl, h):
                        return (abrb[sl][:, 2 * h, :], ("abrb", sl)) if k == 0 else (XX[k % 2][sl % 4][:, 2 * h + 1, :], ("XX", k % 2, sl % 4))

                    for k in range(6):
                        for tt in tiles:
                            sl = tt % NS
                            pb, kp = self.psum()
                            for h in range(2):
                                x, kx = Xk(k, sl, h)
                                xt, kxt = Xtk(k, sl, h)
                                mm(pb[:, (2 * h) * 128:(2 * h + 1) * 128], xt, x, True, True, [kx, kxt], [kp])
                                if k < 5:
                                    mm(pb[:, (2 * h + 1) * 128:(2 * h + 2) * 128], x, xt, True, True, [kx, kxt], [kp])
                            kn = ("XX", (k + 1) % 2, sl % 4)
                            if k < 5:
                                S.op("act", lambda pb=pb, sl=sl, k=k: A.activation(
                                    out=XX[(k + 1) % 2][sl % 4][:].rearrange("p a j -> p (a j)"), in_=pb[:], func=AF.Copy),
                                    r=[kp], w=[kn, kp])
                            else:
                                S.op("act", lambda pb=pb, sl=sl, k=k: A.activation(
                                    out=XX[(k + 1) % 2][sl % 4][:, 0:4:2, :], in_=pb[:].rearrange("p (a j) -> p a j", j=128)[:, 0:4:2, :],
                                    func=AF.Copy), r=[kp], w=[kn, kp])
                        for t2 in range(2):
                            pb, kp = self.psum()
                            for j in range(2):
                                sl = (gi * 4 + t2 * 2 + j) % NS
                                for h in range(2):
                                    x1, kx1 = Xk(k + 1, sl, h)
                                    mm(pb[:, (j * 2 + h) * 128:(j * 2 + h + 1) * 128], x1, Tt[sl][:, h, :], True, True,
                                       [kx1, ("Tt", sl)], [kp])
                            for j in range(2):
                                sl = (gi * 4 + t2 * 2 + j) % NS
                                S.op("dve", lambda pb=pb, sl=sl, j=j: V.tensor_tensor(
                                    out=Tt[sl][:].rearrange("p a j -> p (a j)"), in0=pb[:, j * 256:(j + 1) * 256],
                                    in1=Tt[sl][:].rearrange("p a j -> p (a j)"), op=ALU.add), r=[kp, ("Tt", sl)], w=[("Tt", sl), kp])

                def chain_group(gi):
                    for tt in range(gi * 4, gi * 4 + 4):
                        sl = tt % NS
                        i2 = tt % 2
                        tsl = slice(tt * 128, (tt + 1) * 128)
                        zb, kz = Zbf[i2], ("Zbf", i2)
                        pw, kpw = self.psum()
                        mm(pw[:, 0:128], AR[0][:, tt, 0, :], zb[:], True, False, [("AR", 0), kz], [kpw])
                        mm(pw[:, 0:128], AR[1][:, tt, 0, :], zb[:], False, False, [("AR", 1), kz], [kpw])
                        for h in range(2):
                            mm(pw[:, h * 64:(h + 1) * 64], akrk[sl][:, 2 * h, :], Vtm[:, tt, h * 64:(h + 1) * 64], False, h == 1,
                               [("akrk", sl), "Vtm"], [kpw])
                        S.op("act", lambda pw=pw: A.activation(out=Wsb[i2][:], in_=pw[:, 0:128], func=AF.Copy),
                             r=[kpw], w=[("Wsb", i2), kpw])
                        pu, kpu = self.psum()
                        for h in range(2):
                            mm(pu[:, h * 64:(h + 1) * 64], Tt[sl][:, h, :], Wsb[i2][:, h * 64:(h + 1) * 64], True, True,
                               [("Tt", sl), ("Wsb", i2)], [kpu])
                        S.op("act", lambda pu=pu: A.activation(out=Usb[i2][:], in_=pu[:, 0:128], func=AF.Copy),
                             r=[kpu], w=[("Usb", i2), kpu])
                        py, kpy = self.psum()
                        mm(py[:, 0:128], AR[0][:, tt, 1, :], zb[:], True, False, [("AR", 0), kz], [kpy])
                        mm(py[:, 0:128], AR[1][:, tt, 1, :], zb[:], False, False, [("AR", 1), kz], [kpy])
                        for h in range(2):
                            mm(py[:, h * 64:(h + 1) * 64], abrb[sl][:, 2 * h + 1, :], Usb[i2][:, h * 64:(h + 1) * 64], False, False,
                               [("abrb", sl), ("Usb", i2)], [kpy])
                            mm(py[:, h * 64:(h + 1) * 64], akrk[sl][:, 2 * h + 1, :], Vtm[:, tt, h * 64:(h + 1) * 64], False, h == 1,
                               [("akrk", sl), "Vtm"], [kpy])
                        S.op("act", lambda py=py: A.activation(out=ytm[:, tt, :], in_=py[:, 0:128], func=AF.Copy),
                             r=[kpy], w=["F2", kpy])
                        pz, kpz = self.psum()
                        mm(pz[:, 0:128], Btm[:, tt, :], Usb[i2][:], True, False, ["Btm", ("Usb", i2)], [kpz])
                        mm(pz[:, 0:128], Ktm[:, tt, :], Vtm[:, tt, :], False, True, ["Ktm", "Vtm"], [kpz])
                        S.op("dve", lambda pz=pz: V.tensor_tensor(out=Zt[:], in0=pz[:, 0:128], in1=bdm[:], op=ALU.mult),
                             r=[kpz, "bdm"], w=["Zt", kpz])
                        S.op("dve", lambda: V.tensor_tensor(out=Zt[:], in0=Zt[:], in1=Z32[:], op=ALU.add), r=["Zt", "Z32"], w=["Zt"])
                        S.op("dve", lambda: V.tensor_scalar(out=Z32[:], in0=Zt[:], scalar1=Pend[:, tt:tt + 1],
                                                            scalar2=None, op0=ALU.mult), r=["Zt", "Pend"], w=["Z32"])
                        S.op("pool", lambda: G.tensor_copy(out=Zbf[(tt + 1) % 2][:], in_=Z32[:]), r=["Z32"], w=[("Zbf", (tt + 1) % 2)])

                S.op("pool", lambda: G.memset(Z32[:], 0.0), w=["Z32"])
                S.op("pool", lambda: G.memset(Zbf[0][:], 0.0), w=[("Zbf", 0)])
                if NS >= 8:
                    inv_group(0)
                    for gi in range(4):
                        if gi + 1 < 4:
                            inv_group(gi + 1)
                        chain_group(gi)
                else:
                    for gi in range(4):
                        inv_group(gi)
                        chain_group(gi)
                y16, yTp = Btm, kT
                y3 = ytm.rearrange("p t (h c) -> p (t h) c", c=64)
                q3 = ysq.rearrange("p t (h c) -> p (t h) c", c=64)
                S.op("act", lambda: A.activation(out=F[3][:], in_=F[2][:], func=AF.Square), r=["F2"], w=["F3"])
                S.op("dve", lambda: V.tensor_reduce(out=st32[:, :, 0], in_=y3, axis=AX.X, op=ALU.add), r=["F2"], w=["st32"])
                S.op("dve", lambda: V.tensor_reduce(out=st32[:, :, 1], in_=q3, axis=AX.X, op=ALU.add), r=["F3"], w=["st32"])
                S.op("dve", lambda: V.tensor_scalar(out=st32[:, :, 0], in0=st32[:, :, 0], scalar1=1.0 / 64.0, scalar2=None,
                                                    op0=ALU.mult), r=["st32"], w=["st32"])
                S.op("dve", lambda: V.tensor_tensor(out=st32[:, :, 2], in0=st32[:, :, 0], in1=st32[:, :, 0], op=ALU.mult),
                     r=["st32"], w=["st32"])
                S.op("dve", lambda: V.scalar_tensor_tensor(out=st32[:, :, 1], in0=st32[:, :, 1], scalar=1.0 / 64.0, in1=st32[:, :, 2],
                                                           op0=ALU.mult, op1=ALU.subtract), r=["st32"], w=["st32"])
                S.op("act", lambda: A.activation(out=st32[:, :, 1], in_=st32[:, :, 1], func=AF.Sqrt, bias=self.epsc[:, 2:3], scale=1.0),
                     r=["st32", "epsc"], w=["st32"])
                S.op("dve", lambda: V.reciprocal(out=st32[:, :, 1], in_=st32[:, :, 1]), r=["st32"], w=["st32"])
                S.op("dve", lambda: V.tensor_tensor(out=y3, in0=y3, in1=st32[:, :, 0:1].to_broadcast([128, 2 * NT, 64]),
                                                    op=ALU.subtract), r=["F2", "st32"], w=["F2"])
                S.op("dve", lambda: V.tensor_tensor(out=y3, in0=y3, in1=st32[:, :, 1:2].to_broadcast([128, 2 * NT, 64]),
                                                    op=ALU.mult), r=["F2", "st32"], w=["F2"])
                S.op("pool", lambda: G.tensor_tensor(out=ytm, in0=ytm, in1=lnw[:].unsqueeze(1).to_broadcast([128, NT, 128]),
                                                     op=ALU.mult), r=["F2", "lnw"], w=["F2"])
                S.op("pool", lambda: G.tensor_tensor(out=ytm, in0=ytm, in1=lnb[:].unsqueeze(1).to_broadcast([128, NT, 128]),
                                                     op=ALU.add), r=["F2", "lnb"], w=["F2"])
                S.op("dve", lambda: V.tensor_tensor(out=q3, in0=Vtm[:].rearrange("p t (h c) -> p (t h) c", c=64),
                                                    in1=bon[:].rearrange("p t h -> p (t h)").unsqueeze(2).to_broadcast([128, 2 * NT, 64]),
                                                    op=ALU.mult), r=["Vtm", "bon", "F3"], w=["F3"])
                S.op("dve", lambda: V.tensor_tensor(out=F[2][:], in0=F[2][:], in1=F[3][:], op=ALU.add), r=["F2", "F3"], w=["F2"])
                for t4 in range(4):
                    pb, kp = self.psum()
                    for j in range(4):
                        tt = t4 * 4 + j
                        mm(pb[:, j * 128:(j + 1) * 128], sgT[:, tt * 128:(tt + 1) * 128], g2[:, fs_], True, True, ["sgT", "g2"], [kp])
                    S.op("dve", lambda pb=pb, t4=t4: V.tensor_tensor(
                        out=y16[:, t4 * 4:(t4 + 1) * 4, :], in0=pb[:].rearrange("p (a j) -> p a j", j=128),
                        in1=ytm[:, t4 * 4:(t4 + 1) * 4, :], op=ALU.mult), r=[kp, "F2"], w=["Btm", kp])
                for t4 in range(4):
                    pb, kp = self.psum()
                    pbv = pb[:].bitcast(BF16)
                    for j in range(4):
                        tt = t4 * 4 + j
                        S.op("pe", lambda j=j, tt=tt, pbv=pbv: PE.transpose(out=pbv[:, j * 128:(j + 1) * 128], in_=y16[:, tt, :],
                                                                           identity=self.identbf[:]), r=["Btm", "identbf"], w=[kp])
                    S.op("act", lambda t4=t4, pbv=pbv: A.activation(out=yTp[:, t4 * 512:(t4 + 1) * 512], in_=pbv[:, 0:512], func=AF.Copy),
                         r=[kp], w=["kT", kp])
                S.dma("sp", ydst[fs_, :], yTp[:], r=["kT"], w=[("yT1", p)])
            S.barrier()


    def merge(self, l):
        nc, S = self.nc, self.S
        V, A, G, PE = nc.vector, nc.scalar, nc.gpsimd, nc.tensor
        Wl = self.P["w_in"][l]
        brw = [self.P["w_br_ssd"][l], self.P["w_br_rwkv"][l], self.P["w_br_hgrn"][l]]
        hTk = [("hT", t) for t in range(NT)]
        with contextlib.ExitStack() as st:
            mT = self.sb(st, "mT", [128, KT, T], F32)
            wbr = self.sb(st, "wbr", [128, KT, D], BF16)
            wgt = self.sb(st, "wgt", [128, KT, D], BF16)
            yq = [self.sb(st, "yq", [128, KT, 512], BF16) for _ in range(2)]
            sg = [self.sb(st, "sgm", [128, 512], BF16) for _ in range(2)]
            tmp = [self.sb(st, "tmpm", [128, 512], F32) for _ in range(2)]
            cnt = 0
            for i in range(3):
                S.dma("pool", wbr[:], brw[i].rearrange("(kt p) n -> p kt n", p=128), w=["wbr"])
                c0 = OFF_GATES + i * 1024
                S.dma("pool", wgt[:], Wl[:, c0:c0 + 1024].rearrange("(kt p) n -> p kt n", p=128), w=["wgt"])
                for q in range(4):
                    qs_ = slice(q * 512, (q + 1) * 512)
                    yb = (i * 4 + q) % 2
                    S.dma("sp", yq[yb][:], self.yT_dram[i][:, qs_].rearrange("(kt p) n -> p kt n", p=128),
                          r=[("yT%d" % i, k) for k in range(8)], w=[("yq", yb)])
                    for ot in range(KT):
                        os_ = slice(ot * 128, (ot + 1) * 128)
                        pg, kg = self.psum()
                        pb, kb = self.psum()
                        for kt in range(KT):
                            S.op("pe", lambda kt=kt, pg=pg: PE.matmul(pg[:], lhsT=wgt[:, kt, os_], rhs=self.hT[:, kt, qs_],
                                                                      start=(kt == 0), stop=(kt == KT - 1)),
                                 r=["wgt"] + hTk[q * 4:q * 4 + 4], w=[kg])
                        for kt in range(KT):
                            S.op("pe", lambda kt=kt, pb=pb: PE.matmul(pb[:], lhsT=wbr[:, kt, os_], rhs=yq[yb][:, kt, :],
                                                                      start=(kt == 0), stop=(kt == KT - 1)),
                                 r=["wbr", ("yq", yb)], w=[kb])
                        c2 = cnt % 2
                        cnt += 1
                        S.op("act", lambda pg=pg, c2=c2: A.activation(out=sg[c2][:], in_=pg[:], func=AF.Sigmoid),
                             r=[kg], w=[("sgm", c2), kg])
                        if i == 0:
                            S.op("dve", lambda pb=pb, c2=c2: V.tensor_tensor(out=mT[:, ot, qs_], in0=pb[:], in1=sg[c2][:], op=ALU.mult),
                                 r=[kb, ("sgm", c2)], w=[("mT", q), kb])
                        else:
                            S.op("dve", lambda pb=pb, c2=c2: V.tensor_tensor(out=tmp[c2][:], in0=pb[:], in1=sg[c2][:], op=ALU.mult),
                                 r=[kb, ("sgm", c2)], w=[("tmpm", c2), kb])
                            S.op("pool", lambda c2=c2: G.tensor_tensor(out=mT[:, ot, qs_], in0=mT[:, ot, qs_], in1=tmp[c2][:],
                                                                       op=ALU.add), r=[("tmpm", c2), ("mT", q)], w=[("mT", q)])
            self.dbg_dump("merged%d" % l, lambda o: S.dma("sp", o.rearrange("(kt p) n -> p kt n", p=128), mT[:],
                                                          r=[("mT", q) for q in range(4)]))
            wo = wbr
            S.dma("pool", wo[:], self.P["w_out"][l].rearrange("(kt p) n -> p kt n", p=128), w=["wbr"])
            gbc = self.sb(st, "gbc1", [128, D], F32)
            bbc = self.sb(st, "bbc1", [128, D], F32)
            S.dma("sp", gbc[:], self.P["ln1_g"][l].partition_broadcast(128), w=["gbc"])
            S.dma("sp", bbc[:], self.P["ln1_b"][l].partition_broadcast(128), w=["bbc"])
            lnw = self.ln_alloc(st)
            h1 = [self.sb(st, "h1m", [128, D], F32) for _ in range(2)]
            mbf = [self.sb(st, "mbf", [128, KT, 128], BF16) for _ in range(2)]
            for tt in range(NT):
                s2 = tt % 2
                q = tt // 4
                tsl = slice(tt * 128, (tt + 1) * 128)
                S.op("act", lambda: A.activation(out=mbf[s2][:], in_=mT[:, :, tsl], func=AF.Copy), r=[("mT", q)], w=[("mbf", s2)])
                S.dma("sp", h1[s2][:], self.h_dram[tsl, :], r=[("hd", tt)], w=[("h1m", s2)])
                xin, kx = self.ln_xin(lnw, tt)
                for half in range(2):
                    po, ko = self.psum()
                    for kt in range(KT):
                        S.op("pe", lambda kt=kt, po=po: PE.matmul(po[:], lhsT=mbf[s2][:, kt, :], rhs=wo[:, kt, half * 512:(half + 1) * 512],
                                                                  start=(kt == 0), stop=(kt == KT - 1)), r=[("mbf", s2), "wbr"], w=[ko])
                    S.op("dve", lambda po=po, half=half: V.scalar_tensor_tensor(
                        out=xin[:, half * 512:(half + 1) * 512], in0=h1[s2][:, half * 512:(half + 1) * 512], scalar=ALPHA, in1=po[:],
                        op0=ALU.mult, op1=ALU.add), r=[ko, ("h1m", s2)], w=[kx, ko])
                self.ln_tile(lnw, tt, gbc, bbc, self.h_dram, router=True, extra=self.dbg_out.get("h1_%d" % l))
            S.barrier()

    def layer(self, l):
        S = self.S
        if "ssd" in self.stages:
            self.ssd(l)
            self.dbg_dump("ya%d" % l, lambda o: S.dma("sp", o, self.yT_dram[0], r=[("yT0", h) for h in range(8)]))
        if "rwkv" in self.stages:
            self.rwkv(l)
            self.dbg_dump("yb%d" % l, lambda o: S.dma("sp", o, self.yT_dram[1], r=[("yT1", h) for h in range(8)]))
        if "hgrn" in self.stages:
            self.hgrn(l)
            self.dbg_dump("yc%d" % l, lambda o: S.dma("sp", o, self.yT_dram[2], r=[("yT2", h) for h in range(8)]))
        if "merge" in self.stages:
            self.merge(l)
        if "moe" in self.stages:
            self.moe(l, last=(l == self.depth - 1))


_NC_CACHE = {}


def _get_nc():
    if "nc" not in _NC_CACHE:
        _NC_CACHE["nc"] = Builder().build()
    return _NC_CACHE["nc"]


def kernel(**inputs):
    nc = _get_nc()
    x = np.ascontiguousarray(inputs["x"], dtype=np.float32)
    base = {k: np.ascontiguousarray(inputs[k], dtype=np.float32) for k in PARAM_SHAPES}
    in_maps = []
    for c in range(8):
        m = dict(base)
        m["x"] = x[c]
        in_maps.append(m)
    res = run_bass_kernel_spmd(nc, in_maps, core_ids=list(range(8)))
    return np.stack([res.results[c]["out"] for c in range(8)], axis=0)
```

```python
import contextlib
import os
import numpy as np
CUT = int(os.environ.get('CUT', '99'))
HC = int(os.environ.get('HC', '99'))
HL = int(os.environ.get('HL', '99'))
import concourse.bass as bass
import concourse.mybir as mybir
from concourse.bass_utils import run_bass_kernel_spmd

F32 = mybir.dt.float32
BF16 = mybir.dt.bfloat16
AF = mybir.ActivationFunctionType
ALU = mybir.AluOpType
AX = mybir.AxisListType

D = 1024
T = 2048
NT = T // 128
KT = D // 128
DEPTH = 2
NE = 16
DEXP = 512
N_IN = 13072
ALPHA = (2 * DEPTH) ** 0.25
LN_EPS = 1e-5
RMS_EPS = 1e-6
GN_EPS = 64e-5
OFF_Z = 0
OFF_XBC = 1024
OFF_DT = 2560
OFF_RWKV = 2576
OFF_HGRN = OFF_RWKV + 3328
OFF_GATES = OFF_HGRN + 4096

PARAM_SHAPES = {
    "ln_in_g": [1024], "ln_in_b": [1024], "w_in": [2, 1024, 13072],
    "ssd_conv_w": [2, 4, 1536], "ssd_conv_b": [2, 1536], "ssd_dt_bias": [2, 16],
    "ssd_a_log": [2, 16], "ssd_d": [2, 16], "ssd_norm_w": [2, 1024],
    "rwkv_mu": [2, 3328], "rwkv_w0": [2, 1024], "rwkv_w2": [2, 64, 1024],
    "rwkv_a0": [2, 1024], "rwkv_a2": [2, 64, 1024], "rwkv_g2": [2, 128, 1024],
    "rwkv_k_k": [2, 1024], "rwkv_k_a": [2, 1024], "rwkv_r_k": [2, 16, 64],
    "rwkv_ln_w": [2, 1024], "rwkv_ln_b": [2, 1024], "hgrn_lb": [2, 1024],
    "hgrn_norm_w": [2, 128], "w_br_ssd": [2, 1024, 1024], "w_br_rwkv": [2, 1024, 1024],
    "w_br_hgrn": [2, 1024, 1024], "w_out": [2, 1024, 1024], "ln1_g": [2, 1024],
    "ln1_b": [2, 1024], "router_w": [1024, 16], "router_bias": [16],
    "exp_w_gate": [2, 16, 1024, 512], "exp_w_up": [2, 16, 1024, 512],
    "exp_w_down": [2, 16, 512, 1024], "ln2_g": [2, 1024], "ln2_b": [2, 1024],
}


class Sched:
    ENG = ["pe", "act", "dve", "pool", "sp"]

    def __init__(self, nc, es, n_dma=32, n_pdma=24):
        self.nc = nc
        self.e = {"pe": nc.tensor, "act": nc.scalar, "dve": nc.vector, "pool": nc.gpsimd, "sp": nc.sync}
        self.sem = {k: es.enter_context(nc.semaphore("sem_" + k)) for k in self.ENG}
        self.cnt = {k: 0 for k in self.ENG}
        self.dsem = [es.enter_context(nc.semaphore("dsem%d" % i)) for i in range(n_dma)]
        self.dtot = [0] * n_dma
        self.drr = 0
        self.psem = [es.enter_context(nc.semaphore("psem%d" % i)) for i in range(n_pdma)]
        self.pused = [False] * n_pdma
        self.pwaiters = [[] for _ in range(n_pdma)]
        self.pclr = [None] * n_pdma
        self.prr = 0
        self.msem = {k: es.enter_context(nc.semaphore("msem_" + k)) for k in self.ENG}
        self.mcnt = {k: 0 for k in self.ENG}
        self.seen = {k: {} for k in self.ENG}
        self.lastw = {}
        self.readers = {}
        self.nwait = 0

    def _semh(self, sk):
        if isinstance(sk, str):
            return self.sem[sk]
        if sk[0] == "m":
            return self.msem[sk[1]]
        return self.dsem[sk[1]] if sk[0] == "d" else self.psem[sk[1]]

    def _wait(self, e, tag):
        sk, val = tag
        if val <= 0 or self.seen[e].get(sk, 0) >= val:
            return
        if not isinstance(sk, str) and sk[0] == "p" and self.pclr[sk[1]] is not None and e != "pool":
            self._wait(e, self.pclr[sk[1]])
        self.e[e].wait_ge(self._semh(sk), val)
        self.seen[e][sk] = val
        self.nwait += 1
        if not isinstance(sk, str) and sk[0] == "p":
            self.pwaiters[sk[1]].append(self._marker(e))

    def _marker(self, e):
        self.e[e].sem_inc(self.msem[e], 1)
        self.mcnt[e] += 1
        return (("m", e), self.mcnt[e])

    def _deps(self, e, r, w):
        for k in r:
            t = self.lastw.get(k)
            if t is not None:
                self._wait(e, t)
        for k in w:
            t = self.lastw.get(k)
            if t is not None and (t[0] != e or e != "pe"):
                self._wait(e, t)
            for sk, val in self.readers.get(k, {}).items():
                if sk != e or e != "pe":
                    self._wait(e, (sk, val))

    def _record(self, tag, r, w):
        for k in r:
            d = self.readers.setdefault(k, {})
            if d.get(tag[0], 0) < tag[1]:
                d[tag[0]] = tag[1]
        for k in w:
            self.lastw[k] = tag
            self.readers[k] = {}

    def op(self, e, fn, r=(), w=()):
        self._deps(e, r, w)
        ins = fn()
        self.cnt[e] += 1
        ins.then_inc(self.sem[e], 1)
        if os.environ.get("OPLOG"):
            self.oplog = getattr(self, "oplog", {})
            self.oplog[(e, self.cnt[e])] = fn.__code__.co_firstlineno
        self._record((e, self.cnt[e]), r, w)

    def _dma_sw(self, out, in_, r, w):
        q = "pool"
        self._deps(q, r, w)
        i = self.prr
        self.prr = (self.prr + 1) % len(self.psem)
        sk = ("p", i)
        if self.pused[i]:
            self._wait(q, (sk, 16))
            for e in self.ENG:
                if e != q:
                    self._wait(e, (sk, 16))
            for tg in self.pwaiters[i]:
                if tg[0][1] != q:
                    self._wait(q, tg)
            self.e[q].sem_clear(self.psem[i])
            tclr = self._marker(q)
            self.pclr[i] = tclr
            for k, t in list(self.lastw.items()):
                if t[0] == sk:
                    self.lastw[k] = tclr
            for k, d in self.readers.items():
                if sk in d:
                    d.pop(sk)
                    d[tclr[0]] = tclr[1]
            for e in self.ENG:
                self.seen[e].pop(sk, None)
            self.pwaiters[i] = []
        ins = self.e[q].dma_start(out=out, in_=in_)
        ins.then_inc(self.psem[i], 16)
        self.pused[i] = True
        self._record((sk, 16), r, w)

    def dma(self, q, out, in_, r=(), w=()):
        if q == "pool" and os.environ.get("PSEM_CLEAR"):
            return self._dma_sw(out, in_, r, w)
        self._deps(q, r, w)
        i = self.drr
        self.drr = (self.drr + 1) % len(self.dsem)
        self._wait(q, (("d", i), self.dtot[i]))
        with self.nc.allow_non_contiguous_dma(reason="small per-feature parameter columns"):
            ins = self.e[q].dma_start(out=out, in_=in_)
        self.dtot[i] += 16
        ins.then_inc(self.dsem[i], 16)
        self._record((("d", i), self.dtot[i]), r, w)

    def barrier(self):
        for e in self.ENG:
            for o in self.ENG:
                if o != e:
                    self._wait(e, (o, self.cnt[o]))
            for i in range(len(self.dsem)):
                self._wait(e, (("d", i), self.dtot[i]))
            for i in range(len(self.psem)):
                if self.pused[i]:
                    self._wait(e, (("p", i), 16))

    def finish(self):
        for i in range(len(self.dsem)):
            self._wait("sp", (("d", i), self.dtot[i]))
        for i in range(len(self.psem)):
            if self.pused[i]:
                self._wait("sp", (("p", i), 16))
        for o in self.ENG:
            if o != "sp":
                self._wait("sp", (o, self.cnt[o]))


class Builder:
    def __init__(self, debug=None, stages=("pre", "hgrn", "ssd", "rwkv", "merge", "moe"), depth=DEPTH, pre_router=False):
        self.pre_router = pre_router
        self.debug = debug or {}
        self.stages = stages
        self.depth = depth
        self.nc = bass.Bass("TRN2", target_bir_lowering=False)
        nc = self.nc
        self.x = nc.dram_tensor("x", [T, D], F32, kind="ExternalInput").ap()
        self.P = {k: nc.dram_tensor(k, s, F32, kind="ExternalInput").ap() for k, s in PARAM_SHAPES.items()}
        self.out = nc.dram_tensor("out", [T, D], F32, kind="ExternalOutput").ap()
        self.h_dram = nc.dram_tensor("h_scr", [T, D], F32, kind="Internal").ap()
        self.yT_dram = [nc.dram_tensor("yT_scr%d" % i, [D, T], BF16, kind="Internal").ap() for i in range(3)]
        self.dbg_out = {}
        for name, (shape, dt) in self.debug.items():
            self.dbg_out[name] = nc.dram_tensor("dbg_" + name, shape, dt, kind="ExternalOutput").ap()
        self.uid = 0

    def sb(self, es, name, shape, dt):
        self.uid += 1
        return es.enter_context(self.nc.sbuf_tensor("%s_%d" % (name, self.uid), shape, dt))

    def psum(self):
        i = self.ps_rr
        self.ps_rr = (self.ps_rr + 1) % 8
        return self.ps[i], ("ps", i)

    def build(self):
        nc = self.nc
        with contextlib.ExitStack() as es:
            self.S = Sched(nc, es)
            S = self.S
            self.ps = [es.enter_context(nc.psum_tensor("psb%d" % i, [128, 512], F32)) for i in range(8)]
            self.ps_rr = 0
            self.ident32 = self.sb(es, "ident32", [128, 128], F32)
            self.identbf = self.sb(es, "identbf", [128, 128], BF16)
            self.zeros = self.sb(es, "zeros", [128, 128], F32)
            self.ones = self.sb(es, "ones", [128, 128], F32)
            self.onesbf = self.sb(es, "onesbf", [128, 128], BF16)
            self.epsc = self.sb(es, "epsc", [128, 4], F32)
            S.op("pool", lambda: nc.gpsimd.memset(self.zeros[:], 0.0), w=["zeros"])
            S.op("pool", lambda: nc.gpsimd.memset(self.ones[:], 1.0), w=["ones"])
            S.op("pool", lambda: nc.gpsimd.memset(self.onesbf[:], 1.0), w=["onesbf"])
            S.op("pool", lambda: nc.gpsimd.memset(self.epsc[:, 0:1], LN_EPS), w=["epsc"])
            S.op("pool", lambda: nc.gpsimd.memset(self.epsc[:, 1:2], RMS_EPS), w=["epsc"])
            S.op("pool", lambda: nc.gpsimd.memset(self.epsc[:, 2:3], GN_EPS), w=["epsc"])
            S.op("pool", lambda: nc.gpsimd.memset(self.epsc[:, 3:4], 1.0), w=["epsc"])
            S.op("pool", lambda: nc.gpsimd.affine_select(
                out=self.ident32[:], in_=self.zeros[:], pattern=[[1, 128]], compare_op=ALU.not_equal,
                fill=1.0, base=0, channel_multiplier=-1), r=["zeros"], w=["ident32"])
            S.op("pool", lambda: nc.gpsimd.tensor_copy(out=self.identbf[:], in_=self.ident32[:]),
                 r=["ident32"], w=["identbf"])
            self.hT = self.sb(es, "hT", [128, KT, T], BF16)
            self.gates = self.sb(es, "gates", [128, NT, NE], F32)
            self.logits = self.sb(es, "logits", [128, NT, NE], F32)
            self.rw32 = self.sb(es, "rw32", [128, KT, NE], F32)
            self.rbias = self.sb(es, "rbias", [128, NE], F32)
            S.dma("sp", self.rw32[:], self.P["router_w"].rearrange("(kt p) e -> p kt e", p=128), w=["rw32"])
            S.dma("sp", self.rbias[:], self.P["router_bias"].partition_broadcast(128), w=["rbias"])

            with contextlib.ExitStack() as st:
                gbc = self.sb(st, "gbc", [128, D], F32)
                bbc = self.sb(st, "bbc", [128, D], F32)
                S.dma("sp", gbc[:], self.P["ln_in_g"].partition_broadcast(128), w=["gbc"])
                S.dma("sp", bbc[:], self.P["ln_in_b"].partition_broadcast(128), w=["bbc"])
                lnw = self.ln_alloc(st)
                for tt in range(NT):
                    xin, kx = self.ln_xin(lnw, tt)
                    S.dma("sp", xin[:], self.x[tt * 128:(tt + 1) * 128, :], w=[kx])
                    self.ln_tile(lnw, tt, gbc, bbc, self.h_dram, router=self.pre_router, extra=self.dbg_out.get("h0"))
                S.barrier()
            self.dbg_dump("hT", lambda o: S.dma("sp", o, self.hT[:], r=[("hT", t) for t in range(NT)]))
            self.dbg_dump("logits", lambda o: S.dma("sp", o, self.logits[:], r=["logits"]))

            for l in range(self.depth):
                self.layer(l)
            S.finish()
        return nc

    def dbg_dump(self, name, fn):
        if name in self.dbg_out:
            fn(self.dbg_out[name])

    def ln_alloc(self, st):
        w = {}
        w["xin"] = [self.sb(st, "xin", [128, D], F32) for _ in range(2)]
        w["hh"] = [self.sb(st, "hh", [128, D], F32) for _ in range(2)]
        w["bst"] = [self.sb(st, "bst", [128, 2, 6], F32) for _ in range(2)]
        w["mv"] = [self.sb(st, "mv", [128, 4], F32) for _ in range(2)]
        w["h32"] = [self.sb(st, "h32", [128, KT, 128], F32) for _ in range(2)]
        w["id"] = self.uid
        return w

    def ln_xin(self, w, tt):
        return w["xin"][tt % 2], ("xin", w["id"], tt % 2)

    def ln_tile(self, w, tt, gbc, bbc, dst_dram, router, extra=None):
        nc, S = self.nc, self.S
        s = tt % 2
        wid = w["id"]
        xin, kx = w["xin"][s], ("xin", wid, s)
        hh, kh = w["hh"][s], ("hh", wid, s)
        bst, kb = w["bst"][s], ("bst", wid, s)
        mv, km = w["mv"][s], ("mv", wid, s)
        h32, k32 = w["h32"][s], ("h32", wid, s)
        for c in range(2):
            S.op("dve", lambda c=c: nc.vector.bn_stats(out=bst[:, c, :], in_=xin[:, c * 512:(c + 1) * 512]),
                 r=[kx], w=[kb])
        S.op("dve", lambda: nc.vector.bn_aggr(out=mv[:, 0:2], in_=bst[:].rearrange("p a b -> p (a b)")),
             r=[kb], w=[km])
        if CUT < 2:
            return
        S.op("act", lambda: nc.scalar.activation(out=mv[:, 2:3], in_=mv[:, 1:2], func=AF.Sqrt,
                                                 bias=self.epsc[:, 0:1], scale=1.0), r=[km, "epsc"], w=[km])
        S.op("dve", lambda: nc.vector.reciprocal(out=mv[:, 3:4], in_=mv[:, 2:3]), r=[km], w=[km])
        S.op("dve", lambda: nc.vector.tensor_scalar(out=xin[:], in0=xin[:], scalar1=mv[:, 0:1], scalar2=mv[:, 3:4],
                                                    op0=ALU.subtract, op1=ALU.mult), r=[kx, km], w=[kx])
        if CUT < 3:
            return
        S.op("pool", lambda: nc.gpsimd.tensor_tensor(out=hh[:], in0=xin[:], in1=gbc[:], op=ALU.mult),
             r=[kx, "gbc"], w=[kh])
        S.op("pool", lambda: nc.gpsimd.tensor_tensor(out=hh[:], in0=hh[:], in1=bbc[:], op=ALU.add),
             r=[kh, "bbc"], w=[kh])
        S.dma("sp", dst_dram[tt * 128:(tt + 1) * 128, :], hh[:], r=[kh], w=[("hd", tt)])
        if extra is not None:
            S.dma("sp", extra[tt * 128:(tt + 1) * 128, :], hh[:], r=[kh], w=[("hdx", tt)])
        if CUT < 4:
            return
        for half in range(2):
            pb, kp = self.psum()
            for j in range(4):
                kt = half * 4 + j
                S.op("pe", lambda j=j, kt=kt: nc.tensor.transpose(
                    out=pb[:, j * 128:(j + 1) * 128], in_=hh[:, kt * 128:(kt + 1) * 128], identity=self.ident32[:]),
                    r=[kh, "ident32"], w=[kp])
            if os.environ.get("EVAC", "act") == "act":
                S.op("act", lambda half=half, pb=pb: nc.scalar.activation(
                    out=self.hT[:, half * 4:(half + 1) * 4, tt * 128:(tt + 1) * 128],
                    in_=pb[:].rearrange("p (a b) -> p a b", a=4), func=AF.Copy), r=[kp], w=[("hT", tt), kp])
            else:
                S.op("dve", lambda half=half, pb=pb: nc.vector.tensor_copy(
                    out=self.hT[:, half * 4:(half + 1) * 4, tt * 128:(tt + 1) * 128],
                    in_=pb[:].rearrange("p (a b) -> p a b", a=4)), r=[kp], w=[("hT", tt), kp])
            if router:
                S.op("dve", lambda half=half, pb=pb: nc.vector.tensor_copy(
                    out=h32[:, half * 4:(half + 1) * 4, :], in_=pb[:].rearrange("p (a b) -> p a b", a=4)),
                    r=[kp], w=[k32, kp])
        if router and CUT >= 5:
            pb, kp = self.psum()
            for kt in range(KT):
                S.op("pe", lambda kt=kt: nc.tensor.matmul(pb[:, 0:NE], lhsT=h32[:, kt, :], rhs=self.rw32[:, kt, :],
                                                          start=(kt == 0), stop=(kt == KT - 1)),
                     r=[k32, "rw32"], w=[kp])
            S.op("dve", lambda: nc.vector.tensor_copy(out=self.logits[:, tt, :], in_=pb[:, 0:NE]),
                 r=[kp], w=["logits"])

    def router(self, st):
        nc, S = self.nc, self.S
        V = nc.vector
        L = self.logits
        t1 = self.sb(st, "rt1", [128, NT, NE], F32)
        probs = self.sb(st, "probs", [128, NT, NE], F32)
        sel = self.sb(st, "sel", [128, NT, NE], F32)
        p6 = self.sb(st, "p6", [128, NT, 4, 6], F32)
        gs = self.sb(st, "gs", [128, NT, 4], F32)
        gm = self.sb(st, "gm", [128, NT, 4], F32)
        gt = self.sb(st, "gt", [128, NT, 4], F32)
        red = self.sb(st, "red", [128, NT], F32)
        red2 = self.sb(st, "red2", [128, NT], F32)
        msk = self.sb(st, "msk", [128, NT, NE], F32)
        eq = self.sb(st, "eq", [128, NT, NE], F32)
        BIG = 1.0e9

        def bc(a):
            return a[:].unsqueeze(2).to_broadcast([128, NT, NE])

        S.op("dve", lambda: V.tensor_reduce(out=red[:], in_=L[:], axis=AX.X, op=ALU.max), r=["logits"], w=["red"])
        S.op("dve", lambda: V.tensor_tensor(out=t1[:], in0=L[:], in1=bc(red), op=ALU.subtract),
             r=["logits", "red"], w=["rt1"])
        S.op("act", lambda: nc.scalar.activation(out=t1[:], in_=t1[:], func=AF.Exp), r=["rt1"], w=["rt1"])
        S.op("dve", lambda: V.tensor_reduce(out=red[:], in_=t1[:], axis=AX.X, op=ALU.add), r=["rt1"], w=["red"])
        S.op("dve", lambda: V.reciprocal(out=red[:], in_=red[:]), r=["red"], w=["red"])
        S.op("dve", lambda: V.tensor_tensor(out=probs[:], in0=t1[:], in1=bc(red), op=ALU.mult),
             r=["rt1", "red"], w=["probs"])
        S.op("dve", lambda: V.tensor_tensor(out=sel[:], in0=probs[:],
                                            in1=self.rbias[:].unsqueeze(1).to_broadcast([128, NT, NE]), op=ALU.add),
             r=["probs", "rbias"], w=["sel"])
        s4 = sel[:].rearrange("p t (g e) -> p t g e", g=4)
        S.op("dve", lambda: V.tensor_tensor(out=p6[:, :, :, 0:3], in0=s4[:, :, :, 0:3], in1=s4[:, :, :, 1:4],
                                            op=ALU.add), r=["sel"], w=["p6"])
        S.op("dve", lambda: V.tensor_tensor(out=p6[:, :, :, 3:5], in0=s4[:, :, :, 0:2], in1=s4[:, :, :, 2:4],
                                            op=ALU.add), r=["sel"], w=["p6"])
        S.op("dve", lambda: V.tensor_tensor(out=p6[:, :, :, 5:6], in0=s4[:, :, :, 0:1], in1=s4[:, :, :, 3:4],
                                            op=ALU.add), r=["sel"], w=["p6"])
        S.op("dve", lambda: V.tensor_reduce(out=gs[:], in_=p6[:], axis=AX.X, op=ALU.max), r=["p6"], w=["gs"])
        S.op("dve", lambda: V.tensor_reduce(out=red[:], in_=gs[:], axis=AX.X, op=ALU.max), r=["gs"], w=["red"])
        S.op("dve", lambda: V.tensor_tensor(out=gm[:], in0=gs[:], in1=red[:].unsqueeze(2).to_broadcast([128, NT, 4]),
                                            op=ALU.is_ge), r=["gs", "red"], w=["gm"])
        S.op("dve", lambda: V.tensor_scalar(out=gt[:], in0=gm[:], scalar1=BIG, scalar2=-BIG, op0=ALU.mult,
                                            op1=ALU.add), r=["gm"], w=["gt"])
        m4 = msk[:].rearrange("p t (g e) -> p t g e", g=4)
        S.op("dve", lambda: V.tensor_tensor(out=m4, in0=s4, in1=gm[:].unsqueeze(3).to_broadcast([128, NT, 4, 4]),
                                            op=ALU.mult), r=["sel", "gm"], w=["msk"])
        S.op("dve", lambda: V.tensor_tensor(out=m4, in0=m4, in1=gt[:].unsqueeze(3).to_broadcast([128, NT, 4, 4]),
                                            op=ALU.add), r=["msk", "gt"], w=["msk"])
        S.op("dve", lambda: V.tensor_reduce(out=red[:], in_=msk[:], axis=AX.X, op=ALU.max), r=["msk"], w=["red"])
        S.op("dve", lambda: V.tensor_tensor(out=eq[:], in0=msk[:], in1=bc(red), op=ALU.is_equal),
             r=["msk", "red"], w=["eq"])
        S.op("dve", lambda: V.scalar_tensor_tensor(out=eq[:], in0=eq[:], scalar=-BIG, in1=msk[:], op0=ALU.mult,
                                                   op1=ALU.add), r=["eq", "msk"], w=["eq"])
        S.op("dve", lambda: V.tensor_reduce(out=red2[:], in_=eq[:], axis=AX.X, op=ALU.max), r=["eq"], w=["red2"])
        S.op("dve", lambda: V.tensor_tensor(out=eq[:], in0=msk[:], in1=bc(red2), op=ALU.is_ge),
             r=["msk", "red2"], w=["eq"])
        S.op("dve", lambda: V.tensor_tensor(out=eq[:], in0=eq[:], in1=probs[:], op=ALU.mult),
             r=["eq", "probs"], w=["eq"])
        S.op("dve", lambda: V.tensor_reduce(out=red[:], in_=eq[:], axis=AX.X, op=ALU.add), r=["eq"], w=["red"])
        S.op("dve", lambda: V.reciprocal(out=red[:], in_=red[:]), r=["red"], w=["red"])
        S.op("dve", lambda: V.tensor_tensor(out=self.gates[:], in0=eq[:], in1=bc(red), op=ALU.mult),
             r=["eq", "red"], w=["gates"])

    def moe(self, l, last):
        nc, S = self.nc, self.S
        wg_d, wu_d, wd_d = self.P["exp_w_gate"], self.P["exp_w_up"], self.P["exp_w_down"]
        with contextlib.ExitStack() as st:
            self.router(st)
            self.dbg_dump("gates%d" % l, lambda o: S.dma("sp", o, self.gates[:], r=["gates"]))
            acc = self.sb(st, "acc", [128, NT, D], F32)
            wg = [self.sb(st, "wg", [128, KT, DEXP], BF16) for _ in range(2)]
            wu = [self.sb(st, "wu", [128, KT, DEXP], BF16) for _ in range(2)]
            wd = [self.sb(st, "wd", [128, 4, D], BF16) for _ in range(2)]
            hg = [self.sb(st, "hg", [128, 4, 512], BF16) for _ in range(2)]
            sg = [self.sb(st, "sg", [128, 512], BF16) for _ in range(2)]
            gbc = self.sb(st, "gbc2", [128, D], F32)
            bbc = self.sb(st, "bbc2", [128, D], F32)
            S.dma("sp", gbc[:], self.P["ln2_g"][l].partition_broadcast(128), w=["gbc"])
            S.dma("sp", bbc[:], self.P["ln2_b"][l].partition_broadcast(128), w=["bbc"])

            def load_w(e):
                b = e % 2
                S.dma("pool", wg[b][:], wg_d[l, e].rearrange("(kt p) n -> p kt n", p=128), w=[("wg", b)])
                S.dma("pool", wu[b][:], wu_d[l, e].rearrange("(kt p) n -> p kt n", p=128), w=[("wu", b)])
                S.dma("pool", wd[b][:], wd_d[l, e].rearrange("(kt p) n -> p kt n", p=128), w=[("wd", b)])

            items = [(e, q) for e in range(int(os.environ.get('ME', NE))) for q in range(4)]
            sgi = [0]

            def G(i):
                e, q = items[i]
                b = e % 2
                hb = i % 2
                for dt_ in range(4):
                    pa, ka = self.psum()
                    pu, ku = self.psum()
                    for kt in range(KT):
                        S.op("pe", lambda kt=kt, pa=pa: nc.tensor.matmul(
                            pa[:], lhsT=wg[b][:, kt, dt_ * 128:(dt_ + 1) * 128], rhs=self.hT[:, kt, q * 512:(q + 1) * 512],
                            start=(kt == 0), stop=(kt == KT - 1)),
                            r=[("wg", b)] + [("hT", q * 4 + j) for j in range(4)], w=[ka])
                    for kt in range(KT):
                        S.op("pe", lambda kt=kt, pu=pu: nc.tensor.matmul(
                            pu[:], lhsT=wu[b][:, kt, dt_ * 128:(dt_ + 1) * 128], rhs=self.hT[:, kt, q * 512:(q + 1) * 512],
                            start=(kt == 0), stop=(kt == KT - 1)),
                            r=[("wu", b)] + [("hT", q * 4 + j) for j in range(4)], w=[ku])
                    si = sgi[0] % 2
                    sgi[0] += 1
                    S.op("act", lambda pa=pa, si=si: nc.scalar.activation(out=sg[si][:], in_=pa[:], func=AF.Silu),
                         r=[ka], w=[("sg", si)])
                    S.op("dve", lambda pu=pu, si=si: nc.vector.tensor_tensor(
                        out=hg[hb][:, dt_, :], in0=pu[:], in1=sg[si][:], op=ALU.mult),
                        r=[ku, ("sg", si)], w=[("hg", hb)])

            def Dn(i):
                e, q = items[i]
                b = e % 2
                hb = i % 2
                for j in range(4):
                    tt = q * 4 + j
                    for half in range(2):
                        pc, kc = self.psum()
                        for dt_ in range(4):
                            S.op("pe", lambda dt_=dt_, pc=pc: nc.tensor.matmul(
                                pc[:], lhsT=hg[hb][:, dt_, j * 128:(j + 1) * 128],
                                rhs=wd[b][:, dt_, half * 512:(half + 1) * 512], start=(dt_ == 0), stop=(dt_ == 3)),
                                r=[("hg", hb), ("wd", b)], w=[kc])
                        dst = acc[:, tt, half * 512:(half + 1) * 512]
                        if e == 0:
                            S.op("dve", lambda pc=pc, dst=dst: nc.vector.tensor_scalar(
                                out=dst, in0=pc[:], scalar1=self.gates[:, tt, e:e + 1], scalar2=None, op0=ALU.mult),
                                r=[kc, "gates"], w=[("acc", tt)])
                        else:
                            S.op("dve", lambda pc=pc, dst=dst: nc.vector.scalar_tensor_tensor(
                                out=dst, in0=pc[:], scalar=self.gates[:, tt, e:e + 1], in1=dst, op0=ALU.mult,
                                op1=ALU.add), r=[kc, "gates", ("acc", tt)], w=[("acc", tt)])

            load_w(0)
            for i in range(len(items)):
                e, q = items[i]
                G(i)
                if i >= 1:
                    Dn(i - 1)
                if q == 0 and e + 1 < int(os.environ.get('ME', NE)):
                    load_w(e + 1)
            Dn(len(items) - 1)
            self.dbg_dump("moe%d" % l, lambda o: S.dma("sp", o.rearrange("(t p) d -> p t d", p=128), acc[:],
                                                       r=[("acc", t) for t in range(NT)]))
            lnw = self.ln_alloc(st)
            h1 = [self.sb(st, "h1t", [128, D], F32) for _ in range(2)]
            dst = self.out if last else self.h_dram
            for tt in range(NT):
                s = tt % 2
                S.dma("sp", h1[s][:], self.h_dram[tt * 128:(tt + 1) * 128, :], r=[("hd", tt)], w=[("h1t", s)])
                xin, kx = self.ln_xin(lnw, tt)
                S.op("dve", lambda s=s, xin=xin: nc.vector.scalar_tensor_tensor(
                    out=xin[:], in0=h1[s][:], scalar=ALPHA, in1=acc[:, tt, :], op0=ALU.mult, op1=ALU.add),
                    r=[("h1t", s), ("acc", tt)], w=[kx])
                self.ln_tile(lnw, tt, gbc, bbc, dst, router=False)
            S.barrier()


    def hgrn(self, l):
        nc, S = self.nc, self.S
        V, A, G, PE = nc.vector, nc.scalar, nc.gpsimd, nc.tensor
        Wl = self.P["w_in"][l]
        ydst = self.yT_dram[2]
        with contextlib.ExitStack() as st:
            mask2 = self.sb(st, "mask2", [128, 128], F32)
            rm = self.sb(st, "rm", [128, T], F32)
            nw = self.sb(st, "nw", [128, 1], F32)
            lbt = self.sb(st, "lbt", [128, 8, 2], F32)
            lbv = self.sb(st, "lbv", [128, 8], F32)
            oml = self.sb(st, "oml", [128, 8], F32)
            S.op("pool", lambda: G.affine_select(out=mask2[:], in_=self.ones[:], pattern=[[1, 128]],
                                                 compare_op=ALU.is_ge, fill=0.0, base=0, channel_multiplier=-1),
                 r=["ones"], w=["mask2"])
            S.op("pool", lambda: G.memset(mask2[0:64, 64:128], 0.0), w=["mask2"])
            S.op("pool", lambda: G.memset(rm[:], 1.0), w=["rm"])
            S.op("pool", lambda: G.memset(rm[:].rearrange("p (c j) -> p c j", j=64)[:, :, 0:1], 0.0), w=["rm"])
            S.dma("sp", nw[:], self.P["hgrn_norm_w"][l].rearrange("(p o) -> p o", o=1), w=["nw"])
            if l == 0:
                S.op("pool", lambda: G.memset(lbv[:], 0.0), w=["lbv"])
                S.op("pool", lambda: G.memset(oml[:], 1.0), w=["oml"])
            else:
                for j in range(2):
                    S.dma("sp", lbt[:, :, j:j + 1],
                          self.P["hgrn_lb"][j].rearrange("(h p o) -> p h o", p=128, o=1), w=["lbt"])
                S.op("dve", lambda: V.tensor_tensor(out=lbv[:], in0=lbt[:, :, 1], in1=lbt[:, :, 0], op=ALU.subtract),
                     r=["lbt"], w=["lbv"])
                S.op("act", lambda: A.activation(out=lbv[:], in_=lbv[:], func=AF.Sigmoid), r=["lbv"], w=["lbv"])
                S.op("dve", lambda: V.tensor_scalar(out=oml[:], in0=lbv[:], scalar1=-1.0, scalar2=1.0, op0=ALU.mult,
                                                    op1=ALU.add), r=["lbv"], w=["oml"])
            w4 = [self.sb(st, "w4", [128, 4, KT, 128], BF16) for _ in range(2)]
            qs = self.sb(st, "qs", [128, T], F32)
            fs = self.sb(st, "fs", [128, T], F32)
            lf = self.sb(st, "lf", [128, T], F32)
            bc = self.sb(st, "bc", [128, T], F32)
            eb = self.sb(st, "eb", [128, T], F32)
            enb = self.sb(st, "enb", [128, T], F32)
            qb = self.sb(st, "qb", [128, T], BF16)
            kb = self.sb(st, "kb", [128, T], BF16)
            gs = self.sb(st, "gs", [128, T], BF16)
            yt = self.sb(st, "yt", [128, T], BF16)
            v = self.sb(st, "v", [128, NT, 128], BF16)
            kbt = self.sb(st, "kbt", [128, NT, 128], BF16)
            kbtB = self.sb(st, "kbtB", [128, NT, 128], BF16)
            mAB = self.sb(st, "mAB", [128, 2], F32)
            S.op("pool", lambda: G.memset(mAB[0:64, 0:1], 1.0), w=["mAB"])
            S.op("pool", lambda: G.memset(mAB[64:128, 0:1], 0.0), w=["mAB"])
            S.op("pool", lambda: G.memset(mAB[0:64, 1:2], 0.0), w=["mAB"])
            S.op("pool", lambda: G.memset(mAB[64:128, 1:2], 1.0), w=["mAB"])
            S32 = self.sb(st, "S32", [128, 128], F32)
            Sbf = [self.sb(st, "Sbf", [128, 128], BF16) for _ in range(4)]
            attm = [self.sb(st, "attm", [128, 128], BF16) for _ in range(2)]
            osb = [self.sb(st, "osb", [128, 128], F32) for _ in range(2)]
            osq = [self.sb(st, "osq", [128, 128], BF16) for _ in range(2)]
            sd = [self.sb(st, "sd", [128, 128], F32) for _ in range(2)]
            hTk = [("hT", t) for t in range(NT)]

            def load_w(h):
                b = h % 2
                for j in range(4):
                    c0 = OFF_HGRN + j * 1024 + h * 128
                    S.dma("pool", w4[b][:, j], Wl[:, c0:c0 + 128].rearrange("(kt p) n -> p kt n", p=128),
                          w=[("w4", b)])

            if HC >= 2:
                load_w(0)
            for h in range(8 if HC >= 6 else (1 if HC >= 2 else 0)):
                b = h % 2
                if h + 1 < 8 and HC >= 6:
                    load_w(h + 1)
                for (j, func, dst, kd) in ((0, AF.Silu, qs, "qs"), (1, AF.Sigmoid, fs, "fs"), (3, AF.Sigmoid, gs, "gs")):
                    for tq in range(4):
                        pb, kp = self.psum()
                        for kt in range(KT):
                            S.op("pe", lambda kt=kt, pb=pb, j=j, tq=tq: PE.matmul(
                                pb[:], lhsT=w4[b][:, j, kt, :], rhs=self.hT[:, kt, tq * 512:(tq + 1) * 512],
                                start=(kt == 0), stop=(kt == KT - 1)), r=[("w4", b)] + hTk[tq * 4:tq * 4 + 4], w=[kp])
                        S.op("act", lambda pb=pb, dst=dst, func=func, tq=tq: A.activation(
                            out=dst[:, tq * 512:(tq + 1) * 512], in_=pb[:], func=func), r=[kp], w=[kd, kp])
                for t4 in range(4):
                    pb, kp = self.psum()
                    for j4 in range(4):
                        tt = t4 * 4 + j4
                        for kt in range(KT):
                            S.op("pe", lambda kt=kt, pb=pb, j4=j4, tt=tt: PE.matmul(
                                pb[:, j4 * 128:(j4 + 1) * 128], lhsT=self.hT[:, kt, tt * 128:(tt + 1) * 128],
                                rhs=w4[b][:, 2, kt, :], start=(kt == 0), stop=(kt == KT - 1)),
                                r=[("w4", b), ("hT", tt)], w=[kp])
                    S.op("dve", lambda pb=pb, t4=t4: V.tensor_copy(
                        out=v[:, t4 * 4:(t4 + 1) * 4, :], in_=pb[:].rearrange("p (a b) -> p a b", a=4)),
                        r=[kp], w=["v", kp])
                if HC < 3:
                    continue
                S.op("dve", lambda: V.tensor_scalar(out=fs[:], in0=fs[:], scalar1=oml[:, h:h + 1], scalar2=lbv[:, h:h + 1],
                                                    op0=ALU.mult, op1=ALU.add), r=["fs", "oml", "lbv"], w=["fs"])
                S.op("act", lambda: A.activation(out=lf[:], in_=fs[:], func=AF.Ln), r=["fs"], w=["lf"])
                S.op("dve", lambda: V.tensor_tensor_scan(out=bc[:], data0=rm[:], data1=lf[:], initial=0.0,
                                                         op0=ALU.mult, op1=ALU.add), r=["rm", "lf"], w=["bc"])
                S.op("act", lambda: A.activation(out=eb[:], in_=bc[:], func=AF.Exp), r=["bc"], w=["eb"])
                S.op("act", lambda: A.activation(out=enb[:], in_=bc[:], func=AF.Exp, scale=-1.0), r=["bc"], w=["enb"])
                S.op("dve", lambda: V.tensor_scalar(out=fs[:], in0=fs[:], scalar1=-1.0, scalar2=1.0, op0=ALU.mult,
                                                    op1=ALU.add), r=["fs", "lf"], w=["fs"])
                S.op("pool", lambda: G.tensor_tensor(out=qb[:], in0=qs[:], in1=eb[:], op=ALU.mult),
                     r=["qs", "eb"], w=["qb"])
                S.op("dve", lambda: V.tensor_tensor(out=kb[:], in0=fs[:], in1=enb[:], op=ALU.mult),
                     r=["fs", "enb"], w=["kb"])
                if HC < 4:
                    continue
                for t4 in range(4):
                    pb, kp = self.psum()
                    pbv = pb[:].bitcast(BF16)
                    for j4 in range(4):
                        tt = t4 * 4 + j4
                        S.op("pe", lambda pbv=pbv, j4=j4, tt=tt: PE.transpose(
                            out=pbv[:, j4 * 128:(j4 + 1) * 128], in_=kb[:, tt * 128:(tt + 1) * 128],
                            identity=self.identbf[:]), r=["kb", "identbf"], w=[kp])
                    S.op("dve", lambda pbv=pbv, t4=t4: V.tensor_scalar(
                        out=kbt[:, t4 * 4:(t4 + 1) * 4, :], in0=pbv[:, 0:512].rearrange("p (a b) -> p a b", a=4),
                        scalar1=mAB[:, 0:1], scalar2=None, op0=ALU.mult), r=[kp, "mAB"], w=["kbt", kp])
                    S.op("dve", lambda pbv=pbv, t4=t4: V.tensor_scalar(
                        out=kbtB[:, t4 * 4:(t4 + 1) * 4, :], in0=pbv[:, 0:512].rearrange("p (a b) -> p a b", a=4),
                        scalar1=mAB[:, 1:2], scalar2=None, op0=ALU.mult), r=[kp, "mAB"], w=["kbtB", kp])
                if HC < 5:
                    continue
                S.op("pool", lambda: G.memset(S32[:], 0.0), w=["S32"])
                S.op("pool", lambda: G.memset(Sbf[0][:], 0.0), w=[("Sbf", 0)])
                for tt in range(NT):
                    cA, cB = 2 * tt, 2 * tt + 1
                    tsl = slice(tt * 128, (tt + 1) * 128)
                    i2 = tt % 2
                    pa, kpa = self.psum()
                    S.op("pe", lambda pa=pa: PE.matmul(pa[:, 0:128], lhsT=kb[:, tsl], rhs=qb[:, tsl], start=True, stop=True),
                         r=["kb", "qb"], w=[kpa])
                    S.op("dve", lambda pa=pa: V.tensor_tensor(out=attm[i2][:], in0=pa[:, 0:128], in1=mask2[:], op=ALU.mult),
                         r=[kpa, "mask2"], w=[("attm", i2), kpa])
                    if HL < 2:
                        continue
                    pr, kpr = self.psum()
                    S.op("pe", lambda pr=pr: PE.matmul(pr[:, 0:128], lhsT=kbt[:, tt, :], rhs=v[:, tt, :],
                                                       start=True, stop=True), r=["kbt", "v"], w=[kpr])
                    S.op("pe", lambda pr=pr: PE.matmul(pr[:, 128:256], lhsT=kbtB[:, tt, :], rhs=v[:, tt, :],
                                                       start=True, stop=True), r=["kbtB", "v"], w=[kpr])
                    for (ci, off) in ((cA, 0), (cB, 128)):
                        S.op("dve", lambda pr=pr, off=off: V.tensor_tensor(
                            out=S32[:], in0=pr[:, off:off + 128], in1=S32[:], op=ALU.add), r=[kpr, "S32"], w=["S32", kpr])
                        S.op("dve", lambda ci=ci: V.tensor_scalar(
                            out=S32[:], in0=S32[:], scalar1=eb[:, ci * 64 + 63:ci * 64 + 64], scalar2=None, op0=ALU.mult),
                            r=["S32", "eb"], w=["S32"])
                        S.op("pool", lambda ci=ci: G.tensor_copy(out=Sbf[(ci + 1) % 4][:], in_=S32[:]),
                             r=["S32"], w=[("Sbf", (ci + 1) % 4)])
                    if HL < 3:
                        continue
                    po, kpo = self.psum()
                    S.op("pe", lambda po=po: PE.matmul(po[:, 0:128], lhsT=v[:, tt, :], rhs=attm[i2][:], start=True, stop=False),
                         r=["v", ("attm", i2)], w=[kpo])
                    S.op("pe", lambda po=po: PE.matmul(po[:, 0:64], lhsT=Sbf[cA % 4][:], rhs=qb[:, tt * 128:tt * 128 + 64],
                                                       start=False, stop=False), r=[("Sbf", cA % 4), "qb"], w=[kpo])
                    S.op("pe", lambda po=po: PE.matmul(po[:, 64:128], lhsT=Sbf[cB % 4][:],
                                                       rhs=qb[:, tt * 128 + 64:(tt + 1) * 128], start=False, stop=True),
                         r=[("Sbf", cB % 4), "qb"], w=[kpo])
                    if HL < 4:
                        continue
                    S.op("act", lambda po=po: A.activation(out=osb[i2][:], in_=po[:, 0:128], func=AF.Copy),
                         r=[kpo], w=[("osb", i2), kpo])
                    S.op("act", lambda po=po: A.activation(out=osq[i2][:], in_=po[:, 0:128], func=AF.Square),
                         r=[kpo], w=[("osq", i2), kpo])
                    if HL < 5:
                        continue
                    pss, kps = self.psum()
                    S.op("pe", lambda pss=pss: PE.matmul(pss[:, 0:128], lhsT=self.onesbf[:], rhs=osq[i2][:], start=True,
                                                         stop=True), r=["onesbf", ("osq", i2)], w=[kps])
                    S.op("act", lambda pss=pss: A.activation(out=sd[i2][:], in_=pss[:, 0:128], func=AF.Sqrt,
                                                             bias=self.epsc[:, 1:2], scale=1.0 / 128.0),
                         r=[kps, "epsc"], w=[("sd", i2), kps])
                    S.op("dve", lambda: V.reciprocal(out=sd[i2][:], in_=sd[i2][:]), r=[("sd", i2)], w=[("sd", i2)])
                    S.op("dve", lambda: V.scalar_tensor_tensor(out=osb[i2][:], in0=osb[i2][:], scalar=nw[:, 0:1],
                                                               in1=sd[i2][:], op0=ALU.mult, op1=ALU.mult),
                         r=[("osb", i2), ("sd", i2), "nw"], w=[("osb", i2)])
                    S.op("dve", lambda: V.tensor_tensor(out=yt[:, tsl], in0=osb[i2][:], in1=gs[:, tsl], op=ALU.mult),
                         r=[("osb", i2), "gs"], w=["yt"])
                S.dma("sp", ydst[h * 128:(h + 1) * 128, :], yt[:], r=["yt"], w=[("yT2", h)])
            S.barrier()


    def ssd(self, l):
        nc, S = self.nc, self.S
        V, A, G, PE = nc.vector, nc.scalar, nc.gpsimd, nc.tensor
        Wl = self.P["w_in"][l]
        ydst = self.yT_dram[0]
        NEG = -30000.0
        hTk = [("hT", t) for t in range(NT)]
        with contextlib.ExitStack() as st:
            tri2 = self.sb(st, "tri2", [128, 128], F32)
            same2 = self.sb(st, "same2", [128, 128], F32)
            indA = self.sb(st, "indA", [128, 128], F32)
            indB = self.sb(st, "indB", [128, 128], F32)
            mAB = self.sb(st, "mABs", [128, 2], F32)
            negmask = self.sb(st, "negmask", [128, 8, 128], F32)
            bd1 = self.sb(st, "bd", [16, 8, 128], F32)
            bd = [bd1, bd1]
            S.op("pool", lambda: G.affine_select(out=tri2[:], in_=self.ones[:], pattern=[[1, 128]], compare_op=ALU.is_ge,
                                                 fill=0.0, base=0, channel_multiplier=-1), r=["ones"], w=["tri2"])
            S.op("pool", lambda: G.memset(tri2[0:64, 64:128], 0.0), w=["tri2"])
            S.op("pool", lambda: G.memset(same2[:], 0.0), w=["same2"])
            S.op("pool", lambda: G.memset(same2[0:64, 0:64], 1.0), w=["same2"])
            S.op("pool", lambda: G.memset(same2[64:128, 64:128], 1.0), w=["same2"])
            S.op("pool", lambda: G.memset(indA[0:64, :], 1.0), w=["indA"])
            S.op("pool", lambda: G.memset(indA[64:128, :], 0.0), w=["indA"])
            S.op("pool", lambda: G.memset(indB[0:64, :], 0.0), w=["indB"])
            S.op("pool", lambda: G.memset(indB[64:128, :], 1.0), w=["indB"])
            S.op("pool", lambda: G.memset(mAB[0:64, 0:1], 1.0), w=["mAB"])
            S.op("pool", lambda: G.memset(mAB[64:128, 0:1], 0.0), w=["mAB"])
            S.op("pool", lambda: G.memset(mAB[0:64, 1:2], 0.0), w=["mAB"])
            S.op("pool", lambda: G.memset(mAB[64:128, 1:2], 1.0), w=["mAB"])
            S.op("pool", lambda: G.memset(negmask[:], 0.0), w=["negmask"])
            S.op("pool", lambda: G.affine_select(out=negmask[:], in_=negmask[:], pattern=[[0, 8], [1, 128]],
                                                 compare_op=ALU.is_ge, fill=NEG, base=0, channel_multiplier=-1),
                 r=["negmask"], w=["negmask"])
            S.op("pool", lambda: G.memset(negmask[0:64, :, 64:128], NEG), w=["negmask"])
            dtb = self.sb(st, "dtb", [128, 16], F32)
            alog = self.sb(st, "alog", [128, 16], F32)
            dsk = self.sb(st, "dsk", [128, 16], F32)
            nwbc = self.sb(st, "nwbc", [128, D], F32)
            S.dma("sp", dtb[:], self.P["ssd_dt_bias"][l].partition_broadcast(128), w=["dtb"])
            S.dma("sp", alog[:], self.P["ssd_a_log"][l].partition_broadcast(128), w=["alog"])
            S.dma("sp", dsk[:], self.P["ssd_d"][l].partition_broadcast(128), w=["dsk"])
            S.dma("sp", nwbc[:], self.P["ssd_norm_w"][l].partition_broadcast(128), w=["nwbc"])
            S.op("act", lambda: A.activation(out=alog[:], in_=alog[:], func=AF.Exp), r=["alog"], w=["alog"])
            S.op("dve", lambda: V.tensor_scalar(out=alog[:], in0=alog[:], scalar1=-1.0, scalar2=None, op0=ALU.mult),
                 r=["alog"], w=["alog"])
            wdt = self.sb(st, "wdt", [128, KT, 16], BF16)
            S.dma("pool", wdt[:], Wl[:, OFF_DT:OFF_DT + 16].rearrange("(kt p) n -> p kt n", p=128), w=["wdt"])
            dt = self.sb(st, "dt", [128, NT, 16], F32)
            da = self.sb(st, "da", [128, NT, 16], F32)
            cum4 = self.sb(st, "cum4", [128, NT, 4, 16], F32)
            eacs = self.sb(st, "eacs", [128, NT, 16], F32)
            eend = self.sb(st, "eend", [128, NT, 16], F32)
            edec = self.sb(st, "edec", [128, NT, 2, 16], F32)
            acsTt = [self.sb(st, "acsTt", [16, 128], F32) for _ in range(2)]
            nacsTt = [self.sb(st, "nacsTt", [16, 128], F32) for _ in range(2)]
            pb, kp = self.psum()
            for tt in range(NT):
                for kt in range(KT):
                    S.op("pe", lambda kt=kt, tt=tt: PE.matmul(pb[:, tt * 16:(tt + 1) * 16], lhsT=self.hT[:, kt, tt * 128:(tt + 1) * 128],
                                                              rhs=wdt[:, kt, :], start=(kt == 0), stop=(kt == KT - 1)),
                         r=["wdt", ("hT", tt)], w=[kp])
            S.op("dve", lambda: V.tensor_tensor(out=dt[:], in0=pb[:, 0:256].rearrange("p (t h) -> p t h", h=16),
                                                in1=dtb[:].unsqueeze(1).to_broadcast([128, NT, 16]), op=ALU.add),
                 r=[kp, "dtb"], w=["dt", kp])
            S.op("act", lambda: A.activation(out=dt[:], in_=dt[:], func=AF.Exp), r=["dt"], w=["dt"])
            S.op("act", lambda: A.activation(out=dt[:], in_=dt[:], func=AF.Ln, bias=self.epsc[:, 3:4], scale=1.0),
                 r=["dt", "epsc"], w=["dt"])
            S.op("dve", lambda: V.tensor_tensor(out=da[:], in0=dt[:], in1=alog[:].unsqueeze(1).to_broadcast([128, NT, 16]),
                                                op=ALU.mult), r=["dt", "alog"], w=["da"])
            for half in range(2):
                pb, kp = self.psum()
                for j in range(8):
                    tt = half * 8 + j
                    for qi, L in enumerate((tri2, same2, indA, indB)):
                        S.op("pe", lambda j=j, qi=qi, L=L, tt=tt, pb=pb: PE.matmul(
                            pb[:, j * 64 + qi * 16:j * 64 + (qi + 1) * 16], lhsT=L[:], rhs=da[:, tt, :], start=True, stop=True),
                            r=["da", "tri2", "same2", "indA", "indB"], w=[kp])
                S.op("dve", lambda pb=pb, half=half: V.tensor_copy(
                    out=cum4[:, half * 8:(half + 1) * 8].rearrange("p t q h -> p (t q h)"), in_=pb[:]),
                    r=[kp], w=["cum4", kp])
            S.op("act", lambda: A.activation(out=eacs[:], in_=cum4[:, :, 0, :], func=AF.Exp), r=["cum4"], w=["eacs"])
            S.op("dve", lambda: V.tensor_tensor(out=eend[:], in0=cum4[:, :, 1, :], in1=cum4[:, :, 0, :], op=ALU.subtract),
                 r=["cum4"], w=["eend"])
            S.op("act", lambda: A.activation(out=eend[:], in_=eend[:], func=AF.Exp), r=["eend"], w=["eend"])
            S.op("act", lambda: A.activation(out=edec[:], in_=cum4[:, :, 2:4, :], func=AF.Exp), r=["cum4"], w=["edec"])
            wx = self.sb(st, "wx", [128, KT, 768], BF16)
            wz = self.sb(st, "wz", [128, KT, 512], BF16)
            cw = self.sb(st, "cw", [128, 6, 4], F32)
            cbi = self.sb(st, "cbi", [128, 6], F32)
            xp1 = self.sb(st, "xp", [128, T + 3], F32)
            xp = [xp1, xp1]
            fTa = [self.sb(st, "fT", [128, T], BF16) for _ in range(4)]
            fT = [fTa[0], fTa[1], fTa[0], fTa[1], fTa[2], fTa[3]]
            fk = [("fT", 0), ("fT", 1), ("fT", 0), ("fT", 1), ("fT", 2), ("fT", 3)]
            cmTA = self.sb(st, "cmTA", [128, T], BF16)
            cmTB = self.sb(st, "cmTB", [128, T], BF16)
            xs = self.sb(st, "xs", [128, NT, 512], BF16)
            xdtt = [self.sb(st, "xdtt", [128, 512], BF16) for _ in range(2)]
            xendt = [self.sb(st, "xendt", [128, 512], BF16) for _ in range(2)]
            bmA = self.sb(st, "bmA", [128, NT, 128], BF16)
            bmB = self.sb(st, "bmB", [128, NT, 128], BF16)
            yTg = self.sb(st, "yTg", [128, 4, T], BF16)
            S32 = self.sb(st, "S32s", [128, 512], F32)
            Sbf = [self.sb(st, "Sbfs", [128, 512], BF16) for _ in range(4)]
            cbs = [self.sb(st, "cbs", [128, 128], BF16) for _ in range(2)]
            Dx = [self.sb(st, "Dx", [16, 8, 128], F32) for _ in range(2)]
            Es = [self.sb(st, "Es", [128, 8, 128], BF16) for _ in range(2)]
            wT = [self.sb(st, "wT", [128, 8, 128], BF16) for _ in range(2)]
            t1 = [self.sb(st, "t1", [128, 512], F32) for _ in range(2)]
            t2 = [self.sb(st, "t2", [128, 512], F32) for _ in range(2)]
            zs = [self.sb(st, "zs", [128, 512], BF16) for _ in range(2)]
            ytm = [self.sb(st, "ytm", [128, 512], BF16) for _ in range(2)]
            ss = [self.sb(st, "ss", [128, 2], F32) for _ in range(2)]
            S.op("pool", lambda: G.memset(xp[0][:, 0:3], 0.0), w=[("xp", 0)])
            for g in range(2):
                S.op("pool", lambda g=g: G.memset(bd[g][:], 1.0), r=[("bd", 0), ("bd", 1)], w=[("bd", 0), ("bd", 1)])
                S.op("pool", lambda g=g: G.affine_select(out=bd[g][:], in_=bd[g][:], pattern=[[1, 8], [0, 128]],
                                                         compare_op=ALU.is_equal, fill=0.0, base=8 * g,
                                                         channel_multiplier=-1), r=[("bd", 0), ("bd", 1)], w=[("bd", 0), ("bd", 1)])
                choff = [g * 512 + i * 128 for i in range(4)] + [1024 + g * 128, 1280 + g * 128]
                S.dma("pool", wx[:, :, 0:512], Wl[:, OFF_XBC + g * 512:OFF_XBC + (g + 1) * 512].rearrange("(kt p) n -> p kt n", p=128), w=["wx"])
                S.dma("pool", wx[:, :, 512:640], Wl[:, OFF_XBC + 1024 + g * 128:OFF_XBC + 1024 + (g + 1) * 128].rearrange("(kt p) n -> p kt n", p=128), w=["wx"])
                S.dma("pool", wx[:, :, 640:768], Wl[:, OFF_XBC + 1280 + g * 128:OFF_XBC + 1280 + (g + 1) * 128].rearrange("(kt p) n -> p kt n", p=128), w=["wx"])
                S.dma("pool", wz[:], Wl[:, OFF_Z + g * 512:OFF_Z + (g + 1) * 512].rearrange("(kt p) n -> p kt n", p=128), w=["wz"])
                for ci in range(6):
                    for j in range(4):
                        S.dma("sp", cw[:, ci, j:j + 1], self.P["ssd_conv_w"][l, j, choff[ci]:choff[ci] + 128].rearrange("(p o) -> p o", o=1), w=["cw"])
                    S.dma("sp", cbi[:, ci:ci + 1], self.P["ssd_conv_b"][l, choff[ci]:choff[ci] + 128].rearrange("(p o) -> p o", o=1), w=["cbi"])
                for ci in range(6):
                    xb = xp[0]
                    kx = ("xp", 0)
                    for tq in range(4):
                        pb, kp = self.psum()
                        for kt in range(KT):
                            S.op("pe", lambda kt=kt, pb=pb, ci=ci, tq=tq: PE.matmul(
                                pb[:], lhsT=wx[:, kt, ci * 128:(ci + 1) * 128], rhs=self.hT[:, kt, tq * 512:(tq + 1) * 512],
                                start=(kt == 0), stop=(kt == KT - 1)), r=["wx"] + hTk[tq * 4:tq * 4 + 4], w=[kp])
                        S.op("act", lambda pb=pb, xb=xb, tq=tq: A.activation(out=xb[:, 3 + tq * 512:3 + (tq + 1) * 512], in_=pb[:],
                                                                            func=AF.Copy), r=[kp], w=[kx, kp])
                    acc = t1[0] if False else None
                    cacc = self.sb(st, "cacc", [128, T], F32) if (g == 0 and ci == 0) else self._cacc
                    self._cacc = cacc
                    S.op("dve", lambda xb=xb, ci=ci, cacc=cacc: V.tensor_scalar(
                        out=cacc[:], in0=xb[:, 3:3 + T], scalar1=cw[:, ci, 3:4], scalar2=cbi[:, ci:ci + 1], op0=ALU.mult,
                        op1=ALU.add), r=[kx, "cw", "cbi"], w=["cacc"])
                    for j in range(3):
                        S.op("dve", lambda xb=xb, ci=ci, j=j, cacc=cacc: V.scalar_tensor_tensor(
                            out=cacc[:], in0=xb[:, j:j + T], scalar=cw[:, ci, j:j + 1], in1=cacc[:], op0=ALU.mult, op1=ALU.add),
                            r=[kx, "cw", "cacc"], w=["cacc"])
                    S.op("act", lambda ci=ci, cacc=cacc: A.activation(out=fT[ci][:], in_=cacc[:], func=AF.Silu),
                         r=["cacc"], w=[fk[ci]])
                    if ci < 5:
                        for t4 in range(4):
                            pb, kp = self.psum()
                            pbv = pb[:].bitcast(BF16)
                            for j in range(4):
                                tt = t4 * 4 + j
                                S.op("pe", lambda j=j, tt=tt, pbv=pbv, ci=ci: PE.transpose(
                                    out=pbv[:, j * 128:(j + 1) * 128], in_=fT[ci][:, tt * 128:(tt + 1) * 128],
                                    identity=self.identbf[:]), r=[fk[ci], "identbf"], w=[kp])
                            src = pbv[:, 0:512].rearrange("p (a b) -> p a b", a=4)
                            if ci < 4:
                                S.op("act", lambda t4=t4, src=src, ci=ci: A.activation(
                                    out=xs[:, t4 * 4:(t4 + 1) * 4, ci * 128:(ci + 1) * 128], in_=src, func=AF.Copy),
                                    r=[kp], w=["xs", kp])
                            else:
                                S.op("dve", lambda t4=t4, src=src: V.tensor_scalar(
                                    out=bmA[:, t4 * 4:(t4 + 1) * 4, :], in0=src, scalar1=mAB[:, 0:1], scalar2=None, op0=ALU.mult),
                                    r=[kp, "mAB"], w=["bmA", kp])
                                S.op("dve", lambda t4=t4, src=src: V.tensor_scalar(
                                    out=bmB[:, t4 * 4:(t4 + 1) * 4, :], in0=src, scalar1=mAB[:, 1:2], scalar2=None, op0=ALU.mult),
                                    r=[kp, "mAB"], w=["bmB", kp])
                bmT, cmT = fT[4], fT[5]
                cv = cmT[:].rearrange("p (t c j) -> p t c j", c=2, j=64)
                cva = cmTA[:].rearrange("p (t c j) -> p t c j", c=2, j=64)
                cvb = cmTB[:].rearrange("p (t c j) -> p t c j", c=2, j=64)
                S.op("pool", lambda: G.tensor_copy(out=cva[:, :, 0, :], in_=cv[:, :, 0, :]), r=[("fT", 3)], w=["cmTA"])
                S.op("pool", lambda: G.memset(cva[:, :, 1, :], 0.0), w=["cmTA"])
                S.op("pool", lambda: G.tensor_copy(out=cvb[:, :, 1, :], in_=cv[:, :, 1, :]), r=[("fT", 3)], w=["cmTB"])
                S.op("pool", lambda: G.memset(cvb[:, :, 0, :], 0.0), w=["cmTB"])
                hs = slice(g * 8, (g + 1) * 8)
                S.op("pool", lambda: G.memset(S32[:], 0.0), w=["S32"])
                S.op("pool", lambda: G.memset(Sbf[0][:], 0.0), w=[("Sbf", 0)])
                for tt in range(NT):
                    i2 = tt % 2
                    tsl = slice(tt * 128, (tt + 1) * 128)
                    cA, cB = 2 * tt, 2 * tt + 1
                    pc, kpc = self.psum()
                    S.op("pe", lambda pc=pc: PE.matmul(pc[:, 0:128], lhsT=bmT[:, tsl], rhs=cmT[:, tsl], start=True, stop=True),
                         r=[("fT", 2), ("fT", 3)], w=[kpc])
                    S.op("act", lambda pc=pc: A.activation(out=cbs[i2][:], in_=pc[:, 0:128], func=AF.Copy),
                         r=[kpc], w=[("cbs", i2), kpc])
                    pq, kpq = self.psum()
                    S.op("pe", lambda pq=pq: PE.matmul(pq[0:16, 0:128], lhsT=da[:, tt, :], rhs=tri2[:], start=True, stop=True),
                         r=["da", "tri2"], w=[kpq])
                    S.op("dve", lambda pq=pq: V.tensor_copy(out=acsTt[i2][:], in_=pq[0:16, 0:128]), r=[kpq], w=[("acsTt", i2), kpq])
                    S.op("dve", lambda pq=pq: V.tensor_scalar(out=nacsTt[i2][:], in0=pq[0:16, 0:128], scalar1=-1.0, scalar2=None,
                                                              op0=ALU.mult), r=[kpq], w=[("nacsTt", i2), kpq])
                    S.op("pool", lambda: G.tensor_tensor(out=Dx[i2][:], in0=bd[g][:],
                                                         in1=acsTt[i2][:].unsqueeze(1).to_broadcast([16, 8, 128]), op=ALU.mult),
                         r=[("bd", g), ("acsTt", i2)], w=[("Dx", i2)])
                    for hh in range(2):
                        pe_, kpe = self.psum()
                        csl = slice(hh * 512, (hh + 1) * 512)
                        S.op("pe", lambda pe_=pe_, csl=csl: PE.matmul(
                            pe_[:], lhsT=self.ones[0:16, :], rhs=Dx[i2][:].rearrange("p h l -> p (h l)")[:, csl],
                            start=True, stop=False), r=["ones", ("Dx", i2)], w=[kpe])
                        S.op("pe", lambda pe_=pe_, csl=csl: PE.matmul(
                            pe_[:], lhsT=nacsTt[i2][:], rhs=bd[g][:].rearrange("p h l -> p (h l)")[:, csl],
                            start=False, stop=False), r=[("nacsTt", i2), ("bd", g)], w=[kpe])
                        S.op("pe", lambda pe_=pe_, csl=csl: PE.matmul(
                            pe_[:], lhsT=self.ident32[:], rhs=negmask[:].rearrange("p h l -> p (h l)")[:, csl],
                            start=False, stop=True), r=["ident32", "negmask"], w=[kpe])
                        S.op("act", lambda pe_=pe_, hh=hh: A.activation(
                            out=Es[i2][:, hh * 4:(hh + 1) * 4, :], in_=pe_[:].rearrange("p (h l) -> p h l", h=4), func=AF.Exp),
                            r=[kpe], w=[("Es", i2), kpe])
                    S.op("dve", lambda: V.tensor_tensor(out=wT[i2][:], in0=Es[i2][:],
                                                        in1=cbs[i2][:].unsqueeze(1).to_broadcast([128, 8, 128]), op=ALU.mult),
                         r=[("Es", i2), ("cbs", i2)], w=[("wT", i2)])
                    pr, kpr = self.psum()
                    pr2, kpr2 = self.psum()
                    S.op("pool", lambda: G.tensor_tensor(out=xdtt[i2][:].rearrange("p (h c) -> p h c", c=64),
                                                         in0=xs[:, tt, :].rearrange("p (h c) -> p h c", c=64),
                                                         in1=dt[:, tt, hs].unsqueeze(2).to_broadcast([128, 8, 64]), op=ALU.mult),
                         r=["xs", "dt"], w=[("xdtt", i2)])
                    S.op("pool", lambda: G.tensor_tensor(out=xendt[i2][:].rearrange("p (h c) -> p h c", c=64),
                                                         in0=xdtt[i2][:].rearrange("p (h c) -> p h c", c=64),
                                                         in1=eend[:, tt, hs].unsqueeze(2).to_broadcast([128, 8, 64]), op=ALU.mult),
                         r=[("xdtt", i2), "eend"], w=[("xendt", i2)])
                    S.op("pe", lambda pr=pr: PE.matmul(pr[:], lhsT=bmA[:, tt, :], rhs=xendt[i2][:], start=True, stop=True),
                         r=["bmA", ("xendt", i2)], w=[kpr])
                    S.op("pe", lambda pr2=pr2: PE.matmul(pr2[:], lhsT=bmB[:, tt, :], rhs=xendt[i2][:], start=True, stop=True),
                         r=["bmB", ("xendt", i2)], w=[kpr2])
                    for (ci_, prx, kprx, cc) in ((cA, pr, kpr, 0), (cB, pr2, kpr2, 1)):
                        S.op("dve", lambda cc=cc: V.tensor_tensor(
                            out=S32[:].rearrange("p (h c) -> p h c", c=64), in0=S32[:].rearrange("p (h c) -> p h c", c=64),
                            in1=edec[:, tt, cc, hs].unsqueeze(2).to_broadcast([128, 8, 64]), op=ALU.mult),
                            r=["S32", "edec"], w=["S32"])
                        S.op("dve", lambda prx=prx: V.tensor_tensor(out=S32[:], in0=prx[:], in1=S32[:], op=ALU.add),
                             r=[kprx, "S32"], w=["S32", kprx])
                        S.op("pool", lambda ci_=ci_: G.tensor_copy(out=Sbf[(ci_ + 1) % 4][:], in_=S32[:]),
                             r=["S32"], w=[("Sbf", (ci_ + 1) % 4)])
                    py, kpy = self.psum()
                    for hh in range(8):
                        S.op("pe", lambda hh=hh, py=py: PE.matmul(py[:, hh * 64:(hh + 1) * 64], lhsT=wT[i2][:, hh, :],
                                                                  rhs=xdtt[i2][:, hh * 64:(hh + 1) * 64], start=True, stop=True),
                             r=[("wT", i2), ("xdtt", i2)], w=[kpy])
                    po, kpo = self.psum()
                    S.op("pe", lambda po=po: PE.matmul(po[:], lhsT=cmTA[:, tsl], rhs=Sbf[cA % 4][:], start=True, stop=False),
                         r=["cmTA", ("Sbf", cA % 4)], w=[kpo])
                    S.op("pe", lambda po=po: PE.matmul(po[:], lhsT=cmTB[:, tsl], rhs=Sbf[cB % 4][:], start=False, stop=True),
                         r=["cmTB", ("Sbf", cB % 4)], w=[kpo])
                    pz, kpz = self.psum()
                    for kt in range(KT):
                        S.op("pe", lambda kt=kt, pz=pz: PE.matmul(pz[:], lhsT=self.hT[:, kt, tsl], rhs=wz[:, kt, :],
                                                                  start=(kt == 0), stop=(kt == KT - 1)), r=["wz", ("hT", tt)], w=[kpz])
                    S.op("act", lambda pz=pz: A.activation(out=zs[i2][:], in_=pz[:], func=AF.Silu), r=[kpz], w=[("zs", i2), kpz])
                    S.op("dve", lambda po=po: V.tensor_tensor(
                        out=t1[i2][:].rearrange("p (h c) -> p h c", c=64), in0=po[:].rearrange("p (h c) -> p h c", c=64),
                        in1=eacs[:, tt, hs].unsqueeze(2).to_broadcast([128, 8, 64]), op=ALU.mult),
                        r=[kpo, "eacs"], w=[("t1", i2), kpo])
                    S.op("dve", lambda py=py: V.tensor_tensor(out=t1[i2][:], in0=py[:], in1=t1[i2][:], op=ALU.add),
                         r=[kpy, ("t1", i2)], w=[("t1", i2), kpy])
                    S.op("pool", lambda: G.tensor_tensor(
                        out=t2[i2][:].rearrange("p (h c) -> p h c", c=64), in0=xs[:, tt, :].rearrange("p (h c) -> p h c", c=64),
                        in1=dsk[:, hs].unsqueeze(2).to_broadcast([128, 8, 64]), op=ALU.mult), r=["xs", "dsk"], w=[("t2", i2)])
                    S.op("pool", lambda: G.tensor_tensor(out=t2[i2][:], in0=t2[i2][:], in1=t1[i2][:], op=ALU.add),
                         r=[("t2", i2), ("t1", i2)], w=[("t2", i2)])
                    S.op("pool", lambda: G.tensor_tensor(out=t2[i2][:], in0=t2[i2][:], in1=zs[i2][:], op=ALU.mult),
                         r=[("t2", i2), ("zs", i2)], w=[("t2", i2)])
                    S.op("act", lambda: A.activation(out=t1[i2][:], in_=t2[i2][:], func=AF.Square, accum_out=ss[i2][:, 0:1]),
                         r=[("t2", i2)], w=[("t1", i2), ("ss", i2)])
                    S.op("act", lambda: A.activation(out=ss[i2][:, 1:2], in_=ss[i2][:, 0:1], func=AF.Sqrt, bias=self.epsc[:, 1:2],
                                                     scale=1.0 / 512.0), r=[("ss", i2), "epsc"], w=[("ss", i2)])
                    S.op("dve", lambda: V.reciprocal(out=ss[i2][:, 1:2], in_=ss[i2][:, 1:2]), r=[("ss", i2)], w=[("ss", i2)])
                    S.op("dve", lambda: V.scalar_tensor_tensor(out=ytm[i2][:], in0=t2[i2][:], scalar=ss[i2][:, 1:2],
                                                               in1=nwbc[:, g * 512:(g + 1) * 512], op0=ALU.mult, op1=ALU.mult),
                         r=[("t2", i2), ("ss", i2), "nwbc"], w=[("ytm", i2)])
                    pt, kpt = self.psum()
                    ptv = pt[:].bitcast(BF16)
                    for i in range(4):
                        S.op("pe", lambda i=i, ptv=ptv: PE.transpose(out=ptv[:, i * 128:(i + 1) * 128],
                                                                     in_=ytm[i2][:, i * 128:(i + 1) * 128], identity=self.identbf[:]),
                             r=[("ytm", i2), "identbf"], w=[kpt])
                    S.op("act", lambda ptv=ptv: A.activation(out=yTg[:, :, tsl], in_=ptv[:, 0:512].rearrange("p (a b) -> p a b", a=4),
                                                             func=AF.Copy), r=[kpt], w=["yTg", kpt])
                for i in range(4):
                    S.dma("sp", ydst[g * 512 + i * 128:g * 512 + (i + 1) * 128, :], yTg[:, i, :], r=["yTg"], w=[("yT0", g * 4 + i)])
            S.barrier()


    def rwkv(self, l):
        nc, S = self.nc, self.S
        V, A, G, PE = nc.vector, nc.scalar, nc.gpsimd, nc.tensor
        Wl = self.P["w_in"][l]
        ydst = self.yT_dram[1]
        hTk = [("hT", t) for t in range(NT)]
        P_ = self.P

        def mm(out, lhsT, rhs, start, stop, r, w):
            S.op("pe", lambda: PE.matmul(out, lhsT=lhsT, rhs=rhs, start=start, stop=stop), r=r, w=w)

        with contextlib.ExitStack() as st:
            mask4 = self.sb(st, "mask4", [128, 4, 128], F32)
            maskL = self.sb(st, "maskL", [128, 2, 128], F32)
            bdm = self.sb(st, "bdm", [128, 128], F32)
            mEO = self.sb(st, "mEO", [128, 2], F32)
            hsel = self.sb(st, "hsel", [128, 2], F32)
            rm = self.sb(st, "rm128", [128, T], BF16)
            c05 = self.sb(st, "c05", [128, 1], F32)
            for j in range(4):
                S.op("pool", lambda j=j: G.affine_select(out=mask4[:, j, :], in_=self.ones[:], pattern=[[1, 128]],
                                                         compare_op=(ALU.is_gt if j % 2 == 0 else ALU.is_ge), fill=0.0,
                                                         base=0, channel_multiplier=-1), r=["ones"], w=["mask4"])
            for j in range(2):
                S.op("pool", lambda j=j: G.affine_select(out=maskL[:, j, :], in_=self.ones[:], pattern=[[-1, 128]],
                                                         compare_op=ALU.is_gt, fill=0.0, base=0, channel_multiplier=1),
                     r=["ones"], w=["maskL"])
            S.op("pool", lambda: G.memset(bdm[:], 0.0), w=["bdm"])
            S.op("pool", lambda: G.memset(bdm[0:64, 0:64], 1.0), w=["bdm"])
            S.op("pool", lambda: G.memset(bdm[64:128, 64:128], 1.0), w=["bdm"])
            for (t_, nm) in ((mEO, "mEO"), (hsel, "hsel")):
                S.op("pool", lambda t_=t_: G.memset(t_[0:64, 0:1], 1.0), w=[nm])
                S.op("pool", lambda t_=t_: G.memset(t_[64:128, 0:1], 0.0), w=[nm])
                S.op("pool", lambda t_=t_: G.memset(t_[0:64, 1:2], 0.0), w=[nm])
                S.op("pool", lambda t_=t_: G.memset(t_[64:128, 1:2], 1.0), w=[nm])
            S.op("pool", lambda: G.memset(rm[:], 1.0), w=["rm"])
            S.op("pool", lambda: G.memset(rm[:].rearrange("p (c j) -> p c j", j=128)[:, :, 0:1], 0.0), w=["rm"])
            S.op("pool", lambda: G.memset(c05[:], -0.5), w=["c05"])
            pc = {}
            for nm, src in (("mu_r", P_["rwkv_mu"][l, 0:1024]), ("mu_k", P_["rwkv_mu"][l, 1024:2048]),
                            ("mu_v", P_["rwkv_mu"][l, 2048:3072]), ("w0", P_["rwkv_w0"][l]), ("a0", P_["rwkv_a0"][l]),
                            ("k_k", P_["rwkv_k_k"][l]), ("k_a", P_["rwkv_k_a"][l]),
                            ("r_k", P_["rwkv_r_k"][l].rearrange("h k -> (h k)"))):
                t_ = self.sb(st, "pc_" + nm, [128, 8, 1], F32)
                S.dma("sp", t_[:], src.rearrange("(q p o) -> p q o", p=128, o=1), w=["pc_" + nm])
                pc[nm] = t_
            mul = self.sb(st, "mul", [128, 3], F32)
            S.dma("sp", mul[0:64, 0:1], P_["rwkv_mu"][l, 3072:3136].rearrange("(p o) -> p o", o=1), w=["mul"])
            S.dma("sp", mul[0:64, 1:2], P_["rwkv_mu"][l, 3136:3200].rearrange("(p o) -> p o", o=1), w=["mul"])
            S.dma("sp", mul[:, 2:3], P_["rwkv_mu"][l, 3200:3328].rearrange("(p o) -> p o", o=1), w=["mul"])
            nw0 = self.sb(st, "nw0", [128, 8, 1], F32)
            omka = self.sb(st, "omka", [128, 8, 1], F32)
            S.op("dve", lambda: V.tensor_scalar(out=nw0[:], in0=pc["w0"][:], scalar1=-1.0, scalar2=None, op0=ALU.mult),
                 r=["pc_w0"], w=["nw0"])
            S.op("dve", lambda: V.tensor_scalar(out=omka[:], in0=pc["k_a"][:], scalar1=-1.0, scalar2=1.0, op0=ALU.mult,
                                                op1=ALU.add), r=["pc_k_a"], w=["omka"])
            wl = self.sb(st, "wl", [128, KT, 256], BF16)
            w2 = self.sb(st, "w2", [64, D], BF16)
            a2 = self.sb(st, "a2", [64, D], BF16)
            g2 = self.sb(st, "g2", [128, D], BF16)
            S.dma("pool", wl[:], Wl[:, OFF_RWKV + 3072:OFF_RWKV + 3328].rearrange("(kt p) n -> p kt n", p=128), w=["wl"])
            S.dma("pool", w2[:], P_["rwkv_w2"][l], w=["w2"])
            S.dma("pool", a2[:], P_["rwkv_a2"][l], w=["a2"])
            S.dma("pool", g2[:], P_["rwkv_g2"][l], w=["g2"])
            txw = self.sb(st, "txw", [64, T], BF16)
            xaT = self.sb(st, "xaT", [64, T], BF16)
            sgT = self.sb(st, "sgT", [128, T], BF16)
            xraw = self.sb(st, "xraw", [128, T + 1], F32)
            F = [self.sb(st, "F%d" % i, [128, T], F32) for i in range(5)]
            S.op("pool", lambda: G.memset(xraw[:, 0:1], 0.0), w=["xraw"])

            def proj_shift(wt, c0, m, mucol, dst, dkey, func=None, rows=128):
                for tq in range(4):
                    pb, kp = self.psum()
                    for kt in range(KT):
                        mm(pb[0:rows, :], wt[:, kt, c0:c0 + m], self.hT[:, kt, tq * 512:(tq + 1) * 512], kt == 0, kt == KT - 1,
                           [wt_key] + hTk[tq * 4:tq * 4 + 4], [kp])
                    S.op("act", lambda pb=pb, tq=tq: A.activation(out=xraw[0:rows, 1 + tq * 512:1 + (tq + 1) * 512],
                                                                  in_=pb[0:rows, :], func=AF.Copy), r=[kp], w=["xraw", kp])
                S.op("dve", lambda: V.tensor_tensor(out=F[4][0:rows, :], in0=xraw[0:rows, 0:T], in1=xraw[0:rows, 1:T + 1],
                                                    op=ALU.subtract), r=["xraw"], w=["F4"])
                if func is None:
                    S.op("dve", lambda: V.scalar_tensor_tensor(out=dst[0:rows, :], in0=F[4][0:rows, :], scalar=mucol,
                                                               in1=xraw[0:rows, 1:T + 1], op0=ALU.mult, op1=ALU.add),
                         r=["F4", "xraw", "mul"] + list(pc_keys), w=[dkey])
                else:
                    S.op("dve", lambda: V.scalar_tensor_tensor(out=F[4][0:rows, :], in0=F[4][0:rows, :], scalar=mucol,
                                                               in1=xraw[0:rows, 1:T + 1], op0=ALU.mult, op1=ALU.add),
                         r=["F4", "xraw", "mul"] + list(pc_keys), w=["F4"])
                    S.op("act", lambda: A.activation(out=dst[0:rows, :], in_=F[4][0:rows, :], func=func), r=["F4"], w=[dkey])

            pc_keys = ["pc_mu_r", "pc_mu_k", "pc_mu_v"]
            wt_key = "wl"
            proj_shift(wl, 0, 64, mul[0:64, 0:1], txw, "txw", AF.Tanh, rows=64)
            proj_shift(wl, 64, 64, mul[0:64, 1:2], xaT, "xaT", AF.Copy, rows=64)
            proj_shift(wl, 128, 128, mul[:, 2:3], sgT, "sgT", AF.Sigmoid, rows=128)
            wrkv1 = self.sb(st, "wrkv", [128, KT, 384], BF16)
            wrkv = [wrkv1, wrkv1]
            Pp = self.sb(st, "Pp", [128, T], BF16)
            Pc = self.sb(st, "Pc", [128, T], BF16)
            Pend = self.sb(st, "Pend", [128, NT], F32)
            iP = self.sb(st, "iP", [128, T], BF16)
            bT = self.sb(st, "bT", [128, T], BF16)
            kT = self.sb(st, "kT", [128, T], BF16)
            vT = self.sb(st, "vT", [128, T], BF16)
            AR = [self.sb(st, "AR", [128, NT, 2, 128], BF16) for _ in range(2)]
            Vtm = self.sb(st, "Vtm", [128, NT, 128], BF16)
            Btm = self.sb(st, "Btm", [128, NT, 128], BF16)
            aT = Vtm[:].rearrange("p t j -> p (t j)")
            rT = Btm[:].rearrange("p t j -> p (t j)")
            Ktm = self.sb(st, "Ktm", [128, NT, 128], BF16)
            bon = self.sb(st, "bon", [128, NT, 2], F32)
            st32 = self.sb(st, "st32", [128, 2 * NT, 4], F32)
            lnw = self.sb(st, "lnwb", [128, 128], F32)
            lnb = self.sb(st, "lnbb", [128, 128], F32)
            Z32 = self.sb(st, "Z32", [128, 128], F32)
            Zt = self.sb(st, "Zt", [128, 128], F32)
            Zbf = [self.sb(st, "Zbf", [128, 128], BF16) for _ in range(2)]
            Wsb = [self.sb(st, "Wsb", [128, 128], BF16) for _ in range(2)]
            Usb = [self.sb(st, "Usb", [128, 128], BF16) for _ in range(2)]
            NS = 8
            abrb = [self.sb(st, "abrb", [128, 4, 128], BF16) for _ in range(NS)]
            akrk = [self.sb(st, "akrk", [128, 4, 128], BF16) for _ in range(NS)]
            L0 = [self.sb(st, "L0", [128, 2, 128], BF16) for _ in range(4)]
            XX = [[self.sb(st, "XX", [128, 4, 128], BF16) for _ in range(4)] for _ in range(2)]
            Tt = [self.sb(st, "Tt", [128, 2, 128], BF16) for _ in range(NS)]

            def load_w(p):
                b = 0
                for j in range(3):
                    c0 = OFF_RWKV + j * 1024 + p * 128
                    S.dma("pool", wrkv[b][:, :, j * 128:(j + 1) * 128], Wl[:, c0:c0 + 128].rearrange("(kt p) n -> p kt n", p=128),
                          w=[("wrkv", b)])

            for p in range(8):
                b = 0
                load_w(p)
                fs_ = slice(p * 128, (p + 1) * 128)
                S.dma("sp", lnw[:], P_["rwkv_ln_w"][l, fs_].partition_broadcast(128), w=["lnw"])
                S.dma("sp", lnb[:], P_["rwkv_ln_b"][l, fs_].partition_broadcast(128), w=["lnb"])
                wt_key = ("wrkv", b)
                proj_shift(wrkv[b], 128, 128, pc["mu_k"][:, p, :], F[0], "F0")
                proj_shift(wrkv[b], 0, 128, pc["mu_r"][:, p, :], F[1], "F1")
                proj_shift(wrkv[b], 256, 128, pc["mu_v"][:, p, :], vT, "vT", AF.Copy)
                for tq in range(4):
                    pb, kp = self.psum()
                    qs_ = slice(tq * 512, (tq + 1) * 512)
                    mm(pb[:], w2[:, fs_], txw[:, qs_], True, True, ["w2", "txw"], [kp])
                    S.op("act", lambda pb=pb: A.activation(out=F[2][:, qs_], in_=pb[:], func=AF.Exp, bias=nw0[:, p, :], scale=-1.0),
                         r=[kp, "nw0"], w=["F2", kp])
                S.op("act", lambda: A.activation(out=F[2][:], in_=F[2][:], func=AF.Ln, bias=self.epsc[:, 3:4], scale=1.0),
                     r=["F2", "epsc"], w=["F2"])
                S.op("act", lambda: A.activation(out=F[2][:], in_=F[2][:], func=AF.Exp, bias=c05[:, 0:1], scale=-1.0),
                     r=["F2", "c05"], w=["F2"])
                S.op("dve", lambda: V.tensor_tensor_scan(out=F[3][:], data0=rm[:], data1=F[2][:], initial=0.0, op0=ALU.mult,
                                                         op1=ALU.add), r=["rm", "F2"], w=["F3"])
                S.op("dve", lambda: V.tensor_tensor(out=F[2][:], in0=F[3][:], in1=F[2][:], op=ALU.subtract),
                     r=["F2", "F3"], w=["F2"])
                S.op("act", lambda: A.activation(out=Pp[:], in_=F[2][:], func=AF.Exp, scale=-1.0), r=["F2"], w=["Pp"])
                S.op("act", lambda: A.activation(out=Pc[:], in_=F[3][:], func=AF.Exp, scale=-1.0), r=["F3"], w=["Pc"])
                S.op("act", lambda: A.activation(out=iP[:], in_=F[3][:], func=AF.Exp), r=["F3"], w=["iP"])
                S.op("act", lambda: A.activation(out=Pend[:], in_=F[3][:].rearrange("p (t j) -> p t j", j=128)[:, :, 127],
                                                 func=AF.Exp, scale=-1.0), r=["F3"], w=["Pend"])
                for tq in range(4):
                    pb, kp = self.psum()
                    qs_ = slice(tq * 512, (tq + 1) * 512)
                    mm(pb[:], a2[:, fs_], xaT[:, qs_], True, True, ["a2", "xaT"], [kp])
                    S.op("act", lambda pb=pb: A.activation(out=F[2][:, qs_], in_=pb[:], func=AF.Sigmoid, bias=pc["a0"][:, p, :],
                                                           scale=1.0), r=[kp, "pc_a0"], w=["F2", kp])
                S.op("dve", lambda: V.tensor_scalar(out=F[3][:], in0=F[0][:], scalar1=pc["k_k"][:, p, :], scalar2=None,
                                                    op0=ALU.mult), r=["F0", "pc_k_k"], w=["F3"])
                S.op("act", lambda: A.activation(out=F[4][:], in_=F[3][:], func=AF.Square), r=["F3"], w=["F4"])
                for tq in range(4):
                    pb, kp = self.psum()
                    qs_ = slice(tq * 512, (tq + 1) * 512)
                    mm(pb[:], bdm[:], F[4][:, qs_], True, True, ["bdm", "F4"], [kp])
                    S.op("act", lambda pb=pb: A.activation(out=F[4][:, qs_], in_=pb[:], func=AF.Sqrt), r=[kp], w=["F4", kp])
                S.op("dve", lambda: V.tensor_scalar(out=F[4][:], in0=F[4][:], scalar1=1e-12, scalar2=None, op0=ALU.max),
                     r=["F4"], w=["F4"])
                S.op("dve", lambda: V.reciprocal(out=F[4][:], in_=F[4][:]), r=["F4"], w=["F4"])
                S.op("dve", lambda: V.tensor_tensor(out=F[3][:], in0=F[3][:], in1=F[4][:], op=ALU.mult), r=["F3", "F4"], w=["F3"])
                S.op("dve", lambda: V.scalar_tensor_tensor(out=aT, in0=F[3][:], scalar=-1.0, in1=Pp[:], op0=ALU.mult,
                                                           op1=ALU.mult), r=["F3", "Pp"], w=["Vtm"])
                S.op("pool", lambda: G.tensor_tensor(out=F[4][:], in0=F[3][:], in1=F[2][:], op=ALU.mult), r=["F3", "F2"], w=["F4"])
                S.op("pool", lambda: G.tensor_tensor(out=bT[:], in0=F[4][:], in1=iP[:], op=ALU.mult), r=["F4", "iP"], w=["bT"])
                S.op("dve", lambda: V.tensor_scalar(out=F[2][:], in0=F[2][:], scalar1=pc["k_a"][:, p, :], scalar2=omka[:, p, :],
                                                    op0=ALU.mult, op1=ALU.add), r=["F2", "pc_k_a", "omka"], w=["F2"])
                S.op("dve", lambda: V.tensor_tensor(out=F[0][:], in0=F[0][:], in1=F[2][:], op=ALU.mult), r=["F0", "F2"], w=["F0"])
                S.op("pool", lambda: G.tensor_tensor(out=kT[:], in0=F[0][:], in1=iP[:], op=ALU.mult), r=["F0", "iP"], w=["kT"])
                S.op("dve", lambda: V.tensor_tensor(out=rT, in0=F[1][:], in1=Pc[:], op=ALU.mult), r=["F1", "Pc"], w=["Btm"])
                S.op("dve", lambda: V.scalar_tensor_tensor(out=F[1][:], in0=F[1][:], scalar=pc["r_k"][:, p, :], in1=F[0][:],
                                                           op0=ALU.mult, op1=ALU.mult), r=["F1", "F0", "pc_r_k"], w=["F1"])
                for h in range(2):
                    S.op("act", lambda h=h: A.activation(out=AR[h][:, :, 0, :], in_=Vtm[:], func=AF.Identity,
                                                         scale=mEO[:, h:h + 1]), r=["Vtm", "mEO"], w=[("AR", h)])
                    S.op("dve", lambda h=h: V.tensor_scalar(out=AR[h][:, :, 1, :], in0=Btm[:],
                                                            scalar1=mEO[:, h:h + 1], scalar2=None, op0=ALU.mult),
                         r=["Btm", "mEO"], w=[("AR", h)])
                for (src, skey, dst, dkey) in ((vT, "vT", Vtm, "Vtm"), (bT, "bT", Btm, "Btm"), (kT, "kT", Ktm, "Ktm")):
                    for t4 in range(4):
                        pb, kp = self.psum()
                        pbv = pb[:].bitcast(BF16)
                        for j in range(4):
                            tt = t4 * 4 + j
                            S.op("pe", lambda j=j, tt=tt, pbv=pbv, src=src: PE.transpose(
                                out=pbv[:, j * 128:(j + 1) * 128], in_=src[:, tt * 128:(tt + 1) * 128], identity=self.identbf[:]),
                                r=[skey, "identbf"], w=[kp])
                        S.op("act", lambda t4=t4, pbv=pbv, dst=dst: A.activation(
                            out=dst[:, t4 * 4:(t4 + 1) * 4, :], in_=pbv[:, 0:512].rearrange("p (a b) -> p a b", a=4), func=AF.Copy),
                            r=[kp], w=[dkey, kp])
                pb, kp = self.psum()
                for tt in range(NT):
                    mm(pb[:, tt * 2:(tt + 1) * 2], F[1][:, tt * 128:(tt + 1) * 128], hsel[:], True, True, ["F1", "hsel"], [kp])
                S.op("dve", lambda pb=pb: V.tensor_copy(out=bon[:].rearrange("p t h -> p (t h)"), in_=pb[:, 0:2 * NT]),
                     r=[kp], w=["bon", kp])
                ytm = F[2][:].rearrange("p (t j) -> p t j", j=128)
                ysq = F[3][:].rearrange("p (t j) -> p t j", j=128)

                def inv_group(gi, pending=()):
                    pending = list(pending)
                    tiles = range(gi * 4, gi * 4 + 4)
                    for tt in tiles:
                        sl = tt % NS
                        tsl = slice(tt * 128, (tt + 1) * 128)
                        p1, k1 = self.psum()
                        p2, k2 = self.psum()
                        p3, k3 = self.psum()
                        for h in range(2):
                            rhs = AR[h][:, tt].rearrange("p a j -> p (a j)")
                            mm(p1[:, h * 256:(h + 1) * 256], bT[:, tsl], rhs, True, True, ["bT", ("AR", h)], [k1])
                            mm(p2[:, h * 256:(h + 1) * 256], kT[:, tsl], rhs, True, True, ["kT", ("AR", h)], [k2])
                            mm(p3[:, h * 128:(h + 1) * 128], AR[h][:, tt, 0, :], bT[:, tsl], True, True, [("AR", h), "bT"], [k3])
                        S.op("dve", lambda p1=p1, sl=sl: V.tensor_tensor(out=abrb[sl][:].rearrange("p a j -> p (a j)"), in0=p1[:],
                                                                        in1=mask4[:].rearrange("p a j -> p (a j)"), op=ALU.mult),
                             r=[k1, "mask4"], w=[("abrb", sl), k1])
                        S.op("dve", lambda p2=p2, sl=sl: V.tensor_tensor(out=akrk[sl][:].rearrange("p a j -> p (a j)"), in0=p2[:],
                                                                        in1=mask4[:].rearrange("p a j -> p (a j)"), op=ALU.mult),
                             r=[k2, "mask4"], w=[("akrk", sl), k2])
                        S.op("dve", lambda p3=p3, sl=sl: V.tensor_tensor(out=L0[sl % 4][:].rearrange("p a j -> p (a j)"), in0=p3[:, 0:256],
                                                                        in1=maskL[:].rearrange("p a j -> p (a j)"), op=ALU.mult),
                             r=[k3, "maskL"], w=[("L0", sl % 4), k3])
                        for h in range(2):
                            S.op("pool", lambda h=h, sl=sl: G.tensor_tensor(out=Tt[sl][:, h, :], in0=abrb[sl][:, 2 * h, :],
                                                                            in1=self.identbf[:], op=ALU.add),
                                 r=[("abrb", sl), "identbf"], w=[("Tt", sl)])

                    def Xk(k, sl, h):
                        return (L0[sl % 4][:, h, :], ("L0", sl % 4)) if k == 0 else (XX[k % 2][sl % 4][:, 2 * h, :], ("XX", k % 2, sl % 4))

                    def Xtk(k, sl, h):
                        return (abrb[sl][:, 2 * h, :], ("abrb", sl)) if k == 0 else (XX[k % 2][sl % 4][:, 2 * h + 1, :], ("XX", k % 2, sl % 4))

                    for k in range(7):
                        sqb = {}
                        if k <= 5:
                            for tt in tiles:
                                sl = tt % NS
                                pb, kp = self.psum()
                                sqb[tt] = (pb, kp)
                                for h in range(2):
                                    x, kx = Xk(k, sl, h)
                                    xt, kxt = Xtk(k, sl, h)
                                    mm(pb[:, (2 * h) * 128:(2 * h + 1) * 128], xt, x, True, True, [kx, kxt], [kp])
                                    if k < 5:
                                        mm(pb[:, (2 * h + 1) * 128:(2 * h + 2) * 128], x, xt, True, True, [kx, kxt], [kp])
                        ttb = []
                        if k >= 1:
                            for t2 in range(2):
                                pb, kp = self.psum()
                                ttb.append((pb, kp))
                                for j in range(2):
                                    sl = (gi * 4 + t2 * 2 + j) % NS
                                    for h in range(2):
                                        x1, kx1 = Xk(k, sl, h)
                                        mm(pb[:, (j * 2 + h) * 128:(j * 2 + h + 1) * 128], x1, Tt[sl][:, h, :], True, True,
                                           [kx1, ("Tt", sl)], [kp])
                        if k <= 5:
                            for tt in tiles:
                                sl = tt % NS
                                pb, kp = sqb[tt]
                                kn = ("XX", (k + 1) % 2, sl % 4)
                                if k < 5:
                                    S.op("act", lambda pb=pb, sl=sl, k=k: A.activation(
                                        out=XX[(k + 1) % 2][sl % 4][:].rearrange("p a j -> p (a j)"), in_=pb[:], func=AF.Copy),
                                        r=[kp], w=[kn, kp])
                                else:
                                    S.op("act", lambda pb=pb, sl=sl, k=k: A.activation(
                                        out=XX[(k + 1) % 2][sl % 4][:, 0:4:2, :],
                                        in_=pb[:].rearrange("p (a j) -> p a j", j=128)[:, 0:4:2, :], func=AF.Copy), r=[kp], w=[kn, kp])
                        for t2, (pb, kp) in enumerate(ttb):
                            for j in range(2):
                                sl = (gi * 4 + t2 * 2 + j) % NS
                                S.op("dve", lambda pb=pb, sl=sl, j=j: V.tensor_tensor(
                                    out=Tt[sl][:].rearrange("p a j -> p (a j)"), in0=pb[:, j * 256:(j + 1) * 256],
                                    in1=Tt[sl][:].rearrange("p a j -> p (a j)"), op=ALU.add), r=[kp, ("Tt", sl)], w=[("Tt", sl), kp])
                        if pending and k >= 1:
                            chain_tile(pending.pop(0))
                    while pending:
                        chain_tile(pending.pop(0))

                def chain_tile(tt):
                    if True:
                        sl = tt % NS
                        i2 = tt % 2
                        tsl = slice(tt * 128, (tt + 1) * 128)
                        zb, kz = Zbf[i2], ("Zbf", i2)
                        pw, kpw = self.psum()
                        mm(pw[:, 0:128], AR[0][:, tt, 0, :], zb[:], True, False, [("AR", 0), kz], [kpw])
                        mm(pw[:, 0:128], AR[1][:, tt, 0, :], zb[:], False, False, [("AR", 1), kz], [kpw])
                        for h in range(2):
                            mm(pw[:, h * 64:(h + 1) * 64], akrk[sl][:, 2 * h, :], Vtm[:, tt, h * 64:(h + 1) * 64], False, h == 1,
                               [("akrk", sl), "Vtm"], [kpw])
                        S.op("act", lambda pw=pw: A.activation(out=Wsb[i2][:], in_=pw[:, 0:128], func=AF.Copy),
                             r=[kpw], w=[("Wsb", i2), kpw])
                        pu, kpu = self.psum()
                        for h in range(2):
                            mm(pu[:, h * 64:(h + 1) * 64], Tt[sl][:, h, :], Wsb[i2][:, h * 64:(h + 1) * 64], True, True,
                               [("Tt", sl), ("Wsb", i2)], [kpu])
                        S.op("act", lambda pu=pu: A.activation(out=Usb[i2][:], in_=pu[:, 0:128], func=AF.Copy),
                             r=[kpu], w=[("Usb", i2), kpu])
                        py, kpy = self.psum()
                        mm(py[:, 0:128], AR[0][:, tt, 1, :], zb[:], True, False, [("AR", 0), kz], [kpy])
                        mm(py[:, 0:128], AR[1][:, tt, 1, :], zb[:], False, False, [("AR", 1), kz], [kpy])
                        for h in range(2):
                            mm(py[:, h * 64:(h + 1) * 64], abrb[sl][:, 2 * h + 1, :], Usb[i2][:, h * 64:(h + 1) * 64], False, False,
                               [("abrb", sl), ("Usb", i2)], [kpy])
                            mm(py[:, h * 64:(h + 1) * 64], akrk[sl][:, 2 * h + 1, :], Vtm[:, tt, h * 64:(h + 1) * 64], False, h == 1,
                               [("akrk", sl), "Vtm"], [kpy])
                        S.op("act", lambda py=py: A.activation(out=ytm[:, tt, :], in_=py[:, 0:128], func=AF.Copy),
                             r=[kpy], w=["F2", kpy])
                        pz, kpz = self.psum()
                        mm(pz[:, 0:128], Btm[:, tt, :], Usb[i2][:], True, False, ["Btm", ("Usb", i2)], [kpz])
                        mm(pz[:, 0:128], Ktm[:, tt, :], Vtm[:, tt, :], False, True, ["Ktm", "Vtm"], [kpz])
                        S.op("dve", lambda pz=pz: V.tensor_tensor(out=Zt[:], in0=pz[:, 0:128], in1=bdm[:], op=ALU.mult),
                             r=[kpz, "bdm"], w=["Zt", kpz])
                        S.op("dve", lambda: V.tensor_tensor(out=Zt[:], in0=Zt[:], in1=Z32[:], op=ALU.add), r=["Zt", "Z32"], w=["Zt"])
                        S.op("dve", lambda: V.tensor_scalar(out=Z32[:], in0=Zt[:], scalar1=Pend[:, tt:tt + 1],
                                                            scalar2=None, op0=ALU.mult), r=["Zt", "Pend"], w=["Z32"])
                        S.op("pool", lambda: G.tensor_copy(out=Zbf[(tt + 1) % 2][:], in_=Z32[:]), r=["Z32"], w=[("Zbf", (tt + 1) % 2)])

                S.op("pool", lambda: G.memset(Z32[:], 0.0), w=["Z32"])
                S.op("pool", lambda: G.memset(Zbf[0][:], 0.0), w=[("Zbf", 0)])
                inv_group(0)
                for gi in range(1, 4):
                    inv_group(gi, pending=range((gi - 1) * 4, gi * 4))
                for tt in range(12, 16):
                    chain_tile(tt)
                y16, yTp = Btm, kT
                y3 = ytm.rearrange("p t (h c) -> p (t h) c", c=64)
                q3 = ysq.rearrange("p t (h c) -> p (t h) c", c=64)
                S.op("act", lambda: A.activation(out=F[3][:], in_=F[2][:], func=AF.Square), r=["F2"], w=["F3"])
                S.op("dve", lambda: V.tensor_reduce(out=st32[:, :, 0], in_=y3, axis=AX.X, op=ALU.add), r=["F2"], w=["st32"])
                S.op("dve", lambda: V.tensor_reduce(out=st32[:, :, 1], in_=q3, axis=AX.X, op=ALU.add), r=["F3"], w=["st32"])
                S.op("dve", lambda: V.tensor_scalar(out=st32[:, :, 0], in0=st32[:, :, 0], scalar1=1.0 / 64.0, scalar2=None,
                                                    op0=ALU.mult), r=["st32"], w=["st32"])
                S.op("dve", lambda: V.tensor_tensor(out=st32[:, :, 2], in0=st32[:, :, 0], in1=st32[:, :, 0], op=ALU.mult),
                     r=["st32"], w=["st32"])
                S.op("dve", lambda: V.scalar_tensor_tensor(out=st32[:, :, 1], in0=st32[:, :, 1], scalar=1.0 / 64.0, in1=st32[:, :, 2],
                                                           op0=ALU.mult, op1=ALU.subtract), r=["st32"], w=["st32"])
                S.op("act", lambda: A.activation(out=st32[:, :, 1], in_=st32[:, :, 1], func=AF.Sqrt, bias=self.epsc[:, 2:3], scale=1.0),
                     r=["st32", "epsc"], w=["st32"])
                S.op("dve", lambda: V.reciprocal(out=st32[:, :, 1], in_=st32[:, :, 1]), r=["st32"], w=["st32"])
                S.op("dve", lambda: V.tensor_tensor(out=y3, in0=y3, in1=st32[:, :, 0:1].to_broadcast([128, 2 * NT, 64]),
                                                    op=ALU.subtract), r=["F2", "st32"], w=["F2"])
                S.op("dve", lambda: V.tensor_tensor(out=y3, in0=y3, in1=st32[:, :, 1:2].to_broadcast([128, 2 * NT, 64]),
                                                    op=ALU.mult), r=["F2", "st32"], w=["F2"])
                S.op("pool", lambda: G.tensor_tensor(out=ytm, in0=ytm, in1=lnw[:].unsqueeze(1).to_broadcast([128, NT, 128]),
                                                     op=ALU.mult), r=["F2", "lnw"], w=["F2"])
                S.op("pool", lambda: G.tensor_tensor(out=ytm, in0=ytm, in1=lnb[:].unsqueeze(1).to_broadcast([128, NT, 128]),
                                                     op=ALU.add), r=["F2", "lnb"], w=["F2"])
                S.op("dve", lambda: V.tensor_tensor(out=q3, in0=Vtm[:].rearrange("p t (h c) -> p (t h) c", c=64),
                                                    in1=bon[:].rearrange("p t h -> p (t h)").unsqueeze(2).to_broadcast([128, 2 * NT, 64]),
                                                    op=ALU.mult), r=["Vtm", "bon", "F3"], w=["F3"])
                S.op("dve", lambda: V.tensor_tensor(out=F[2][:], in0=F[2][:], in1=F[3][:], op=ALU.add), r=["F2", "F3"], w=["F2"])
                for t4 in range(4):
                    pb, kp = self.psum()
                    for j in range(4):
                        tt = t4 * 4 + j
                        mm(pb[:, j * 128:(j + 1) * 128], sgT[:, tt * 128:(tt + 1) * 128], g2[:, fs_], True, True, ["sgT", "g2"], [kp])
                    S.op("dve", lambda pb=pb, t4=t4: V.tensor_tensor(
                        out=y16[:, t4 * 4:(t4 + 1) * 4, :], in0=pb[:].rearrange("p (a j) -> p a j", j=128),
                        in1=ytm[:, t4 * 4:(t4 + 1) * 4, :], op=ALU.mult), r=[kp, "F2"], w=["Btm", kp])
                for t4 in range(4):
                    pb, kp = self.psum()
                    pbv = pb[:].bitcast(BF16)
                    for j in range(4):
                        tt = t4 * 4 + j
                        S.op("pe", lambda j=j, tt=tt, pbv=pbv: PE.transpose(out=pbv[:, j * 128:(j + 1) * 128], in_=y16[:, tt, :],
                                                                           identity=self.identbf[:]), r=["Btm", "identbf"], w=[kp])
                    S.op("act", lambda t4=t4, pbv=pbv: A.activation(out=yTp[:, t4 * 512:(t4 + 1) * 512], in_=pbv[:, 0:512], func=AF.Copy),
                         r=[kp], w=["kT", kp])
                S.dma("sp", ydst[fs_, :], yTp[:], r=["kT"], w=[("yT1", p)])
            S.barrier()


    def merge(self, l):
        nc, S = self.nc, self.S
        V, A, G, PE = nc.vector, nc.scalar, nc.gpsimd, nc.tensor
        Wl = self.P["w_in"][l]
        brw = [self.P["w_br_ssd"][l], self.P["w_br_rwkv"][l], self.P["w_br_hgrn"][l]]
        hTk = [("hT", t) for t in range(NT)]
        with contextlib.ExitStack() as st:
            mT = self.sb(st, "mT", [128, KT, T], F32)
            wbrs = [self.sb(st, "wbr", [128, KT, D], BF16) for _ in range(2)]
            wgts = [self.sb(st, "wgt", [128, KT, D], BF16) for _ in range(2)]
            st1 = contextlib.ExitStack()
            st1.__enter__()
            yq = [self.sb(st1, "yq", [128, KT, 512], BF16) for _ in range(2)]
            sg = [self.sb(st1, "sgm", [128, 512], BF16) for _ in range(2)]
            tmp = [self.sb(st1, "tmpm", [128, 512], F32) for _ in range(2)]
            cnt = 0
            def load_br(i):
                S.dma("pool", wbrs[i % 2][:], brw[i].rearrange("(kt p) n -> p kt n", p=128), w=[("wbr", i % 2)])
                c0 = OFF_GATES + i * 1024
                S.dma("pool", wgts[i % 2][:], Wl[:, c0:c0 + 1024].rearrange("(kt p) n -> p kt n", p=128), w=[("wgt", i % 2)])

            load_br(0)
            load_br(1)
            for i in range(3):
                wbr, wgt = wbrs[i % 2], wgts[i % 2]
                kwb, kwg = ("wbr", i % 2), ("wgt", i % 2)
                if i == 2:
                    load_br(2)
                for q in range(4):
                    qs_ = slice(q * 512, (q + 1) * 512)
                    yb = (i * 4 + q) % 2
                    S.dma("sp", yq[yb][:], self.yT_dram[i][:, qs_].rearrange("(kt p) n -> p kt n", p=128),
                          r=[("yT%d" % i, k) for k in range(8)], w=[("yq", yb)])
                    for ot in range(KT):
                        os_ = slice(ot * 128, (ot + 1) * 128)
                        pg, kg = self.psum()
                        pb, kb = self.psum()
                        for kt in range(KT):
                            S.op("pe", lambda kt=kt, pg=pg: PE.matmul(pg[:], lhsT=wgt[:, kt, os_], rhs=self.hT[:, kt, qs_],
                                                                      start=(kt == 0), stop=(kt == KT - 1)),
                                 r=[kwg] + hTk[q * 4:q * 4 + 4], w=[kg])
                        for kt in range(KT):
                            S.op("pe", lambda kt=kt, pb=pb: PE.matmul(pb[:], lhsT=wbr[:, kt, os_], rhs=yq[yb][:, kt, :],
                                                                      start=(kt == 0), stop=(kt == KT - 1)),
                                 r=[kwb, ("yq", yb)], w=[kb])
                        c2 = cnt % 2
                        cnt += 1
                        S.op("act", lambda pg=pg, c2=c2: A.activation(out=sg[c2][:], in_=pg[:], func=AF.Sigmoid),
                             r=[kg], w=[("sgm", c2), kg])
                        if i == 0:
                            S.op("dve", lambda pb=pb, c2=c2: V.tensor_tensor(out=mT[:, ot, qs_], in0=pb[:], in1=sg[c2][:], op=ALU.mult),
                                 r=[kb, ("sgm", c2)], w=[("mT", q), kb])
                        else:
                            S.op("dve", lambda pb=pb, c2=c2: V.tensor_tensor(out=tmp[c2][:], in0=pb[:], in1=sg[c2][:], op=ALU.mult),
                                 r=[kb, ("sgm", c2)], w=[("tmpm", c2), kb])
                            S.op("pool", lambda c2=c2: G.tensor_tensor(out=mT[:, ot, qs_], in0=mT[:, ot, qs_], in1=tmp[c2][:],
                                                                       op=ALU.add), r=[("tmpm", c2), ("mT", q)], w=[("mT", q)])
            self.dbg_dump("merged%d" % l, lambda o: S.dma("sp", o.rearrange("(kt p) n -> p kt n", p=128), mT[:],
                                                          r=[("mT", q) for q in range(4)]))
            S.barrier()
            st1.__exit__(None, None, None)
            wo = wbrs[1]
            S.dma("pool", wo[:], self.P["w_out"][l].rearrange("(kt p) n -> p kt n", p=128), w=[("wbr", 1)])
            gbc = self.sb(st, "gbc1", [128, D], F32)
            bbc = self.sb(st, "bbc1", [128, D], F32)
            S.dma("sp", gbc[:], self.P["ln1_g"][l].partition_broadcast(128), w=["gbc"])
            S.dma("sp", bbc[:], self.P["ln1_b"][l].partition_broadcast(128), w=["bbc"])
            lnw = self.ln_alloc(st)
            h1 = [self.sb(st, "h1m", [128, D], F32) for _ in range(2)]
            mbf1 = self.sb(st, "mbf", [128, KT, 128], BF16)
            mbf = [mbf1, mbf1]
            for tt in range(NT):
                s2 = tt % 2
                q = tt // 4
                tsl = slice(tt * 128, (tt + 1) * 128)
                S.op("act", lambda: A.activation(out=mbf[s2][:], in_=mT[:, :, tsl], func=AF.Copy), r=[("mT", q)], w=[("mbf", 0)])
                S.dma("sp", h1[s2][:], self.h_dram[tsl, :], r=[("hd", tt)], w=[("h1m", s2)])
                xin, kx = self.ln_xin(lnw, tt)
                for half in range(2):
                    po, ko = self.psum()
                    for kt in range(KT):
                        S.op("pe", lambda kt=kt, po=po: PE.matmul(po[:], lhsT=mbf[s2][:, kt, :], rhs=wo[:, kt, half * 512:(half + 1) * 512],
                                                                  start=(kt == 0), stop=(kt == KT - 1)), r=[("mbf", 0), ("wbr", 1)], w=[ko])
                    S.op("dve", lambda po=po, half=half: V.scalar_tensor_tensor(
                        out=xin[:, half * 512:(half + 1) * 512], in0=h1[s2][:, half * 512:(half + 1) * 512], scalar=ALPHA, in1=po[:],
                        op0=ALU.mult, op1=ALU.add), r=[ko, ("h1m", s2)], w=[kx, ko])
                self.ln_tile(lnw, tt, gbc, bbc, self.h_dram, router=True, extra=self.dbg_out.get("h1_%d" % l))
            S.barrier()

    def layer(self, l):
        S = self.S
        if "ssd" in self.stages:
            self.ssd(l)
            self.dbg_dump("ya%d" % l, lambda o: S.dma("sp", o, self.yT_dram[0], r=[("yT0", h) for h in range(8)]))
        if "rwkv" in self.stages:
            self.rwkv(l)
            self.dbg_dump("yb%d" % l, lambda o: S.dma("sp", o, self.yT_dram[1], r=[("yT1", h) for h in range(8)]))
        if "hgrn" in self.stages:
            self.hgrn(l)
            self.dbg_dump("yc%d" % l, lambda o: S.dma("sp", o, self.yT_dram[2], r=[("yT2", h) for h in range(8)]))
        if "merge" in self.stages:
            self.merge(l)
        if "moe" in self.stages:
            self.moe(l, last=(l == self.depth - 1))


_NC_CACHE = {}


def _get_nc():
    if "nc" not in _NC_CACHE:
        _NC_CACHE["nc"] = Builder().build()
    return _NC_CACHE["nc"]


def kernel(**inputs):
    nc = _get_nc()
    x = np.ascontiguousarray(inputs["x"], dtype=np.float32)
    base = {k: np.ascontiguousarray(inputs[k], dtype=np.float32) for k in PARAM_SHAPES}
    in_maps = []
    for c in range(8):
        m = dict(base)
        m["x"] = x[c]
        in_maps.append(m)
    res = run_bass_kernel_spmd(nc, in_maps, core_ids=list(range(8)))
    return np.stack([res.results[c]["out"] for c in range(8)], axis=0)
```

```python
import contextlib
import os
import numpy as np
CUT = int(os.environ.get('CUT', '99'))
HC = int(os.environ.get('HC', '99'))
HL = int(os.environ.get('HL', '99'))
import concourse.bass as bass
import concourse.mybir as mybir
from concourse.bass_utils import run_bass_kernel_spmd

F32 = mybir.dt.float32
BF16 = mybir.dt.bfloat16
AF = mybir.ActivationFunctionType
ALU = mybir.AluOpType
AX = mybir.AxisListType

D = 1024
T = 2048
NT = T // 128
KT = D // 128
DEPTH = 2
NE = 16
DEXP = 512
N_IN = 13072
ALPHA = (2 * DEPTH) ** 0.25
LN_EPS = 1e-5
RMS_EPS = 1e-6
GN_EPS = 64e-5
OFF_Z = 0
OFF_XBC = 1024
OFF_DT = 2560
OFF_RWKV = 2576
OFF_HGRN = OFF_RWKV + 3328
OFF_GATES = OFF_HGRN + 4096

PARAM_SHAPES = {
    "ln_in_g": [1024], "ln_in_b": [1024], "w_in": [2, 1024, 13072],
    "ssd_conv_w": [2, 4, 1536], "ssd_conv_b": [2, 1536], "ssd_dt_bias": [2, 16],
    "ssd_a_log": [2, 16], "ssd_d": [2, 16], "ssd_norm_w": [2, 1024],
    "rwkv_mu": [2, 3328], "rwkv_w0": [2, 1024], "rwkv_w2": [2, 64, 1024],
    "rwkv_a0": [2, 1024], "rwkv_a2": [2, 64, 1024], "rwkv_g2": [2, 128, 1024],
    "rwkv_k_k": [2, 1024], "rwkv_k_a": [2, 1024], "rwkv_r_k": [2, 16, 64],
    "rwkv_ln_w": [2, 1024], "rwkv_ln_b": [2, 1024], "hgrn_lb": [2, 1024],
    "hgrn_norm_w": [2, 128], "w_br_ssd": [2, 1024, 1024], "w_br_rwkv": [2, 1024, 1024],
    "w_br_hgrn": [2, 1024, 1024], "w_out": [2, 1024, 1024], "ln1_g": [2, 1024],
    "ln1_b": [2, 1024], "router_w": [1024, 16], "router_bias": [16],
    "exp_w_gate": [2, 16, 1024, 512], "exp_w_up": [2, 16, 1024, 512],
    "exp_w_down": [2, 16, 512, 1024], "ln2_g": [2, 1024], "ln2_b": [2, 1024],
}


class Sched:
    ENG = ["pe", "act", "dve", "pool", "sp"]

    def __init__(self, nc, es, n_dma=32, n_pdma=24):
        self.nc = nc
        self.e = {"pe": nc.tensor, "act": nc.scalar, "dve": nc.vector, "pool": nc.gpsimd, "sp": nc.sync}
        self.sem = {k: es.enter_context(nc.semaphore("sem_" + k)) for k in self.ENG}
        self.cnt = {k: 0 for k in self.ENG}
        self.dsem = [es.enter_context(nc.semaphore("dsem%d" % i)) for i in range(n_dma)]
        self.dtot = [0] * n_dma
        self.drr = 0
        self.psem = [es.enter_context(nc.semaphore("psem%d" % i)) for i in range(n_pdma)]
        self.pused = [False] * n_pdma
        self.pwaiters = [[] for _ in range(n_pdma)]
        self.pclr = [None] * n_pdma
        self.prr = 0
        self.msem = {k: es.enter_context(nc.semaphore("msem_" + k)) for k in self.ENG}
        self.mcnt = {k: 0 for k in self.ENG}
        self.seen = {k: {} for k in self.ENG}
        self.lastw = {}
        self.readers = {}
        self.nwait = 0

    def _semh(self, sk):
        if isinstance(sk, str):
            return self.sem[sk]
        if sk[0] == "m":
            return self.msem[sk[1]]
        return self.dsem[sk[1]] if sk[0] == "d" else self.psem[sk[1]]

    def _wait(self, e, tag):
        sk, val = tag
        if val <= 0 or self.seen[e].get(sk, 0) >= val:
            return
        if not isinstance(sk, str) and sk[0] == "p" and self.pclr[sk[1]] is not None and e != "pool":
            self._wait(e, self.pclr[sk[1]])
        self.e[e].wait_ge(self._semh(sk), val)
        self.seen[e][sk] = val
        self.nwait += 1
        if not isinstance(sk, str) and sk[0] == "p":
            self.pwaiters[sk[1]].append(self._marker(e))

    def _marker(self, e):
        self.e[e].sem_inc(self.msem[e], 1)
        self.mcnt[e] += 1
        return (("m", e), self.mcnt[e])

    def _deps(self, e, r, w):
        for k in r:
            t = self.lastw.get(k)
            if t is not None:
                self._wait(e, t)
        for k in w:
            t = self.lastw.get(k)
            if t is not None and (t[0] != e or e != "pe"):
                self._wait(e, t)
            for sk, val in self.readers.get(k, {}).items():
                if sk != e or e != "pe":
                    self._wait(e, (sk, val))

    def _record(self, tag, r, w):
        for k in r:
            d = self.readers.setdefault(k, {})
            if d.get(tag[0], 0) < tag[1]:
                d[tag[0]] = tag[1]
        for k in w:
            self.lastw[k] = tag
            self.readers[k] = {}

    def op(self, e, fn, r=(), w=()):
        self._deps(e, r, w)
        ins = fn()
        self.cnt[e] += 1
        ins.then_inc(self.sem[e], 1)
        if os.environ.get("OPLOG"):
            self.oplog = getattr(self, "oplog", {})
            self.oplog[(e, self.cnt[e])] = fn.__code__.co_firstlineno
        self._record((e, self.cnt[e]), r, w)

    def _dma_sw(self, out, in_, r, w):
        q = "pool"
        self._deps(q, r, w)
        i = self.prr
        self.prr = (self.prr + 1) % len(self.psem)
        sk = ("p", i)
        if self.pused[i]:
            self._wait(q, (sk, 16))
            for e in self.ENG:
                if e != q:
                    self._wait(e, (sk, 16))
            for tg in self.pwaiters[i]:
                if tg[0][1] != q:
                    self._wait(q, tg)
            self.e[q].sem_clear(self.psem[i])
            tclr = self._marker(q)
            self.pclr[i] = tclr
            for k, t in list(self.lastw.items()):
                if t[0] == sk:
                    self.lastw[k] = tclr
            for k, d in self.readers.items():
                if sk in d:
                    d.pop(sk)
                    d[tclr[0]] = tclr[1]
            for e in self.ENG:
                self.seen[e].pop(sk, None)
            self.pwaiters[i] = []
        ins = self.e[q].dma_start(out=out, in_=in_)
        ins.then_inc(self.psem[i], 16)
        self.pused[i] = True
        self._record((sk, 16), r, w)

    def dma(self, q, out, in_, r=(), w=()):
        if q == "pool" and os.environ.get("PSEM_CLEAR"):
            return self._dma_sw(out, in_, r, w)
        self._deps(q, r, w)
        i = self.drr
        self.drr = (self.drr + 1) % len(self.dsem)
        self._wait(q, (("d", i), self.dtot[i]))
        with self.nc.allow_non_contiguous_dma(reason="small per-feature parameter columns"):
            ins = self.e[q].dma_start(out=out, in_=in_)
        self.dtot[i] += 16
        ins.then_inc(self.dsem[i], 16)
        self._record((("d", i), self.dtot[i]), r, w)

    def barrier(self):
        for e in self.ENG:
            for o in self.ENG:
                if o != e:
                    self._wait(e, (o, self.cnt[o]))
            for i in range(len(self.dsem)):
                self._wait(e, (("d", i), self.dtot[i]))
            for i in range(len(self.psem)):
                if self.pused[i]:
                    self._wait(e, (("p", i), 16))

    def finish(self):
        for i in range(len(self.dsem)):
            self._wait("sp", (("d", i), self.dtot[i]))
        for i in range(len(self.psem)):
            if self.pused[i]:
                self._wait("sp", (("p", i), 16))
        for o in self.ENG:
            if o != "sp":
                self._wait("sp", (o, self.cnt[o]))


class Builder:
    def __init__(self, debug=None, stages=("pre", "hgrn", "ssd", "rwkv", "merge", "moe"), depth=DEPTH, pre_router=False):
        self.pre_router = pre_router
        self.debug = debug or {}
        self.stages = stages
        self.depth = depth
        self.nc = bass.Bass("TRN2", target_bir_lowering=False)
        nc = self.nc
        self.x = nc.dram_tensor("x", [T, D], F32, kind="ExternalInput").ap()
        self.P = {k: nc.dram_tensor(k, s, F32, kind="ExternalInput").ap() for k, s in PARAM_SHAPES.items()}
        self.out = nc.dram_tensor("out", [T, D], F32, kind="ExternalOutput").ap()
        self.h_dram = nc.dram_tensor("h_scr", [T, D], F32, kind="Internal").ap()
        self.yT_dram = [nc.dram_tensor("yT_scr%d" % i, [D, T], BF16, kind="Internal").ap() for i in range(3)]
        self.dbg_out = {}
        for name, (shape, dt) in self.debug.items():
            self.dbg_out[name] = nc.dram_tensor("dbg_" + name, shape, dt, kind="ExternalOutput").ap()
        self.uid = 0

    def sb(self, es, name, shape, dt):
        self.uid += 1
        return es.enter_context(self.nc.sbuf_tensor("%s_%d" % (name, self.uid), shape, dt))

    def psum(self):
        i = self.ps_rr
        self.ps_rr = (self.ps_rr + 1) % 8
        return self.ps[i], ("ps", i)

    def build(self):
        nc = self.nc
        with contextlib.ExitStack() as es:
            self.S = Sched(nc, es)
            S = self.S
            self.ps = [es.enter_context(nc.psum_tensor("psb%d" % i, [128, 512], F32)) for i in range(8)]
            self.ps_rr = 0
            self.ident32 = self.sb(es, "ident32", [128, 128], F32)
            self.identbf = self.sb(es, "identbf", [128, 128], BF16)
            self.zeros = self.sb(es, "zeros", [128, 128], F32)
            self.ones = self.sb(es, "ones", [128, 128], F32)
            self.onesbf = self.sb(es, "onesbf", [128, 128], BF16)
            self.epsc = self.sb(es, "epsc", [128, 4], F32)
            S.op("pool", lambda: nc.gpsimd.memset(self.zeros[:], 0.0), w=["zeros"])
            S.op("pool", lambda: nc.gpsimd.memset(self.ones[:], 1.0), w=["ones"])
            S.op("pool", lambda: nc.gpsimd.memset(self.onesbf[:], 1.0), w=["onesbf"])
            S.op("pool", lambda: nc.gpsimd.memset(self.epsc[:, 0:1], LN_EPS), w=["epsc"])
            S.op("pool", lambda: nc.gpsimd.memset(self.epsc[:, 1:2], RMS_EPS), w=["epsc"])
            S.op("pool", lambda: nc.gpsimd.memset(self.epsc[:, 2:3], GN_EPS), w=["epsc"])
            S.op("pool", lambda: nc.gpsimd.memset(self.epsc[:, 3:4], 1.0), w=["epsc"])
            S.op("pool", lambda: nc.gpsimd.affine_select(
                out=self.ident32[:], in_=self.zeros[:], pattern=[[1, 128]], compare_op=ALU.not_equal,
                fill=1.0, base=0, channel_multiplier=-1), r=["zeros"], w=["ident32"])
            S.op("pool", lambda: nc.gpsimd.tensor_copy(out=self.identbf[:], in_=self.ident32[:]),
                 r=["ident32"], w=["identbf"])
            self.hT = self.sb(es, "hT", [128, KT, T], BF16)
            self.gates = self.sb(es, "gates", [128, NT, NE], F32)
            self.logits = self.sb(es, "logits", [128, NT, NE], F32)
            self.rw32 = self.sb(es, "rw32", [128, KT, NE], F32)
            self.rbias = self.sb(es, "rbias", [128, NE], F32)
            S.dma("sp", self.rw32[:], self.P["router_w"].rearrange("(kt p) e -> p kt e", p=128), w=["rw32"])
            S.dma("sp", self.rbias[:], self.P["router_bias"].partition_broadcast(128), w=["rbias"])

            with contextlib.ExitStack() as st:
                gbc = self.sb(st, "gbc", [128, D], F32)
                bbc = self.sb(st, "bbc", [128, D], F32)
                S.dma("sp", gbc[:], self.P["ln_in_g"].partition_broadcast(128), w=["gbc"])
                S.dma("sp", bbc[:], self.P["ln_in_b"].partition_broadcast(128), w=["bbc"])
                lnw = self.ln_alloc(st)
                for tt in range(NT):
                    xin, kx = self.ln_xin(lnw, tt)
                    S.dma("sp", xin[:], self.x[tt * 128:(tt + 1) * 128, :], w=[kx])
                    self.ln_tile(lnw, tt, gbc, bbc, self.h_dram, router=self.pre_router, extra=self.dbg_out.get("h0"))
                S.barrier()
            self.dbg_dump("hT", lambda o: S.dma("sp", o, self.hT[:], r=[("hT", t) for t in range(NT)]))
            self.dbg_dump("logits", lambda o: S.dma("sp", o, self.logits[:], r=["logits"]))

            for l in range(self.depth):
                self.layer(l)
            S.finish()
        return nc

    def dbg_dump(self, name, fn):
        if name in self.dbg_out:
            fn(self.dbg_out[name])

    def ln_alloc(self, st):
        w = {}
        w["xin"] = [self.sb(st, "xin", [128, D], F32) for _ in range(2)]
        w["hh"] = [self.sb(st, "hh", [128, D], F32) for _ in range(2)]
        w["bst"] = [self.sb(st, "bst", [128, 2, 6], F32) for _ in range(2)]
        w["mv"] = [self.sb(st, "mv", [128, 4], F32) for _ in range(2)]
        w["h32"] = [self.sb(st, "h32", [128, KT, 128], F32) for _ in range(2)]
        w["id"] = self.uid
        return w

    def ln_xin(self, w, tt):
        return w["xin"][tt % 2], ("xin", w["id"], tt % 2)

    def ln_tile(self, w, tt, gbc, bbc, dst_dram, router, extra=None):
        nc, S = self.nc, self.S
        s = tt % 2
        wid = w["id"]
        xin, kx = w["xin"][s], ("xin", wid, s)
        hh, kh = w["hh"][s], ("hh", wid, s)
        bst, kb = w["bst"][s], ("bst", wid, s)
        mv, km = w["mv"][s], ("mv", wid, s)
        h32, k32 = w["h32"][s], ("h32", wid, s)
        for c in range(2):
            S.op("dve", lambda c=c: nc.vector.bn_stats(out=bst[:, c, :], in_=xin[:, c * 512:(c + 1) * 512]),
                 r=[kx], w=[kb])
        S.op("dve", lambda: nc.vector.bn_aggr(out=mv[:, 0:2], in_=bst[:].rearrange("p a b -> p (a b)")),
             r=[kb], w=[km])
        if CUT < 2:
            return
        S.op("act", lambda: nc.scalar.activation(out=mv[:, 2:3], in_=mv[:, 1:2], func=AF.Sqrt,
                                                 bias=self.epsc[:, 0:1], scale=1.0), r=[km, "epsc"], w=[km])
        S.op("dve", lambda: nc.vector.reciprocal(out=mv[:, 3:4], in_=mv[:, 2:3]), r=[km], w=[km])
        S.op("dve", lambda: nc.vector.tensor_scalar(out=xin[:], in0=xin[:], scalar1=mv[:, 0:1], scalar2=mv[:, 3:4],
                                                    op0=ALU.subtract, op1=ALU.mult), r=[kx, km], w=[kx])
        if CUT < 3:
            return
        S.op("pool", lambda: nc.gpsimd.tensor_tensor(out=hh[:], in0=xin[:], in1=gbc[:], op=ALU.mult),
             r=[kx, "gbc"], w=[kh])
        S.op("pool", lambda: nc.gpsimd.tensor_tensor(out=hh[:], in0=hh[:], in1=bbc[:], op=ALU.add),
             r=[kh, "bbc"], w=[kh])
        S.dma("sp", dst_dram[tt * 128:(tt + 1) * 128, :], hh[:], r=[kh], w=[("hd", tt)])
        if extra is not None:
            S.dma("sp", extra[tt * 128:(tt + 1) * 128, :], hh[:], r=[kh], w=[("hdx", tt)])
        if CUT < 4:
            return
        for half in range(2):
            pb, kp = self.psum()
            for j in range(4):
                kt = half * 4 + j
                S.op("pe", lambda j=j, kt=kt: nc.tensor.transpose(
                    out=pb[:, j * 128:(j + 1) * 128], in_=hh[:, kt * 128:(kt + 1) * 128], identity=self.ident32[:]),
                    r=[kh, "ident32"], w=[kp])
            if os.environ.get("EVAC", "act") == "act":
                S.op("act", lambda half=half, pb=pb: nc.scalar.activation(
                    out=self.hT[:, half * 4:(half + 1) * 4, tt * 128:(tt + 1) * 128],
                    in_=pb[:].rearrange("p (a b) -> p a b", a=4), func=AF.Copy), r=[kp], w=[("hT", tt), kp])
            else:
                S.op("dve", lambda half=half, pb=pb: nc.vector.tensor_copy(
                    out=self.hT[:, half * 4:(half + 1) * 4, tt * 128:(tt + 1) * 128],
                    in_=pb[:].rearrange("p (a b) -> p a b", a=4)), r=[kp], w=[("hT", tt), kp])
            if router:
                S.op("dve", lambda half=half, pb=pb: nc.vector.tensor_copy(
                    out=h32[:, half * 4:(half + 1) * 4, :], in_=pb[:].rearrange("p (a b) -> p a b", a=4)),
                    r=[kp], w=[k32, kp])
        if router and CUT >= 5:
            pb, kp = self.psum()
            for kt in range(KT):
                S.op("pe", lambda kt=kt: nc.tensor.matmul(pb[:, 0:NE], lhsT=h32[:, kt, :], rhs=self.rw32[:, kt, :],
                                                          start=(kt == 0), stop=(kt == KT - 1)),
                     r=[k32, "rw32"], w=[kp])
            S.op("dve", lambda: nc.vector.tensor_copy(out=self.logits[:, tt, :], in_=pb[:, 0:NE]),
                 r=[kp], w=["logits"])

    def router(self, st):
        nc, S = self.nc, self.S
        V = nc.vector
        L = self.logits
        t1 = self.sb(st, "rt1", [128, NT, NE], F32)
        probs = self.sb(st, "probs", [128, NT, NE], F32)
        sel = self.sb(st, "sel", [128, NT, NE], F32)
        p6 = self.sb(st, "p6", [128, NT, 4, 6], F32)
        gs = self.sb(st, "gs", [128, NT, 4], F32)
        gm = self.sb(st, "gm", [128, NT, 4], F32)
        gt = self.sb(st, "gt", [128, NT, 4], F32)
        red = self.sb(st, "red", [128, NT], F32)
        red2 = self.sb(st, "red2", [128, NT], F32)
        msk = self.sb(st, "msk", [128, NT, NE], F32)
        eq = self.sb(st, "eq", [128, NT, NE], F32)
        BIG = 1.0e9

        def bc(a):
            return a[:].unsqueeze(2).to_broadcast([128, NT, NE])

        S.op("dve", lambda: V.tensor_reduce(out=red[:], in_=L[:], axis=AX.X, op=ALU.max), r=["logits"], w=["red"])
        S.op("dve", lambda: V.tensor_tensor(out=t1[:], in0=L[:], in1=bc(red), op=ALU.subtract),
             r=["logits", "red"], w=["rt1"])
        S.op("act", lambda: nc.scalar.activation(out=t1[:], in_=t1[:], func=AF.Exp), r=["rt1"], w=["rt1"])
        S.op("dve", lambda: V.tensor_reduce(out=red[:], in_=t1[:], axis=AX.X, op=ALU.add), r=["rt1"], w=["red"])
        S.op("dve", lambda: V.reciprocal(out=red[:], in_=red[:]), r=["red"], w=["red"])
        S.op("dve", lambda: V.tensor_tensor(out=probs[:], in0=t1[:], in1=bc(red), op=ALU.mult),
             r=["rt1", "red"], w=["probs"])
        S.op("dve", lambda: V.tensor_tensor(out=sel[:], in0=probs[:],
                                            in1=self.rbias[:].unsqueeze(1).to_broadcast([128, NT, NE]), op=ALU.add),
             r=["probs", "rbias"], w=["sel"])
        s4 = sel[:].rearrange("p t (g e) -> p t g e", g=4)
        S.op("dve", lambda: V.tensor_tensor(out=p6[:, :, :, 0:3], in0=s4[:, :, :, 0:3], in1=s4[:, :, :, 1:4],
                                            op=ALU.add), r=["sel"], w=["p6"])
        S.op("dve", lambda: V.tensor_tensor(out=p6[:, :, :, 3:5], in0=s4[:, :, :, 0:2], in1=s4[:, :, :, 2:4],
                                            op=ALU.add), r=["sel"], w=["p6"])
        S.op("dve", lambda: V.tensor_tensor(out=p6[:, :, :, 5:6], in0=s4[:, :, :, 0:1], in1=s4[:, :, :, 3:4],
                                            op=ALU.add), r=["sel"], w=["p6"])
        S.op("dve", lambda: V.tensor_reduce(out=gs[:], in_=p6[:], axis=AX.X, op=ALU.max), r=["p6"], w=["gs"])
        S.op("dve", lambda: V.tensor_reduce(out=red[:], in_=gs[:], axis=AX.X, op=ALU.max), r=["gs"], w=["red"])
        S.op("dve", lambda: V.tensor_tensor(out=gm[:], in0=gs[:], in1=red[:].unsqueeze(2).to_broadcast([128, NT, 4]),
                                            op=ALU.is_ge), r=["gs", "red"], w=["gm"])
        S.op("dve", lambda: V.tensor_scalar(out=gt[:], in0=gm[:], scalar1=BIG, scalar2=-BIG, op0=ALU.mult,
                                            op1=ALU.add), r=["gm"], w=["gt"])
        m4 = msk[:].rearrange("p t (g e) -> p t g e", g=4)
        S.op("dve", lambda: V.tensor_tensor(out=m4, in0=s4, in1=gm[:].unsqueeze(3).to_broadcast([128, NT, 4, 4]),
                                            op=ALU.mult), r=["sel", "gm"], w=["msk"])
        S.op("dve", lambda: V.tensor_tensor(out=m4, in0=m4, in1=gt[:].unsqueeze(3).to_broadcast([128, NT, 4, 4]),
                                            op=ALU.add), r=["msk", "gt"], w=["msk"])
        S.op("dve", lambda: V.tensor_reduce(out=red[:], in_=msk[:], axis=AX.X, op=ALU.max), r=["msk"], w=["red"])
        S.op("dve", lambda: V.tensor_tensor(out=eq[:], in0=msk[:], in1=bc(red), op=ALU.is_equal),
             r=["msk", "red"], w=["eq"])
        S.op("dve", lambda: V.scalar_tensor_tensor(out=eq[:], in0=eq[:], scalar=-BIG, in1=msk[:], op0=ALU.mult,
                                                   op1=ALU.add), r=["eq", "msk"], w=["eq"])
        S.op("dve", lambda: V.tensor_reduce(out=red2[:], in_=eq[:], axis=AX.X, op=ALU.max), r=["eq"], w=["red2"])
        S.op("dve", lambda: V.tensor_tensor(out=eq[:], in0=msk[:], in1=bc(red2), op=ALU.is_ge),
             r=["msk", "red2"], w=["eq"])
        S.op("dve", lambda: V.tensor_tensor(out=eq[:], in0=eq[:], in1=probs[:], op=ALU.mult),
             r=["eq", "probs"], w=["eq"])
        S.op("dve", lambda: V.tensor_reduce(out=red[:], in_=eq[:], axis=AX.X, op=ALU.add), r=["eq"], w=["red"])
        S.op("dve", lambda: V.reciprocal(out=red[:], in_=red[:]), r=["red"], w=["red"])
        S.op("dve", lambda: V.tensor_tensor(out=self.gates[:], in0=eq[:], in1=bc(red), op=ALU.mult),
             r=["eq", "red"], w=["gates"])

    def moe(self, l, last):
        nc, S = self.nc, self.S
        wg_d, wu_d, wd_d = self.P["exp_w_gate"], self.P["exp_w_up"], self.P["exp_w_down"]
        with contextlib.ExitStack() as st:
            self.router(st)
            self.dbg_dump("gates%d" % l, lambda o: S.dma("sp", o, self.gates[:], r=["gates"]))
            acc = self.sb(st, "acc", [128, NT, D], F32)
            wg = [self.sb(st, "wg", [128, KT, DEXP], BF16) for _ in range(2)]
            wu = [self.sb(st, "wu", [128, KT, DEXP], BF16) for _ in range(2)]
            wd = [self.sb(st, "wd", [128, 4, D], BF16) for _ in range(2)]
            hg = [self.sb(st, "hg", [128, 4, 512], BF16) for _ in range(2)]
            sg = [self.sb(st, "sg", [128, 512], BF16) for _ in range(2)]
            gbc = self.sb(st, "gbc2", [128, D], F32)
            bbc = self.sb(st, "bbc2", [128, D], F32)
            S.dma("sp", gbc[:], self.P["ln2_g"][l].partition_broadcast(128), w=["gbc"])
            S.dma("sp", bbc[:], self.P["ln2_b"][l].partition_broadcast(128), w=["bbc"])

            def load_w(e):
                b = e % 2
                S.dma("pool", wg[b][:], wg_d[l, e].rearrange("(kt p) n -> p kt n", p=128), w=[("wg", b)])
                S.dma("pool", wu[b][:], wu_d[l, e].rearrange("(kt p) n -> p kt n", p=128), w=[("wu", b)])
                S.dma("pool", wd[b][:], wd_d[l, e].rearrange("(kt p) n -> p kt n", p=128), w=[("wd", b)])

            items = [(e, q) for e in range(int(os.environ.get('ME', NE))) for q in range(4)]
            sgi = [0]

            def G(i):
                e, q = items[i]
                b = e % 2
                hb = i % 2
                for dt_ in range(4):
                    pa, ka = self.psum()
                    pu, ku = self.psum()
                    for kt in range(KT):
                        S.op("pe", lambda kt=kt, pa=pa: nc.tensor.matmul(
                            pa[:], lhsT=wg[b][:, kt, dt_ * 128:(dt_ + 1) * 128], rhs=self.hT[:, kt, q * 512:(q + 1) * 512],
                            start=(kt == 0), stop=(kt == KT - 1)),
                            r=[("wg", b)] + [("hT", q * 4 + j) for j in range(4)], w=[ka])
                    for kt in range(KT):
                        S.op("pe", lambda kt=kt, pu=pu: nc.tensor.matmul(
                            pu[:], lhsT=wu[b][:, kt, dt_ * 128:(dt_ + 1) * 128], rhs=self.hT[:, kt, q * 512:(q + 1) * 512],
                            start=(kt == 0), stop=(kt == KT - 1)),
                            r=[("wu", b)] + [("hT", q * 4 + j) for j in range(4)], w=[ku])
                    si = sgi[0] % 2
                    sgi[0] += 1
                    S.op("act", lambda pa=pa, si=si: nc.scalar.activation(out=sg[si][:], in_=pa[:], func=AF.Silu),
                         r=[ka], w=[("sg", si)])
                    S.op("dve", lambda pu=pu, si=si: nc.vector.tensor_tensor(
                        out=hg[hb][:, dt_, :], in0=pu[:], in1=sg[si][:], op=ALU.mult),
                        r=[ku, ("sg", si)], w=[("hg", hb)])

            def Dn(i):
                e, q = items[i]
                b = e % 2
                hb = i % 2
                for j in range(4):
                    tt = q * 4 + j
                    for half in range(2):
                        pc, kc = self.psum()
                        for dt_ in range(4):
                            S.op("pe", lambda dt_=dt_, pc=pc: nc.tensor.matmul(
                                pc[:], lhsT=hg[hb][:, dt_, j * 128:(j + 1) * 128],
                                rhs=wd[b][:, dt_, half * 512:(half + 1) * 512], start=(dt_ == 0), stop=(dt_ == 3)),
                                r=[("hg", hb), ("wd", b)], w=[kc])
                        dst = acc[:, tt, half * 512:(half + 1) * 512]
                        if e == 0:
                            S.op("dve", lambda pc=pc, dst=dst: nc.vector.tensor_scalar(
                                out=dst, in0=pc[:], scalar1=self.gates[:, tt, e:e + 1], scalar2=None, op0=ALU.mult),
                                r=[kc, "gates"], w=[("acc", tt)])
                        else:
                            S.op("dve", lambda pc=pc, dst=dst: nc.vector.scalar_tensor_tensor(
                                out=dst, in0=pc[:], scalar=self.gates[:, tt, e:e + 1], in1=dst, op0=ALU.mult,
                                op1=ALU.add), r=[kc, "gates", ("acc", tt)], w=[("acc", tt)])

            load_w(0)
            for i in range(len(items)):
                e, q = items[i]
                G(i)
                if i >= 1:
                    Dn(i - 1)
                if q == 0 and e + 1 < int(os.environ.get('ME', NE)):
                    load_w(e + 1)
            Dn(len(items) - 1)
            self.dbg_dump("moe%d" % l, lambda o: S.dma("sp", o.rearrange("(t p) d -> p t d", p=128), acc[:],
                                                       r=[("acc", t) for t in range(NT)]))
            lnw = self.ln_alloc(st)
            h1 = [self.sb(st, "h1t", [128, D], F32) for _ in range(2)]
            dst = self.out if last else self.h_dram
            for tt in range(NT):
                s = tt % 2
                S.dma("sp", h1[s][:], self.h_dram[tt * 128:(tt + 1) * 128, :], r=[("hd", tt)], w=[("h1t", s)])
                xin, kx = self.ln_xin(lnw, tt)
                S.op("dve", lambda s=s, xin=xin: nc.vector.scalar_tensor_tensor(
                    out=xin[:], in0=h1[s][:], scalar=ALPHA, in1=acc[:, tt, :], op0=ALU.mult, op1=ALU.add),
                    r=[("h1t", s), ("acc", tt)], w=[kx])
                self.ln_tile(lnw, tt, gbc, bbc, dst, router=False)
            S.barrier()


    def hgrn(self, l):
        nc, S = self.nc, self.S
        V, A, G, PE = nc.vector, nc.scalar, nc.gpsimd, nc.tensor
        Wl = self.P["w_in"][l]
        ydst = self.yT_dram[2]
        with contextlib.ExitStack() as st:
            mask2 = self.sb(st, "mask2", [128, 128], F32)
            rm = self.sb(st, "rm", [128, T], F32)
            nw = self.sb(st, "nw", [128, 1], F32)
            lbt = self.sb(st, "lbt", [128, 8, 2], F32)
            lbv = self.sb(st, "lbv", [128, 8], F32)
            oml = self.sb(st, "oml", [128, 8], F32)
            S.op("pool", lambda: G.affine_select(out=mask2[:], in_=self.ones[:], pattern=[[1, 128]],
                                                 compare_op=ALU.is_ge, fill=0.0, base=0, channel_multiplier=-1),
                 r=["ones"], w=["mask2"])
            S.op("pool", lambda: G.memset(mask2[0:64, 64:128], 0.0), w=["mask2"])
            S.op("pool", lambda: G.memset(rm[:], 1.0), w=["rm"])
            S.op("pool", lambda: G.memset(rm[:].rearrange("p (c j) -> p c j", j=64)[:, :, 0:1], 0.0), w=["rm"])
            S.dma("sp", nw[:], self.P["hgrn_norm_w"][l].rearrange("(p o) -> p o", o=1), w=["nw"])
            if l == 0:
                S.op("pool", lambda: G.memset(lbv[:], 0.0), w=["lbv"])
                S.op("pool", lambda: G.memset(oml[:], 1.0), w=["oml"])
            else:
                for j in range(2):
                    S.dma("sp", lbt[:, :, j:j + 1],
                          self.P["hgrn_lb"][j].rearrange("(h p o) -> p h o", p=128, o=1), w=["lbt"])
                S.op("dve", lambda: V.tensor_tensor(out=lbv[:], in0=lbt[:, :, 1], in1=lbt[:, :, 0], op=ALU.subtract),
                     r=["lbt"], w=["lbv"])
                S.op("act", lambda: A.activation(out=lbv[:], in_=lbv[:], func=AF.Sigmoid), r=["lbv"], w=["lbv"])
                S.op("dve", lambda: V.tensor_scalar(out=oml[:], in0=lbv[:], scalar1=-1.0, scalar2=1.0, op0=ALU.mult,
                                                    op1=ALU.add), r=["lbv"], w=["oml"])
            w4 = [self.sb(st, "w4", [128, 4, KT, 128], BF16) for _ in range(2)]
            qs = self.sb(st, "qs", [128, T], F32)
            fs = self.sb(st, "fs", [128, T], F32)
            lf = self.sb(st, "lf", [128, T], F32)
            bc = self.sb(st, "bc", [128, T], F32)
            enb = self.sb(st, "enb", [128, T], F32)
            def two(name, shape, dt):
                return [self.sb(st, name, shape, dt) for _ in range(2)]
            qbL, kbL, gsL, ytL = two("qb", [128, T], BF16), two("kb", [128, T], BF16), two("gs", [128, T], BF16), two("yt", [128, T], BF16)
            vL, kbtL, kbtBL = two("v", [128, NT, 128], BF16), two("kbt", [128, NT, 128], BF16), two("kbtB", [128, NT, 128], BF16)
            ebL = two("ebh", [128, T], F32)
            S32L = two("S32", [128, 128], F32)
            SbfL = [[self.sb(st, "Sbf", [128, 128], BF16) for _ in range(4)] for _ in range(2)]
            attmL = [two("attm", [128, 128], BF16) for _ in range(2)]
            osbL = [two("osb", [128, 128], F32) for _ in range(2)]
            osqL = [two("osq", [128, 128], BF16) for _ in range(2)]
            sdL = [two("sd", [128, 128], F32) for _ in range(2)]
            mAB = self.sb(st, "mAB", [128, 2], F32)
            S.op("pool", lambda: G.memset(mAB[0:64, 0:1], 1.0), w=["mAB"])
            S.op("pool", lambda: G.memset(mAB[64:128, 0:1], 0.0), w=["mAB"])
            S.op("pool", lambda: G.memset(mAB[0:64, 1:2], 0.0), w=["mAB"])
            S.op("pool", lambda: G.memset(mAB[64:128, 1:2], 1.0), w=["mAB"])
            hTk = [("hT", t) for t in range(NT)]

            def load_w(h):
                b = h % 2
                for j in range(4):
                    c0 = OFF_HGRN + j * 1024 + h * 128
                    S.dma("pool", w4[b][:, j], Wl[:, c0:c0 + 128].rearrange("(kt p) n -> p kt n", p=128),
                          w=[("w4", b)])

            def prep(h):
                hb = b = h % 2
                qb, kb, gs, v, kbt, kbtB, eb = qbL[hb], kbL[hb], gsL[hb], vL[hb], kbtL[hb], kbtBL[hb], ebL[hb]
                K = lambda n: (n, hb)
                if h + 1 < 8:
                    load_w(h + 1)
                for (j, func, dst, kd) in ((0, AF.Silu, qs, "qs"), (1, AF.Sigmoid, fs, "fs"), (3, AF.Sigmoid, gs, K("gs"))):
                    for tq in range(4):
                        pb, kp = self.psum()
                        for kt in range(KT):
                            S.op("pe", lambda kt=kt, pb=pb, j=j, tq=tq: PE.matmul(
                                pb[:], lhsT=w4[b][:, j, kt, :], rhs=self.hT[:, kt, tq * 512:(tq + 1) * 512],
                                start=(kt == 0), stop=(kt == KT - 1)), r=[("w4", b)] + hTk[tq * 4:tq * 4 + 4], w=[kp])
                        S.op("act", lambda pb=pb, dst=dst, func=func, tq=tq: A.activation(
                            out=dst[:, tq * 512:(tq + 1) * 512], in_=pb[:], func=func), r=[kp], w=[kd, kp])
                for t4 in range(4):
                    pb, kp = self.psum()
                    for j4 in range(4):
                        tt = t4 * 4 + j4
                        for kt in range(KT):
                            S.op("pe", lambda kt=kt, pb=pb, j4=j4, tt=tt: PE.matmul(
                                pb[:, j4 * 128:(j4 + 1) * 128], lhsT=self.hT[:, kt, tt * 128:(tt + 1) * 128],
                                rhs=w4[b][:, 2, kt, :], start=(kt == 0), stop=(kt == KT - 1)),
                                r=[("w4", b), ("hT", tt)], w=[kp])
                    S.op("dve", lambda pb=pb, t4=t4: V.tensor_copy(
                        out=v[:, t4 * 4:(t4 + 1) * 4, :], in_=pb[:].rearrange("p (a b) -> p a b", a=4)),
                        r=[kp], w=[K("v"), kp])
                S.op("dve", lambda: V.tensor_scalar(out=fs[:], in0=fs[:], scalar1=oml[:, h:h + 1], scalar2=lbv[:, h:h + 1],
                                                    op0=ALU.mult, op1=ALU.add), r=["fs", "oml", "lbv"], w=["fs"])
                S.op("act", lambda: A.activation(out=lf[:], in_=fs[:], func=AF.Ln), r=["fs"], w=["lf"])
                S.op("dve", lambda: V.tensor_tensor_scan(out=bc[:], data0=rm[:], data1=lf[:], initial=0.0,
                                                         op0=ALU.mult, op1=ALU.add), r=["rm", "lf"], w=["bc"])
                S.op("act", lambda: A.activation(out=eb[:], in_=bc[:], func=AF.Exp), r=["bc"], w=[K("eb")])
                S.op("act", lambda: A.activation(out=enb[:], in_=bc[:], func=AF.Exp, scale=-1.0), r=["bc"], w=["enb"])
                S.op("dve", lambda: V.tensor_scalar(out=fs[:], in0=fs[:], scalar1=-1.0, scalar2=1.0, op0=ALU.mult,
                                                    op1=ALU.add), r=["fs", "lf"], w=["fs"])
                S.op("pool", lambda: G.tensor_tensor(out=qb[:], in0=qs[:], in1=eb[:], op=ALU.mult),
                     r=["qs", K("eb")], w=[K("qb")])
                S.op("dve", lambda: V.tensor_tensor(out=kb[:], in0=fs[:], in1=enb[:], op=ALU.mult),
                     r=["fs", "enb"], w=[K("kb")])
                for t4 in range(4):
                    pb, kp = self.psum()
                    pbv = pb[:].bitcast(BF16)
                    for j4 in range(4):
                        tt = t4 * 4 + j4
                        S.op("pe", lambda pbv=pbv, j4=j4, tt=tt: PE.transpose(
                            out=pbv[:, j4 * 128:(j4 + 1) * 128], in_=kb[:, tt * 128:(tt + 1) * 128],
                            identity=self.identbf[:]), r=[K("kb"), "identbf"], w=[kp])
                    S.op("dve", lambda pbv=pbv, t4=t4: V.tensor_scalar(
                        out=kbt[:, t4 * 4:(t4 + 1) * 4, :], in0=pbv[:, 0:512].rearrange("p (a b) -> p a b", a=4),
                        scalar1=mAB[:, 0:1], scalar2=None, op0=ALU.mult), r=[kp, "mAB"], w=[K("kbt"), kp])
                    S.op("dve", lambda pbv=pbv, t4=t4: V.tensor_scalar(
                        out=kbtB[:, t4 * 4:(t4 + 1) * 4, :], in0=pbv[:, 0:512].rearrange("p (a b) -> p a b", a=4),
                        scalar1=mAB[:, 1:2], scalar2=None, op0=ALU.mult), r=[kp, "mAB"], w=[K("kbtB"), kp])
                S.op("pool", lambda: G.memset(S32L[hb][:], 0.0), w=[K("S32")])
                S.op("pool", lambda: G.memset(SbfL[hb][0][:], 0.0), w=[("Sbf", hb, 0)])

            def tile(h, tt):
                hb = h % 2
                qb, kb, gs, yt, v, kbt, kbtB, eb = qbL[hb], kbL[hb], gsL[hb], ytL[hb], vL[hb], kbtL[hb], kbtBL[hb], ebL[hb]
                S32, Sbf = S32L[hb], SbfL[hb]
                K = lambda n: (n, hb)
                cA, cB = 2 * tt, 2 * tt + 1
                tsl = slice(tt * 128, (tt + 1) * 128)
                i2 = tt % 2
                attm, osb, osq, sd = attmL[hb][i2], osbL[hb][i2], osqL[hb][i2], sdL[hb][i2]
                ka, ko, kq, ks = ("attm", hb, i2), ("osb", hb, i2), ("osq", hb, i2), ("sd", hb, i2)
                pa, kpa = self.psum()
                S.op("pe", lambda: PE.matmul(pa[:, 0:128], lhsT=kb[:, tsl], rhs=qb[:, tsl], start=True, stop=True),
                     r=[K("kb"), K("qb")], w=[kpa])
                yield
                S.op("dve", lambda: V.tensor_tensor(out=attm[:], in0=pa[:, 0:128], in1=mask2[:], op=ALU.mult),
                     r=[kpa, "mask2"], w=[ka, kpa])
                yield
                pr, kpr = self.psum()
                S.op("pe", lambda: PE.matmul(pr[:, 0:128], lhsT=kbt[:, tt, :], rhs=v[:, tt, :], start=True, stop=True),
                     r=[K("kbt"), K("v")], w=[kpr])
                S.op("pe", lambda: PE.matmul(pr[:, 128:256], lhsT=kbtB[:, tt, :], rhs=v[:, tt, :], start=True, stop=True),
                     r=[K("kbtB"), K("v")], w=[kpr])
                yield
                for (ci, off) in ((cA, 0), (cB, 128)):
                    S.op("dve", lambda off=off: V.tensor_tensor(
                        out=S32[:], in0=pr[:, off:off + 128], in1=S32[:], op=ALU.add), r=[kpr, K("S32")], w=[K("S32"), kpr])
                    S.op("dve", lambda ci=ci: V.tensor_scalar(
                        out=S32[:], in0=S32[:], scalar1=eb[:, ci * 64 + 63:ci * 64 + 64], scalar2=None, op0=ALU.mult),
                        r=[K("S32"), K("eb")], w=[K("S32")])
                    S.op("pool", lambda ci=ci: G.tensor_copy(out=Sbf[(ci + 1) % 4][:], in_=S32[:]),
                         r=[K("S32")], w=[("Sbf", hb, (ci + 1) % 4)])
                    yield
                po, kpo = self.psum()
                S.op("pe", lambda: PE.matmul(po[:, 0:128], lhsT=v[:, tt, :], rhs=attm[:], start=True, stop=False),
                     r=[K("v"), ka], w=[kpo])
                S.op("pe", lambda: PE.matmul(po[:, 0:64], lhsT=Sbf[cA % 4][:], rhs=qb[:, tt * 128:tt * 128 + 64],
                                             start=False, stop=False), r=[("Sbf", hb, cA % 4), K("qb")], w=[kpo])
                S.op("pe", lambda: PE.matmul(po[:, 64:128], lhsT=Sbf[cB % 4][:], rhs=qb[:, tt * 128 + 64:(tt + 1) * 128],
                                             start=False, stop=True), r=[("Sbf", hb, cB % 4), K("qb")], w=[kpo])
                yield
                S.op("act", lambda: A.activation(out=osb[:], in_=po[:, 0:128], func=AF.Copy), r=[kpo], w=[ko, kpo])
                S.op("act", lambda: A.activation(out=osq[:], in_=po[:, 0:128], func=AF.Square), r=[kpo], w=[kq, kpo])
                yield
                pss, kps = self.psum()
                S.op("pe", lambda: PE.matmul(pss[:, 0:128], lhsT=self.onesbf[:], rhs=osq[:], start=True, stop=True),
                     r=["onesbf", kq], w=[kps])
                yield
                S.op("act", lambda: A.activation(out=sd[:], in_=pss[:, 0:128], func=AF.Sqrt, bias=self.epsc[:, 1:2],
                                                 scale=1.0 / 128.0), r=[kps, "epsc"], w=[ks, kps])
                yield
                S.op("dve", lambda: V.reciprocal(out=sd[:], in_=sd[:]), r=[ks], w=[ks])
                yield
                S.op("dve", lambda: V.scalar_tensor_tensor(out=osb[:], in0=osb[:], scalar=nw[:, 0:1], in1=sd[:],
                                                           op0=ALU.mult, op1=ALU.mult), r=[ko, ks, "nw"], w=[ko])
                S.op("dve", lambda: V.tensor_tensor(out=yt[:, tsl], in0=osb[:], in1=gs[:, tsl], op=ALU.mult),
                     r=[ko, K("gs")], w=[K("yt")])

            load_w(0)
            for hp in range(4):
                prep(2 * hp)
                prep(2 * hp + 1)
                for tt in range(NT):
                    gens = [tile(2 * hp, tt), tile(2 * hp + 1, tt)]
                    while gens:
                        for g_ in list(gens):
                            try:
                                next(g_)
                            except StopIteration:
                                gens.remove(g_)
                for hb in range(2):
                    h = 2 * hp + hb
                    S.dma("sp", ydst[h * 128:(h + 1) * 128, :], ytL[hb][:], r=[("yt", hb)], w=[("yT2", h)])
            S.barrier()

    def ssd(self, l):
        nc, S = self.nc, self.S
        V, A, G, PE = nc.vector, nc.scalar, nc.gpsimd, nc.tensor
        Wl = self.P["w_in"][l]
        ydst = self.yT_dram[0]
        NEG = -30000.0
        hTk = [("hT", t) for t in range(NT)]
        with contextlib.ExitStack() as st:
            tri2 = self.sb(st, "tri2", [128, 128], F32)
            same2 = self.sb(st, "same2", [128, 128], F32)
            indA = self.sb(st, "indA", [128, 128], F32)
            indB = self.sb(st, "indB", [128, 128], F32)
            mAB = self.sb(st, "mABs", [128, 2], F32)
            negmask = self.sb(st, "negmask", [128, 8, 128], F32)
            bd1 = self.sb(st, "bd", [16, 8, 128], F32)
            bd = [bd1, bd1]
            S.op("pool", lambda: G.affine_select(out=tri2[:], in_=self.ones[:], pattern=[[1, 128]], compare_op=ALU.is_ge,
                                                 fill=0.0, base=0, channel_multiplier=-1), r=["ones"], w=["tri2"])
            S.op("pool", lambda: G.memset(tri2[0:64, 64:128], 0.0), w=["tri2"])
            S.op("pool", lambda: G.memset(same2[:], 0.0), w=["same2"])
            S.op("pool", lambda: G.memset(same2[0:64, 0:64], 1.0), w=["same2"])
            S.op("pool", lambda: G.memset(same2[64:128, 64:128], 1.0), w=["same2"])
            S.op("pool", lambda: G.memset(indA[0:64, :], 1.0), w=["indA"])
            S.op("pool", lambda: G.memset(indA[64:128, :], 0.0), w=["indA"])
            S.op("pool", lambda: G.memset(indB[0:64, :], 0.0), w=["indB"])
            S.op("pool", lambda: G.memset(indB[64:128, :], 1.0), w=["indB"])
            S.op("pool", lambda: G.memset(mAB[0:64, 0:1], 1.0), w=["mAB"])
            S.op("pool", lambda: G.memset(mAB[64:128, 0:1], 0.0), w=["mAB"])
            S.op("pool", lambda: G.memset(mAB[0:64, 1:2], 0.0), w=["mAB"])
            S.op("pool", lambda: G.memset(mAB[64:128, 1:2], 1.0), w=["mAB"])
            S.op("pool", lambda: G.memset(negmask[:], 0.0), w=["negmask"])
            S.op("pool", lambda: G.affine_select(out=negmask[:], in_=negmask[:], pattern=[[0, 8], [1, 128]],
                                                 compare_op=ALU.is_ge, fill=NEG, base=0, channel_multiplier=-1),
                 r=["negmask"], w=["negmask"])
            S.op("pool", lambda: G.memset(negmask[0:64, :, 64:128], NEG), w=["negmask"])
            dtb = self.sb(st, "dtb", [128, 16], F32)
            alog = self.sb(st, "alog", [128, 16], F32)
            dsk = self.sb(st, "dsk", [128, 16], F32)
            nwbc = self.sb(st, "nwbc", [128, D], F32)
            S.dma("sp", dtb[:], self.P["ssd_dt_bias"][l].partition_broadcast(128), w=["dtb"])
            S.dma("sp", alog[:], self.P["ssd_a_log"][l].partition_broadcast(128), w=["alog"])
            S.dma("sp", dsk[:], self.P["ssd_d"][l].partition_broadcast(128), w=["dsk"])
            S.dma("sp", nwbc[:], self.P["ssd_norm_w"][l].partition_broadcast(128), w=["nwbc"])
            S.op("act", lambda: A.activation(out=alog[:], in_=alog[:], func=AF.Exp), r=["alog"], w=["alog"])
            S.op("dve", lambda: V.tensor_scalar(out=alog[:], in0=alog[:], scalar1=-1.0, scalar2=None, op0=ALU.mult),
                 r=["alog"], w=["alog"])
            wdt = self.sb(st, "wdt", [128, KT, 16], BF16)
            S.dma("pool", wdt[:], Wl[:, OFF_DT:OFF_DT + 16].rearrange("(kt p) n -> p kt n", p=128), w=["wdt"])
            dt = self.sb(st, "dt", [128, NT, 16], F32)
            da = self.sb(st, "da", [128, NT, 16], F32)
            cum4 = self.sb(st, "cum4", [128, NT, 4, 16], F32)
            eacs = self.sb(st, "eacs", [128, NT, 16], F32)
            eend = self.sb(st, "eend", [128, NT, 16], F32)
            edec = self.sb(st, "edec", [128, NT, 2, 16], F32)
            acsTt = [self.sb(st, "acsTt", [16, 128], F32) for _ in range(2)]
            nacsTt = [self.sb(st, "nacsTt", [16, 128], F32) for _ in range(2)]
            pb, kp = self.psum()
            for tt in range(NT):
                for kt in range(KT):
                    S.op("pe", lambda kt=kt, tt=tt: PE.matmul(pb[:, tt * 16:(tt + 1) * 16], lhsT=self.hT[:, kt, tt * 128:(tt + 1) * 128],
                                                              rhs=wdt[:, kt, :], start=(kt == 0), stop=(kt == KT - 1)),
                         r=["wdt", ("hT", tt)], w=[kp])
            S.op("dve", lambda: V.tensor_tensor(out=dt[:], in0=pb[:, 0:256].rearrange("p (t h) -> p t h", h=16),
                                                in1=dtb[:].unsqueeze(1).to_broadcast([128, NT, 16]), op=ALU.add),
                 r=[kp, "dtb"], w=["dt", kp])
            S.op("act", lambda: A.activation(out=dt[:], in_=dt[:], func=AF.Exp), r=["dt"], w=["dt"])
            S.op("act", lambda: A.activation(out=dt[:], in_=dt[:], func=AF.Ln, bias=self.epsc[:, 3:4], scale=1.0),
                 r=["dt", "epsc"], w=["dt"])
            S.op("dve", lambda: V.tensor_tensor(out=da[:], in0=dt[:], in1=alog[:].unsqueeze(1).to_broadcast([128, NT, 16]),
                                                op=ALU.mult), r=["dt", "alog"], w=["da"])
            for half in range(2):
                pb, kp = self.psum()
                for j in range(8):
                    tt = half * 8 + j
                    for qi, L in enumerate((tri2, same2, indA, indB)):
                        S.op("pe", lambda j=j, qi=qi, L=L, tt=tt, pb=pb: PE.matmul(
                            pb[:, j * 64 + qi * 16:j * 64 + (qi + 1) * 16], lhsT=L[:], rhs=da[:, tt, :], start=True, stop=True),
                            r=["da", "tri2", "same2", "indA", "indB"], w=[kp])
                S.op("dve", lambda pb=pb, half=half: V.tensor_copy(
                    out=cum4[:, half * 8:(half + 1) * 8].rearrange("p t q h -> p (t q h)"), in_=pb[:]),
                    r=[kp], w=["cum4", kp])
            S.op("act", lambda: A.activation(out=eacs[:], in_=cum4[:, :, 0, :], func=AF.Exp), r=["cum4"], w=["eacs"])
            S.op("dve", lambda: V.tensor_tensor(out=eend[:], in0=cum4[:, :, 1, :], in1=cum4[:, :, 0, :], op=ALU.subtract),
                 r=["cum4"], w=["eend"])
            S.op("act", lambda: A.activation(out=eend[:], in_=eend[:], func=AF.Exp), r=["eend"], w=["eend"])
            S.op("act", lambda: A.activation(out=edec[:], in_=cum4[:, :, 2:4, :], func=AF.Exp), r=["cum4"], w=["edec"])
            wx = self.sb(st, "wx", [128, KT, 768], BF16)
            wz = self.sb(st, "wz", [128, KT, 512], BF16)
            cw = self.sb(st, "cw", [128, 6, 4], F32)
            cbi = self.sb(st, "cbi", [128, 6], F32)
            xp1 = self.sb(st, "xp", [128, T + 3], F32)
            xp = [xp1, xp1]
            fTa = [self.sb(st, "fT", [128, T], BF16) for _ in range(4)]
            fT = [fTa[0], fTa[1], fTa[0], fTa[1], fTa[2], fTa[3]]
            fk = [("fT", 0), ("fT", 1), ("fT", 0), ("fT", 1), ("fT", 2), ("fT", 3)]
            cmTA = self.sb(st, "cmTA", [128, T], BF16)
            cmTB = self.sb(st, "cmTB", [128, T], BF16)
            xs = self.sb(st, "xs", [128, NT, 512], BF16)
            xdtt = [self.sb(st, "xdtt", [128, 512], BF16) for _ in range(2)]
            xendt = [self.sb(st, "xendt", [128, 512], BF16) for _ in range(2)]
            bmA = self.sb(st, "bmA", [128, NT, 128], BF16)
            bmB = self.sb(st, "bmB", [128, NT, 128], BF16)
            yTg = self.sb(st, "yTg", [128, 4, T], BF16)
            S32 = self.sb(st, "S32s", [128, 512], F32)
            Sbf = [self.sb(st, "Sbfs", [128, 512], BF16) for _ in range(4)]
            cbs = [self.sb(st, "cbs", [128, 128], BF16) for _ in range(2)]
            Dx = [self.sb(st, "Dx", [16, 8, 128], F32) for _ in range(2)]
            Es = [self.sb(st, "Es", [128, 8, 128], BF16) for _ in range(2)]
            wT = [self.sb(st, "wT", [128, 8, 128], BF16) for _ in range(2)]
            t1 = [self.sb(st, "t1", [128, 512], F32) for _ in range(2)]
            t2 = [self.sb(st, "t2", [128, 512], F32) for _ in range(2)]
            zs = [self.sb(st, "zs", [128, 512], BF16) for _ in range(2)]
            ytm = [self.sb(st, "ytm", [128, 512], BF16) for _ in range(2)]
            ss = [self.sb(st, "ss", [128, 2], F32) for _ in range(2)]
            S.op("pool", lambda: G.memset(xp[0][:, 0:3], 0.0), w=[("xp", 0)])
            for g in range(2):
                S.op("pool", lambda g=g: G.memset(bd[g][:], 1.0), r=[("bd", 0), ("bd", 1)], w=[("bd", 0), ("bd", 1)])
                S.op("pool", lambda g=g: G.affine_select(out=bd[g][:], in_=bd[g][:], pattern=[[1, 8], [0, 128]],
                                                         compare_op=ALU.is_equal, fill=0.0, base=8 * g,
                                                         channel_multiplier=-1), r=[("bd", 0), ("bd", 1)], w=[("bd", 0), ("bd", 1)])
                choff = [g * 512 + i * 128 for i in range(4)] + [1024 + g * 128, 1280 + g * 128]
                S.dma("pool", wx[:, :, 0:512], Wl[:, OFF_XBC + g * 512:OFF_XBC + (g + 1) * 512].rearrange("(kt p) n -> p kt n", p=128), w=["wx"])
                S.dma("pool", wx[:, :, 512:640], Wl[:, OFF_XBC + 1024 + g * 128:OFF_XBC + 1024 + (g + 1) * 128].rearrange("(kt p) n -> p kt n", p=128), w=["wx"])
                S.dma("pool", wx[:, :, 640:768], Wl[:, OFF_XBC + 1280 + g * 128:OFF_XBC + 1280 + (g + 1) * 128].rearrange("(kt p) n -> p kt n", p=128), w=["wx"])
                S.dma("pool", wz[:], Wl[:, OFF_Z + g * 512:OFF_Z + (g + 1) * 512].rearrange("(kt p) n -> p kt n", p=128), w=["wz"])
                for ci in range(6):
                    for j in range(4):
                        S.dma("sp", cw[:, ci, j:j + 1], self.P["ssd_conv_w"][l, j, choff[ci]:choff[ci] + 128].rearrange("(p o) -> p o", o=1), w=["cw"])
                    S.dma("sp", cbi[:, ci:ci + 1], self.P["ssd_conv_b"][l, choff[ci]:choff[ci] + 128].rearrange("(p o) -> p o", o=1), w=["cbi"])
                for ci in range(6):
                    xb = xp[0]
                    kx = ("xp", 0)
                    for tq in range(4):
                        pb, kp = self.psum()
                        for kt in range(KT):
                            S.op("pe", lambda kt=kt, pb=pb, ci=ci, tq=tq: PE.matmul(
                                pb[:], lhsT=wx[:, kt, ci * 128:(ci + 1) * 128], rhs=self.hT[:, kt, tq * 512:(tq + 1) * 512],
                                start=(kt == 0), stop=(kt == KT - 1)), r=["wx"] + hTk[tq * 4:tq * 4 + 4], w=[kp])
                        S.op("act", lambda pb=pb, xb=xb, tq=tq: A.activation(out=xb[:, 3 + tq * 512:3 + (tq + 1) * 512], in_=pb[:],
                                                                            func=AF.Copy), r=[kp], w=[kx, kp])
                    acc = t1[0] if False else None
                    cacc = self.sb(st, "cacc", [128, T], F32) if (g == 0 and ci == 0) else self._cacc
                    self._cacc = cacc
                    S.op("dve", lambda xb=xb, ci=ci, cacc=cacc: V.tensor_scalar(
                        out=cacc[:], in0=xb[:, 3:3 + T], scalar1=cw[:, ci, 3:4], scalar2=cbi[:, ci:ci + 1], op0=ALU.mult,
                        op1=ALU.add), r=[kx, "cw", "cbi"], w=["cacc"])
                    for j in range(3):
                        S.op("dve", lambda xb=xb, ci=ci, j=j, cacc=cacc: V.scalar_tensor_tensor(
                            out=cacc[:], in0=xb[:, j:j + T], scalar=cw[:, ci, j:j + 1], in1=cacc[:], op0=ALU.mult, op1=ALU.add),
                            r=[kx, "cw", "cacc"], w=["cacc"])
                    S.op("act", lambda ci=ci, cacc=cacc: A.activation(out=fT[ci][:], in_=cacc[:], func=AF.Silu),
                         r=["cacc"], w=[fk[ci]])
                    if ci < 5:
                        for t4 in range(4):
                            pb, kp = self.psum()
                            pbv = pb[:].bitcast(BF16)
                            for j in range(4):
                                tt = t4 * 4 + j
                                S.op("pe", lambda j=j, tt=tt, pbv=pbv, ci=ci: PE.transpose(
                                    out=pbv[:, j * 128:(j + 1) * 128], in_=fT[ci][:, tt * 128:(tt + 1) * 128],
                                    identity=self.identbf[:]), r=[fk[ci], "identbf"], w=[kp])
                            src = pbv[:, 0:512].rearrange("p (a b) -> p a b", a=4)
                            if ci < 4:
                                S.op("act", lambda t4=t4, src=src, ci=ci: A.activation(
                                    out=xs[:, t4 * 4:(t4 + 1) * 4, ci * 128:(ci + 1) * 128], in_=src, func=AF.Copy),
                                    r=[kp], w=["xs", kp])
                            else:
                                S.op("dve", lambda t4=t4, src=src: V.tensor_scalar(
                                    out=bmA[:, t4 * 4:(t4 + 1) * 4, :], in0=src, scalar1=mAB[:, 0:1], scalar2=None, op0=ALU.mult),
                                    r=[kp, "mAB"], w=["bmA", kp])
                                S.op("dve", lambda t4=t4, src=src: V.tensor_scalar(
                                    out=bmB[:, t4 * 4:(t4 + 1) * 4, :], in0=src, scalar1=mAB[:, 1:2], scalar2=None, op0=ALU.mult),
                                    r=[kp, "mAB"], w=["bmB", kp])
                bmT, cmT = fT[4], fT[5]
                cv = cmT[:].rearrange("p (t c j) -> p t c j", c=2, j=64)
                cva = cmTA[:].rearrange("p (t c j) -> p t c j", c=2, j=64)
                cvb = cmTB[:].rearrange("p (t c j) -> p t c j", c=2, j=64)
                S.op("pool", lambda: G.tensor_copy(out=cva[:, :, 0, :], in_=cv[:, :, 0, :]), r=[("fT", 3)], w=["cmTA"])
                S.op("pool", lambda: G.memset(cva[:, :, 1, :], 0.0), w=["cmTA"])
                S.op("pool", lambda: G.tensor_copy(out=cvb[:, :, 1, :], in_=cv[:, :, 1, :]), r=[("fT", 3)], w=["cmTB"])
                S.op("pool", lambda: G.memset(cvb[:, :, 0, :], 0.0), w=["cmTB"])
                hs = slice(g * 8, (g + 1) * 8)
                S.op("pool", lambda: G.memset(S32[:], 0.0), w=["S32"])
                S.op("pool", lambda: G.memset(Sbf[0][:], 0.0), w=[("Sbf", 0)])
                for tt in range(NT):
                    i2 = tt % 2
                    tsl = slice(tt * 128, (tt + 1) * 128)
                    cA, cB = 2 * tt, 2 * tt + 1
                    pc, kpc = self.psum()
                    S.op("pe", lambda pc=pc: PE.matmul(pc[:, 0:128], lhsT=bmT[:, tsl], rhs=cmT[:, tsl], start=True, stop=True),
                         r=[("fT", 2), ("fT", 3)], w=[kpc])
                    S.op("act", lambda pc=pc: A.activation(out=cbs[i2][:], in_=pc[:, 0:128], func=AF.Copy),
                         r=[kpc], w=[("cbs", i2), kpc])
                    pq, kpq = self.psum()
                    S.op("pe", lambda pq=pq: PE.matmul(pq[0:16, 0:128], lhsT=da[:, tt, :], rhs=tri2[:], start=True, stop=True),
                         r=["da", "tri2"], w=[kpq])
                    S.op("dve", lambda pq=pq: V.tensor_copy(out=acsTt[i2][:], in_=pq[0:16, 0:128]), r=[kpq], w=[("acsTt", i2), kpq])
                    S.op("dve", lambda pq=pq: V.tensor_scalar(out=nacsTt[i2][:], in0=pq[0:16, 0:128], scalar1=-1.0, scalar2=None,
                                                              op0=ALU.mult), r=[kpq], w=[("nacsTt", i2), kpq])
                    S.op("pool", lambda: G.tensor_tensor(out=Dx[i2][:], in0=bd[g][:],
                                                         in1=acsTt[i2][:].unsqueeze(1).to_broadcast([16, 8, 128]), op=ALU.mult),
                         r=[("bd", g), ("acsTt", i2)], w=[("Dx", i2)])
                    for hh in range(2):
                        pe_, kpe = self.psum()
                        csl = slice(hh * 512, (hh + 1) * 512)
                        S.op("pe", lambda pe_=pe_, csl=csl: PE.matmul(
                            pe_[:], lhsT=self.ones[0:16, :], rhs=Dx[i2][:].rearrange("p h l -> p (h l)")[:, csl],
                            start=True, stop=False), r=["ones", ("Dx", i2)], w=[kpe])
                        S.op("pe", lambda pe_=pe_, csl=csl: PE.matmul(
                            pe_[:], lhsT=nacsTt[i2][:], rhs=bd[g][:].rearrange("p h l -> p (h l)")[:, csl],
                            start=False, stop=False), r=[("nacsTt", i2), ("bd", g)], w=[kpe])
                        S.op("pe", lambda pe_=pe_, csl=csl: PE.matmul(
                            pe_[:], lhsT=self.ident32[:], rhs=negmask[:].rearrange("p h l -> p (h l)")[:, csl],
                            start=False, stop=True), r=["ident32", "negmask"], w=[kpe])
                        S.op("act", lambda pe_=pe_, hh=hh: A.activation(
                            out=Es[i2][:, hh * 4:(hh + 1) * 4, :], in_=pe_[:].rearrange("p (h l) -> p h l", h=4), func=AF.Exp),
                            r=[kpe], w=[("Es", i2), kpe])
                    S.op("dve", lambda: V.tensor_tensor(out=wT[i2][:], in0=Es[i2][:],
                                                        in1=cbs[i2][:].unsqueeze(1).to_broadcast([128, 8, 128]), op=ALU.mult),
                         r=[("Es", i2), ("cbs", i2)], w=[("wT", i2)])
                    pr, kpr = self.psum()
                    pr2, kpr2 = self.psum()
                    S.op("pool", lambda: G.tensor_tensor(out=xdtt[i2][:].rearrange("p (h c) -> p h c", c=64),
                                                         in0=xs[:, tt, :].rearrange("p (h c) -> p h c", c=64),
                                                         in1=dt[:, tt, hs].unsqueeze(2).to_broadcast([128, 8, 64]), op=ALU.mult),
                         r=["xs", "dt"], w=[("xdtt", i2)])
                    S.op("pool", lambda: G.tensor_tensor(out=xendt[i2][:].rearrange("p (h c) -> p h c", c=64),
                                                         in0=xdtt[i2][:].rearrange("p (h c) -> p h c", c=64),
                                                         in1=eend[:, tt, hs].unsqueeze(2).to_broadcast([128, 8, 64]), op=ALU.mult),
                         r=[("xdtt", i2), "eend"], w=[("xendt", i2)])
                    S.op("pe", lambda pr=pr: PE.matmul(pr[:], lhsT=bmA[:, tt, :], rhs=xendt[i2][:], start=True, stop=True),
                         r=["bmA", ("xendt", i2)], w=[kpr])
                    S.op("pe", lambda pr2=pr2: PE.matmul(pr2[:], lhsT=bmB[:, tt, :], rhs=xendt[i2][:], start=True, stop=True),
                         r=["bmB", ("xendt", i2)], w=[kpr2])
                    for (ci_, prx, kprx, cc) in ((cA, pr, kpr, 0), (cB, pr2, kpr2, 1)):
                        S.op("dve", lambda cc=cc: V.tensor_tensor(
                            out=S32[:].rearrange("p (h c) -> p h c", c=64), in0=S32[:].rearrange("p (h c) -> p h c", c=64),
                            in1=edec[:, tt, cc, hs].unsqueeze(2).to_broadcast([128, 8, 64]), op=ALU.mult),
                            r=["S32", "edec"], w=["S32"])
                        S.op("dve", lambda prx=prx: V.tensor_tensor(out=S32[:], in0=prx[:], in1=S32[:], op=ALU.add),
                             r=[kprx, "S32"], w=["S32", kprx])
                        S.op("pool", lambda ci_=ci_: G.tensor_copy(out=Sbf[(ci_ + 1) % 4][:], in_=S32[:]),
                             r=["S32"], w=[("Sbf", (ci_ + 1) % 4)])
                    py, kpy = self.psum()
                    for hh in range(8):
                        S.op("pe", lambda hh=hh, py=py: PE.matmul(py[:, hh * 64:(hh + 1) * 64], lhsT=wT[i2][:, hh, :],
                                                                  rhs=xdtt[i2][:, hh * 64:(hh + 1) * 64], start=True, stop=True),
                             r=[("wT", i2), ("xdtt", i2)], w=[kpy])
                    po, kpo = self.psum()
                    S.op("pe", lambda po=po: PE.matmul(po[:], lhsT=cmTA[:, tsl], rhs=Sbf[cA % 4][:], start=True, stop=False),
                         r=["cmTA", ("Sbf", cA % 4)], w=[kpo])
                    S.op("pe", lambda po=po: PE.matmul(po[:], lhsT=cmTB[:, tsl], rhs=Sbf[cB % 4][:], start=False, stop=True),
                         r=["cmTB", ("Sbf", cB % 4)], w=[kpo])
                    pz, kpz = self.psum()
                    for kt in range(KT):
                        S.op("pe", lambda kt=kt, pz=pz: PE.matmul(pz[:], lhsT=self.hT[:, kt, tsl], rhs=wz[:, kt, :],
                                                                  start=(kt == 0), stop=(kt == KT - 1)), r=["wz", ("hT", tt)], w=[kpz])
                    S.op("act", lambda pz=pz: A.activation(out=zs[i2][:], in_=pz[:], func=AF.Silu), r=[kpz], w=[("zs", i2), kpz])
                    S.op("dve", lambda po=po: V.tensor_tensor(
                        out=t1[i2][:].rearrange("p (h c) -> p h c", c=64), in0=po[:].rearrange("p (h c) -> p h c", c=64),
                        in1=eacs[:, tt, hs].unsqueeze(2).to_broadcast([128, 8, 64]), op=ALU.mult),
                        r=[kpo, "eacs"], w=[("t1", i2), kpo])
                    S.op("dve", lambda py=py: V.tensor_tensor(out=t1[i2][:], in0=py[:], in1=t1[i2][:], op=ALU.add),
                         r=[kpy, ("t1", i2)], w=[("t1", i2), kpy])
                    S.op("pool", lambda: G.tensor_tensor(
                        out=t2[i2][:].rearrange("p (h c) -> p h c", c=64), in0=xs[:, tt, :].rearrange("p (h c) -> p h c", c=64),
                        in1=dsk[:, hs].unsqueeze(2).to_broadcast([128, 8, 64]), op=ALU.mult), r=["xs", "dsk"], w=[("t2", i2)])
                    S.op("pool", lambda: G.tensor_tensor(out=t2[i2][:], in0=t2[i2][:], in1=t1[i2][:], op=ALU.add),
                         r=[("t2", i2), ("t1", i2)], w=[("t2", i2)])
                    S.op("pool", lambda: G.tensor_tensor(out=t2[i2][:], in0=t2[i2][:], in1=zs[i2][:], op=ALU.mult),
                         r=[("t2", i2), ("zs", i2)], w=[("t2", i2)])
                    S.op("act", lambda: A.activation(out=t1[i2][:], in_=t2[i2][:], func=AF.Square, accum_out=ss[i2][:, 0:1]),
                         r=[("t2", i2)], w=[("t1", i2), ("ss", i2)])
                    S.op("act", lambda: A.activation(out=ss[i2][:, 1:2], in_=ss[i2][:, 0:1], func=AF.Sqrt, bias=self.epsc[:, 1:2],
                                                     scale=1.0 / 512.0), r=[("ss", i2), "epsc"], w=[("ss", i2)])
                    S.op("dve", lambda: V.reciprocal(out=ss[i2][:, 1:2], in_=ss[i2][:, 1:2]), r=[("ss", i2)], w=[("ss", i2)])
                    S.op("dve", lambda: V.scalar_tensor_tensor(out=ytm[i2][:], in0=t2[i2][:], scalar=ss[i2][:, 1:2],
                                                               in1=nwbc[:, g * 512:(g + 1) * 512], op0=ALU.mult, op1=ALU.mult),
                         r=[("t2", i2), ("ss", i2), "nwbc"], w=[("ytm", i2)])
                    pt, kpt = self.psum()
                    ptv = pt[:].bitcast(BF16)
                    for i in range(4):
                        S.op("pe", lambda i=i, ptv=ptv: PE.transpose(out=ptv[:, i * 128:(i + 1) * 128],
                                                                     in_=ytm[i2][:, i * 128:(i + 1) * 128], identity=self.identbf[:]),
                             r=[("ytm", i2), "identbf"], w=[kpt])
                    S.op("act", lambda ptv=ptv: A.activation(out=yTg[:, :, tsl], in_=ptv[:, 0:512].rearrange("p (a b) -> p a b", a=4),
                                                             func=AF.Copy), r=[kpt], w=["yTg", kpt])
                for i in range(4):
                    S.dma("sp", ydst[g * 512 + i * 128:g * 512 + (i + 1) * 128, :], yTg[:, i, :], r=["yTg"], w=[("yT0", g * 4 + i)])
            S.barrier()


    def rwkv(self, l):
        nc, S = self.nc, self.S
        V, A, G, PE = nc.vector, nc.scalar, nc.gpsimd, nc.tensor
        Wl = self.P["w_in"][l]
        ydst = self.yT_dram[1]
        hTk = [("hT", t) for t in range(NT)]
        P_ = self.P

        def mm(out, lhsT, rhs, start, stop, r, w):
            S.op("pe", lambda: PE.matmul(out, lhsT=lhsT, rhs=rhs, start=start, stop=stop), r=r, w=w)

        with contextlib.ExitStack() as st:
            mask4 = self.sb(st, "mask4", [128, 4, 128], F32)
            maskL = self.sb(st, "maskL", [128, 2, 128], F32)
            bdm = self.sb(st, "bdm", [128, 128], F32)
            mEO = self.sb(st, "mEO", [128, 2], F32)
            hsel = self.sb(st, "hsel", [128, 2], F32)
            rm = self.sb(st, "rm128", [128, T], BF16)
            c05 = self.sb(st, "c05", [128, 1], F32)
            for j in range(4):
                S.op("pool", lambda j=j: G.affine_select(out=mask4[:, j, :], in_=self.ones[:], pattern=[[1, 128]],
                                                         compare_op=(ALU.is_gt if j % 2 == 0 else ALU.is_ge), fill=0.0,
                                                         base=0, channel_multiplier=-1), r=["ones"], w=["mask4"])
            for j in range(2):
                S.op("pool", lambda j=j: G.affine_select(out=maskL[:, j, :], in_=self.ones[:], pattern=[[-1, 128]],
                                                         compare_op=ALU.is_gt, fill=0.0, base=0, channel_multiplier=1),
                     r=["ones"], w=["maskL"])
            S.op("pool", lambda: G.memset(bdm[:], 0.0), w=["bdm"])
            S.op("pool", lambda: G.memset(bdm[0:64, 0:64], 1.0), w=["bdm"])
            S.op("pool", lambda: G.memset(bdm[64:128, 64:128], 1.0), w=["bdm"])
            for (t_, nm) in ((mEO, "mEO"), (hsel, "hsel")):
                S.op("pool", lambda t_=t_: G.memset(t_[0:64, 0:1], 1.0), w=[nm])
                S.op("pool", lambda t_=t_: G.memset(t_[64:128, 0:1], 0.0), w=[nm])
                S.op("pool", lambda t_=t_: G.memset(t_[0:64, 1:2], 0.0), w=[nm])
                S.op("pool", lambda t_=t_: G.memset(t_[64:128, 1:2], 1.0), w=[nm])
            S.op("pool", lambda: G.memset(rm[:], 1.0), w=["rm"])
            S.op("pool", lambda: G.memset(rm[:].rearrange("p (c j) -> p c j", j=128)[:, :, 0:1], 0.0), w=["rm"])
            S.op("pool", lambda: G.memset(c05[:], -0.5), w=["c05"])
            pc = {}
            for nm, src in (("mu_r", P_["rwkv_mu"][l, 0:1024]), ("mu_k", P_["rwkv_mu"][l, 1024:2048]),
                            ("mu_v", P_["rwkv_mu"][l, 2048:3072]), ("w0", P_["rwkv_w0"][l]), ("a0", P_["rwkv_a0"][l]),
                            ("k_k", P_["rwkv_k_k"][l]), ("k_a", P_["rwkv_k_a"][l]),
                            ("r_k", P_["rwkv_r_k"][l].rearrange("h k -> (h k)"))):
                t_ = self.sb(st, "pc_" + nm, [128, 8, 1], F32)
                S.dma("sp", t_[:], src.rearrange("(q p o) -> p q o", p=128, o=1), w=["pc_" + nm])
                pc[nm] = t_
            mul = self.sb(st, "mul", [128, 3], F32)
            S.dma("sp", mul[0:64, 0:1], P_["rwkv_mu"][l, 3072:3136].rearrange("(p o) -> p o", o=1), w=["mul"])
            S.dma("sp", mul[0:64, 1:2], P_["rwkv_mu"][l, 3136:3200].rearrange("(p o) -> p o", o=1), w=["mul"])
            S.dma("sp", mul[:, 2:3], P_["rwkv_mu"][l, 3200:3328].rearrange("(p o) -> p o", o=1), w=["mul"])
            nw0 = self.sb(st, "nw0", [128, 8, 1], F32)
            omka = self.sb(st, "omka", [128, 8, 1], F32)
            S.op("dve", lambda: V.tensor_scalar(out=nw0[:], in0=pc["w0"][:], scalar1=-1.0, scalar2=None, op0=ALU.mult),
                 r=["pc_w0"], w=["nw0"])
            S.op("dve", lambda: V.tensor_scalar(out=omka[:], in0=pc["k_a"][:], scalar1=-1.0, scalar2=1.0, op0=ALU.mult,
                                                op1=ALU.add), r=["pc_k_a"], w=["omka"])
            wl = self.sb(st, "wl", [128, KT, 256], BF16)
            w2 = self.sb(st, "w2", [64, D], BF16)
            a2 = self.sb(st, "a2", [64, D], BF16)
            g2 = self.sb(st, "g2", [128, D], BF16)
            S.dma("pool", wl[:], Wl[:, OFF_RWKV + 3072:OFF_RWKV + 3328].rearrange("(kt p) n -> p kt n", p=128), w=["wl"])
            S.dma("pool", w2[:], P_["rwkv_w2"][l], w=["w2"])
            S.dma("pool", a2[:], P_["rwkv_a2"][l], w=["a2"])
            S.dma("pool", g2[:], P_["rwkv_g2"][l], w=["g2"])
            txw = self.sb(st, "txw", [64, T], BF16)
            xaT = self.sb(st, "xaT", [64, T], BF16)
            sgT = self.sb(st, "sgT", [128, T], BF16)
            xraw = self.sb(st, "xraw", [128, T + 1], F32)
            F = [self.sb(st, "F%d" % i, [128, T], F32) for i in range(5)]
            S.op("pool", lambda: G.memset(xraw[:, 0:1], 0.0), w=["xraw"])

            def proj_shift(wt, c0, m, mucol, dst, dkey, func=None, rows=128):
                for tq in range(4):
                    pb, kp = self.psum()
                    for kt in range(KT):
                        mm(pb[0:rows, :], wt[:, kt, c0:c0 + m], self.hT[:, kt, tq * 512:(tq + 1) * 512], kt == 0, kt == KT - 1,
                           [wt_key] + hTk[tq * 4:tq * 4 + 4], [kp])
                    S.op("act", lambda pb=pb, tq=tq: A.activation(out=xraw[0:rows, 1 + tq * 512:1 + (tq + 1) * 512],
                                                                  in_=pb[0:rows, :], func=AF.Copy), r=[kp], w=["xraw", kp])
                S.op("dve", lambda: V.tensor_tensor(out=F[4][0:rows, :], in0=xraw[0:rows, 0:T], in1=xraw[0:rows, 1:T + 1],
                                                    op=ALU.subtract), r=["xraw"], w=["F4"])
                if func is None:
                    S.op("dve", lambda: V.scalar_tensor_tensor(out=dst[0:rows, :], in0=F[4][0:rows, :], scalar=mucol,
                                                               in1=xraw[0:rows, 1:T + 1], op0=ALU.mult, op1=ALU.add),
                         r=["F4", "xraw", "mul"] + list(pc_keys), w=[dkey])
                else:
                    S.op("dve", lambda: V.scalar_tensor_tensor(out=F[4][0:rows, :], in0=F[4][0:rows, :], scalar=mucol,
                                                               in1=xraw[0:rows, 1:T + 1], op0=ALU.mult, op1=ALU.add),
                         r=["F4", "xraw", "mul"] + list(pc_keys), w=["F4"])
                    S.op("act", lambda: A.activation(out=dst[0:rows, :], in_=F[4][0:rows, :], func=func), r=["F4"], w=[dkey])

            pc_keys = ["pc_mu_r", "pc_mu_k", "pc_mu_v"]
            wt_key = "wl"
            proj_shift(wl, 0, 64, mul[0:64, 0:1], txw, "txw", AF.Tanh, rows=64)
            proj_shift(wl, 64, 64, mul[0:64, 1:2], xaT, "xaT", AF.Copy, rows=64)
            proj_shift(wl, 128, 128, mul[:, 2:3], sgT, "sgT", AF.Sigmoid, rows=128)
            wrkv1 = self.sb(st, "wrkv", [128, KT, 384], BF16)
            wrkv = [wrkv1, wrkv1]
            Pp = self.sb(st, "Pp", [128, T], BF16)
            Pc = self.sb(st, "Pc", [128, T], BF16)
            Pend = self.sb(st, "Pend", [128, NT], F32)
            iP = self.sb(st, "iP", [128, T], BF16)
            bT = self.sb(st, "bT", [128, T], BF16)
            kT = self.sb(st, "kT", [128, T], BF16)
            vT = self.sb(st, "vT", [128, T], BF16)
            AR = [self.sb(st, "AR", [128, NT, 2, 128], BF16) for _ in range(2)]
            Vtm = self.sb(st, "Vtm", [128, NT, 128], BF16)
            Btm = self.sb(st, "Btm", [128, NT, 128], BF16)
            aT = Vtm[:].rearrange("p t j -> p (t j)")
            rT = Btm[:].rearrange("p t j -> p (t j)")
            Ktm = self.sb(st, "Ktm", [128, NT, 128], BF16)
            bon = self.sb(st, "bon", [128, NT, 2], F32)
            st32 = self.sb(st, "st32", [128, 2 * NT, 4], F32)
            lnw = self.sb(st, "lnwb", [128, 128], F32)
            lnb = self.sb(st, "lnbb", [128, 128], F32)
            Z32 = self.sb(st, "Z32", [128, 128], F32)
            Zt = self.sb(st, "Zt", [128, 128], F32)
            Zbf = [self.sb(st, "Zbf", [128, 128], BF16) for _ in range(2)]
            Wsb = [self.sb(st, "Wsb", [128, 128], BF16) for _ in range(2)]
            Usb = [self.sb(st, "Usb", [128, 128], BF16) for _ in range(2)]
            NS = 8
            abrb = [self.sb(st, "abrb", [128, 4, 128], BF16) for _ in range(NS)]
            akrk = [self.sb(st, "akrk", [128, 4, 128], BF16) for _ in range(NS)]
            L0 = [self.sb(st, "L0", [128, 2, 128], BF16) for _ in range(4)]
            XX = [[self.sb(st, "XX", [128, 4, 128], BF16) for _ in range(4)] for _ in range(2)]
            Tt = [self.sb(st, "Tt", [128, 2, 128], BF16) for _ in range(NS)]

            def load_w(p):
                b = 0
                for j in range(3):
                    c0 = OFF_RWKV + j * 1024 + p * 128
                    S.dma("pool", wrkv[b][:, :, j * 128:(j + 1) * 128], Wl[:, c0:c0 + 128].rearrange("(kt p) n -> p kt n", p=128),
                          w=[("wrkv", b)])

            def projA(p):
                nonlocal wt_key
                load_w(p)
                wt_key = ("wrkv", 0)
                proj_shift(wrkv[0], 128, 128, pc["mu_k"][:, p, :], F[0], "F0")
                proj_shift(wrkv[0], 0, 128, pc["mu_r"][:, p, :], F[1], "F1")
                proj_shift(wrkv[0], 256, 128, pc["mu_v"][:, p, :], vT, "vT", AF.Copy)

            projA(0)
            for p in range(8):
                b = 0
                fs_ = slice(p * 128, (p + 1) * 128)
                S.dma("sp", lnw[:], P_["rwkv_ln_w"][l, fs_].partition_broadcast(128), w=["lnw"])
                S.dma("sp", lnb[:], P_["rwkv_ln_b"][l, fs_].partition_broadcast(128), w=["lnb"])
                for tq in range(4):
                    pb, kp = self.psum()
                    qs_ = slice(tq * 512, (tq + 1) * 512)
                    mm(pb[:], w2[:, fs_], txw[:, qs_], True, True, ["w2", "txw"], [kp])
                    S.op("act", lambda pb=pb: A.activation(out=F[2][:, qs_], in_=pb[:], func=AF.Exp, bias=nw0[:, p, :], scale=-1.0),
                         r=[kp, "nw0"], w=["F2", kp])
                S.op("act", lambda: A.activation(out=F[2][:], in_=F[2][:], func=AF.Ln, bias=self.epsc[:, 3:4], scale=1.0),
                     r=["F2", "epsc"], w=["F2"])
                S.op("act", lambda: A.activation(out=F[2][:], in_=F[2][:], func=AF.Exp, bias=c05[:, 0:1], scale=-1.0),
                     r=["F2", "c05"], w=["F2"])
                S.op("dve", lambda: V.tensor_tensor_scan(out=F[3][:], data0=rm[:], data1=F[2][:], initial=0.0, op0=ALU.mult,
                                                         op1=ALU.add), r=["rm", "F2"], w=["F3"])
                S.op("dve", lambda: V.tensor_tensor(out=F[2][:], in0=F[3][:], in1=F[2][:], op=ALU.subtract),
                     r=["F2", "F3"], w=["F2"])
                S.op("act", lambda: A.activation(out=Pp[:], in_=F[2][:], func=AF.Exp, scale=-1.0), r=["F2"], w=["Pp"])
                S.op("act", lambda: A.activation(out=Pc[:], in_=F[3][:], func=AF.Exp, scale=-1.0), r=["F3"], w=["Pc"])
                S.op("act", lambda: A.activation(out=iP[:], in_=F[3][:], func=AF.Exp), r=["F3"], w=["iP"])
                S.op("act", lambda: A.activation(out=Pend[:], in_=F[3][:].rearrange("p (t j) -> p t j", j=128)[:, :, 127],
                                                 func=AF.Exp, scale=-1.0), r=["F3"], w=["Pend"])
                for tq in range(4):
                    pb, kp = self.psum()
                    qs_ = slice(tq * 512, (tq + 1) * 512)
                    mm(pb[:], a2[:, fs_], xaT[:, qs_], True, True, ["a2", "xaT"], [kp])
                    S.op("act", lambda pb=pb: A.activation(out=F[2][:, qs_], in_=pb[:], func=AF.Sigmoid, bias=pc["a0"][:, p, :],
                                                           scale=1.0), r=[kp, "pc_a0"], w=["F2", kp])
                S.op("dve", lambda: V.tensor_scalar(out=F[3][:], in0=F[0][:], scalar1=pc["k_k"][:, p, :], scalar2=None,
                                                    op0=ALU.mult), r=["F0", "pc_k_k"], w=["F3"])
                S.op("act", lambda: A.activation(out=F[4][:], in_=F[3][:], func=AF.Square), r=["F3"], w=["F4"])
                for tq in range(4):
                    pb, kp = self.psum()
                    qs_ = slice(tq * 512, (tq + 1) * 512)
                    mm(pb[:], bdm[:], F[4][:, qs_], True, True, ["bdm", "F4"], [kp])
                    S.op("act", lambda pb=pb: A.activation(out=F[4][:, qs_], in_=pb[:], func=AF.Sqrt), r=[kp], w=["F4", kp])
                S.op("dve", lambda: V.tensor_scalar(out=F[4][:], in0=F[4][:], scalar1=1e-12, scalar2=None, op0=ALU.max),
                     r=["F4"], w=["F4"])
                S.op("dve", lambda: V.reciprocal(out=F[4][:], in_=F[4][:]), r=["F4"], w=["F4"])
                S.op("dve", lambda: V.tensor_tensor(out=F[3][:], in0=F[3][:], in1=F[4][:], op=ALU.mult), r=["F3", "F4"], w=["F3"])
                S.op("dve", lambda: V.scalar_tensor_tensor(out=aT, in0=F[3][:], scalar=-1.0, in1=Pp[:], op0=ALU.mult,
                                                           op1=ALU.mult), r=["F3", "Pp"], w=["Vtm"])
                S.op("pool", lambda: G.tensor_tensor(out=F[4][:], in0=F[3][:], in1=F[2][:], op=ALU.mult), r=["F3", "F2"], w=["F4"])
                S.op("pool", lambda: G.tensor_tensor(out=bT[:], in0=F[4][:], in1=iP[:], op=ALU.mult), r=["F4", "iP"], w=["bT"])
                S.op("dve", lambda: V.tensor_scalar(out=F[2][:], in0=F[2][:], scalar1=pc["k_a"][:, p, :], scalar2=omka[:, p, :],
                                                    op0=ALU.mult, op1=ALU.add), r=["F2", "pc_k_a", "omka"], w=["F2"])
                S.op("dve", lambda: V.tensor_tensor(out=F[0][:], in0=F[0][:], in1=F[2][:], op=ALU.mult), r=["F0", "F2"], w=["F0"])
                S.op("pool", lambda: G.tensor_tensor(out=kT[:], in0=F[0][:], in1=iP[:], op=ALU.mult), r=["F0", "iP"], w=["kT"])
                S.op("dve", lambda: V.tensor_tensor(out=rT, in0=F[1][:], in1=Pc[:], op=ALU.mult), r=["F1", "Pc"], w=["Btm"])
                S.op("dve", lambda: V.scalar_tensor_tensor(out=F[1][:], in0=F[1][:], scalar=pc["r_k"][:, p, :], in1=F[0][:],
                                                           op0=ALU.mult, op1=ALU.mult), r=["F1", "F0", "pc_r_k"], w=["F1"])
                for h in range(2):
                    S.op("act", lambda h=h: A.activation(out=AR[h][:, :, 0, :], in_=Vtm[:], func=AF.Identity,
                                                         scale=mEO[:, h:h + 1]), r=["Vtm", "mEO"], w=[("AR", h)])
                    S.op("dve", lambda h=h: V.tensor_scalar(out=AR[h][:, :, 1, :], in0=Btm[:],
                                                            scalar1=mEO[:, h:h + 1], scalar2=None, op0=ALU.mult),
                         r=["Btm", "mEO"], w=[("AR", h)])
                for (src, skey, dst, dkey) in ((vT, "vT", Vtm, "Vtm"), (bT, "bT", Btm, "Btm"), (kT, "kT", Ktm, "Ktm")):
                    for t4 in range(4):
                        pb, kp = self.psum()
                        pbv = pb[:].bitcast(BF16)
                        for j in range(4):
                            tt = t4 * 4 + j
                            S.op("pe", lambda j=j, tt=tt, pbv=pbv, src=src: PE.transpose(
                                out=pbv[:, j * 128:(j + 1) * 128], in_=src[:, tt * 128:(tt + 1) * 128], identity=self.identbf[:]),
                                r=[skey, "identbf"], w=[kp])
                        S.op("act", lambda t4=t4, pbv=pbv, dst=dst: A.activation(
                            out=dst[:, t4 * 4:(t4 + 1) * 4, :], in_=pbv[:, 0:512].rearrange("p (a b) -> p a b", a=4), func=AF.Copy),
                            r=[kp], w=[dkey, kp])
                pb, kp = self.psum()
                for tt in range(NT):
                    mm(pb[:, tt * 2:(tt + 1) * 2], F[1][:, tt * 128:(tt + 1) * 128], hsel[:], True, True, ["F1", "hsel"], [kp])
                S.op("dve", lambda pb=pb: V.tensor_copy(out=bon[:].rearrange("p t h -> p (t h)"), in_=pb[:, 0:2 * NT]),
                     r=[kp], w=["bon", kp])
                ytm = F[2][:].rearrange("p (t j) -> p t j", j=128)
                ysq = F[3][:].rearrange("p (t j) -> p t j", j=128)

                def inv_group(gi, pending=()):
                    pending = list(pending)
                    tiles = range(gi * 4, gi * 4 + 4)
                    for tt in tiles:
                        sl = tt % NS
                        tsl = slice(tt * 128, (tt + 1) * 128)
                        p1, k1 = self.psum()
                        p2, k2 = self.psum()
                        p3, k3 = self.psum()
                        for h in range(2):
                            rhs = AR[h][:, tt].rearrange("p a j -> p (a j)")
                            mm(p1[:, h * 256:(h + 1) * 256], bT[:, tsl], rhs, True, True, ["bT", ("AR", h)], [k1])
                            mm(p2[:, h * 256:(h + 1) * 256], kT[:, tsl], rhs, True, True, ["kT", ("AR", h)], [k2])
                            mm(p3[:, h * 128:(h + 1) * 128], AR[h][:, tt, 0, :], bT[:, tsl], True, True, [("AR", h), "bT"], [k3])
                        S.op("dve", lambda p1=p1, sl=sl: V.tensor_tensor(out=abrb[sl][:].rearrange("p a j -> p (a j)"), in0=p1[:],
                                                                        in1=mask4[:].rearrange("p a j -> p (a j)"), op=ALU.mult),
                             r=[k1, "mask4"], w=[("abrb", sl), k1])
                        S.op("dve", lambda p2=p2, sl=sl: V.tensor_tensor(out=akrk[sl][:].rearrange("p a j -> p (a j)"), in0=p2[:],
                                                                        in1=mask4[:].rearrange("p a j -> p (a j)"), op=ALU.mult),
                             r=[k2, "mask4"], w=[("akrk", sl), k2])
                        S.op("dve", lambda p3=p3, sl=sl: V.tensor_tensor(out=L0[sl % 4][:].rearrange("p a j -> p (a j)"), in0=p3[:, 0:256],
                                                                        in1=maskL[:].rearrange("p a j -> p (a j)"), op=ALU.mult),
                             r=[k3, "maskL"], w=[("L0", sl % 4), k3])
                        for h in range(2):
                            S.op("pool", lambda h=h, sl=sl: G.tensor_tensor(out=Tt[sl][:, h, :], in0=abrb[sl][:, 2 * h, :],
                                                                            in1=self.identbf[:], op=ALU.add),
                                 r=[("abrb", sl), "identbf"], w=[("Tt", sl)])

                    def Xk(k, sl, h):
                        return (L0[sl % 4][:, h, :], ("L0", sl % 4)) if k == 0 else (XX[k % 2][sl % 4][:, 2 * h, :], ("XX", k % 2, sl % 4))

                    def Xtk(k, sl, h):
                        return (abrb[sl][:, 2 * h, :], ("abrb", sl)) if k == 0 else (XX[k % 2][sl % 4][:, 2 * h + 1, :], ("XX", k % 2, sl % 4))

                    for k in range(7):
                        sqb = {}
                        if k <= 5:
                            for tt in tiles:
                                sl = tt % NS
                                pb, kp = self.psum()
                                sqb[tt] = (pb, kp)
                                for h in range(2):
                                    x, kx = Xk(k, sl, h)
                                    xt, kxt = Xtk(k, sl, h)
                                    mm(pb[:, (2 * h) * 128:(2 * h + 1) * 128], xt, x, True, True, [kx, kxt], [kp])
                                    if k < 5:
                                        mm(pb[:, (2 * h + 1) * 128:(2 * h + 2) * 128], x, xt, True, True, [kx, kxt], [kp])
                        ttb = []
                        if k >= 1:
                            for t2 in range(2):
                                pb, kp = self.psum()
                                ttb.append((pb, kp))
                                for j in range(2):
                                    sl = (gi * 4 + t2 * 2 + j) % NS
                                    for h in range(2):
                                        x1, kx1 = Xk(k, sl, h)
                                        mm(pb[:, (j * 2 + h) * 128:(j * 2 + h + 1) * 128], x1, Tt[sl][:, h, :], True, True,
                                           [kx1, ("Tt", sl)], [kp])
                        if k <= 5:
                            for tt in tiles:
                                sl = tt % NS
                                pb, kp = sqb[tt]
                                kn = ("XX", (k + 1) % 2, sl % 4)
                                if k < 5:
                                    S.op("act", lambda pb=pb, sl=sl, k=k: A.activation(
                                        out=XX[(k + 1) % 2][sl % 4][:].rearrange("p a j -> p (a j)"), in_=pb[:], func=AF.Copy),
                                        r=[kp], w=[kn, kp])
                                else:
                                    S.op("act", lambda pb=pb, sl=sl, k=k: A.activation(
                                        out=XX[(k + 1) % 2][sl % 4][:, 0:4:2, :],
                                        in_=pb[:].rearrange("p (a j) -> p a j", j=128)[:, 0:4:2, :], func=AF.Copy), r=[kp], w=[kn, kp])
                        for t2, (pb, kp) in enumerate(ttb):
                            for j in range(2):
                                sl = (gi * 4 + t2 * 2 + j) % NS
                                S.op("dve", lambda pb=pb, sl=sl, j=j: V.tensor_tensor(
                                    out=Tt[sl][:].rearrange("p a j -> p (a j)"), in0=pb[:, j * 256:(j + 1) * 256],
                                    in1=Tt[sl][:].rearrange("p a j -> p (a j)"), op=ALU.add), r=[kp, ("Tt", sl)], w=[("Tt", sl), kp])
                        if pending and k >= 1:
                            chain_tile(pending.pop(0))
                    while pending:
                        chain_tile(pending.pop(0))

                def chain_tile(tt):
                    if True:
                        sl = tt % NS
                        i2 = tt % 2
                        tsl = slice(tt * 128, (tt + 1) * 128)
                        zb, kz = Zbf[i2], ("Zbf", i2)
                        pw, kpw = self.psum()
                        mm(pw[:, 0:128], AR[0][:, tt, 0, :], zb[:], True, False, [("AR", 0), kz], [kpw])
                        mm(pw[:, 0:128], AR[1][:, tt, 0, :], zb[:], False, False, [("AR", 1), kz], [kpw])
                        for h in range(2):
                            mm(pw[:, h * 64:(h + 1) * 64], akrk[sl][:, 2 * h, :], Vtm[:, tt, h * 64:(h + 1) * 64], False, h == 1,
                               [("akrk", sl), "Vtm"], [kpw])
                        S.op("act", lambda pw=pw: A.activation(out=Wsb[i2][:], in_=pw[:, 0:128], func=AF.Copy),
                             r=[kpw], w=[("Wsb", i2), kpw])
                        pu, kpu = self.psum()
                        for h in range(2):
                            mm(pu[:, h * 64:(h + 1) * 64], Tt[sl][:, h, :], Wsb[i2][:, h * 64:(h + 1) * 64], True, True,
                               [("Tt", sl), ("Wsb", i2)], [kpu])
                        S.op("act", lambda pu=pu: A.activation(out=Usb[i2][:], in_=pu[:, 0:128], func=AF.Copy),
                             r=[kpu], w=[("Usb", i2), kpu])
                        py, kpy = self.psum()
                        mm(py[:, 0:128], AR[0][:, tt, 1, :], zb[:], True, False, [("AR", 0), kz], [kpy])
                        mm(py[:, 0:128], AR[1][:, tt, 1, :], zb[:], False, False, [("AR", 1), kz], [kpy])
                        for h in range(2):
                            mm(py[:, h * 64:(h + 1) * 64], abrb[sl][:, 2 * h + 1, :], Usb[i2][:, h * 64:(h + 1) * 64], False, False,
                               [("abrb", sl), ("Usb", i2)], [kpy])
                            mm(py[:, h * 64:(h + 1) * 64], akrk[sl][:, 2 * h + 1, :], Vtm[:, tt, h * 64:(h + 1) * 64], False, h == 1,
                               [("akrk", sl), "Vtm"], [kpy])
                        S.op("act", lambda py=py: A.activation(out=ytm[:, tt, :], in_=py[:, 0:128], func=AF.Copy),
                             r=[kpy], w=["F2", kpy])
                        pz, kpz = self.psum()
                        mm(pz[:, 0:128], Btm[:, tt, :], Usb[i2][:], True, False, ["Btm", ("Usb", i2)], [kpz])
                        mm(pz[:, 0:128], Ktm[:, tt, :], Vtm[:, tt, :], False, True, ["Ktm", "Vtm"], [kpz])
                        S.op("dve", lambda pz=pz: V.tensor_tensor(out=Zt[:], in0=pz[:, 0:128], in1=bdm[:], op=ALU.mult),
                             r=[kpz, "bdm"], w=["Zt", kpz])
                        S.op("dve", lambda: V.tensor_tensor(out=Zt[:], in0=Zt[:], in1=Z32[:], op=ALU.add), r=["Zt", "Z32"], w=["Zt"])
                        S.op("dve", lambda: V.tensor_scalar(out=Z32[:], in0=Zt[:], scalar1=Pend[:, tt:tt + 1],
                                                            scalar2=None, op0=ALU.mult), r=["Zt", "Pend"], w=["Z32"])
                        S.op("pool", lambda: G.tensor_copy(out=Zbf[(tt + 1) % 2][:], in_=Z32[:]), r=["Z32"], w=[("Zbf", (tt + 1) % 2)])

                S.op("pool", lambda: G.memset(Z32[:], 0.0), w=["Z32"])
                S.op("pool", lambda: G.memset(Zbf[0][:], 0.0), w=[("Zbf", 0)])
                inv_group(0)
                for gi in range(1, 4):
                    inv_group(gi, pending=range((gi - 1) * 4, gi * 4))
                for tt in range(12, 16):
                    chain_tile(tt)
                y16, yTp = Btm, kT
                def output_phase(p=p, fs_=fs_, ytm=ytm, ysq=ysq):
                    y3 = ytm.rearrange("p t (h c) -> p (t h) c", c=64)
                    q3 = ysq.rearrange("p t (h c) -> p (t h) c", c=64)
                    S.op("act", lambda: A.activation(out=F[3][:], in_=F[2][:], func=AF.Square), r=["F2"], w=["F3"])
                    S.op("dve", lambda: V.tensor_reduce(out=st32[:, :, 0], in_=y3, axis=AX.X, op=ALU.add), r=["F2"], w=["st32"])
                    S.op("dve", lambda: V.tensor_reduce(out=st32[:, :, 1], in_=q3, axis=AX.X, op=ALU.add), r=["F3"], w=["st32"])
                    S.op("dve", lambda: V.tensor_scalar(out=st32[:, :, 0], in0=st32[:, :, 0], scalar1=1.0 / 64.0, scalar2=None,
                                                        op0=ALU.mult), r=["st32"], w=["st32"])
                    S.op("dve", lambda: V.tensor_tensor(out=st32[:, :, 2], in0=st32[:, :, 0], in1=st32[:, :, 0], op=ALU.mult),
                         r=["st32"], w=["st32"])
                    S.op("dve", lambda: V.scalar_tensor_tensor(out=st32[:, :, 1], in0=st32[:, :, 1], scalar=1.0 / 64.0, in1=st32[:, :, 2],
                                                               op0=ALU.mult, op1=ALU.subtract), r=["st32"], w=["st32"])
                    S.op("act", lambda: A.activation(out=st32[:, :, 1], in_=st32[:, :, 1], func=AF.Sqrt, bias=self.epsc[:, 2:3], scale=1.0),
                         r=["st32", "epsc"], w=["st32"])
                    S.op("dve", lambda: V.reciprocal(out=st32[:, :, 1], in_=st32[:, :, 1]), r=["st32"], w=["st32"])
                    S.op("dve", lambda: V.tensor_tensor(out=y3, in0=y3, in1=st32[:, :, 0:1].to_broadcast([128, 2 * NT, 64]),
                                                        op=ALU.subtract), r=["F2", "st32"], w=["F2"])
                    S.op("dve", lambda: V.tensor_tensor(out=y3, in0=y3, in1=st32[:, :, 1:2].to_broadcast([128, 2 * NT, 64]),
                                                        op=ALU.mult), r=["F2", "st32"], w=["F2"])
                    S.op("pool", lambda: G.tensor_tensor(out=ytm, in0=ytm, in1=lnw[:].unsqueeze(1).to_broadcast([128, NT, 128]),
                                                         op=ALU.mult), r=["F2", "lnw"], w=["F2"])
                    S.op("pool", lambda: G.tensor_tensor(out=ytm, in0=ytm, in1=lnb[:].unsqueeze(1).to_broadcast([128, NT, 128]),
                                                         op=ALU.add), r=["F2", "lnb"], w=["F2"])
                    S.op("dve", lambda: V.tensor_tensor(out=q3, in0=Vtm[:].rearrange("p t (h c) -> p (t h) c", c=64),
                                                        in1=bon[:].rearrange("p t h -> p (t h)").unsqueeze(2).to_broadcast([128, 2 * NT, 64]),
                                                        op=ALU.mult), r=["Vtm", "bon", "F3"], w=["F3"])
                    S.op("dve", lambda: V.tensor_tensor(out=F[2][:], in0=F[2][:], in1=F[3][:], op=ALU.add), r=["F2", "F3"], w=["F2"])
                    for t4 in range(4):
                        pb, kp = self.psum()
                        for j in range(4):
                            tt = t4 * 4 + j
                            mm(pb[:, j * 128:(j + 1) * 128], sgT[:, tt * 128:(tt + 1) * 128], g2[:, fs_], True, True, ["sgT", "g2"], [kp])
                        S.op("dve", lambda pb=pb, t4=t4: V.tensor_tensor(
                            out=y16[:, t4 * 4:(t4 + 1) * 4, :], in0=pb[:].rearrange("p (a j) -> p a j", j=128),
                            in1=ytm[:, t4 * 4:(t4 + 1) * 4, :], op=ALU.mult), r=[kp, "F2"], w=["Btm", kp])
                    for t4 in range(4):
                        pb, kp = self.psum()
                        pbv = pb[:].bitcast(BF16)
                        for j in range(4):
                            tt = t4 * 4 + j
                            S.op("pe", lambda j=j, tt=tt, pbv=pbv: PE.transpose(out=pbv[:, j * 128:(j + 1) * 128], in_=y16[:, tt, :],
                                                                               identity=self.identbf[:]), r=["Btm", "identbf"], w=[kp])
                        S.op("act", lambda t4=t4, pbv=pbv: A.activation(out=yTp[:, t4 * 512:(t4 + 1) * 512], in_=pbv[:, 0:512], func=AF.Copy),
                             r=[kp], w=["kT", kp])
                    S.dma("sp", ydst[fs_, :], yTp[:], r=["kT"], w=[("yT1", p)])
                if p + 1 < 8:
                    projA(p + 1)
                output_phase()
            S.barrier()


    def merge(self, l):
        nc, S = self.nc, self.S
        V, A, G, PE = nc.vector, nc.scalar, nc.gpsimd, nc.tensor
        Wl = self.P["w_in"][l]
        brw = [self.P["w_br_ssd"][l], self.P["w_br_rwkv"][l], self.P["w_br_hgrn"][l]]
        hTk = [("hT", t) for t in range(NT)]
        with contextlib.ExitStack() as st:
            mT = self.sb(st, "mT", [128, KT, T], F32)
            wbrs = [self.sb(st, "wbr", [128, KT, D], BF16) for _ in range(2)]
            wgts = [self.sb(st, "wgt", [128, KT, D], BF16) for _ in range(2)]
            st1 = contextlib.ExitStack()
            st1.__enter__()
            yq = [self.sb(st1, "yq", [128, KT, 512], BF16) for _ in range(2)]
            sg = [self.sb(st1, "sgm", [128, 512], BF16) for _ in range(2)]
            tmp = [self.sb(st1, "tmpm", [128, 512], F32) for _ in range(2)]
            cnt = 0
            def load_br(i):
                S.dma("pool", wbrs[i % 2][:], brw[i].rearrange("(kt p) n -> p kt n", p=128), w=[("wbr", i % 2)])
                c0 = OFF_GATES + i * 1024
                S.dma("pool", wgts[i % 2][:], Wl[:, c0:c0 + 1024].rearrange("(kt p) n -> p kt n", p=128), w=[("wgt", i % 2)])

            load_br(0)
            load_br(1)
            for i in range(3):
                wbr, wgt = wbrs[i % 2], wgts[i % 2]
                kwb, kwg = ("wbr", i % 2), ("wgt", i % 2)
                if i == 2:
                    load_br(2)
                for q in range(4):
                    qs_ = slice(q * 512, (q + 1) * 512)
                    yb = (i * 4 + q) % 2
                    S.dma("sp", yq[yb][:], self.yT_dram[i][:, qs_].rearrange("(kt p) n -> p kt n", p=128),
                          r=[("yT%d" % i, k) for k in range(8)], w=[("yq", yb)])
                    for ot in range(KT):
                        os_ = slice(ot * 128, (ot + 1) * 128)
                        pg, kg = self.psum()
                        pb, kb = self.psum()
                        for kt in range(KT):
                            S.op("pe", lambda kt=kt, pg=pg: PE.matmul(pg[:], lhsT=wgt[:, kt, os_], rhs=self.hT[:, kt, qs_],
                                                                      start=(kt == 0), stop=(kt == KT - 1)),
                                 r=[kwg] + hTk[q * 4:q * 4 + 4], w=[kg])
                        for kt in range(KT):
                            S.op("pe", lambda kt=kt, pb=pb: PE.matmul(pb[:], lhsT=wbr[:, kt, os_], rhs=yq[yb][:, kt, :],
                                                                      start=(kt == 0), stop=(kt == KT - 1)),
                                 r=[kwb, ("yq", yb)], w=[kb])
                        c2 = cnt % 2
                        cnt += 1
                        S.op("act", lambda pg=pg, c2=c2: A.activation(out=sg[c2][:], in_=pg[:], func=AF.Sigmoid),
                             r=[kg], w=[("sgm", c2), kg])
                        if i == 0:
                            S.op("dve", lambda pb=pb, c2=c2: V.tensor_tensor(out=mT[:, ot, qs_], in0=pb[:], in1=sg[c2][:], op=ALU.mult),
                                 r=[kb, ("sgm", c2)], w=[("mT", q), kb])
                        else:
                            S.op("dve", lambda pb=pb, c2=c2: V.tensor_tensor(out=tmp[c2][:], in0=pb[:], in1=sg[c2][:], op=ALU.mult),
                                 r=[kb, ("sgm", c2)], w=[("tmpm", c2), kb])
                            S.op("pool", lambda c2=c2: G.tensor_tensor(out=mT[:, ot, qs_], in0=mT[:, ot, qs_], in1=tmp[c2][:],
                                                                       op=ALU.add), r=[("tmpm", c2), ("mT", q)], w=[("mT", q)])
            self.dbg_dump("merged%d" % l, lambda o: S.dma("sp", o.rearrange("(kt p) n -> p kt n", p=128), mT[:],
                                                          r=[("mT", q) for q in range(4)]))
            S.barrier()
            st1.__exit__(None, None, None)
            wo = wbrs[1]
            S.dma("pool", wo[:], self.P["w_out"][l].rearrange("(kt p) n -> p kt n", p=128), w=[("wbr", 1)])
            gbc = self.sb(st, "gbc1", [128, D], F32)
            bbc = self.sb(st, "bbc1", [128, D], F32)
            S.dma("sp", gbc[:], self.P["ln1_g"][l].partition_broadcast(128), w=["gbc"])
            S.dma("sp", bbc[:], self.P["ln1_b"][l].partition_broadcast(128), w=["bbc"])
            lnw = self.ln_alloc(st)
            h1 = [self.sb(st, "h1m", [128, D], F32) for _ in range(2)]
            mbf1 = self.sb(st, "mbf", [128, KT, 128], BF16)
            mbf = [mbf1, mbf1]
            for tt in range(NT):
                s2 = tt % 2
                q = tt // 4
                tsl = slice(tt * 128, (tt + 1) * 128)
                S.op("act", lambda: A.activation(out=mbf[s2][:], in_=mT[:, :, tsl], func=AF.Copy), r=[("mT", q)], w=[("mbf", 0)])
                S.dma("sp", h1[s2][:], self.h_dram[tsl, :], r=[("hd", tt)], w=[("h1m", s2)])
                xin, kx = self.ln_xin(lnw, tt)
                for half in range(2):
                    po, ko = self.psum()
                    for kt in range(KT):
                        S.op("pe", lambda kt=kt, po=po: PE.matmul(po[:], lhsT=mbf[s2][:, kt, :], rhs=wo[:, kt, half * 512:(half + 1) * 512],
                                                                  start=(kt == 0), stop=(kt == KT - 1)), r=[("mbf", 0), ("wbr", 1)], w=[ko])
                    S.op("dve", lambda po=po, half=half: V.scalar_tensor_tensor(
                        out=xin[:, half * 512:(half + 1) * 512], in0=h1[s2][:, half * 512:(half + 1) * 512], scalar=ALPHA, in1=po[:],
                        op0=ALU.mult, op1=ALU.add), r=[ko, ("h1m", s2)], w=[kx, ko])
                self.ln_tile(lnw, tt, gbc, bbc, self.h_dram, router=True, extra=self.dbg_out.get("h1_%d" % l))
            S.barrier()

    def layer(self, l):
        S = self.S
        if "ssd" in self.stages:
            self.ssd(l)
            self.dbg_dump("ya%d" % l, lambda o: S.dma("sp", o, self.yT_dram[0], r=[("yT0", h) for h in range(8)]))
        if "rwkv" in self.stages:
            self.rwkv(l)
            self.dbg_dump("yb%d" % l, lambda o: S.dma("sp", o, self.yT_dram[1], r=[("yT1", h) for h in range(8)]))
        if "hgrn" in self.stages:
            self.hgrn(l)
            self.dbg_dump("yc%d" % l, lambda o: S.dma("sp", o, self.yT_dram[2], r=[("yT2", h) for h in range(8)]))
        if "merge" in self.stages:
            self.merge(l)
        if "moe" in self.stages:
            self.moe(l, last=(l == self.depth - 1))


_NC_CACHE = {}


def _get_nc():
    if "nc" not in _NC_CACHE:
        _NC_CACHE["nc"] = Builder().build()
    return _NC_CACHE["nc"]


def kernel(**inputs):
    nc = _get_nc()
    x = np.ascontiguousarray(inputs["x"], dtype=np.float32)
    base = {k: np.ascontiguousarray(inputs[k], dtype=np.float32) for k in PARAM_SHAPES}
    in_maps = []
    for c in range(8):
        m = dict(base)
        m["x"] = x[c]
        in_maps.append(m)
    res = run_bass_kernel_spmd(nc, in_maps, core_ids=list(range(8)))
    return np.stack([res.results[c]["out"] for c in range(8)], axis=0)
```

```python
import contextlib
import os
import numpy as np
CUT = int(os.environ.get('CUT', '99'))
HC = int(os.environ.get('HC', '99'))
HL = int(os.environ.get('HL', '99'))
import concourse.bass as bass
import concourse.mybir as mybir
from concourse.bass_utils import run_bass_kernel_spmd

F32 = mybir.dt.float32
BF16 = mybir.dt.bfloat16
AF = mybir.ActivationFunctionType
ALU = mybir.AluOpType
AX = mybir.AxisListType

D = 1024
T = 2048
NT = T // 128
KT = D // 128
DEPTH = 2
NE = 16
DEXP = 512
N_IN = 13072
ALPHA = (2 * DEPTH) ** 0.25
LN_EPS = 1e-5
RMS_EPS = 1e-6
GN_EPS = 64e-5
OFF_Z = 0
OFF_XBC = 1024
OFF_DT = 2560
OFF_RWKV = 2576
OFF_HGRN = OFF_RWKV + 3328
OFF_GATES = OFF_HGRN + 4096

PARAM_SHAPES = {
    "ln_in_g": [1024], "ln_in_b": [1024], "w_in": [2, 1024, 13072],
    "ssd_conv_w": [2, 4, 1536], "ssd_conv_b": [2, 1536], "ssd_dt_bias": [2, 16],
    "ssd_a_log": [2, 16], "ssd_d": [2, 16], "ssd_norm_w": [2, 1024],
    "rwkv_mu": [2, 3328], "rwkv_w0": [2, 1024], "rwkv_w2": [2, 64, 1024],
    "rwkv_a0": [2, 1024], "rwkv_a2": [2, 64, 1024], "rwkv_g2": [2, 128, 1024],
    "rwkv_k_k": [2, 1024], "rwkv_k_a": [2, 1024], "rwkv_r_k": [2, 16, 64],
    "rwkv_ln_w": [2, 1024], "rwkv_ln_b": [2, 1024], "hgrn_lb": [2, 1024],
    "hgrn_norm_w": [2, 128], "w_br_ssd": [2, 1024, 1024], "w_br_rwkv": [2, 1024, 1024],
    "w_br_hgrn": [2, 1024, 1024], "w_out": [2, 1024, 1024], "ln1_g": [2, 1024],
    "ln1_b": [2, 1024], "router_w": [1024, 16], "router_bias": [16],
    "exp_w_gate": [2, 16, 1024, 512], "exp_w_up": [2, 16, 1024, 512],
    "exp_w_down": [2, 16, 512, 1024], "ln2_g": [2, 1024], "ln2_b": [2, 1024],
}


class Sched:
    ENG = ["pe", "act", "dve", "pool", "sp"]

    def __init__(self, nc, es, n_dma=32, n_pdma=24):
        self.nc = nc
        self.e = {"pe": nc.tensor, "act": nc.scalar, "dve": nc.vector, "pool": nc.gpsimd, "sp": nc.sync}
        self.sem = {k: es.enter_context(nc.semaphore("sem_" + k)) for k in self.ENG}
        self.cnt = {k: 0 for k in self.ENG}
        self.dsem = [es.enter_context(nc.semaphore("dsem%d" % i)) for i in range(n_dma)]
        self.dtot = [0] * n_dma
        self.drr = 0
        self.psem = [es.enter_context(nc.semaphore("psem%d" % i)) for i in range(n_pdma)]
        self.pused = [False] * n_pdma
        self.pwaiters = [[] for _ in range(n_pdma)]
        self.pclr = [None] * n_pdma
        self.prr = 0
        self.msem = {k: es.enter_context(nc.semaphore("msem_" + k)) for k in self.ENG}
        self.mcnt = {k: 0 for k in self.ENG}
        self.seen = {k: {} for k in self.ENG}
        self.lastw = {}
        self.readers = {}
        self.nwait = 0
        self.qset = set()

    def _semh(self, sk):
        if isinstance(sk, str):
            return self.sem[sk]
        if sk[0] == "m":
            return self.msem[sk[1]]
        return self.dsem[sk[1]] if sk[0] == "d" else self.psem[sk[1]]

    def _wait(self, e, tag):
        sk, val = tag
        if val <= 0 or self.seen[e].get(sk, 0) >= val:
            return
        if not isinstance(sk, str) and sk[0] == "p" and self.pclr[sk[1]] is not None and e != "pool":
            self._wait(e, self.pclr[sk[1]])
        self.e[e].wait_ge(self._semh(sk), val)
        self.seen[e][sk] = val
        self.nwait += 1
        if not isinstance(sk, str) and sk[0] == "p":
            self.pwaiters[sk[1]].append(self._marker(e))

    def _marker(self, e):
        self.e[e].sem_inc(self.msem[e], 1)
        self.mcnt[e] += 1
        return (("m", e), self.mcnt[e])

    def _deps(self, e, r, w):
        for k in r:
            t = self.lastw.get(k)
            if t is not None:
                self._wait(e, t)
        for k in w:
            t = self.lastw.get(k)
            if t is not None and (t[0] != e or e != "pe"):
                self._wait(e, t)
            for sk, val in self.readers.get(k, {}).items():
                if sk != e or e != "pe":
                    self._wait(e, (sk, val))

    def _record(self, tag, r, w):
        for k in r:
            d = self.readers.setdefault(k, {})
            if d.get(tag[0], 0) < tag[1]:
                d[tag[0]] = tag[1]
        for k in w:
            self.lastw[k] = tag
            self.readers[k] = {}

    def _exp(self, keys):
        out = []
        for k in keys:
            if k in self.qset:
                out.extend((k, q) for q in range(4))
            else:
                out.append(k)
        return out

    def op(self, e, fn, r=(), w=()):
        r, w = self._exp(r), self._exp(w)
        self._deps(e, r, w)
        ins = fn()
        self.cnt[e] += 1
        ins.then_inc(self.sem[e], 1)
        if os.environ.get("OPLOG"):
            self.oplog = getattr(self, "oplog", {})
            self.oplog[(e, self.cnt[e])] = fn.__code__.co_firstlineno
        self._record((e, self.cnt[e]), r, w)

    def _dma_sw(self, out, in_, r, w):
        q = "pool"
        self._deps(q, r, w)
        i = self.prr
        self.prr = (self.prr + 1) % len(self.psem)
        sk = ("p", i)
        if self.pused[i]:
            self._wait(q, (sk, 16))
            for e in self.ENG:
                if e != q:
                    self._wait(e, (sk, 16))
            for tg in self.pwaiters[i]:
                if tg[0][1] != q:
                    self._wait(q, tg)
            self.e[q].sem_clear(self.psem[i])
            tclr = self._marker(q)
            self.pclr[i] = tclr
            for k, t in list(self.lastw.items()):
                if t[0] == sk:
                    self.lastw[k] = tclr
            for k, d in self.readers.items():
                if sk in d:
                    d.pop(sk)
                    d[tclr[0]] = tclr[1]
            for e in self.ENG:
                self.seen[e].pop(sk, None)
            self.pwaiters[i] = []
        ins = self.e[q].dma_start(out=out, in_=in_)
        ins.then_inc(self.psem[i], 16)
        self.pused[i] = True
        self._record((sk, 16), r, w)

    def dma(self, q, out, in_, r=(), w=()):
        r, w = self._exp(r), self._exp(w)
        if q == "pool" and os.environ.get("PSEM_CLEAR"):
            return self._dma_sw(out, in_, r, w)
        self._deps(q, r, w)
        i = self.drr
        self.drr = (self.drr + 1) % len(self.dsem)
        self._wait(q, (("d", i), self.dtot[i]))
        with self.nc.allow_non_contiguous_dma(reason="small per-feature parameter columns"):
            ins = self.e[q].dma_start(out=out, in_=in_)
        self.dtot[i] += 16
        ins.then_inc(self.dsem[i], 16)
        self._record((("d", i), self.dtot[i]), r, w)

    def barrier(self):
        for e in self.ENG:
            for o in self.ENG:
                if o != e:
                    self._wait(e, (o, self.cnt[o]))
            for i in range(len(self.dsem)):
                self._wait(e, (("d", i), self.dtot[i]))
            for i in range(len(self.psem)):
                if self.pused[i]:
                    self._wait(e, (("p", i), 16))

    def finish(self):
        for i in range(len(self.dsem)):
            self._wait("sp", (("d", i), self.dtot[i]))
        for i in range(len(self.psem)):
            if self.pused[i]:
                self._wait("sp", (("p", i), 16))
        for o in self.ENG:
            if o != "sp":
                self._wait("sp", (o, self.cnt[o]))


class Builder:
    def __init__(self, debug=None, stages=("pre", "hgrn", "ssd", "rwkv", "merge", "moe"), depth=DEPTH, pre_router=False):
        self.pre_router = pre_router
        self.debug = debug or {}
        self.stages = stages
        self.depth = depth
        self.nc = bass.Bass("TRN2", target_bir_lowering=False)
        nc = self.nc
        self.x = nc.dram_tensor("x", [T, D], F32, kind="ExternalInput").ap()
        self.P = {k: nc.dram_tensor(k, s, F32, kind="ExternalInput").ap() for k, s in PARAM_SHAPES.items()}
        self.out = nc.dram_tensor("out", [T, D], F32, kind="ExternalOutput").ap()
        self.h_dram = nc.dram_tensor("h_scr", [T, D], F32, kind="Internal").ap()
        self.yT_dram = [nc.dram_tensor("yT_scr%d" % i, [D, T], BF16, kind="Internal").ap() for i in range(3)]
        self.dbg_out = {}
        for name, (shape, dt) in self.debug.items():
            self.dbg_out[name] = nc.dram_tensor("dbg_" + name, shape, dt, kind="ExternalOutput").ap()
        self.uid = 0

    def sb(self, es, name, shape, dt):
        self.uid += 1
        return es.enter_context(self.nc.sbuf_tensor("%s_%d" % (name, self.uid), shape, dt))

    def psum(self):
        i = self.ps_rr
        self.ps_rr = (self.ps_rr + 1) % 8
        return self.ps[i], ("ps", i)

    def build(self):
        nc = self.nc
        with contextlib.ExitStack() as es:
            self.S = Sched(nc, es)
            S = self.S
            self.ps = [es.enter_context(nc.psum_tensor("psb%d" % i, [128, 512], F32)) for i in range(8)]
            self.ps_rr = 0
            self.ident32 = self.sb(es, "ident32", [128, 128], F32)
            self.identbf = self.sb(es, "identbf", [128, 128], BF16)
            self.zeros = self.sb(es, "zeros", [128, 128], F32)
            self.ones = self.sb(es, "ones", [128, 128], F32)
            self.onesbf = self.sb(es, "onesbf", [128, 128], BF16)
            self.epsc = self.sb(es, "epsc", [128, 4], F32)
            S.op("pool", lambda: nc.gpsimd.memset(self.zeros[:], 0.0), w=["zeros"])
            S.op("pool", lambda: nc.gpsimd.memset(self.ones[:], 1.0), w=["ones"])
            S.op("pool", lambda: nc.gpsimd.memset(self.onesbf[:], 1.0), w=["onesbf"])
            S.op("pool", lambda: nc.gpsimd.memset(self.epsc[:, 0:1], LN_EPS), w=["epsc"])
            S.op("pool", lambda: nc.gpsimd.memset(self.epsc[:, 1:2], RMS_EPS), w=["epsc"])
            S.op("pool", lambda: nc.gpsimd.memset(self.epsc[:, 2:3], GN_EPS), w=["epsc"])
            S.op("pool", lambda: nc.gpsimd.memset(self.epsc[:, 3:4], 1.0), w=["epsc"])
            S.op("pool", lambda: nc.gpsimd.affine_select(
                out=self.ident32[:], in_=self.zeros[:], pattern=[[1, 128]], compare_op=ALU.not_equal,
                fill=1.0, base=0, channel_multiplier=-1), r=["zeros"], w=["ident32"])
            S.op("pool", lambda: nc.gpsimd.tensor_copy(out=self.identbf[:], in_=self.ident32[:]),
                 r=["ident32"], w=["identbf"])
            self.hT = self.sb(es, "hT", [128, KT, T], BF16)
            self.gates = self.sb(es, "gates", [128, NT, NE], F32)
            self.logits = self.sb(es, "logits", [128, NT, NE], F32)
            self.rw32 = self.sb(es, "rw32", [128, KT, NE], F32)
            self.rbias = self.sb(es, "rbias", [128, NE], F32)
            S.dma("sp", self.rw32[:], self.P["router_w"].rearrange("(kt p) e -> p kt e", p=128), w=["rw32"])
            S.dma("sp", self.rbias[:], self.P["router_bias"].partition_broadcast(128), w=["rbias"])

            with contextlib.ExitStack() as st:
                gbc = self.sb(st, "gbc", [128, D], F32)
                bbc = self.sb(st, "bbc", [128, D], F32)
                S.dma("sp", gbc[:], self.P["ln_in_g"].partition_broadcast(128), w=["gbc"])
                S.dma("sp", bbc[:], self.P["ln_in_b"].partition_broadcast(128), w=["bbc"])
                lnw = self.ln_alloc(st)
                for tt in range(NT):
                    xin, kx = self.ln_xin(lnw, tt)
                    S.dma("sp", xin[:], self.x[tt * 128:(tt + 1) * 128, :], w=[kx])
                    self.ln_tile(lnw, tt, gbc, bbc, self.h_dram, router=self.pre_router, extra=self.dbg_out.get("h0"))
                S.barrier()
            self.dbg_dump("hT", lambda o: S.dma("sp", o, self.hT[:], r=[("hT", t) for t in range(NT)]))
            self.dbg_dump("logits", lambda o: S.dma("sp", o, self.logits[:], r=["logits"]))

            for l in range(self.depth):
                self.layer(l)
            S.finish()
        return nc

    def dbg_dump(self, name, fn):
        if name in self.dbg_out:
            fn(self.dbg_out[name])

    def ln_alloc(self, st):
        w = {}
        w["xin"] = [self.sb(st, "xin", [128, D], F32) for _ in range(2)]
        w["hh"] = [self.sb(st, "hh", [128, D], F32) for _ in range(2)]
        w["bst"] = [self.sb(st, "bst", [128, 2, 6], F32) for _ in range(2)]
        w["mv"] = [self.sb(st, "mv", [128, 4], F32) for _ in range(2)]
        w["h32"] = [self.sb(st, "h32", [128, KT, 128], F32) for _ in range(2)]
        w["id"] = self.uid
        return w

    def ln_xin(self, w, tt):
        return w["xin"][tt % 2], ("xin", w["id"], tt % 2)

    def ln_tile(self, w, tt, gbc, bbc, dst_dram, router, extra=None):
        nc, S = self.nc, self.S
        s = tt % 2
        wid = w["id"]
        xin, kx = w["xin"][s], ("xin", wid, s)
        hh, kh = w["hh"][s], ("hh", wid, s)
        bst, kb = w["bst"][s], ("bst", wid, s)
        mv, km = w["mv"][s], ("mv", wid, s)
        h32, k32 = w["h32"][s], ("h32", wid, s)
        for c in range(2):
            S.op("dve", lambda c=c: nc.vector.bn_stats(out=bst[:, c, :], in_=xin[:, c * 512:(c + 1) * 512]),
                 r=[kx], w=[kb])
        S.op("dve", lambda: nc.vector.bn_aggr(out=mv[:, 0:2], in_=bst[:].rearrange("p a b -> p (a b)")),
             r=[kb], w=[km])
        if CUT < 2:
            return
        S.op("act", lambda: nc.scalar.activation(out=mv[:, 2:3], in_=mv[:, 1:2], func=AF.Sqrt,
                                                 bias=self.epsc[:, 0:1], scale=1.0), r=[km, "epsc"], w=[km])
        S.op("dve", lambda: nc.vector.reciprocal(out=mv[:, 3:4], in_=mv[:, 2:3]), r=[km], w=[km])
        S.op("dve", lambda: nc.vector.tensor_scalar(out=xin[:], in0=xin[:], scalar1=mv[:, 0:1], scalar2=mv[:, 3:4],
                                                    op0=ALU.subtract, op1=ALU.mult), r=[kx, km], w=[kx])
        if CUT < 3:
            return
        S.op("pool", lambda: nc.gpsimd.tensor_tensor(out=hh[:], in0=xin[:], in1=gbc[:], op=ALU.mult),
             r=[kx, "gbc"], w=[kh])
        S.op("pool", lambda: nc.gpsimd.tensor_tensor(out=hh[:], in0=hh[:], in1=bbc[:], op=ALU.add),
             r=[kh, "bbc"], w=[kh])
        S.dma("sp", dst_dram[tt * 128:(tt + 1) * 128, :], hh[:], r=[kh], w=[("hd", tt)])
        if extra is not None:
            S.dma("sp", extra[tt * 128:(tt + 1) * 128, :], hh[:], r=[kh], w=[("hdx", tt)])
        if CUT < 4:
            return
        for half in range(2):
            pb, kp = self.psum()
            for j in range(4):
                kt = half * 4 + j
                S.op("pe", lambda j=j, kt=kt: nc.tensor.transpose(
                    out=pb[:, j * 128:(j + 1) * 128], in_=hh[:, kt * 128:(kt + 1) * 128], identity=self.ident32[:]),
                    r=[kh, "ident32"], w=[kp])
            if os.environ.get("EVAC", "act") == "act":
                S.op("act", lambda half=half, pb=pb: nc.scalar.activation(
                    out=self.hT[:, half * 4:(half + 1) * 4, tt * 128:(tt + 1) * 128],
                    in_=pb[:].rearrange("p (a b) -> p a b", a=4), func=AF.Copy), r=[kp], w=[("hT", tt), kp])
            else:
                S.op("dve", lambda half=half, pb=pb: nc.vector.tensor_copy(
                    out=self.hT[:, half * 4:(half + 1) * 4, tt * 128:(tt + 1) * 128],
                    in_=pb[:].rearrange("p (a b) -> p a b", a=4)), r=[kp], w=[("hT", tt), kp])
            if router:
                S.op("dve", lambda half=half, pb=pb: nc.vector.tensor_copy(
                    out=h32[:, half * 4:(half + 1) * 4, :], in_=pb[:].rearrange("p (a b) -> p a b", a=4)),
                    r=[kp], w=[k32, kp])
        if router and CUT >= 5:
            pb, kp = self.psum()
            for kt in range(KT):
                S.op("pe", lambda kt=kt: nc.tensor.matmul(pb[:, 0:NE], lhsT=h32[:, kt, :], rhs=self.rw32[:, kt, :],
                                                          start=(kt == 0), stop=(kt == KT - 1)),
                     r=[k32, "rw32"], w=[kp])
            S.op("dve", lambda: nc.vector.tensor_copy(out=self.logits[:, tt, :], in_=pb[:, 0:NE]),
                 r=[kp], w=["logits"])

    def router(self, st):
        nc, S = self.nc, self.S
        V = nc.vector
        L = self.logits
        t1 = self.sb(st, "rt1", [128, NT, NE], F32)
        probs = self.sb(st, "probs", [128, NT, NE], F32)
        sel = self.sb(st, "sel", [128, NT, NE], F32)
        p6 = self.sb(st, "p6", [128, NT, 4, 6], F32)
        gs = self.sb(st, "gs", [128, NT, 4], F32)
        gm = self.sb(st, "gm", [128, NT, 4], F32)
        gt = self.sb(st, "gt", [128, NT, 4], F32)
        red = self.sb(st, "red", [128, NT], F32)
        red2 = self.sb(st, "red2", [128, NT], F32)
        msk = self.sb(st, "msk", [128, NT, NE], F32)
        eq = self.sb(st, "eq", [128, NT, NE], F32)
        BIG = 1.0e9

        def bc(a):
            return a[:].unsqueeze(2).to_broadcast([128, NT, NE])

        S.op("dve", lambda: V.tensor_reduce(out=red[:], in_=L[:], axis=AX.X, op=ALU.max), r=["logits"], w=["red"])
        S.op("dve", lambda: V.tensor_tensor(out=t1[:], in0=L[:], in1=bc(red), op=ALU.subtract),
             r=["logits", "red"], w=["rt1"])
        S.op("act", lambda: nc.scalar.activation(out=t1[:], in_=t1[:], func=AF.Exp), r=["rt1"], w=["rt1"])
        S.op("dve", lambda: V.tensor_reduce(out=red[:], in_=t1[:], axis=AX.X, op=ALU.add), r=["rt1"], w=["red"])
        S.op("dve", lambda: V.reciprocal(out=red[:], in_=red[:]), r=["red"], w=["red"])
        S.op("dve", lambda: V.tensor_tensor(out=probs[:], in0=t1[:], in1=bc(red), op=ALU.mult),
             r=["rt1", "red"], w=["probs"])
        S.op("dve", lambda: V.tensor_tensor(out=sel[:], in0=probs[:],
                                            in1=self.rbias[:].unsqueeze(1).to_broadcast([128, NT, NE]), op=ALU.add),
             r=["probs", "rbias"], w=["sel"])
        s4 = sel[:].rearrange("p t (g e) -> p t g e", g=4)
        S.op("dve", lambda: V.tensor_tensor(out=p6[:, :, :, 0:3], in0=s4[:, :, :, 0:3], in1=s4[:, :, :, 1:4],
                                            op=ALU.add), r=["sel"], w=["p6"])
        S.op("dve", lambda: V.tensor_tensor(out=p6[:, :, :, 3:5], in0=s4[:, :, :, 0:2], in1=s4[:, :, :, 2:4],
                                            op=ALU.add), r=["sel"], w=["p6"])
        S.op("dve", lambda: V.tensor_tensor(out=p6[:, :, :, 5:6], in0=s4[:, :, :, 0:1], in1=s4[:, :, :, 3:4],
                                            op=ALU.add), r=["sel"], w=["p6"])
        S.op("dve", lambda: V.tensor_reduce(out=gs[:], in_=p6[:], axis=AX.X, op=ALU.max), r=["p6"], w=["gs"])
        S.op("dve", lambda: V.tensor_reduce(out=red[:], in_=gs[:], axis=AX.X, op=ALU.max), r=["gs"], w=["red"])
        S.op("dve", lambda: V.tensor_tensor(out=gm[:], in0=gs[:], in1=red[:].unsqueeze(2).to_broadcast([128, NT, 4]),
                                            op=ALU.is_ge), r=["gs", "red"], w=["gm"])
        S.op("dve", lambda: V.tensor_scalar(out=gt[:], in0=gm[:], scalar1=BIG, scalar2=-BIG, op0=ALU.mult,
                                            op1=ALU.add), r=["gm"], w=["gt"])
        m4 = msk[:].rearrange("p t (g e) -> p t g e", g=4)
        S.op("dve", lambda: V.tensor_tensor(out=m4, in0=s4, in1=gm[:].unsqueeze(3).to_broadcast([128, NT, 4, 4]),
                                            op=ALU.mult), r=["sel", "gm"], w=["msk"])
        S.op("dve", lambda: V.tensor_tensor(out=m4, in0=m4, in1=gt[:].unsqueeze(3).to_broadcast([128, NT, 4, 4]),
                                            op=ALU.add), r=["msk", "gt"], w=["msk"])
        S.op("dve", lambda: V.tensor_reduce(out=red[:], in_=msk[:], axis=AX.X, op=ALU.max), r=["msk"], w=["red"])
        S.op("dve", lambda: V.tensor_tensor(out=eq[:], in0=msk[:], in1=bc(red), op=ALU.is_equal),
             r=["msk", "red"], w=["eq"])
        S.op("dve", lambda: V.scalar_tensor_tensor(out=eq[:], in0=eq[:], scalar=-BIG, in1=msk[:], op0=ALU.mult,
                                                   op1=ALU.add), r=["eq", "msk"], w=["eq"])
        S.op("dve", lambda: V.tensor_reduce(out=red2[:], in_=eq[:], axis=AX.X, op=ALU.max), r=["eq"], w=["red2"])
        S.op("dve", lambda: V.tensor_tensor(out=eq[:], in0=msk[:], in1=bc(red2), op=ALU.is_ge),
             r=["msk", "red2"], w=["eq"])
        S.op("dve", lambda: V.tensor_tensor(out=eq[:], in0=eq[:], in1=probs[:], op=ALU.mult),
             r=["eq", "probs"], w=["eq"])
        S.op("dve", lambda: V.tensor_reduce(out=red[:], in_=eq[:], axis=AX.X, op=ALU.add), r=["eq"], w=["red"])
        S.op("dve", lambda: V.reciprocal(out=red[:], in_=red[:]), r=["red"], w=["red"])
        S.op("dve", lambda: V.tensor_tensor(out=self.gates[:], in0=eq[:], in1=bc(red), op=ALU.mult),
             r=["eq", "red"], w=["gates"])

    def moe(self, l, last):
        nc, S = self.nc, self.S
        wg_d, wu_d, wd_d = self.P["exp_w_gate"], self.P["exp_w_up"], self.P["exp_w_down"]
        with contextlib.ExitStack() as st:
            self.router(st)
            self.dbg_dump("gates%d" % l, lambda o: S.dma("sp", o, self.gates[:], r=["gates"]))
            acc = self.sb(st, "acc", [128, NT, D], F32)
            wg = [self.sb(st, "wg", [128, KT, DEXP], BF16) for _ in range(2)]
            wu = [self.sb(st, "wu", [128, KT, DEXP], BF16) for _ in range(2)]
            wd = [self.sb(st, "wd", [128, 4, D], BF16) for _ in range(2)]
            hg = [self.sb(st, "hg", [128, 4, 512], BF16) for _ in range(2)]
            sg = [self.sb(st, "sg", [128, 512], BF16) for _ in range(2)]
            gbc = self.sb(st, "gbc2", [128, D], F32)
            bbc = self.sb(st, "bbc2", [128, D], F32)
            S.dma("sp", gbc[:], self.P["ln2_g"][l].partition_broadcast(128), w=["gbc"])
            S.dma("sp", bbc[:], self.P["ln2_b"][l].partition_broadcast(128), w=["bbc"])

            def load_w(e):
                b = e % 2
                S.dma("pool", wg[b][:], wg_d[l, e].rearrange("(kt p) n -> p kt n", p=128), w=[("wg", b)])
                S.dma("pool", wu[b][:], wu_d[l, e].rearrange("(kt p) n -> p kt n", p=128), w=[("wu", b)])
                S.dma("pool", wd[b][:], wd_d[l, e].rearrange("(kt p) n -> p kt n", p=128), w=[("wd", b)])

            items = [(e, q) for e in range(int(os.environ.get('ME', NE))) for q in range(4)]
            sgi = [0]

            def G(i):
                e, q = items[i]
                b = e % 2
                hb = i % 2
                for dt_ in range(4):
                    pa, ka = self.psum()
                    pu, ku = self.psum()
                    for kt in range(KT):
                        S.op("pe", lambda kt=kt, pa=pa: nc.tensor.matmul(
                            pa[:], lhsT=wg[b][:, kt, dt_ * 128:(dt_ + 1) * 128], rhs=self.hT[:, kt, q * 512:(q + 1) * 512],
                            start=(kt == 0), stop=(kt == KT - 1)),
                            r=[("wg", b)] + [("hT", q * 4 + j) for j in range(4)], w=[ka])
                    for kt in range(KT):
                        S.op("pe", lambda kt=kt, pu=pu: nc.tensor.matmul(
                            pu[:], lhsT=wu[b][:, kt, dt_ * 128:(dt_ + 1) * 128], rhs=self.hT[:, kt, q * 512:(q + 1) * 512],
                            start=(kt == 0), stop=(kt == KT - 1)),
                            r=[("wu", b)] + [("hT", q * 4 + j) for j in range(4)], w=[ku])
                    si = sgi[0] % 2
                    sgi[0] += 1
                    S.op("act", lambda pa=pa, si=si: nc.scalar.activation(out=sg[si][:], in_=pa[:], func=AF.Silu),
                         r=[ka], w=[("sg", si)])
                    S.op("dve", lambda pu=pu, si=si: nc.vector.tensor_tensor(
                        out=hg[hb][:, dt_, :], in0=pu[:], in1=sg[si][:], op=ALU.mult),
                        r=[ku, ("sg", si)], w=[("hg", hb)])

            def Dn(i):
                e, q = items[i]
                b = e % 2
                hb = i % 2
                for j in range(4):
                    tt = q * 4 + j
                    for half in range(2):
                        pc, kc = self.psum()
                        for dt_ in range(4):
                            S.op("pe", lambda dt_=dt_, pc=pc: nc.tensor.matmul(
                                pc[:], lhsT=hg[hb][:, dt_, j * 128:(j + 1) * 128],
                                rhs=wd[b][:, dt_, half * 512:(half + 1) * 512], start=(dt_ == 0), stop=(dt_ == 3)),
                                r=[("hg", hb), ("wd", b)], w=[kc])
                        dst = acc[:, tt, half * 512:(half + 1) * 512]
                        if e == 0:
                            S.op("dve", lambda pc=pc, dst=dst: nc.vector.tensor_scalar(
                                out=dst, in0=pc[:], scalar1=self.gates[:, tt, e:e + 1], scalar2=None, op0=ALU.mult),
                                r=[kc, "gates"], w=[("acc", tt)])
                        else:
                            S.op("dve", lambda pc=pc, dst=dst: nc.vector.scalar_tensor_tensor(
                                out=dst, in0=pc[:], scalar=self.gates[:, tt, e:e + 1], in1=dst, op0=ALU.mult,
                                op1=ALU.add), r=[kc, "gates", ("acc", tt)], w=[("acc", tt)])

            load_w(0)
            for i in range(len(items)):
                e, q = items[i]
                G(i)
                if i >= 1:
                    Dn(i - 1)
                if q == 0 and e + 1 < int(os.environ.get('ME', NE)):
                    load_w(e + 1)
            Dn(len(items) - 1)
            self.dbg_dump("moe%d" % l, lambda o: S.dma("sp", o.rearrange("(t p) d -> p t d", p=128), acc[:],
                                                       r=[("acc", t) for t in range(NT)]))
            lnw = self.ln_alloc(st)
            h1 = [self.sb(st, "h1t", [128, D], F32) for _ in range(2)]
            dst = self.out if last else self.h_dram
            for tt in range(NT):
                s = tt % 2
                S.dma("sp", h1[s][:], self.h_dram[tt * 128:(tt + 1) * 128, :], r=[("hd", tt)], w=[("h1t", s)])
                xin, kx = self.ln_xin(lnw, tt)
                S.op("dve", lambda s=s, xin=xin: nc.vector.scalar_tensor_tensor(
                    out=xin[:], in0=h1[s][:], scalar=ALPHA, in1=acc[:, tt, :], op0=ALU.mult, op1=ALU.add),
                    r=[("h1t", s), ("acc", tt)], w=[kx])
                self.ln_tile(lnw, tt, gbc, bbc, dst, router=False)
            S.barrier()


    def hgrn(self, l):
        nc, S = self.nc, self.S
        V, A, G, PE = nc.vector, nc.scalar, nc.gpsimd, nc.tensor
        Wl = self.P["w_in"][l]
        ydst = self.yT_dram[2]
        with contextlib.ExitStack() as st:
            mask2 = self.sb(st, "mask2", [128, 128], F32)
            rm = self.sb(st, "rm", [128, T], F32)
            nw = self.sb(st, "nw", [128, 1], F32)
            lbt = self.sb(st, "lbt", [128, 8, 2], F32)
            lbv = self.sb(st, "lbv", [128, 8], F32)
            oml = self.sb(st, "oml", [128, 8], F32)
            S.op("pool", lambda: G.affine_select(out=mask2[:], in_=self.ones[:], pattern=[[1, 128]],
                                                 compare_op=ALU.is_ge, fill=0.0, base=0, channel_multiplier=-1),
                 r=["ones"], w=["mask2"])
            S.op("pool", lambda: G.memset(mask2[0:64, 64:128], 0.0), w=["mask2"])
            S.op("pool", lambda: G.memset(rm[:], 1.0), w=["rm"])
            S.op("pool", lambda: G.memset(rm[:].rearrange("p (c j) -> p c j", j=64)[:, :, 0:1], 0.0), w=["rm"])
            S.dma("sp", nw[:], self.P["hgrn_norm_w"][l].rearrange("(p o) -> p o", o=1), w=["nw"])
            if l == 0:
                S.op("pool", lambda: G.memset(lbv[:], 0.0), w=["lbv"])
                S.op("pool", lambda: G.memset(oml[:], 1.0), w=["oml"])
            else:
                for j in range(2):
                    S.dma("sp", lbt[:, :, j:j + 1],
                          self.P["hgrn_lb"][j].rearrange("(h p o) -> p h o", p=128, o=1), w=["lbt"])
                S.op("dve", lambda: V.tensor_tensor(out=lbv[:], in0=lbt[:, :, 1], in1=lbt[:, :, 0], op=ALU.subtract),
                     r=["lbt"], w=["lbv"])
                S.op("act", lambda: A.activation(out=lbv[:], in_=lbv[:], func=AF.Sigmoid), r=["lbv"], w=["lbv"])
                S.op("dve", lambda: V.tensor_scalar(out=oml[:], in0=lbv[:], scalar1=-1.0, scalar2=1.0, op0=ALU.mult,
                                                    op1=ALU.add), r=["lbv"], w=["oml"])
            w4 = [self.sb(st, "w4", [128, 4, KT, 128], BF16) for _ in range(2)]
            qs = self.sb(st, "qs", [128, T], F32)
            fs = self.sb(st, "fs", [128, T], F32)
            lf = self.sb(st, "lf", [128, T], F32)
            bc = self.sb(st, "bc", [128, T], F32)
            enb = self.sb(st, "enb", [128, T], F32)
            def two(name, shape, dt):
                return [self.sb(st, name, shape, dt) for _ in range(2)]
            qbL, kbL, gsL, ytL = two("qb", [128, T], BF16), two("kb", [128, T], BF16), two("gs", [128, T], BF16), two("yt", [128, T], BF16)
            vL, kbtL, kbtBL = two("v", [128, NT, 128], BF16), two("kbt", [128, NT, 128], BF16), two("kbtB", [128, NT, 128], BF16)
            ebL = two("ebh", [128, T], F32)
            S32L = two("S32", [128, 128], F32)
            SbfL = [[self.sb(st, "Sbf", [128, 128], BF16) for _ in range(4)] for _ in range(2)]
            attmL = [two("attm", [128, 128], BF16) for _ in range(2)]
            osbL = [two("osb", [128, 128], F32) for _ in range(2)]
            osqL = [two("osq", [128, 128], BF16) for _ in range(2)]
            sdL = [two("sd", [128, 128], F32) for _ in range(2)]
            mAB = self.sb(st, "mAB", [128, 2], F32)
            S.op("pool", lambda: G.memset(mAB[0:64, 0:1], 1.0), w=["mAB"])
            S.op("pool", lambda: G.memset(mAB[64:128, 0:1], 0.0), w=["mAB"])
            S.op("pool", lambda: G.memset(mAB[0:64, 1:2], 0.0), w=["mAB"])
            S.op("pool", lambda: G.memset(mAB[64:128, 1:2], 1.0), w=["mAB"])
            hTk = [("hT", t) for t in range(NT)]

            def load_w(h):
                b = h % 2
                for j in range(4):
                    c0 = OFF_HGRN + j * 1024 + h * 128
                    S.dma("pool", w4[b][:, j], Wl[:, c0:c0 + 128].rearrange("(kt p) n -> p kt n", p=128),
                          w=[("w4", b)])

            def prep(h):
                hb = b = h % 2
                qb, kb, gs, v, kbt, kbtB, eb = qbL[hb], kbL[hb], gsL[hb], vL[hb], kbtL[hb], kbtBL[hb], ebL[hb]
                K = lambda n: (n, hb)
                if h + 1 < 8:
                    load_w(h + 1)
                for (j, func, dst, kd) in ((0, AF.Silu, qs, "qs"), (1, AF.Sigmoid, fs, "fs"), (3, AF.Sigmoid, gs, K("gs"))):
                    for tq in range(4):
                        pb, kp = self.psum()
                        for kt in range(KT):
                            S.op("pe", lambda kt=kt, pb=pb, j=j, tq=tq: PE.matmul(
                                pb[:], lhsT=w4[b][:, j, kt, :], rhs=self.hT[:, kt, tq * 512:(tq + 1) * 512],
                                start=(kt == 0), stop=(kt == KT - 1)), r=[("w4", b)] + hTk[tq * 4:tq * 4 + 4], w=[kp])
                        S.op("act", lambda pb=pb, dst=dst, func=func, tq=tq: A.activation(
                            out=dst[:, tq * 512:(tq + 1) * 512], in_=pb[:], func=func), r=[kp], w=[kd, kp])
                for t4 in range(4):
                    pb, kp = self.psum()
                    for j4 in range(4):
                        tt = t4 * 4 + j4
                        for kt in range(KT):
                            S.op("pe", lambda kt=kt, pb=pb, j4=j4, tt=tt: PE.matmul(
                                pb[:, j4 * 128:(j4 + 1) * 128], lhsT=self.hT[:, kt, tt * 128:(tt + 1) * 128],
                                rhs=w4[b][:, 2, kt, :], start=(kt == 0), stop=(kt == KT - 1)),
                                r=[("w4", b), ("hT", tt)], w=[kp])
                    S.op("dve", lambda pb=pb, t4=t4: V.tensor_copy(
                        out=v[:, t4 * 4:(t4 + 1) * 4, :], in_=pb[:].rearrange("p (a b) -> p a b", a=4)),
                        r=[kp], w=[K("v"), kp])
                S.op("dve", lambda: V.tensor_scalar(out=fs[:], in0=fs[:], scalar1=oml[:, h:h + 1], scalar2=lbv[:, h:h + 1],
                                                    op0=ALU.mult, op1=ALU.add), r=["fs", "oml", "lbv"], w=["fs"])
                S.op("act", lambda: A.activation(out=lf[:], in_=fs[:], func=AF.Ln), r=["fs"], w=["lf"])
                S.op("dve", lambda: V.tensor_tensor_scan(out=bc[:], data0=rm[:], data1=lf[:], initial=0.0,
                                                         op0=ALU.mult, op1=ALU.add), r=["rm", "lf"], w=["bc"])
                S.op("act", lambda: A.activation(out=eb[:], in_=bc[:], func=AF.Exp), r=["bc"], w=[K("eb")])
                S.op("act", lambda: A.activation(out=enb[:], in_=bc[:], func=AF.Exp, scale=-1.0), r=["bc"], w=["enb"])
                S.op("dve", lambda: V.tensor_scalar(out=fs[:], in0=fs[:], scalar1=-1.0, scalar2=1.0, op0=ALU.mult,
                                                    op1=ALU.add), r=["fs", "lf"], w=["fs"])
                S.op("pool", lambda: G.tensor_tensor(out=qb[:], in0=qs[:], in1=eb[:], op=ALU.mult),
                     r=["qs", K("eb")], w=[K("qb")])
                S.op("dve", lambda: V.tensor_tensor(out=kb[:], in0=fs[:], in1=enb[:], op=ALU.mult),
                     r=["fs", "enb"], w=[K("kb")])
                for t4 in range(4):
                    pb, kp = self.psum()
                    pbv = pb[:].bitcast(BF16)
                    for j4 in range(4):
                        tt = t4 * 4 + j4
                        S.op("pe", lambda pbv=pbv, j4=j4, tt=tt: PE.transpose(
                            out=pbv[:, j4 * 128:(j4 + 1) * 128], in_=kb[:, tt * 128:(tt + 1) * 128],
                            identity=self.identbf[:]), r=[K("kb"), "identbf"], w=[kp])
                    S.op("dve", lambda pbv=pbv, t4=t4: V.tensor_scalar(
                        out=kbt[:, t4 * 4:(t4 + 1) * 4, :], in0=pbv[:, 0:512].rearrange("p (a b) -> p a b", a=4),
                        scalar1=mAB[:, 0:1], scalar2=None, op0=ALU.mult), r=[kp, "mAB"], w=[K("kbt"), kp])
                    S.op("dve", lambda pbv=pbv, t4=t4: V.tensor_scalar(
                        out=kbtB[:, t4 * 4:(t4 + 1) * 4, :], in0=pbv[:, 0:512].rearrange("p (a b) -> p a b", a=4),
                        scalar1=mAB[:, 1:2], scalar2=None, op0=ALU.mult), r=[kp, "mAB"], w=[K("kbtB"), kp])
                S.op("pool", lambda: G.memset(S32L[hb][:], 0.0), w=[K("S32")])
                S.op("pool", lambda: G.memset(SbfL[hb][0][:], 0.0), w=[("Sbf", hb, 0)])

            def tile(h, tt):
                hb = h % 2
                qb, kb, gs, yt, v, kbt, kbtB, eb = qbL[hb], kbL[hb], gsL[hb], ytL[hb], vL[hb], kbtL[hb], kbtBL[hb], ebL[hb]
                S32, Sbf = S32L[hb], SbfL[hb]
                K = lambda n: (n, hb)
                cA, cB = 2 * tt, 2 * tt + 1
                tsl = slice(tt * 128, (tt + 1) * 128)
                i2 = tt % 2
                attm, osb, osq, sd = attmL[hb][i2], osbL[hb][i2], osqL[hb][i2], sdL[hb][i2]
                ka, ko, kq, ks = ("attm", hb, i2), ("osb", hb, i2), ("osq", hb, i2), ("sd", hb, i2)
                pa, kpa = self.psum()
                S.op("pe", lambda: PE.matmul(pa[:, 0:128], lhsT=kb[:, tsl], rhs=qb[:, tsl], start=True, stop=True),
                     r=[K("kb"), K("qb")], w=[kpa])
                yield
                S.op("dve", lambda: V.tensor_tensor(out=attm[:], in0=pa[:, 0:128], in1=mask2[:], op=ALU.mult),
                     r=[kpa, "mask2"], w=[ka, kpa])
                yield
                pr, kpr = self.psum()
                S.op("pe", lambda: PE.matmul(pr[:, 0:128], lhsT=kbt[:, tt, :], rhs=v[:, tt, :], start=True, stop=True),
                     r=[K("kbt"), K("v")], w=[kpr])
                S.op("pe", lambda: PE.matmul(pr[:, 128:256], lhsT=kbtB[:, tt, :], rhs=v[:, tt, :], start=True, stop=True),
                     r=[K("kbtB"), K("v")], w=[kpr])
                yield
                for (ci, off) in ((cA, 0), (cB, 128)):
                    S.op("dve", lambda off=off: V.tensor_tensor(
                        out=S32[:], in0=pr[:, off:off + 128], in1=S32[:], op=ALU.add), r=[kpr, K("S32")], w=[K("S32"), kpr])
                    S.op("dve", lambda ci=ci: V.tensor_scalar(
                        out=S32[:], in0=S32[:], scalar1=eb[:, ci * 64 + 63:ci * 64 + 64], scalar2=None, op0=ALU.mult),
                        r=[K("S32"), K("eb")], w=[K("S32")])
                    S.op("pool", lambda ci=ci: G.tensor_copy(out=Sbf[(ci + 1) % 4][:], in_=S32[:]),
                         r=[K("S32")], w=[("Sbf", hb, (ci + 1) % 4)])
                    yield
                po, kpo = self.psum()
                S.op("pe", lambda: PE.matmul(po[:, 0:128], lhsT=v[:, tt, :], rhs=attm[:], start=True, stop=False),
                     r=[K("v"), ka], w=[kpo])
                S.op("pe", lambda: PE.matmul(po[:, 0:64], lhsT=Sbf[cA % 4][:], rhs=qb[:, tt * 128:tt * 128 + 64],
                                             start=False, stop=False), r=[("Sbf", hb, cA % 4), K("qb")], w=[kpo])
                S.op("pe", lambda: PE.matmul(po[:, 64:128], lhsT=Sbf[cB % 4][:], rhs=qb[:, tt * 128 + 64:(tt + 1) * 128],
                                             start=False, stop=True), r=[("Sbf", hb, cB % 4), K("qb")], w=[kpo])
                yield
                S.op("act", lambda: A.activation(out=osb[:], in_=po[:, 0:128], func=AF.Copy), r=[kpo], w=[ko, kpo])
                S.op("act", lambda: A.activation(out=osq[:], in_=po[:, 0:128], func=AF.Square), r=[kpo], w=[kq, kpo])
                yield
                pss, kps = self.psum()
                S.op("pe", lambda: PE.matmul(pss[:, 0:128], lhsT=self.onesbf[:], rhs=osq[:], start=True, stop=True),
                     r=["onesbf", kq], w=[kps])
                yield
                S.op("act", lambda: A.activation(out=sd[:], in_=pss[:, 0:128], func=AF.Sqrt, bias=self.epsc[:, 1:2],
                                                 scale=1.0 / 128.0), r=[kps, "epsc"], w=[ks, kps])
                yield
                S.op("dve", lambda: V.reciprocal(out=sd[:], in_=sd[:]), r=[ks], w=[ks])
                yield
                S.op("dve", lambda: V.scalar_tensor_tensor(out=osb[:], in0=osb[:], scalar=nw[:, 0:1], in1=sd[:],
                                                           op0=ALU.mult, op1=ALU.mult), r=[ko, ks, "nw"], w=[ko])
                S.op("dve", lambda: V.tensor_tensor(out=yt[:, tsl], in0=osb[:], in1=gs[:, tsl], op=ALU.mult),
                     r=[ko, K("gs")], w=[K("yt")])

            load_w(0)
            for hp in range(4):
                prep(2 * hp)
                prep(2 * hp + 1)
                for tt in range(NT):
                    gens = [tile(2 * hp, tt), tile(2 * hp + 1, tt)]
                    while gens:
                        for g_ in list(gens):
                            try:
                                next(g_)
                            except StopIteration:
                                gens.remove(g_)
                for hb in range(2):
                    h = 2 * hp + hb
                    S.dma("sp", ydst[h * 128:(h + 1) * 128, :], ytL[hb][:], r=[("yt", hb)], w=[("yT2", h)])
            S.barrier()

    def ssd(self, l):
        nc, S = self.nc, self.S
        V, A, G, PE = nc.vector, nc.scalar, nc.gpsimd, nc.tensor
        Wl = self.P["w_in"][l]
        ydst = self.yT_dram[0]
        NEG = -30000.0
        hTk = [("hT", t) for t in range(NT)]
        with contextlib.ExitStack() as st:
            tri2 = self.sb(st, "tri2", [128, 128], F32)
            same2 = self.sb(st, "same2", [128, 128], F32)
            indA = self.sb(st, "indA", [128, 128], F32)
            indB = self.sb(st, "indB", [128, 128], F32)
            mAB = self.sb(st, "mABs", [128, 2], F32)
            negmask = self.sb(st, "negmask", [128, 8, 128], F32)
            bd1 = self.sb(st, "bd", [16, 8, 128], F32)
            bd = [bd1, bd1]
            S.op("pool", lambda: G.affine_select(out=tri2[:], in_=self.ones[:], pattern=[[1, 128]], compare_op=ALU.is_ge,
                                                 fill=0.0, base=0, channel_multiplier=-1), r=["ones"], w=["tri2"])
            S.op("pool", lambda: G.memset(tri2[0:64, 64:128], 0.0), w=["tri2"])
            S.op("pool", lambda: G.memset(same2[:], 0.0), w=["same2"])
            S.op("pool", lambda: G.memset(same2[0:64, 0:64], 1.0), w=["same2"])
            S.op("pool", lambda: G.memset(same2[64:128, 64:128], 1.0), w=["same2"])
            S.op("pool", lambda: G.memset(indA[0:64, :], 1.0), w=["indA"])
            S.op("pool", lambda: G.memset(indA[64:128, :], 0.0), w=["indA"])
            S.op("pool", lambda: G.memset(indB[0:64, :], 0.0), w=["indB"])
            S.op("pool", lambda: G.memset(indB[64:128, :], 1.0), w=["indB"])
            S.op("pool", lambda: G.memset(mAB[0:64, 0:1], 1.0), w=["mAB"])
            S.op("pool", lambda: G.memset(mAB[64:128, 0:1], 0.0), w=["mAB"])
            S.op("pool", lambda: G.memset(mAB[0:64, 1:2], 0.0), w=["mAB"])
            S.op("pool", lambda: G.memset(mAB[64:128, 1:2], 1.0), w=["mAB"])
            S.op("pool", lambda: G.memset(negmask[:], 0.0), w=["negmask"])
            S.op("pool", lambda: G.affine_select(out=negmask[:], in_=negmask[:], pattern=[[0, 8], [1, 128]],
                                                 compare_op=ALU.is_ge, fill=NEG, base=0, channel_multiplier=-1),
                 r=["negmask"], w=["negmask"])
            S.op("pool", lambda: G.memset(negmask[0:64, :, 64:128], NEG), w=["negmask"])
            dtb = self.sb(st, "dtb", [128, 16], F32)
            alog = self.sb(st, "alog", [128, 16], F32)
            dsk = self.sb(st, "dsk", [128, 16], F32)
            nwbc = self.sb(st, "nwbc", [128, D], F32)
            S.dma("sp", dtb[:], self.P["ssd_dt_bias"][l].partition_broadcast(128), w=["dtb"])
            S.dma("sp", alog[:], self.P["ssd_a_log"][l].partition_broadcast(128), w=["alog"])
            S.dma("sp", dsk[:], self.P["ssd_d"][l].partition_broadcast(128), w=["dsk"])
            S.dma("sp", nwbc[:], self.P["ssd_norm_w"][l].partition_broadcast(128), w=["nwbc"])
            S.op("act", lambda: A.activation(out=alog[:], in_=alog[:], func=AF.Exp), r=["alog"], w=["alog"])
            S.op("dve", lambda: V.tensor_scalar(out=alog[:], in0=alog[:], scalar1=-1.0, scalar2=None, op0=ALU.mult),
                 r=["alog"], w=["alog"])
            wdt = self.sb(st, "wdt", [128, KT, 16], BF16)
            S.dma("pool", wdt[:], Wl[:, OFF_DT:OFF_DT + 16].rearrange("(kt p) n -> p kt n", p=128), w=["wdt"])
            dt = self.sb(st, "dt", [128, NT, 16], F32)
            da = self.sb(st, "da", [128, NT, 16], F32)
            cum4 = self.sb(st, "cum4", [128, NT, 4, 16], F32)
            eacs = self.sb(st, "eacs", [128, NT, 16], F32)
            eend = self.sb(st, "eend", [128, NT, 16], F32)
            edec = self.sb(st, "edec", [128, NT, 2, 16], F32)
            acsTt = [self.sb(st, "acsTt", [16, 128], F32) for _ in range(2)]
            nacsTt = [self.sb(st, "nacsTt", [16, 128], F32) for _ in range(2)]
            pb, kp = self.psum()
            for tt in range(NT):
                for kt in range(KT):
                    S.op("pe", lambda kt=kt, tt=tt: PE.matmul(pb[:, tt * 16:(tt + 1) * 16], lhsT=self.hT[:, kt, tt * 128:(tt + 1) * 128],
                                                              rhs=wdt[:, kt, :], start=(kt == 0), stop=(kt == KT - 1)),
                         r=["wdt", ("hT", tt)], w=[kp])
            S.op("dve", lambda: V.tensor_tensor(out=dt[:], in0=pb[:, 0:256].rearrange("p (t h) -> p t h", h=16),
                                                in1=dtb[:].unsqueeze(1).to_broadcast([128, NT, 16]), op=ALU.add),
                 r=[kp, "dtb"], w=["dt", kp])
            S.op("act", lambda: A.activation(out=dt[:], in_=dt[:], func=AF.Exp), r=["dt"], w=["dt"])
            S.op("act", lambda: A.activation(out=dt[:], in_=dt[:], func=AF.Ln, bias=self.epsc[:, 3:4], scale=1.0),
                 r=["dt", "epsc"], w=["dt"])
            S.op("dve", lambda: V.tensor_tensor(out=da[:], in0=dt[:], in1=alog[:].unsqueeze(1).to_broadcast([128, NT, 16]),
                                                op=ALU.mult), r=["dt", "alog"], w=["da"])
            for half in range(2):
                pb, kp = self.psum()
                for j in range(8):
                    tt = half * 8 + j
                    for qi, L in enumerate((tri2, same2, indA, indB)):
                        S.op("pe", lambda j=j, qi=qi, L=L, tt=tt, pb=pb: PE.matmul(
                            pb[:, j * 64 + qi * 16:j * 64 + (qi + 1) * 16], lhsT=L[:], rhs=da[:, tt, :], start=True, stop=True),
                            r=["da", "tri2", "same2", "indA", "indB"], w=[kp])
                S.op("dve", lambda pb=pb, half=half: V.tensor_copy(
                    out=cum4[:, half * 8:(half + 1) * 8].rearrange("p t q h -> p (t q h)"), in_=pb[:]),
                    r=[kp], w=["cum4", kp])
            S.op("act", lambda: A.activation(out=eacs[:], in_=cum4[:, :, 0, :], func=AF.Exp), r=["cum4"], w=["eacs"])
            S.op("dve", lambda: V.tensor_tensor(out=eend[:], in0=cum4[:, :, 1, :], in1=cum4[:, :, 0, :], op=ALU.subtract),
                 r=["cum4"], w=["eend"])
            S.op("act", lambda: A.activation(out=eend[:], in_=eend[:], func=AF.Exp), r=["eend"], w=["eend"])
            S.op("act", lambda: A.activation(out=edec[:], in_=cum4[:, :, 2:4, :], func=AF.Exp), r=["cum4"], w=["edec"])
            wx = self.sb(st, "wx", [128, KT, 768], BF16)
            wz = self.sb(st, "wz", [128, KT, 512], BF16)
            cw = self.sb(st, "cw", [128, 6, 4], F32)
            cbi = self.sb(st, "cbi", [128, 6], F32)
            xp1 = self.sb(st, "xp", [128, T + 3], F32)
            xp = [xp1, xp1]
            fTa = [self.sb(st, "fT", [128, T], BF16) for _ in range(4)]
            fT = [fTa[0], fTa[1], fTa[0], fTa[1], fTa[2], fTa[3]]
            fk = [("fT", 0), ("fT", 1), ("fT", 0), ("fT", 1), ("fT", 2), ("fT", 3)]
            cmTA = self.sb(st, "cmTA", [128, T], BF16)
            cmTB = self.sb(st, "cmTB", [128, T], BF16)
            xs = self.sb(st, "xs", [128, NT, 512], BF16)
            xdtt = [self.sb(st, "xdtt", [128, 512], BF16) for _ in range(2)]
            xendt = [self.sb(st, "xendt", [128, 512], BF16) for _ in range(2)]
            bmA = self.sb(st, "bmA", [128, NT, 128], BF16)
            bmB = self.sb(st, "bmB", [128, NT, 128], BF16)
            yTg = self.sb(st, "yTg", [128, 4, T], BF16)
            S32 = self.sb(st, "S32s", [128, 512], F32)
            Sbf = [self.sb(st, "Sbfs", [128, 512], BF16) for _ in range(4)]
            cbs = [self.sb(st, "cbs", [128, 128], BF16) for _ in range(2)]
            Dx = [self.sb(st, "Dx", [16, 8, 128], F32) for _ in range(2)]
            Es = [self.sb(st, "Es", [128, 8, 128], BF16) for _ in range(2)]
            wT = [self.sb(st, "wT", [128, 8, 128], BF16) for _ in range(2)]
            t1 = [self.sb(st, "t1", [128, 512], F32) for _ in range(2)]
            t2 = [self.sb(st, "t2", [128, 512], F32) for _ in range(2)]
            zs = [self.sb(st, "zs", [128, 512], BF16) for _ in range(2)]
            ytm = [self.sb(st, "ytm", [128, 512], BF16) for _ in range(2)]
            ss = [self.sb(st, "ss", [128, 2], F32) for _ in range(2)]
            S.op("pool", lambda: G.memset(xp[0][:, 0:3], 0.0), w=[("xp", 0)])
            for g in range(2):
                S.op("pool", lambda g=g: G.memset(bd[g][:], 1.0), r=[("bd", 0), ("bd", 1)], w=[("bd", 0), ("bd", 1)])
                S.op("pool", lambda g=g: G.affine_select(out=bd[g][:], in_=bd[g][:], pattern=[[1, 8], [0, 128]],
                                                         compare_op=ALU.is_equal, fill=0.0, base=8 * g,
                                                         channel_multiplier=-1), r=[("bd", 0), ("bd", 1)], w=[("bd", 0), ("bd", 1)])
                choff = [g * 512 + i * 128 for i in range(4)] + [1024 + g * 128, 1280 + g * 128]
                S.dma("pool", wx[:, :, 0:512], Wl[:, OFF_XBC + g * 512:OFF_XBC + (g + 1) * 512].rearrange("(kt p) n -> p kt n", p=128), w=["wx"])
                S.dma("pool", wx[:, :, 512:640], Wl[:, OFF_XBC + 1024 + g * 128:OFF_XBC + 1024 + (g + 1) * 128].rearrange("(kt p) n -> p kt n", p=128), w=["wx"])
                S.dma("pool", wx[:, :, 640:768], Wl[:, OFF_XBC + 1280 + g * 128:OFF_XBC + 1280 + (g + 1) * 128].rearrange("(kt p) n -> p kt n", p=128), w=["wx"])
                S.dma("pool", wz[:], Wl[:, OFF_Z + g * 512:OFF_Z + (g + 1) * 512].rearrange("(kt p) n -> p kt n", p=128), w=["wz"])
                for ci in range(6):
                    for j in range(4):
                        S.dma("sp", cw[:, ci, j:j + 1], self.P["ssd_conv_w"][l, j, choff[ci]:choff[ci] + 128].rearrange("(p o) -> p o", o=1), w=["cw"])
                    S.dma("sp", cbi[:, ci:ci + 1], self.P["ssd_conv_b"][l, choff[ci]:choff[ci] + 128].rearrange("(p o) -> p o", o=1), w=["cbi"])
                for ci in range(6):
                    xb = xp[0]
                    kx = ("xp", 0)
                    for tq in range(4):
                        pb, kp = self.psum()
                        for kt in range(KT):
                            S.op("pe", lambda kt=kt, pb=pb, ci=ci, tq=tq: PE.matmul(
                                pb[:], lhsT=wx[:, kt, ci * 128:(ci + 1) * 128], rhs=self.hT[:, kt, tq * 512:(tq + 1) * 512],
                                start=(kt == 0), stop=(kt == KT - 1)), r=["wx"] + hTk[tq * 4:tq * 4 + 4], w=[kp])
                        S.op("act", lambda pb=pb, xb=xb, tq=tq: A.activation(out=xb[:, 3 + tq * 512:3 + (tq + 1) * 512], in_=pb[:],
                                                                            func=AF.Copy), r=[kp], w=[kx, kp])
                    acc = t1[0] if False else None
                    cacc = self.sb(st, "cacc", [128, T], F32) if (g == 0 and ci == 0) else self._cacc
                    self._cacc = cacc
                    S.op("dve", lambda xb=xb, ci=ci, cacc=cacc: V.tensor_scalar(
                        out=cacc[:], in0=xb[:, 3:3 + T], scalar1=cw[:, ci, 3:4], scalar2=cbi[:, ci:ci + 1], op0=ALU.mult,
                        op1=ALU.add), r=[kx, "cw", "cbi"], w=["cacc"])
                    for j in range(3):
                        S.op("dve", lambda xb=xb, ci=ci, j=j, cacc=cacc: V.scalar_tensor_tensor(
                            out=cacc[:], in0=xb[:, j:j + T], scalar=cw[:, ci, j:j + 1], in1=cacc[:], op0=ALU.mult, op1=ALU.add),
                            r=[kx, "cw", "cacc"], w=["cacc"])
                    S.op("act", lambda ci=ci, cacc=cacc: A.activation(out=fT[ci][:], in_=cacc[:], func=AF.Silu),
                         r=["cacc"], w=[fk[ci]])
                    if ci < 5:
                        for t4 in range(4):
                            pb, kp = self.psum()
                            pbv = pb[:].bitcast(BF16)
                            for j in range(4):
                                tt = t4 * 4 + j
                                S.op("pe", lambda j=j, tt=tt, pbv=pbv, ci=ci: PE.transpose(
                                    out=pbv[:, j * 128:(j + 1) * 128], in_=fT[ci][:, tt * 128:(tt + 1) * 128],
                                    identity=self.identbf[:]), r=[fk[ci], "identbf"], w=[kp])
                            src = pbv[:, 0:512].rearrange("p (a b) -> p a b", a=4)
                            if ci < 4:
                                S.op("act", lambda t4=t4, src=src, ci=ci: A.activation(
                                    out=xs[:, t4 * 4:(t4 + 1) * 4, ci * 128:(ci + 1) * 128], in_=src, func=AF.Copy),
                                    r=[kp], w=["xs", kp])
                            else:
                                S.op("dve", lambda t4=t4, src=src: V.tensor_scalar(
                                    out=bmA[:, t4 * 4:(t4 + 1) * 4, :], in0=src, scalar1=mAB[:, 0:1], scalar2=None, op0=ALU.mult),
                                    r=[kp, "mAB"], w=["bmA", kp])
                                S.op("dve", lambda t4=t4, src=src: V.tensor_scalar(
                                    out=bmB[:, t4 * 4:(t4 + 1) * 4, :], in0=src, scalar1=mAB[:, 1:2], scalar2=None, op0=ALU.mult),
                                    r=[kp, "mAB"], w=["bmB", kp])
                bmT, cmT = fT[4], fT[5]
                cv = cmT[:].rearrange("p (t c j) -> p t c j", c=2, j=64)
                cva = cmTA[:].rearrange("p (t c j) -> p t c j", c=2, j=64)
                cvb = cmTB[:].rearrange("p (t c j) -> p t c j", c=2, j=64)
                S.op("pool", lambda: G.tensor_copy(out=cva[:, :, 0, :], in_=cv[:, :, 0, :]), r=[("fT", 3)], w=["cmTA"])
                S.op("pool", lambda: G.memset(cva[:, :, 1, :], 0.0), w=["cmTA"])
                S.op("pool", lambda: G.tensor_copy(out=cvb[:, :, 1, :], in_=cv[:, :, 1, :]), r=[("fT", 3)], w=["cmTB"])
                S.op("pool", lambda: G.memset(cvb[:, :, 0, :], 0.0), w=["cmTB"])
                hs = slice(g * 8, (g + 1) * 8)
                S.op("pool", lambda: G.memset(S32[:], 0.0), w=["S32"])
                S.op("pool", lambda: G.memset(Sbf[0][:], 0.0), w=[("Sbf", 0)])
                def tile_gen(tt):
                    i2 = tt % 2
                    tsl = slice(tt * 128, (tt + 1) * 128)
                    cA, cB = 2 * tt, 2 * tt + 1
                    pc, kpc = self.psum()
                    S.op("pe", lambda pc=pc: PE.matmul(pc[:, 0:128], lhsT=bmT[:, tsl], rhs=cmT[:, tsl], start=True, stop=True),
                         r=[("fT", 2), ("fT", 3)], w=[kpc])
                    S.op("act", lambda pc=pc: A.activation(out=cbs[i2][:], in_=pc[:, 0:128], func=AF.Copy),
                         r=[kpc], w=[("cbs", i2), kpc])
                    pq, kpq = self.psum()
                    S.op("pe", lambda pq=pq: PE.matmul(pq[0:16, 0:128], lhsT=da[:, tt, :], rhs=tri2[:], start=True, stop=True),
                         r=["da", "tri2"], w=[kpq])
                    S.op("dve", lambda pq=pq: V.tensor_copy(out=acsTt[i2][:], in_=pq[0:16, 0:128]), r=[kpq], w=[("acsTt", i2), kpq])
                    S.op("dve", lambda pq=pq: V.tensor_scalar(out=nacsTt[i2][:], in0=pq[0:16, 0:128], scalar1=-1.0, scalar2=None,
                                                              op0=ALU.mult), r=[kpq], w=[("nacsTt", i2), kpq])
                    S.op("pool", lambda: G.tensor_tensor(out=Dx[i2][:], in0=bd[g][:],
                                                         in1=acsTt[i2][:].unsqueeze(1).to_broadcast([16, 8, 128]), op=ALU.mult),
                         r=[("bd", g), ("acsTt", i2)], w=[("Dx", i2)])
                    yield
                    for hh in range(2):
                        pe_, kpe = self.psum()
                        csl = slice(hh * 512, (hh + 1) * 512)
                        S.op("pe", lambda pe_=pe_, csl=csl: PE.matmul(
                            pe_[:], lhsT=self.ones[0:16, :], rhs=Dx[i2][:].rearrange("p h l -> p (h l)")[:, csl],
                            start=True, stop=False), r=["ones", ("Dx", i2)], w=[kpe])
                        S.op("pe", lambda pe_=pe_, csl=csl: PE.matmul(
                            pe_[:], lhsT=nacsTt[i2][:], rhs=bd[g][:].rearrange("p h l -> p (h l)")[:, csl],
                            start=False, stop=False), r=[("nacsTt", i2), ("bd", g)], w=[kpe])
                        S.op("pe", lambda pe_=pe_, csl=csl: PE.matmul(
                            pe_[:], lhsT=self.ident32[:], rhs=negmask[:].rearrange("p h l -> p (h l)")[:, csl],
                            start=False, stop=True), r=["ident32", "negmask"], w=[kpe])
                        S.op("act", lambda pe_=pe_, hh=hh: A.activation(
                            out=Es[i2][:, hh * 4:(hh + 1) * 4, :], in_=pe_[:].rearrange("p (h l) -> p h l", h=4), func=AF.Exp),
                            r=[kpe], w=[("Es", i2), kpe])
                    S.op("dve", lambda: V.tensor_tensor(out=wT[i2][:], in0=Es[i2][:],
                                                        in1=cbs[i2][:].unsqueeze(1).to_broadcast([128, 8, 128]), op=ALU.mult),
                         r=[("Es", i2), ("cbs", i2)], w=[("wT", i2)])
                    yield
                    pr, kpr = self.psum()
                    pr2, kpr2 = self.psum()
                    S.op("pool", lambda: G.tensor_tensor(out=xdtt[i2][:].rearrange("p (h c) -> p h c", c=64),
                                                         in0=xs[:, tt, :].rearrange("p (h c) -> p h c", c=64),
                                                         in1=dt[:, tt, hs].unsqueeze(2).to_broadcast([128, 8, 64]), op=ALU.mult),
                         r=["xs", "dt"], w=[("xdtt", i2)])
                    S.op("pool", lambda: G.tensor_tensor(out=xendt[i2][:].rearrange("p (h c) -> p h c", c=64),
                                                         in0=xdtt[i2][:].rearrange("p (h c) -> p h c", c=64),
                                                         in1=eend[:, tt, hs].unsqueeze(2).to_broadcast([128, 8, 64]), op=ALU.mult),
                         r=[("xdtt", i2), "eend"], w=[("xendt", i2)])
                    S.op("pe", lambda pr=pr: PE.matmul(pr[:], lhsT=bmA[:, tt, :], rhs=xendt[i2][:], start=True, stop=True),
                         r=["bmA", ("xendt", i2)], w=[kpr])
                    S.op("pe", lambda pr2=pr2: PE.matmul(pr2[:], lhsT=bmB[:, tt, :], rhs=xendt[i2][:], start=True, stop=True),
                         r=["bmB", ("xendt", i2)], w=[kpr2])
                    for (ci_, prx, kprx, cc) in ((cA, pr, kpr, 0), (cB, pr2, kpr2, 1)):
                        S.op("dve", lambda cc=cc: V.tensor_tensor(
                            out=S32[:].rearrange("p (h c) -> p h c", c=64), in0=S32[:].rearrange("p (h c) -> p h c", c=64),
                            in1=edec[:, tt, cc, hs].unsqueeze(2).to_broadcast([128, 8, 64]), op=ALU.mult),
                            r=["S32", "edec"], w=["S32"])
                        S.op("dve", lambda prx=prx: V.tensor_tensor(out=S32[:], in0=prx[:], in1=S32[:], op=ALU.add),
                             r=[kprx, "S32"], w=["S32", kprx])
                        S.op("pool", lambda ci_=ci_: G.tensor_copy(out=Sbf[(ci_ + 1) % 4][:], in_=S32[:]),
                             r=["S32"], w=[("Sbf", (ci_ + 1) % 4)])
                    yield
                    pz, kpz = self.psum()
                    for kt in range(KT):
                        S.op("pe", lambda kt=kt, pz=pz: PE.matmul(pz[:], lhsT=self.hT[:, kt, tsl], rhs=wz[:, kt, :],
                                                                  start=(kt == 0), stop=(kt == KT - 1)), r=["wz", ("hT", tt)], w=[kpz])
                    S.op("act", lambda pz=pz: A.activation(out=zs[i2][:], in_=pz[:], func=AF.Silu), r=[kpz], w=[("zs", i2), kpz])
                    yield
                    py, kpy = self.psum()
                    for hh in range(8):
                        S.op("pe", lambda hh=hh, py=py: PE.matmul(py[:, hh * 64:(hh + 1) * 64], lhsT=wT[i2][:, hh, :],
                                                                  rhs=xdtt[i2][:, hh * 64:(hh + 1) * 64], start=True, stop=True),
                             r=[("wT", i2), ("xdtt", i2)], w=[kpy])
                    po, kpo = self.psum()
                    S.op("pe", lambda po=po: PE.matmul(po[:], lhsT=cmTA[:, tsl], rhs=Sbf[cA % 4][:], start=True, stop=False),
                         r=["cmTA", ("Sbf", cA % 4)], w=[kpo])
                    S.op("pe", lambda po=po: PE.matmul(po[:], lhsT=cmTB[:, tsl], rhs=Sbf[cB % 4][:], start=False, stop=True),
                         r=["cmTB", ("Sbf", cB % 4)], w=[kpo])
                    S.op("dve", lambda po=po: V.tensor_tensor(
                        out=t1[i2][:].rearrange("p (h c) -> p h c", c=64), in0=po[:].rearrange("p (h c) -> p h c", c=64),
                        in1=eacs[:, tt, hs].unsqueeze(2).to_broadcast([128, 8, 64]), op=ALU.mult),
                        r=[kpo, "eacs"], w=[("t1", i2), kpo])
                    S.op("dve", lambda py=py: V.tensor_tensor(out=t1[i2][:], in0=py[:], in1=t1[i2][:], op=ALU.add),
                         r=[kpy, ("t1", i2)], w=[("t1", i2), kpy])
                    S.op("pool", lambda: G.tensor_tensor(
                        out=t2[i2][:].rearrange("p (h c) -> p h c", c=64), in0=xs[:, tt, :].rearrange("p (h c) -> p h c", c=64),
                        in1=dsk[:, hs].unsqueeze(2).to_broadcast([128, 8, 64]), op=ALU.mult), r=["xs", "dsk"], w=[("t2", i2)])
                    S.op("pool", lambda: G.tensor_tensor(out=t2[i2][:], in0=t2[i2][:], in1=t1[i2][:], op=ALU.add),
                         r=[("t2", i2), ("t1", i2)], w=[("t2", i2)])
                    S.op("pool", lambda: G.tensor_tensor(out=t2[i2][:], in0=t2[i2][:], in1=zs[i2][:], op=ALU.mult),
                         r=[("t2", i2), ("zs", i2)], w=[("t2", i2)])
                    yield
                    S.op("act", lambda: A.activation(out=t1[i2][:], in_=t2[i2][:], func=AF.Square, accum_out=ss[i2][:, 0:1]),
                         r=[("t2", i2)], w=[("t1", i2), ("ss", i2)])
                    S.op("act", lambda: A.activation(out=ss[i2][:, 1:2], in_=ss[i2][:, 0:1], func=AF.Sqrt, bias=self.epsc[:, 1:2],
                                                     scale=1.0 / 512.0), r=[("ss", i2), "epsc"], w=[("ss", i2)])
                    S.op("dve", lambda: V.reciprocal(out=ss[i2][:, 1:2], in_=ss[i2][:, 1:2]), r=[("ss", i2)], w=[("ss", i2)])
                    S.op("dve", lambda: V.scalar_tensor_tensor(out=ytm[i2][:], in0=t2[i2][:], scalar=ss[i2][:, 1:2],
                                                               in1=nwbc[:, g * 512:(g + 1) * 512], op0=ALU.mult, op1=ALU.mult),
                         r=[("t2", i2), ("ss", i2), "nwbc"], w=[("ytm", i2)])
                    yield
                    pt, kpt = self.psum()
                    ptv = pt[:].bitcast(BF16)
                    for i in range(4):
                        S.op("pe", lambda i=i, ptv=ptv: PE.transpose(out=ptv[:, i * 128:(i + 1) * 128],
                                                                     in_=ytm[i2][:, i * 128:(i + 1) * 128], identity=self.identbf[:]),
                             r=[("ytm", i2), "identbf"], w=[kpt])
                    S.op("act", lambda ptv=ptv: A.activation(out=yTg[:, :, tsl], in_=ptv[:, 0:512].rearrange("p (a b) -> p a b", a=4),
                                                             func=AF.Copy), r=[kpt], w=["yTg", kpt])

                gens, nxt, rnd = [], 0, 0
                while nxt < NT or gens:
                    if nxt < NT and rnd % 4 == 0:
                        gens.append(tile_gen(nxt))
                        nxt += 1
                    for g_ in list(gens):
                        try:
                            next(g_)
                        except StopIteration:
                            gens.remove(g_)
                    rnd += 1
                for i in range(4):
                    S.dma("sp", ydst[g * 512 + i * 128:g * 512 + (i + 1) * 128, :], yTg[:, i, :], r=["yTg"], w=[("yT0", g * 4 + i)])
            S.barrier()


    def rwkv(self, l):
        nc, S = self.nc, self.S
        V, A, G, PE = nc.vector, nc.scalar, nc.gpsimd, nc.tensor
        Wl = self.P["w_in"][l]
        ydst = self.yT_dram[1]
        hTk = [("hT", t) for t in range(NT)]
        P_ = self.P

        def mm(out, lhsT, rhs, start, stop, r, w):
            S.op("pe", lambda: PE.matmul(out, lhsT=lhsT, rhs=rhs, start=start, stop=stop), r=r, w=w)

        with contextlib.ExitStack() as st:
            mask4 = self.sb(st, "mask4", [128, 4, 128], F32)
            maskL = self.sb(st, "maskL", [128, 2, 128], F32)
            bdm = self.sb(st, "bdm", [128, 128], F32)
            mEO = self.sb(st, "mEO", [128, 2], F32)
            hsel = self.sb(st, "hsel", [128, 2], F32)
            rm = self.sb(st, "rm128", [128, T], BF16)
            c05 = self.sb(st, "c05", [128, 1], F32)
            for j in range(4):
                S.op("pool", lambda j=j: G.affine_select(out=mask4[:, j, :], in_=self.ones[:], pattern=[[1, 128]],
                                                         compare_op=(ALU.is_gt if j % 2 == 0 else ALU.is_ge), fill=0.0,
                                                         base=0, channel_multiplier=-1), r=["ones"], w=["mask4"])
            for j in range(2):
                S.op("pool", lambda j=j: G.affine_select(out=maskL[:, j, :], in_=self.ones[:], pattern=[[-1, 128]],
                                                         compare_op=ALU.is_gt, fill=0.0, base=0, channel_multiplier=1),
                     r=["ones"], w=["maskL"])
            S.op("pool", lambda: G.memset(bdm[:], 0.0), w=["bdm"])
            S.op("pool", lambda: G.memset(bdm[0:64, 0:64], 1.0), w=["bdm"])
            S.op("pool", lambda: G.memset(bdm[64:128, 64:128], 1.0), w=["bdm"])
            for (t_, nm) in ((mEO, "mEO"), (hsel, "hsel")):
                S.op("pool", lambda t_=t_: G.memset(t_[0:64, 0:1], 1.0), w=[nm])
                S.op("pool", lambda t_=t_: G.memset(t_[64:128, 0:1], 0.0), w=[nm])
                S.op("pool", lambda t_=t_: G.memset(t_[0:64, 1:2], 0.0), w=[nm])
                S.op("pool", lambda t_=t_: G.memset(t_[64:128, 1:2], 1.0), w=[nm])
            S.op("pool", lambda: G.memset(rm[:], 1.0), w=["rm"])
            S.op("pool", lambda: G.memset(rm[:].rearrange("p (c j) -> p c j", j=128)[:, :, 0:1], 0.0), w=["rm"])
            S.op("pool", lambda: G.memset(c05[:], -0.5), w=["c05"])
            pc = {}
            for nm, src in (("mu_r", P_["rwkv_mu"][l, 0:1024]), ("mu_k", P_["rwkv_mu"][l, 1024:2048]),
                            ("mu_v", P_["rwkv_mu"][l, 2048:3072]), ("w0", P_["rwkv_w0"][l]), ("a0", P_["rwkv_a0"][l]),
                            ("k_k", P_["rwkv_k_k"][l]), ("k_a", P_["rwkv_k_a"][l]),
                            ("r_k", P_["rwkv_r_k"][l].rearrange("h k -> (h k)"))):
                t_ = self.sb(st, "pc_" + nm, [128, 8, 1], F32)
                S.dma("sp", t_[:], src.rearrange("(q p o) -> p q o", p=128, o=1), w=["pc_" + nm])
                pc[nm] = t_
            mul = self.sb(st, "mul", [128, 3], F32)
            S.dma("sp", mul[0:64, 0:1], P_["rwkv_mu"][l, 3072:3136].rearrange("(p o) -> p o", o=1), w=["mul"])
            S.dma("sp", mul[0:64, 1:2], P_["rwkv_mu"][l, 3136:3200].rearrange("(p o) -> p o", o=1), w=["mul"])
            S.dma("sp", mul[:, 2:3], P_["rwkv_mu"][l, 3200:3328].rearrange("(p o) -> p o", o=1), w=["mul"])
            nw0 = self.sb(st, "nw0", [128, 8, 1], F32)
            omka = self.sb(st, "omka", [128, 8, 1], F32)
            S.op("dve", lambda: V.tensor_scalar(out=nw0[:], in0=pc["w0"][:], scalar1=-1.0, scalar2=None, op0=ALU.mult),
                 r=["pc_w0"], w=["nw0"])
            S.op("dve", lambda: V.tensor_scalar(out=omka[:], in0=pc["k_a"][:], scalar1=-1.0, scalar2=1.0, op0=ALU.mult,
                                                op1=ALU.add), r=["pc_k_a"], w=["omka"])
            wl = self.sb(st, "wl", [128, KT, 256], BF16)
            w2 = self.sb(st, "w2", [64, D], BF16)
            a2 = self.sb(st, "a2", [64, D], BF16)
            g2 = self.sb(st, "g2", [128, D], BF16)
            S.dma("pool", wl[:], Wl[:, OFF_RWKV + 3072:OFF_RWKV + 3328].rearrange("(kt p) n -> p kt n", p=128), w=["wl"])
            S.dma("pool", w2[:], P_["rwkv_w2"][l], w=["w2"])
            S.dma("pool", a2[:], P_["rwkv_a2"][l], w=["a2"])
            S.dma("pool", g2[:], P_["rwkv_g2"][l], w=["g2"])
            txw = self.sb(st, "txw", [64, T], BF16)
            xaT = self.sb(st, "xaT", [64, T], BF16)
            sgT = self.sb(st, "sgT", [128, T], BF16)
            xraw = self.sb(st, "xraw", [128, T + 1], F32)
            F = [self.sb(st, "F%d" % i, [128, T], F32) for i in range(5)]
            S.op("pool", lambda: G.memset(xraw[:, 0:1], 0.0), w=["xraw0"])

            S.qset = {"F0", "F1", "F2", "F3", "F4", "xraw", "Pp", "Pc", "iP", "bT", "kT", "vT", "Vtm", "Btm", "Ktm",
                      ("AR", 0), ("AR", 1), "Pend", "bon"}

            def Q(eng, fn, r=(), w=()):
                for q in range(4):
                    qs_ = slice(q * 512, (q + 1) * 512)
                    rr = [(k, q) if k in S.qset else k for k in r]
                    ww = [(k, q) if k in S.qset else k for k in w]
                    S.op(eng, lambda: fn(qs_, q), r=rr, w=ww)

            def proj_shift(wt, c0, m, mucol, dst, dkey, func=None, rows=128):
                for tq in range(4):
                    pb, kp = self.psum()
                    for kt in range(KT):
                        mm(pb[0:rows, :], wt[:, kt, c0:c0 + m], self.hT[:, kt, tq * 512:(tq + 1) * 512], kt == 0, kt == KT - 1,
                           [wt_key] + hTk[tq * 4:tq * 4 + 4], [kp])
                    S.op("act", lambda pb=pb, tq=tq: A.activation(out=xraw[0:rows, 1 + tq * 512:1 + (tq + 1) * 512],
                                                                  in_=pb[0:rows, :], func=AF.Copy), r=[kp], w=[("xraw", tq), kp])
                dk = (lambda q: (dkey, q)) if dkey in S.qset else (lambda q: dkey)
                for q in range(4):
                    lo, hi = q * 512, (q + 1) * 512
                    xr = [("xraw", q)] + ([("xraw", q - 1)] if q else ["xraw0"])
                    S.op("dve", lambda: V.tensor_tensor(out=F[4][0:rows, lo:hi], in0=xraw[0:rows, lo:hi], in1=xraw[0:rows, lo + 1:hi + 1],
                                                        op=ALU.subtract), r=xr, w=[("F4", q)])
                    if func is None:
                        S.op("dve", lambda: V.scalar_tensor_tensor(out=dst[0:rows, lo:hi], in0=F[4][0:rows, lo:hi], scalar=mucol,
                                                                   in1=xraw[0:rows, lo + 1:hi + 1], op0=ALU.mult, op1=ALU.add),
                             r=[("F4", q), ("xraw", q), "mul"] + list(pc_keys), w=[dk(q)])
                    else:
                        S.op("dve", lambda: V.scalar_tensor_tensor(out=F[4][0:rows, lo:hi], in0=F[4][0:rows, lo:hi], scalar=mucol,
                                                                   in1=xraw[0:rows, lo + 1:hi + 1], op0=ALU.mult, op1=ALU.add),
                             r=[("F4", q), ("xraw", q), "mul"] + list(pc_keys), w=[("F4", q)])
                        S.op("act", lambda: A.activation(out=dst[0:rows, lo:hi], in_=F[4][0:rows, lo:hi], func=func),
                             r=[("F4", q)], w=[dk(q)])

            pc_keys = ["pc_mu_r", "pc_mu_k", "pc_mu_v"]
            wt_key = "wl"
            proj_shift(wl, 0, 64, mul[0:64, 0:1], txw, "txw", AF.Tanh, rows=64)
            proj_shift(wl, 64, 64, mul[0:64, 1:2], xaT, "xaT", AF.Copy, rows=64)
            proj_shift(wl, 128, 128, mul[:, 2:3], sgT, "sgT", AF.Sigmoid, rows=128)
            wrkv1 = self.sb(st, "wrkv", [128, KT, 384], BF16)
            wrkv = [wrkv1, wrkv1]
            Pp = self.sb(st, "Pp", [128, T], BF16)
            Pc = self.sb(st, "Pc", [128, T], BF16)
            Pend = self.sb(st, "Pend", [128, NT], F32)
            iP = self.sb(st, "iP", [128, T], BF16)
            bT = self.sb(st, "bT", [128, T], BF16)
            kT = self.sb(st, "kT", [128, T], BF16)
            vT = self.sb(st, "vT", [128, T], BF16)
            AR = [self.sb(st, "AR", [128, NT, 2, 128], BF16) for _ in range(2)]
            Vtm = self.sb(st, "Vtm", [128, NT, 128], BF16)
            Btm = self.sb(st, "Btm", [128, NT, 128], BF16)
            aT = Vtm[:].rearrange("p t j -> p (t j)")
            rT = Btm[:].rearrange("p t j -> p (t j)")
            Ktm = self.sb(st, "Ktm", [128, NT, 128], BF16)
            bon = self.sb(st, "bon", [128, NT, 2], F32)
            st32 = self.sb(st, "st32", [128, 2 * NT, 4], F32)
            lnw = self.sb(st, "lnwb", [128, 128], F32)
            lnb = self.sb(st, "lnbb", [128, 128], F32)
            Z32 = self.sb(st, "Z32", [128, 128], F32)
            Zt = self.sb(st, "Zt", [128, 128], F32)
            Zbf = [self.sb(st, "Zbf", [128, 128], BF16) for _ in range(2)]
            Wsb = [self.sb(st, "Wsb", [128, 128], BF16) for _ in range(2)]
            Usb = [self.sb(st, "Usb", [128, 128], BF16) for _ in range(2)]
            NS = 8
            abrb = [self.sb(st, "abrb", [128, 4, 128], BF16) for _ in range(NS)]
            akrk = [self.sb(st, "akrk", [128, 4, 128], BF16) for _ in range(NS)]
            L0 = [self.sb(st, "L0", [128, 2, 128], BF16) for _ in range(4)]
            XX = [[self.sb(st, "XX", [128, 4, 128], BF16) for _ in range(4)] for _ in range(2)]
            Tt = [self.sb(st, "Tt", [128, 2, 128], BF16) for _ in range(NS)]

            def load_w(p):
                b = 0
                for j in range(3):
                    c0 = OFF_RWKV + j * 1024 + p * 128
                    S.dma("pool", wrkv[b][:, :, j * 128:(j + 1) * 128], Wl[:, c0:c0 + 128].rearrange("(kt p) n -> p kt n", p=128),
                          w=[("wrkv", b)])

            def projA(p):
                nonlocal wt_key
                load_w(p)
                wt_key = ("wrkv", 0)
                proj_shift(wrkv[0], 128, 128, pc["mu_k"][:, p, :], F[0], "F0")
                proj_shift(wrkv[0], 0, 128, pc["mu_r"][:, p, :], F[1], "F1")
                proj_shift(wrkv[0], 256, 128, pc["mu_v"][:, p, :], vT, "vT", AF.Copy)

            projA(0)
            for p in range(8):
                b = 0
                fs_ = slice(p * 128, (p + 1) * 128)
                S.dma("sp", lnw[:], P_["rwkv_ln_w"][l, fs_].partition_broadcast(128), w=["lnw"])
                S.dma("sp", lnb[:], P_["rwkv_ln_b"][l, fs_].partition_broadcast(128), w=["lnb"])
                for tq in range(4):
                    pb, kp = self.psum()
                    qs_ = slice(tq * 512, (tq + 1) * 512)
                    mm(pb[:], w2[:, fs_], txw[:, qs_], True, True, ["w2", "txw"], [kp])
                    S.op("act", lambda pb=pb: A.activation(out=F[2][:, qs_], in_=pb[:], func=AF.Exp, bias=nw0[:, p, :], scale=-1.0),
                         r=[kp, "nw0"], w=[("F2", tq), kp])
                Q("act", lambda qs, q: A.activation(out=F[2][:, qs], in_=F[2][:, qs], func=AF.Ln, bias=self.epsc[:, 3:4], scale=1.0),
                  r=["F2", "epsc"], w=["F2"])
                Q("act", lambda qs, q: A.activation(out=F[2][:, qs], in_=F[2][:, qs], func=AF.Exp, bias=c05[:, 0:1], scale=-1.0),
                  r=["F2", "c05"], w=["F2"])
                Q("dve", lambda qs, q: V.tensor_tensor_scan(out=F[3][:, qs], data0=rm[:, qs], data1=F[2][:, qs], initial=0.0,
                                                            op0=ALU.mult, op1=ALU.add), r=["rm", "F2"], w=["F3"])
                Q("dve", lambda qs, q: V.tensor_tensor(out=F[2][:, qs], in0=F[3][:, qs], in1=F[2][:, qs], op=ALU.subtract),
                  r=["F2", "F3"], w=["F2"])
                Q("act", lambda qs, q: A.activation(out=Pp[:, qs], in_=F[2][:, qs], func=AF.Exp, scale=-1.0), r=["F2"], w=["Pp"])
                Q("act", lambda qs, q: A.activation(out=Pc[:, qs], in_=F[3][:, qs], func=AF.Exp, scale=-1.0), r=["F3"], w=["Pc"])
                Q("act", lambda qs, q: A.activation(out=iP[:, qs], in_=F[3][:, qs], func=AF.Exp), r=["F3"], w=["iP"])
                Q("act", lambda qs, q: A.activation(out=Pend[:, q * 4:(q + 1) * 4],
                                                    in_=F[3][:, qs].rearrange("p (t j) -> p t j", j=128)[:, :, 127],
                                                    func=AF.Exp, scale=-1.0), r=["F3"], w=["Pend"])
                for tq in range(4):
                    pb, kp = self.psum()
                    qs_ = slice(tq * 512, (tq + 1) * 512)
                    mm(pb[:], a2[:, fs_], xaT[:, qs_], True, True, ["a2", "xaT"], [kp])
                    S.op("act", lambda pb=pb: A.activation(out=F[2][:, qs_], in_=pb[:], func=AF.Sigmoid, bias=pc["a0"][:, p, :],
                                                           scale=1.0), r=[kp, "pc_a0", ("Pp", tq)], w=[("F2", tq), kp])
                Q("dve", lambda qs, q: V.tensor_scalar(out=F[3][:, qs], in0=F[0][:, qs], scalar1=pc["k_k"][:, p, :], scalar2=None,
                                                       op0=ALU.mult), r=["F0", "pc_k_k", "Pc", "iP", "Pend"], w=["F3"])
                Q("act", lambda qs, q: A.activation(out=F[4][:, qs], in_=F[3][:, qs], func=AF.Square), r=["F3"], w=["F4"])
                for tq in range(4):
                    pb, kp = self.psum()
                    qs_ = slice(tq * 512, (tq + 1) * 512)
                    mm(pb[:], bdm[:], F[4][:, qs_], True, True, ["bdm", ("F4", tq)], [kp])
                    S.op("act", lambda pb=pb: A.activation(out=F[4][:, qs_], in_=pb[:], func=AF.Sqrt), r=[kp], w=[("F4", tq), kp])
                Q("dve", lambda qs, q: V.tensor_scalar(out=F[4][:, qs], in0=F[4][:, qs], scalar1=1e-12, scalar2=None, op0=ALU.max),
                  r=["F4"], w=["F4"])
                Q("dve", lambda qs, q: V.reciprocal(out=F[4][:, qs], in_=F[4][:, qs]), r=["F4"], w=["F4"])
                Q("dve", lambda qs, q: V.tensor_tensor(out=F[3][:, qs], in0=F[3][:, qs], in1=F[4][:, qs], op=ALU.mult),
                  r=["F3", "F4"], w=["F3"])
                Q("dve", lambda qs, q: V.scalar_tensor_tensor(out=aT[:, qs], in0=F[3][:, qs], scalar=-1.0, in1=Pp[:, qs], op0=ALU.mult,
                                                              op1=ALU.mult), r=["F3", "Pp"], w=["Vtm"])
                Q("pool", lambda qs, q: G.tensor_tensor(out=F[4][:, qs], in0=F[3][:, qs], in1=F[2][:, qs], op=ALU.mult),
                  r=["F3", "F2"], w=["F4"])
                Q("pool", lambda qs, q: G.tensor_tensor(out=bT[:, qs], in0=F[4][:, qs], in1=iP[:, qs], op=ALU.mult),
                  r=["F4", "iP"], w=["bT"])
                Q("dve", lambda qs, q: V.tensor_scalar(out=F[2][:, qs], in0=F[2][:, qs], scalar1=pc["k_a"][:, p, :],
                                                       scalar2=omka[:, p, :], op0=ALU.mult, op1=ALU.add),
                  r=["F2", "pc_k_a", "omka", "F4"], w=["F2"])
                Q("dve", lambda qs, q: V.tensor_tensor(out=F[0][:, qs], in0=F[0][:, qs], in1=F[2][:, qs], op=ALU.mult),
                  r=["F0", "F2", "F3"], w=["F0"])
                Q("pool", lambda qs, q: G.tensor_tensor(out=kT[:, qs], in0=F[0][:, qs], in1=iP[:, qs], op=ALU.mult),
                  r=["F0", "iP"], w=["kT"])
                Q("dve", lambda qs, q: V.tensor_tensor(out=rT[:, qs], in0=F[1][:, qs], in1=Pc[:, qs], op=ALU.mult),
                  r=["F1", "Pc"], w=["Btm"])
                Q("dve", lambda qs, q: V.scalar_tensor_tensor(out=F[1][:, qs], in0=F[1][:, qs], scalar=pc["r_k"][:, p, :],
                                                              in1=F[0][:, qs], op0=ALU.mult, op1=ALU.mult),
                  r=["F1", "F0", "pc_r_k", "Btm"], w=["F1"])
                for h in range(2):
                    Q("act", lambda qs, q, h=h: A.activation(out=AR[h][:, q * 4:(q + 1) * 4, 0, :], in_=Vtm[:, q * 4:(q + 1) * 4, :],
                                                             func=AF.Identity, scale=mEO[:, h:h + 1]), r=["Vtm", "mEO"], w=[("AR", h)])
                    Q("dve", lambda qs, q, h=h: V.tensor_scalar(out=AR[h][:, q * 4:(q + 1) * 4, 1, :], in0=Btm[:, q * 4:(q + 1) * 4, :],
                                                                scalar1=mEO[:, h:h + 1], scalar2=None, op0=ALU.mult),
                      r=["Btm", "mEO"], w=[("AR", h)])
                for (src, skey, dst, dkey) in ((vT, "vT", Vtm, "Vtm"), (bT, "bT", Btm, "Btm"), (kT, "kT", Ktm, "Ktm")):
                    for t4 in range(4):
                        pb, kp = self.psum()
                        pbv = pb[:].bitcast(BF16)
                        for j in range(4):
                            tt = t4 * 4 + j
                            S.op("pe", lambda j=j, tt=tt, pbv=pbv, src=src: PE.transpose(
                                out=pbv[:, j * 128:(j + 1) * 128], in_=src[:, tt * 128:(tt + 1) * 128], identity=self.identbf[:]),
                                r=[(skey, t4), "identbf"], w=[kp])
                        S.op("act", lambda t4=t4, pbv=pbv, dst=dst: A.activation(
                            out=dst[:, t4 * 4:(t4 + 1) * 4, :], in_=pbv[:, 0:512].rearrange("p (a b) -> p a b", a=4), func=AF.Copy),
                            r=[kp, (("AR", 0), t4), (("AR", 1), t4)], w=[(dkey, t4), kp])
                for t4 in range(4):
                    pb, kp = self.psum()
                    for j in range(4):
                        tt = t4 * 4 + j
                        mm(pb[:, j * 2:(j + 1) * 2], F[1][:, tt * 128:(tt + 1) * 128], hsel[:], True, True, [("F1", t4), "hsel"], [kp])
                    S.op("dve", lambda pb=pb, t4=t4: V.tensor_copy(out=bon[:, t4 * 4:(t4 + 1) * 4, :].rearrange("p t h -> p (t h)"),
                                                                   in_=pb[:, 0:8]), r=[kp], w=[("bon", t4), kp])
                ytm = F[2][:].rearrange("p (t j) -> p t j", j=128)
                ysq = F[3][:].rearrange("p (t j) -> p t j", j=128)

                def inv_group(gi, pending=()):
                    pending = list(pending)
                    tiles = range(gi * 4, gi * 4 + 4)
                    for tt in tiles:
                        sl = tt % NS
                        tsl = slice(tt * 128, (tt + 1) * 128)
                        p1, k1 = self.psum()
                        p2, k2 = self.psum()
                        p3, k3 = self.psum()
                        for h in range(2):
                            rhs = AR[h][:, tt].rearrange("p a j -> p (a j)")
                            mm(p1[:, h * 256:(h + 1) * 256], bT[:, tsl], rhs, True, True, ["bT", ("AR", h)], [k1])
                            mm(p2[:, h * 256:(h + 1) * 256], kT[:, tsl], rhs, True, True, ["kT", ("AR", h)], [k2])
                            mm(p3[:, h * 128:(h + 1) * 128], AR[h][:, tt, 0, :], bT[:, tsl], True, True, [("AR", h), "bT"], [k3])
                        S.op("dve", lambda p1=p1, sl=sl: V.tensor_tensor(out=abrb[sl][:].rearrange("p a j -> p (a j)"), in0=p1[:],
                                                                        in1=mask4[:].rearrange("p a j -> p (a j)"), op=ALU.mult),
                             r=[k1, "mask4"], w=[("abrb", sl), k1])
                        S.op("dve", lambda p2=p2, sl=sl: V.tensor_tensor(out=akrk[sl][:].rearrange("p a j -> p (a j)"), in0=p2[:],
                                                                        in1=mask4[:].rearrange("p a j -> p (a j)"), op=ALU.mult),
                             r=[k2, "mask4"], w=[("akrk", sl), k2])
                        S.op("dve", lambda p3=p3, sl=sl: V.tensor_tensor(out=L0[sl % 4][:].rearrange("p a j -> p (a j)"), in0=p3[:, 0:256],
                                                                        in1=maskL[:].rearrange("p a j -> p (a j)"), op=ALU.mult),
                             r=[k3, "maskL"], w=[("L0", sl % 4), k3])
                        for h in range(2):
                            S.op("pool", lambda h=h, sl=sl: G.tensor_tensor(out=Tt[sl][:, h, :], in0=abrb[sl][:, 2 * h, :],
                                                                            in1=self.identbf[:], op=ALU.add),
                                 r=[("abrb", sl), "identbf"], w=[("Tt", sl)])

                    def Xk(k, sl, h):
                        return (L0[sl % 4][:, h, :], ("L0", sl % 4)) if k == 0 else (XX[k % 2][sl % 4][:, 2 * h, :], ("XX", k % 2, sl % 4))

                    def Xtk(k, sl, h):
                        return (abrb[sl][:, 2 * h, :], ("abrb", sl)) if k == 0 else (XX[k % 2][sl % 4][:, 2 * h + 1, :], ("XX", k % 2, sl % 4))

                    for k in range(7):
                        sqb = {}
                        if k <= 5:
                            for tt in tiles:
                                sl = tt % NS
                                pb, kp = self.psum()
                                sqb[tt] = (pb, kp)
                                for h in range(2):
                                    x, kx = Xk(k, sl, h)
                                    xt, kxt = Xtk(k, sl, h)
                                    mm(pb[:, (2 * h) * 128:(2 * h + 1) * 128], xt, x, True, True, [kx, kxt], [kp])
                                    if k < 5:
                                        mm(pb[:, (2 * h + 1) * 128:(2 * h + 2) * 128], x, xt, True, True, [kx, kxt], [kp])
                        ttb = []
                        if k >= 1:
                            for t2 in range(2):
                                pb, kp = self.psum()
                                ttb.append((pb, kp))
                                for j in range(2):
                                    sl = (gi * 4 + t2 * 2 + j) % NS
                                    for h in range(2):
                                        x1, kx1 = Xk(k, sl, h)
                                        mm(pb[:, (j * 2 + h) * 128:(j * 2 + h + 1) * 128], x1, Tt[sl][:, h, :], True, True,
                                           [kx1, ("Tt", sl)], [kp])
                        if k <= 5:
                            for tt in tiles:
                                sl = tt % NS
                                pb, kp = sqb[tt]
                                kn = ("XX", (k + 1) % 2, sl % 4)
                                if k < 5:
                                    S.op("act", lambda pb=pb, sl=sl, k=k: A.activation(
                                        out=XX[(k + 1) % 2][sl % 4][:].rearrange("p a j -> p (a j)"), in_=pb[:], func=AF.Copy),
                                        r=[kp], w=[kn, kp])
                                else:
                                    S.op("act", lambda pb=pb, sl=sl, k=k: A.activation(
                                        out=XX[(k + 1) % 2][sl % 4][:, 0:4:2, :],
                                        in_=pb[:].rearrange("p (a j) -> p a j", j=128)[:, 0:4:2, :], func=AF.Copy), r=[kp], w=[kn, kp])
                        for t2, (pb, kp) in enumerate(ttb):
                            for j in range(2):
                                sl = (gi * 4 + t2 * 2 + j) % NS
                                S.op("dve", lambda pb=pb, sl=sl, j=j: V.tensor_tensor(
                                    out=Tt[sl][:].rearrange("p a j -> p (a j)"), in0=pb[:, j * 256:(j + 1) * 256],
                                    in1=Tt[sl][:].rearrange("p a j -> p (a j)"), op=ALU.add), r=[kp, ("Tt", sl)], w=[("Tt", sl), kp])
                        if pending and k >= 1:
                            chain_tile(pending.pop(0))
                    while pending:
                        chain_tile(pending.pop(0))

                def chain_tile(tt):
                    if True:
                        sl = tt % NS
                        i2 = tt % 2
                        tsl = slice(tt * 128, (tt + 1) * 128)
                        zb, kz = Zbf[i2], ("Zbf", i2)
                        pw, kpw = self.psum()
                        mm(pw[:, 0:128], AR[0][:, tt, 0, :], zb[:], True, False, [("AR", 0), kz], [kpw])
                        mm(pw[:, 0:128], AR[1][:, tt, 0, :], zb[:], False, False, [("AR", 1), kz], [kpw])
                        for h in range(2):
                            mm(pw[:, h * 64:(h + 1) * 64], akrk[sl][:, 2 * h, :], Vtm[:, tt, h * 64:(h + 1) * 64], False, h == 1,
                               [("akrk", sl), "Vtm"], [kpw])
                        S.op("act", lambda pw=pw: A.activation(out=Wsb[i2][:], in_=pw[:, 0:128], func=AF.Copy),
                             r=[kpw], w=[("Wsb", i2), kpw])
                        pu, kpu = self.psum()
                        for h in range(2):
                            mm(pu[:, h * 64:(h + 1) * 64], Tt[sl][:, h, :], Wsb[i2][:, h * 64:(h + 1) * 64], True, True,
                               [("Tt", sl), ("Wsb", i2)], [kpu])
                        S.op("act", lambda pu=pu: A.activation(out=Usb[i2][:], in_=pu[:, 0:128], func=AF.Copy),
                             r=[kpu], w=[("Usb", i2), kpu])
                        py, kpy = self.psum()
                        mm(py[:, 0:128], AR[0][:, tt, 1, :], zb[:], True, False, [("AR", 0), kz], [kpy])
                        mm(py[:, 0:128], AR[1][:, tt, 1, :], zb[:], False, False, [("AR", 1), kz], [kpy])
                        for h in range(2):
                            mm(py[:, h * 64:(h + 1) * 64], abrb[sl][:, 2 * h + 1, :], Usb[i2][:, h * 64:(h + 1) * 64], False, False,
                               [("abrb", sl), ("Usb", i2)], [kpy])
                            mm(py[:, h * 64:(h + 1) * 64], akrk[sl][:, 2 * h + 1, :], Vtm[:, tt, h * 64:(h + 1) * 64], False, h == 1,
                               [("akrk", sl), "Vtm"], [kpy])
                        S.op("act", lambda py=py: A.activation(out=ytm[:, tt, :], in_=py[:, 0:128], func=AF.Copy),
                             r=[kpy], w=["F2", kpy])
                        pz, kpz = self.psum()
                        mm(pz[:, 0:128], Btm[:, tt, :], Usb[i2][:], True, False, ["Btm", ("Usb", i2)], [kpz])
                        mm(pz[:, 0:128], Ktm[:, tt, :], Vtm[:, tt, :], False, True, ["Ktm", "Vtm"], [kpz])
                        S.op("dve", lambda pz=pz: V.tensor_tensor(out=Zt[:], in0=pz[:, 0:128], in1=bdm[:], op=ALU.mult),
                             r=[kpz, "bdm"], w=["Zt", kpz])
                        S.op("dve", lambda: V.tensor_tensor(out=Zt[:], in0=Zt[:], in1=Z32[:], op=ALU.add), r=["Zt", "Z32"], w=["Zt"])
                        S.op("dve", lambda: V.tensor_scalar(out=Z32[:], in0=Zt[:], scalar1=Pend[:, tt:tt + 1],
                                                            scalar2=None, op0=ALU.mult), r=["Zt", "Pend"], w=["Z32"])
                        S.op("pool", lambda: G.tensor_copy(out=Zbf[(tt + 1) % 2][:], in_=Z32[:]), r=["Z32"], w=[("Zbf", (tt + 1) % 2)])

                S.op("pool", lambda: G.memset(Z32[:], 0.0), w=["Z32"])
                S.op("pool", lambda: G.memset(Zbf[0][:], 0.0), w=[("Zbf", 0)])
                inv_group(0)
                for gi in range(1, 4):
                    inv_group(gi, pending=range((gi - 1) * 4, gi * 4))
                for tt in range(12, 16):
                    chain_tile(tt)
                y16, yTp = Btm, kT
                def output_phase(p=p, fs_=fs_, ytm=ytm, ysq=ysq):
                    y3 = ytm.rearrange("p t (h c) -> p (t h) c", c=64)
                    q3 = ysq.rearrange("p t (h c) -> p (t h) c", c=64)
                    S.op("act", lambda: A.activation(out=F[3][:], in_=F[2][:], func=AF.Square), r=["F2"], w=["F3"])
                    S.op("dve", lambda: V.tensor_reduce(out=st32[:, :, 0], in_=y3, axis=AX.X, op=ALU.add), r=["F2"], w=["st32"])
                    S.op("dve", lambda: V.tensor_reduce(out=st32[:, :, 1], in_=q3, axis=AX.X, op=ALU.add), r=["F3"], w=["st32"])
                    S.op("dve", lambda: V.tensor_scalar(out=st32[:, :, 0], in0=st32[:, :, 0], scalar1=1.0 / 64.0, scalar2=None,
                                                        op0=ALU.mult), r=["st32"], w=["st32"])
                    S.op("dve", lambda: V.tensor_tensor(out=st32[:, :, 2], in0=st32[:, :, 0], in1=st32[:, :, 0], op=ALU.mult),
                         r=["st32"], w=["st32"])
                    S.op("dve", lambda: V.scalar_tensor_tensor(out=st32[:, :, 1], in0=st32[:, :, 1], scalar=1.0 / 64.0, in1=st32[:, :, 2],
                                                               op0=ALU.mult, op1=ALU.subtract), r=["st32"], w=["st32"])
                    S.op("act", lambda: A.activation(out=st32[:, :, 1], in_=st32[:, :, 1], func=AF.Sqrt, bias=self.epsc[:, 2:3], scale=1.0),
                         r=["st32", "epsc"], w=["st32"])
                    S.op("dve", lambda: V.reciprocal(out=st32[:, :, 1], in_=st32[:, :, 1]), r=["st32"], w=["st32"])
                    S.op("dve", lambda: V.tensor_tensor(out=y3, in0=y3, in1=st32[:, :, 0:1].to_broadcast([128, 2 * NT, 64]),
                                                        op=ALU.subtract), r=["F2", "st32"], w=["F2"])
                    S.op("dve", lambda: V.tensor_tensor(out=y3, in0=y3, in1=st32[:, :, 1:2].to_broadcast([128, 2 * NT, 64]),
                                                        op=ALU.mult), r=["F2", "st32"], w=["F2"])
                    S.op("pool", lambda: G.tensor_tensor(out=ytm, in0=ytm, in1=lnw[:].unsqueeze(1).to_broadcast([128, NT, 128]),
                                                         op=ALU.mult), r=["F2", "lnw"], w=["F2"])
                    S.op("pool", lambda: G.tensor_tensor(out=ytm, in0=ytm, in1=lnb[:].unsqueeze(1).to_broadcast([128, NT, 128]),
                                                         op=ALU.add), r=["F2", "lnb"], w=["F2"])
                    S.op("dve", lambda: V.tensor_tensor(out=q3, in0=Vtm[:].rearrange("p t (h c) -> p (t h) c", c=64),
                                                        in1=bon[:].rearrange("p t h -> p (t h)").unsqueeze(2).to_broadcast([128, 2 * NT, 64]),
                                                        op=ALU.mult), r=["Vtm", "bon", "F3"], w=["F3"])
                    S.op("dve", lambda: V.tensor_tensor(out=F[2][:], in0=F[2][:], in1=F[3][:], op=ALU.add), r=["F2", "F3"], w=["F2"])
                    for t4 in range(4):
                        pb, kp = self.psum()
                        for j in range(4):
                            tt = t4 * 4 + j
                            mm(pb[:, j * 128:(j + 1) * 128], sgT[:, tt * 128:(tt + 1) * 128], g2[:, fs_], True, True, ["sgT", "g2"], [kp])
                        S.op("dve", lambda pb=pb, t4=t4: V.tensor_tensor(
                            out=y16[:, t4 * 4:(t4 + 1) * 4, :], in0=pb[:].rearrange("p (a j) -> p a j", j=128),
                            in1=ytm[:, t4 * 4:(t4 + 1) * 4, :], op=ALU.mult), r=[kp, "F2"], w=["Btm", kp])
                    for t4 in range(4):
                        pb, kp = self.psum()
                        pbv = pb[:].bitcast(BF16)
                        for j in range(4):
                            tt = t4 * 4 + j
                            S.op("pe", lambda j=j, tt=tt, pbv=pbv: PE.transpose(out=pbv[:, j * 128:(j + 1) * 128], in_=y16[:, tt, :],
                                                                               identity=self.identbf[:]), r=["Btm", "identbf"], w=[kp])
                        S.op("act", lambda t4=t4, pbv=pbv: A.activation(out=yTp[:, t4 * 512:(t4 + 1) * 512], in_=pbv[:, 0:512], func=AF.Copy),
                             r=[kp], w=["kT", kp])
                    S.dma("sp", ydst[fs_, :], yTp[:], r=["kT"], w=[("yT1", p)])
                if p + 1 < 8:
                    projA(p + 1)
                output_phase()
            S.barrier()
            S.qset = set()


    def merge(self, l):
        nc, S = self.nc, self.S
        V, A, G, PE = nc.vector, nc.scalar, nc.gpsimd, nc.tensor
        Wl = self.P["w_in"][l]
        brw = [self.P["w_br_ssd"][l], self.P["w_br_rwkv"][l], self.P["w_br_hgrn"][l]]
        hTk = [("hT", t) for t in range(NT)]
        with contextlib.ExitStack() as st:
            mT = self.sb(st, "mT", [128, KT, T], F32)
            wbrs = [self.sb(st, "wbr", [128, KT, D], BF16) for _ in range(2)]
            wgts = [self.sb(st, "wgt", [128, KT, D], BF16) for _ in range(2)]
            st1 = contextlib.ExitStack()
            st1.__enter__()
            yq = [self.sb(st1, "yq", [128, KT, 512], BF16) for _ in range(2)]
            sg = [self.sb(st1, "sgm", [128, 512], BF16) for _ in range(2)]
            tmp = [self.sb(st1, "tmpm", [128, 512], F32) for _ in range(2)]
            cnt = 0
            def load_br(i):
                S.dma("pool", wbrs[i % 2][:], brw[i].rearrange("(kt p) n -> p kt n", p=128), w=[("wbr", i % 2)])
                c0 = OFF_GATES + i * 1024
                S.dma("pool", wgts[i % 2][:], Wl[:, c0:c0 + 1024].rearrange("(kt p) n -> p kt n", p=128), w=[("wgt", i % 2)])

            load_br(0)
            load_br(1)
            for i in range(3):
                wbr, wgt = wbrs[i % 2], wgts[i % 2]
                kwb, kwg = ("wbr", i % 2), ("wgt", i % 2)
                if i == 2:
                    load_br(2)
                for q in range(4):
                    qs_ = slice(q * 512, (q + 1) * 512)
                    yb = (i * 4 + q) % 2
                    S.dma("sp", yq[yb][:], self.yT_dram[i][:, qs_].rearrange("(kt p) n -> p kt n", p=128),
                          r=[("yT%d" % i, k) for k in range(8)], w=[("yq", yb)])
                    for ot in range(KT):
                        os_ = slice(ot * 128, (ot + 1) * 128)
                        pg, kg = self.psum()
                        pb, kb = self.psum()
                        for kt in range(KT):
                            S.op("pe", lambda kt=kt, pg=pg: PE.matmul(pg[:], lhsT=wgt[:, kt, os_], rhs=self.hT[:, kt, qs_],
                                                                      start=(kt == 0), stop=(kt == KT - 1)),
                                 r=[kwg] + hTk[q * 4:q * 4 + 4], w=[kg])
                        for kt in range(KT):
                            S.op("pe", lambda kt=kt, pb=pb: PE.matmul(pb[:], lhsT=wbr[:, kt, os_], rhs=yq[yb][:, kt, :],
                                                                      start=(kt == 0), stop=(kt == KT - 1)),
                                 r=[kwb, ("yq", yb)], w=[kb])
                        c2 = cnt % 2
                        cnt += 1
                        S.op("act", lambda pg=pg, c2=c2: A.activation(out=sg[c2][:], in_=pg[:], func=AF.Sigmoid),
                             r=[kg], w=[("sgm", c2), kg])
                        if i == 0:
                            S.op("dve", lambda pb=pb, c2=c2: V.tensor_tensor(out=mT[:, ot, qs_], in0=pb[:], in1=sg[c2][:], op=ALU.mult),
                                 r=[kb, ("sgm", c2)], w=[("mT", q), kb])
                        else:
                            S.op("dve", lambda pb=pb, c2=c2: V.tensor_tensor(out=tmp[c2][:], in0=pb[:], in1=sg[c2][:], op=ALU.mult),
                                 r=[kb, ("sgm", c2)], w=[("tmpm", c2), kb])
                            S.op("pool", lambda c2=c2: G.tensor_tensor(out=mT[:, ot, qs_], in0=mT[:, ot, qs_], in1=tmp[c2][:],
                                                                       op=ALU.add), r=[("tmpm", c2), ("mT", q)], w=[("mT", q)])
            self.dbg_dump("merged%d" % l, lambda o: S.dma("sp", o.rearrange("(kt p) n -> p kt n", p=128), mT[:],
                                                          r=[("mT", q) for q in range(4)]))
            S.barrier()
            st1.__exit__(None, None, None)
            wo = wbrs[1]
            S.dma("pool", wo[:], self.P["w_out"][l].rearrange("(kt p) n -> p kt n", p=128), w=[("wbr", 1)])
            gbc = self.sb(st, "gbc1", [128, D], F32)
            bbc = self.sb(st, "bbc1", [128, D], F32)
            S.dma("sp", gbc[:], self.P["ln1_g"][l].partition_broadcast(128), w=["gbc"])
            S.dma("sp", bbc[:], self.P["ln1_b"][l].partition_broadcast(128), w=["bbc"])
            lnw = self.ln_alloc(st)
            h1 = [self.sb(st, "h1m", [128, D], F32) for _ in range(2)]
            mbf1 = self.sb(st, "mbf", [128, KT, 128], BF16)
            mbf = [mbf1, mbf1]
            for tt in range(NT):
                s2 = tt % 2
                q = tt // 4
                tsl = slice(tt * 128, (tt + 1) * 128)
                S.op("act", lambda: A.activation(out=mbf[s2][:], in_=mT[:, :, tsl], func=AF.Copy), r=[("mT", q)], w=[("mbf", 0)])
                S.dma("sp", h1[s2][:], self.h_dram[tsl, :], r=[("hd", tt)], w=[("h1m", s2)])
                xin, kx = self.ln_xin(lnw, tt)
                for half in range(2):
                    po, ko = self.psum()
                    for kt in range(KT):
                        S.op("pe", lambda kt=kt, po=po: PE.matmul(po[:], lhsT=mbf[s2][:, kt, :], rhs=wo[:, kt, half * 512:(half + 1) * 512],
                                                                  start=(kt == 0), stop=(kt == KT - 1)), r=[("mbf", 0), ("wbr", 1)], w=[ko])
                    S.op("dve", lambda po=po, half=half: V.scalar_tensor_tensor(
                        out=xin[:, half * 512:(half + 1) * 512], in0=h1[s2][:, half * 512:(half + 1) * 512], scalar=ALPHA, in1=po[:],
                        op0=ALU.mult, op1=ALU.add), r=[ko, ("h1m", s2)], w=[kx, ko])
                self.ln_tile(lnw, tt, gbc, bbc, self.h_dram, router=True, extra=self.dbg_out.get("h1_%d" % l))
            S.barrier()

    def layer(self, l):
        S = self.S
        if "ssd" in self.stages:
            self.ssd(l)
            self.dbg_dump("ya%d" % l, lambda o: S.dma("sp", o, self.yT_dram[0], r=[("yT0", h) for h in range(8)]))
        if "rwkv" in self.stages:
            self.rwkv(l)
            self.dbg_dump("yb%d" % l, lambda o: S.dma("sp", o, self.yT_dram[1], r=[("yT1", h) for h in range(8)]))
        if "hgrn" in self.stages:
            self.hgrn(l)
            self.dbg_dump("yc%d" % l, lambda o: S.dma("sp", o, self.yT_dram[2], r=[("yT2", h) for h in range(8)]))
        if "merge" in self.stages:
            self.merge(l)
        if "moe" in self.stages:
            self.moe(l, last=(l == self.depth - 1))


_NC_CACHE = {}


def _get_nc():
    if "nc" not in _NC_CACHE:
        _NC_CACHE["nc"] = Builder().build()
    return _NC_CACHE["nc"]


def kernel(**inputs):
    nc = _get_nc()
    x = np.ascontiguousarray(inputs["x"], dtype=np.float32)
    base = {k: np.ascontiguousarray(inputs[k], dtype=np.float32) for k in PARAM_SHAPES}
    in_maps = []
    for c in range(8):
        m = dict(base)
        m["x"] = x[c]
        in_maps.append(m)
    res = run_bass_kernel_spmd(nc, in_maps, core_ids=list(range(8)))
    return np.stack([res.results[c]["out"] for c in range(8)], axis=0)
```

```python
import contextlib
import os
import numpy as np
CUT = int(os.environ.get('CUT', '99'))
HC = int(os.environ.get('HC', '99'))
HL = int(os.environ.get('HL', '99'))
import concourse.bass as bass
import concourse.mybir as mybir
from concourse.bass_utils import run_bass_kernel_spmd

F32 = mybir.dt.float32
BF16 = mybir.dt.bfloat16
AF = mybir.ActivationFunctionType
ALU = mybir.AluOpType
AX = mybir.AxisListType

D = 1024
T = 2048
NT = T // 128
KT = D // 128
DEPTH = 2
NE = 16
DEXP = 512
N_IN = 13072
ALPHA = (2 * DEPTH) ** 0.25
LN_EPS = 1e-5
RMS_EPS = 1e-6
GN_EPS = 64e-5
OFF_Z = 0
OFF_XBC = 1024
OFF_DT = 2560
OFF_RWKV = 2576
OFF_HGRN = OFF_RWKV + 3328
OFF_GATES = OFF_HGRN + 4096

PARAM_SHAPES = {
    "ln_in_g": [1024], "ln_in_b": [1024], "w_in": [2, 1024, 13072],
    "ssd_conv_w": [2, 4, 1536], "ssd_conv_b": [2, 1536], "ssd_dt_bias": [2, 16],
    "ssd_a_log": [2, 16], "ssd_d": [2, 16], "ssd_norm_w": [2, 1024],
    "rwkv_mu": [2, 3328], "rwkv_w0": [2, 1024], "rwkv_w2": [2, 64, 1024],
    "rwkv_a0": [2, 1024], "rwkv_a2": [2, 64, 1024], "rwkv_g2": [2, 128, 1024],
    "rwkv_k_k": [2, 1024], "rwkv_k_a": [2, 1024], "rwkv_r_k": [2, 16, 64],
    "rwkv_ln_w": [2, 1024], "rwkv_ln_b": [2, 1024], "hgrn_lb": [2, 1024],
    "hgrn_norm_w": [2, 128], "w_br_ssd": [2, 1024, 1024], "w_br_rwkv": [2, 1024, 1024],
    "w_br_hgrn": [2, 1024, 1024], "w_out": [2, 1024, 1024], "ln1_g": [2, 1024],
    "ln1_b": [2, 1024], "router_w": [1024, 16], "router_bias": [16],
    "exp_w_gate": [2, 16, 1024, 512], "exp_w_up": [2, 16, 1024, 512],
    "exp_w_down": [2, 16, 512, 1024], "ln2_g": [2, 1024], "ln2_b": [2, 1024],
}


class Sched:
    ENG = ["pe", "act", "dve", "pool", "sp"]

    def __init__(self, nc, es, n_dma=32, n_pdma=24):
        self.nc = nc
        self.e = {"pe": nc.tensor, "act": nc.scalar, "dve": nc.vector, "pool": nc.gpsimd, "sp": nc.sync}
        self.sem = {k: es.enter_context(nc.semaphore("sem_" + k)) for k in self.ENG}
        self.cnt = {k: 0 for k in self.ENG}
        self.dsem = [es.enter_context(nc.semaphore("dsem%d" % i)) for i in range(n_dma)]
        self.dtot = [0] * n_dma
        self.drr = 0
        self.psem = [es.enter_context(nc.semaphore("psem%d" % i)) for i in range(n_pdma)]
        self.pused = [False] * n_pdma
        self.pwaiters = [[] for _ in range(n_pdma)]
        self.pclr = [None] * n_pdma
        self.prr = 0
        self.msem = {k: es.enter_context(nc.semaphore("msem_" + k)) for k in self.ENG}
        self.mcnt = {k: 0 for k in self.ENG}
        self.seen = {k: {} for k in self.ENG}
        self.lastw = {}
        self.readers = {}
        self.nwait = 0
        self.qset = set()

    def _semh(self, sk):
        if isinstance(sk, str):
            return self.sem[sk]
        if sk[0] == "m":
            return self.msem[sk[1]]
        return self.dsem[sk[1]] if sk[0] == "d" else self.psem[sk[1]]

    def _wait(self, e, tag):
        sk, val = tag
        if val <= 0 or self.seen[e].get(sk, 0) >= val:
            return
        if not isinstance(sk, str) and sk[0] == "p" and self.pclr[sk[1]] is not None and e != "pool":
            self._wait(e, self.pclr[sk[1]])
        self.e[e].wait_ge(self._semh(sk), val)
        self.seen[e][sk] = val
        self.nwait += 1
        if not isinstance(sk, str) and sk[0] == "p":
            self.pwaiters[sk[1]].append(self._marker(e))

    def _marker(self, e):
        self.e[e].sem_inc(self.msem[e], 1)
        self.mcnt[e] += 1
        return (("m", e), self.mcnt[e])

    def _deps(self, e, r, w):
        for k in r:
            t = self.lastw.get(k)
            if t is not None:
                self._wait(e, t)
        for k in w:
            t = self.lastw.get(k)
            if t is not None and (t[0] != e or e != "pe"):
                self._wait(e, t)
            for sk, val in self.readers.get(k, {}).items():
                if sk != e or e != "pe":
                    self._wait(e, (sk, val))

    def _record(self, tag, r, w):
        for k in r:
            d = self.readers.setdefault(k, {})
            if d.get(tag[0], 0) < tag[1]:
                d[tag[0]] = tag[1]
        for k in w:
            self.lastw[k] = tag
            self.readers[k] = {}

    def _exp(self, keys):
        out = []
        for k in keys:
            if k in self.qset:
                out.extend((k, q) for q in range(4))
            else:
                out.append(k)
        return out

    def op(self, e, fn, r=(), w=()):
        r, w = self._exp(r), self._exp(w)
        self._deps(e, r, w)
        ins = fn()
        self.cnt[e] += 1
        ins.then_inc(self.sem[e], 1)
        if os.environ.get("OPLOG"):
            self.oplog = getattr(self, "oplog", {})
            self.oplog[(e, self.cnt[e])] = fn.__code__.co_firstlineno
        self._record((e, self.cnt[e]), r, w)

    def _dma_sw(self, out, in_, r, w):
        q = "pool"
        self._deps(q, r, w)
        i = self.prr
        self.prr = (self.prr + 1) % len(self.psem)
        sk = ("p", i)
        if self.pused[i]:
            self._wait(q, (sk, 16))
            for e in self.ENG:
                if e != q:
                    self._wait(e, (sk, 16))
            for tg in self.pwaiters[i]:
                if tg[0][1] != q:
                    self._wait(q, tg)
            self.e[q].sem_clear(self.psem[i])
            tclr = self._marker(q)
            self.pclr[i] = tclr
            for k, t in list(self.lastw.items()):
                if t[0] == sk:
                    self.lastw[k] = tclr
            for k, d in self.readers.items():
                if sk in d:
                    d.pop(sk)
                    d[tclr[0]] = tclr[1]
            for e in self.ENG:
                self.seen[e].pop(sk, None)
            self.pwaiters[i] = []
        ins = self.e[q].dma_start(out=out, in_=in_)
        ins.then_inc(self.psem[i], 16)
        self.pused[i] = True
        self._record((sk, 16), r, w)

    def dma(self, q, out, in_, r=(), w=()):
        r, w = self._exp(r), self._exp(w)
        if q == "pool" and os.environ.get("PSEM_CLEAR"):
            return self._dma_sw(out, in_, r, w)
        self._deps(q, r, w)
        i = self.drr
        self.drr = (self.drr + 1) % len(self.dsem)
        self._wait(q, (("d", i), self.dtot[i]))
        with self.nc.allow_non_contiguous_dma(reason="small per-feature parameter columns"):
            ins = self.e[q].dma_start(out=out, in_=in_)
        self.dtot[i] += 16
        ins.then_inc(self.dsem[i], 16)
        self._record((("d", i), self.dtot[i]), r, w)

    def barrier(self):
        for e in self.ENG:
            for o in self.ENG:
                if o != e:
                    self._wait(e, (o, self.cnt[o]))
            for i in range(len(self.dsem)):
                self._wait(e, (("d", i), self.dtot[i]))
            for i in range(len(self.psem)):
                if self.pused[i]:
                    self._wait(e, (("p", i), 16))

    def finish(self):
        for i in range(len(self.dsem)):
            self._wait("sp", (("d", i), self.dtot[i]))
        for i in range(len(self.psem)):
            if self.pused[i]:
                self._wait("sp", (("p", i), 16))
        for o in self.ENG:
            if o != "sp":
                self._wait("sp", (o, self.cnt[o]))


class Builder:
    def __init__(self, debug=None, stages=("pre", "hgrn", "ssd", "rwkv", "merge", "moe"), depth=DEPTH, pre_router=False):
        self.pre_router = pre_router
        self.debug = debug or {}
        self.stages = stages
        self.depth = depth
        self.nc = bass.Bass("TRN2", target_bir_lowering=False)
        nc = self.nc
        self.x = nc.dram_tensor("x", [T, D], F32, kind="ExternalInput").ap()
        self.P = {k: nc.dram_tensor(k, s, F32, kind="ExternalInput").ap() for k, s in PARAM_SHAPES.items()}
        self.out = nc.dram_tensor("out", [T, D], F32, kind="ExternalOutput").ap()
        self.h_dram = nc.dram_tensor("h_scr", [T, D], F32, kind="Internal").ap()
        self.yT_dram = [nc.dram_tensor("yT_scr%d" % i, [D, T], BF16, kind="Internal").ap() for i in range(3)]
        self.dbg_out = {}
        for name, (shape, dt) in self.debug.items():
            self.dbg_out[name] = nc.dram_tensor("dbg_" + name, shape, dt, kind="ExternalOutput").ap()
        self.uid = 0

    def sb(self, es, name, shape, dt):
        self.uid += 1
        return es.enter_context(self.nc.sbuf_tensor("%s_%d" % (name, self.uid), shape, dt))

    def psum(self):
        i = self.ps_rr
        self.ps_rr = (self.ps_rr + 1) % 8
        return self.ps[i], ("ps", i)

    def build(self):
        nc = self.nc
        with contextlib.ExitStack() as es:
            self.S = Sched(nc, es)
            S = self.S
            self.ps = [es.enter_context(nc.psum_tensor("psb%d" % i, [128, 512], F32)) for i in range(8)]
            self.ps_rr = 0
            self.ident32 = self.sb(es, "ident32", [128, 128], F32)
            self.identbf = self.sb(es, "identbf", [128, 128], BF16)
            self.zeros = self.sb(es, "zeros", [128, 128], F32)
            self.ones = self.sb(es, "ones", [128, 128], F32)
            self.onesbf = self.sb(es, "onesbf", [128, 128], BF16)
            self.epsc = self.sb(es, "epsc", [128, 4], F32)
            S.op("pool", lambda: nc.gpsimd.memset(self.zeros[:], 0.0), w=["zeros"])
            S.op("pool", lambda: nc.gpsimd.memset(self.ones[:], 1.0), w=["ones"])
            S.op("pool", lambda: nc.gpsimd.memset(self.onesbf[:], 1.0), w=["onesbf"])
            S.op("pool", lambda: nc.gpsimd.memset(self.epsc[:, 0:1], LN_EPS), w=["epsc"])
            S.op("pool", lambda: nc.gpsimd.memset(self.epsc[:, 1:2], RMS_EPS), w=["epsc"])
            S.op("pool", lambda: nc.gpsimd.memset(self.epsc[:, 2:3], GN_EPS), w=["epsc"])
            S.op("pool", lambda: nc.gpsimd.memset(self.epsc[:, 3:4], 1.0), w=["epsc"])
            S.op("pool", lambda: nc.gpsimd.affine_select(
                out=self.ident32[:], in_=self.zeros[:], pattern=[[1, 128]], compare_op=ALU.not_equal,
                fill=1.0, base=0, channel_multiplier=-1), r=["zeros"], w=["ident32"])
            S.op("pool", lambda: nc.gpsimd.tensor_copy(out=self.identbf[:], in_=self.ident32[:]),
                 r=["ident32"], w=["identbf"])
            self.hT = self.sb(es, "hT", [128, KT, T], BF16)
            self.gates = self.sb(es, "gates", [128, NT, NE], F32)
            self.logits = self.sb(es, "logits", [128, NT, NE], F32)
            self.rw32 = self.sb(es, "rw32", [128, KT, NE], F32)
            self.rbias = self.sb(es, "rbias", [128, NE], F32)
            S.dma("sp", self.rw32[:], self.P["router_w"].rearrange("(kt p) e -> p kt e", p=128), w=["rw32"])
            S.dma("sp", self.rbias[:], self.P["router_bias"].partition_broadcast(128), w=["rbias"])

            with contextlib.ExitStack() as st:
                gbc = self.sb(st, "gbc", [128, D], F32)
                bbc = self.sb(st, "bbc", [128, D], F32)
                S.dma("sp", gbc[:], self.P["ln_in_g"].partition_broadcast(128), w=["gbc"])
                S.dma("sp", bbc[:], self.P["ln_in_b"].partition_broadcast(128), w=["bbc"])
                lnw = self.ln_alloc(st)
                for tt in range(NT):
                    xin, kx = self.ln_xin(lnw, tt)
                    S.dma("sp", xin[:], self.x[tt * 128:(tt + 1) * 128, :], w=[kx])
                    self.ln_tile(lnw, tt, gbc, bbc, self.h_dram, router=self.pre_router, extra=self.dbg_out.get("h0"))
                S.barrier()
            self.dbg_dump("hT", lambda o: S.dma("sp", o, self.hT[:], r=[("hT", t) for t in range(NT)]))
            self.dbg_dump("logits", lambda o: S.dma("sp", o, self.logits[:], r=["logits"]))

            for l in range(self.depth):
                self.layer(l)
            S.finish()
        return nc

    def dbg_dump(self, name, fn):
        if name in self.dbg_out:
            fn(self.dbg_out[name])

    def ln_alloc(self, st):
        w = {}
        w["xin"] = [self.sb(st, "xin", [128, D], F32) for _ in range(2)]
        w["hh"] = [self.sb(st, "hh", [128, D], F32) for _ in range(2)]
        w["bst"] = [self.sb(st, "bst", [128, 2, 6], F32) for _ in range(2)]
        w["mv"] = [self.sb(st, "mv", [128, 4], F32) for _ in range(2)]
        w["h32"] = [self.sb(st, "h32", [128, KT, 128], F32) for _ in range(2)]
        w["id"] = self.uid
        return w

    def ln_xin(self, w, tt):
        return w["xin"][tt % 2], ("xin", w["id"], tt % 2)

    def ln_tile(self, w, tt, gbc, bbc, dst_dram, router, extra=None):
        nc, S = self.nc, self.S
        s = tt % 2
        wid = w["id"]
        xin, kx = w["xin"][s], ("xin", wid, s)
        hh, kh = w["hh"][s], ("hh", wid, s)
        bst, kb = w["bst"][s], ("bst", wid, s)
        mv, km = w["mv"][s], ("mv", wid, s)
        h32, k32 = w["h32"][s], ("h32", wid, s)
        for c in range(2):
            S.op("dve", lambda c=c: nc.vector.bn_stats(out=bst[:, c, :], in_=xin[:, c * 512:(c + 1) * 512]),
                 r=[kx], w=[kb])
        S.op("dve", lambda: nc.vector.bn_aggr(out=mv[:, 0:2], in_=bst[:].rearrange("p a b -> p (a b)")),
             r=[kb], w=[km])
        if CUT < 2:
            return
        S.op("act", lambda: nc.scalar.activation(out=mv[:, 2:3], in_=mv[:, 1:2], func=AF.Sqrt,
                                                 bias=self.epsc[:, 0:1], scale=1.0), r=[km, "epsc"], w=[km])
        S.op("dve", lambda: nc.vector.reciprocal(out=mv[:, 3:4], in_=mv[:, 2:3]), r=[km], w=[km])
        S.op("dve", lambda: nc.vector.tensor_scalar(out=xin[:], in0=xin[:], scalar1=mv[:, 0:1], scalar2=mv[:, 3:4],
                                                    op0=ALU.subtract, op1=ALU.mult), r=[kx, km], w=[kx])
        if CUT < 3:
            return
        S.op("pool", lambda: nc.gpsimd.tensor_tensor(out=hh[:], in0=xin[:], in1=gbc[:], op=ALU.mult),
             r=[kx, "gbc"], w=[kh])
        S.op("pool", lambda: nc.gpsimd.tensor_tensor(out=hh[:], in0=hh[:], in1=bbc[:], op=ALU.add),
             r=[kh, "bbc"], w=[kh])
        S.dma("sp", dst_dram[tt * 128:(tt + 1) * 128, :], hh[:], r=[kh], w=[("hd", tt)])
        if extra is not None:
            S.dma("sp", extra[tt * 128:(tt + 1) * 128, :], hh[:], r=[kh], w=[("hdx", tt)])
        if CUT < 4:
            return
        for half in range(2):
            pb, kp = self.psum()
            for j in range(4):
                kt = half * 4 + j
                S.op("pe", lambda j=j, kt=kt: nc.tensor.transpose(
                    out=pb[:, j * 128:(j + 1) * 128], in_=hh[:, kt * 128:(kt + 1) * 128], identity=self.ident32[:]),
                    r=[kh, "ident32"], w=[kp])
            if os.environ.get("EVAC", "act") == "act":
                S.op("act", lambda half=half, pb=pb: nc.scalar.activation(
                    out=self.hT[:, half * 4:(half + 1) * 4, tt * 128:(tt + 1) * 128],
                    in_=pb[:].rearrange("p (a b) -> p a b", a=4), func=AF.Copy), r=[kp], w=[("hT", tt), kp])
            else:
                S.op("dve", lambda half=half, pb=pb: nc.vector.tensor_copy(
                    out=self.hT[:, half * 4:(half + 1) * 4, tt * 128:(tt + 1) * 128],
                    in_=pb[:].rearrange("p (a b) -> p a b", a=4)), r=[kp], w=[("hT", tt), kp])
            if router:
                S.op("dve", lambda half=half, pb=pb: nc.vector.tensor_copy(
                    out=h32[:, half * 4:(half + 1) * 4, :], in_=pb[:].rearrange("p (a b) -> p a b", a=4)),
                    r=[kp], w=[k32, kp])
        if router and CUT >= 5:
            pb, kp = self.psum()
            for kt in range(KT):
                S.op("pe", lambda kt=kt: nc.tensor.matmul(pb[:, 0:NE], lhsT=h32[:, kt, :], rhs=self.rw32[:, kt, :],
                                                          start=(kt == 0), stop=(kt == KT - 1)),
                     r=[k32, "rw32"], w=[kp])
            S.op("dve", lambda: nc.vector.tensor_copy(out=self.logits[:, tt, :], in_=pb[:, 0:NE]),
                 r=[kp], w=["logits"])

    def router(self, st):
        nc, S = self.nc, self.S
        V = nc.vector
        L = self.logits
        t1 = self.sb(st, "rt1", [128, NT, NE], F32)
        probs = self.sb(st, "probs", [128, NT, NE], F32)
        sel = self.sb(st, "sel", [128, NT, NE], F32)
        p6 = self.sb(st, "p6", [128, NT, 4, 6], F32)
        gs = self.sb(st, "gs", [128, NT, 4], F32)
        gm = self.sb(st, "gm", [128, NT, 4], F32)
        gt = self.sb(st, "gt", [128, NT, 4], F32)
        red = self.sb(st, "red", [128, NT], F32)
        red2 = self.sb(st, "red2", [128, NT], F32)
        msk = self.sb(st, "msk", [128, NT, NE], F32)
        eq = self.sb(st, "eq", [128, NT, NE], F32)
        BIG = 1.0e9

        def bc(a):
            return a[:].unsqueeze(2).to_broadcast([128, NT, NE])

        S.op("dve", lambda: V.tensor_reduce(out=red[:], in_=L[:], axis=AX.X, op=ALU.max), r=["logits"], w=["red"])
        S.op("dve", lambda: V.tensor_tensor(out=t1[:], in0=L[:], in1=bc(red), op=ALU.subtract),
             r=["logits", "red"], w=["rt1"])
        S.op("act", lambda: nc.scalar.activation(out=t1[:], in_=t1[:], func=AF.Exp), r=["rt1"], w=["rt1"])
        S.op("dve", lambda: V.tensor_reduce(out=red[:], in_=t1[:], axis=AX.X, op=ALU.add), r=["rt1"], w=["red"])
        S.op("dve", lambda: V.reciprocal(out=red[:], in_=red[:]), r=["red"], w=["red"])
        S.op("dve", lambda: V.tensor_tensor(out=probs[:], in0=t1[:], in1=bc(red), op=ALU.mult),
             r=["rt1", "red"], w=["probs"])
        S.op("dve", lambda: V.tensor_tensor(out=sel[:], in0=probs[:],
                                            in1=self.rbias[:].unsqueeze(1).to_broadcast([128, NT, NE]), op=ALU.add),
             r=["probs", "rbias"], w=["sel"])
        s4 = sel[:].rearrange("p t (g e) -> p t g e", g=4)
        S.op("dve", lambda: V.tensor_tensor(out=p6[:, :, :, 0:3], in0=s4[:, :, :, 0:3], in1=s4[:, :, :, 1:4],
                                            op=ALU.add), r=["sel"], w=["p6"])
        S.op("dve", lambda: V.tensor_tensor(out=p6[:, :, :, 3:5], in0=s4[:, :, :, 0:2], in1=s4[:, :, :, 2:4],
                                            op=ALU.add), r=["sel"], w=["p6"])
        S.op("dve", lambda: V.tensor_tensor(out=p6[:, :, :, 5:6], in0=s4[:, :, :, 0:1], in1=s4[:, :, :, 3:4],
                                            op=ALU.add), r=["sel"], w=["p6"])
        S.op("dve", lambda: V.tensor_reduce(out=gs[:], in_=p6[:], axis=AX.X, op=ALU.max), r=["p6"], w=["gs"])
        S.op("dve", lambda: V.tensor_reduce(out=red[:], in_=gs[:], axis=AX.X, op=ALU.max), r=["gs"], w=["red"])
        S.op("dve", lambda: V.tensor_tensor(out=gm[:], in0=gs[:], in1=red[:].unsqueeze(2).to_broadcast([128, NT, 4]),
                                            op=ALU.is_ge), r=["gs", "red"], w=["gm"])
        S.op("dve", lambda: V.tensor_scalar(out=gt[:], in0=gm[:], scalar1=BIG, scalar2=-BIG, op0=ALU.mult,
                                            op1=ALU.add), r=["gm"], w=["gt"])
        m4 = msk[:].rearrange("p t (g e) -> p t g e", g=4)
        S.op("dve", lambda: V.tensor_tensor(out=m4, in0=s4, in1=gm[:].unsqueeze(3).to_broadcast([128, NT, 4, 4]),
                                            op=ALU.mult), r=["sel", "gm"], w=["msk"])
        S.op("dve", lambda: V.tensor_tensor(out=m4, in0=m4, in1=gt[:].unsqueeze(3).to_broadcast([128, NT, 4, 4]),
                                            op=ALU.add), r=["msk", "gt"], w=["msk"])
        S.op("dve", lambda: V.tensor_reduce(out=red[:], in_=msk[:], axis=AX.X, op=ALU.max), r=["msk"], w=["red"])
        S.op("dve", lambda: V.tensor_tensor(out=eq[:], in0=msk[:], in1=bc(red), op=ALU.is_equal),
             r=["msk", "red"], w=["eq"])
        S.op("dve", lambda: V.scalar_tensor_tensor(out=eq[:], in0=eq[:], scalar=-BIG, in1=msk[:], op0=ALU.mult,
                                                   op1=ALU.add), r=["eq", "msk"], w=["eq"])
        S.op("dve", lambda: V.tensor_reduce(out=red2[:], in_=eq[:], axis=AX.X, op=ALU.max), r=["eq"], w=["red2"])
        S.op("dve", lambda: V.tensor_tensor(out=eq[:], in0=msk[:], in1=bc(red2), op=ALU.is_ge),
             r=["msk", "red2"], w=["eq"])
        S.op("dve", lambda: V.tensor_tensor(out=eq[:], in0=eq[:], in1=probs[:], op=ALU.mult),
             r=["eq", "probs"], w=["eq"])
        S.op("dve", lambda: V.tensor_reduce(out=red[:], in_=eq[:], axis=AX.X, op=ALU.add), r=["eq"], w=["red"])
        S.op("dve", lambda: V.reciprocal(out=red[:], in_=red[:]), r=["red"], w=["red"])
        S.op("dve", lambda: V.tensor_tensor(out=self.gates[:], in0=eq[:], in1=bc(red), op=ALU.mult),
             r=["eq", "red"], w=["gates"])

    def moe(self, l, last):
        nc, S = self.nc, self.S
        wg_d, wu_d, wd_d = self.P["exp_w_gate"], self.P["exp_w_up"], self.P["exp_w_down"]
        with contextlib.ExitStack() as st:
            self.router(st)
            self.dbg_dump("gates%d" % l, lambda o: S.dma("sp", o, self.gates[:], r=["gates"]))
            acc = self.sb(st, "acc", [128, NT, D], F32)
            wg = [self.sb(st, "wg", [128, KT, DEXP], BF16) for _ in range(2)]
            wu = [self.sb(st, "wu", [128, KT, DEXP], BF16) for _ in range(2)]
            wd = [self.sb(st, "wd", [128, 4, D], BF16) for _ in range(2)]
            hg = [self.sb(st, "hg", [128, 4, 512], BF16) for _ in range(2)]
            sg = [self.sb(st, "sg", [128, 512], BF16) for _ in range(2)]
            gbc = self.sb(st, "gbc2", [128, D], F32)
            bbc = self.sb(st, "bbc2", [128, D], F32)
            S.dma("sp", gbc[:], self.P["ln2_g"][l].partition_broadcast(128), w=["gbc"])
            S.dma("sp", bbc[:], self.P["ln2_b"][l].partition_broadcast(128), w=["bbc"])

            def load_w(e):
                b = e % 2
                S.dma("pool", wg[b][:], wg_d[l, e].rearrange("(kt p) n -> p kt n", p=128), w=[("wg", b)])
                S.dma("pool", wu[b][:], wu_d[l, e].rearrange("(kt p) n -> p kt n", p=128), w=[("wu", b)])
                S.dma("pool", wd[b][:], wd_d[l, e].rearrange("(kt p) n -> p kt n", p=128), w=[("wd", b)])

            items = [(e, q) for e in range(int(os.environ.get('ME', NE))) for q in range(4)]
            sgi = [0]

            def G(i):
                e, q = items[i]
                b = e % 2
                hb = i % 2
                for dt_ in range(4):
                    pa, ka = self.psum()
                    pu, ku = self.psum()
                    for kt in range(KT):
                        S.op("pe", lambda kt=kt, pa=pa: nc.tensor.matmul(
                            pa[:], lhsT=wg[b][:, kt, dt_ * 128:(dt_ + 1) * 128], rhs=self.hT[:, kt, q * 512:(q + 1) * 512],
                            start=(kt == 0), stop=(kt == KT - 1)),
                            r=[("wg", b)] + [("hT", q * 4 + j) for j in range(4)], w=[ka])
                    for kt in range(KT):
                        S.op("pe", lambda kt=kt, pu=pu: nc.tensor.matmul(
                            pu[:], lhsT=wu[b][:, kt, dt_ * 128:(dt_ + 1) * 128], rhs=self.hT[:, kt, q * 512:(q + 1) * 512],
                            start=(kt == 0), stop=(kt == KT - 1)),
                            r=[("wu", b)] + [("hT", q * 4 + j) for j in range(4)], w=[ku])
                    si = sgi[0] % 2
                    sgi[0] += 1
                    S.op("act", lambda pa=pa, si=si: nc.scalar.activation(out=sg[si][:], in_=pa[:], func=AF.Silu),
                         r=[ka], w=[("sg", si)])
                    S.op("dve", lambda pu=pu, si=si: nc.vector.tensor_tensor(
                        out=hg[hb][:, dt_, :], in0=pu[:], in1=sg[si][:], op=ALU.mult),
                        r=[ku, ("sg", si)], w=[("hg", hb)])

            def Dn(i):
                e, q = items[i]
                b = e % 2
                hb = i % 2
                for j in range(4):
                    tt = q * 4 + j
                    for half in range(2):
                        pc, kc = self.psum()
                        for dt_ in range(4):
                            S.op("pe", lambda dt_=dt_, pc=pc: nc.tensor.matmul(
                                pc[:], lhsT=hg[hb][:, dt_, j * 128:(j + 1) * 128],
                                rhs=wd[b][:, dt_, half * 512:(half + 1) * 512], start=(dt_ == 0), stop=(dt_ == 3)),
                                r=[("hg", hb), ("wd", b)], w=[kc])
                        dst = acc[:, tt, half * 512:(half + 1) * 512]
                        if e == 0:
                            S.op("dve", lambda pc=pc, dst=dst: nc.vector.tensor_scalar(
                                out=dst, in0=pc[:], scalar1=self.gates[:, tt, e:e + 1], scalar2=None, op0=ALU.mult),
                                r=[kc, "gates"], w=[("acc", tt)])
                        else:
                            S.op("dve", lambda pc=pc, dst=dst: nc.vector.scalar_tensor_tensor(
                                out=dst, in0=pc[:], scalar=self.gates[:, tt, e:e + 1], in1=dst, op0=ALU.mult,
                                op1=ALU.add), r=[kc, "gates", ("acc", tt)], w=[("acc", tt)])

            load_w(0)
            for i in range(len(items)):
                e, q = items[i]
                G(i)
                if i >= 1:
                    Dn(i - 1)
                if q == 0 and e + 1 < int(os.environ.get('ME', NE)):
                    load_w(e + 1)
            Dn(len(items) - 1)
            self.dbg_dump("moe%d" % l, lambda o: S.dma("sp", o.rearrange("(t p) d -> p t d", p=128), acc[:],
                                                       r=[("acc", t) for t in range(NT)]))
            lnw = self.ln_alloc(st)
            h1 = [self.sb(st, "h1t", [128, D], F32) for _ in range(2)]
            dst = self.out if last else self.h_dram
            for tt in range(NT):
                s = tt % 2
                S.dma("sp", h1[s][:], self.h_dram[tt * 128:(tt + 1) * 128, :], r=[("hd", tt)], w=[("h1t", s)])
                xin, kx = self.ln_xin(lnw, tt)
                S.op("dve", lambda s=s, xin=xin: nc.vector.scalar_tensor_tensor(
                    out=xin[:], in0=h1[s][:], scalar=ALPHA, in1=acc[:, tt, :], op0=ALU.mult, op1=ALU.add),
                    r=[("h1t", s), ("acc", tt)], w=[kx])
                self.ln_tile(lnw, tt, gbc, bbc, dst, router=False)
            S.barrier()


    def hgrn(self, l):
        nc, S = self.nc, self.S
        V, A, G, PE = nc.vector, nc.scalar, nc.gpsimd, nc.tensor
        Wl = self.P["w_in"][l]
        ydst = self.yT_dram[2]
        with contextlib.ExitStack() as st:
            mask2 = self.sb(st, "mask2", [128, 128], F32)
            rm = self.sb(st, "rm", [128, T], F32)
            nw = self.sb(st, "nw", [128, 1], F32)
            lbt = self.sb(st, "lbt", [128, 8, 2], F32)
            lbv = self.sb(st, "lbv", [128, 8], F32)
            oml = self.sb(st, "oml", [128, 8], F32)
            S.op("pool", lambda: G.affine_select(out=mask2[:], in_=self.ones[:], pattern=[[1, 128]],
                                                 compare_op=ALU.is_ge, fill=0.0, base=0, channel_multiplier=-1),
                 r=["ones"], w=["mask2"])
            S.op("pool", lambda: G.memset(mask2[0:64, 64:128], 0.0), w=["mask2"])
            S.op("pool", lambda: G.memset(rm[:], 1.0), w=["rm"])
            S.op("pool", lambda: G.memset(rm[:].rearrange("p (c j) -> p c j", j=64)[:, :, 0:1], 0.0), w=["rm"])
            S.dma("sp", nw[:], self.P["hgrn_norm_w"][l].rearrange("(p o) -> p o", o=1), w=["nw"])
            if l == 0:
                S.op("pool", lambda: G.memset(lbv[:], 0.0), w=["lbv"])
                S.op("pool", lambda: G.memset(oml[:], 1.0), w=["oml"])
            else:
                for j in range(2):
                    S.dma("sp", lbt[:, :, j:j + 1],
                          self.P["hgrn_lb"][j].rearrange("(h p o) -> p h o", p=128, o=1), w=["lbt"])
                S.op("dve", lambda: V.tensor_tensor(out=lbv[:], in0=lbt[:, :, 1], in1=lbt[:, :, 0], op=ALU.subtract),
                     r=["lbt"], w=["lbv"])
                S.op("act", lambda: A.activation(out=lbv[:], in_=lbv[:], func=AF.Sigmoid), r=["lbv"], w=["lbv"])
                S.op("dve", lambda: V.tensor_scalar(out=oml[:], in0=lbv[:], scalar1=-1.0, scalar2=1.0, op0=ALU.mult,
                                                    op1=ALU.add), r=["lbv"], w=["oml"])
            w4 = [self.sb(st, "w4", [128, 4, KT, 128], BF16) for _ in range(2)]
            qs = self.sb(st, "qs", [128, T], F32)
            fs = self.sb(st, "fs", [128, T], F32)
            lf = self.sb(st, "lf", [128, T], F32)
            bc = self.sb(st, "bc", [128, T], F32)
            enb = self.sb(st, "enb", [128, T], F32)
            def two(name, shape, dt):
                return [self.sb(st, name, shape, dt) for _ in range(2)]
            qbL, kbL, gsL, ytL = two("qb", [128, T], BF16), two("kb", [128, T], BF16), two("gs", [128, T], BF16), two("yt", [128, T], BF16)
            vL, kbtL, kbtBL = two("v", [128, NT, 128], BF16), two("kbt", [128, NT, 128], BF16), two("kbtB", [128, NT, 128], BF16)
            ebL = two("ebh", [128, T], F32)
            S32L = two("S32", [128, 128], F32)
            SbfL = [[self.sb(st, "Sbf", [128, 128], BF16) for _ in range(4)] for _ in range(2)]
            attmL = [two("attm", [128, 128], BF16) for _ in range(2)]
            osbL = [two("osb", [128, 128], F32) for _ in range(2)]
            osqL = [two("osq", [128, 128], BF16) for _ in range(2)]
            sdL = [two("sd", [128, 128], F32) for _ in range(2)]
            mAB = self.sb(st, "mAB", [128, 2], F32)
            S.op("pool", lambda: G.memset(mAB[0:64, 0:1], 1.0), w=["mAB"])
            S.op("pool", lambda: G.memset(mAB[64:128, 0:1], 0.0), w=["mAB"])
            S.op("pool", lambda: G.memset(mAB[0:64, 1:2], 0.0), w=["mAB"])
            S.op("pool", lambda: G.memset(mAB[64:128, 1:2], 1.0), w=["mAB"])
            hTk = [("hT", t) for t in range(NT)]

            def load_w(h):
                b = h % 2
                for j in range(4):
                    c0 = OFF_HGRN + j * 1024 + h * 128
                    S.dma("pool", w4[b][:, j], Wl[:, c0:c0 + 128].rearrange("(kt p) n -> p kt n", p=128),
                          w=[("w4", b)])

            def prep(h):
                hb = b = h % 2
                qb, kb, gs, v, kbt, kbtB, eb = qbL[hb], kbL[hb], gsL[hb], vL[hb], kbtL[hb], kbtBL[hb], ebL[hb]
                K = lambda n: (n, hb)
                if h + 1 < 8:
                    load_w(h + 1)
                for (j, func, dst, kd) in ((0, AF.Silu, qs, "qs"), (1, AF.Sigmoid, fs, "fs"), (3, AF.Sigmoid, gs, K("gs"))):
                    for tq in range(4):
                        pb, kp = self.psum()
                        for kt in range(KT):
                            S.op("pe", lambda kt=kt, pb=pb, j=j, tq=tq: PE.matmul(
                                pb[:], lhsT=w4[b][:, j, kt, :], rhs=self.hT[:, kt, tq * 512:(tq + 1) * 512],
                                start=(kt == 0), stop=(kt == KT - 1)), r=[("w4", b)] + hTk[tq * 4:tq * 4 + 4], w=[kp])
                        S.op("act", lambda pb=pb, dst=dst, func=func, tq=tq: A.activation(
                            out=dst[:, tq * 512:(tq + 1) * 512], in_=pb[:], func=func), r=[kp], w=[kd, kp])
                for t4 in range(4):
                    pb, kp = self.psum()
                    for j4 in range(4):
                        tt = t4 * 4 + j4
                        for kt in range(KT):
                            S.op("pe", lambda kt=kt, pb=pb, j4=j4, tt=tt: PE.matmul(
                                pb[:, j4 * 128:(j4 + 1) * 128], lhsT=self.hT[:, kt, tt * 128:(tt + 1) * 128],
                                rhs=w4[b][:, 2, kt, :], start=(kt == 0), stop=(kt == KT - 1)),
                                r=[("w4", b), ("hT", tt)], w=[kp])
                    S.op("dve", lambda pb=pb, t4=t4: V.tensor_copy(
                        out=v[:, t4 * 4:(t4 + 1) * 4, :], in_=pb[:].rearrange("p (a b) -> p a b", a=4)),
                        r=[kp], w=[K("v"), kp])
                S.op("dve", lambda: V.tensor_scalar(out=fs[:], in0=fs[:], scalar1=oml[:, h:h + 1], scalar2=lbv[:, h:h + 1],
                                                    op0=ALU.mult, op1=ALU.add), r=["fs", "oml", "lbv"], w=["fs"])
                S.op("act", lambda: A.activation(out=lf[:], in_=fs[:], func=AF.Ln), r=["fs"], w=["lf"])
                S.op("dve", lambda: V.tensor_tensor_scan(out=bc[:], data0=rm[:], data1=lf[:], initial=0.0,
                                                         op0=ALU.mult, op1=ALU.add), r=["rm", "lf"], w=["bc"])
                S.op("act", lambda: A.activation(out=eb[:], in_=bc[:], func=AF.Exp), r=["bc"], w=[K("eb")])
                S.op("act", lambda: A.activation(out=enb[:], in_=bc[:], func=AF.Exp, scale=-1.0), r=["bc"], w=["enb"])
                S.op("dve", lambda: V.tensor_scalar(out=fs[:], in0=fs[:], scalar1=-1.0, scalar2=1.0, op0=ALU.mult,
                                                    op1=ALU.add), r=["fs", "lf"], w=["fs"])
                S.op("pool", lambda: G.tensor_tensor(out=qb[:], in0=qs[:], in1=eb[:], op=ALU.mult),
                     r=["qs", K("eb")], w=[K("qb")])
                S.op("dve", lambda: V.tensor_tensor(out=kb[:], in0=fs[:], in1=enb[:], op=ALU.mult),
                     r=["fs", "enb"], w=[K("kb")])
                for t4 in range(4):
                    pb, kp = self.psum()
                    pbv = pb[:].bitcast(BF16)
                    for j4 in range(4):
                        tt = t4 * 4 + j4
                        S.op("pe", lambda pbv=pbv, j4=j4, tt=tt: PE.transpose(
                            out=pbv[:, j4 * 128:(j4 + 1) * 128], in_=kb[:, tt * 128:(tt + 1) * 128],
                            identity=self.identbf[:]), r=[K("kb"), "identbf"], w=[kp])
                    S.op("dve", lambda pbv=pbv, t4=t4: V.tensor_scalar(
                        out=kbt[:, t4 * 4:(t4 + 1) * 4, :], in0=pbv[:, 0:512].rearrange("p (a b) -> p a b", a=4),
                        scalar1=mAB[:, 0:1], scalar2=None, op0=ALU.mult), r=[kp, "mAB"], w=[K("kbt"), kp])
                    S.op("dve", lambda pbv=pbv, t4=t4: V.tensor_scalar(
                        out=kbtB[:, t4 * 4:(t4 + 1) * 4, :], in0=pbv[:, 0:512].rearrange("p (a b) -> p a b", a=4),
                        scalar1=mAB[:, 1:2], scalar2=None, op0=ALU.mult), r=[kp, "mAB"], w=[K("kbtB"), kp])
                S.op("pool", lambda: G.memset(S32L[hb][:], 0.0), w=[K("S32")])
                S.op("pool", lambda: G.memset(SbfL[hb][0][:], 0.0), w=[("Sbf", hb, 0)])

            def tile(h, tt):
                hb = h % 2
                qb, kb, gs, yt, v, kbt, kbtB, eb = qbL[hb], kbL[hb], gsL[hb], ytL[hb], vL[hb], kbtL[hb], kbtBL[hb], ebL[hb]
                S32, Sbf = S32L[hb], SbfL[hb]
                K = lambda n: (n, hb)
                cA, cB = 2 * tt, 2 * tt + 1
                tsl = slice(tt * 128, (tt + 1) * 128)
                i2 = tt % 2
                attm, osb, osq, sd = attmL[hb][i2], osbL[hb][i2], osqL[hb][i2], sdL[hb][i2]
                ka, ko, kq, ks = ("attm", hb, i2), ("osb", hb, i2), ("osq", hb, i2), ("sd", hb, i2)
                pa, kpa = self.psum()
                S.op("pe", lambda: PE.matmul(pa[:, 0:128], lhsT=kb[:, tsl], rhs=qb[:, tsl], start=True, stop=True),
                     r=[K("kb"), K("qb")], w=[kpa])
                S.op("dve", lambda: V.tensor_tensor(out=attm[:], in0=pa[:, 0:128], in1=mask2[:], op=ALU.mult),
                     r=[kpa, "mask2"], w=[ka, kpa])
                yield
                pr, kpr = self.psum()
                S.op("pe", lambda: PE.matmul(pr[:, 0:128], lhsT=kbt[:, tt, :], rhs=v[:, tt, :], start=True, stop=True),
                     r=[K("kbt"), K("v")], w=[kpr])
                S.op("pe", lambda: PE.matmul(pr[:, 128:256], lhsT=kbtB[:, tt, :], rhs=v[:, tt, :], start=True, stop=True),
                     r=[K("kbtB"), K("v")], w=[kpr])
                for (ci, off) in ((cA, 0), (cB, 128)):
                    S.op("dve", lambda off=off: V.tensor_tensor(
                        out=S32[:], in0=pr[:, off:off + 128], in1=S32[:], op=ALU.add), r=[kpr, K("S32")], w=[K("S32"), kpr])
                    S.op("dve", lambda ci=ci: V.tensor_scalar(
                        out=S32[:], in0=S32[:], scalar1=eb[:, ci * 64 + 63:ci * 64 + 64], scalar2=None, op0=ALU.mult),
                        r=[K("S32"), K("eb")], w=[K("S32")])
                    S.op("pool", lambda ci=ci: G.tensor_copy(out=Sbf[(ci + 1) % 4][:], in_=S32[:]),
                         r=[K("S32")], w=[("Sbf", hb, (ci + 1) % 4)])
                    yield
                po, kpo = self.psum()
                S.op("pe", lambda: PE.matmul(po[:, 0:128], lhsT=v[:, tt, :], rhs=attm[:], start=True, stop=False),
                     r=[K("v"), ka], w=[kpo])
                S.op("pe", lambda: PE.matmul(po[:, 0:64], lhsT=Sbf[cA % 4][:], rhs=qb[:, tt * 128:tt * 128 + 64],
                                             start=False, stop=False), r=[("Sbf", hb, cA % 4), K("qb")], w=[kpo])
                S.op("pe", lambda: PE.matmul(po[:, 64:128], lhsT=Sbf[cB % 4][:], rhs=qb[:, tt * 128 + 64:(tt + 1) * 128],
                                             start=False, stop=True), r=[("Sbf", hb, cB % 4), K("qb")], w=[kpo])
                S.op("act", lambda: A.activation(out=osb[:], in_=po[:, 0:128], func=AF.Copy), r=[kpo], w=[ko, kpo])
                S.op("act", lambda: A.activation(out=osq[:], in_=po[:, 0:128], func=AF.Square), r=[kpo], w=[kq, kpo])
                yield
                pss, kps = self.psum()
                S.op("pe", lambda: PE.matmul(pss[:, 0:128], lhsT=self.onesbf[:], rhs=osq[:], start=True, stop=True),
                     r=["onesbf", kq], w=[kps])
                S.op("act", lambda: A.activation(out=sd[:], in_=pss[:, 0:128], func=AF.Sqrt, bias=self.epsc[:, 1:2],
                                                 scale=1.0 / 128.0), r=[kps, "epsc"], w=[ks, kps])
                S.op("dve", lambda: V.reciprocal(out=sd[:], in_=sd[:]), r=[ks], w=[ks])
                S.op("dve", lambda: V.scalar_tensor_tensor(out=osb[:], in0=osb[:], scalar=nw[:, 0:1], in1=sd[:],
                                                           op0=ALU.mult, op1=ALU.mult), r=[ko, ks, "nw"], w=[ko])
                S.op("dve", lambda: V.tensor_tensor(out=yt[:, tsl], in0=osb[:], in1=gs[:, tsl], op=ALU.mult),
                     r=[ko, K("gs")], w=[K("yt")])

            load_w(0)
            for hp in range(4):
                prep(2 * hp)
                prep(2 * hp + 1)
                gens, nxt, rnd = [], 0, 0
                while nxt < NT or gens:
                    if nxt < NT and rnd % 2 == 0:
                        gens.append(tile(2 * hp, nxt))
                        gens.append(tile(2 * hp + 1, nxt))
                        nxt += 1
                    for g_ in list(gens):
                        try:
                            next(g_)
                        except StopIteration:
                            gens.remove(g_)
                    rnd += 1
                for hb in range(2):
                    h = 2 * hp + hb
                    S.dma("sp", ydst[h * 128:(h + 1) * 128, :], ytL[hb][:], r=[("yt", hb)], w=[("yT2", h)])
            S.barrier()

    def ssd(self, l):
        nc, S = self.nc, self.S
        V, A, G, PE = nc.vector, nc.scalar, nc.gpsimd, nc.tensor
        Wl = self.P["w_in"][l]
        ydst = self.yT_dram[0]
        NEG = -30000.0
        hTk = [("hT", t) for t in range(NT)]
        with contextlib.ExitStack() as st:
            tri2 = self.sb(st, "tri2", [128, 128], F32)
            same2 = self.sb(st, "same2", [128, 128], F32)
            indA = self.sb(st, "indA", [128, 128], F32)
            indB = self.sb(st, "indB", [128, 128], F32)
            mAB = self.sb(st, "mABs", [128, 2], F32)
            negmask = self.sb(st, "negmask", [128, 8, 128], F32)
            bd1 = self.sb(st, "bd", [16, 8, 128], F32)
            bd = [bd1, bd1]
            S.op("pool", lambda: G.affine_select(out=tri2[:], in_=self.ones[:], pattern=[[1, 128]], compare_op=ALU.is_ge,
                                                 fill=0.0, base=0, channel_multiplier=-1), r=["ones"], w=["tri2"])
            S.op("pool", lambda: G.memset(tri2[0:64, 64:128], 0.0), w=["tri2"])
            S.op("pool", lambda: G.memset(same2[:], 0.0), w=["same2"])
            S.op("pool", lambda: G.memset(same2[0:64, 0:64], 1.0), w=["same2"])
            S.op("pool", lambda: G.memset(same2[64:128, 64:128], 1.0), w=["same2"])
            S.op("pool", lambda: G.memset(indA[0:64, :], 1.0), w=["indA"])
            S.op("pool", lambda: G.memset(indA[64:128, :], 0.0), w=["indA"])
            S.op("pool", lambda: G.memset(indB[0:64, :], 0.0), w=["indB"])
            S.op("pool", lambda: G.memset(indB[64:128, :], 1.0), w=["indB"])
            S.op("pool", lambda: G.memset(mAB[0:64, 0:1], 1.0), w=["mAB"])
            S.op("pool", lambda: G.memset(mAB[64:128, 0:1], 0.0), w=["mAB"])
            S.op("pool", lambda: G.memset(mAB[0:64, 1:2], 0.0), w=["mAB"])
            S.op("pool", lambda: G.memset(mAB[64:128, 1:2], 1.0), w=["mAB"])
            S.op("pool", lambda: G.memset(negmask[:], 0.0), w=["negmask"])
            S.op("pool", lambda: G.affine_select(out=negmask[:], in_=negmask[:], pattern=[[0, 8], [1, 128]],
                                                 compare_op=ALU.is_ge, fill=NEG, base=0, channel_multiplier=-1),
                 r=["negmask"], w=["negmask"])
            S.op("pool", lambda: G.memset(negmask[0:64, :, 64:128], NEG), w=["negmask"])
            dtb = self.sb(st, "dtb", [128, 16], F32)
            alog = self.sb(st, "alog", [128, 16], F32)
            dsk = self.sb(st, "dsk", [128, 16], F32)
            nwbc = self.sb(st, "nwbc", [128, D], F32)
            S.dma("sp", dtb[:], self.P["ssd_dt_bias"][l].partition_broadcast(128), w=["dtb"])
            S.dma("sp", alog[:], self.P["ssd_a_log"][l].partition_broadcast(128), w=["alog"])
            S.dma("sp", dsk[:], self.P["ssd_d"][l].partition_broadcast(128), w=["dsk"])
            S.dma("sp", nwbc[:], self.P["ssd_norm_w"][l].partition_broadcast(128), w=["nwbc"])
            S.op("act", lambda: A.activation(out=alog[:], in_=alog[:], func=AF.Exp), r=["alog"], w=["alog"])
            S.op("dve", lambda: V.tensor_scalar(out=alog[:], in0=alog[:], scalar1=-1.0, scalar2=None, op0=ALU.mult),
                 r=["alog"], w=["alog"])
            wdt = self.sb(st, "wdt", [128, KT, 16], BF16)
            S.dma("pool", wdt[:], Wl[:, OFF_DT:OFF_DT + 16].rearrange("(kt p) n -> p kt n", p=128), w=["wdt"])
            dt = self.sb(st, "dt", [128, NT, 16], F32)
            da = self.sb(st, "da", [128, NT, 16], F32)
            cum4 = self.sb(st, "cum4", [128, NT, 4, 16], F32)
            eacs = self.sb(st, "eacs", [128, NT, 16], F32)
            eend = self.sb(st, "eend", [128, NT, 16], F32)
            edec = self.sb(st, "edec", [128, NT, 2, 16], F32)
            acsTt = [self.sb(st, "acsTt", [16, 128], F32) for _ in range(2)]
            nacsTt = [self.sb(st, "nacsTt", [16, 128], F32) for _ in range(2)]
            pb, kp = self.psum()
            for tt in range(NT):
                for kt in range(KT):
                    S.op("pe", lambda kt=kt, tt=tt: PE.matmul(pb[:, tt * 16:(tt + 1) * 16], lhsT=self.hT[:, kt, tt * 128:(tt + 1) * 128],
                                                              rhs=wdt[:, kt, :], start=(kt == 0), stop=(kt == KT - 1)),
                         r=["wdt", ("hT", tt)], w=[kp])
            S.op("dve", lambda: V.tensor_tensor(out=dt[:], in0=pb[:, 0:256].rearrange("p (t h) -> p t h", h=16),
                                                in1=dtb[:].unsqueeze(1).to_broadcast([128, NT, 16]), op=ALU.add),
                 r=[kp, "dtb"], w=["dt", kp])
            S.op("act", lambda: A.activation(out=dt[:], in_=dt[:], func=AF.Exp), r=["dt"], w=["dt"])
            S.op("act", lambda: A.activation(out=dt[:], in_=dt[:], func=AF.Ln, bias=self.epsc[:, 3:4], scale=1.0),
                 r=["dt", "epsc"], w=["dt"])
            S.op("dve", lambda: V.tensor_tensor(out=da[:], in0=dt[:], in1=alog[:].unsqueeze(1).to_broadcast([128, NT, 16]),
                                                op=ALU.mult), r=["dt", "alog"], w=["da"])
            for half in range(2):
                pb, kp = self.psum()
                for j in range(8):
                    tt = half * 8 + j
                    for qi, L in enumerate((tri2, same2, indA, indB)):
                        S.op("pe", lambda j=j, qi=qi, L=L, tt=tt, pb=pb: PE.matmul(
                            pb[:, j * 64 + qi * 16:j * 64 + (qi + 1) * 16], lhsT=L[:], rhs=da[:, tt, :], start=True, stop=True),
                            r=["da", "tri2", "same2", "indA", "indB"], w=[kp])
                S.op("dve", lambda pb=pb, half=half: V.tensor_copy(
                    out=cum4[:, half * 8:(half + 1) * 8].rearrange("p t q h -> p (t q h)"), in_=pb[:]),
                    r=[kp], w=["cum4", kp])
            S.op("act", lambda: A.activation(out=eacs[:], in_=cum4[:, :, 0, :], func=AF.Exp), r=["cum4"], w=["eacs"])
            S.op("dve", lambda: V.tensor_tensor(out=eend[:], in0=cum4[:, :, 1, :], in1=cum4[:, :, 0, :], op=ALU.subtract),
                 r=["cum4"], w=["eend"])
            S.op("act", lambda: A.activation(out=eend[:], in_=eend[:], func=AF.Exp), r=["eend"], w=["eend"])
            S.op("act", lambda: A.activation(out=edec[:], in_=cum4[:, :, 2:4, :], func=AF.Exp), r=["cum4"], w=["edec"])
            wx = self.sb(st, "wx", [128, KT, 768], BF16)
            wz = self.sb(st, "wz", [128, KT, 512], BF16)
            cw = self.sb(st, "cw", [128, 6, 4], F32)
            cbi = self.sb(st, "cbi", [128, 6], F32)
            xp1 = self.sb(st, "xp", [128, T + 3], F32)
            xp = [xp1, xp1]
            fTa = [self.sb(st, "fT", [128, T], BF16) for _ in range(4)]
            fT = [fTa[0], fTa[1], fTa[0], fTa[1], fTa[2], fTa[3]]
            fk = [("fT", 0), ("fT", 1), ("fT", 0), ("fT", 1), ("fT", 2), ("fT", 3)]
            cmTA = self.sb(st, "cmTA", [128, T], BF16)
            cmTB = self.sb(st, "cmTB", [128, T], BF16)
            xs = self.sb(st, "xs", [128, NT, 512], BF16)
            xdtt = [self.sb(st, "xdtt", [128, 512], BF16) for _ in range(2)]
            xendt = [self.sb(st, "xendt", [128, 512], BF16) for _ in range(2)]
            bmA = self.sb(st, "bmA", [128, NT, 128], BF16)
            bmB = self.sb(st, "bmB", [128, NT, 128], BF16)
            yTg = self.sb(st, "yTg", [128, 4, T], BF16)
            S32 = self.sb(st, "S32s", [128, 512], F32)
            Sbf = [self.sb(st, "Sbfs", [128, 512], BF16) for _ in range(4)]
            cbs = [self.sb(st, "cbs", [128, 128], BF16) for _ in range(2)]
            Dx = [self.sb(st, "Dx", [16, 8, 128], F32) for _ in range(2)]
            Es = [self.sb(st, "Es", [128, 8, 128], BF16) for _ in range(2)]
            wT = [self.sb(st, "wT", [128, 8, 128], BF16) for _ in range(2)]
            t1 = [self.sb(st, "t1", [128, 512], F32) for _ in range(2)]
            t2 = [self.sb(st, "t2", [128, 512], F32) for _ in range(2)]
            zs = [self.sb(st, "zs", [128, 512], BF16) for _ in range(2)]
            ytm = [self.sb(st, "ytm", [128, 512], BF16) for _ in range(2)]
            ss = [self.sb(st, "ss", [128, 2], F32) for _ in range(2)]
            S.op("pool", lambda: G.memset(xp[0][:, 0:3], 0.0), w=[("xp", 0)])
            for g in range(2):
                S.op("pool", lambda g=g: G.memset(bd[g][:], 1.0), r=[("bd", 0), ("bd", 1)], w=[("bd", 0), ("bd", 1)])
                S.op("pool", lambda g=g: G.affine_select(out=bd[g][:], in_=bd[g][:], pattern=[[1, 8], [0, 128]],
                                                         compare_op=ALU.is_equal, fill=0.0, base=8 * g,
                                                         channel_multiplier=-1), r=[("bd", 0), ("bd", 1)], w=[("bd", 0), ("bd", 1)])
                choff = [g * 512 + i * 128 for i in range(4)] + [1024 + g * 128, 1280 + g * 128]
                S.dma("pool", wx[:, :, 0:512], Wl[:, OFF_XBC + g * 512:OFF_XBC + (g + 1) * 512].rearrange("(kt p) n -> p kt n", p=128), w=["wx"])
                S.dma("pool", wx[:, :, 512:640], Wl[:, OFF_XBC + 1024 + g * 128:OFF_XBC + 1024 + (g + 1) * 128].rearrange("(kt p) n -> p kt n", p=128), w=["wx"])
                S.dma("pool", wx[:, :, 640:768], Wl[:, OFF_XBC + 1280 + g * 128:OFF_XBC + 1280 + (g + 1) * 128].rearrange("(kt p) n -> p kt n", p=128), w=["wx"])
                S.dma("pool", wz[:], Wl[:, OFF_Z + g * 512:OFF_Z + (g + 1) * 512].rearrange("(kt p) n -> p kt n", p=128), w=["wz"])
                for ci in range(6):
                    for j in range(4):
                        S.dma("sp", cw[:, ci, j:j + 1], self.P["ssd_conv_w"][l, j, choff[ci]:choff[ci] + 128].rearrange("(p o) -> p o", o=1), w=["cw"])
                    S.dma("sp", cbi[:, ci:ci + 1], self.P["ssd_conv_b"][l, choff[ci]:choff[ci] + 128].rearrange("(p o) -> p o", o=1), w=["cbi"])
                for ci in range(6):
                    xb = xp[0]
                    kx = ("xp", 0)
                    for tq in range(4):
                        pb, kp = self.psum()
                        for kt in range(KT):
                            S.op("pe", lambda kt=kt, pb=pb, ci=ci, tq=tq: PE.matmul(
                                pb[:], lhsT=wx[:, kt, ci * 128:(ci + 1) * 128], rhs=self.hT[:, kt, tq * 512:(tq + 1) * 512],
                                start=(kt == 0), stop=(kt == KT - 1)), r=["wx"] + hTk[tq * 4:tq * 4 + 4], w=[kp])
                        S.op("act", lambda pb=pb, xb=xb, tq=tq: A.activation(out=xb[:, 3 + tq * 512:3 + (tq + 1) * 512], in_=pb[:],
                                                                            func=AF.Copy), r=[kp], w=[kx, kp])
                    acc = t1[0] if False else None
                    cacc = self.sb(st, "cacc", [128, T], F32) if (g == 0 and ci == 0) else self._cacc
                    self._cacc = cacc
                    S.op("dve", lambda xb=xb, ci=ci, cacc=cacc: V.tensor_scalar(
                        out=cacc[:], in0=xb[:, 3:3 + T], scalar1=cw[:, ci, 3:4], scalar2=cbi[:, ci:ci + 1], op0=ALU.mult,
                        op1=ALU.add), r=[kx, "cw", "cbi"], w=["cacc"])
                    for j in range(3):
                        S.op("dve", lambda xb=xb, ci=ci, j=j, cacc=cacc: V.scalar_tensor_tensor(
                            out=cacc[:], in0=xb[:, j:j + T], scalar=cw[:, ci, j:j + 1], in1=cacc[:], op0=ALU.mult, op1=ALU.add),
                            r=[kx, "cw", "cacc"], w=["cacc"])
                    S.op("act", lambda ci=ci, cacc=cacc: A.activation(out=fT[ci][:], in_=cacc[:], func=AF.Silu),
                         r=["cacc"], w=[fk[ci]])
                    if ci < 5:
                        for t4 in range(4):
                            pb, kp = self.psum()
                            pbv = pb[:].bitcast(BF16)
                            for j in range(4):
                                tt = t4 * 4 + j
                                S.op("pe", lambda j=j, tt=tt, pbv=pbv, ci=ci: PE.transpose(
                                    out=pbv[:, j * 128:(j + 1) * 128], in_=fT[ci][:, tt * 128:(tt + 1) * 128],
                                    identity=self.identbf[:]), r=[fk[ci], "identbf"], w=[kp])
                            src = pbv[:, 0:512].rearrange("p (a b) -> p a b", a=4)
                            if ci < 4:
                                S.op("act", lambda t4=t4, src=src, ci=ci: A.activation(
                                    out=xs[:, t4 * 4:(t4 + 1) * 4, ci * 128:(ci + 1) * 128], in_=src, func=AF.Copy),
                                    r=[kp], w=["xs", kp])
                            else:
                                S.op("dve", lambda t4=t4, src=src: V.tensor_scalar(
                                    out=bmA[:, t4 * 4:(t4 + 1) * 4, :], in0=src, scalar1=mAB[:, 0:1], scalar2=None, op0=ALU.mult),
                                    r=[kp, "mAB"], w=["bmA", kp])
                                S.op("dve", lambda t4=t4, src=src: V.tensor_scalar(
                                    out=bmB[:, t4 * 4:(t4 + 1) * 4, :], in0=src, scalar1=mAB[:, 1:2], scalar2=None, op0=ALU.mult),
                                    r=[kp, "mAB"], w=["bmB", kp])
                bmT, cmT = fT[4], fT[5]
                cv = cmT[:].rearrange("p (t c j) -> p t c j", c=2, j=64)
                cva = cmTA[:].rearrange("p (t c j) -> p t c j", c=2, j=64)
                cvb = cmTB[:].rearrange("p (t c j) -> p t c j", c=2, j=64)
                S.op("pool", lambda: G.tensor_copy(out=cva[:, :, 0, :], in_=cv[:, :, 0, :]), r=[("fT", 3)], w=["cmTA"])
                S.op("pool", lambda: G.memset(cva[:, :, 1, :], 0.0), w=["cmTA"])
                S.op("pool", lambda: G.tensor_copy(out=cvb[:, :, 1, :], in_=cv[:, :, 1, :]), r=[("fT", 3)], w=["cmTB"])
                S.op("pool", lambda: G.memset(cvb[:, :, 0, :], 0.0), w=["cmTB"])
                hs = slice(g * 8, (g + 1) * 8)
                S.op("pool", lambda: G.memset(S32[:], 0.0), w=["S32"])
                S.op("pool", lambda: G.memset(Sbf[0][:], 0.0), w=[("Sbf", 0)])
                def tile_gen(tt):
                    i2 = tt % 2
                    tsl = slice(tt * 128, (tt + 1) * 128)
                    cA, cB = 2 * tt, 2 * tt + 1
                    pc, kpc = self.psum()
                    S.op("pe", lambda pc=pc: PE.matmul(pc[:, 0:128], lhsT=bmT[:, tsl], rhs=cmT[:, tsl], start=True, stop=True),
                         r=[("fT", 2), ("fT", 3)], w=[kpc])
                    S.op("act", lambda pc=pc: A.activation(out=cbs[i2][:], in_=pc[:, 0:128], func=AF.Copy),
                         r=[kpc], w=[("cbs", i2), kpc])
                    pq, kpq = self.psum()
                    S.op("pe", lambda pq=pq: PE.matmul(pq[0:16, 0:128], lhsT=da[:, tt, :], rhs=tri2[:], start=True, stop=True),
                         r=["da", "tri2"], w=[kpq])
                    S.op("dve", lambda pq=pq: V.tensor_copy(out=acsTt[i2][:], in_=pq[0:16, 0:128]), r=[kpq], w=[("acsTt", i2), kpq])
                    S.op("dve", lambda pq=pq: V.tensor_scalar(out=nacsTt[i2][:], in0=pq[0:16, 0:128], scalar1=-1.0, scalar2=None,
                                                              op0=ALU.mult), r=[kpq], w=[("nacsTt", i2), kpq])
                    S.op("pool", lambda: G.tensor_tensor(out=Dx[i2][:], in0=bd[g][:],
                                                         in1=acsTt[i2][:].unsqueeze(1).to_broadcast([16, 8, 128]), op=ALU.mult),
                         r=[("bd", g), ("acsTt", i2)], w=[("Dx", i2)])
                    yield
                    for hh in range(2):
                        pe_, kpe = self.psum()
                        csl = slice(hh * 512, (hh + 1) * 512)
                        S.op("pe", lambda pe_=pe_, csl=csl: PE.matmul(
                            pe_[:], lhsT=self.ones[0:16, :], rhs=Dx[i2][:].rearrange("p h l -> p (h l)")[:, csl],
                            start=True, stop=False), r=["ones", ("Dx", i2)], w=[kpe])
                        S.op("pe", lambda pe_=pe_, csl=csl: PE.matmul(
                            pe_[:], lhsT=nacsTt[i2][:], rhs=bd[g][:].rearrange("p h l -> p (h l)")[:, csl],
                            start=False, stop=False), r=[("nacsTt", i2), ("bd", g)], w=[kpe])
                        S.op("pe", lambda pe_=pe_, csl=csl: PE.matmul(
                            pe_[:], lhsT=self.ident32[:], rhs=negmask[:].rearrange("p h l -> p (h l)")[:, csl],
                            start=False, stop=True), r=["ident32", "negmask"], w=[kpe])
                        S.op("act", lambda pe_=pe_, hh=hh: A.activation(
                            out=Es[i2][:, hh * 4:(hh + 1) * 4, :], in_=pe_[:].rearrange("p (h l) -> p h l", h=4), func=AF.Exp),
                            r=[kpe], w=[("Es", i2), kpe])
                    S.op("dve", lambda: V.tensor_tensor(out=wT[i2][:], in0=Es[i2][:],
                                                        in1=cbs[i2][:].unsqueeze(1).to_broadcast([128, 8, 128]), op=ALU.mult),
                         r=[("Es", i2), ("cbs", i2)], w=[("wT", i2)])
                    yield
                    pr, kpr = self.psum()
                    pr2, kpr2 = self.psum()
                    S.op("pool", lambda: G.tensor_tensor(out=xdtt[i2][:].rearrange("p (h c) -> p h c", c=64),
                                                         in0=xs[:, tt, :].rearrange("p (h c) -> p h c", c=64),
                                                         in1=dt[:, tt, hs].unsqueeze(2).to_broadcast([128, 8, 64]), op=ALU.mult),
                         r=["xs", "dt"], w=[("xdtt", i2)])
                    S.op("pool", lambda: G.tensor_tensor(out=xendt[i2][:].rearrange("p (h c) -> p h c", c=64),
                                                         in0=xdtt[i2][:].rearrange("p (h c) -> p h c", c=64),
                                                         in1=eend[:, tt, hs].unsqueeze(2).to_broadcast([128, 8, 64]), op=ALU.mult),
                         r=[("xdtt", i2), "eend"], w=[("xendt", i2)])
                    S.op("pe", lambda pr=pr: PE.matmul(pr[:], lhsT=bmA[:, tt, :], rhs=xendt[i2][:], start=True, stop=True),
                         r=["bmA", ("xendt", i2)], w=[kpr])
                    S.op("pe", lambda pr2=pr2: PE.matmul(pr2[:], lhsT=bmB[:, tt, :], rhs=xendt[i2][:], start=True, stop=True),
                         r=["bmB", ("xendt", i2)], w=[kpr2])
                    for (ci_, prx, kprx, cc) in ((cA, pr, kpr, 0), (cB, pr2, kpr2, 1)):
                        S.op("dve", lambda cc=cc: V.tensor_tensor(
                            out=S32[:].rearrange("p (h c) -> p h c", c=64), in0=S32[:].rearrange("p (h c) -> p h c", c=64),
                            in1=edec[:, tt, cc, hs].unsqueeze(2).to_broadcast([128, 8, 64]), op=ALU.mult),
                            r=["S32", "edec"], w=["S32"])
                        S.op("dve", lambda prx=prx: V.tensor_tensor(out=S32[:], in0=prx[:], in1=S32[:], op=ALU.add),
                             r=[kprx, "S32"], w=["S32", kprx])
                        S.op("pool", lambda ci_=ci_: G.tensor_copy(out=Sbf[(ci_ + 1) % 4][:], in_=S32[:]),
                             r=["S32"], w=[("Sbf", (ci_ + 1) % 4)])
                    yield
                    pz, kpz = self.psum()
                    for kt in range(KT):
                        S.op("pe", lambda kt=kt, pz=pz: PE.matmul(pz[:], lhsT=self.hT[:, kt, tsl], rhs=wz[:, kt, :],
                                                                  start=(kt == 0), stop=(kt == KT - 1)), r=["wz", ("hT", tt)], w=[kpz])
                    S.op("act", lambda pz=pz: A.activation(out=zs[i2][:], in_=pz[:], func=AF.Silu), r=[kpz], w=[("zs", i2), kpz])
                    yield
                    py, kpy = self.psum()
                    for hh in range(8):
                        S.op("pe", lambda hh=hh, py=py: PE.matmul(py[:, hh * 64:(hh + 1) * 64], lhsT=wT[i2][:, hh, :],
                                                                  rhs=xdtt[i2][:, hh * 64:(hh + 1) * 64], start=True, stop=True),
                             r=[("wT", i2), ("xdtt", i2)], w=[kpy])
                    po, kpo = self.psum()
                    S.op("pe", lambda po=po: PE.matmul(po[:], lhsT=cmTA[:, tsl], rhs=Sbf[cA % 4][:], start=True, stop=False),
                         r=["cmTA", ("Sbf", cA % 4)], w=[kpo])
                    S.op("pe", lambda po=po: PE.matmul(po[:], lhsT=cmTB[:, tsl], rhs=Sbf[cB % 4][:], start=False, stop=True),
                         r=["cmTB", ("Sbf", cB % 4)], w=[kpo])
                    S.op("dve", lambda po=po: V.tensor_tensor(
                        out=t1[i2][:].rearrange("p (h c) -> p h c", c=64), in0=po[:].rearrange("p (h c) -> p h c", c=64),
                        in1=eacs[:, tt, hs].unsqueeze(2).to_broadcast([128, 8, 64]), op=ALU.mult),
                        r=[kpo, "eacs"], w=[("t1", i2), kpo])
                    S.op("dve", lambda py=py: V.tensor_tensor(out=t1[i2][:], in0=py[:], in1=t1[i2][:], op=ALU.add),
                         r=[kpy, ("t1", i2)], w=[("t1", i2), kpy])
                    S.op("pool", lambda: G.tensor_tensor(
                        out=t2[i2][:].rearrange("p (h c) -> p h c", c=64), in0=xs[:, tt, :].rearrange("p (h c) -> p h c", c=64),
                        in1=dsk[:, hs].unsqueeze(2).to_broadcast([128, 8, 64]), op=ALU.mult), r=["xs", "dsk"], w=[("t2", i2)])
                    S.op("pool", lambda: G.tensor_tensor(out=t2[i2][:], in0=t2[i2][:], in1=t1[i2][:], op=ALU.add),
                         r=[("t2", i2), ("t1", i2)], w=[("t2", i2)])
                    S.op("pool", lambda: G.tensor_tensor(out=t2[i2][:], in0=t2[i2][:], in1=zs[i2][:], op=ALU.mult),
                         r=[("t2", i2), ("zs", i2)], w=[("t2", i2)])
                    yield
                    S.op("act", lambda: A.activation(out=t1[i2][:], in_=t2[i2][:], func=AF.Square, accum_out=ss[i2][:, 0:1]),
                         r=[("t2", i2)], w=[("t1", i2), ("ss", i2)])
                    S.op("act", lambda: A.activation(out=ss[i2][:, 1:2], in_=ss[i2][:, 0:1], func=AF.Sqrt, bias=self.epsc[:, 1:2],
                                                     scale=1.0 / 512.0), r=[("ss", i2), "epsc"], w=[("ss", i2)])
                    S.op("dve", lambda: V.reciprocal(out=ss[i2][:, 1:2], in_=ss[i2][:, 1:2]), r=[("ss", i2)], w=[("ss", i2)])
                    S.op("dve", lambda: V.scalar_tensor_tensor(out=ytm[i2][:], in0=t2[i2][:], scalar=ss[i2][:, 1:2],
                                                               in1=nwbc[:, g * 512:(g + 1) * 512], op0=ALU.mult, op1=ALU.mult),
                         r=[("t2", i2), ("ss", i2), "nwbc"], w=[("ytm", i2)])
                    yield
                    pt, kpt = self.psum()
                    ptv = pt[:].bitcast(BF16)
                    for i in range(4):
                        S.op("pe", lambda i=i, ptv=ptv: PE.transpose(out=ptv[:, i * 128:(i + 1) * 128],
                                                                     in_=ytm[i2][:, i * 128:(i + 1) * 128], identity=self.identbf[:]),
                             r=[("ytm", i2), "identbf"], w=[kpt])
                    S.op("act", lambda ptv=ptv: A.activation(out=yTg[:, :, tsl], in_=ptv[:, 0:512].rearrange("p (a b) -> p a b", a=4),
                                                             func=AF.Copy), r=[kpt], w=["yTg", kpt])

                gens, nxt, rnd = [], 0, 0
                while nxt < NT or gens:
                    if nxt < NT and rnd % 4 == 0:
                        gens.append(tile_gen(nxt))
                        nxt += 1
                    for g_ in list(gens):
                        try:
                            next(g_)
                        except StopIteration:
                            gens.remove(g_)
                    rnd += 1
                for i in range(4):
                    S.dma("sp", ydst[g * 512 + i * 128:g * 512 + (i + 1) * 128, :], yTg[:, i, :], r=["yTg"], w=[("yT0", g * 4 + i)])
            S.barrier()


    def rwkv(self, l):
        nc, S = self.nc, self.S
        V, A, G, PE = nc.vector, nc.scalar, nc.gpsimd, nc.tensor
        Wl = self.P["w_in"][l]
        ydst = self.yT_dram[1]
        hTk = [("hT", t) for t in range(NT)]
        P_ = self.P

        def mm(out, lhsT, rhs, start, stop, r, w):
            S.op("pe", lambda: PE.matmul(out, lhsT=lhsT, rhs=rhs, start=start, stop=stop), r=r, w=w)

        with contextlib.ExitStack() as st:
            mask4 = self.sb(st, "mask4", [128, 4, 128], F32)
            maskL = self.sb(st, "maskL", [128, 2, 128], F32)
            bdm = self.sb(st, "bdm", [128, 128], F32)
            mEO = self.sb(st, "mEO", [128, 2], F32)
            hsel = self.sb(st, "hsel", [128, 2], F32)
            rm = self.sb(st, "rm128", [128, T], BF16)
            c05 = self.sb(st, "c05", [128, 1], F32)
            for j in range(4):
                S.op("pool", lambda j=j: G.affine_select(out=mask4[:, j, :], in_=self.ones[:], pattern=[[1, 128]],
                                                         compare_op=(ALU.is_gt if j % 2 == 0 else ALU.is_ge), fill=0.0,
                                                         base=0, channel_multiplier=-1), r=["ones"], w=["mask4"])
            for j in range(2):
                S.op("pool", lambda j=j: G.affine_select(out=maskL[:, j, :], in_=self.ones[:], pattern=[[-1, 128]],
                                                         compare_op=ALU.is_gt, fill=0.0, base=0, channel_multiplier=1),
                     r=["ones"], w=["maskL"])
            S.op("pool", lambda: G.memset(bdm[:], 0.0), w=["bdm"])
            S.op("pool", lambda: G.memset(bdm[0:64, 0:64], 1.0), w=["bdm"])
            S.op("pool", lambda: G.memset(bdm[64:128, 64:128], 1.0), w=["bdm"])
            for (t_, nm) in ((mEO, "mEO"), (hsel, "hsel")):
                S.op("pool", lambda t_=t_: G.memset(t_[0:64, 0:1], 1.0), w=[nm])
                S.op("pool", lambda t_=t_: G.memset(t_[64:128, 0:1], 0.0), w=[nm])
                S.op("pool", lambda t_=t_: G.memset(t_[0:64, 1:2], 0.0), w=[nm])
                S.op("pool", lambda t_=t_: G.memset(t_[64:128, 1:2], 1.0), w=[nm])
            S.op("pool", lambda: G.memset(rm[:], 1.0), w=["rm"])
            S.op("pool", lambda: G.memset(rm[:].rearrange("p (c j) -> p c j", j=128)[:, :, 0:1], 0.0), w=["rm"])
            S.op("pool", lambda: G.memset(c05[:], -0.5), w=["c05"])
            pc = {}
            for nm, src in (("mu_r", P_["rwkv_mu"][l, 0:1024]), ("mu_k", P_["rwkv_mu"][l, 1024:2048]),
                            ("mu_v", P_["rwkv_mu"][l, 2048:3072]), ("w0", P_["rwkv_w0"][l]), ("a0", P_["rwkv_a0"][l]),
                            ("k_k", P_["rwkv_k_k"][l]), ("k_a", P_["rwkv_k_a"][l]),
                            ("r_k", P_["rwkv_r_k"][l].rearrange("h k -> (h k)"))):
                t_ = self.sb(st, "pc_" + nm, [128, 8, 1], F32)
                S.dma("sp", t_[:], src.rearrange("(q p o) -> p q o", p=128, o=1), w=["pc_" + nm])
                pc[nm] = t_
            mul = self.sb(st, "mul", [128, 3], F32)
            S.dma("sp", mul[0:64, 0:1], P_["rwkv_mu"][l, 3072:3136].rearrange("(p o) -> p o", o=1), w=["mul"])
            S.dma("sp", mul[0:64, 1:2], P_["rwkv_mu"][l, 3136:3200].rearrange("(p o) -> p o", o=1), w=["mul"])
            S.dma("sp", mul[:, 2:3], P_["rwkv_mu"][l, 3200:3328].rearrange("(p o) -> p o", o=1), w=["mul"])
            nw0 = self.sb(st, "nw0", [128, 8, 1], F32)
            omka = self.sb(st, "omka", [128, 8, 1], F32)
            S.op("dve", lambda: V.tensor_scalar(out=nw0[:], in0=pc["w0"][:], scalar1=-1.0, scalar2=None, op0=ALU.mult),
                 r=["pc_w0"], w=["nw0"])
            S.op("dve", lambda: V.tensor_scalar(out=omka[:], in0=pc["k_a"][:], scalar1=-1.0, scalar2=1.0, op0=ALU.mult,
                                                op1=ALU.add), r=["pc_k_a"], w=["omka"])
            wl = self.sb(st, "wl", [128, KT, 256], BF16)
            w2 = self.sb(st, "w2", [64, D], BF16)
            a2 = self.sb(st, "a2", [64, D], BF16)
            g2 = self.sb(st, "g2", [128, D], BF16)
            S.dma("pool", wl[:], Wl[:, OFF_RWKV + 3072:OFF_RWKV + 3328].rearrange("(kt p) n -> p kt n", p=128), w=["wl"])
            S.dma("pool", w2[:], P_["rwkv_w2"][l], w=["w2"])
            S.dma("pool", a2[:], P_["rwkv_a2"][l], w=["a2"])
            S.dma("pool", g2[:], P_["rwkv_g2"][l], w=["g2"])
            txw = self.sb(st, "txw", [64, T], BF16)
            xaT = self.sb(st, "xaT", [64, T], BF16)
            sgT = self.sb(st, "sgT", [128, T], BF16)
            xraw = self.sb(st, "xraw", [128, T + 1], F32)
            F = [self.sb(st, "F%d" % i, [128, T], F32) for i in range(5)]
            S.op("pool", lambda: G.memset(xraw[:, 0:1], 0.0), w=["xraw0"])
            Y2 = xraw[:, 1:T + 1]
            Y3 = F[4]

            S.qset = {"F0", "F1", "F2", "F3", "F4", "xraw", "Pp", "Pc", "iP", "bT", "kT", "vT", "Vtm", "Btm", "Ktm",
                      ("AR", 0), ("AR", 1), "Pend0", "Pend1", "bon"}

            def Q(eng, fn, r=(), w=()):
                for q in range(4):
                    qs_ = slice(q * 512, (q + 1) * 512)
                    rr = [(k, q) if k in S.qset else k for k in r]
                    ww = [(k, q) if k in S.qset else k for k in w]
                    S.op(eng, lambda: fn(qs_, q), r=rr, w=ww)

            def proj_shift(wt, c0, m, mucol, dst, dkey, func=None, rows=128):
                for tq in range(4):
                    pb, kp = self.psum()
                    for kt in range(KT):
                        mm(pb[0:rows, :], wt[:, kt, c0:c0 + m], self.hT[:, kt, tq * 512:(tq + 1) * 512], kt == 0, kt == KT - 1,
                           [wt_key] + hTk[tq * 4:tq * 4 + 4], [kp])
                    S.op("act", lambda pb=pb, tq=tq: A.activation(out=xraw[0:rows, 1 + tq * 512:1 + (tq + 1) * 512],
                                                                  in_=pb[0:rows, :], func=AF.Copy), r=[kp], w=[("xraw", tq), kp])
                dk = (lambda q: (dkey, q)) if dkey in S.qset else (lambda q: dkey)
                for q in range(4):
                    lo, hi = q * 512, (q + 1) * 512
                    xr = [("xraw", q)] + ([("xraw", q - 1)] if q else ["xraw0"])
                    S.op("dve", lambda: V.tensor_tensor(out=F[4][0:rows, lo:hi], in0=xraw[0:rows, lo:hi], in1=xraw[0:rows, lo + 1:hi + 1],
                                                        op=ALU.subtract), r=xr, w=[("F4", q)])
                    if func is None:
                        S.op("dve", lambda: V.scalar_tensor_tensor(out=dst[0:rows, lo:hi], in0=F[4][0:rows, lo:hi], scalar=mucol,
                                                                   in1=xraw[0:rows, lo + 1:hi + 1], op0=ALU.mult, op1=ALU.add),
                             r=[("F4", q), ("xraw", q), "mul"] + list(pc_keys), w=[dk(q)])
                    else:
                        S.op("dve", lambda: V.scalar_tensor_tensor(out=F[4][0:rows, lo:hi], in0=F[4][0:rows, lo:hi], scalar=mucol,
                                                                   in1=xraw[0:rows, lo + 1:hi + 1], op0=ALU.mult, op1=ALU.add),
                             r=[("F4", q), ("xraw", q), "mul"] + list(pc_keys), w=[("F4", q)])
                        S.op("act", lambda: A.activation(out=dst[0:rows, lo:hi], in_=F[4][0:rows, lo:hi], func=func),
                             r=[("F4", q)], w=[dk(q)])

            pc_keys = ["pc_mu_r", "pc_mu_k", "pc_mu_v"]
            wt_key = "wl"
            proj_shift(wl, 0, 64, mul[0:64, 0:1], txw, "txw", AF.Tanh, rows=64)
            proj_shift(wl, 64, 64, mul[0:64, 1:2], xaT, "xaT", AF.Copy, rows=64)
            proj_shift(wl, 128, 128, mul[:, 2:3], sgT, "sgT", AF.Sigmoid, rows=128)
            wrkv1 = self.sb(st, "wrkv", [128, KT, 384], BF16)
            wrkv = [wrkv1, wrkv1]
            Pp = self.sb(st, "Pp", [128, T], BF16)
            Pc = self.sb(st, "Pc", [128, T], BF16)
            PendL = [self.sb(st, "Pend", [128, NT], F32) for _ in range(2)]
            iP = self.sb(st, "iP", [128, T], BF16)
            bT = self.sb(st, "bT", [128, T], BF16)
            kT = self.sb(st, "kT", [128, T], BF16)
            vT = self.sb(st, "vT", [128, T], BF16)
            AR = [self.sb(st, "AR", [128, NT, 2, 128], BF16) for _ in range(2)]
            Vtm = self.sb(st, "Vtm", [128, NT, 128], BF16)
            Btm = self.sb(st, "Btm", [128, NT, 128], BF16)
            aT = Vtm[:].rearrange("p t j -> p (t j)")
            rT = Btm[:].rearrange("p t j -> p (t j)")
            Ktm = self.sb(st, "Ktm", [128, NT, 128], BF16)
            bon = self.sb(st, "bon", [128, NT, 2], F32)
            st32 = self.sb(st, "st32", [128, 2 * NT, 4], F32)
            lnw = self.sb(st, "lnwb", [128, 128], F32)
            lnb = self.sb(st, "lnbb", [128, 128], F32)
            Z32 = self.sb(st, "Z32", [128, 128], F32)
            Zt = self.sb(st, "Zt", [128, 128], F32)
            Zbf = [self.sb(st, "Zbf", [128, 128], BF16) for _ in range(2)]
            Wsb = [self.sb(st, "Wsb", [128, 128], BF16) for _ in range(2)]
            Usb = [self.sb(st, "Usb", [128, 128], BF16) for _ in range(2)]
            NS = 8
            abrb = [self.sb(st, "abrb", [128, 4, 128], BF16) for _ in range(NS)]
            akrk = [self.sb(st, "akrk", [128, 4, 128], BF16) for _ in range(NS)]
            L0 = [self.sb(st, "L0", [128, 2, 128], BF16) for _ in range(4)]
            XX = [[self.sb(st, "XX", [128, 4, 128], BF16) for _ in range(4)] for _ in range(2)]
            Tt = [self.sb(st, "Tt", [128, 2, 128], BF16) for _ in range(NS)]

            def load_w(p):
                b = 0
                for j in range(3):
                    c0 = OFF_RWKV + j * 1024 + p * 128
                    S.dma("pool", wrkv[b][:, :, j * 128:(j + 1) * 128], Wl[:, c0:c0 + 128].rearrange("(kt p) n -> p kt n", p=128),
                          w=[("wrkv", b)])

            def projA(p):
                nonlocal wt_key
                load_w(p)
                wt_key = ("wrkv", 0)
                proj_shift(wrkv[0], 128, 128, pc["mu_k"][:, p, :], F[0], "F0")
                proj_shift(wrkv[0], 0, 128, pc["mu_r"][:, p, :], F[1], "F1")
                proj_shift(wrkv[0], 256, 128, pc["mu_v"][:, p, :], vT, "vT", AF.Copy)

            filler = [None]
            projA(0)
            for p in range(8):
                b = 0
                fs_ = slice(p * 128, (p + 1) * 128)
                S.dma("sp", lnw[:], P_["rwkv_ln_w"][l, fs_].partition_broadcast(128), w=["lnw"])
                S.dma("sp", lnb[:], P_["rwkv_ln_b"][l, fs_].partition_broadcast(128), w=["lnb"])
                def prepB1(p, fs_):
                    for tq in range(4):
                        pb, kp = self.psum()
                        qs_ = slice(tq * 512, (tq + 1) * 512)
                        mm(pb[:], w2[:, fs_], txw[:, qs_], True, True, ["w2", "txw"], [kp])
                        S.op("act", lambda pb=pb: A.activation(out=F[2][:, qs_], in_=pb[:], func=AF.Exp, bias=nw0[:, p, :], scale=-1.0),
                             r=[kp, "nw0"], w=[("F2", tq), kp])
                    yield
                    Q("act", lambda qs, q: A.activation(out=F[2][:, qs], in_=F[2][:, qs], func=AF.Ln, bias=self.epsc[:, 3:4], scale=1.0),
                      r=["F2", "epsc"], w=["F2"])
                    yield
                    Q("act", lambda qs, q: A.activation(out=F[2][:, qs], in_=F[2][:, qs], func=AF.Exp, bias=c05[:, 0:1], scale=-1.0),
                      r=["F2", "c05"], w=["F2"])
                    yield
                    Q("dve", lambda qs, q: V.tensor_tensor_scan(out=F[3][:, qs], data0=rm[:, qs], data1=F[2][:, qs], initial=0.0,
                                                                op0=ALU.mult, op1=ALU.add), r=["rm", "F2"], w=["F3"])
                    yield
                    Q("dve", lambda qs, q: V.tensor_tensor(out=F[2][:, qs], in0=F[3][:, qs], in1=F[2][:, qs], op=ALU.subtract),
                      r=["F2", "F3"], w=["F2"])
                    yield
                    Q("act", lambda qs, q: A.activation(out=Pp[:, qs], in_=F[2][:, qs], func=AF.Exp, scale=-1.0), r=["F2"], w=["Pp"])
                    yield
                    Q("act", lambda qs, q: A.activation(out=Pc[:, qs], in_=F[3][:, qs], func=AF.Exp, scale=-1.0), r=["F3"], w=["Pc"])
                    yield
                    Q("act", lambda qs, q: A.activation(out=iP[:, qs], in_=F[3][:, qs], func=AF.Exp), r=["F3"], w=["iP"])
                    yield
                    Q("act", lambda qs, q: A.activation(out=PendL[p % 2][:, q * 4:(q + 1) * 4],
                                                        in_=F[3][:, qs].rearrange("p (t j) -> p t j", j=128)[:, :, 127],
                                                        func=AF.Exp, scale=-1.0), r=["F3"], w=["Pend%d" % (p % 2)])
                    yield
                    for tq in range(4):
                        pb, kp = self.psum()
                        qs_ = slice(tq * 512, (tq + 1) * 512)
                        mm(pb[:], a2[:, fs_], xaT[:, qs_], True, True, ["a2", "xaT"], [kp])
                        S.op("act", lambda pb=pb: A.activation(out=F[2][:, qs_], in_=pb[:], func=AF.Sigmoid, bias=pc["a0"][:, p, :],
                                                               scale=1.0), r=[kp, "pc_a0", ("Pp", tq)], w=[("F2", tq), kp])
                    yield
                    Q("dve", lambda qs, q: V.tensor_scalar(out=F[3][:, qs], in0=F[0][:, qs], scalar1=pc["k_k"][:, p, :], scalar2=None,
                                                           op0=ALU.mult), r=["F0", "pc_k_k", "Pc", "iP", "Pend%d" % (p % 2)], w=["F3"])
                    yield
                    Q("act", lambda qs, q: A.activation(out=F[4][:, qs], in_=F[3][:, qs], func=AF.Square), r=["F3"], w=["F4"])
                    yield
                    for tq in range(4):
                        pb, kp = self.psum()
                        qs_ = slice(tq * 512, (tq + 1) * 512)
                        mm(pb[:], bdm[:], F[4][:, qs_], True, True, ["bdm", ("F4", tq)], [kp])
                        S.op("act", lambda pb=pb: A.activation(out=F[4][:, qs_], in_=pb[:], func=AF.Sqrt), r=[kp], w=[("F4", tq), kp])
                    yield
                    Q("dve", lambda qs, q: V.tensor_scalar(out=F[4][:, qs], in0=F[4][:, qs], scalar1=1e-12, scalar2=None, op0=ALU.max),
                      r=["F4"], w=["F4"])
                    yield
                    Q("dve", lambda qs, q: V.reciprocal(out=F[4][:, qs], in_=F[4][:, qs]), r=["F4"], w=["F4"])
                    yield
                    Q("dve", lambda qs, q: V.tensor_tensor(out=F[3][:, qs], in0=F[3][:, qs], in1=F[4][:, qs], op=ALU.mult),
                      r=["F3", "F4"], w=["F3"])

                    yield

                def prepB2(p, fs_):
                    Q("dve", lambda qs, q: V.scalar_tensor_tensor(out=aT[:, qs], in0=F[3][:, qs], scalar=-1.0, in1=Pp[:, qs], op0=ALU.mult,
                                                                  op1=ALU.mult), r=["F3", "Pp"], w=["Vtm"])
                    Q("pool", lambda qs, q: G.tensor_tensor(out=F[4][:, qs], in0=F[3][:, qs], in1=F[2][:, qs], op=ALU.mult),
                      r=["F3", "F2"], w=["F4"])
                    Q("pool", lambda qs, q: G.tensor_tensor(out=bT[:, qs], in0=F[4][:, qs], in1=iP[:, qs], op=ALU.mult),
                      r=["F4", "iP"], w=["bT"])
                    Q("dve", lambda qs, q: V.tensor_scalar(out=F[2][:, qs], in0=F[2][:, qs], scalar1=pc["k_a"][:, p, :],
                                                           scalar2=omka[:, p, :], op0=ALU.mult, op1=ALU.add),
                      r=["F2", "pc_k_a", "omka", "F4"], w=["F2"])
                    Q("dve", lambda qs, q: V.tensor_tensor(out=F[0][:, qs], in0=F[0][:, qs], in1=F[2][:, qs], op=ALU.mult),
                      r=["F0", "F2", "F3"], w=["F0"])
                    Q("pool", lambda qs, q: G.tensor_tensor(out=kT[:, qs], in0=F[0][:, qs], in1=iP[:, qs], op=ALU.mult),
                      r=["F0", "iP"], w=["kT"])
                    Q("dve", lambda qs, q: V.tensor_tensor(out=rT[:, qs], in0=F[1][:, qs], in1=Pc[:, qs], op=ALU.mult),
                      r=["F1", "Pc"], w=["Btm"])
                    Q("dve", lambda qs, q: V.scalar_tensor_tensor(out=F[1][:, qs], in0=F[1][:, qs], scalar=pc["r_k"][:, p, :],
                                                                  in1=F[0][:, qs], op0=ALU.mult, op1=ALU.mult),
                      r=["F1", "F0", "pc_r_k", "Btm"], w=["F1"])
                    for h in range(2):
                        Q("act", lambda qs, q, h=h: A.activation(out=AR[h][:, q * 4:(q + 1) * 4, 0, :], in_=Vtm[:, q * 4:(q + 1) * 4, :],
                                                                 func=AF.Identity, scale=mEO[:, h:h + 1]), r=["Vtm", "mEO"], w=[("AR", h)])
                        Q("dve", lambda qs, q, h=h: V.tensor_scalar(out=AR[h][:, q * 4:(q + 1) * 4, 1, :], in0=Btm[:, q * 4:(q + 1) * 4, :],
                                                                    scalar1=mEO[:, h:h + 1], scalar2=None, op0=ALU.mult),
                          r=["Btm", "mEO"], w=[("AR", h)])
                    for (src, skey, dst, dkey) in ((vT, "vT", Vtm, "Vtm"), (bT, "bT", Btm, "Btm"), (kT, "kT", Ktm, "Ktm")):
                        for t4 in range(4):
                            pb, kp = self.psum()
                            pbv = pb[:].bitcast(BF16)
                            for j in range(4):
                                tt = t4 * 4 + j
                                S.op("pe", lambda j=j, tt=tt, pbv=pbv, src=src: PE.transpose(
                                    out=pbv[:, j * 128:(j + 1) * 128], in_=src[:, tt * 128:(tt + 1) * 128], identity=self.identbf[:]),
                                    r=[(skey, t4), "identbf"], w=[kp])
                            S.op("act", lambda t4=t4, pbv=pbv, dst=dst: A.activation(
                                out=dst[:, t4 * 4:(t4 + 1) * 4, :], in_=pbv[:, 0:512].rearrange("p (a b) -> p a b", a=4), func=AF.Copy),
                                r=[kp, (("AR", 0), t4), (("AR", 1), t4)], w=[(dkey, t4), kp])
                    for t4 in range(4):
                        pb, kp = self.psum()
                        for j in range(4):
                            tt = t4 * 4 + j
                            mm(pb[:, j * 2:(j + 1) * 2], F[1][:, tt * 128:(tt + 1) * 128], hsel[:], True, True, [("F1", t4), "hsel"], [kp])
                        S.op("dve", lambda pb=pb, t4=t4: V.tensor_copy(out=bon[:, t4 * 4:(t4 + 1) * 4, :].rearrange("p t h -> p (t h)"),
                                                                       in_=pb[:, 0:8]), r=[kp], w=[("bon", t4), kp])

                ytm = Y2[:].rearrange("p (t j) -> p t j", j=128)
                ysq = Y3[:].rearrange("p (t j) -> p t j", j=128)

                def inv_group(gi, pending=()):
                    pending = list(pending)
                    tiles = range(gi * 4, gi * 4 + 4)
                    for tt in tiles:
                        sl = tt % NS
                        tsl = slice(tt * 128, (tt + 1) * 128)
                        p1, k1 = self.psum()
                        p2, k2 = self.psum()
                        p3, k3 = self.psum()
                        for h in range(2):
                            rhs = AR[h][:, tt].rearrange("p a j -> p (a j)")
                            mm(p1[:, h * 256:(h + 1) * 256], bT[:, tsl], rhs, True, True, ["bT", ("AR", h)], [k1])
                            mm(p2[:, h * 256:(h + 1) * 256], kT[:, tsl], rhs, True, True, ["kT", ("AR", h)], [k2])
                            mm(p3[:, h * 128:(h + 1) * 128], AR[h][:, tt, 0, :], bT[:, tsl], True, True, [("AR", h), "bT"], [k3])
                        S.op("dve", lambda p1=p1, sl=sl: V.tensor_tensor(out=abrb[sl][:].rearrange("p a j -> p (a j)"), in0=p1[:],
                                                                        in1=mask4[:].rearrange("p a j -> p (a j)"), op=ALU.mult),
                             r=[k1, "mask4"], w=[("abrb", sl), k1])
                        S.op("dve", lambda p2=p2, sl=sl: V.tensor_tensor(out=akrk[sl][:].rearrange("p a j -> p (a j)"), in0=p2[:],
                                                                        in1=mask4[:].rearrange("p a j -> p (a j)"), op=ALU.mult),
                             r=[k2, "mask4"], w=[("akrk", sl), k2])
                        S.op("dve", lambda p3=p3, sl=sl: V.tensor_tensor(out=L0[sl % 4][:].rearrange("p a j -> p (a j)"), in0=p3[:, 0:256],
                                                                        in1=maskL[:].rearrange("p a j -> p (a j)"), op=ALU.mult),
                             r=[k3, "maskL"], w=[("L0", sl % 4), k3])
                        for h in range(2):
                            S.op("pool", lambda h=h, sl=sl: G.tensor_tensor(out=Tt[sl][:, h, :], in0=abrb[sl][:, 2 * h, :],
                                                                            in1=self.identbf[:], op=ALU.add),
                                 r=[("abrb", sl), "identbf"], w=[("Tt", sl)])

                    def Xk(k, sl, h):
                        return (L0[sl % 4][:, h, :], ("L0", sl % 4)) if k == 0 else (XX[k % 2][sl % 4][:, 2 * h, :], ("XX", k % 2, sl % 4))

                    def Xtk(k, sl, h):
                        return (abrb[sl][:, 2 * h, :], ("abrb", sl)) if k == 0 else (XX[k % 2][sl % 4][:, 2 * h + 1, :], ("XX", k % 2, sl % 4))

                    for k in range(7):
                        sqb = {}
                        if k <= 5:
                            for tt in tiles:
                                sl = tt % NS
                                pb, kp = self.psum()
                                sqb[tt] = (pb, kp)
                                for h in range(2):
                                    x, kx = Xk(k, sl, h)
                                    xt, kxt = Xtk(k, sl, h)
                                    mm(pb[:, (2 * h) * 128:(2 * h + 1) * 128], xt, x, True, True, [kx, kxt], [kp])
                                    if k < 5:
                                        mm(pb[:, (2 * h + 1) * 128:(2 * h + 2) * 128], x, xt, True, True, [kx, kxt], [kp])
                        ttb = []
                        if k >= 1:
                            for t2 in range(2):
                                pb, kp = self.psum()
                                ttb.append((pb, kp))
                                for j in range(2):
                                    sl = (gi * 4 + t2 * 2 + j) % NS
                                    for h in range(2):
                                        x1, kx1 = Xk(k, sl, h)
                                        mm(pb[:, (j * 2 + h) * 128:(j * 2 + h + 1) * 128], x1, Tt[sl][:, h, :], True, True,
                                           [kx1, ("Tt", sl)], [kp])
                        if k <= 5:
                            for tt in tiles:
                                sl = tt % NS
                                pb, kp = sqb[tt]
                                kn = ("XX", (k + 1) % 2, sl % 4)
                                if k < 5:
                                    S.op("act", lambda pb=pb, sl=sl, k=k: A.activation(
                                        out=XX[(k + 1) % 2][sl % 4][:].rearrange("p a j -> p (a j)"), in_=pb[:], func=AF.Copy),
                                        r=[kp], w=[kn, kp])
                                else:
                                    S.op("act", lambda pb=pb, sl=sl, k=k: A.activation(
                                        out=XX[(k + 1) % 2][sl % 4][:, 0:4:2, :],
                                        in_=pb[:].rearrange("p (a j) -> p a j", j=128)[:, 0:4:2, :], func=AF.Copy), r=[kp], w=[kn, kp])
                        for t2, (pb, kp) in enumerate(ttb):
                            for j in range(2):
                                sl = (gi * 4 + t2 * 2 + j) % NS
                                S.op("dve", lambda pb=pb, sl=sl, j=j: V.tensor_tensor(
                                    out=Tt[sl][:].rearrange("p a j -> p (a j)"), in0=pb[:, j * 256:(j + 1) * 256],
                                    in1=Tt[sl][:].rearrange("p a j -> p (a j)"), op=ALU.add), r=[kp, ("Tt", sl)], w=[("Tt", sl), kp])
                        if pending and k >= 1:
                            chain_tile(pending.pop(0))
                        if filler[0] is not None:
                            next(filler[0], None)
                    while pending:
                        chain_tile(pending.pop(0))

                def chain_tile(tt):
                    if True:
                        sl = tt % NS
                        i2 = tt % 2
                        tsl = slice(tt * 128, (tt + 1) * 128)
                        zb, kz = Zbf[i2], ("Zbf", i2)
                        pw, kpw = self.psum()
                        mm(pw[:, 0:128], AR[0][:, tt, 0, :], zb[:], True, False, [("AR", 0), kz], [kpw])
                        mm(pw[:, 0:128], AR[1][:, tt, 0, :], zb[:], False, False, [("AR", 1), kz], [kpw])
                        for h in range(2):
                            mm(pw[:, h * 64:(h + 1) * 64], akrk[sl][:, 2 * h, :], Vtm[:, tt, h * 64:(h + 1) * 64], False, h == 1,
                               [("akrk", sl), "Vtm"], [kpw])
                        S.op("act", lambda pw=pw: A.activation(out=Wsb[i2][:], in_=pw[:, 0:128], func=AF.Copy),
                             r=[kpw], w=[("Wsb", i2), kpw])
                        pu, kpu = self.psum()
                        for h in range(2):
                            mm(pu[:, h * 64:(h + 1) * 64], Tt[sl][:, h, :], Wsb[i2][:, h * 64:(h + 1) * 64], True, True,
                               [("Tt", sl), ("Wsb", i2)], [kpu])
                        S.op("act", lambda pu=pu: A.activation(out=Usb[i2][:], in_=pu[:, 0:128], func=AF.Copy),
                             r=[kpu], w=[("Usb", i2), kpu])
                        py, kpy = self.psum()
                        mm(py[:, 0:128], AR[0][:, tt, 1, :], zb[:], True, False, [("AR", 0), kz], [kpy])
                        mm(py[:, 0:128], AR[1][:, tt, 1, :], zb[:], False, False, [("AR", 1), kz], [kpy])
                        for h in range(2):
                            mm(py[:, h * 64:(h + 1) * 64], abrb[sl][:, 2 * h + 1, :], Usb[i2][:, h * 64:(h + 1) * 64], False, False,
                               [("abrb", sl), ("Usb", i2)], [kpy])
                            mm(py[:, h * 64:(h + 1) * 64], akrk[sl][:, 2 * h + 1, :], Vtm[:, tt, h * 64:(h + 1) * 64], False, h == 1,
                               [("akrk", sl), "Vtm"], [kpy])
                        S.op("act", lambda py=py: A.activation(out=ytm[:, tt, :], in_=py[:, 0:128], func=AF.Copy),
                             r=[kpy], w=["xraw", kpy])
                        pz, kpz = self.psum()
                        mm(pz[:, 0:128], Btm[:, tt, :], Usb[i2][:], True, False, ["Btm", ("Usb", i2)], [kpz])
                        mm(pz[:, 0:128], Ktm[:, tt, :], Vtm[:, tt, :], False, True, ["Ktm", "Vtm"], [kpz])
                        S.op("dve", lambda pz=pz: V.tensor_tensor(out=Zt[:], in0=pz[:, 0:128], in1=bdm[:], op=ALU.mult),
                             r=[kpz, "bdm"], w=["Zt", kpz])
                        S.op("dve", lambda: V.tensor_tensor(out=Zt[:], in0=Zt[:], in1=Z32[:], op=ALU.add), r=["Zt", "Z32"], w=["Zt"])
                        S.op("dve", lambda: V.tensor_scalar(out=Z32[:], in0=Zt[:], scalar1=PendL[p % 2][:, tt:tt + 1],
                                                            scalar2=None, op0=ALU.mult), r=["Zt", "Pend%d" % (p % 2)], w=["Z32"])
                        S.op("pool", lambda: G.tensor_copy(out=Zbf[(tt + 1) % 2][:], in_=Z32[:]), r=["Z32"], w=[("Zbf", (tt + 1) % 2)])

                S.op("pool", lambda: G.memset(Z32[:], 0.0), w=["Z32"])
                S.op("pool", lambda: G.memset(Zbf[0][:], 0.0), w=[("Zbf", 0)])
                if p == 0:
                    for _ in prepB1(0, fs_):
                        pass
                prepB2(p, fs_)
                if p + 1 < 8:
                    projA(p + 1)
                    filler[0] = prepB1(p + 1, slice((p + 1) * 128, (p + 2) * 128))
                inv_group(0)
                for gi in range(1, 4):
                    inv_group(gi, pending=range((gi - 1) * 4, gi * 4))
                for tt in range(12, 16):
                    chain_tile(tt)
                if filler[0] is not None:
                    for _ in filler[0]:
                        pass
                    filler[0] = None
                y16, yTp = Btm, kT
                def output_phase(p=p, fs_=fs_, ytm=ytm, ysq=ysq):
                    y3 = ytm.rearrange("p t (h c) -> p (t h) c", c=64)
                    q3 = ysq.rearrange("p t (h c) -> p (t h) c", c=64)
                    S.op("act", lambda: A.activation(out=Y3[:], in_=Y2[:], func=AF.Square), r=["xraw"], w=["F4"])
                    S.op("dve", lambda: V.tensor_reduce(out=st32[:, :, 0], in_=y3, axis=AX.X, op=ALU.add), r=["xraw"], w=["st32"])
                    S.op("dve", lambda: V.tensor_reduce(out=st32[:, :, 1], in_=q3, axis=AX.X, op=ALU.add), r=["F4"], w=["st32"])
                    S.op("dve", lambda: V.tensor_scalar(out=st32[:, :, 0], in0=st32[:, :, 0], scalar1=1.0 / 64.0, scalar2=None,
                                                        op0=ALU.mult), r=["st32"], w=["st32"])
                    S.op("dve", lambda: V.tensor_tensor(out=st32[:, :, 2], in0=st32[:, :, 0], in1=st32[:, :, 0], op=ALU.mult),
                         r=["st32"], w=["st32"])
                    S.op("dve", lambda: V.scalar_tensor_tensor(out=st32[:, :, 1], in0=st32[:, :, 1], scalar=1.0 / 64.0, in1=st32[:, :, 2],
                                                               op0=ALU.mult, op1=ALU.subtract), r=["st32"], w=["st32"])
                    S.op("act", lambda: A.activation(out=st32[:, :, 1], in_=st32[:, :, 1], func=AF.Sqrt, bias=self.epsc[:, 2:3], scale=1.0),
                         r=["st32", "epsc"], w=["st32"])
                    S.op("dve", lambda: V.reciprocal(out=st32[:, :, 1], in_=st32[:, :, 1]), r=["st32"], w=["st32"])
                    S.op("dve", lambda: V.tensor_tensor(out=y3, in0=y3, in1=st32[:, :, 0:1].to_broadcast([128, 2 * NT, 64]),
                                                        op=ALU.subtract), r=["xraw", "st32"], w=["xraw"])
                    S.op("dve", lambda: V.tensor_tensor(out=y3, in0=y3, in1=st32[:, :, 1:2].to_broadcast([128, 2 * NT, 64]),
                                                        op=ALU.mult), r=["xraw", "st32"], w=["xraw"])
                    S.op("pool", lambda: G.tensor_tensor(out=ytm, in0=ytm, in1=lnw[:].unsqueeze(1).to_broadcast([128, NT, 128]),
                                                         op=ALU.mult), r=["xraw", "lnw"], w=["xraw"])
                    S.op("pool", lambda: G.tensor_tensor(out=ytm, in0=ytm, in1=lnb[:].unsqueeze(1).to_broadcast([128, NT, 128]),
                                                         op=ALU.add), r=["xraw", "lnb"], w=["xraw"])
                    S.op("dve", lambda: V.tensor_tensor(out=q3, in0=Vtm[:].rearrange("p t (h c) -> p (t h) c", c=64),
                                                        in1=bon[:].rearrange("p t h -> p (t h)").unsqueeze(2).to_broadcast([128, 2 * NT, 64]),
                                                        op=ALU.mult), r=["Vtm", "bon", "F4"], w=["F4"])
                    S.op("dve", lambda: V.tensor_tensor(out=Y2[:], in0=Y2[:], in1=Y3[:], op=ALU.add), r=["xraw", "F4"], w=["xraw"])
                    for t4 in range(4):
                        pb, kp = self.psum()
                        for j in range(4):
                            tt = t4 * 4 + j
                            mm(pb[:, j * 128:(j + 1) * 128], sgT[:, tt * 128:(tt + 1) * 128], g2[:, fs_], True, True, ["sgT", "g2"], [kp])
                        S.op("dve", lambda pb=pb, t4=t4: V.tensor_tensor(
                            out=y16[:, t4 * 4:(t4 + 1) * 4, :], in0=pb[:].rearrange("p (a j) -> p a j", j=128),
                            in1=ytm[:, t4 * 4:(t4 + 1) * 4, :], op=ALU.mult), r=[kp, "xraw"], w=["Btm", kp])
                    for t4 in range(4):
                        pb, kp = self.psum()
                        pbv = pb[:].bitcast(BF16)
                        for j in range(4):
                            tt = t4 * 4 + j
                            S.op("pe", lambda j=j, tt=tt, pbv=pbv: PE.transpose(out=pbv[:, j * 128:(j + 1) * 128], in_=y16[:, tt, :],
                                                                               identity=self.identbf[:]), r=["Btm", "identbf"], w=[kp])
                        S.op("act", lambda t4=t4, pbv=pbv: A.activation(out=yTp[:, t4 * 512:(t4 + 1) * 512], in_=pbv[:, 0:512], func=AF.Copy),
                             r=[kp], w=["kT", kp])
                    S.dma("sp", ydst[fs_, :], yTp[:], r=["kT"], w=[("yT1", p)])
                output_phase()
            S.barrier()
            S.qset = set()


    def merge(self, l):
        nc, S = self.nc, self.S
        V, A, G, PE = nc.vector, nc.scalar, nc.gpsimd, nc.tensor
        Wl = self.P["w_in"][l]
        brw = [self.P["w_br_ssd"][l], self.P["w_br_rwkv"][l], self.P["w_br_hgrn"][l]]
        hTk = [("hT", t) for t in range(NT)]
        with contextlib.ExitStack() as st:
            mT = self.sb(st, "mT", [128, KT, T], F32)
            wbrs = [self.sb(st, "wbr", [128, KT, D], BF16) for _ in range(2)]
            wgts = [self.sb(st, "wgt", [128, KT, D], BF16) for _ in range(2)]
            st1 = contextlib.ExitStack()
            st1.__enter__()
            yq = [self.sb(st1, "yq", [128, KT, 512], BF16) for _ in range(2)]
            sg = [self.sb(st1, "sgm", [128, 512], BF16) for _ in range(2)]
            tmp = [self.sb(st1, "tmpm", [128, 512], F32) for _ in range(2)]
            cnt = 0
            def load_br(i):
                S.dma("pool", wbrs[i % 2][:], brw[i].rearrange("(kt p) n -> p kt n", p=128), w=[("wbr", i % 2)])
                c0 = OFF_GATES + i * 1024
                S.dma("pool", wgts[i % 2][:], Wl[:, c0:c0 + 1024].rearrange("(kt p) n -> p kt n", p=128), w=[("wgt", i % 2)])

            load_br(0)
            load_br(1)
            for i in range(3):
                wbr, wgt = wbrs[i % 2], wgts[i % 2]
                kwb, kwg = ("wbr", i % 2), ("wgt", i % 2)
                if i == 2:
                    load_br(2)
                for q in range(4):
                    qs_ = slice(q * 512, (q + 1) * 512)
                    yb = (i * 4 + q) % 2
                    S.dma("sp", yq[yb][:], self.yT_dram[i][:, qs_].rearrange("(kt p) n -> p kt n", p=128),
                          r=[("yT%d" % i, k) for k in range(8)], w=[("yq", yb)])
                    for ot in range(KT):
                        os_ = slice(ot * 128, (ot + 1) * 128)
                        pg, kg = self.psum()
                        pb, kb = self.psum()
                        for kt in range(KT):
                            S.op("pe", lambda kt=kt, pg=pg: PE.matmul(pg[:], lhsT=wgt[:, kt, os_], rhs=self.hT[:, kt, qs_],
                                                                      start=(kt == 0), stop=(kt == KT - 1)),
                                 r=[kwg] + hTk[q * 4:q * 4 + 4], w=[kg])
                        for kt in range(KT):
                            S.op("pe", lambda kt=kt, pb=pb: PE.matmul(pb[:], lhsT=wbr[:, kt, os_], rhs=yq[yb][:, kt, :],
                                                                      start=(kt == 0), stop=(kt == KT - 1)),
                                 r=[kwb, ("yq", yb)], w=[kb])
                        c2 = cnt % 2
                        cnt += 1
                        S.op("act", lambda pg=pg, c2=c2: A.activation(out=sg[c2][:], in_=pg[:], func=AF.Sigmoid),
                             r=[kg], w=[("sgm", c2), kg])
                        if i == 0:
                            S.op("dve", lambda pb=pb, c2=c2: V.tensor_tensor(out=mT[:, ot, qs_], in0=pb[:], in1=sg[c2][:], op=ALU.mult),
                                 r=[kb, ("sgm", c2)], w=[("mT", q), kb])
                        else:
                            S.op("dve", lambda pb=pb, c2=c2: V.tensor_tensor(out=tmp[c2][:], in0=pb[:], in1=sg[c2][:], op=ALU.mult),
                                 r=[kb, ("sgm", c2)], w=[("tmpm", c2), kb])
                            S.op("pool", lambda c2=c2: G.tensor_tensor(out=mT[:, ot, qs_], in0=mT[:, ot, qs_], in1=tmp[c2][:],
                                                                       op=ALU.add), r=[("tmpm", c2), ("mT", q)], w=[("mT", q)])
            self.dbg_dump("merged%d" % l, lambda o: S.dma("sp", o.rearrange("(kt p) n -> p kt n", p=128), mT[:],
                                                          r=[("mT", q) for q in range(4)]))
            S.barrier()
            st1.__exit__(None, None, None)
            wo = wbrs[1]
            S.dma("pool", wo[:], self.P["w_out"][l].rearrange("(kt p) n -> p kt n", p=128), w=[("wbr", 1)])
            gbc = self.sb(st, "gbc1", [128, D], F32)
            bbc = self.sb(st, "bbc1", [128, D], F32)
            S.dma("sp", gbc[:], self.P["ln1_g"][l].partition_broadcast(128), w=["gbc"])
            S.dma("sp", bbc[:], self.P["ln1_b"][l].partition_broadcast(128), w=["bbc"])
            lnw = self.ln_alloc(st)
            h1 = [self.sb(st, "h1m", [128, D], F32) for _ in range(2)]
            mbf1 = self.sb(st, "mbf", [128, KT, 128], BF16)
            mbf = [mbf1, mbf1]
            for tt in range(NT):
                s2 = tt % 2
                q = tt // 4
                tsl = slice(tt * 128, (tt + 1) * 128)
                S.op("act", lambda: A.activation(out=mbf[s2][:], in_=mT[:, :, tsl], func=AF.Copy), r=[("mT", q)], w=[("mbf", 0)])
                S.dma("sp", h1[s2][:], self.h_dram[tsl, :], r=[("hd", tt)], w=[("h1m", s2)])
                xin, kx = self.ln_xin(lnw, tt)
                for half in range(2):
                    po, ko = self.psum()
                    for kt in range(KT):
                        S.op("pe", lambda kt=kt, po=po: PE.matmul(po[:], lhsT=mbf[s2][:, kt, :], rhs=wo[:, kt, half * 512:(half + 1) * 512],
                                                                  start=(kt == 0), stop=(kt == KT - 1)), r=[("mbf", 0), ("wbr", 1)], w=[ko])
                    S.op("dve", lambda po=po, half=half: V.scalar_tensor_tensor(
                        out=xin[:, half * 512:(half + 1) * 512], in0=h1[s2][:, half * 512:(half + 1) * 512], scalar=ALPHA, in1=po[:],
                        op0=ALU.mult, op1=ALU.add), r=[ko, ("h1m", s2)], w=[kx, ko])
                self.ln_tile(lnw, tt, gbc, bbc, self.h_dram, router=True, extra=self.dbg_out.get("h1_%d" % l))
            S.barrier()

    def layer(self, l):
        S = self.S
        if "ssd" in self.stages:
            self.ssd(l)
            self.dbg_dump("ya%d" % l, lambda o: S.dma("sp", o, self.yT_dram[0], r=[("yT0", h) for h in range(8)]))
        if "rwkv" in self.stages:
            self.rwkv(l)
            self.dbg_dump("yb%d" % l, lambda o: S.dma("sp", o, self.yT_dram[1], r=[("yT1", h) for h in range(8)]))
        if "hgrn" in self.stages:
            self.hgrn(l)
            self.dbg_dump("yc%d" % l, lambda o: S.dma("sp", o, self.yT_dram[2], r=[("yT2", h) for h in range(8)]))
        if "merge" in self.stages:
            self.merge(l)
        if "moe" in self.stages:
            self.moe(l, last=(l == self.depth - 1))


_NC_CACHE = {}


def _get_nc():
    if "nc" not in _NC_CACHE:
        _NC_CACHE["nc"] = Builder().build()
    return _NC_CACHE["nc"]


def kernel(**inputs):
    nc = _get_nc()
    x = np.ascontiguousarray(inputs["x"], dtype=np.float32)
    base = {k: np.ascontiguousarray(inputs[k], dtype=np.float32) for k in PARAM_SHAPES}
    in_maps = []
    for c in range(8):
        m = dict(base)
        m["x"] = x[c]
        in_maps.append(m)
    res = run_bass_kernel_spmd(nc, in_maps, core_ids=list(range(8)))
    return np.stack([res.results[c]["out"] for c in range(8)], axis=0)
```

```python
import contextlib
import os
import numpy as np
CUT = int(os.environ.get('CUT', '99'))
HC = int(os.environ.get('HC', '99'))
HL = int(os.environ.get('HL', '99'))
import concourse.bass as bass
import concourse.mybir as mybir
from concourse.bass_utils import run_bass_kernel_spmd

F32 = mybir.dt.float32
BF16 = mybir.dt.bfloat16
AF = mybir.ActivationFunctionType
ALU = mybir.AluOpType
AX = mybir.AxisListType

D = 1024
T = 2048
NT = T // 128
KT = D // 128
DEPTH = 2
NE = 16
DEXP = 512
N_IN = 13072
ALPHA = (2 * DEPTH) ** 0.25
LN_EPS = 1e-5
RMS_EPS = 1e-6
GN_EPS = 64e-5
OFF_Z = 0
OFF_XBC = 1024
OFF_DT = 2560
OFF_RWKV = 2576
OFF_HGRN = OFF_RWKV + 3328
OFF_GATES = OFF_HGRN + 4096

PARAM_SHAPES = {
    "ln_in_g": [1024], "ln_in_b": [1024], "w_in": [2, 1024, 13072],
    "ssd_conv_w": [2, 4, 1536], "ssd_conv_b": [2, 1536], "ssd_dt_bias": [2, 16],
    "ssd_a_log": [2, 16], "ssd_d": [2, 16], "ssd_norm_w": [2, 1024],
    "rwkv_mu": [2, 3328], "rwkv_w0": [2, 1024], "rwkv_w2": [2, 64, 1024],
    "rwkv_a0": [2, 1024], "rwkv_a2": [2, 64, 1024], "rwkv_g2": [2, 128, 1024],
    "rwkv_k_k": [2, 1024], "rwkv_k_a": [2, 1024], "rwkv_r_k": [2, 16, 64],
    "rwkv_ln_w": [2, 1024], "rwkv_ln_b": [2, 1024], "hgrn_lb": [2, 1024],
    "hgrn_norm_w": [2, 128], "w_br_ssd": [2, 1024, 1024], "w_br_rwkv": [2, 1024, 1024],
    "w_br_hgrn": [2, 1024, 1024], "w_out": [2, 1024, 1024], "ln1_g": [2, 1024],
    "ln1_b": [2, 1024], "router_w": [1024, 16], "router_bias": [16],
    "exp_w_gate": [2, 16, 1024, 512], "exp_w_up": [2, 16, 1024, 512],
    "exp_w_down": [2, 16, 512, 1024], "ln2_g": [2, 1024], "ln2_b": [2, 1024],
}


class Sched:
    ENG = ["pe", "act", "dve", "pool", "sp"]

    def __init__(self, nc, es, n_dma=32, n_pdma=24):
        self.nc = nc
        self.e = {"pe": nc.tensor, "act": nc.scalar, "dve": nc.vector, "pool": nc.gpsimd, "sp": nc.sync}
        self.sem = {k: es.enter_context(nc.semaphore("sem_" + k)) for k in self.ENG}
        self.cnt = {k: 0 for k in self.ENG}
        self.dsem = [es.enter_context(nc.semaphore("dsem%d" % i)) for i in range(n_dma)]
        self.dtot = [0] * n_dma
        self.drr = 0
        self.psem = [es.enter_context(nc.semaphore("psem%d" % i)) for i in range(n_pdma)]
        self.pused = [False] * n_pdma
        self.pwaiters = [[] for _ in range(n_pdma)]
        self.pclr = [None] * n_pdma
        self.prr = 0
        self.msem = {k: es.enter_context(nc.semaphore("msem_" + k)) for k in self.ENG}
        self.mcnt = {k: 0 for k in self.ENG}
        self.seen = {k: {} for k in self.ENG}
        self.lastw = {}
        self.readers = {}
        self.nwait = 0
        self.qset = set()

    def _semh(self, sk):
        if isinstance(sk, str):
            return self.sem[sk]
        if sk[0] == "m":
            return self.msem[sk[1]]
        return self.dsem[sk[1]] if sk[0] == "d" else self.psem[sk[1]]

    def _wait(self, e, tag):
        sk, val = tag
        if val <= 0 or self.seen[e].get(sk, 0) >= val:
            return
        if not isinstance(sk, str) and sk[0] == "p" and self.pclr[sk[1]] is not None and e != "pool":
            self._wait(e, self.pclr[sk[1]])
        self.e[e].wait_ge(self._semh(sk), val)
        self.seen[e][sk] = val
        self.nwait += 1
        if not isinstance(sk, str) and sk[0] == "p":
            self.pwaiters[sk[1]].append(self._marker(e))

    def _marker(self, e):
        self.e[e].sem_inc(self.msem[e], 1)
        self.mcnt[e] += 1
        return (("m", e), self.mcnt[e])

    def _deps(self, e, r, w):
        for k in r:
            t = self.lastw.get(k)
            if t is not None:
                self._wait(e, t)
        for k in w:
            t = self.lastw.get(k)
            if t is not None and (t[0] != e or e != "pe"):
                self._wait(e, t)
            for sk, val in self.readers.get(k, {}).items():
                if sk != e or e != "pe":
                    self._wait(e, (sk, val))

    def _record(self, tag, r, w):
        for k in r:
            d = self.readers.setdefault(k, {})
            if d.get(tag[0], 0) < tag[1]:
                d[tag[0]] = tag[1]
        for k in w:
            self.lastw[k] = tag
            self.readers[k] = {}

    def _exp(self, keys):
        out = []
        for k in keys:
            if k in self.qset:
                out.extend((k, q) for q in range(4))
            else:
                out.append(k)
        return out

    def op(self, e, fn, r=(), w=()):
        r, w = self._exp(r), self._exp(w)
        self._deps(e, r, w)
        ins = fn()
        self.cnt[e] += 1
        ins.then_inc(self.sem[e], 1)
        if os.environ.get("OPLOG"):
            self.oplog = getattr(self, "oplog", {})
            self.oplog[(e, self.cnt[e])] = fn.__code__.co_firstlineno
        self._record((e, self.cnt[e]), r, w)

    def _dma_sw(self, out, in_, r, w):
        q = "pool"
        self._deps(q, r, w)
        i = self.prr
        self.prr = (self.prr + 1) % len(self.psem)
        sk = ("p", i)
        if self.pused[i]:
            self._wait(q, (sk, 16))
            for e in self.ENG:
                if e != q:
                    self._wait(e, (sk, 16))
            for tg in self.pwaiters[i]:
                if tg[0][1] != q:
                    self._wait(q, tg)
            self.e[q].sem_clear(self.psem[i])
            tclr = self._marker(q)
            self.pclr[i] = tclr
            for k, t in list(self.lastw.items()):
                if t[0] == sk:
                    self.lastw[k] = tclr
            for k, d in self.readers.items():
                if sk in d:
                    d.pop(sk)
                    d[tclr[0]] = tclr[1]
            for e in self.ENG:
                self.seen[e].pop(sk, None)
            self.pwaiters[i] = []
        ins = self.e[q].dma_start(out=out, in_=in_)
        ins.then_inc(self.psem[i], 16)
        self.pused[i] = True
        self._record((sk, 16), r, w)

    def dma(self, q, out, in_, r=(), w=()):
        r, w = self._exp(r), self._exp(w)
        if q == "pool" and os.environ.get("PSEM_CLEAR"):
            return self._dma_sw(out, in_, r, w)
        self._deps(q, r, w)
        i = self.drr
        self.drr = (self.drr + 1) % len(self.dsem)
        self._wait(q, (("d", i), self.dtot[i]))
        with self.nc.allow_non_contiguous_dma(reason="small per-feature parameter columns"):
            ins = self.e[q].dma_start(out=out, in_=in_)
        self.dtot[i] += 16
        ins.then_inc(self.dsem[i], 16)
        self._record((("d", i), self.dtot[i]), r, w)

    def barrier(self):
        for e in self.ENG:
            for o in self.ENG:
                if o != e:
                    self._wait(e, (o, self.cnt[o]))
            for i in range(len(self.dsem)):
                self._wait(e, (("d", i), self.dtot[i]))
            for i in range(len(self.psem)):
                if self.pused[i]:
                    self._wait(e, (("p", i), 16))

    def finish(self):
        for i in range(len(self.dsem)):
            self._wait("sp", (("d", i), self.dtot[i]))
        for i in range(len(self.psem)):
            if self.pused[i]:
                self._wait("sp", (("p", i), 16))
        for o in self.ENG:
            if o != "sp":
                self._wait("sp", (o, self.cnt[o]))


class Builder:
    def __init__(self, debug=None, stages=("pre", "hgrn", "ssd", "rwkv", "merge", "moe"), depth=DEPTH, pre_router=False):
        self.pre_router = pre_router
        self.debug = debug or {}
        self.stages = stages
        self.depth = depth
        self.nc = bass.Bass("TRN2", target_bir_lowering=False)
        nc = self.nc
        self.x = nc.dram_tensor("x", [T, D], F32, kind="ExternalInput").ap()
        self.P = {k: nc.dram_tensor(k, s, F32, kind="ExternalInput").ap() for k, s in PARAM_SHAPES.items()}
        self.out = nc.dram_tensor("out", [T, D], F32, kind="ExternalOutput").ap()
        self.h_dram = nc.dram_tensor("h_scr", [T, D], F32, kind="Internal").ap()
        self.yT_dram = [nc.dram_tensor("yT_scr%d" % i, [D, T], BF16, kind="Internal").ap() for i in range(3)]
        self.dbg_out = {}
        for name, (shape, dt) in self.debug.items():
            self.dbg_out[name] = nc.dram_tensor("dbg_" + name, shape, dt, kind="ExternalOutput").ap()
        self.uid = 0

    def sb(self, es, name, shape, dt):
        self.uid += 1
        return es.enter_context(self.nc.sbuf_tensor("%s_%d" % (name, self.uid), shape, dt))

    def psum(self):
        i = self.ps_rr
        self.ps_rr = (self.ps_rr + 1) % 8
        return self.ps[i], ("ps", i)

    def build(self):
        nc = self.nc
        with contextlib.ExitStack() as es:
            self.S = Sched(nc, es)
            S = self.S
            self.ps = [es.enter_context(nc.psum_tensor("psb%d" % i, [128, 512], F32)) for i in range(8)]
            self.ps_rr = 0
            self.ident32 = self.sb(es, "ident32", [128, 128], F32)
            self.identbf = self.sb(es, "identbf", [128, 128], BF16)
            self.zeros = self.sb(es, "zeros", [128, 128], F32)
            self.ones = self.sb(es, "ones", [128, 128], F32)
            self.onesbf = self.sb(es, "onesbf", [128, 128], BF16)
            self.epsc = self.sb(es, "epsc", [128, 4], F32)
            S.op("pool", lambda: nc.gpsimd.memset(self.zeros[:], 0.0), w=["zeros"])
            S.op("pool", lambda: nc.gpsimd.memset(self.ones[:], 1.0), w=["ones"])
            S.op("pool", lambda: nc.gpsimd.memset(self.onesbf[:], 1.0), w=["onesbf"])
            S.op("pool", lambda: nc.gpsimd.memset(self.epsc[:, 0:1], LN_EPS), w=["epsc"])
            S.op("pool", lambda: nc.gpsimd.memset(self.epsc[:, 1:2], RMS_EPS), w=["epsc"])
            S.op("pool", lambda: nc.gpsimd.memset(self.epsc[:, 2:3], GN_EPS), w=["epsc"])
            S.op("pool", lambda: nc.gpsimd.memset(self.epsc[:, 3:4], 1.0), w=["epsc"])
            S.op("pool", lambda: nc.gpsimd.affine_select(
                out=self.ident32[:], in_=self.zeros[:], pattern=[[1, 128]], compare_op=ALU.not_equal,
                fill=1.0, base=0, channel_multiplier=-1), r=["zeros"], w=["ident32"])
            S.op("pool", lambda: nc.gpsimd.tensor_copy(out=self.identbf[:], in_=self.ident32[:]),
                 r=["ident32"], w=["identbf"])
            self.hT = self.sb(es, "hT", [128, KT, T], BF16)
            self.gates = self.sb(es, "gates", [128, NT, NE], F32)
            self.logits = self.sb(es, "logits", [128, NT, NE], F32)
            self.rw32 = self.sb(es, "rw32", [128, KT, NE], F32)
            self.rbias = self.sb(es, "rbias", [128, NE], F32)
            S.dma("sp", self.rw32[:], self.P["router_w"].rearrange("(kt p) e -> p kt e", p=128), w=["rw32"])
            S.dma("sp", self.rbias[:], self.P["router_bias"].partition_broadcast(128), w=["rbias"])

            with contextlib.ExitStack() as st:
                gbc = self.sb(st, "gbc", [128, D], F32)
                bbc = self.sb(st, "bbc", [128, D], F32)
                S.dma("sp", gbc[:], self.P["ln_in_g"].partition_broadcast(128), w=["gbc"])
                S.dma("sp", bbc[:], self.P["ln_in_b"].partition_broadcast(128), w=["bbc"])
                lnw = self.ln_alloc(st)
                def pre_tile(tt):
                    xin, kx = self.ln_xin(lnw, tt)
                    S.dma("sp", xin[:], self.x[tt * 128:(tt + 1) * 128, :], w=[kx])
                    yield from self.ln_tile(lnw, tt, gbc, bbc, self.h_dram, router=self.pre_router, extra=self.dbg_out.get("h0"))
                self.pipeline([(lambda tt=tt: pre_tile(tt)) for tt in range(NT)], 2)
                S.barrier()
            self.dbg_dump("hT", lambda o: S.dma("sp", o, self.hT[:], r=[("hT", t) for t in range(NT)]))
            self.dbg_dump("logits", lambda o: S.dma("sp", o, self.logits[:], r=["logits"]))

            for l in range(self.depth):
                self.layer(l)
            S.finish()
        return nc

    def dbg_dump(self, name, fn):
        if name in self.dbg_out:
            fn(self.dbg_out[name])

    @staticmethod
    def pipeline(factories, skew):
        gens, nxt, rnd = [], 0, 0
        while nxt < len(factories) or gens:
            if nxt < len(factories) and rnd % skew == 0:
                gens.append(factories[nxt]())
                nxt += 1
            for g_ in list(gens):
                try:
                    next(g_)
                except StopIteration:
                    gens.remove(g_)
            rnd += 1

    def ln_alloc(self, st):
        w = {}
        w["xin"] = [self.sb(st, "xin", [128, D], F32) for _ in range(2)]
        w["hh"] = [self.sb(st, "hh", [128, D], F32) for _ in range(2)]
        w["bst"] = [self.sb(st, "bst", [128, 2, 6], F32) for _ in range(2)]
        w["mv"] = [self.sb(st, "mv", [128, 4], F32) for _ in range(2)]
        w["h32"] = [self.sb(st, "h32", [128, KT, 128], F32) for _ in range(2)]
        w["id"] = self.uid
        return w

    def ln_xin(self, w, tt):
        return w["xin"][tt % 2], ("xin", w["id"], tt % 2)

    def ln_tile(self, w, tt, gbc, bbc, dst_dram, router, extra=None):
        nc, S = self.nc, self.S
        s = tt % 2
        wid = w["id"]
        xin, kx = w["xin"][s], ("xin", wid, s)
        hh, kh = w["hh"][s], ("hh", wid, s)
        bst, kb = w["bst"][s], ("bst", wid, s)
        mv, km = w["mv"][s], ("mv", wid, s)
        h32, k32 = w["h32"][s], ("h32", wid, s)
        for c in range(2):
            S.op("dve", lambda c=c: nc.vector.bn_stats(out=bst[:, c, :], in_=xin[:, c * 512:(c + 1) * 512]),
                 r=[kx], w=[kb])
        S.op("dve", lambda: nc.vector.bn_aggr(out=mv[:, 0:2], in_=bst[:].rearrange("p a b -> p (a b)")),
             r=[kb], w=[km])
        S.op("act", lambda: nc.scalar.activation(out=mv[:, 2:3], in_=mv[:, 1:2], func=AF.Sqrt,
                                                 bias=self.epsc[:, 0:1], scale=1.0), r=[km, "epsc"], w=[km])
        S.op("dve", lambda: nc.vector.reciprocal(out=mv[:, 3:4], in_=mv[:, 2:3]), r=[km], w=[km])
        S.op("dve", lambda: nc.vector.tensor_scalar(out=xin[:], in0=xin[:], scalar1=mv[:, 0:1], scalar2=mv[:, 3:4],
                                                    op0=ALU.subtract, op1=ALU.mult), r=[kx, km], w=[kx])
        yield
        S.op("pool", lambda: nc.gpsimd.tensor_tensor(out=hh[:], in0=xin[:], in1=gbc[:], op=ALU.mult),
             r=[kx, "gbc"], w=[kh])
        S.op("pool", lambda: nc.gpsimd.tensor_tensor(out=hh[:], in0=hh[:], in1=bbc[:], op=ALU.add),
             r=[kh, "bbc"], w=[kh])
        S.dma("sp", dst_dram[tt * 128:(tt + 1) * 128, :], hh[:], r=[kh], w=[("hd", tt)])
        if extra is not None:
            S.dma("sp", extra[tt * 128:(tt + 1) * 128, :], hh[:], r=[kh], w=[("hdx", tt)])
        yield
        for half in range(2):
            pb, kp = self.psum()
            for j in range(4):
                kt = half * 4 + j
                S.op("pe", lambda j=j, kt=kt: nc.tensor.transpose(
                    out=pb[:, j * 128:(j + 1) * 128], in_=hh[:, kt * 128:(kt + 1) * 128], identity=self.ident32[:]),
                    r=[kh, "ident32"], w=[kp])
            if os.environ.get("EVAC", "act") == "act":
                S.op("act", lambda half=half, pb=pb: nc.scalar.activation(
                    out=self.hT[:, half * 4:(half + 1) * 4, tt * 128:(tt + 1) * 128],
                    in_=pb[:].rearrange("p (a b) -> p a b", a=4), func=AF.Copy), r=[kp], w=[("hT", tt), kp])
            else:
                S.op("dve", lambda half=half, pb=pb: nc.vector.tensor_copy(
                    out=self.hT[:, half * 4:(half + 1) * 4, tt * 128:(tt + 1) * 128],
                    in_=pb[:].rearrange("p (a b) -> p a b", a=4)), r=[kp], w=[("hT", tt), kp])
            if router:
                S.op("dve", lambda half=half, pb=pb: nc.vector.tensor_copy(
                    out=h32[:, half * 4:(half + 1) * 4, :], in_=pb[:].rearrange("p (a b) -> p a b", a=4)),
                    r=[kp], w=[k32, kp])
        yield
        if router:
            pb, kp = self.psum()
            for kt in range(KT):
                S.op("pe", lambda kt=kt: nc.tensor.matmul(pb[:, 0:NE], lhsT=h32[:, kt, :], rhs=self.rw32[:, kt, :],
                                                          start=(kt == 0), stop=(kt == KT - 1)),
                     r=[k32, "rw32"], w=[kp])
            S.op("dve", lambda: nc.vector.tensor_copy(out=self.logits[:, tt, :], in_=pb[:, 0:NE]),
                 r=[kp], w=["logits"])

    def router(self, st):
        nc, S = self.nc, self.S
        V = nc.vector
        L = self.logits
        t1 = self.sb(st, "rt1", [128, NT, NE], F32)
        probs = self.sb(st, "probs", [128, NT, NE], F32)
        sel = self.sb(st, "sel", [128, NT, NE], F32)
        p6 = self.sb(st, "p6", [128, NT, 4, 6], F32)
        gs = self.sb(st, "gs", [128, NT, 4], F32)
        gm = self.sb(st, "gm", [128, NT, 4], F32)
        gt = self.sb(st, "gt", [128, NT, 4], F32)
        red = self.sb(st, "red", [128, NT], F32)
        red2 = self.sb(st, "red2", [128, NT], F32)
        msk = self.sb(st, "msk", [128, NT, NE], F32)
        eq = self.sb(st, "eq", [128, NT, NE], F32)
        BIG = 1.0e9

        def bc(a):
            return a[:].unsqueeze(2).to_broadcast([128, NT, NE])

        S.op("dve", lambda: V.tensor_reduce(out=red[:], in_=L[:], axis=AX.X, op=ALU.max), r=["logits"], w=["red"])
        S.op("dve", lambda: V.tensor_tensor(out=t1[:], in0=L[:], in1=bc(red), op=ALU.subtract),
             r=["logits", "red"], w=["rt1"])
        S.op("act", lambda: nc.scalar.activation(out=t1[:], in_=t1[:], func=AF.Exp), r=["rt1"], w=["rt1"])
        S.op("dve", lambda: V.tensor_reduce(out=red[:], in_=t1[:], axis=AX.X, op=ALU.add), r=["rt1"], w=["red"])
        S.op("dve", lambda: V.reciprocal(out=red[:], in_=red[:]), r=["red"], w=["red"])
        S.op("dve", lambda: V.tensor_tensor(out=probs[:], in0=t1[:], in1=bc(red), op=ALU.mult),
             r=["rt1", "red"], w=["probs"])
        S.op("dve", lambda: V.tensor_tensor(out=sel[:], in0=probs[:],
                                            in1=self.rbias[:].unsqueeze(1).to_broadcast([128, NT, NE]), op=ALU.add),
             r=["probs", "rbias"], w=["sel"])
        s4 = sel[:].rearrange("p t (g e) -> p t g e", g=4)
        S.op("dve", lambda: V.tensor_tensor(out=p6[:, :, :, 0:3], in0=s4[:, :, :, 0:3], in1=s4[:, :, :, 1:4],
                                            op=ALU.add), r=["sel"], w=["p6"])
        S.op("dve", lambda: V.tensor_tensor(out=p6[:, :, :, 3:5], in0=s4[:, :, :, 0:2], in1=s4[:, :, :, 2:4],
                                            op=ALU.add), r=["sel"], w=["p6"])
        S.op("dve", lambda: V.tensor_tensor(out=p6[:, :, :, 5:6], in0=s4[:, :, :, 0:1], in1=s4[:, :, :, 3:4],
                                            op=ALU.add), r=["sel"], w=["p6"])
        S.op("dve", lambda: V.tensor_reduce(out=gs[:], in_=p6[:], axis=AX.X, op=ALU.max), r=["p6"], w=["gs"])
        S.op("dve", lambda: V.tensor_reduce(out=red[:], in_=gs[:], axis=AX.X, op=ALU.max), r=["gs"], w=["red"])
        S.op("dve", lambda: V.tensor_tensor(out=gm[:], in0=gs[:], in1=red[:].unsqueeze(2).to_broadcast([128, NT, 4]),
                                            op=ALU.is_ge), r=["gs", "red"], w=["gm"])
        S.op("dve", lambda: V.tensor_scalar(out=gt[:], in0=gm[:], scalar1=BIG, scalar2=-BIG, op0=ALU.mult,
                                            op1=ALU.add), r=["gm"], w=["gt"])
        m4 = msk[:].rearrange("p t (g e) -> p t g e", g=4)
        S.op("dve", lambda: V.tensor_tensor(out=m4, in0=s4, in1=gm[:].unsqueeze(3).to_broadcast([128, NT, 4, 4]),
                                            op=ALU.mult), r=["sel", "gm"], w=["msk"])
        S.op("dve", lambda: V.tensor_tensor(out=m4, in0=m4, in1=gt[:].unsqueeze(3).to_broadcast([128, NT, 4, 4]),
                                            op=ALU.add), r=["msk", "gt"], w=["msk"])
        S.op("dve", lambda: V.tensor_reduce(out=red[:], in_=msk[:], axis=AX.X, op=ALU.max), r=["msk"], w=["red"])
        S.op("dve", lambda: V.tensor_tensor(out=eq[:], in0=msk[:], in1=bc(red), op=ALU.is_equal),
             r=["msk", "red"], w=["eq"])
        S.op("dve", lambda: V.scalar_tensor_tensor(out=eq[:], in0=eq[:], scalar=-BIG, in1=msk[:], op0=ALU.mult,
                                                   op1=ALU.add), r=["eq", "msk"], w=["eq"])
        S.op("dve", lambda: V.tensor_reduce(out=red2[:], in_=eq[:], axis=AX.X, op=ALU.max), r=["eq"], w=["red2"])
        S.op("dve", lambda: V.tensor_tensor(out=eq[:], in0=msk[:], in1=bc(red2), op=ALU.is_ge),
             r=["msk", "red2"], w=["eq"])
        S.op("dve", lambda: V.tensor_tensor(out=eq[:], in0=eq[:], in1=probs[:], op=ALU.mult),
             r=["eq", "probs"], w=["eq"])
        S.op("dve", lambda: V.tensor_reduce(out=red[:], in_=eq[:], axis=AX.X, op=ALU.add), r=["eq"], w=["red"])
        S.op("dve", lambda: V.reciprocal(out=red[:], in_=red[:]), r=["red"], w=["red"])
        S.op("dve", lambda: V.tensor_tensor(out=self.gates[:], in0=eq[:], in1=bc(red), op=ALU.mult),
             r=["eq", "red"], w=["gates"])

    def moe(self, l, last):
        nc, S = self.nc, self.S
        wg_d, wu_d, wd_d = self.P["exp_w_gate"], self.P["exp_w_up"], self.P["exp_w_down"]
        with contextlib.ExitStack() as st:
            self.router(st)
            self.dbg_dump("gates%d" % l, lambda o: S.dma("sp", o, self.gates[:], r=["gates"]))
            acc = self.sb(st, "acc", [128, NT, D], F32)
            wg = [self.sb(st, "wg", [128, KT, DEXP], BF16) for _ in range(2)]
            wu = [self.sb(st, "wu", [128, KT, DEXP], BF16) for _ in range(2)]
            wd = [self.sb(st, "wd", [128, 4, D], BF16) for _ in range(2)]
            hg = [self.sb(st, "hg", [128, 4, 512], BF16) for _ in range(2)]
            sg = [self.sb(st, "sg", [128, 512], BF16) for _ in range(2)]
            gbc = self.sb(st, "gbc2", [128, D], F32)
            bbc = self.sb(st, "bbc2", [128, D], F32)
            S.dma("sp", gbc[:], self.P["ln2_g"][l].partition_broadcast(128), w=["gbc"])
            S.dma("sp", bbc[:], self.P["ln2_b"][l].partition_broadcast(128), w=["bbc"])

            def load_w(e):
                b = e % 2
                S.dma("pool", wg[b][:], wg_d[l, e].rearrange("(kt p) n -> p kt n", p=128), w=[("wg", b)])
                S.dma("pool", wu[b][:], wu_d[l, e].rearrange("(kt p) n -> p kt n", p=128), w=[("wu", b)])
                S.dma("pool", wd[b][:], wd_d[l, e].rearrange("(kt p) n -> p kt n", p=128), w=[("wd", b)])

            items = [(e, q) for e in range(int(os.environ.get('ME', NE))) for q in range(4)]
            sgi = [0]

            def G(i):
                e, q = items[i]
                b = e % 2
                hb = i % 2
                for dt_ in range(4):
                    pa, ka = self.psum()
                    pu, ku = self.psum()
                    for kt in range(KT):
                        S.op("pe", lambda kt=kt, pa=pa: nc.tensor.matmul(
                            pa[:], lhsT=wg[b][:, kt, dt_ * 128:(dt_ + 1) * 128], rhs=self.hT[:, kt, q * 512:(q + 1) * 512],
                            start=(kt == 0), stop=(kt == KT - 1)),
                            r=[("wg", b)] + [("hT", q * 4 + j) for j in range(4)], w=[ka])
                    for kt in range(KT):
                        S.op("pe", lambda kt=kt, pu=pu: nc.tensor.matmul(
                            pu[:], lhsT=wu[b][:, kt, dt_ * 128:(dt_ + 1) * 128], rhs=self.hT[:, kt, q * 512:(q + 1) * 512],
                            start=(kt == 0), stop=(kt == KT - 1)),
                            r=[("wu", b)] + [("hT", q * 4 + j) for j in range(4)], w=[ku])
                    si = sgi[0] % 2
                    sgi[0] += 1
                    S.op("act", lambda pa=pa, si=si: nc.scalar.activation(out=sg[si][:], in_=pa[:], func=AF.Silu),
                         r=[ka], w=[("sg", si)])
                    S.op("dve", lambda pu=pu, si=si: nc.vector.tensor_tensor(
                        out=hg[hb][:, dt_, :], in0=pu[:], in1=sg[si][:], op=ALU.mult),
                        r=[ku, ("sg", si)], w=[("hg", hb)])

            def Dn(i):
                e, q = items[i]
                b = e % 2
                hb = i % 2
                for j in range(4):
                    tt = q * 4 + j
                    for half in range(2):
                        pc, kc = self.psum()
                        for dt_ in range(4):
                            S.op("pe", lambda dt_=dt_, pc=pc: nc.tensor.matmul(
                                pc[:], lhsT=hg[hb][:, dt_, j * 128:(j + 1) * 128],
                                rhs=wd[b][:, dt_, half * 512:(half + 1) * 512], start=(dt_ == 0), stop=(dt_ == 3)),
                                r=[("hg", hb), ("wd", b)], w=[kc])
                        dst = acc[:, tt, half * 512:(half + 1) * 512]
                        if e == 0:
                            S.op("dve", lambda pc=pc, dst=dst: nc.vector.tensor_scalar(
                                out=dst, in0=pc[:], scalar1=self.gates[:, tt, e:e + 1], scalar2=None, op0=ALU.mult),
                                r=[kc, "gates"], w=[("acc", tt)])
                        else:
                            S.op("dve", lambda pc=pc, dst=dst: nc.vector.scalar_tensor_tensor(
                                out=dst, in0=pc[:], scalar=self.gates[:, tt, e:e + 1], in1=dst, op0=ALU.mult,
                                op1=ALU.add), r=[kc, "gates", ("acc", tt)], w=[("acc", tt)])

            load_w(0)
            for i in range(len(items)):
                e, q = items[i]
                G(i)
                if i >= 1:
                    Dn(i - 1)
                if q == 0 and e + 1 < int(os.environ.get('ME', NE)):
                    load_w(e + 1)
            Dn(len(items) - 1)
            self.dbg_dump("moe%d" % l, lambda o: S.dma("sp", o.rearrange("(t p) d -> p t d", p=128), acc[:],
                                                       r=[("acc", t) for t in range(NT)]))
            lnw = self.ln_alloc(st)
            h1 = [self.sb(st, "h1t", [128, D], F32) for _ in range(2)]
            dst = self.out if last else self.h_dram
            def ln2_tile(tt):
                s = tt % 2
                S.dma("sp", h1[s][:], self.h_dram[tt * 128:(tt + 1) * 128, :], r=[("hd", tt)], w=[("h1t", s)])
                xin, kx = self.ln_xin(lnw, tt)
                S.op("dve", lambda s=s, xin=xin: nc.vector.scalar_tensor_tensor(
                    out=xin[:], in0=h1[s][:], scalar=ALPHA, in1=acc[:, tt, :], op0=ALU.mult, op1=ALU.add),
                    r=[("h1t", s), ("acc", tt)], w=[kx])
                yield from self.ln_tile(lnw, tt, gbc, bbc, dst, router=False)
            self.pipeline([(lambda tt=tt: ln2_tile(tt)) for tt in range(NT)], 2)
            S.barrier()


    def hgrn(self, l):
        nc, S = self.nc, self.S
        V, A, G, PE = nc.vector, nc.scalar, nc.gpsimd, nc.tensor
        Wl = self.P["w_in"][l]
        ydst = self.yT_dram[2]
        with contextlib.ExitStack() as st:
            mask2 = self.sb(st, "mask2", [128, 128], F32)
            rm = self.sb(st, "rm", [128, T], F32)
            nw = self.sb(st, "nw", [128, 1], F32)
            lbt = self.sb(st, "lbt", [128, 8, 2], F32)
            lbv = self.sb(st, "lbv", [128, 8], F32)
            oml = self.sb(st, "oml", [128, 8], F32)
            S.op("pool", lambda: G.affine_select(out=mask2[:], in_=self.ones[:], pattern=[[1, 128]],
                                                 compare_op=ALU.is_ge, fill=0.0, base=0, channel_multiplier=-1),
                 r=["ones"], w=["mask2"])
            S.op("pool", lambda: G.memset(mask2[0:64, 64:128], 0.0), w=["mask2"])
            S.op("pool", lambda: G.memset(rm[:], 1.0), w=["rm"])
            S.op("pool", lambda: G.memset(rm[:].rearrange("p (c j) -> p c j", j=64)[:, :, 0:1], 0.0), w=["rm"])
            S.dma("sp", nw[:], self.P["hgrn_norm_w"][l].rearrange("(p o) -> p o", o=1), w=["nw"])
            if l == 0:
                S.op("pool", lambda: G.memset(lbv[:], 0.0), w=["lbv"])
                S.op("pool", lambda: G.memset(oml[:], 1.0), w=["oml"])
            else:
                for j in range(2):
                    S.dma("sp", lbt[:, :, j:j + 1],
                          self.P["hgrn_lb"][j].rearrange("(h p o) -> p h o", p=128, o=1), w=["lbt"])
                S.op("dve", lambda: V.tensor_tensor(out=lbv[:], in0=lbt[:, :, 1], in1=lbt[:, :, 0], op=ALU.subtract),
                     r=["lbt"], w=["lbv"])
                S.op("act", lambda: A.activation(out=lbv[:], in_=lbv[:], func=AF.Sigmoid), r=["lbv"], w=["lbv"])
                S.op("dve", lambda: V.tensor_scalar(out=oml[:], in0=lbv[:], scalar1=-1.0, scalar2=1.0, op0=ALU.mult,
                                                    op1=ALU.add), r=["lbv"], w=["oml"])
            w4 = [self.sb(st, "w4", [128, 4, KT, 128], BF16) for _ in range(2)]
            qs = self.sb(st, "qs", [128, T], F32)
            fs = self.sb(st, "fs", [128, T], F32)
            lf = self.sb(st, "lf", [128, T], F32)
            bc = self.sb(st, "bc", [128, T], F32)
            enb = self.sb(st, "enb", [128, T], F32)
            def two(name, shape, dt):
                return [self.sb(st, name, shape, dt) for _ in range(2)]
            qbL, kbL, gsL, ytL = two("qb", [128, T], BF16), two("kb", [128, T], BF16), two("gs", [128, T], BF16), two("yt", [128, T], BF16)
            vL, kbtL, kbtBL = two("v", [128, NT, 128], BF16), two("kbt", [128, NT, 128], BF16), two("kbtB", [128, NT, 128], BF16)
            ebL = two("ebh", [128, T], F32)
            S32L = two("S32", [128, 128], F32)
            SbfL = [[self.sb(st, "Sbf", [128, 128], BF16) for _ in range(4)] for _ in range(2)]
            attmL = [two("attm", [128, 128], BF16) for _ in range(2)]
            osbL = [two("osb", [128, 128], F32) for _ in range(2)]
            osqL = [two("osq", [128, 128], BF16) for _ in range(2)]
            sdL = [two("sd", [128, 128], F32) for _ in range(2)]
            mAB = self.sb(st, "mAB", [128, 2], F32)
            S.op("pool", lambda: G.memset(mAB[0:64, 0:1], 1.0), w=["mAB"])
            S.op("pool", lambda: G.memset(mAB[64:128, 0:1], 0.0), w=["mAB"])
            S.op("pool", lambda: G.memset(mAB[0:64, 1:2], 0.0), w=["mAB"])
            S.op("pool", lambda: G.memset(mAB[64:128, 1:2], 1.0), w=["mAB"])
            hTk = [("hT", t) for t in range(NT)]

            def load_w(h):
                b = h % 2
                for j in range(4):
                    c0 = OFF_HGRN + j * 1024 + h * 128
                    S.dma("pool", w4[b][:, j], Wl[:, c0:c0 + 128].rearrange("(kt p) n -> p kt n", p=128),
                          w=[("w4", b)])

            def prep(h):
                hb = b = h % 2
                qb, kb, gs, v, kbt, kbtB, eb = qbL[hb], kbL[hb], gsL[hb], vL[hb], kbtL[hb], kbtBL[hb], ebL[hb]
                K = lambda n: (n, hb)
                if h + 1 < 8:
                    load_w(h + 1)
                for (j, func, dst, kd) in ((0, AF.Silu, qs, "qs"), (1, AF.Sigmoid, fs, "fs"), (3, AF.Sigmoid, gs, K("gs"))):
                    for tq in range(4):
                        pb, kp = self.psum()
                        for kt in range(KT):
                            S.op("pe", lambda kt=kt, pb=pb, j=j, tq=tq: PE.matmul(
                                pb[:], lhsT=w4[b][:, j, kt, :], rhs=self.hT[:, kt, tq * 512:(tq + 1) * 512],
                                start=(kt == 0), stop=(kt == KT - 1)), r=[("w4", b)] + hTk[tq * 4:tq * 4 + 4], w=[kp])
                        S.op("act", lambda pb=pb, dst=dst, func=func, tq=tq: A.activation(
                            out=dst[:, tq * 512:(tq + 1) * 512], in_=pb[:], func=func), r=[kp],
                            w=[((kd, tq) if kd in S.qset else kd), kp])
                for t4 in range(4):
                    pb, kp = self.psum()
                    for j4 in range(4):
                        tt = t4 * 4 + j4
                        for kt in range(KT):
                            S.op("pe", lambda kt=kt, pb=pb, j4=j4, tt=tt: PE.matmul(
                                pb[:, j4 * 128:(j4 + 1) * 128], lhsT=self.hT[:, kt, tt * 128:(tt + 1) * 128],
                                rhs=w4[b][:, 2, kt, :], start=(kt == 0), stop=(kt == KT - 1)),
                                r=[("w4", b), ("hT", tt)], w=[kp])
                    S.op("dve", lambda pb=pb, t4=t4: V.tensor_copy(
                        out=v[:, t4 * 4:(t4 + 1) * 4, :], in_=pb[:].rearrange("p (a b) -> p a b", a=4)),
                        r=[kp], w=[K("v"), kp])
                def Q(eng, fn, r=(), w=()):
                    for q in range(4):
                        qs_ = slice(q * 512, (q + 1) * 512)
                        rr = [(k, q) if k in S.qset else k for k in r]
                        ww = [(k, q) if k in S.qset else k for k in w]
                        S.op(eng, lambda: fn(qs_), r=rr, w=ww)
                Q("dve", lambda qs_: V.tensor_scalar(out=fs[:, qs_], in0=fs[:, qs_], scalar1=oml[:, h:h + 1], scalar2=lbv[:, h:h + 1],
                                                     op0=ALU.mult, op1=ALU.add), r=["fs", "oml", "lbv"], w=["fs"])
                Q("act", lambda qs_: A.activation(out=lf[:, qs_], in_=fs[:, qs_], func=AF.Ln), r=["fs"], w=["lf"])
                Q("dve", lambda qs_: V.tensor_tensor_scan(out=bc[:, qs_], data0=rm[:, qs_], data1=lf[:, qs_], initial=0.0,
                                                          op0=ALU.mult, op1=ALU.add), r=["rm", "lf"], w=["bc"])
                Q("act", lambda qs_: A.activation(out=eb[:, qs_], in_=bc[:, qs_], func=AF.Exp), r=["bc"], w=[K("eb")])
                Q("act", lambda qs_: A.activation(out=enb[:, qs_], in_=bc[:, qs_], func=AF.Exp, scale=-1.0), r=["bc"], w=["enb"])
                Q("dve", lambda qs_: V.tensor_scalar(out=fs[:, qs_], in0=fs[:, qs_], scalar1=-1.0, scalar2=1.0, op0=ALU.mult,
                                                     op1=ALU.add), r=["fs", "lf"], w=["fs"])
                Q("pool", lambda qs_: G.tensor_tensor(out=qb[:, qs_], in0=qs[:, qs_], in1=eb[:, qs_], op=ALU.mult),
                  r=["qs", K("eb")], w=[K("qb")])
                Q("dve", lambda qs_: V.tensor_tensor(out=kb[:, qs_], in0=fs[:, qs_], in1=enb[:, qs_], op=ALU.mult),
                  r=["fs", "enb"], w=[K("kb")])
                for t4 in range(4):
                    pb, kp = self.psum()
                    pbv = pb[:].bitcast(BF16)
                    for j4 in range(4):
                        tt = t4 * 4 + j4
                        S.op("pe", lambda pbv=pbv, j4=j4, tt=tt: PE.transpose(
                            out=pbv[:, j4 * 128:(j4 + 1) * 128], in_=kb[:, tt * 128:(tt + 1) * 128],
                            identity=self.identbf[:]), r=[(K("kb"), t4), "identbf"], w=[kp])
                    S.op("dve", lambda pbv=pbv, t4=t4: V.tensor_scalar(
                        out=kbt[:, t4 * 4:(t4 + 1) * 4, :], in0=pbv[:, 0:512].rearrange("p (a b) -> p a b", a=4),
                        scalar1=mAB[:, 0:1], scalar2=None, op0=ALU.mult), r=[kp, "mAB"], w=[K("kbt"), kp])
                    S.op("dve", lambda pbv=pbv, t4=t4: V.tensor_scalar(
                        out=kbtB[:, t4 * 4:(t4 + 1) * 4, :], in0=pbv[:, 0:512].rearrange("p (a b) -> p a b", a=4),
                        scalar1=mAB[:, 1:2], scalar2=None, op0=ALU.mult), r=[kp, "mAB"], w=[K("kbtB"), kp])
                S.op("pool", lambda: G.memset(S32L[hb][:], 0.0), w=[K("S32")])
                S.op("pool", lambda: G.memset(SbfL[hb][0][:], 0.0), w=[("Sbf", hb, 0)])

            def tile(h, tt):
                hb = h % 2
                qb, kb, gs, yt, v, kbt, kbtB, eb = qbL[hb], kbL[hb], gsL[hb], ytL[hb], vL[hb], kbtL[hb], kbtBL[hb], ebL[hb]
                S32, Sbf = S32L[hb], SbfL[hb]
                K = lambda n: (n, hb)
                cA, cB = 2 * tt, 2 * tt + 1
                tsl = slice(tt * 128, (tt + 1) * 128)
                i2 = tt % 2
                attm, osb, osq, sd = attmL[hb][i2], osbL[hb][i2], osqL[hb][i2], sdL[hb][i2]
                ka, ko, kq, ks = ("attm", hb, i2), ("osb", hb, i2), ("osq", hb, i2), ("sd", hb, i2)
                pa, kpa = self.psum()
                S.op("pe", lambda: PE.matmul(pa[:, 0:128], lhsT=kb[:, tsl], rhs=qb[:, tsl], start=True, stop=True),
                     r=[K("kb"), K("qb")], w=[kpa])
                S.op("dve", lambda: V.tensor_tensor(out=attm[:], in0=pa[:, 0:128], in1=mask2[:], op=ALU.mult),
                     r=[kpa, "mask2"], w=[ka, kpa])
                yield
                pr, kpr = self.psum()
                S.op("pe", lambda: PE.matmul(pr[:, 0:128], lhsT=kbt[:, tt, :], rhs=v[:, tt, :], start=True, stop=True),
                     r=[K("kbt"), K("v")], w=[kpr])
                S.op("pe", lambda: PE.matmul(pr[:, 128:256], lhsT=kbtB[:, tt, :], rhs=v[:, tt, :], start=True, stop=True),
                     r=[K("kbtB"), K("v")], w=[kpr])
                for (ci, off) in ((cA, 0), (cB, 128)):
                    S.op("dve", lambda off=off: V.tensor_tensor(
                        out=S32[:], in0=pr[:, off:off + 128], in1=S32[:], op=ALU.add), r=[kpr, K("S32")], w=[K("S32"), kpr])
                    S.op("dve", lambda ci=ci: V.tensor_scalar(
                        out=S32[:], in0=S32[:], scalar1=eb[:, ci * 64 + 63:ci * 64 + 64], scalar2=None, op0=ALU.mult),
                        r=[K("S32"), K("eb")], w=[K("S32")])
                    S.op("pool", lambda ci=ci: G.tensor_copy(out=Sbf[(ci + 1) % 4][:], in_=S32[:]),
                         r=[K("S32")], w=[("Sbf", hb, (ci + 1) % 4)])
                    yield
                po, kpo = self.psum()
                S.op("pe", lambda: PE.matmul(po[:, 0:128], lhsT=v[:, tt, :], rhs=attm[:], start=True, stop=False),
                     r=[K("v"), ka], w=[kpo])
                S.op("pe", lambda: PE.matmul(po[:, 0:64], lhsT=Sbf[cA % 4][:], rhs=qb[:, tt * 128:tt * 128 + 64],
                                             start=False, stop=False), r=[("Sbf", hb, cA % 4), K("qb")], w=[kpo])
                S.op("pe", lambda: PE.matmul(po[:, 64:128], lhsT=Sbf[cB % 4][:], rhs=qb[:, tt * 128 + 64:(tt + 1) * 128],
                                             start=False, stop=True), r=[("Sbf", hb, cB % 4), K("qb")], w=[kpo])
                S.op("act", lambda: A.activation(out=osb[:], in_=po[:, 0:128], func=AF.Copy), r=[kpo], w=[ko, kpo])
                S.op("act", lambda: A.activation(out=osq[:], in_=po[:, 0:128], func=AF.Square), r=[kpo], w=[kq, kpo])
                yield
                pss, kps = self.psum()
                S.op("pe", lambda: PE.matmul(pss[:, 0:128], lhsT=self.onesbf[:], rhs=osq[:], start=True, stop=True),
                     r=["onesbf", kq], w=[kps])
                S.op("act", lambda: A.activation(out=sd[:], in_=pss[:, 0:128], func=AF.Sqrt, bias=self.epsc[:, 1:2],
                                                 scale=1.0 / 128.0), r=[kps, "epsc"], w=[ks, kps])
                S.op("dve", lambda: V.reciprocal(out=sd[:], in_=sd[:]), r=[ks], w=[ks])
                S.op("dve", lambda: V.scalar_tensor_tensor(out=osb[:], in0=osb[:], scalar=nw[:, 0:1], in1=sd[:],
                                                           op0=ALU.mult, op1=ALU.mult), r=[ko, ks, "nw"], w=[ko])
                S.op("dve", lambda: V.tensor_tensor(out=yt[:, tsl], in0=osb[:], in1=gs[:, tsl], op=ALU.mult),
                     r=[ko, K("gs")], w=[K("yt")])

            S.qset = {"qs", "fs", "lf", "bc", "enb", ("eb", 0), ("eb", 1), ("qb", 0), ("qb", 1), ("kb", 0), ("kb", 1)}
            load_w(0)
            for hp in range(4):
                prep(2 * hp)
                prep(2 * hp + 1)
                gens, nxt, rnd = [], 0, 0
                while nxt < NT or gens:
                    if nxt < NT and rnd % 2 == 0:
                        gens.append(tile(2 * hp, nxt))
                        gens.append(tile(2 * hp + 1, nxt))
                        nxt += 1
                    for g_ in list(gens):
                        try:
                            next(g_)
                        except StopIteration:
                            gens.remove(g_)
                    rnd += 1
                for hb in range(2):
                    h = 2 * hp + hb
                    S.dma("sp", ydst[h * 128:(h + 1) * 128, :], ytL[hb][:], r=[("yt", hb)], w=[("yT2", h)])
            S.barrier()
            S.qset = set()

    def ssd(self, l):
        nc, S = self.nc, self.S
        V, A, G, PE = nc.vector, nc.scalar, nc.gpsimd, nc.tensor
        Wl = self.P["w_in"][l]
        ydst = self.yT_dram[0]
        NEG = -30000.0
        hTk = [("hT", t) for t in range(NT)]
        with contextlib.ExitStack() as st:
            tri2 = self.sb(st, "tri2", [128, 128], F32)
            same2 = self.sb(st, "same2", [128, 128], F32)
            indA = self.sb(st, "indA", [128, 128], F32)
            indB = self.sb(st, "indB", [128, 128], F32)
            mAB = self.sb(st, "mABs", [128, 2], F32)
            negmask = self.sb(st, "negmask", [128, 8, 128], BF16)
            bd1 = self.sb(st, "bd", [16, 8, 128], F32)
            bd = [bd1, bd1]
            S.op("pool", lambda: G.affine_select(out=tri2[:], in_=self.ones[:], pattern=[[1, 128]], compare_op=ALU.is_ge,
                                                 fill=0.0, base=0, channel_multiplier=-1), r=["ones"], w=["tri2"])
            S.op("pool", lambda: G.memset(tri2[0:64, 64:128], 0.0), w=["tri2"])
            S.op("pool", lambda: G.memset(same2[:], 0.0), w=["same2"])
            S.op("pool", lambda: G.memset(same2[0:64, 0:64], 1.0), w=["same2"])
            S.op("pool", lambda: G.memset(same2[64:128, 64:128], 1.0), w=["same2"])
            S.op("pool", lambda: G.memset(indA[0:64, :], 1.0), w=["indA"])
            S.op("pool", lambda: G.memset(indA[64:128, :], 0.0), w=["indA"])
            S.op("pool", lambda: G.memset(indB[0:64, :], 0.0), w=["indB"])
            S.op("pool", lambda: G.memset(indB[64:128, :], 1.0), w=["indB"])
            S.op("pool", lambda: G.memset(mAB[0:64, 0:1], 1.0), w=["mAB"])
            S.op("pool", lambda: G.memset(mAB[64:128, 0:1], 0.0), w=["mAB"])
            S.op("pool", lambda: G.memset(mAB[0:64, 1:2], 0.0), w=["mAB"])
            S.op("pool", lambda: G.memset(mAB[64:128, 1:2], 1.0), w=["mAB"])
            S.op("pool", lambda: G.memset(negmask[:], 0.0), w=["negmask"])
            S.op("pool", lambda: G.affine_select(out=negmask[:], in_=negmask[:], pattern=[[0, 8], [1, 128]],
                                                 compare_op=ALU.is_ge, fill=NEG, base=0, channel_multiplier=-1),
                 r=["negmask"], w=["negmask"])
            S.op("pool", lambda: G.memset(negmask[0:64, :, 64:128], NEG), w=["negmask"])
            dtb = self.sb(st, "dtb", [128, 16], F32)
            alog = self.sb(st, "alog", [128, 16], F32)
            dsk = self.sb(st, "dsk", [128, 16], F32)
            nwbc = self.sb(st, "nwbc", [128, D], F32)
            S.dma("sp", dtb[:], self.P["ssd_dt_bias"][l].partition_broadcast(128), w=["dtb"])
            S.dma("sp", alog[:], self.P["ssd_a_log"][l].partition_broadcast(128), w=["alog"])
            S.dma("sp", dsk[:], self.P["ssd_d"][l].partition_broadcast(128), w=["dsk"])
            S.dma("sp", nwbc[:], self.P["ssd_norm_w"][l].partition_broadcast(128), w=["nwbc"])
            S.op("act", lambda: A.activation(out=alog[:], in_=alog[:], func=AF.Exp), r=["alog"], w=["alog"])
            S.op("dve", lambda: V.tensor_scalar(out=alog[:], in0=alog[:], scalar1=-1.0, scalar2=None, op0=ALU.mult),
                 r=["alog"], w=["alog"])
            wdt = self.sb(st, "wdt", [128, KT, 16], BF16)
            S.dma("pool", wdt[:], Wl[:, OFF_DT:OFF_DT + 16].rearrange("(kt p) n -> p kt n", p=128), w=["wdt"])
            dt = self.sb(st, "dt", [128, NT, 16], F32)
            da = self.sb(st, "da", [128, NT, 16], F32)
            cum4 = self.sb(st, "cum4", [128, NT, 4, 16], F32)
            eacs = self.sb(st, "eacs", [128, NT, 16], F32)
            eend = self.sb(st, "eend", [128, NT, 16], F32)
            edec = self.sb(st, "edec", [128, NT, 2, 16], F32)
            acsTt = [self.sb(st, "acsTt", [16, 128], F32) for _ in range(2)]
            nacsTt = [self.sb(st, "nacsTt", [16, 128], F32) for _ in range(2)]
            pb, kp = self.psum()
            for tt in range(NT):
                for kt in range(KT):
                    S.op("pe", lambda kt=kt, tt=tt: PE.matmul(pb[:, tt * 16:(tt + 1) * 16], lhsT=self.hT[:, kt, tt * 128:(tt + 1) * 128],
                                                              rhs=wdt[:, kt, :], start=(kt == 0), stop=(kt == KT - 1)),
                         r=["wdt", ("hT", tt)], w=[kp])
            S.op("dve", lambda: V.tensor_tensor(out=dt[:], in0=pb[:, 0:256].rearrange("p (t h) -> p t h", h=16),
                                                in1=dtb[:].unsqueeze(1).to_broadcast([128, NT, 16]), op=ALU.add),
                 r=[kp, "dtb"], w=["dt", kp])
            S.op("act", lambda: A.activation(out=dt[:], in_=dt[:], func=AF.Exp), r=["dt"], w=["dt"])
            S.op("act", lambda: A.activation(out=dt[:], in_=dt[:], func=AF.Ln, bias=self.epsc[:, 3:4], scale=1.0),
                 r=["dt", "epsc"], w=["dt"])
            S.op("dve", lambda: V.tensor_tensor(out=da[:], in0=dt[:], in1=alog[:].unsqueeze(1).to_broadcast([128, NT, 16]),
                                                op=ALU.mult), r=["dt", "alog"], w=["da"])
            for half in range(2):
                pb, kp = self.psum()
                for j in range(8):
                    tt = half * 8 + j
                    for qi, L in enumerate((tri2, same2, indA, indB)):
                        S.op("pe", lambda j=j, qi=qi, L=L, tt=tt, pb=pb: PE.matmul(
                            pb[:, j * 64 + qi * 16:j * 64 + (qi + 1) * 16], lhsT=L[:], rhs=da[:, tt, :], start=True, stop=True),
                            r=["da", "tri2", "same2", "indA", "indB"], w=[kp])
                S.op("dve", lambda pb=pb, half=half: V.tensor_copy(
                    out=cum4[:, half * 8:(half + 1) * 8].rearrange("p t q h -> p (t q h)"), in_=pb[:]),
                    r=[kp], w=["cum4", kp])
            S.op("act", lambda: A.activation(out=eacs[:], in_=cum4[:, :, 0, :], func=AF.Exp), r=["cum4"], w=["eacs"])
            S.op("dve", lambda: V.tensor_tensor(out=eend[:], in0=cum4[:, :, 1, :], in1=cum4[:, :, 0, :], op=ALU.subtract),
                 r=["cum4"], w=["eend"])
            S.op("act", lambda: A.activation(out=eend[:], in_=eend[:], func=AF.Exp), r=["eend"], w=["eend"])
            S.op("act", lambda: A.activation(out=edec[:], in_=cum4[:, :, 2:4, :], func=AF.Exp), r=["cum4"], w=["edec"])
            wx = self.sb(st, "wx", [128, KT, 768], BF16)
            wz = self.sb(st, "wz", [128, KT, 512], BF16)
            cw = self.sb(st, "cw", [128, 6, 4], F32)
            cbi = self.sb(st, "cbi", [128, 6], F32)
            xp1 = self.sb(st, "xp", [128, T + 3], F32)
            xp = [xp1, xp1]
            fTa = [self.sb(st, "fT", [128, T], BF16) for _ in range(4)]
            fT = [fTa[0], fTa[1], fTa[0], fTa[1], fTa[2], fTa[3]]
            fk = [("fT", 0), ("fT", 1), ("fT", 0), ("fT", 1), ("fT", 2), ("fT", 3)]
            cmTA = self.sb(st, "cmTA", [128, T], BF16)
            cmTB = self.sb(st, "cmTB", [128, T], BF16)
            xs = self.sb(st, "xs", [128, NT, 512], BF16)
            xdtt = [self.sb(st, "xdtt", [128, 512], BF16) for _ in range(2)]
            xendt = [self.sb(st, "xendt", [128, 512], BF16) for _ in range(2)]
            bmA = self.sb(st, "bmA", [128, NT, 128], BF16)
            bmB = self.sb(st, "bmB", [128, NT, 128], BF16)
            yTg = self.sb(st, "yTg", [128, 4, T], BF16)
            S32 = self.sb(st, "S32s", [128, 512], F32)
            Sbf = [self.sb(st, "Sbfs", [128, 512], BF16) for _ in range(4)]
            cbs = [self.sb(st, "cbs", [128, 128], BF16) for _ in range(2)]
            Dx1 = self.sb(st, "Dx", [16, 8, 128], F32)
            Dx = [Dx1, Dx1]
            Dxh = self.sb(st, "Dxh", [16, 8, 128], BF16)
            Dxl = self.sb(st, "Dxl", [16, 8, 128], BF16)
            bdbf = self.sb(st, "bdbf", [16, 8, 128], BF16)
            nah = [self.sb(st, "nah", [16, 128], BF16) for _ in range(2)]
            nal = [self.sb(st, "nal", [16, 128], BF16) for _ in range(2)]
            Es = [self.sb(st, "Es", [128, 8, 128], BF16) for _ in range(2)]
            wT = [self.sb(st, "wT", [128, 8, 128], BF16) for _ in range(2)]
            t1 = [self.sb(st, "t1", [128, 512], F32) for _ in range(2)]
            t2 = [self.sb(st, "t2", [128, 512], F32) for _ in range(2)]
            zs = [self.sb(st, "zs", [128, 512], BF16) for _ in range(2)]
            ytm = [self.sb(st, "ytm", [128, 512], BF16) for _ in range(2)]
            ss = [self.sb(st, "ss", [128, 2], F32) for _ in range(2)]
            S.op("pool", lambda: G.memset(xp[0][:, 0:3], 0.0), w=[("xp", 0)])
            for g in range(2):
                S.op("pool", lambda g=g: G.memset(bd[g][:], 1.0), r=[("bd", 0), ("bd", 1)], w=[("bd", 0), ("bd", 1)])
                S.op("pool", lambda g=g: G.affine_select(out=bd[g][:], in_=bd[g][:], pattern=[[1, 8], [0, 128]],
                                                         compare_op=ALU.is_equal, fill=0.0, base=8 * g,
                                                         channel_multiplier=-1), r=[("bd", 0), ("bd", 1)], w=[("bd", 0), ("bd", 1)])
                S.op("pool", lambda g=g: G.tensor_copy(out=bdbf[:], in_=bd[g][:]), r=[("bd", 0), ("bd", 1)], w=["bdbf"])
                choff = [g * 512 + i * 128 for i in range(4)] + [1024 + g * 128, 1280 + g * 128]
                S.dma("pool", wx[:, :, 0:512], Wl[:, OFF_XBC + g * 512:OFF_XBC + (g + 1) * 512].rearrange("(kt p) n -> p kt n", p=128), w=["wx"])
                S.dma("pool", wx[:, :, 512:640], Wl[:, OFF_XBC + 1024 + g * 128:OFF_XBC + 1024 + (g + 1) * 128].rearrange("(kt p) n -> p kt n", p=128), w=["wx"])
                S.dma("pool", wx[:, :, 640:768], Wl[:, OFF_XBC + 1280 + g * 128:OFF_XBC + 1280 + (g + 1) * 128].rearrange("(kt p) n -> p kt n", p=128), w=["wx"])
                S.dma("pool", wz[:], Wl[:, OFF_Z + g * 512:OFF_Z + (g + 1) * 512].rearrange("(kt p) n -> p kt n", p=128), w=["wz"])
                for ci in range(6):
                    for j in range(4):
                        S.dma("sp", cw[:, ci, j:j + 1], self.P["ssd_conv_w"][l, j, choff[ci]:choff[ci] + 128].rearrange("(p o) -> p o", o=1), w=["cw"])
                    S.dma("sp", cbi[:, ci:ci + 1], self.P["ssd_conv_b"][l, choff[ci]:choff[ci] + 128].rearrange("(p o) -> p o", o=1), w=["cbi"])
                for ci in range(6):
                    xb = xp[0]
                    kx = ("xp", 0)
                    for tq in range(4):
                        pb, kp = self.psum()
                        for kt in range(KT):
                            S.op("pe", lambda kt=kt, pb=pb, ci=ci, tq=tq: PE.matmul(
                                pb[:], lhsT=wx[:, kt, ci * 128:(ci + 1) * 128], rhs=self.hT[:, kt, tq * 512:(tq + 1) * 512],
                                start=(kt == 0), stop=(kt == KT - 1)), r=["wx"] + hTk[tq * 4:tq * 4 + 4], w=[kp])
                        S.op("act", lambda pb=pb, xb=xb, tq=tq: A.activation(out=xb[:, 3 + tq * 512:3 + (tq + 1) * 512], in_=pb[:],
                                                                            func=AF.Copy), r=[kp], w=[kx, kp])
                    acc = t1[0] if False else None
                    cacc = self.sb(st, "cacc", [128, T], F32) if (g == 0 and ci == 0) else self._cacc
                    self._cacc = cacc
                    S.op("dve", lambda xb=xb, ci=ci, cacc=cacc: V.tensor_scalar(
                        out=cacc[:], in0=xb[:, 3:3 + T], scalar1=cw[:, ci, 3:4], scalar2=cbi[:, ci:ci + 1], op0=ALU.mult,
                        op1=ALU.add), r=[kx, "cw", "cbi"], w=["cacc"])
                    for j in range(3):
                        S.op("dve", lambda xb=xb, ci=ci, j=j, cacc=cacc: V.scalar_tensor_tensor(
                            out=cacc[:], in0=xb[:, j:j + T], scalar=cw[:, ci, j:j + 1], in1=cacc[:], op0=ALU.mult, op1=ALU.add),
                            r=[kx, "cw", "cacc"], w=["cacc"])
                    S.op("act", lambda ci=ci, cacc=cacc: A.activation(out=fT[ci][:], in_=cacc[:], func=AF.Silu),
                         r=["cacc"], w=[fk[ci]])
                    if ci < 5:
                        for t4 in range(4):
                            pb, kp = self.psum()
                            pbv = pb[:].bitcast(BF16)
                            for j in range(4):
                                tt = t4 * 4 + j
                                S.op("pe", lambda j=j, tt=tt, pbv=pbv, ci=ci: PE.transpose(
                                    out=pbv[:, j * 128:(j + 1) * 128], in_=fT[ci][:, tt * 128:(tt + 1) * 128],
                                    identity=self.identbf[:]), r=[fk[ci], "identbf"], w=[kp])
                            src = pbv[:, 0:512].rearrange("p (a b) -> p a b", a=4)
                            if ci < 4:
                                S.op("act", lambda t4=t4, src=src, ci=ci: A.activation(
                                    out=xs[:, t4 * 4:(t4 + 1) * 4, ci * 128:(ci + 1) * 128], in_=src, func=AF.Copy),
                                    r=[kp], w=["xs", kp])
                            else:
                                S.op("dve", lambda t4=t4, src=src: V.tensor_scalar(
                                    out=bmA[:, t4 * 4:(t4 + 1) * 4, :], in0=src, scalar1=mAB[:, 0:1], scalar2=None, op0=ALU.mult),
                                    r=[kp, "mAB"], w=["bmA", kp])
                                S.op("dve", lambda t4=t4, src=src: V.tensor_scalar(
                                    out=bmB[:, t4 * 4:(t4 + 1) * 4, :], in0=src, scalar1=mAB[:, 1:2], scalar2=None, op0=ALU.mult),
                                    r=[kp, "mAB"], w=["bmB", kp])
                bmT, cmT = fT[4], fT[5]
                cv = cmT[:].rearrange("p (t c j) -> p t c j", c=2, j=64)
                cva = cmTA[:].rearrange("p (t c j) -> p t c j", c=2, j=64)
                cvb = cmTB[:].rearrange("p (t c j) -> p t c j", c=2, j=64)
                S.op("pool", lambda: G.tensor_copy(out=cva[:, :, 0, :], in_=cv[:, :, 0, :]), r=[("fT", 3)], w=["cmTA"])
                S.op("pool", lambda: G.memset(cva[:, :, 1, :], 0.0), w=["cmTA"])
                S.op("pool", lambda: G.tensor_copy(out=cvb[:, :, 1, :], in_=cv[:, :, 1, :]), r=[("fT", 3)], w=["cmTB"])
                S.op("pool", lambda: G.memset(cvb[:, :, 0, :], 0.0), w=["cmTB"])
                hs = slice(g * 8, (g + 1) * 8)
                S.op("pool", lambda: G.memset(S32[:], 0.0), w=["S32"])
                S.op("pool", lambda: G.memset(Sbf[0][:], 0.0), w=[("Sbf", 0)])
                def tile_gen(tt):
                    i2 = tt % 2
                    tsl = slice(tt * 128, (tt + 1) * 128)
                    cA, cB = 2 * tt, 2 * tt + 1
                    pc, kpc = self.psum()
                    S.op("pe", lambda pc=pc: PE.matmul(pc[:, 0:128], lhsT=bmT[:, tsl], rhs=cmT[:, tsl], start=True, stop=True),
                         r=[("fT", 2), ("fT", 3)], w=[kpc])
                    S.op("act", lambda pc=pc: A.activation(out=cbs[i2][:], in_=pc[:, 0:128], func=AF.Copy),
                         r=[kpc], w=[("cbs", i2), kpc])
                    pq, kpq = self.psum()
                    S.op("pe", lambda pq=pq: PE.matmul(pq[0:16, 0:128], lhsT=da[:, tt, :], rhs=tri2[:], start=True, stop=True),
                         r=["da", "tri2"], w=[kpq])
                    S.op("dve", lambda pq=pq: V.tensor_copy(out=acsTt[i2][:], in_=pq[0:16, 0:128]), r=[kpq], w=[("acsTt", i2), kpq])
                    S.op("dve", lambda pq=pq: V.tensor_scalar(out=nacsTt[i2][:], in0=pq[0:16, 0:128], scalar1=-1.0, scalar2=None,
                                                              op0=ALU.mult), r=[kpq], w=[("nacsTt", i2), kpq])
                    S.op("pool", lambda: G.tensor_tensor(out=Dx[i2][:], in0=bd[g][:],
                                                         in1=acsTt[i2][:].unsqueeze(1).to_broadcast([16, 8, 128]), op=ALU.mult),
                         r=[("bd", g), ("acsTt", i2)], w=[("Dx", 0)])
                    S.op("pool", lambda: G.tensor_copy(out=Dxh[:], in_=Dx[i2][:]), r=[("Dx", 0)], w=["Dxh"])
                    S.op("pool", lambda: G.tensor_tensor(out=Dxl[:], in0=Dx[i2][:], in1=Dxh[:], op=ALU.subtract),
                         r=[("Dx", 0), "Dxh"], w=["Dxl"])
                    S.op("dve", lambda: V.tensor_copy(out=nah[i2][:], in_=nacsTt[i2][:]), r=[("nacsTt", i2)], w=[("nah", i2)])
                    S.op("dve", lambda: V.tensor_tensor(out=nal[i2][:], in0=nacsTt[i2][:], in1=nah[i2][:], op=ALU.subtract),
                         r=[("nacsTt", i2), ("nah", i2)], w=[("nal", i2)])
                    yield
                    for hh in range(2):
                        pe_, kpe = self.psum()
                        csl = slice(hh * 512, (hh + 1) * 512)
                        for (lh, rh, kk_, first, last) in (
                                (self.onesbf[0:16, :], Dxh[:].rearrange("p h l -> p (h l)")[:, csl], ["onesbf", "Dxh"], True, False),
                                (self.onesbf[0:16, :], Dxl[:].rearrange("p h l -> p (h l)")[:, csl], ["onesbf", "Dxl"], False, False),
                                (nah[i2][:], bdbf[:].rearrange("p h l -> p (h l)")[:, csl], [("nah", i2), "bdbf"], False, False),
                                (nal[i2][:], bdbf[:].rearrange("p h l -> p (h l)")[:, csl], [("nal", i2), "bdbf"], False, False),
                                (self.identbf[:], negmask[:].rearrange("p h l -> p (h l)")[:, csl], ["identbf", "negmask"], False, True)):
                            S.op("pe", lambda lh=lh, rh=rh, first=first, last=last, pe_=pe_: PE.matmul(
                                pe_[:], lhsT=lh, rhs=rh, start=first, stop=last), r=kk_, w=[kpe])
                        S.op("act", lambda pe_=pe_, hh=hh: A.activation(
                            out=Es[i2][:, hh * 4:(hh + 1) * 4, :], in_=pe_[:].rearrange("p (h l) -> p h l", h=4), func=AF.Exp),
                            r=[kpe], w=[("Es", i2), kpe])
                    S.op("dve", lambda: V.tensor_tensor(out=wT[i2][:], in0=Es[i2][:],
                                                        in1=cbs[i2][:].unsqueeze(1).to_broadcast([128, 8, 128]), op=ALU.mult),
                         r=[("Es", i2), ("cbs", i2)], w=[("wT", i2)])
                    yield
                    pr, kpr = self.psum()
                    pr2, kpr2 = self.psum()
                    S.op("pool", lambda: G.tensor_tensor(out=xdtt[i2][:].rearrange("p (h c) -> p h c", c=64),
                                                         in0=xs[:, tt, :].rearrange("p (h c) -> p h c", c=64),
                                                         in1=dt[:, tt, hs].unsqueeze(2).to_broadcast([128, 8, 64]), op=ALU.mult),
                         r=["xs", "dt"], w=[("xdtt", i2)])
                    S.op("pool", lambda: G.tensor_tensor(out=xendt[i2][:].rearrange("p (h c) -> p h c", c=64),
                                                         in0=xdtt[i2][:].rearrange("p (h c) -> p h c", c=64),
                                                         in1=eend[:, tt, hs].unsqueeze(2).to_broadcast([128, 8, 64]), op=ALU.mult),
                         r=[("xdtt", i2), "eend"], w=[("xendt", i2)])
                    S.op("pe", lambda pr=pr: PE.matmul(pr[:], lhsT=bmA[:, tt, :], rhs=xendt[i2][:], start=True, stop=True),
                         r=["bmA", ("xendt", i2)], w=[kpr])
                    S.op("pe", lambda pr2=pr2: PE.matmul(pr2[:], lhsT=bmB[:, tt, :], rhs=xendt[i2][:], start=True, stop=True),
                         r=["bmB", ("xendt", i2)], w=[kpr2])
                    for (ci_, prx, kprx, cc) in ((cA, pr, kpr, 0), (cB, pr2, kpr2, 1)):
                        S.op("dve", lambda cc=cc: V.tensor_tensor(
                            out=S32[:].rearrange("p (h c) -> p h c", c=64), in0=S32[:].rearrange("p (h c) -> p h c", c=64),
                            in1=edec[:, tt, cc, hs].unsqueeze(2).to_broadcast([128, 8, 64]), op=ALU.mult),
                            r=["S32", "edec"], w=["S32"])
                        S.op("dve", lambda prx=prx: V.tensor_tensor(out=S32[:], in0=prx[:], in1=S32[:], op=ALU.add),
                             r=[kprx, "S32"], w=["S32", kprx])
                        S.op("pool", lambda ci_=ci_: G.tensor_copy(out=Sbf[(ci_ + 1) % 4][:], in_=S32[:]),
                             r=["S32"], w=[("Sbf", (ci_ + 1) % 4)])
                    yield
                    pz, kpz = self.psum()
                    for kt in range(KT):
                        S.op("pe", lambda kt=kt, pz=pz: PE.matmul(pz[:], lhsT=self.hT[:, kt, tsl], rhs=wz[:, kt, :],
                                                                  start=(kt == 0), stop=(kt == KT - 1)), r=["wz", ("hT", tt)], w=[kpz])
                    S.op("act", lambda pz=pz: A.activation(out=zs[i2][:], in_=pz[:], func=AF.Silu), r=[kpz], w=[("zs", i2), kpz])
                    yield
                    py, kpy = self.psum()
                    for hh in range(8):
                        S.op("pe", lambda hh=hh, py=py: PE.matmul(py[:, hh * 64:(hh + 1) * 64], lhsT=wT[i2][:, hh, :],
                                                                  rhs=xdtt[i2][:, hh * 64:(hh + 1) * 64], start=True, stop=True),
                             r=[("wT", i2), ("xdtt", i2)], w=[kpy])
                    po, kpo = self.psum()
                    S.op("pe", lambda po=po: PE.matmul(po[:], lhsT=cmTA[:, tsl], rhs=Sbf[cA % 4][:], start=True, stop=False),
                         r=["cmTA", ("Sbf", cA % 4)], w=[kpo])
                    S.op("pe", lambda po=po: PE.matmul(po[:], lhsT=cmTB[:, tsl], rhs=Sbf[cB % 4][:], start=False, stop=True),
                         r=["cmTB", ("Sbf", cB % 4)], w=[kpo])
                    S.op("dve", lambda po=po: V.tensor_tensor(
                        out=t1[i2][:].rearrange("p (h c) -> p h c", c=64), in0=po[:].rearrange("p (h c) -> p h c", c=64),
                        in1=eacs[:, tt, hs].unsqueeze(2).to_broadcast([128, 8, 64]), op=ALU.mult),
                        r=[kpo, "eacs"], w=[("t1", i2), kpo])
                    S.op("dve", lambda py=py: V.tensor_tensor(out=t1[i2][:], in0=py[:], in1=t1[i2][:], op=ALU.add),
                         r=[kpy, ("t1", i2)], w=[("t1", i2), kpy])
                    S.op("pool", lambda: G.tensor_tensor(
                        out=t2[i2][:].rearrange("p (h c) -> p h c", c=64), in0=xs[:, tt, :].rearrange("p (h c) -> p h c", c=64),
                        in1=dsk[:, hs].unsqueeze(2).to_broadcast([128, 8, 64]), op=ALU.mult), r=["xs", "dsk"], w=[("t2", i2)])
                    S.op("pool", lambda: G.tensor_tensor(out=t2[i2][:], in0=t2[i2][:], in1=t1[i2][:], op=ALU.add),
                         r=[("t2", i2), ("t1", i2)], w=[("t2", i2)])
                    S.op("pool", lambda: G.tensor_tensor(out=t2[i2][:], in0=t2[i2][:], in1=zs[i2][:], op=ALU.mult),
                         r=[("t2", i2), ("zs", i2)], w=[("t2", i2)])
                    yield
                    S.op("act", lambda: A.activation(out=t1[i2][:], in_=t2[i2][:], func=AF.Square, accum_out=ss[i2][:, 0:1]),
                         r=[("t2", i2)], w=[("t1", i2), ("ss", i2)])
                    S.op("act", lambda: A.activation(out=ss[i2][:, 1:2], in_=ss[i2][:, 0:1], func=AF.Sqrt, bias=self.epsc[:, 1:2],
                                                     scale=1.0 / 512.0), r=[("ss", i2), "epsc"], w=[("ss", i2)])
                    S.op("dve", lambda: V.reciprocal(out=ss[i2][:, 1:2], in_=ss[i2][:, 1:2]), r=[("ss", i2)], w=[("ss", i2)])
                    S.op("dve", lambda: V.scalar_tensor_tensor(out=ytm[i2][:], in0=t2[i2][:], scalar=ss[i2][:, 1:2],
                                                               in1=nwbc[:, g * 512:(g + 1) * 512], op0=ALU.mult, op1=ALU.mult),
                         r=[("t2", i2), ("ss", i2), "nwbc"], w=[("ytm", i2)])
                    yield
                    pt, kpt = self.psum()
                    ptv = pt[:].bitcast(BF16)
                    for i in range(4):
                        S.op("pe", lambda i=i, ptv=ptv: PE.transpose(out=ptv[:, i * 128:(i + 1) * 128],
                                                                     in_=ytm[i2][:, i * 128:(i + 1) * 128], identity=self.identbf[:]),
                             r=[("ytm", i2), "identbf"], w=[kpt])
                    S.op("act", lambda ptv=ptv: A.activation(out=yTg[:, :, tsl], in_=ptv[:, 0:512].rearrange("p (a b) -> p a b", a=4),
                                                             func=AF.Copy), r=[kpt], w=["yTg", kpt])

                gens, nxt, rnd = [], 0, 0
                while nxt < NT or gens:
                    if nxt < NT and rnd % 4 == 0:
                        gens.append(tile_gen(nxt))
                        nxt += 1
                    for g_ in list(gens):
                        try:
                            next(g_)
                        except StopIteration:
                            gens.remove(g_)
                    rnd += 1
                for i in range(4):
                    S.dma("sp", ydst[g * 512 + i * 128:g * 512 + (i + 1) * 128, :], yTg[:, i, :], r=["yTg"], w=[("yT0", g * 4 + i)])
            S.barrier()


    def rwkv(self, l):
        nc, S = self.nc, self.S
        V, A, G, PE = nc.vector, nc.scalar, nc.gpsimd, nc.tensor
        Wl = self.P["w_in"][l]
        ydst = self.yT_dram[1]
        hTk = [("hT", t) for t in range(NT)]
        P_ = self.P

        def mm(out, lhsT, rhs, start, stop, r, w):
            S.op("pe", lambda: PE.matmul(out, lhsT=lhsT, rhs=rhs, start=start, stop=stop), r=r, w=w)

        with contextlib.ExitStack() as st:
            mask4 = self.sb(st, "mask4", [128, 4, 128], F32)
            maskL = self.sb(st, "maskL", [128, 2, 128], F32)
            bdm = self.sb(st, "bdm", [128, 128], F32)
            mEO = self.sb(st, "mEO", [128, 2], F32)
            hsel = self.sb(st, "hsel", [128, 2], F32)
            rm = self.sb(st, "rm128", [128, T], BF16)
            c05 = self.sb(st, "c05", [128, 1], F32)
            for j in range(4):
                S.op("pool", lambda j=j: G.affine_select(out=mask4[:, j, :], in_=self.ones[:], pattern=[[1, 128]],
                                                         compare_op=(ALU.is_gt if j % 2 == 0 else ALU.is_ge), fill=0.0,
                                                         base=0, channel_multiplier=-1), r=["ones"], w=["mask4"])
            for j in range(2):
                S.op("pool", lambda j=j: G.affine_select(out=maskL[:, j, :], in_=self.ones[:], pattern=[[-1, 128]],
                                                         compare_op=ALU.is_gt, fill=0.0, base=0, channel_multiplier=1),
                     r=["ones"], w=["maskL"])
            S.op("pool", lambda: G.memset(bdm[:], 0.0), w=["bdm"])
            S.op("pool", lambda: G.memset(bdm[0:64, 0:64], 1.0), w=["bdm"])
            S.op("pool", lambda: G.memset(bdm[64:128, 64:128], 1.0), w=["bdm"])
            for (t_, nm) in ((mEO, "mEO"), (hsel, "hsel")):
                S.op("pool", lambda t_=t_: G.memset(t_[0:64, 0:1], 1.0), w=[nm])
                S.op("pool", lambda t_=t_: G.memset(t_[64:128, 0:1], 0.0), w=[nm])
                S.op("pool", lambda t_=t_: G.memset(t_[0:64, 1:2], 0.0), w=[nm])
                S.op("pool", lambda t_=t_: G.memset(t_[64:128, 1:2], 1.0), w=[nm])
            S.op("pool", lambda: G.memset(rm[:], 1.0), w=["rm"])
            S.op("pool", lambda: G.memset(rm[:].rearrange("p (c j) -> p c j", j=128)[:, :, 0:1], 0.0), w=["rm"])
            S.op("pool", lambda: G.memset(c05[:], -0.5), w=["c05"])
            pc = {}
            for nm, src in (("mu_r", P_["rwkv_mu"][l, 0:1024]), ("mu_k", P_["rwkv_mu"][l, 1024:2048]),
                            ("mu_v", P_["rwkv_mu"][l, 2048:3072]), ("w0", P_["rwkv_w0"][l]), ("a0", P_["rwkv_a0"][l]),
                            ("k_k", P_["rwkv_k_k"][l]), ("k_a", P_["rwkv_k_a"][l]),
                            ("r_k", P_["rwkv_r_k"][l].rearrange("h k -> (h k)"))):
                t_ = self.sb(st, "pc_" + nm, [128, 8, 1], F32)
                S.dma("sp", t_[:], src.rearrange("(q p o) -> p q o", p=128, o=1), w=["pc_" + nm])
                pc[nm] = t_
            mul = self.sb(st, "mul", [128, 3], F32)
            S.dma("sp", mul[0:64, 0:1], P_["rwkv_mu"][l, 3072:3136].rearrange("(p o) -> p o", o=1), w=["mul"])
            S.dma("sp", mul[0:64, 1:2], P_["rwkv_mu"][l, 3136:3200].rearrange("(p o) -> p o", o=1), w=["mul"])
            S.dma("sp", mul[:, 2:3], P_["rwkv_mu"][l, 3200:3328].rearrange("(p o) -> p o", o=1), w=["mul"])
            nw0 = self.sb(st, "nw0", [128, 8, 1], F32)
            omka = self.sb(st, "omka", [128, 8, 1], F32)
            S.op("dve", lambda: V.tensor_scalar(out=nw0[:], in0=pc["w0"][:], scalar1=-1.0, scalar2=None, op0=ALU.mult),
                 r=["pc_w0"], w=["nw0"])
            S.op("dve", lambda: V.tensor_scalar(out=omka[:], in0=pc["k_a"][:], scalar1=-1.0, scalar2=1.0, op0=ALU.mult,
                                                op1=ALU.add), r=["pc_k_a"], w=["omka"])
            wl = self.sb(st, "wl", [128, KT, 256], BF16)
            w2 = self.sb(st, "w2", [64, D], BF16)
            a2 = self.sb(st, "a2", [64, D], BF16)
            g2 = self.sb(st, "g2", [128, D], BF16)
            S.dma("pool", wl[:], Wl[:, OFF_RWKV + 3072:OFF_RWKV + 3328].rearrange("(kt p) n -> p kt n", p=128), w=["wl"])
            S.dma("pool", w2[:], P_["rwkv_w2"][l], w=["w2"])
            S.dma("pool", a2[:], P_["rwkv_a2"][l], w=["a2"])
            S.dma("pool", g2[:], P_["rwkv_g2"][l], w=["g2"])
            txw = self.sb(st, "txw", [64, T], BF16)
            xaT = self.sb(st, "xaT", [64, T], BF16)
            sgT = self.sb(st, "sgT", [128, T], BF16)
            xraw = self.sb(st, "xraw", [128, T + 1], F32)
            F = [self.sb(st, "F%d" % i, [128, T], F32) for i in range(5)]
            S.op("pool", lambda: G.memset(xraw[:, 0:1], 0.0), w=["xraw0"])
            Y2 = xraw[:, 1:T + 1]
            Y3 = F[4]

            S.qset = {"F0", "F1", "F2", "F3", "F4", "xraw", "Pp", "Pc", "iP", "bT", "kT", "vT", "Vtm", "Btm", "Ktm",
                      ("AR", 0), ("AR", 1), "Pend0", "Pend1", "bon"}

            def Q(eng, fn, r=(), w=()):
                for q in range(4):
                    qs_ = slice(q * 512, (q + 1) * 512)
                    rr = [(k, q) if k in S.qset else k for k in r]
                    ww = [(k, q) if k in S.qset else k for k in w]
                    S.op(eng, lambda: fn(qs_, q), r=rr, w=ww)

            def proj_shift(wt, c0, m, mucol, dst, dkey, func=None, rows=128):
                for tq in range(4):
                    pb, kp = self.psum()
                    for kt in range(KT):
                        mm(pb[0:rows, :], wt[:, kt, c0:c0 + m], self.hT[:, kt, tq * 512:(tq + 1) * 512], kt == 0, kt == KT - 1,
                           [wt_key] + hTk[tq * 4:tq * 4 + 4], [kp])
                    S.op("act", lambda pb=pb, tq=tq: A.activation(out=xraw[0:rows, 1 + tq * 512:1 + (tq + 1) * 512],
                                                                  in_=pb[0:rows, :], func=AF.Copy), r=[kp], w=[("xraw", tq), kp])
                dk = (lambda q: (dkey, q)) if dkey in S.qset else (lambda q: dkey)
                for q in range(4):
                    lo, hi = q * 512, (q + 1) * 512
                    xr = [("xraw", q)] + ([("xraw", q - 1)] if q else ["xraw0"])
                    S.op("dve", lambda: V.tensor_tensor(out=F[4][0:rows, lo:hi], in0=xraw[0:rows, lo:hi], in1=xraw[0:rows, lo + 1:hi + 1],
                                                        op=ALU.subtract), r=xr, w=[("F4", q)])
                    if func is None:
                        S.op("dve", lambda: V.scalar_tensor_tensor(out=dst[0:rows, lo:hi], in0=F[4][0:rows, lo:hi], scalar=mucol,
                                                                   in1=xraw[0:rows, lo + 1:hi + 1], op0=ALU.mult, op1=ALU.add),
                             r=[("F4", q), ("xraw", q), "mul"] + list(pc_keys), w=[dk(q)])
                    else:
                        S.op("dve", lambda: V.scalar_tensor_tensor(out=F[4][0:rows, lo:hi], in0=F[4][0:rows, lo:hi], scalar=mucol,
                                                                   in1=xraw[0:rows, lo + 1:hi + 1], op0=ALU.mult, op1=ALU.add),
                             r=[("F4", q), ("xraw", q), "mul"] + list(pc_keys), w=[("F4", q)])
                        S.op("act", lambda: A.activation(out=dst[0:rows, lo:hi], in_=F[4][0:rows, lo:hi], func=func),
                             r=[("F4", q)], w=[dk(q)])

            pc_keys = ["pc_mu_r", "pc_mu_k", "pc_mu_v"]
            wt_key = "wl"
            proj_shift(wl, 0, 64, mul[0:64, 0:1], txw, "txw", AF.Tanh, rows=64)
            proj_shift(wl, 64, 64, mul[0:64, 1:2], xaT, "xaT", AF.Copy, rows=64)
            proj_shift(wl, 128, 128, mul[:, 2:3], sgT, "sgT", AF.Sigmoid, rows=128)
            wrkv1 = self.sb(st, "wrkv", [128, KT, 384], BF16)
            wrkv = [wrkv1, wrkv1]
            Pp = self.sb(st, "Pp", [128, T], BF16)
            Pc = self.sb(st, "Pc", [128, T], BF16)
            PendL = [self.sb(st, "Pend", [128, NT], F32) for _ in range(2)]
            iP = self.sb(st, "iP", [128, T], BF16)
            bT = self.sb(st, "bT", [128, T], BF16)
            kT = self.sb(st, "kT", [128, T], BF16)
            vT = self.sb(st, "vT", [128, T], BF16)
            AR = [self.sb(st, "AR", [128, NT, 2, 128], BF16) for _ in range(2)]
            Vtm = self.sb(st, "Vtm", [128, NT, 128], BF16)
            Btm = self.sb(st, "Btm", [128, NT, 128], BF16)
            aT = Vtm[:].rearrange("p t j -> p (t j)")
            rT = Btm[:].rearrange("p t j -> p (t j)")
            Ktm = self.sb(st, "Ktm", [128, NT, 128], BF16)
            bon = self.sb(st, "bon", [128, NT, 2], F32)
            st32 = self.sb(st, "st32", [128, 2 * NT, 4], F32)
            lnw = self.sb(st, "lnwb", [128, 128], F32)
            lnb = self.sb(st, "lnbb", [128, 128], F32)
            Z32 = self.sb(st, "Z32", [128, 128], F32)
            Zt = self.sb(st, "Zt", [128, 128], F32)
            Zbf = [self.sb(st, "Zbf", [128, 128], BF16) for _ in range(2)]
            Wsb = [self.sb(st, "Wsb", [128, 128], BF16) for _ in range(2)]
            Usb = [self.sb(st, "Usb", [128, 128], BF16) for _ in range(2)]
            NS = 8
            abrb = [self.sb(st, "abrb", [128, 4, 128], BF16) for _ in range(NS)]
            akrk = [self.sb(st, "akrk", [128, 4, 128], BF16) for _ in range(NS)]
            L0 = [self.sb(st, "L0", [128, 2, 128], BF16) for _ in range(4)]
            XX = [[self.sb(st, "XX", [128, 4, 128], BF16) for _ in range(4)] for _ in range(2)]
            Tt = [self.sb(st, "Tt", [128, 2, 128], BF16) for _ in range(NS)]

            def load_w(p):
                b = 0
                for j in range(3):
                    c0 = OFF_RWKV + j * 1024 + p * 128
                    S.dma("pool", wrkv[b][:, :, j * 128:(j + 1) * 128], Wl[:, c0:c0 + 128].rearrange("(kt p) n -> p kt n", p=128),
                          w=[("wrkv", b)])

            def projA(p):
                nonlocal wt_key
                load_w(p)
                wt_key = ("wrkv", 0)
                proj_shift(wrkv[0], 128, 128, pc["mu_k"][:, p, :], F[0], "F0")
                proj_shift(wrkv[0], 0, 128, pc["mu_r"][:, p, :], F[1], "F1")
                proj_shift(wrkv[0], 256, 128, pc["mu_v"][:, p, :], vT, "vT", AF.Copy)

            filler = [None]
            projA(0)
            for p in range(8):
                b = 0
                fs_ = slice(p * 128, (p + 1) * 128)
                S.dma("sp", lnw[:], P_["rwkv_ln_w"][l, fs_].partition_broadcast(128), w=["lnw"])
                S.dma("sp", lnb[:], P_["rwkv_ln_b"][l, fs_].partition_broadcast(128), w=["lnb"])
                def prepB1(p, fs_):
                    for tq in range(4):
                        pb, kp = self.psum()
                        qs_ = slice(tq * 512, (tq + 1) * 512)
                        mm(pb[:], w2[:, fs_], txw[:, qs_], True, True, ["w2", "txw"], [kp])
                        S.op("act", lambda pb=pb: A.activation(out=F[2][:, qs_], in_=pb[:], func=AF.Exp, bias=nw0[:, p, :], scale=-1.0),
                             r=[kp, "nw0"], w=[("F2", tq), kp])
                    yield
                    Q("act", lambda qs, q: A.activation(out=F[2][:, qs], in_=F[2][:, qs], func=AF.Ln, bias=self.epsc[:, 3:4], scale=1.0),
                      r=["F2", "epsc"], w=["F2"])
                    yield
                    Q("act", lambda qs, q: A.activation(out=F[2][:, qs], in_=F[2][:, qs], func=AF.Exp, bias=c05[:, 0:1], scale=-1.0),
                      r=["F2", "c05"], w=["F2"])
                    yield
                    Q("dve", lambda qs, q: V.tensor_tensor_scan(out=F[3][:, qs], data0=rm[:, qs], data1=F[2][:, qs], initial=0.0,
                                                                op0=ALU.mult, op1=ALU.add), r=["rm", "F2"], w=["F3"])
                    yield
                    Q("dve", lambda qs, q: V.tensor_tensor(out=F[2][:, qs], in0=F[3][:, qs], in1=F[2][:, qs], op=ALU.subtract),
                      r=["F2", "F3"], w=["F2"])
                    yield
                    Q("act", lambda qs, q: A.activation(out=Pp[:, qs], in_=F[2][:, qs], func=AF.Exp, scale=-1.0), r=["F2"], w=["Pp"])
                    yield
                    Q("act", lambda qs, q: A.activation(out=Pc[:, qs], in_=F[3][:, qs], func=AF.Exp, scale=-1.0), r=["F3"], w=["Pc"])
                    yield
                    Q("act", lambda qs, q: A.activation(out=iP[:, qs], in_=F[3][:, qs], func=AF.Exp), r=["F3"], w=["iP"])
                    yield
                    Q("act", lambda qs, q: A.activation(out=PendL[p % 2][:, q * 4:(q + 1) * 4],
                                                        in_=F[3][:, qs].rearrange("p (t j) -> p t j", j=128)[:, :, 127],
                                                        func=AF.Exp, scale=-1.0), r=["F3"], w=["Pend%d" % (p % 2)])
                    yield
                    for tq in range(4):
                        pb, kp = self.psum()
                        qs_ = slice(tq * 512, (tq + 1) * 512)
                        mm(pb[:], a2[:, fs_], xaT[:, qs_], True, True, ["a2", "xaT"], [kp])
                        S.op("act", lambda pb=pb: A.activation(out=F[2][:, qs_], in_=pb[:], func=AF.Sigmoid, bias=pc["a0"][:, p, :],
                                                               scale=1.0), r=[kp, "pc_a0", ("Pp", tq)], w=[("F2", tq), kp])
                    yield
                    Q("dve", lambda qs, q: V.tensor_scalar(out=F[3][:, qs], in0=F[0][:, qs], scalar1=pc["k_k"][:, p, :], scalar2=None,
                                                           op0=ALU.mult), r=["F0", "pc_k_k", "Pc", "iP", "Pend%d" % (p % 2)], w=["F3"])
                    yield
                    Q("act", lambda qs, q: A.activation(out=F[4][:, qs], in_=F[3][:, qs], func=AF.Square), r=["F3"], w=["F4"])
                    yield
                    for tq in range(4):
                        pb, kp = self.psum()
                        qs_ = slice(tq * 512, (tq + 1) * 512)
                        mm(pb[:], bdm[:], F[4][:, qs_], True, True, ["bdm", ("F4", tq)], [kp])
                        S.op("act", lambda pb=pb: A.activation(out=F[4][:, qs_], in_=pb[:], func=AF.Sqrt), r=[kp], w=[("F4", tq), kp])
                    yield
                    Q("dve", lambda qs, q: V.tensor_scalar(out=F[4][:, qs], in0=F[4][:, qs], scalar1=1e-12, scalar2=None, op0=ALU.max),
                      r=["F4"], w=["F4"])
                    yield
                    Q("dve", lambda qs, q: V.reciprocal(out=F[4][:, qs], in_=F[4][:, qs]), r=["F4"], w=["F4"])
                    yield
                    Q("dve", lambda qs, q: V.tensor_tensor(out=F[3][:, qs], in0=F[3][:, qs], in1=F[4][:, qs], op=ALU.mult),
                      r=["F3", "F4"], w=["F3"])

                    yield

                def prepB2(p, fs_):
                    Q("dve", lambda qs, q: V.scalar_tensor_tensor(out=aT[:, qs], in0=F[3][:, qs], scalar=-1.0, in1=Pp[:, qs], op0=ALU.mult,
                                                                  op1=ALU.mult), r=["F3", "Pp"], w=["Vtm"])
                    Q("pool", lambda qs, q: G.tensor_tensor(out=F[4][:, qs], in0=F[3][:, qs], in1=F[2][:, qs], op=ALU.mult),
                      r=["F3", "F2"], w=["F4"])
                    Q("pool", lambda qs, q: G.tensor_tensor(out=bT[:, qs], in0=F[4][:, qs], in1=iP[:, qs], op=ALU.mult),
                      r=["F4", "iP"], w=["bT"])
                    Q("dve", lambda qs, q: V.tensor_scalar(out=F[2][:, qs], in0=F[2][:, qs], scalar1=pc["k_a"][:, p, :],
                                                           scalar2=omka[:, p, :], op0=ALU.mult, op1=ALU.add),
                      r=["F2", "pc_k_a", "omka", "F4"], w=["F2"])
                    Q("dve", lambda qs, q: V.tensor_tensor(out=F[0][:, qs], in0=F[0][:, qs], in1=F[2][:, qs], op=ALU.mult),
                      r=["F0", "F2", "F3"], w=["F0"])
                    Q("pool", lambda qs, q: G.tensor_tensor(out=kT[:, qs], in0=F[0][:, qs], in1=iP[:, qs], op=ALU.mult),
                      r=["F0", "iP"], w=["kT"])
                    Q("dve", lambda qs, q: V.tensor_tensor(out=rT[:, qs], in0=F[1][:, qs], in1=Pc[:, qs], op=ALU.mult),
                      r=["F1", "Pc"], w=["Btm"])
                    Q("dve", lambda qs, q: V.scalar_tensor_tensor(out=F[1][:, qs], in0=F[1][:, qs], scalar=pc["r_k"][:, p, :],
                                                                  in1=F[0][:, qs], op0=ALU.mult, op1=ALU.mult),
                      r=["F1", "F0", "pc_r_k", "Btm"], w=["F1"])
                    for h in range(2):
                        Q("act", lambda qs, q, h=h: A.activation(out=AR[h][:, q * 4:(q + 1) * 4, 0, :], in_=Vtm[:, q * 4:(q + 1) * 4, :],
                                                                 func=AF.Identity, scale=mEO[:, h:h + 1]), r=["Vtm", "mEO"], w=[("AR", h)])
                        Q("dve", lambda qs, q, h=h: V.tensor_scalar(out=AR[h][:, q * 4:(q + 1) * 4, 1, :], in0=Btm[:, q * 4:(q + 1) * 4, :],
                                                                    scalar1=mEO[:, h:h + 1], scalar2=None, op0=ALU.mult),
                          r=["Btm", "mEO"], w=[("AR", h)])
                    for (src, skey, dst, dkey) in ((vT, "vT", Vtm, "Vtm"), (bT, "bT", Btm, "Btm"), (kT, "kT", Ktm, "Ktm")):
                        for t4 in range(4):
                            pb, kp = self.psum()
                            pbv = pb[:].bitcast(BF16)
                            for j in range(4):
                                tt = t4 * 4 + j
                                S.op("pe", lambda j=j, tt=tt, pbv=pbv, src=src: PE.transpose(
                                    out=pbv[:, j * 128:(j + 1) * 128], in_=src[:, tt * 128:(tt + 1) * 128], identity=self.identbf[:]),
                                    r=[(skey, t4), "identbf"], w=[kp])
                            S.op("act", lambda t4=t4, pbv=pbv, dst=dst: A.activation(
                                out=dst[:, t4 * 4:(t4 + 1) * 4, :], in_=pbv[:, 0:512].rearrange("p (a b) -> p a b", a=4), func=AF.Copy),
                                r=[kp, (("AR", 0), t4), (("AR", 1), t4)], w=[(dkey, t4), kp])
                    for t4 in range(4):
                        pb, kp = self.psum()
                        for j in range(4):
                            tt = t4 * 4 + j
                            mm(pb[:, j * 2:(j + 1) * 2], F[1][:, tt * 128:(tt + 1) * 128], hsel[:], True, True, [("F1", t4), "hsel"], [kp])
                        S.op("dve", lambda pb=pb, t4=t4: V.tensor_copy(out=bon[:, t4 * 4:(t4 + 1) * 4, :].rearrange("p t h -> p (t h)"),
                                                                       in_=pb[:, 0:8]), r=[kp], w=[("bon", t4), kp])

                ytm = Y2[:].rearrange("p (t j) -> p t j", j=128)
                ysq = Y3[:].rearrange("p (t j) -> p t j", j=128)

                def inv_group(gi, pending=()):
                    pending = list(pending)
                    tiles = range(gi * 4, gi * 4 + 4)
                    for tt in tiles:
                        sl = tt % NS
                        tsl = slice(tt * 128, (tt + 1) * 128)
                        p1, k1 = self.psum()
                        p2, k2 = self.psum()
                        p3, k3 = self.psum()
                        for h in range(2):
                            rhs = AR[h][:, tt].rearrange("p a j -> p (a j)")
                            mm(p1[:, h * 256:(h + 1) * 256], bT[:, tsl], rhs, True, True, ["bT", ("AR", h)], [k1])
                            mm(p2[:, h * 256:(h + 1) * 256], kT[:, tsl], rhs, True, True, ["kT", ("AR", h)], [k2])
                            mm(p3[:, h * 128:(h + 1) * 128], AR[h][:, tt, 0, :], bT[:, tsl], True, True, [("AR", h), "bT"], [k3])
                        S.op("dve", lambda p1=p1, sl=sl: V.tensor_tensor(out=abrb[sl][:].rearrange("p a j -> p (a j)"), in0=p1[:],
                                                                        in1=mask4[:].rearrange("p a j -> p (a j)"), op=ALU.mult),
                             r=[k1, "mask4"], w=[("abrb", sl), k1])
                        S.op("dve", lambda p2=p2, sl=sl: V.tensor_tensor(out=akrk[sl][:].rearrange("p a j -> p (a j)"), in0=p2[:],
                                                                        in1=mask4[:].rearrange("p a j -> p (a j)"), op=ALU.mult),
                             r=[k2, "mask4"], w=[("akrk", sl), k2])
                        S.op("dve", lambda p3=p3, sl=sl: V.tensor_tensor(out=L0[sl % 4][:].rearrange("p a j -> p (a j)"), in0=p3[:, 0:256],
                                                                        in1=maskL[:].rearrange("p a j -> p (a j)"), op=ALU.mult),
                             r=[k3, "maskL"], w=[("L0", sl % 4), k3])
                        for h in range(2):
                            S.op("pool", lambda h=h, sl=sl: G.tensor_tensor(out=Tt[sl][:, h, :], in0=abrb[sl][:, 2 * h, :],
                                                                            in1=self.identbf[:], op=ALU.add),
                                 r=[("abrb", sl), "identbf"], w=[("Tt", sl)])

                    def Xk(k, sl, h):
                        return (L0[sl % 4][:, h, :], ("L0", sl % 4)) if k == 0 else (XX[k % 2][sl % 4][:, 2 * h, :], ("XX", k % 2, sl % 4))

                    def Xtk(k, sl, h):
                        return (abrb[sl][:, 2 * h, :], ("abrb", sl)) if k == 0 else (XX[k % 2][sl % 4][:, 2 * h + 1, :], ("XX", k % 2, sl % 4))

                    for k in range(7):
                        sqb = {}
                        if k <= 5:
                            for tt in tiles:
                                sl = tt % NS
                                pb, kp = self.psum()
                                sqb[tt] = (pb, kp)
                                for h in range(2):
                                    x, kx = Xk(k, sl, h)
                                    xt, kxt = Xtk(k, sl, h)
                                    mm(pb[:, (2 * h) * 128:(2 * h + 1) * 128], xt, x, True, True, [kx, kxt], [kp])
                                    if k < 5:
                                        mm(pb[:, (2 * h + 1) * 128:(2 * h + 2) * 128], x, xt, True, True, [kx, kxt], [kp])
                        ttb = []
                        if k >= 1:
                            for t2 in range(2):
                                pb, kp = self.psum()
                                ttb.append((pb, kp))
                                for j in range(2):
                                    sl = (gi * 4 + t2 * 2 + j) % NS
                                    for h in range(2):
                                        x1, kx1 = Xk(k, sl, h)
                                        mm(pb[:, (j * 2 + h) * 128:(j * 2 + h + 1) * 128], x1, Tt[sl][:, h, :], True, True,
                                           [kx1, ("Tt", sl)], [kp])
                        if k <= 5:
                            for tt in tiles:
                                sl = tt % NS
                                pb, kp = sqb[tt]
                                kn = ("XX", (k + 1) % 2, sl % 4)
                                if k < 5:
                                    S.op("act", lambda pb=pb, sl=sl, k=k: A.activation(
                                        out=XX[(k + 1) % 2][sl % 4][:].rearrange("p a j -> p (a j)"), in_=pb[:], func=AF.Copy),
                                        r=[kp], w=[kn, kp])
                                else:
                                    S.op("act", lambda pb=pb, sl=sl, k=k: A.activation(
                                        out=XX[(k + 1) % 2][sl % 4][:, 0:4:2, :],
                                        in_=pb[:].rearrange("p (a j) -> p a j", j=128)[:, 0:4:2, :], func=AF.Copy), r=[kp], w=[kn, kp])
                        for t2, (pb, kp) in enumerate(ttb):
                            for j in range(2):
                                sl = (gi * 4 + t2 * 2 + j) % NS
                                S.op("dve", lambda pb=pb, sl=sl, j=j: V.tensor_tensor(
                                    out=Tt[sl][:].rearrange("p a j -> p (a j)"), in0=pb[:, j * 256:(j + 1) * 256],
                                    in1=Tt[sl][:].rearrange("p a j -> p (a j)"), op=ALU.add), r=[kp, ("Tt", sl)], w=[("Tt", sl), kp])
                        if pending and k >= 1:
                            chain_tile(pending.pop(0))
                        if filler[0] is not None:
                            next(filler[0], None)
                    while pending:
                        chain_tile(pending.pop(0))

                def chain_tile(tt):
                    if True:
                        sl = tt % NS
                        i2 = tt % 2
                        tsl = slice(tt * 128, (tt + 1) * 128)
                        zb, kz = Zbf[i2], ("Zbf", i2)
                        pw, kpw = self.psum()
                        mm(pw[:, 0:128], AR[0][:, tt, 0, :], zb[:], True, False, [("AR", 0), kz], [kpw])
                        mm(pw[:, 0:128], AR[1][:, tt, 0, :], zb[:], False, False, [("AR", 1), kz], [kpw])
                        for h in range(2):
                            mm(pw[:, h * 64:(h + 1) * 64], akrk[sl][:, 2 * h, :], Vtm[:, tt, h * 64:(h + 1) * 64], False, h == 1,
                               [("akrk", sl), "Vtm"], [kpw])
                        S.op("act", lambda pw=pw: A.activation(out=Wsb[i2][:], in_=pw[:, 0:128], func=AF.Copy),
                             r=[kpw], w=[("Wsb", i2), kpw])
                        pu, kpu = self.psum()
                        for h in range(2):
                            mm(pu[:, h * 64:(h + 1) * 64], Tt[sl][:, h, :], Wsb[i2][:, h * 64:(h + 1) * 64], True, True,
                               [("Tt", sl), ("Wsb", i2)], [kpu])
                        S.op("act", lambda pu=pu: A.activation(out=Usb[i2][:], in_=pu[:, 0:128], func=AF.Copy),
                             r=[kpu], w=[("Usb", i2), kpu])
                        py, kpy = self.psum()
                        mm(py[:, 0:128], AR[0][:, tt, 1, :], zb[:], True, False, [("AR", 0), kz], [kpy])
                        mm(py[:, 0:128], AR[1][:, tt, 1, :], zb[:], False, False, [("AR", 1), kz], [kpy])
                        for h in range(2):
                            mm(py[:, h * 64:(h + 1) * 64], abrb[sl][:, 2 * h + 1, :], Usb[i2][:, h * 64:(h + 1) * 64], False, False,
                               [("abrb", sl), ("Usb", i2)], [kpy])
                            mm(py[:, h * 64:(h + 1) * 64], akrk[sl][:, 2 * h + 1, :], Vtm[:, tt, h * 64:(h + 1) * 64], False, h == 1,
                               [("akrk", sl), "Vtm"], [kpy])
                        S.op("act", lambda py=py: A.activation(out=ytm[:, tt, :], in_=py[:, 0:128], func=AF.Copy),
                             r=[kpy], w=["xraw", kpy])
                        pz, kpz = self.psum()
                        mm(pz[:, 0:128], Btm[:, tt, :], Usb[i2][:], True, False, ["Btm", ("Usb", i2)], [kpz])
                        mm(pz[:, 0:128], Ktm[:, tt, :], Vtm[:, tt, :], False, True, ["Ktm", "Vtm"], [kpz])
                        S.op("dve", lambda pz=pz: V.tensor_tensor(out=Zt[:], in0=pz[:, 0:128], in1=bdm[:], op=ALU.mult),
                             r=[kpz, "bdm"], w=["Zt", kpz])
                        S.op("dve", lambda: V.tensor_tensor(out=Zt[:], in0=Zt[:], in1=Z32[:], op=ALU.add), r=["Zt", "Z32"], w=["Zt"])
                        S.op("dve", lambda: V.tensor_scalar(out=Z32[:], in0=Zt[:], scalar1=PendL[p % 2][:, tt:tt + 1],
                                                            scalar2=None, op0=ALU.mult), r=["Zt", "Pend%d" % (p % 2)], w=["Z32"])
                        S.op("pool", lambda: G.tensor_copy(out=Zbf[(tt + 1) % 2][:], in_=Z32[:]), r=["Z32"], w=[("Zbf", (tt + 1) % 2)])

                S.op("pool", lambda: G.memset(Z32[:], 0.0), w=["Z32"])
                S.op("pool", lambda: G.memset(Zbf[0][:], 0.0), w=[("Zbf", 0)])
                if p == 0:
                    for _ in prepB1(0, fs_):
                        pass
                prepB2(p, fs_)
                if p + 1 < 8:
                    projA(p + 1)
                    filler[0] = prepB1(p + 1, slice((p + 1) * 128, (p + 2) * 128))
                inv_group(0)
                for gi in range(1, 4):
                    inv_group(gi, pending=range((gi - 1) * 4, gi * 4))
                for tt in range(12, 16):
                    chain_tile(tt)
                if filler[0] is not None:
                    for _ in filler[0]:
                        pass
                    filler[0] = None
                y16, yTp = Btm, kT
                def output_phase(p=p, fs_=fs_, ytm=ytm, ysq=ysq):
                    y3 = ytm.rearrange("p t (h c) -> p (t h) c", c=64)
                    q3 = ysq.rearrange("p t (h c) -> p (t h) c", c=64)
                    S.op("act", lambda: A.activation(out=Y3[:], in_=Y2[:], func=AF.Square), r=["xraw"], w=["F4"])
                    S.op("dve", lambda: V.tensor_reduce(out=st32[:, :, 0], in_=y3, axis=AX.X, op=ALU.add), r=["xraw"], w=["st32"])
                    S.op("dve", lambda: V.tensor_reduce(out=st32[:, :, 1], in_=q3, axis=AX.X, op=ALU.add), r=["F4"], w=["st32"])
                    S.op("dve", lambda: V.tensor_scalar(out=st32[:, :, 0], in0=st32[:, :, 0], scalar1=1.0 / 64.0, scalar2=None,
                                                        op0=ALU.mult), r=["st32"], w=["st32"])
                    S.op("dve", lambda: V.tensor_tensor(out=st32[:, :, 2], in0=st32[:, :, 0], in1=st32[:, :, 0], op=ALU.mult),
                         r=["st32"], w=["st32"])
                    S.op("dve", lambda: V.scalar_tensor_tensor(out=st32[:, :, 1], in0=st32[:, :, 1], scalar=1.0 / 64.0, in1=st32[:, :, 2],
                                                               op0=ALU.mult, op1=ALU.subtract), r=["st32"], w=["st32"])
                    S.op("act", lambda: A.activation(out=st32[:, :, 1], in_=st32[:, :, 1], func=AF.Sqrt, bias=self.epsc[:, 2:3], scale=1.0),
                         r=["st32", "epsc"], w=["st32"])
                    S.op("dve", lambda: V.reciprocal(out=st32[:, :, 1], in_=st32[:, :, 1]), r=["st32"], w=["st32"])
                    S.op("dve", lambda: V.tensor_tensor(out=y3, in0=y3, in1=st32[:, :, 0:1].to_broadcast([128, 2 * NT, 64]),
                                                        op=ALU.subtract), r=["xraw", "st32"], w=["xraw"])
                    S.op("dve", lambda: V.tensor_tensor(out=y3, in0=y3, in1=st32[:, :, 1:2].to_broadcast([128, 2 * NT, 64]),
                                                        op=ALU.mult), r=["xraw", "st32"], w=["xraw"])
                    S.op("pool", lambda: G.tensor_tensor(out=ytm, in0=ytm, in1=lnw[:].unsqueeze(1).to_broadcast([128, NT, 128]),
                                                         op=ALU.mult), r=["xraw", "lnw"], w=["xraw"])
                    S.op("pool", lambda: G.tensor_tensor(out=ytm, in0=ytm, in1=lnb[:].unsqueeze(1).to_broadcast([128, NT, 128]),
                                                         op=ALU.add), r=["xraw", "lnb"], w=["xraw"])
                    S.op("dve", lambda: V.tensor_tensor(out=q3, in0=Vtm[:].rearrange("p t (h c) -> p (t h) c", c=64),
                                                        in1=bon[:].rearrange("p t h -> p (t h)").unsqueeze(2).to_broadcast([128, 2 * NT, 64]),
                                                        op=ALU.mult), r=["Vtm", "bon", "F4"], w=["F4"])
                    S.op("dve", lambda: V.tensor_tensor(out=Y2[:], in0=Y2[:], in1=Y3[:], op=ALU.add), r=["xraw", "F4"], w=["xraw"])
                    for t4 in range(4):
                        pb, kp = self.psum()
                        for j in range(4):
                            tt = t4 * 4 + j
                            mm(pb[:, j * 128:(j + 1) * 128], sgT[:, tt * 128:(tt + 1) * 128], g2[:, fs_], True, True, ["sgT", "g2"], [kp])
                        S.op("dve", lambda pb=pb, t4=t4: V.tensor_tensor(
                            out=y16[:, t4 * 4:(t4 + 1) * 4, :], in0=pb[:].rearrange("p (a j) -> p a j", j=128),
                            in1=ytm[:, t4 * 4:(t4 + 1) * 4, :], op=ALU.mult), r=[kp, "xraw"], w=["Btm", kp])
                    for t4 in range(4):
                        pb, kp = self.psum()
                        pbv = pb[:].bitcast(BF16)
                        for j in range(4):
                            tt = t4 * 4 + j
                            S.op("pe", lambda j=j, tt=tt, pbv=pbv: PE.transpose(out=pbv[:, j * 128:(j + 1) * 128], in_=y16[:, tt, :],
                                                                               identity=self.identbf[:]), r=["Btm", "identbf"], w=[kp])
                        S.op("act", lambda t4=t4, pbv=pbv: A.activation(out=yTp[:, t4 * 512:(t4 + 1) * 512], in_=pbv[:, 0:512], func=AF.Copy),
                             r=[kp], w=["kT", kp])
                    S.dma("sp", ydst[fs_, :], yTp[:], r=["kT"], w=[("yT1", p)])
                output_phase()
            S.barrier()
            S.qset = set()


    def merge(self, l):
        nc, S = self.nc, self.S
        V, A, G, PE = nc.vector, nc.scalar, nc.gpsimd, nc.tensor
        Wl = self.P["w_in"][l]
        brw = [self.P["w_br_ssd"][l], self.P["w_br_rwkv"][l], self.P["w_br_hgrn"][l]]
        hTk = [("hT", t) for t in range(NT)]
        with contextlib.ExitStack() as st:
            mT = self.sb(st, "mT", [128, KT, T], F32)
            wbrs = [self.sb(st, "wbr", [128, KT, D], BF16) for _ in range(2)]
            wgts = [self.sb(st, "wgt", [128, KT, D], BF16) for _ in range(2)]
            st1 = contextlib.ExitStack()
            st1.__enter__()
            yq = [self.sb(st1, "yq", [128, KT, 512], BF16) for _ in range(2)]
            sg = [self.sb(st1, "sgm", [128, 512], BF16) for _ in range(2)]
            tmp = [self.sb(st1, "tmpm", [128, 512], F32) for _ in range(2)]
            cnt = 0
            def load_br(i):
                S.dma("pool", wbrs[i % 2][:], brw[i].rearrange("(kt p) n -> p kt n", p=128), w=[("wbr", i % 2)])
                c0 = OFF_GATES + i * 1024
                S.dma("pool", wgts[i % 2][:], Wl[:, c0:c0 + 1024].rearrange("(kt p) n -> p kt n", p=128), w=[("wgt", i % 2)])

            load_br(0)
            load_br(1)
            for i in range(3):
                wbr, wgt = wbrs[i % 2], wgts[i % 2]
                kwb, kwg = ("wbr", i % 2), ("wgt", i % 2)
                if i == 2:
                    load_br(2)
                for q in range(4):
                    qs_ = slice(q * 512, (q + 1) * 512)
                    yb = (i * 4 + q) % 2
                    S.dma("sp", yq[yb][:], self.yT_dram[i][:, qs_].rearrange("(kt p) n -> p kt n", p=128),
                          r=[("yT%d" % i, k) for k in range(8)], w=[("yq", yb)])
                    for ot in range(KT):
                        os_ = slice(ot * 128, (ot + 1) * 128)
                        pg, kg = self.psum()
                        pb, kb = self.psum()
                        for kt in range(KT):
                            S.op("pe", lambda kt=kt, pg=pg: PE.matmul(pg[:], lhsT=wgt[:, kt, os_], rhs=self.hT[:, kt, qs_],
                                                                      start=(kt == 0), stop=(kt == KT - 1)),
                                 r=[kwg] + hTk[q * 4:q * 4 + 4], w=[kg])
                        for kt in range(KT):
                            S.op("pe", lambda kt=kt, pb=pb: PE.matmul(pb[:], lhsT=wbr[:, kt, os_], rhs=yq[yb][:, kt, :],
                                                                      start=(kt == 0), stop=(kt == KT - 1)),
                                 r=[kwb, ("yq", yb)], w=[kb])
                        c2 = cnt % 2
                        cnt += 1
                        S.op("act", lambda pg=pg, c2=c2: A.activation(out=sg[c2][:], in_=pg[:], func=AF.Sigmoid),
                             r=[kg], w=[("sgm", c2), kg])
                        if i == 0:
                            S.op("dve", lambda pb=pb, c2=c2: V.tensor_tensor(out=mT[:, ot, qs_], in0=pb[:], in1=sg[c2][:], op=ALU.mult),
                                 r=[kb, ("sgm", c2)], w=[("mT", q), kb])
                        else:
                            S.op("dve", lambda pb=pb, c2=c2: V.tensor_tensor(out=tmp[c2][:], in0=pb[:], in1=sg[c2][:], op=ALU.mult),
                                 r=[kb, ("sgm", c2)], w=[("tmpm", c2), kb])
                            S.op("pool", lambda c2=c2: G.tensor_tensor(out=mT[:, ot, qs_], in0=mT[:, ot, qs_], in1=tmp[c2][:],
                                                                       op=ALU.add), r=[("tmpm", c2), ("mT", q)], w=[("mT", q)])
            self.dbg_dump("merged%d" % l, lambda o: S.dma("sp", o.rearrange("(kt p) n -> p kt n", p=128), mT[:],
                                                          r=[("mT", q) for q in range(4)]))
            S.barrier()
            st1.__exit__(None, None, None)
            wo = wbrs[1]
            S.dma("pool", wo[:], self.P["w_out"][l].rearrange("(kt p) n -> p kt n", p=128), w=[("wbr", 1)])
            gbc = self.sb(st, "gbc1", [128, D], F32)
            bbc = self.sb(st, "bbc1", [128, D], F32)
            S.dma("sp", gbc[:], self.P["ln1_g"][l].partition_broadcast(128), w=["gbc"])
            S.dma("sp", bbc[:], self.P["ln1_b"][l].partition_broadcast(128), w=["bbc"])
            lnw = self.ln_alloc(st)
            h1 = [self.sb(st, "h1m", [128, D], F32) for _ in range(2)]
            mbf1 = self.sb(st, "mbf", [128, KT, 128], BF16)
            mbf = [mbf1, mbf1]
            def m2_tile(tt):
                s2 = tt % 2
                q = tt // 4
                tsl = slice(tt * 128, (tt + 1) * 128)
                S.op("act", lambda: A.activation(out=mbf[s2][:], in_=mT[:, :, tsl], func=AF.Copy), r=[("mT", q)], w=[("mbf", 0)])
                S.dma("sp", h1[s2][:], self.h_dram[tsl, :], r=[("hd", tt)], w=[("h1m", s2)])
                xin, kx = self.ln_xin(lnw, tt)
                for half in range(2):
                    po, ko = self.psum()
                    for kt in range(KT):
                        S.op("pe", lambda kt=kt, po=po: PE.matmul(po[:], lhsT=mbf[s2][:, kt, :], rhs=wo[:, kt, half * 512:(half + 1) * 512],
                                                                  start=(kt == 0), stop=(kt == KT - 1)), r=[("mbf", 0), ("wbr", 1)], w=[ko])
                    S.op("dve", lambda po=po, half=half: V.scalar_tensor_tensor(
                        out=xin[:, half * 512:(half + 1) * 512], in0=h1[s2][:, half * 512:(half + 1) * 512], scalar=ALPHA, in1=po[:],
                        op0=ALU.mult, op1=ALU.add), r=[ko, ("h1m", s2)], w=[kx, ko])
                yield from self.ln_tile(lnw, tt, gbc, bbc, self.h_dram, router=True, extra=self.dbg_out.get("h1_%d" % l))
            self.pipeline([(lambda tt=tt: m2_tile(tt)) for tt in range(NT)], 2)
            S.barrier()

    def layer(self, l):
        S = self.S
        if "ssd" in self.stages:
            self.ssd(l)
            self.dbg_dump("ya%d" % l, lambda o: S.dma("sp", o, self.yT_dram[0], r=[("yT0", h) for h in range(8)]))
        if "rwkv" in self.stages:
            self.rwkv(l)
            self.dbg_dump("yb%d" % l, lambda o: S.dma("sp", o, self.yT_dram[1], r=[("yT1", h) for h in range(8)]))
        if "hgrn" in self.stages:
            self.hgrn(l)
            self.dbg_dump("yc%d" % l, lambda o: S.dma("sp", o, self.yT_dram[2], r=[("yT2", h) for h in range(8)]))
        if "merge" in self.stages:
            self.merge(l)
        if "moe" in self.stages:
            self.moe(l, last=(l == self.depth - 1))


_NC_CACHE = {}


def _get_nc():
    if "nc" not in _NC_CACHE:
        _NC_CACHE["nc"] = Builder().build()
    return _NC_CACHE["nc"]


def kernel(**inputs):
    nc = _get_nc()
    x = np.ascontiguousarray(inputs["x"], dtype=np.float32)
    base = {k: np.ascontiguousarray(inputs[k], dtype=np.float32) for k in PARAM_SHAPES}
    in_maps = []
    for c in range(8):
        m = dict(base)
        m["x"] = x[c]
        in_maps.append(m)
    res = run_bass_kernel_spmd(nc, in_maps, core_ids=list(range(8)))
    return np.stack([res.results[c]["out"] for c in range(8)], axis=0)
```

```python
import contextlib
import os
import numpy as np
CUT = int(os.environ.get('CUT', '99'))
HC = int(os.environ.get('HC', '99'))
HL = int(os.environ.get('HL', '99'))
import concourse.bass as bass
import concourse.mybir as mybir
from concourse.bass_utils import run_bass_kernel_spmd

F32 = mybir.dt.float32
BF16 = mybir.dt.bfloat16
AF = mybir.ActivationFunctionType
ALU = mybir.AluOpType
AX = mybir.AxisListType

D = 1024
T = 2048
NT = T // 128
KT = D // 128
DEPTH = 2
NE = 16
DEXP = 512
N_IN = 13072
ALPHA = (2 * DEPTH) ** 0.25
LN_EPS = 1e-5
RMS_EPS = 1e-6
GN_EPS = 64e-5
OFF_Z = 0
OFF_XBC = 1024
OFF_DT = 2560
OFF_RWKV = 2576
OFF_HGRN = OFF_RWKV + 3328
OFF_GATES = OFF_HGRN + 4096

PARAM_SHAPES = {
    "ln_in_g": [1024], "ln_in_b": [1024], "w_in": [2, 1024, 13072],
    "ssd_conv_w": [2, 4, 1536], "ssd_conv_b": [2, 1536], "ssd_dt_bias": [2, 16],
    "ssd_a_log": [2, 16], "ssd_d": [2, 16], "ssd_norm_w": [2, 1024],
    "rwkv_mu": [2, 3328], "rwkv_w0": [2, 1024], "rwkv_w2": [2, 64, 1024],
    "rwkv_a0": [2, 1024], "rwkv_a2": [2, 64, 1024], "rwkv_g2": [2, 128, 1024],
    "rwkv_k_k": [2, 1024], "rwkv_k_a": [2, 1024], "rwkv_r_k": [2, 16, 64],
    "rwkv_ln_w": [2, 1024], "rwkv_ln_b": [2, 1024], "hgrn_lb": [2, 1024],
    "hgrn_norm_w": [2, 128], "w_br_ssd": [2, 1024, 1024], "w_br_rwkv": [2, 1024, 1024],
    "w_br_hgrn": [2, 1024, 1024], "w_out": [2, 1024, 1024], "ln1_g": [2, 1024],
    "ln1_b": [2, 1024], "router_w": [1024, 16], "router_bias": [16],
    "exp_w_gate": [2, 16, 1024, 512], "exp_w_up": [2, 16, 1024, 512],
    "exp_w_down": [2, 16, 512, 1024], "ln2_g": [2, 1024], "ln2_b": [2, 1024],
}


class Sched:
    ENG = ["pe", "act", "dve", "pool", "sp"]

    def __init__(self, nc, es, n_dma=32, n_pdma=24):
        self.nc = nc
        self.e = {"pe": nc.tensor, "act": nc.scalar, "dve": nc.vector, "pool": nc.gpsimd, "sp": nc.sync}
        self.sem = {k: es.enter_context(nc.semaphore("sem_" + k)) for k in self.ENG}
        self.cnt = {k: 0 for k in self.ENG}
        self.dsem = [es.enter_context(nc.semaphore("dsem%d" % i)) for i in range(n_dma)]
        self.dtot = [0] * n_dma
        self.drr = 0
        self.psem = [es.enter_context(nc.semaphore("psem%d" % i)) for i in range(n_pdma)]
        self.pused = [False] * n_pdma
        self.pwaiters = [[] for _ in range(n_pdma)]
        self.pclr = [None] * n_pdma
        self.prr = 0
        self.msem = {k: es.enter_context(nc.semaphore("msem_" + k)) for k in self.ENG}
        self.mcnt = {k: 0 for k in self.ENG}
        self.seen = {k: {} for k in self.ENG}
        self.lastw = {}
        self.readers = {}
        self.nwait = 0
        self.qset = set()

    def _semh(self, sk):
        if isinstance(sk, str):
            return self.sem[sk]
        if sk[0] == "m":
            return self.msem[sk[1]]
        return self.dsem[sk[1]] if sk[0] == "d" else self.psem[sk[1]]

    def _wait(self, e, tag):
        sk, val = tag
        if val <= 0 or self.seen[e].get(sk, 0) >= val:
            return
        if not isinstance(sk, str) and sk[0] == "p" and self.pclr[sk[1]] is not None and e != "pool":
            self._wait(e, self.pclr[sk[1]])
        self.e[e].wait_ge(self._semh(sk), val)
        self.seen[e][sk] = val
        self.nwait += 1
        if not isinstance(sk, str) and sk[0] == "p":
            self.pwaiters[sk[1]].append(self._marker(e))

    def _marker(self, e):
        self.e[e].sem_inc(self.msem[e], 1)
        self.mcnt[e] += 1
        return (("m", e), self.mcnt[e])

    def _deps(self, e, r, w):
        for k in r:
            t = self.lastw.get(k)
            if t is not None:
                self._wait(e, t)
        for k in w:
            t = self.lastw.get(k)
            if t is not None and (t[0] != e or e != "pe"):
                self._wait(e, t)
            for sk, val in self.readers.get(k, {}).items():
                if sk != e or e != "pe":
                    self._wait(e, (sk, val))

    def _record(self, tag, r, w):
        for k in r:
            d = self.readers.setdefault(k, {})
            if d.get(tag[0], 0) < tag[1]:
                d[tag[0]] = tag[1]
        for k in w:
            self.lastw[k] = tag
            self.readers[k] = {}

    def _exp(self, keys):
        out = []
        for k in keys:
            if k in self.qset:
                out.extend((k, q) for q in range(4))
            else:
                out.append(k)
        return out

    def op(self, e, fn, r=(), w=()):
        r, w = self._exp(r), self._exp(w)
        self._deps(e, r, w)
        ins = fn()
        self.cnt[e] += 1
        ins.then_inc(self.sem[e], 1)
        if os.environ.get("OPLOG"):
            self.oplog = getattr(self, "oplog", {})
            self.oplog[(e, self.cnt[e])] = fn.__code__.co_firstlineno
        self._record((e, self.cnt[e]), r, w)

    def _dma_sw(self, out, in_, r, w):
        q = "pool"
        self._deps(q, r, w)
        i = self.prr
        self.prr = (self.prr + 1) % len(self.psem)
        sk = ("p", i)
        if self.pused[i]:
            self._wait(q, (sk, 16))
            for e in self.ENG:
                if e != q:
                    self._wait(e, (sk, 16))
            for tg in self.pwaiters[i]:
                if tg[0][1] != q:
                    self._wait(q, tg)
            self.e[q].sem_clear(self.psem[i])
            tclr = self._marker(q)
            self.pclr[i] = tclr
            for k, t in list(self.lastw.items()):
                if t[0] == sk:
                    self.lastw[k] = tclr
            for k, d in self.readers.items():
                if sk in d:
                    d.pop(sk)
                    d[tclr[0]] = tclr[1]
            for e in self.ENG:
                self.seen[e].pop(sk, None)
            self.pwaiters[i] = []
        ins = self.e[q].dma_start(out=out, in_=in_)
        ins.then_inc(self.psem[i], 16)
        self.pused[i] = True
        self._record((sk, 16), r, w)

    def dma(self, q, out, in_, r=(), w=()):
        r, w = self._exp(r), self._exp(w)
        if q == "pool" and os.environ.get("PSEM_CLEAR"):
            return self._dma_sw(out, in_, r, w)
        self._deps(q, r, w)
        i = self.drr
        self.drr = (self.drr + 1) % len(self.dsem)
        self._wait(q, (("d", i), self.dtot[i]))
        with self.nc.allow_non_contiguous_dma(reason="small per-feature parameter columns"):
            ins = self.e[q].dma_start(out=out, in_=in_)
        self.dtot[i] += 16
        ins.then_inc(self.dsem[i], 16)
        self._record((("d", i), self.dtot[i]), r, w)

    def barrier(self):
        for e in self.ENG:
            for o in self.ENG:
                if o != e:
                    self._wait(e, (o, self.cnt[o]))
            for i in range(len(self.dsem)):
                self._wait(e, (("d", i), self.dtot[i]))
            for i in range(len(self.psem)):
                if self.pused[i]:
                    self._wait(e, (("p", i), 16))

    def finish(self):
        for i in range(len(self.dsem)):
            self._wait("sp", (("d", i), self.dtot[i]))
        for i in range(len(self.psem)):
            if self.pused[i]:
                self._wait("sp", (("p", i), 16))
        for o in self.ENG:
            if o != "sp":
                self._wait("sp", (o, self.cnt[o]))


class Builder:
    def __init__(self, debug=None, stages=("pre", "hgrn", "ssd", "rwkv", "merge", "moe"), depth=DEPTH, pre_router=False):
        self.pre_router = pre_router
        self.debug = debug or {}
        self.stages = stages
        self.depth = depth
        self.nc = bass.Bass("TRN2", target_bir_lowering=False)
        nc = self.nc
        self.x = nc.dram_tensor("x", [T, D], F32, kind="ExternalInput").ap()
        self.P = {k: nc.dram_tensor(k, s, F32, kind="ExternalInput").ap() for k, s in PARAM_SHAPES.items()}
        self.out = nc.dram_tensor("out", [T, D], F32, kind="ExternalOutput").ap()
        self.h_dram = nc.dram_tensor("h_scr", [T, D], F32, kind="Internal").ap()
        self.yT_dram = [nc.dram_tensor("yT_scr%d" % i, [D, T], BF16, kind="Internal").ap() for i in range(3)]
        self.dbg_out = {}
        for name, (shape, dt) in self.debug.items():
            self.dbg_out[name] = nc.dram_tensor("dbg_" + name, shape, dt, kind="ExternalOutput").ap()
        self.uid = 0

    def sb(self, es, name, shape, dt):
        self.uid += 1
        return es.enter_context(self.nc.sbuf_tensor("%s_%d" % (name, self.uid), shape, dt))

    def psum(self):
        i = self.ps_rr
        self.ps_rr = (self.ps_rr + 1) % 8
        return self.ps[i], ("ps", i)

    def build(self):
        nc = self.nc
        with contextlib.ExitStack() as es:
            self.S = Sched(nc, es)
            S = self.S
            self.ps = [es.enter_context(nc.psum_tensor("psb%d" % i, [128, 512], F32)) for i in range(8)]
            self.ps_rr = 0
            self.ident32 = self.sb(es, "ident32", [128, 128], F32)
            self.identbf = self.sb(es, "identbf", [128, 128], BF16)
            self.zeros = self.sb(es, "zeros", [128, 128], F32)
            self.ones = self.sb(es, "ones", [128, 128], F32)
            self.onesbf = self.sb(es, "onesbf", [128, 128], BF16)
            self.epsc = self.sb(es, "epsc", [128, 4], F32)
            S.op("pool", lambda: nc.gpsimd.memset(self.zeros[:], 0.0), w=["zeros"])
            S.op("pool", lambda: nc.gpsimd.memset(self.ones[:], 1.0), w=["ones"])
            S.op("pool", lambda: nc.gpsimd.memset(self.onesbf[:], 1.0), w=["onesbf"])
            S.op("pool", lambda: nc.gpsimd.memset(self.epsc[:, 0:1], LN_EPS), w=["epsc"])
            S.op("pool", lambda: nc.gpsimd.memset(self.epsc[:, 1:2], RMS_EPS), w=["epsc"])
            S.op("pool", lambda: nc.gpsimd.memset(self.epsc[:, 2:3], GN_EPS), w=["epsc"])
            S.op("pool", lambda: nc.gpsimd.memset(self.epsc[:, 3:4], 1.0), w=["epsc"])
            S.op("pool", lambda: nc.gpsimd.affine_select(
                out=self.ident32[:], in_=self.zeros[:], pattern=[[1, 128]], compare_op=ALU.not_equal,
                fill=1.0, base=0, channel_multiplier=-1), r=["zeros"], w=["ident32"])
            S.op("pool", lambda: nc.gpsimd.tensor_copy(out=self.identbf[:], in_=self.ident32[:]),
                 r=["ident32"], w=["identbf"])
            self.hT = self.sb(es, "hT", [128, KT, T], BF16)
            self.gates = self.sb(es, "gates", [128, NT, NE], F32)
            self.logits = self.sb(es, "logits", [128, NT, NE], F32)
            self.rw32 = self.sb(es, "rw32", [128, KT, NE], F32)
            self.rbias = self.sb(es, "rbias", [128, NE], F32)
            S.dma("sp", self.rw32[:], self.P["router_w"].rearrange("(kt p) e -> p kt e", p=128), w=["rw32"])
            S.dma("sp", self.rbias[:], self.P["router_bias"].partition_broadcast(128), w=["rbias"])

            with contextlib.ExitStack() as st:
                gbc = self.sb(st, "gbc", [128, D], F32)
                bbc = self.sb(st, "bbc", [128, D], F32)
                S.dma("sp", gbc[:], self.P["ln_in_g"].partition_broadcast(128), w=["gbc"])
                S.dma("sp", bbc[:], self.P["ln_in_b"].partition_broadcast(128), w=["bbc"])
                lnw = self.ln_alloc(st)
                def pre_tile(tt):
                    xin, kx = self.ln_xin(lnw, tt)
                    S.dma("sp", xin[:], self.x[tt * 128:(tt + 1) * 128, :], w=[kx])
                    yield from self.ln_tile(lnw, tt, gbc, bbc, self.h_dram, router=self.pre_router, extra=self.dbg_out.get("h0"))
                self.pipeline([(lambda tt=tt: pre_tile(tt)) for tt in range(NT)], 2)
                S.barrier()
            self.dbg_dump("hT", lambda o: S.dma("sp", o, self.hT[:], r=[("hT", t) for t in range(NT)]))
            self.dbg_dump("logits", lambda o: S.dma("sp", o, self.logits[:], r=["logits"]))

            for l in range(self.depth):
                self.layer(l)
            S.finish()
        return nc

    def dbg_dump(self, name, fn):
        if name in self.dbg_out:
            fn(self.dbg_out[name])

    @staticmethod
    def pipeline(factories, skew):
        gens, nxt, rnd = [], 0, 0
        while nxt < len(factories) or gens:
            if nxt < len(factories) and rnd % skew == 0:
                gens.append(factories[nxt]())
                nxt += 1
            for g_ in list(gens):
                try:
                    next(g_)
                except StopIteration:
                    gens.remove(g_)
            rnd += 1

    def ln_alloc(self, st):
        w = {}
        w["xin"] = [self.sb(st, "xin", [128, D], F32) for _ in range(2)]
        w["hh"] = [self.sb(st, "hh", [128, D], F32) for _ in range(2)]
        w["bst"] = [self.sb(st, "bst", [128, 2, 6], F32) for _ in range(2)]
        w["mv"] = [self.sb(st, "mv", [128, 4], F32) for _ in range(2)]
        w["h32"] = [self.sb(st, "h32", [128, KT, 128], F32) for _ in range(2)]
        w["id"] = self.uid
        return w

    def ln_xin(self, w, tt):
        return w["xin"][tt % 2], ("xin", w["id"], tt % 2)

    def ln_tile(self, w, tt, gbc, bbc, dst_dram, router, extra=None):
        nc, S = self.nc, self.S
        s = tt % 2
        wid = w["id"]
        xin, kx = w["xin"][s], ("xin", wid, s)
        hh, kh = w["hh"][s], ("hh", wid, s)
        bst, kb = w["bst"][s], ("bst", wid, s)
        mv, km = w["mv"][s], ("mv", wid, s)
        h32, k32 = w["h32"][s], ("h32", wid, s)
        for c in range(2):
            S.op("dve", lambda c=c: nc.vector.bn_stats(out=bst[:, c, :], in_=xin[:, c * 512:(c + 1) * 512]),
                 r=[kx], w=[kb])
        S.op("dve", lambda: nc.vector.bn_aggr(out=mv[:, 0:2], in_=bst[:].rearrange("p a b -> p (a b)")),
             r=[kb], w=[km])
        S.op("act", lambda: nc.scalar.activation(out=mv[:, 2:3], in_=mv[:, 1:2], func=AF.Sqrt,
                                                 bias=self.epsc[:, 0:1], scale=1.0), r=[km, "epsc"], w=[km])
        S.op("dve", lambda: nc.vector.reciprocal(out=mv[:, 3:4], in_=mv[:, 2:3]), r=[km], w=[km])
        S.op("dve", lambda: nc.vector.tensor_scalar(out=xin[:], in0=xin[:], scalar1=mv[:, 0:1], scalar2=mv[:, 3:4],
                                                    op0=ALU.subtract, op1=ALU.mult), r=[kx, km], w=[kx])
        yield
        S.op("pool", lambda: nc.gpsimd.tensor_tensor(out=hh[:], in0=xin[:], in1=gbc[:], op=ALU.mult),
             r=[kx, "gbc"], w=[kh])
        S.op("pool", lambda: nc.gpsimd.tensor_tensor(out=hh[:], in0=hh[:], in1=bbc[:], op=ALU.add),
             r=[kh, "bbc"], w=[kh])
        S.dma("sp", dst_dram[tt * 128:(tt + 1) * 128, :], hh[:], r=[kh], w=[("hd", tt)])
        if extra is not None:
            S.dma("sp", extra[tt * 128:(tt + 1) * 128, :], hh[:], r=[kh], w=[("hdx", tt)])
        yield
        for half in range(2):
            pb, kp = self.psum()
            for j in range(4):
                kt = half * 4 + j
                S.op("pe", lambda j=j, kt=kt: nc.tensor.transpose(
                    out=pb[:, j * 128:(j + 1) * 128], in_=hh[:, kt * 128:(kt + 1) * 128], identity=self.ident32[:]),
                    r=[kh, "ident32"], w=[kp])
            if os.environ.get("EVAC", "act") == "act":
                S.op("act", lambda half=half, pb=pb: nc.scalar.activation(
                    out=self.hT[:, half * 4:(half + 1) * 4, tt * 128:(tt + 1) * 128],
                    in_=pb[:].rearrange("p (a b) -> p a b", a=4), func=AF.Copy), r=[kp], w=[("hT", tt), kp])
            else:
                S.op("dve", lambda half=half, pb=pb: nc.vector.tensor_copy(
                    out=self.hT[:, half * 4:(half + 1) * 4, tt * 128:(tt + 1) * 128],
                    in_=pb[:].rearrange("p (a b) -> p a b", a=4)), r=[kp], w=[("hT", tt), kp])
            if router:
                S.op("dve", lambda half=half, pb=pb: nc.vector.tensor_copy(
                    out=h32[:, half * 4:(half + 1) * 4, :], in_=pb[:].rearrange("p (a b) -> p a b", a=4)),
                    r=[kp], w=[k32, kp])
        yield
        if router:
            pb, kp = self.psum()
            for kt in range(KT):
                S.op("pe", lambda kt=kt: nc.tensor.matmul(pb[:, 0:NE], lhsT=h32[:, kt, :], rhs=self.rw32[:, kt, :],
                                                          start=(kt == 0), stop=(kt == KT - 1)),
                     r=[k32, "rw32"], w=[kp])
            S.op("dve", lambda: nc.vector.tensor_copy(out=self.logits[:, tt, :], in_=pb[:, 0:NE]),
                 r=[kp], w=["logits"])

    def router(self, st):
        nc, S = self.nc, self.S
        V = nc.vector
        L = self.logits
        t1 = self.sb(st, "rt1", [128, NT, NE], F32)
        probs = self.sb(st, "probs", [128, NT, NE], F32)
        sel = self.sb(st, "sel", [128, NT, NE], F32)
        p6 = self.sb(st, "p6", [128, NT, 4, 6], F32)
        gs = self.sb(st, "gs", [128, NT, 4], F32)
        gm = self.sb(st, "gm", [128, NT, 4], F32)
        gt = self.sb(st, "gt", [128, NT, 4], F32)
        red = self.sb(st, "red", [128, NT], F32)
        red2 = self.sb(st, "red2", [128, NT], F32)
        msk = self.sb(st, "msk", [128, NT, NE], F32)
        eq = self.sb(st, "eq", [128, NT, NE], F32)
        BIG = 1.0e9

        def bc(a):
            return a[:].unsqueeze(2).to_broadcast([128, NT, NE])

        S.op("dve", lambda: V.tensor_reduce(out=red[:], in_=L[:], axis=AX.X, op=ALU.max), r=["logits"], w=["red"])
        S.op("dve", lambda: V.tensor_tensor(out=t1[:], in0=L[:], in1=bc(red), op=ALU.subtract),
             r=["logits", "red"], w=["rt1"])
        S.op("act", lambda: nc.scalar.activation(out=t1[:], in_=t1[:], func=AF.Exp), r=["rt1"], w=["rt1"])
        S.op("dve", lambda: V.tensor_reduce(out=red[:], in_=t1[:], axis=AX.X, op=ALU.add), r=["rt1"], w=["red"])
        S.op("dve", lambda: V.reciprocal(out=red[:], in_=red[:]), r=["red"], w=["red"])
        S.op("dve", lambda: V.tensor_tensor(out=probs[:], in0=t1[:], in1=bc(red), op=ALU.mult),
             r=["rt1", "red"], w=["probs"])
        S.op("dve", lambda: V.tensor_tensor(out=sel[:], in0=probs[:],
                                            in1=self.rbias[:].unsqueeze(1).to_broadcast([128, NT, NE]), op=ALU.add),
             r=["probs", "rbias"], w=["sel"])
        s4 = sel[:].rearrange("p t (g e) -> p t g e", g=4)
        S.op("dve", lambda: V.tensor_tensor(out=p6[:, :, :, 0:3], in0=s4[:, :, :, 0:3], in1=s4[:, :, :, 1:4],
                                            op=ALU.add), r=["sel"], w=["p6"])
        S.op("dve", lambda: V.tensor_tensor(out=p6[:, :, :, 3:5], in0=s4[:, :, :, 0:2], in1=s4[:, :, :, 2:4],
                                            op=ALU.add), r=["sel"], w=["p6"])
        S.op("dve", lambda: V.tensor_tensor(out=p6[:, :, :, 5:6], in0=s4[:, :, :, 0:1], in1=s4[:, :, :, 3:4],
                                            op=ALU.add), r=["sel"], w=["p6"])
        S.op("dve", lambda: V.tensor_reduce(out=gs[:], in_=p6[:], axis=AX.X, op=ALU.max), r=["p6"], w=["gs"])
        S.op("dve", lambda: V.tensor_reduce(out=red[:], in_=gs[:], axis=AX.X, op=ALU.max), r=["gs"], w=["red"])
        S.op("dve", lambda: V.tensor_tensor(out=gm[:], in0=gs[:], in1=red[:].unsqueeze(2).to_broadcast([128, NT, 4]),
                                            op=ALU.is_ge), r=["gs", "red"], w=["gm"])
        S.op("dve", lambda: V.tensor_scalar(out=gt[:], in0=gm[:], scalar1=BIG, scalar2=-BIG, op0=ALU.mult,
                                            op1=ALU.add), r=["gm"], w=["gt"])
        m4 = msk[:].rearrange("p t (g e) -> p t g e", g=4)
        S.op("dve", lambda: V.tensor_tensor(out=m4, in0=s4, in1=gm[:].unsqueeze(3).to_broadcast([128, NT, 4, 4]),
                                            op=ALU.mult), r=["sel", "gm"], w=["msk"])
        S.op("dve", lambda: V.tensor_tensor(out=m4, in0=m4, in1=gt[:].unsqueeze(3).to_broadcast([128, NT, 4, 4]),
                                            op=ALU.add), r=["msk", "gt"], w=["msk"])
        S.op("dve", lambda: V.tensor_reduce(out=red[:], in_=msk[:], axis=AX.X, op=ALU.max), r=["msk"], w=["red"])
        S.op("dve", lambda: V.tensor_tensor(out=eq[:], in0=msk[:], in1=bc(red), op=ALU.is_equal),
             r=["msk", "red"], w=["eq"])
        S.op("dve", lambda: V.scalar_tensor_tensor(out=eq[:], in0=eq[:], scalar=-BIG, in1=msk[:], op0=ALU.mult,
                                                   op1=ALU.add), r=["eq", "msk"], w=["eq"])
        S.op("dve", lambda: V.tensor_reduce(out=red2[:], in_=eq[:], axis=AX.X, op=ALU.max), r=["eq"], w=["red2"])
        S.op("dve", lambda: V.tensor_tensor(out=eq[:], in0=msk[:], in1=bc(red2), op=ALU.is_ge),
             r=["msk", "red2"], w=["eq"])
        S.op("dve", lambda: V.tensor_tensor(out=eq[:], in0=eq[:], in1=probs[:], op=ALU.mult),
             r=["eq", "probs"], w=["eq"])
        S.op("dve", lambda: V.tensor_reduce(out=red[:], in_=eq[:], axis=AX.X, op=ALU.add), r=["eq"], w=["red"])
        S.op("dve", lambda: V.reciprocal(out=red[:], in_=red[:]), r=["red"], w=["red"])
        S.op("dve", lambda: V.tensor_tensor(out=self.gates[:], in0=eq[:], in1=bc(red), op=ALU.mult),
             r=["eq", "red"], w=["gates"])

    def moe(self, l, last):
        nc, S = self.nc, self.S
        wg_d, wu_d, wd_d = self.P["exp_w_gate"], self.P["exp_w_up"], self.P["exp_w_down"]
        with contextlib.ExitStack() as st:
            self.router(st)
            self.dbg_dump("gates%d" % l, lambda o: S.dma("sp", o, self.gates[:], r=["gates"]))
            acc = self.sb(st, "acc", [128, NT, D], F32)
            wg = [self.sb(st, "wg", [128, KT, DEXP], BF16) for _ in range(2)]
            wu = [self.sb(st, "wu", [128, KT, DEXP], BF16) for _ in range(2)]
            wd = [self.sb(st, "wd", [128, 4, D], BF16) for _ in range(2)]
            hg = [self.sb(st, "hg", [128, 4, 512], BF16) for _ in range(2)]
            sg = [self.sb(st, "sg", [128, 512], BF16) for _ in range(2)]
            gbc = self.sb(st, "gbc2", [128, D], F32)
            bbc = self.sb(st, "bbc2", [128, D], F32)
            S.dma("sp", gbc[:], self.P["ln2_g"][l].partition_broadcast(128), w=["gbc"])
            S.dma("sp", bbc[:], self.P["ln2_b"][l].partition_broadcast(128), w=["bbc"])

            def load_w(e):
                b = e % 2
                S.dma("pool", wg[b][:], wg_d[l, e].rearrange("(kt p) n -> p kt n", p=128), w=[("wg", b)])
                S.dma("pool", wu[b][:], wu_d[l, e].rearrange("(kt p) n -> p kt n", p=128), w=[("wu", b)])
                S.dma("pool", wd[b][:], wd_d[l, e].rearrange("(kt p) n -> p kt n", p=128), w=[("wd", b)])

            items = [(e, q) for e in range(int(os.environ.get('ME', NE))) for q in range(4)]
            sgi = [0]

            def G(i):
                e, q = items[i]
                b = e % 2
                hb = i % 2
                for dt_ in range(4):
                    pa, ka = self.psum()
                    pu, ku = self.psum()
                    for kt in range(KT):
                        S.op("pe", lambda kt=kt, pa=pa: nc.tensor.matmul(
                            pa[:], lhsT=wg[b][:, kt, dt_ * 128:(dt_ + 1) * 128], rhs=self.hT[:, kt, q * 512:(q + 1) * 512],
                            start=(kt == 0), stop=(kt == KT - 1)),
                            r=[("wg", b)] + [("hT", q * 4 + j) for j in range(4)], w=[ka])
                    for kt in range(KT):
                        S.op("pe", lambda kt=kt, pu=pu: nc.tensor.matmul(
                            pu[:], lhsT=wu[b][:, kt, dt_ * 128:(dt_ + 1) * 128], rhs=self.hT[:, kt, q * 512:(q + 1) * 512],
                            start=(kt == 0), stop=(kt == KT - 1)),
                            r=[("wu", b)] + [("hT", q * 4 + j) for j in range(4)], w=[ku])
                    si = sgi[0] % 2
                    sgi[0] += 1
                    S.op("act", lambda pa=pa, si=si: nc.scalar.activation(out=sg[si][:], in_=pa[:], func=AF.Silu),
                         r=[ka], w=[("sg", si)])
                    S.op("dve", lambda pu=pu, si=si: nc.vector.tensor_tensor(
                        out=hg[hb][:, dt_, :], in0=pu[:], in1=sg[si][:], op=ALU.mult),
                        r=[ku, ("sg", si)], w=[("hg", hb)])

            def Dn(i):
                e, q = items[i]
                b = e % 2
                hb = i % 2
                for j in range(4):
                    tt = q * 4 + j
                    for half in range(2):
                        pc, kc = self.psum()
                        for dt_ in range(4):
                            S.op("pe", lambda dt_=dt_, pc=pc: nc.tensor.matmul(
                                pc[:], lhsT=hg[hb][:, dt_, j * 128:(j + 1) * 128],
                                rhs=wd[b][:, dt_, half * 512:(half + 1) * 512], start=(dt_ == 0), stop=(dt_ == 3)),
                                r=[("hg", hb), ("wd", b)], w=[kc])
                        dst = acc[:, tt, half * 512:(half + 1) * 512]
                        if e == 0:
                            S.op("dve", lambda pc=pc, dst=dst: nc.vector.tensor_scalar(
                                out=dst, in0=pc[:], scalar1=self.gates[:, tt, e:e + 1], scalar2=None, op0=ALU.mult),
                                r=[kc, "gates"], w=[("acc", tt)])
                        else:
                            S.op("dve", lambda pc=pc, dst=dst: nc.vector.scalar_tensor_tensor(
                                out=dst, in0=pc[:], scalar=self.gates[:, tt, e:e + 1], in1=dst, op0=ALU.mult,
                                op1=ALU.add), r=[kc, "gates", ("acc", tt)], w=[("acc", tt)])

            load_w(0)
            for i in range(len(items)):
                e, q = items[i]
                G(i)
                if i >= 1:
                    Dn(i - 1)
                if q == 0 and e + 1 < int(os.environ.get('ME', NE)):
                    load_w(e + 1)
            Dn(len(items) - 1)
            self.dbg_dump("moe%d" % l, lambda o: S.dma("sp", o.rearrange("(t p) d -> p t d", p=128), acc[:],
                                                       r=[("acc", t) for t in range(NT)]))
            lnw = self.ln_alloc(st)
            h1 = [self.sb(st, "h1t", [128, D], F32) for _ in range(2)]
            dst = self.out if last else self.h_dram
            def ln2_tile(tt):
                s = tt % 2
                S.dma("sp", h1[s][:], self.h_dram[tt * 128:(tt + 1) * 128, :], r=[("hd", tt)], w=[("h1t", s)])
                xin, kx = self.ln_xin(lnw, tt)
                S.op("dve", lambda s=s, xin=xin: nc.vector.scalar_tensor_tensor(
                    out=xin[:], in0=h1[s][:], scalar=ALPHA, in1=acc[:, tt, :], op0=ALU.mult, op1=ALU.add),
                    r=[("h1t", s), ("acc", tt)], w=[kx])
                yield from self.ln_tile(lnw, tt, gbc, bbc, dst, router=False)
            self.pipeline([(lambda tt=tt: ln2_tile(tt)) for tt in range(NT)], 2)
            S.barrier()


    def hgrn(self, l):
        nc, S = self.nc, self.S
        V, A, G, PE = nc.vector, nc.scalar, nc.gpsimd, nc.tensor
        Wl = self.P["w_in"][l]
        ydst = self.yT_dram[2]
        with contextlib.ExitStack() as st:
            mask2 = self.sb(st, "mask2", [128, 128], F32)
            rm = self.sb(st, "rm", [128, T], F32)
            nw = self.sb(st, "nw", [128, 1], F32)
            lbt = self.sb(st, "lbt", [128, 8, 2], F32)
            lbv = self.sb(st, "lbv", [128, 8], F32)
            oml = self.sb(st, "oml", [128, 8], F32)
            S.op("pool", lambda: G.affine_select(out=mask2[:], in_=self.ones[:], pattern=[[1, 128]],
                                                 compare_op=ALU.is_ge, fill=0.0, base=0, channel_multiplier=-1),
                 r=["ones"], w=["mask2"])
            S.op("pool", lambda: G.memset(mask2[0:64, 64:128], 0.0), w=["mask2"])
            S.op("pool", lambda: G.memset(rm[:], 1.0), w=["rm"])
            S.op("pool", lambda: G.memset(rm[:].rearrange("p (c j) -> p c j", j=64)[:, :, 0:1], 0.0), w=["rm"])
            S.dma("sp", nw[:], self.P["hgrn_norm_w"][l].rearrange("(p o) -> p o", o=1), w=["nw"])
            if l == 0:
                S.op("pool", lambda: G.memset(lbv[:], 0.0), w=["lbv"])
                S.op("pool", lambda: G.memset(oml[:], 1.0), w=["oml"])
            else:
                for j in range(2):
                    S.dma("sp", lbt[:, :, j:j + 1],
                          self.P["hgrn_lb"][j].rearrange("(h p o) -> p h o", p=128, o=1), w=["lbt"])
                S.op("dve", lambda: V.tensor_tensor(out=lbv[:], in0=lbt[:, :, 1], in1=lbt[:, :, 0], op=ALU.subtract),
                     r=["lbt"], w=["lbv"])
                S.op("act", lambda: A.activation(out=lbv[:], in_=lbv[:], func=AF.Sigmoid), r=["lbv"], w=["lbv"])
                S.op("dve", lambda: V.tensor_scalar(out=oml[:], in0=lbv[:], scalar1=-1.0, scalar2=1.0, op0=ALU.mult,
                                                    op1=ALU.add), r=["lbv"], w=["oml"])
            w4 = [self.sb(st, "w4", [128, 4, KT, 128], BF16) for _ in range(2)]
            qs = self.sb(st, "qs", [128, T], F32)
            fs = self.sb(st, "fs", [128, T], F32)
            lf = self.sb(st, "lf", [128, T], F32)
            bc = self.sb(st, "bc", [128, T], F32)
            enb = self.sb(st, "enb", [128, T], F32)
            def two(name, shape, dt):
                return [self.sb(st, name, shape, dt) for _ in range(2)]
            qbL, kbL, gsL, ytL = two("qb", [128, T], BF16), two("kb", [128, T], BF16), two("gs", [128, T], BF16), two("yt", [128, T], BF16)
            vL, kbtL, kbtBL = two("v", [128, NT, 128], BF16), two("kbt", [128, NT, 128], BF16), two("kbtB", [128, NT, 128], BF16)
            ebL = two("ebh", [128, T], F32)
            S32L = two("S32", [128, 128], F32)
            SbfL = [[self.sb(st, "Sbf", [128, 128], BF16) for _ in range(4)] for _ in range(2)]
            attmL = [two("attm", [128, 128], BF16) for _ in range(2)]
            osbL = [two("osb", [128, 128], F32) for _ in range(2)]
            osqL = [two("osq", [128, 128], BF16) for _ in range(2)]
            sdL = [two("sd", [128, 128], F32) for _ in range(2)]
            mAB = self.sb(st, "mAB", [128, 2], F32)
            S.op("pool", lambda: G.memset(mAB[0:64, 0:1], 1.0), w=["mAB"])
            S.op("pool", lambda: G.memset(mAB[64:128, 0:1], 0.0), w=["mAB"])
            S.op("pool", lambda: G.memset(mAB[0:64, 1:2], 0.0), w=["mAB"])
            S.op("pool", lambda: G.memset(mAB[64:128, 1:2], 1.0), w=["mAB"])
            hTk = [("hT", t) for t in range(NT)]

            def load_w(h):
                b = h % 2
                for j in range(4):
                    c0 = OFF_HGRN + j * 1024 + h * 128
                    S.dma("pool", w4[b][:, j], Wl[:, c0:c0 + 128].rearrange("(kt p) n -> p kt n", p=128),
                          w=[("w4", b)])

            def prep(h):
                hb = b = h % 2
                qb, kb, gs, v, kbt, kbtB, eb = qbL[hb], kbL[hb], gsL[hb], vL[hb], kbtL[hb], kbtBL[hb], ebL[hb]
                K = lambda n: (n, hb)
                if h + 1 < 8:
                    load_w(h + 1)
                for (j, func, dst, kd) in ((0, AF.Silu, qs, "qs"), (1, AF.Sigmoid, fs, "fs"), (3, AF.Sigmoid, gs, K("gs"))):
                    for tq in range(4):
                        pb, kp = self.psum()
                        for kt in range(KT):
                            S.op("pe", lambda kt=kt, pb=pb, j=j, tq=tq: PE.matmul(
                                pb[:], lhsT=w4[b][:, j, kt, :], rhs=self.hT[:, kt, tq * 512:(tq + 1) * 512],
                                start=(kt == 0), stop=(kt == KT - 1)), r=[("w4", b)] + hTk[tq * 4:tq * 4 + 4], w=[kp])
                        S.op("act", lambda pb=pb, dst=dst, func=func, tq=tq: A.activation(
                            out=dst[:, tq * 512:(tq + 1) * 512], in_=pb[:], func=func), r=[kp],
                            w=[((kd, tq) if kd in S.qset else kd), kp])
                for t4 in range(4):
                    pb, kp = self.psum()
                    for j4 in range(4):
                        tt = t4 * 4 + j4
                        for kt in range(KT):
                            S.op("pe", lambda kt=kt, pb=pb, j4=j4, tt=tt: PE.matmul(
                                pb[:, j4 * 128:(j4 + 1) * 128], lhsT=self.hT[:, kt, tt * 128:(tt + 1) * 128],
                                rhs=w4[b][:, 2, kt, :], start=(kt == 0), stop=(kt == KT - 1)),
                                r=[("w4", b), ("hT", tt)], w=[kp])
                    S.op("dve", lambda pb=pb, t4=t4: V.tensor_copy(
                        out=v[:, t4 * 4:(t4 + 1) * 4, :], in_=pb[:].rearrange("p (a b) -> p a b", a=4)),
                        r=[kp], w=[K("v"), kp])
                def Q(eng, fn, r=(), w=()):
                    for q in range(4):
                        qs_ = slice(q * 512, (q + 1) * 512)
                        rr = [(k, q) if k in S.qset else k for k in r]
                        ww = [(k, q) if k in S.qset else k for k in w]
                        S.op(eng, lambda: fn(qs_), r=rr, w=ww)
                Q("dve", lambda qs_: V.tensor_scalar(out=fs[:, qs_], in0=fs[:, qs_], scalar1=oml[:, h:h + 1], scalar2=lbv[:, h:h + 1],
                                                     op0=ALU.mult, op1=ALU.add), r=["fs", "oml", "lbv"], w=["fs"])
                Q("act", lambda qs_: A.activation(out=lf[:, qs_], in_=fs[:, qs_], func=AF.Ln), r=["fs"], w=["lf"])
                Q("dve", lambda qs_: V.tensor_tensor_scan(out=bc[:, qs_], data0=rm[:, qs_], data1=lf[:, qs_], initial=0.0,
                                                          op0=ALU.mult, op1=ALU.add), r=["rm", "lf"], w=["bc"])
                Q("act", lambda qs_: A.activation(out=eb[:, qs_], in_=bc[:, qs_], func=AF.Exp), r=["bc"], w=[K("eb")])
                Q("act", lambda qs_: A.activation(out=enb[:, qs_], in_=bc[:, qs_], func=AF.Exp, scale=-1.0), r=["bc"], w=["enb"])
                Q("dve", lambda qs_: V.tensor_scalar(out=fs[:, qs_], in0=fs[:, qs_], scalar1=-1.0, scalar2=1.0, op0=ALU.mult,
                                                     op1=ALU.add), r=["fs", "lf"], w=["fs"])
                Q("pool", lambda qs_: G.tensor_tensor(out=qb[:, qs_], in0=qs[:, qs_], in1=eb[:, qs_], op=ALU.mult),
                  r=["qs", K("eb")], w=[K("qb")])
                Q("dve", lambda qs_: V.tensor_tensor(out=kb[:, qs_], in0=fs[:, qs_], in1=enb[:, qs_], op=ALU.mult),
                  r=["fs", "enb"], w=[K("kb")])
                for t4 in range(4):
                    pb, kp = self.psum()
                    pbv = pb[:].bitcast(BF16)
                    for j4 in range(4):
                        tt = t4 * 4 + j4
                        S.op("pe", lambda pbv=pbv, j4=j4, tt=tt: PE.transpose(
                            out=pbv[:, j4 * 128:(j4 + 1) * 128], in_=kb[:, tt * 128:(tt + 1) * 128],
                            identity=self.identbf[:]), r=[(K("kb"), t4), "identbf"], w=[kp])
                    S.op("dve", lambda pbv=pbv, t4=t4: V.tensor_scalar(
                        out=kbt[:, t4 * 4:(t4 + 1) * 4, :], in0=pbv[:, 0:512].rearrange("p (a b) -> p a b", a=4),
                        scalar1=mAB[:, 0:1], scalar2=None, op0=ALU.mult), r=[kp, "mAB"], w=[K("kbt"), kp])
                    S.op("dve", lambda pbv=pbv, t4=t4: V.tensor_scalar(
                        out=kbtB[:, t4 * 4:(t4 + 1) * 4, :], in0=pbv[:, 0:512].rearrange("p (a b) -> p a b", a=4),
                        scalar1=mAB[:, 1:2], scalar2=None, op0=ALU.mult), r=[kp, "mAB"], w=[K("kbtB"), kp])
                S.op("pool", lambda: G.memset(S32L[hb][:], 0.0), w=[K("S32")])
                S.op("pool", lambda: G.memset(SbfL[hb][0][:], 0.0), w=[("Sbf", hb, 0)])

            def tile(h, tt):
                hb = h % 2
                qb, kb, gs, yt, v, kbt, kbtB, eb = qbL[hb], kbL[hb], gsL[hb], ytL[hb], vL[hb], kbtL[hb], kbtBL[hb], ebL[hb]
                S32, Sbf = S32L[hb], SbfL[hb]
                K = lambda n: (n, hb)
                cA, cB = 2 * tt, 2 * tt + 1
                tsl = slice(tt * 128, (tt + 1) * 128)
                i2 = tt % 2
                attm, osb, osq, sd = attmL[hb][i2], osbL[hb][i2], osqL[hb][i2], sdL[hb][i2]
                ka, ko, kq, ks = ("attm", hb, i2), ("osb", hb, i2), ("osq", hb, i2), ("sd", hb, i2)
                pa, kpa = self.psum()
                S.op("pe", lambda: PE.matmul(pa[:, 0:128], lhsT=kb[:, tsl], rhs=qb[:, tsl], start=True, stop=True),
                     r=[K("kb"), K("qb")], w=[kpa])
                S.op("dve", lambda: V.tensor_tensor(out=attm[:], in0=pa[:, 0:128], in1=mask2[:], op=ALU.mult),
                     r=[kpa, "mask2"], w=[ka, kpa])
                yield
                pr, kpr = self.psum()
                S.op("pe", lambda: PE.matmul(pr[:, 0:128], lhsT=kbt[:, tt, :], rhs=v[:, tt, :], start=True, stop=True),
                     r=[K("kbt"), K("v")], w=[kpr])
                S.op("pe", lambda: PE.matmul(pr[:, 128:256], lhsT=kbtB[:, tt, :], rhs=v[:, tt, :], start=True, stop=True),
                     r=[K("kbtB"), K("v")], w=[kpr])
                for (ci, off) in ((cA, 0), (cB, 128)):
                    S.op("dve", lambda off=off: V.tensor_tensor(
                        out=S32[:], in0=pr[:, off:off + 128], in1=S32[:], op=ALU.add), r=[kpr, K("S32")], w=[K("S32"), kpr])
                    S.op("dve", lambda ci=ci: V.tensor_scalar(
                        out=S32[:], in0=S32[:], scalar1=eb[:, ci * 64 + 63:ci * 64 + 64], scalar2=None, op0=ALU.mult),
                        r=[K("S32"), K("eb")], w=[K("S32")])
                    S.op("pool", lambda ci=ci: G.tensor_copy(out=Sbf[(ci + 1) % 4][:], in_=S32[:]),
                         r=[K("S32")], w=[("Sbf", hb, (ci + 1) % 4)])
                    yield
                po, kpo = self.psum()
                S.op("pe", lambda: PE.matmul(po[:, 0:128], lhsT=v[:, tt, :], rhs=attm[:], start=True, stop=False),
                     r=[K("v"), ka], w=[kpo])
                S.op("pe", lambda: PE.matmul(po[:, 0:64], lhsT=Sbf[cA % 4][:], rhs=qb[:, tt * 128:tt * 128 + 64],
                                             start=False, stop=False), r=[("Sbf", hb, cA % 4), K("qb")], w=[kpo])
                S.op("pe", lambda: PE.matmul(po[:, 64:128], lhsT=Sbf[cB % 4][:], rhs=qb[:, tt * 128 + 64:(tt + 1) * 128],
                                             start=False, stop=True), r=[("Sbf", hb, cB % 4), K("qb")], w=[kpo])
                S.op("act", lambda: A.activation(out=osb[:], in_=po[:, 0:128], func=AF.Copy), r=[kpo], w=[ko, kpo])
                S.op("act", lambda: A.activation(out=osq[:], in_=po[:, 0:128], func=AF.Square), r=[kpo], w=[kq, kpo])
                yield
                pss, kps = self.psum()
                S.op("pe", lambda: PE.matmul(pss[:, 0:128], lhsT=self.onesbf[:], rhs=osq[:], start=True, stop=True),
                     r=["onesbf", kq], w=[kps])
                S.op("act", lambda: A.activation(out=sd[:], in_=pss[:, 0:128], func=AF.Sqrt, bias=self.epsc[:, 1:2],
                                                 scale=1.0 / 128.0), r=[kps, "epsc"], w=[ks, kps])
                S.op("dve", lambda: V.reciprocal(out=sd[:], in_=sd[:]), r=[ks], w=[ks])
                S.op("dve", lambda: V.scalar_tensor_tensor(out=osb[:], in0=osb[:], scalar=nw[:, 0:1], in1=sd[:],
                                                           op0=ALU.mult, op1=ALU.mult), r=[ko, ks, "nw"], w=[ko])
                S.op("dve", lambda: V.tensor_tensor(out=yt[:, tsl], in0=osb[:], in1=gs[:, tsl], op=ALU.mult),
                     r=[ko, K("gs")], w=[K("yt")])

            S.qset = {"qs", "fs", "lf", "bc", "enb", ("eb", 0), ("eb", 1), ("qb", 0), ("qb", 1), ("kb", 0), ("kb", 1)}
            load_w(0)
            for hp in range(4):
                prep(2 * hp)
                prep(2 * hp + 1)
                gens, nxt, rnd = [], 0, 0
                while nxt < NT or gens:
                    if nxt < NT and rnd % 2 == 0:
                        gens.append(tile(2 * hp, nxt))
                        gens.append(tile(2 * hp + 1, nxt))
                        nxt += 1
                    for g_ in list(gens):
                        try:
                            next(g_)
                        except StopIteration:
                            gens.remove(g_)
                    rnd += 1
                for hb in range(2):
                    h = 2 * hp + hb
                    S.dma("sp", ydst[h * 128:(h + 1) * 128, :], ytL[hb][:], r=[("yt", hb)], w=[("yT2", h)])
            S.barrier()
            S.qset = set()

    def ssd(self, l):
        nc, S = self.nc, self.S
        V, A, G, PE = nc.vector, nc.scalar, nc.gpsimd, nc.tensor
        Wl = self.P["w_in"][l]
        ydst = self.yT_dram[0]
        NEG = -30000.0
        hTk = [("hT", t) for t in range(NT)]
        with contextlib.ExitStack() as st:
            tri2 = self.sb(st, "tri2", [128, 128], F32)
            same2 = self.sb(st, "same2", [128, 128], F32)
            indA = self.sb(st, "indA", [128, 128], F32)
            indB = self.sb(st, "indB", [128, 128], F32)
            mAB = self.sb(st, "mABs", [128, 2], F32)
            negmask = self.sb(st, "negmask", [128, 8, 128], BF16)
            bd1 = self.sb(st, "bd", [16, 8, 128], F32)
            bd = [bd1, bd1]
            S.op("pool", lambda: G.affine_select(out=tri2[:], in_=self.ones[:], pattern=[[1, 128]], compare_op=ALU.is_ge,
                                                 fill=0.0, base=0, channel_multiplier=-1), r=["ones"], w=["tri2"])
            S.op("pool", lambda: G.memset(tri2[0:64, 64:128], 0.0), w=["tri2"])
            S.op("pool", lambda: G.memset(same2[:], 0.0), w=["same2"])
            S.op("pool", lambda: G.memset(same2[0:64, 0:64], 1.0), w=["same2"])
            S.op("pool", lambda: G.memset(same2[64:128, 64:128], 1.0), w=["same2"])
            S.op("pool", lambda: G.memset(indA[0:64, :], 1.0), w=["indA"])
            S.op("pool", lambda: G.memset(indA[64:128, :], 0.0), w=["indA"])
            S.op("pool", lambda: G.memset(indB[0:64, :], 0.0), w=["indB"])
            S.op("pool", lambda: G.memset(indB[64:128, :], 1.0), w=["indB"])
            S.op("pool", lambda: G.memset(mAB[0:64, 0:1], 1.0), w=["mAB"])
            S.op("pool", lambda: G.memset(mAB[64:128, 0:1], 0.0), w=["mAB"])
            S.op("pool", lambda: G.memset(mAB[0:64, 1:2], 0.0), w=["mAB"])
            S.op("pool", lambda: G.memset(mAB[64:128, 1:2], 1.0), w=["mAB"])
            S.op("pool", lambda: G.memset(negmask[:], 0.0), w=["negmask"])
            S.op("pool", lambda: G.affine_select(out=negmask[:], in_=negmask[:], pattern=[[0, 8], [1, 128]],
                                                 compare_op=ALU.is_ge, fill=NEG, base=0, channel_multiplier=-1),
                 r=["negmask"], w=["negmask"])
            S.op("pool", lambda: G.memset(negmask[0:64, :, 64:128], NEG), w=["negmask"])
            dtb = self.sb(st, "dtb", [128, 16], F32)
            alog = self.sb(st, "alog", [128, 16], F32)
            dsk = self.sb(st, "dsk", [128, 16], F32)
            nwbc = self.sb(st, "nwbc", [128, D], F32)
            S.dma("sp", dtb[:], self.P["ssd_dt_bias"][l].partition_broadcast(128), w=["dtb"])
            S.dma("sp", alog[:], self.P["ssd_a_log"][l].partition_broadcast(128), w=["alog"])
            S.dma("sp", dsk[:], self.P["ssd_d"][l].partition_broadcast(128), w=["dsk"])
            S.dma("sp", nwbc[:], self.P["ssd_norm_w"][l].partition_broadcast(128), w=["nwbc"])
            S.op("act", lambda: A.activation(out=alog[:], in_=alog[:], func=AF.Exp), r=["alog"], w=["alog"])
            S.op("dve", lambda: V.tensor_scalar(out=alog[:], in0=alog[:], scalar1=-1.0, scalar2=None, op0=ALU.mult),
                 r=["alog"], w=["alog"])
            wdt = self.sb(st, "wdt", [128, KT, 16], BF16)
            S.dma("pool", wdt[:], Wl[:, OFF_DT:OFF_DT + 16].rearrange("(kt p) n -> p kt n", p=128), w=["wdt"])
            dt = self.sb(st, "dt", [128, NT, 16], F32)
            da = self.sb(st, "da", [128, NT, 16], F32)
            cum4 = self.sb(st, "cum4", [128, NT, 4, 16], F32)
            eacs = self.sb(st, "eacs", [128, NT, 16], F32)
            eend = self.sb(st, "eend", [128, NT, 16], F32)
            edec = self.sb(st, "edec", [128, NT, 2, 16], F32)
            acsTt = [self.sb(st, "acsTt", [16, 128], F32) for _ in range(2)]
            nacsTt = [self.sb(st, "nacsTt", [16, 128], F32) for _ in range(2)]
            pb, kp = self.psum()
            for tt in range(NT):
                for kt in range(KT):
                    S.op("pe", lambda kt=kt, tt=tt: PE.matmul(pb[:, tt * 16:(tt + 1) * 16], lhsT=self.hT[:, kt, tt * 128:(tt + 1) * 128],
                                                              rhs=wdt[:, kt, :], start=(kt == 0), stop=(kt == KT - 1)),
                         r=["wdt", ("hT", tt)], w=[kp])
            S.op("dve", lambda: V.tensor_tensor(out=dt[:], in0=pb[:, 0:256].rearrange("p (t h) -> p t h", h=16),
                                                in1=dtb[:].unsqueeze(1).to_broadcast([128, NT, 16]), op=ALU.add),
                 r=[kp, "dtb"], w=["dt", kp])
            S.op("act", lambda: A.activation(out=dt[:], in_=dt[:], func=AF.Exp), r=["dt"], w=["dt"])
            S.op("act", lambda: A.activation(out=dt[:], in_=dt[:], func=AF.Ln, bias=self.epsc[:, 3:4], scale=1.0),
                 r=["dt", "epsc"], w=["dt"])
            S.op("dve", lambda: V.tensor_tensor(out=da[:], in0=dt[:], in1=alog[:].unsqueeze(1).to_broadcast([128, NT, 16]),
                                                op=ALU.mult), r=["dt", "alog"], w=["da"])
            for half in range(2):
                pb, kp = self.psum()
                for j in range(8):
                    tt = half * 8 + j
                    for qi, L in enumerate((tri2, same2, indA, indB)):
                        S.op("pe", lambda j=j, qi=qi, L=L, tt=tt, pb=pb: PE.matmul(
                            pb[:, j * 64 + qi * 16:j * 64 + (qi + 1) * 16], lhsT=L[:], rhs=da[:, tt, :], start=True, stop=True),
                            r=["da", "tri2", "same2", "indA", "indB"], w=[kp])
                S.op("dve", lambda pb=pb, half=half: V.tensor_copy(
                    out=cum4[:, half * 8:(half + 1) * 8].rearrange("p t q h -> p (t q h)"), in_=pb[:]),
                    r=[kp], w=["cum4", kp])
            S.op("act", lambda: A.activation(out=eacs[:], in_=cum4[:, :, 0, :], func=AF.Exp), r=["cum4"], w=["eacs"])
            S.op("dve", lambda: V.tensor_tensor(out=eend[:], in0=cum4[:, :, 1, :], in1=cum4[:, :, 0, :], op=ALU.subtract),
                 r=["cum4"], w=["eend"])
            S.op("act", lambda: A.activation(out=eend[:], in_=eend[:], func=AF.Exp), r=["eend"], w=["eend"])
            S.op("act", lambda: A.activation(out=edec[:], in_=cum4[:, :, 2:4, :], func=AF.Exp), r=["cum4"], w=["edec"])
            wx = self.sb(st, "wx", [128, KT, 768], BF16)
            wz = self.sb(st, "wz", [128, KT, 512], BF16)
            cw = self.sb(st, "cw", [128, 6, 4], F32)
            cbi = self.sb(st, "cbi", [128, 6], F32)
            xp1 = self.sb(st, "xp", [128, T + 3], F32)
            xp = [xp1, xp1]
            fTa = [self.sb(st, "fT", [128, T], BF16) for _ in range(4)]
            fT = [fTa[0], fTa[1], fTa[0], fTa[1], fTa[2], fTa[3]]
            fk = [("fT", 0), ("fT", 1), ("fT", 0), ("fT", 1), ("fT", 2), ("fT", 3)]
            cmTA = self.sb(st, "cmTA", [128, T], BF16)
            cmTB = self.sb(st, "cmTB", [128, T], BF16)
            xs = self.sb(st, "xs", [128, NT, 512], BF16)
            xdtt = [self.sb(st, "xdtt", [128, 512], BF16) for _ in range(2)]
            xendt = [self.sb(st, "xendt", [128, 512], BF16) for _ in range(2)]
            bmA = self.sb(st, "bmA", [128, NT, 128], BF16)
            bmB = self.sb(st, "bmB", [128, NT, 128], BF16)
            yTg = self.sb(st, "yTg", [128, 4, T], BF16)
            S32 = self.sb(st, "S32s", [128, 512], F32)
            Sbf = [self.sb(st, "Sbfs", [128, 512], BF16) for _ in range(4)]
            cbs = [self.sb(st, "cbs", [128, 128], BF16) for _ in range(2)]
            Dx1 = self.sb(st, "Dx", [16, 8, 128], F32)
            Dx = [Dx1, Dx1]
            Dxh = self.sb(st, "Dxh", [16, 8, 128], BF16)
            Dxl = self.sb(st, "Dxl", [16, 8, 128], BF16)
            bdbf = self.sb(st, "bdbf", [16, 8, 128], BF16)
            nah = [self.sb(st, "nah", [16, 128], BF16) for _ in range(2)]
            nal = [self.sb(st, "nal", [16, 128], BF16) for _ in range(2)]
            Es = [self.sb(st, "Es", [128, 8, 128], BF16) for _ in range(2)]
            wT = [self.sb(st, "wT", [128, 8, 128], BF16) for _ in range(2)]
            t1 = [self.sb(st, "t1", [128, 512], F32) for _ in range(2)]
            t2 = [self.sb(st, "t2", [128, 512], F32) for _ in range(2)]
            zs = [self.sb(st, "zs", [128, 512], BF16) for _ in range(2)]
            ytm = [self.sb(st, "ytm", [128, 512], BF16) for _ in range(2)]
            ss = [self.sb(st, "ss", [128, 2], F32) for _ in range(2)]
            S.op("pool", lambda: G.memset(xp[0][:, 0:3], 0.0), w=["xp0"])
            S.qset = {("fT", 0), ("fT", 1), ("fT", 2), ("fT", 3)}
            for g in range(2):
                S.op("pool", lambda g=g: G.memset(bd[g][:], 1.0), r=[("bd", 0), ("bd", 1)], w=[("bd", 0), ("bd", 1)])
                S.op("pool", lambda g=g: G.affine_select(out=bd[g][:], in_=bd[g][:], pattern=[[1, 8], [0, 128]],
                                                         compare_op=ALU.is_equal, fill=0.0, base=8 * g,
                                                         channel_multiplier=-1), r=[("bd", 0), ("bd", 1)], w=[("bd", 0), ("bd", 1)])
                S.op("pool", lambda g=g: G.tensor_copy(out=bdbf[:], in_=bd[g][:]), r=[("bd", 0), ("bd", 1)], w=["bdbf"])
                choff = [g * 512 + i * 128 for i in range(4)] + [1024 + g * 128, 1280 + g * 128]
                S.dma("pool", wx[:, :, 0:512], Wl[:, OFF_XBC + g * 512:OFF_XBC + (g + 1) * 512].rearrange("(kt p) n -> p kt n", p=128), w=["wx"])
                S.dma("pool", wx[:, :, 512:640], Wl[:, OFF_XBC + 1024 + g * 128:OFF_XBC + 1024 + (g + 1) * 128].rearrange("(kt p) n -> p kt n", p=128), w=["wx"])
                S.dma("pool", wx[:, :, 640:768], Wl[:, OFF_XBC + 1280 + g * 128:OFF_XBC + 1280 + (g + 1) * 128].rearrange("(kt p) n -> p kt n", p=128), w=["wx"])
                S.dma("pool", wz[:], Wl[:, OFF_Z + g * 512:OFF_Z + (g + 1) * 512].rearrange("(kt p) n -> p kt n", p=128), w=["wz"])
                for ci in range(6):
                    for j in range(4):
                        S.dma("sp", cw[:, ci, j:j + 1], self.P["ssd_conv_w"][l, j, choff[ci]:choff[ci] + 128].rearrange("(p o) -> p o", o=1), w=["cw"])
                    S.dma("sp", cbi[:, ci:ci + 1], self.P["ssd_conv_b"][l, choff[ci]:choff[ci] + 128].rearrange("(p o) -> p o", o=1), w=["cbi"])
                for ci in range(6):
                    xb = xp[0]
                    kx = ("xp", 0)
                    for tq in range(4):
                        pb, kp = self.psum()
                        for kt in range(KT):
                            S.op("pe", lambda kt=kt, pb=pb, ci=ci, tq=tq: PE.matmul(
                                pb[:], lhsT=wx[:, kt, ci * 128:(ci + 1) * 128], rhs=self.hT[:, kt, tq * 512:(tq + 1) * 512],
                                start=(kt == 0), stop=(kt == KT - 1)), r=["wx"] + hTk[tq * 4:tq * 4 + 4], w=[kp])
                        S.op("act", lambda pb=pb, xb=xb, tq=tq: A.activation(out=xb[:, 3 + tq * 512:3 + (tq + 1) * 512], in_=pb[:],
                                                                            func=AF.Copy), r=[kp], w=[("xp", tq), kp])
                    cacc = self.sb(st, "cacc", [128, T], F32) if (g == 0 and ci == 0) else self._cacc
                    self._cacc = cacc
                    for q in range(4):
                        lo = q * 512
                        xr = [("xp", q)] + ([("xp", q - 1)] if q else ["xp0"])
                        S.op("dve", lambda: V.tensor_scalar(
                            out=cacc[:, lo:lo + 512], in0=xb[:, 3 + lo:3 + lo + 512], scalar1=cw[:, ci, 3:4], scalar2=cbi[:, ci:ci + 1],
                            op0=ALU.mult, op1=ALU.add), r=xr + ["cw", "cbi"], w=[("cacc", q)])
                        for j in range(3):
                            S.op("dve", lambda j=j: V.scalar_tensor_tensor(
                                out=cacc[:, lo:lo + 512], in0=xb[:, j + lo:j + lo + 512], scalar=cw[:, ci, j:j + 1],
                                in1=cacc[:, lo:lo + 512], op0=ALU.mult, op1=ALU.add), r=xr + ["cw", ("cacc", q)], w=[("cacc", q)])
                        S.op("act", lambda: A.activation(out=fT[ci][:, lo:lo + 512], in_=cacc[:, lo:lo + 512], func=AF.Silu),
                             r=[("cacc", q)], w=[(fk[ci], q)])
                    if ci < 5:
                        for t4 in range(4):
                            pb, kp = self.psum()
                            pbv = pb[:].bitcast(BF16)
                            for j in range(4):
                                tt = t4 * 4 + j
                                S.op("pe", lambda j=j, tt=tt, pbv=pbv, ci=ci: PE.transpose(
                                    out=pbv[:, j * 128:(j + 1) * 128], in_=fT[ci][:, tt * 128:(tt + 1) * 128],
                                    identity=self.identbf[:]), r=[(fk[ci], t4), "identbf"], w=[kp])
                            src = pbv[:, 0:512].rearrange("p (a b) -> p a b", a=4)
                            if ci < 4:
                                S.op("act", lambda t4=t4, src=src, ci=ci: A.activation(
                                    out=xs[:, t4 * 4:(t4 + 1) * 4, ci * 128:(ci + 1) * 128], in_=src, func=AF.Copy),
                                    r=[kp], w=["xs", kp])
                            else:
                                S.op("dve", lambda t4=t4, src=src: V.tensor_scalar(
                                    out=bmA[:, t4 * 4:(t4 + 1) * 4, :], in0=src, scalar1=mAB[:, 0:1], scalar2=None, op0=ALU.mult),
                                    r=[kp, "mAB"], w=["bmA", kp])
                                S.op("dve", lambda t4=t4, src=src: V.tensor_scalar(
                                    out=bmB[:, t4 * 4:(t4 + 1) * 4, :], in0=src, scalar1=mAB[:, 1:2], scalar2=None, op0=ALU.mult),
                                    r=[kp, "mAB"], w=["bmB", kp])
                bmT, cmT = fT[4], fT[5]
                cv = cmT[:].rearrange("p (t c j) -> p t c j", c=2, j=64)
                cva = cmTA[:].rearrange("p (t c j) -> p t c j", c=2, j=64)
                cvb = cmTB[:].rearrange("p (t c j) -> p t c j", c=2, j=64)
                S.op("pool", lambda: G.tensor_copy(out=cva[:, :, 0, :], in_=cv[:, :, 0, :]), r=[("fT", 3)], w=["cmTA"])
                S.op("pool", lambda: G.memset(cva[:, :, 1, :], 0.0), w=["cmTA"])
                S.op("pool", lambda: G.tensor_copy(out=cvb[:, :, 1, :], in_=cv[:, :, 1, :]), r=[("fT", 3)], w=["cmTB"])
                S.op("pool", lambda: G.memset(cvb[:, :, 0, :], 0.0), w=["cmTB"])
                hs = slice(g * 8, (g + 1) * 8)
                S.op("pool", lambda: G.memset(S32[:], 0.0), w=["S32"])
                S.op("pool", lambda: G.memset(Sbf[0][:], 0.0), w=[("Sbf", 0)])
                def tile_gen(tt):
                    i2 = tt % 2
                    tsl = slice(tt * 128, (tt + 1) * 128)
                    cA, cB = 2 * tt, 2 * tt + 1
                    pc, kpc = self.psum()
                    S.op("pe", lambda pc=pc: PE.matmul(pc[:, 0:128], lhsT=bmT[:, tsl], rhs=cmT[:, tsl], start=True, stop=True),
                         r=[("fT", 2), ("fT", 3)], w=[kpc])
                    S.op("act", lambda pc=pc: A.activation(out=cbs[i2][:], in_=pc[:, 0:128], func=AF.Copy),
                         r=[kpc], w=[("cbs", i2), kpc])
                    pq, kpq = self.psum()
                    S.op("pe", lambda pq=pq: PE.matmul(pq[0:16, 0:128], lhsT=da[:, tt, :], rhs=tri2[:], start=True, stop=True),
                         r=["da", "tri2"], w=[kpq])
                    S.op("dve", lambda pq=pq: V.tensor_copy(out=acsTt[i2][:], in_=pq[0:16, 0:128]), r=[kpq], w=[("acsTt", i2), kpq])
                    S.op("dve", lambda pq=pq: V.tensor_scalar(out=nacsTt[i2][:], in0=pq[0:16, 0:128], scalar1=-1.0, scalar2=None,
                                                              op0=ALU.mult), r=[kpq], w=[("nacsTt", i2), kpq])
                    S.op("pool", lambda: G.tensor_tensor(out=Dx[i2][:], in0=bd[g][:],
                                                         in1=acsTt[i2][:].unsqueeze(1).to_broadcast([16, 8, 128]), op=ALU.mult),
                         r=[("bd", g), ("acsTt", i2)], w=[("Dx", 0)])
                    S.op("pool", lambda: G.tensor_copy(out=Dxh[:], in_=Dx[i2][:]), r=[("Dx", 0)], w=["Dxh"])
                    S.op("pool", lambda: G.tensor_tensor(out=Dxl[:], in0=Dx[i2][:], in1=Dxh[:], op=ALU.subtract),
                         r=[("Dx", 0), "Dxh"], w=["Dxl"])
                    S.op("dve", lambda: V.tensor_copy(out=nah[i2][:], in_=nacsTt[i2][:]), r=[("nacsTt", i2)], w=[("nah", i2)])
                    S.op("dve", lambda: V.tensor_tensor(out=nal[i2][:], in0=nacsTt[i2][:], in1=nah[i2][:], op=ALU.subtract),
                         r=[("nacsTt", i2), ("nah", i2)], w=[("nal", i2)])
                    yield
                    for hh in range(2):
                        pe_, kpe = self.psum()
                        csl = slice(hh * 512, (hh + 1) * 512)
                        for (lh, rh, kk_, first, last) in (
                                (self.onesbf[0:16, :], Dxh[:].rearrange("p h l -> p (h l)")[:, csl], ["onesbf", "Dxh"], True, False),
                                (self.onesbf[0:16, :], Dxl[:].rearrange("p h l -> p (h l)")[:, csl], ["onesbf", "Dxl"], False, False),
                                (nah[i2][:], bdbf[:].rearrange("p h l -> p (h l)")[:, csl], [("nah", i2), "bdbf"], False, False),
                                (nal[i2][:], bdbf[:].rearrange("p h l -> p (h l)")[:, csl], [("nal", i2), "bdbf"], False, False),
                                (self.identbf[:], negmask[:].rearrange("p h l -> p (h l)")[:, csl], ["identbf", "negmask"], False, True)):
                            S.op("pe", lambda lh=lh, rh=rh, first=first, last=last, pe_=pe_: PE.matmul(
                                pe_[:], lhsT=lh, rhs=rh, start=first, stop=last), r=kk_, w=[kpe])
                        S.op("act", lambda pe_=pe_, hh=hh: A.activation(
                            out=Es[i2][:, hh * 4:(hh + 1) * 4, :], in_=pe_[:].rearrange("p (h l) -> p h l", h=4), func=AF.Exp),
                            r=[kpe], w=[("Es", i2), kpe])
                    S.op("dve", lambda: V.tensor_tensor(out=wT[i2][:], in0=Es[i2][:],
                                                        in1=cbs[i2][:].unsqueeze(1).to_broadcast([128, 8, 128]), op=ALU.mult),
                         r=[("Es", i2), ("cbs", i2)], w=[("wT", i2)])
                    yield
                    pr, kpr = self.psum()
                    pr2, kpr2 = self.psum()
                    S.op("pool", lambda: G.tensor_tensor(out=xdtt[i2][:].rearrange("p (h c) -> p h c", c=64),
                                                         in0=xs[:, tt, :].rearrange("p (h c) -> p h c", c=64),
                                                         in1=dt[:, tt, hs].unsqueeze(2).to_broadcast([128, 8, 64]), op=ALU.mult),
                         r=["xs", "dt"], w=[("xdtt", i2)])
                    S.op("pool", lambda: G.tensor_tensor(out=xendt[i2][:].rearrange("p (h c) -> p h c", c=64),
                                                         in0=xdtt[i2][:].rearrange("p (h c) -> p h c", c=64),
                                                         in1=eend[:, tt, hs].unsqueeze(2).to_broadcast([128, 8, 64]), op=ALU.mult),
                         r=[("xdtt", i2), "eend"], w=[("xendt", i2)])
                    S.op("pe", lambda pr=pr: PE.matmul(pr[:], lhsT=bmA[:, tt, :], rhs=xendt[i2][:], start=True, stop=True),
                         r=["bmA", ("xendt", i2)], w=[kpr])
                    S.op("pe", lambda pr2=pr2: PE.matmul(pr2[:], lhsT=bmB[:, tt, :], rhs=xendt[i2][:], start=True, stop=True),
                         r=["bmB", ("xendt", i2)], w=[kpr2])
                    for (ci_, prx, kprx, cc) in ((cA, pr, kpr, 0), (cB, pr2, kpr2, 1)):
                        S.op("dve", lambda cc=cc: V.tensor_tensor(
                            out=S32[:].rearrange("p (h c) -> p h c", c=64), in0=S32[:].rearrange("p (h c) -> p h c", c=64),
                            in1=edec[:, tt, cc, hs].unsqueeze(2).to_broadcast([128, 8, 64]), op=ALU.mult),
                            r=["S32", "edec"], w=["S32"])
                        S.op("dve", lambda prx=prx: V.tensor_tensor(out=S32[:], in0=prx[:], in1=S32[:], op=ALU.add),
                             r=[kprx, "S32"], w=["S32", kprx])
                        S.op("pool", lambda ci_=ci_: G.tensor_copy(out=Sbf[(ci_ + 1) % 4][:], in_=S32[:]),
                             r=["S32"], w=[("Sbf", (ci_ + 1) % 4)])
                    yield
                    pz, kpz = self.psum()
                    for kt in range(KT):
                        S.op("pe", lambda kt=kt, pz=pz: PE.matmul(pz[:], lhsT=self.hT[:, kt, tsl], rhs=wz[:, kt, :],
                                                                  start=(kt == 0), stop=(kt == KT - 1)), r=["wz", ("hT", tt)], w=[kpz])
                    S.op("act", lambda pz=pz: A.activation(out=zs[i2][:], in_=pz[:], func=AF.Silu), r=[kpz], w=[("zs", i2), kpz])
                    yield
                    py, kpy = self.psum()
                    for hh in range(8):
                        S.op("pe", lambda hh=hh, py=py: PE.matmul(py[:, hh * 64:(hh + 1) * 64], lhsT=wT[i2][:, hh, :],
                                                                  rhs=xdtt[i2][:, hh * 64:(hh + 1) * 64], start=True, stop=True),
                             r=[("wT", i2), ("xdtt", i2)], w=[kpy])
                    po, kpo = self.psum()
                    S.op("pe", lambda po=po: PE.matmul(po[:], lhsT=cmTA[:, tsl], rhs=Sbf[cA % 4][:], start=True, stop=False),
                         r=["cmTA", ("Sbf", cA % 4)], w=[kpo])
                    S.op("pe", lambda po=po: PE.matmul(po[:], lhsT=cmTB[:, tsl], rhs=Sbf[cB % 4][:], start=False, stop=True),
                         r=["cmTB", ("Sbf", cB % 4)], w=[kpo])
                    S.op("dve", lambda po=po: V.tensor_tensor(
                        out=t1[i2][:].rearrange("p (h c) -> p h c", c=64), in0=po[:].rearrange("p (h c) -> p h c", c=64),
                        in1=eacs[:, tt, hs].unsqueeze(2).to_broadcast([128, 8, 64]), op=ALU.mult),
                        r=[kpo, "eacs"], w=[("t1", i2), kpo])
                    S.op("dve", lambda py=py: V.tensor_tensor(out=t1[i2][:], in0=py[:], in1=t1[i2][:], op=ALU.add),
                         r=[kpy, ("t1", i2)], w=[("t1", i2), kpy])
                    S.op("pool", lambda: G.tensor_tensor(
                        out=t2[i2][:].rearrange("p (h c) -> p h c", c=64), in0=xs[:, tt, :].rearrange("p (h c) -> p h c", c=64),
                        in1=dsk[:, hs].unsqueeze(2).to_broadcast([128, 8, 64]), op=ALU.mult), r=["xs", "dsk"], w=[("t2", i2)])
                    S.op("pool", lambda: G.tensor_tensor(out=t2[i2][:], in0=t2[i2][:], in1=t1[i2][:], op=ALU.add),
                         r=[("t2", i2), ("t1", i2)], w=[("t2", i2)])
                    S.op("pool", lambda: G.tensor_tensor(out=t2[i2][:], in0=t2[i2][:], in1=zs[i2][:], op=ALU.mult),
                         r=[("t2", i2), ("zs", i2)], w=[("t2", i2)])
                    yield
                    S.op("act", lambda: A.activation(out=t1[i2][:], in_=t2[i2][:], func=AF.Square, accum_out=ss[i2][:, 0:1]),
                         r=[("t2", i2)], w=[("t1", i2), ("ss", i2)])
                    S.op("act", lambda: A.activation(out=ss[i2][:, 1:2], in_=ss[i2][:, 0:1], func=AF.Sqrt, bias=self.epsc[:, 1:2],
                                                     scale=1.0 / 512.0), r=[("ss", i2), "epsc"], w=[("ss", i2)])
                    S.op("dve", lambda: V.reciprocal(out=ss[i2][:, 1:2], in_=ss[i2][:, 1:2]), r=[("ss", i2)], w=[("ss", i2)])
                    S.op("dve", lambda: V.scalar_tensor_tensor(out=ytm[i2][:], in0=t2[i2][:], scalar=ss[i2][:, 1:2],
                                                               in1=nwbc[:, g * 512:(g + 1) * 512], op0=ALU.mult, op1=ALU.mult),
                         r=[("t2", i2), ("ss", i2), "nwbc"], w=[("ytm", i2)])
                    yield
                    pt, kpt = self.psum()
                    ptv = pt[:].bitcast(BF16)
                    for i in range(4):
                        S.op("pe", lambda i=i, ptv=ptv: PE.transpose(out=ptv[:, i * 128:(i + 1) * 128],
                                                                     in_=ytm[i2][:, i * 128:(i + 1) * 128], identity=self.identbf[:]),
                             r=[("ytm", i2), "identbf"], w=[kpt])
                    S.op("act", lambda ptv=ptv: A.activation(out=yTg[:, :, tsl], in_=ptv[:, 0:512].rearrange("p (a b) -> p a b", a=4),
                                                             func=AF.Copy), r=[kpt], w=["yTg", kpt])

                gens, nxt, rnd = [], 0, 0
                while nxt < NT or gens:
                    if nxt < NT and rnd % 4 == 0:
                        gens.append(tile_gen(nxt))
                        nxt += 1
                    for g_ in list(gens):
                        try:
                            next(g_)
                        except StopIteration:
                            gens.remove(g_)
                    rnd += 1
                for i in range(4):
                    S.dma("sp", ydst[g * 512 + i * 128:g * 512 + (i + 1) * 128, :], yTg[:, i, :], r=["yTg"], w=[("yT0", g * 4 + i)])
            S.barrier()
            S.qset = set()


    def rwkv(self, l):
        nc, S = self.nc, self.S
        V, A, G, PE = nc.vector, nc.scalar, nc.gpsimd, nc.tensor
        Wl = self.P["w_in"][l]
        ydst = self.yT_dram[1]
        hTk = [("hT", t) for t in range(NT)]
        P_ = self.P

        def mm(out, lhsT, rhs, start, stop, r, w):
            S.op("pe", lambda: PE.matmul(out, lhsT=lhsT, rhs=rhs, start=start, stop=stop), r=r, w=w)

        with contextlib.ExitStack() as st:
            mask4 = self.sb(st, "mask4", [128, 4, 128], F32)
            maskL = self.sb(st, "maskL", [128, 2, 128], F32)
            bdm = self.sb(st, "bdm", [128, 128], F32)
            mEO = self.sb(st, "mEO", [128, 2], F32)
            hsel = self.sb(st, "hsel", [128, 2], F32)
            rm = self.sb(st, "rm128", [128, T], BF16)
            c05 = self.sb(st, "c05", [128, 1], F32)
            for j in range(4):
                S.op("pool", lambda j=j: G.affine_select(out=mask4[:, j, :], in_=self.ones[:], pattern=[[1, 128]],
                                                         compare_op=(ALU.is_gt if j % 2 == 0 else ALU.is_ge), fill=0.0,
                                                         base=0, channel_multiplier=-1), r=["ones"], w=["mask4"])
            for j in range(2):
                S.op("pool", lambda j=j: G.affine_select(out=maskL[:, j, :], in_=self.ones[:], pattern=[[-1, 128]],
                                                         compare_op=ALU.is_gt, fill=0.0, base=0, channel_multiplier=1),
                     r=["ones"], w=["maskL"])
            S.op("pool", lambda: G.memset(bdm[:], 0.0), w=["bdm"])
            S.op("pool", lambda: G.memset(bdm[0:64, 0:64], 1.0), w=["bdm"])
            S.op("pool", lambda: G.memset(bdm[64:128, 64:128], 1.0), w=["bdm"])
            for (t_, nm) in ((mEO, "mEO"), (hsel, "hsel")):
                S.op("pool", lambda t_=t_: G.memset(t_[0:64, 0:1], 1.0), w=[nm])
                S.op("pool", lambda t_=t_: G.memset(t_[64:128, 0:1], 0.0), w=[nm])
                S.op("pool", lambda t_=t_: G.memset(t_[0:64, 1:2], 0.0), w=[nm])
                S.op("pool", lambda t_=t_: G.memset(t_[64:128, 1:2], 1.0), w=[nm])
            S.op("pool", lambda: G.memset(rm[:], 1.0), w=["rm"])
            S.op("pool", lambda: G.memset(rm[:].rearrange("p (c j) -> p c j", j=128)[:, :, 0:1], 0.0), w=["rm"])
            S.op("pool", lambda: G.memset(c05[:], -0.5), w=["c05"])
            pc = {}
            for nm, src in (("mu_r", P_["rwkv_mu"][l, 0:1024]), ("mu_k", P_["rwkv_mu"][l, 1024:2048]),
                            ("mu_v", P_["rwkv_mu"][l, 2048:3072]), ("w0", P_["rwkv_w0"][l]), ("a0", P_["rwkv_a0"][l]),
                            ("k_k", P_["rwkv_k_k"][l]), ("k_a", P_["rwkv_k_a"][l]),
                            ("r_k", P_["rwkv_r_k"][l].rearrange("h k -> (h k)"))):
                t_ = self.sb(st, "pc_" + nm, [128, 8, 1], F32)
                S.dma("sp", t_[:], src.rearrange("(q p o) -> p q o", p=128, o=1), w=["pc_" + nm])
                pc[nm] = t_
            mul = self.sb(st, "mul", [128, 3], F32)
            S.dma("sp", mul[0:64, 0:1], P_["rwkv_mu"][l, 3072:3136].rearrange("(p o) -> p o", o=1), w=["mul"])
            S.dma("sp", mul[0:64, 1:2], P_["rwkv_mu"][l, 3136:3200].rearrange("(p o) -> p o", o=1), w=["mul"])
            S.dma("sp", mul[:, 2:3], P_["rwkv_mu"][l, 3200:3328].rearrange("(p o) -> p o", o=1), w=["mul"])
            nw0 = self.sb(st, "nw0", [128, 8, 1], F32)
            omka = self.sb(st, "omka", [128, 8, 1], F32)
            S.op("dve", lambda: V.tensor_scalar(out=nw0[:], in0=pc["w0"][:], scalar1=-1.0, scalar2=None, op0=ALU.mult),
                 r=["pc_w0"], w=["nw0"])
            S.op("dve", lambda: V.tensor_scalar(out=omka[:], in0=pc["k_a"][:], scalar1=-1.0, scalar2=1.0, op0=ALU.mult,
                                                op1=ALU.add), r=["pc_k_a"], w=["omka"])
            wl = self.sb(st, "wl", [128, KT, 256], BF16)
            w2 = self.sb(st, "w2", [64, D], BF16)
            a2 = self.sb(st, "a2", [64, D], BF16)
            g2 = self.sb(st, "g2", [128, D], BF16)
            S.dma("pool", wl[:], Wl[:, OFF_RWKV + 3072:OFF_RWKV + 3328].rearrange("(kt p) n -> p kt n", p=128), w=["wl"])
            S.dma("pool", w2[:], P_["rwkv_w2"][l], w=["w2"])
            S.dma("pool", a2[:], P_["rwkv_a2"][l], w=["a2"])
            S.dma("pool", g2[:], P_["rwkv_g2"][l], w=["g2"])
            txw = self.sb(st, "txw", [64, T], BF16)
            xaT = self.sb(st, "xaT", [64, T], BF16)
            sgT = self.sb(st, "sgT", [128, T], BF16)
            xraw = self.sb(st, "xraw", [128, T + 1], F32)
            F = [self.sb(st, "F%d" % i, [128, T], F32) for i in range(5)]
            S.op("pool", lambda: G.memset(xraw[:, 0:1], 0.0), w=["xraw0"])
            Y2 = xraw[:, 1:T + 1]
            Y3 = F[4]

            S.qset = {"F0", "F1", "F2", "F3", "F4", "xraw", "Pp", "Pc", "iP", "bT", "kT", "vT", "Vtm", "Btm", "Ktm",
                      ("AR", 0), ("AR", 1), "Pend0", "Pend1", "bon"}

            def Q(eng, fn, r=(), w=()):
                for q in range(4):
                    qs_ = slice(q * 512, (q + 1) * 512)
                    rr = [(k, q) if k in S.qset else k for k in r]
                    ww = [(k, q) if k in S.qset else k for k in w]
                    S.op(eng, lambda: fn(qs_, q), r=rr, w=ww)

            def proj_shift(wt, c0, m, mucol, dst, dkey, func=None, rows=128):
                for tq in range(4):
                    pb, kp = self.psum()
                    for kt in range(KT):
                        mm(pb[0:rows, :], wt[:, kt, c0:c0 + m], self.hT[:, kt, tq * 512:(tq + 1) * 512], kt == 0, kt == KT - 1,
                           [wt_key] + hTk[tq * 4:tq * 4 + 4], [kp])
                    S.op("act", lambda pb=pb, tq=tq: A.activation(out=xraw[0:rows, 1 + tq * 512:1 + (tq + 1) * 512],
                                                                  in_=pb[0:rows, :], func=AF.Copy), r=[kp], w=[("xraw", tq), kp])
                dk = (lambda q: (dkey, q)) if dkey in S.qset else (lambda q: dkey)
                for q in range(4):
                    lo, hi = q * 512, (q + 1) * 512
                    xr = [("xraw", q)] + ([("xraw", q - 1)] if q else ["xraw0"])
                    S.op("dve", lambda: V.tensor_tensor(out=F[4][0:rows, lo:hi], in0=xraw[0:rows, lo:hi], in1=xraw[0:rows, lo + 1:hi + 1],
                                                        op=ALU.subtract), r=xr, w=[("F4", q)])
                    if func is None:
                        S.op("dve", lambda: V.scalar_tensor_tensor(out=dst[0:rows, lo:hi], in0=F[4][0:rows, lo:hi], scalar=mucol,
                                                                   in1=xraw[0:rows, lo + 1:hi + 1], op0=ALU.mult, op1=ALU.add),
                             r=[("F4", q), ("xraw", q), "mul"] + list(pc_keys), w=[dk(q)])
                    else:
                        S.op("dve", lambda: V.scalar_tensor_tensor(out=F[4][0:rows, lo:hi], in0=F[4][0:rows, lo:hi], scalar=mucol,
                                                                   in1=xraw[0:rows, lo + 1:hi + 1], op0=ALU.mult, op1=ALU.add),
                             r=[("F4", q), ("xraw", q), "mul"] + list(pc_keys), w=[("F4", q)])
                        S.op("act", lambda: A.activation(out=dst[0:rows, lo:hi], in_=F[4][0:rows, lo:hi], func=func),
                             r=[("F4", q)], w=[dk(q)])

            pc_keys = ["pc_mu_r", "pc_mu_k", "pc_mu_v"]
            wt_key = "wl"
            proj_shift(wl, 0, 64, mul[0:64, 0:1], txw, "txw", AF.Tanh, rows=64)
            proj_shift(wl, 64, 64, mul[0:64, 1:2], xaT, "xaT", AF.Copy, rows=64)
            proj_shift(wl, 128, 128, mul[:, 2:3], sgT, "sgT", AF.Sigmoid, rows=128)
            wrkv1 = self.sb(st, "wrkv", [128, KT, 384], BF16)
            wrkv = [wrkv1, wrkv1]
            Pp = self.sb(st, "Pp", [128, T], BF16)
            Pc = self.sb(st, "Pc", [128, T], BF16)
            PendL = [self.sb(st, "Pend", [128, NT], F32) for _ in range(2)]
            iP = self.sb(st, "iP", [128, T], BF16)
            bT = self.sb(st, "bT", [128, T], BF16)
            kT = self.sb(st, "kT", [128, T], BF16)
            vT = self.sb(st, "vT", [128, T], BF16)
            AR = [self.sb(st, "AR", [128, NT, 2, 128], BF16) for _ in range(2)]
            Vtm = self.sb(st, "Vtm", [128, NT, 128], BF16)
            Btm = self.sb(st, "Btm", [128, NT, 128], BF16)
            aT = Vtm[:].rearrange("p t j -> p (t j)")
            rT = Btm[:].rearrange("p t j -> p (t j)")
            Ktm = self.sb(st, "Ktm", [128, NT, 128], BF16)
            bon = self.sb(st, "bon", [128, NT, 2], F32)
            st32 = self.sb(st, "st32", [128, 2 * NT, 4], F32)
            lnw = self.sb(st, "lnwb", [128, 128], F32)
            lnb = self.sb(st, "lnbb", [128, 128], F32)
            Z32 = self.sb(st, "Z32", [128, 128], F32)
            Zt = self.sb(st, "Zt", [128, 128], F32)
            Zbf = [self.sb(st, "Zbf", [128, 128], BF16) for _ in range(2)]
            Wsb = [self.sb(st, "Wsb", [128, 128], BF16) for _ in range(2)]
            Usb = [self.sb(st, "Usb", [128, 128], BF16) for _ in range(2)]
            NS = 8
            abrb = [self.sb(st, "abrb", [128, 4, 128], BF16) for _ in range(NS)]
            akrk = [self.sb(st, "akrk", [128, 4, 128], BF16) for _ in range(NS)]
            L0 = [self.sb(st, "L0", [128, 2, 128], BF16) for _ in range(4)]
            XX = [[self.sb(st, "XX", [128, 4, 128], BF16) for _ in range(4)] for _ in range(2)]
            Tt = [self.sb(st, "Tt", [128, 2, 128], BF16) for _ in range(NS)]

            def load_w(p):
                b = 0
                for j in range(3):
                    c0 = OFF_RWKV + j * 1024 + p * 128
                    S.dma("pool", wrkv[b][:, :, j * 128:(j + 1) * 128], Wl[:, c0:c0 + 128].rearrange("(kt p) n -> p kt n", p=128),
                          w=[("wrkv", b)])

            def projA(p):
                nonlocal wt_key
                load_w(p)
                wt_key = ("wrkv", 0)
                proj_shift(wrkv[0], 128, 128, pc["mu_k"][:, p, :], F[0], "F0")
                proj_shift(wrkv[0], 0, 128, pc["mu_r"][:, p, :], F[1], "F1")
                proj_shift(wrkv[0], 256, 128, pc["mu_v"][:, p, :], vT, "vT", AF.Copy)

            filler = [None]
            projA(0)
            for p in range(8):
                b = 0
                fs_ = slice(p * 128, (p + 1) * 128)
                S.dma("sp", lnw[:], P_["rwkv_ln_w"][l, fs_].partition_broadcast(128), w=["lnw"])
                S.dma("sp", lnb[:], P_["rwkv_ln_b"][l, fs_].partition_broadcast(128), w=["lnb"])
                def prepB1(p, fs_):
                    for tq in range(4):
                        pb, kp = self.psum()
                        qs_ = slice(tq * 512, (tq + 1) * 512)
                        mm(pb[:], w2[:, fs_], txw[:, qs_], True, True, ["w2", "txw"], [kp])
                        S.op("act", lambda pb=pb: A.activation(out=F[2][:, qs_], in_=pb[:], func=AF.Exp, bias=nw0[:, p, :], scale=-1.0),
                             r=[kp, "nw0"], w=[("F2", tq), kp])
                    yield
                    Q("act", lambda qs, q: A.activation(out=F[2][:, qs], in_=F[2][:, qs], func=AF.Ln, bias=self.epsc[:, 3:4], scale=1.0),
                      r=["F2", "epsc"], w=["F2"])
                    yield
                    Q("act", lambda qs, q: A.activation(out=F[2][:, qs], in_=F[2][:, qs], func=AF.Exp, bias=c05[:, 0:1], scale=-1.0),
                      r=["F2", "c05"], w=["F2"])
                    yield
                    Q("dve", lambda qs, q: V.tensor_tensor_scan(out=F[3][:, qs], data0=rm[:, qs], data1=F[2][:, qs], initial=0.0,
                                                                op0=ALU.mult, op1=ALU.add), r=["rm", "F2"], w=["F3"])
                    yield
                    Q("dve", lambda qs, q: V.tensor_tensor(out=F[2][:, qs], in0=F[3][:, qs], in1=F[2][:, qs], op=ALU.subtract),
                      r=["F2", "F3"], w=["F2"])
                    yield
                    Q("act", lambda qs, q: A.activation(out=Pp[:, qs], in_=F[2][:, qs], func=AF.Exp, scale=-1.0), r=["F2"], w=["Pp"])
                    yield
                    Q("act", lambda qs, q: A.activation(out=Pc[:, qs], in_=F[3][:, qs], func=AF.Exp, scale=-1.0), r=["F3"], w=["Pc"])
                    yield
                    Q("act", lambda qs, q: A.activation(out=iP[:, qs], in_=F[3][:, qs], func=AF.Exp), r=["F3"], w=["iP"])
                    yield
                    Q("act", lambda qs, q: A.activation(out=PendL[p % 2][:, q * 4:(q + 1) * 4],
                                                        in_=F[3][:, qs].rearrange("p (t j) -> p t j", j=128)[:, :, 127],
                                                        func=AF.Exp, scale=-1.0), r=["F3"], w=["Pend%d" % (p % 2)])
                    yield
                    for tq in range(4):
                        pb, kp = self.psum()
                        qs_ = slice(tq * 512, (tq + 1) * 512)
                        mm(pb[:], a2[:, fs_], xaT[:, qs_], True, True, ["a2", "xaT"], [kp])
                        S.op("act", lambda pb=pb: A.activation(out=F[2][:, qs_], in_=pb[:], func=AF.Sigmoid, bias=pc["a0"][:, p, :],
                                                               scale=1.0), r=[kp, "pc_a0", ("Pp", tq)], w=[("F2", tq), kp])
                    yield
                    Q("dve", lambda qs, q: V.tensor_scalar(out=F[3][:, qs], in0=F[0][:, qs], scalar1=pc["k_k"][:, p, :], scalar2=None,
                                                           op0=ALU.mult), r=["F0", "pc_k_k", "Pc", "iP", "Pend%d" % (p % 2)], w=["F3"])
                    yield
                    Q("act", lambda qs, q: A.activation(out=F[4][:, qs], in_=F[3][:, qs], func=AF.Square), r=["F3"], w=["F4"])
                    yield
                    for tq in range(4):
                        pb, kp = self.psum()
                        qs_ = slice(tq * 512, (tq + 1) * 512)
                        mm(pb[:], bdm[:], F[4][:, qs_], True, True, ["bdm", ("F4", tq)], [kp])
                        S.op("act", lambda pb=pb: A.activation(out=F[4][:, qs_], in_=pb[:], func=AF.Sqrt), r=[kp], w=[("F4", tq), kp])
                    yield
                    Q("dve", lambda qs, q: V.tensor_scalar(out=F[4][:, qs], in0=F[4][:, qs], scalar1=1e-12, scalar2=None, op0=ALU.max),
                      r=["F4"], w=["F4"])
                    yield
                    Q("dve", lambda qs, q: V.reciprocal(out=F[4][:, qs], in_=F[4][:, qs]), r=["F4"], w=["F4"])
                    yield
                    Q("dve", lambda qs, q: V.tensor_tensor(out=F[3][:, qs], in0=F[3][:, qs], in1=F[4][:, qs], op=ALU.mult),
                      r=["F3", "F4"], w=["F3"])

                    yield

                def prepB2(p, fs_):
                    Q("dve", lambda qs, q: V.scalar_tensor_tensor(out=aT[:, qs], in0=F[3][:, qs], scalar=-1.0, in1=Pp[:, qs], op0=ALU.mult,
                                                                  op1=ALU.mult), r=["F3", "Pp"], w=["Vtm"])
                    Q("pool", lambda qs, q: G.tensor_tensor(out=F[4][:, qs], in0=F[3][:, qs], in1=F[2][:, qs], op=ALU.mult),
                      r=["F3", "F2"], w=["F4"])
                    Q("pool", lambda qs, q: G.tensor_tensor(out=bT[:, qs], in0=F[4][:, qs], in1=iP[:, qs], op=ALU.mult),
                      r=["F4", "iP"], w=["bT"])
                    Q("dve", lambda qs, q: V.tensor_scalar(out=F[2][:, qs], in0=F[2][:, qs], scalar1=pc["k_a"][:, p, :],
                                                           scalar2=omka[:, p, :], op0=ALU.mult, op1=ALU.add),
                      r=["F2", "pc_k_a", "omka", "F4"], w=["F2"])
                    Q("dve", lambda qs, q: V.tensor_tensor(out=F[0][:, qs], in0=F[0][:, qs], in1=F[2][:, qs], op=ALU.mult),
                      r=["F0", "F2", "F3"], w=["F0"])
                    Q("pool", lambda qs, q: G.tensor_tensor(out=kT[:, qs], in0=F[0][:, qs], in1=iP[:, qs], op=ALU.mult),
                      r=["F0", "iP"], w=["kT"])
                    Q("dve", lambda qs, q: V.tensor_tensor(out=rT[:, qs], in0=F[1][:, qs], in1=Pc[:, qs], op=ALU.mult),
                      r=["F1", "Pc"], w=["Btm"])
                    Q("dve", lambda qs, q: V.scalar_tensor_tensor(out=F[1][:, qs], in0=F[1][:, qs], scalar=pc["r_k"][:, p, :],
                                                                  in1=F[0][:, qs], op0=ALU.mult, op1=ALU.mult),
                      r=["F1", "F0", "pc_r_k", "Btm"], w=["F1"])
                    for h in range(2):
                        Q("act", lambda qs, q, h=h: A.activation(out=AR[h][:, q * 4:(q + 1) * 4, 0, :], in_=Vtm[:, q * 4:(q + 1) * 4, :],
                                                                 func=AF.Identity, scale=mEO[:, h:h + 1]), r=["Vtm", "mEO"], w=[("AR", h)])
                        Q("dve", lambda qs, q, h=h: V.tensor_scalar(out=AR[h][:, q * 4:(q + 1) * 4, 1, :], in0=Btm[:, q * 4:(q + 1) * 4, :],
                                                                    scalar1=mEO[:, h:h + 1], scalar2=None, op0=ALU.mult),
                          r=["Btm", "mEO"], w=[("AR", h)])
                    for (src, skey, dst, dkey) in ((vT, "vT", Vtm, "Vtm"), (bT, "bT", Btm, "Btm"), (kT, "kT", Ktm, "Ktm")):
                        for t4 in range(4):
                            pb, kp = self.psum()
                            pbv = pb[:].bitcast(BF16)
                            for j in range(4):
                                tt = t4 * 4 + j
                                S.op("pe", lambda j=j, tt=tt, pbv=pbv, src=src: PE.transpose(
                                    out=pbv[:, j * 128:(j + 1) * 128], in_=src[:, tt * 128:(tt + 1) * 128], identity=self.identbf[:]),
                                    r=[(skey, t4), "identbf"], w=[kp])
                            S.op("act", lambda t4=t4, pbv=pbv, dst=dst: A.activation(
                                out=dst[:, t4 * 4:(t4 + 1) * 4, :], in_=pbv[:, 0:512].rearrange("p (a b) -> p a b", a=4), func=AF.Copy),
                                r=[kp, (("AR", 0), t4), (("AR", 1), t4)], w=[(dkey, t4), kp])
                    for t4 in range(4):
                        pb, kp = self.psum()
                        for j in range(4):
                            tt = t4 * 4 + j
                            mm(pb[:, j * 2:(j + 1) * 2], F[1][:, tt * 128:(tt + 1) * 128], hsel[:], True, True, [("F1", t4), "hsel"], [kp])
                        S.op("dve", lambda pb=pb, t4=t4: V.tensor_copy(out=bon[:, t4 * 4:(t4 + 1) * 4, :].rearrange("p t h -> p (t h)"),
                                                                       in_=pb[:, 0:8]), r=[kp], w=[("bon", t4), kp])

                ytm = Y2[:].rearrange("p (t j) -> p t j", j=128)
                ysq = Y3[:].rearrange("p (t j) -> p t j", j=128)

                def inv_group(gi, pending=()):
                    pending = list(pending)
                    tiles = range(gi * 4, gi * 4 + 4)
                    for tt in tiles:
                        sl = tt % NS
                        tsl = slice(tt * 128, (tt + 1) * 128)
                        p1, k1 = self.psum()
                        p2, k2 = self.psum()
                        p3, k3 = self.psum()
                        for h in range(2):
                            rhs = AR[h][:, tt].rearrange("p a j -> p (a j)")
                            mm(p1[:, h * 256:(h + 1) * 256], bT[:, tsl], rhs, True, True, ["bT", ("AR", h)], [k1])
                            mm(p2[:, h * 256:(h + 1) * 256], kT[:, tsl], rhs, True, True, ["kT", ("AR", h)], [k2])
                            mm(p3[:, h * 128:(h + 1) * 128], AR[h][:, tt, 0, :], bT[:, tsl], True, True, [("AR", h), "bT"], [k3])
                        S.op("dve", lambda p1=p1, sl=sl: V.tensor_tensor(out=abrb[sl][:].rearrange("p a j -> p (a j)"), in0=p1[:],
                                                                        in1=mask4[:].rearrange("p a j -> p (a j)"), op=ALU.mult),
                             r=[k1, "mask4"], w=[("abrb", sl), k1])
                        S.op("dve", lambda p2=p2, sl=sl: V.tensor_tensor(out=akrk[sl][:].rearrange("p a j -> p (a j)"), in0=p2[:],
                                                                        in1=mask4[:].rearrange("p a j -> p (a j)"), op=ALU.mult),
                             r=[k2, "mask4"], w=[("akrk", sl), k2])
                        S.op("dve", lambda p3=p3, sl=sl: V.tensor_tensor(out=L0[sl % 4][:].rearrange("p a j -> p (a j)"), in0=p3[:, 0:256],
                                                                        in1=maskL[:].rearrange("p a j -> p (a j)"), op=ALU.mult),
                             r=[k3, "maskL"], w=[("L0", sl % 4), k3])
                        for h in range(2):
                            S.op("pool", lambda h=h, sl=sl: G.tensor_tensor(out=Tt[sl][:, h, :], in0=abrb[sl][:, 2 * h, :],
                                                                            in1=self.identbf[:], op=ALU.add),
                                 r=[("abrb", sl), "identbf"], w=[("Tt", sl)])

                    def Xk(k, sl, h):
                        return (L0[sl % 4][:, h, :], ("L0", sl % 4)) if k == 0 else (XX[k % 2][sl % 4][:, 2 * h, :], ("XX", k % 2, sl % 4))

                    def Xtk(k, sl, h):
                        return (abrb[sl][:, 2 * h, :], ("abrb", sl)) if k == 0 else (XX[k % 2][sl % 4][:, 2 * h + 1, :], ("XX", k % 2, sl % 4))

                    for k in range(7):
                        sqb = {}
                        if k <= 5:
                            for tt in tiles:
                                sl = tt % NS
                                pb, kp = self.psum()
                                sqb[tt] = (pb, kp)
                                for h in range(2):
                                    x, kx = Xk(k, sl, h)
                                    xt, kxt = Xtk(k, sl, h)
                                    mm(pb[:, (2 * h) * 128:(2 * h + 1) * 128], xt, x, True, True, [kx, kxt], [kp])
                                    if k < 5:
                                        mm(pb[:, (2 * h + 1) * 128:(2 * h + 2) * 128], x, xt, True, True, [kx, kxt], [kp])
                        ttb = []
                        if k >= 1:
                            for t2 in range(2):
                                pb, kp = self.psum()
                                ttb.append((pb, kp))
                                for j in range(2):
                                    sl = (gi * 4 + t2 * 2 + j) % NS
                                    for h in range(2):
                                        x1, kx1 = Xk(k, sl, h)
                                        mm(pb[:, (j * 2 + h) * 128:(j * 2 + h + 1) * 128], x1, Tt[sl][:, h, :], True, True,
                                           [kx1, ("Tt", sl)], [kp])
                        if k <= 5:
                            for tt in tiles:
                                sl = tt % NS
                                pb, kp = sqb[tt]
                                kn = ("XX", (k + 1) % 2, sl % 4)
                                if k < 5:
                                    S.op("act", lambda pb=pb, sl=sl, k=k: A.activation(
                                        out=XX[(k + 1) % 2][sl % 4][:].rearrange("p a j -> p (a j)"), in_=pb[:], func=AF.Copy),
                                        r=[kp], w=[kn, kp])
                                else:
                                    S.op("act", lambda pb=pb, sl=sl, k=k: A.activation(
                                        out=XX[(k + 1) % 2][sl % 4][:, 0:4:2, :],
                                        in_=pb[:].rearrange("p (a j) -> p a j", j=128)[:, 0:4:2, :], func=AF.Copy), r=[kp], w=[kn, kp])
                        for t2, (pb, kp) in enumerate(ttb):
                            for j in range(2):
                                sl = (gi * 4 + t2 * 2 + j) % NS
                                S.op("dve", lambda pb=pb, sl=sl, j=j: V.tensor_tensor(
                                    out=Tt[sl][:].rearrange("p a j -> p (a j)"), in0=pb[:, j * 256:(j + 1) * 256],
                                    in1=Tt[sl][:].rearrange("p a j -> p (a j)"), op=ALU.add), r=[kp, ("Tt", sl)], w=[("Tt", sl), kp])
                        if pending and k >= 1:
                            chain_tile(pending.pop(0))
                        if filler[0] is not None:
                            next(filler[0], None)
                    while pending:
                        chain_tile(pending.pop(0))

                def chain_tile(tt):
                    if True:
                        sl = tt % NS
                        i2 = tt % 2
                        tsl = slice(tt * 128, (tt + 1) * 128)
                        zb, kz = Zbf[i2], ("Zbf", i2)
                        pw, kpw = self.psum()
                        mm(pw[:, 0:128], AR[0][:, tt, 0, :], zb[:], True, False, [("AR", 0), kz], [kpw])
                        mm(pw[:, 0:128], AR[1][:, tt, 0, :], zb[:], False, False, [("AR", 1), kz], [kpw])
                        for h in range(2):
                            mm(pw[:, h * 64:(h + 1) * 64], akrk[sl][:, 2 * h, :], Vtm[:, tt, h * 64:(h + 1) * 64], False, h == 1,
                               [("akrk", sl), "Vtm"], [kpw])
                        S.op("act", lambda pw=pw: A.activation(out=Wsb[i2][:], in_=pw[:, 0:128], func=AF.Copy),
                             r=[kpw], w=[("Wsb", i2), kpw])
                        pu, kpu = self.psum()
                        for h in range(2):
                            mm(pu[:, h * 64:(h + 1) * 64], Tt[sl][:, h, :], Wsb[i2][:, h * 64:(h + 1) * 64], True, True,
                               [("Tt", sl), ("Wsb", i2)], [kpu])
                        S.op("act", lambda pu=pu: A.activation(out=Usb[i2][:], in_=pu[:, 0:128], func=AF.Copy),
                             r=[kpu], w=[("Usb", i2), kpu])
                        py, kpy = self.psum()
                        mm(py[:, 0:128], AR[0][:, tt, 1, :], zb[:], True, False, [("AR", 0), kz], [kpy])
                        mm(py[:, 0:128], AR[1][:, tt, 1, :], zb[:], False, False, [("AR", 1), kz], [kpy])
                        for h in range(2):
                            mm(py[:, h * 64:(h + 1) * 64], abrb[sl][:, 2 * h + 1, :], Usb[i2][:, h * 64:(h + 1) * 64], False, False,
                               [("abrb", sl), ("Usb", i2)], [kpy])
                            mm(py[:, h * 64:(h + 1) * 64], akrk[sl][:, 2 * h + 1, :], Vtm[:, tt, h * 64:(h + 1) * 64], False, h == 1,
                               [("akrk", sl), "Vtm"], [kpy])
                        S.op("act", lambda py=py: A.activation(out=ytm[:, tt, :], in_=py[:, 0:128], func=AF.Copy),
                             r=[kpy], w=["xraw", kpy])
                        pz, kpz = self.psum()
                        mm(pz[:, 0:128], Btm[:, tt, :], Usb[i2][:], True, False, ["Btm", ("Usb", i2)], [kpz])
                        mm(pz[:, 0:128], Ktm[:, tt, :], Vtm[:, tt, :], False, True, ["Ktm", "Vtm"], [kpz])
                        S.op("dve", lambda pz=pz: V.tensor_tensor(out=Zt[:], in0=pz[:, 0:128], in1=bdm[:], op=ALU.mult),
                             r=[kpz, "bdm"], w=["Zt", kpz])
                        S.op("dve", lambda: V.tensor_tensor(out=Zt[:], in0=Zt[:], in1=Z32[:], op=ALU.add), r=["Zt", "Z32"], w=["Zt"])
                        S.op("dve", lambda: V.tensor_scalar(out=Z32[:], in0=Zt[:], scalar1=PendL[p % 2][:, tt:tt + 1],
                                                            scalar2=None, op0=ALU.mult), r=["Zt", "Pend%d" % (p % 2)], w=["Z32"])
                        S.op("pool", lambda: G.tensor_copy(out=Zbf[(tt + 1) % 2][:], in_=Z32[:]), r=["Z32"], w=[("Zbf", (tt + 1) % 2)])

                S.op("pool", lambda: G.memset(Z32[:], 0.0), w=["Z32"])
                S.op("pool", lambda: G.memset(Zbf[0][:], 0.0), w=[("Zbf", 0)])
                if p == 0:
                    for _ in prepB1(0, fs_):
                        pass
                prepB2(p, fs_)
                if p + 1 < 8:
                    projA(p + 1)
                    filler[0] = prepB1(p + 1, slice((p + 1) * 128, (p + 2) * 128))
                inv_group(0)
                for gi in range(1, 4):
                    inv_group(gi, pending=range((gi - 1) * 4, gi * 4))
                for tt in range(12, 16):
                    chain_tile(tt)
                if filler[0] is not None:
                    for _ in filler[0]:
                        pass
                    filler[0] = None
                y16, yTp = Btm, kT
                def output_phase(p=p, fs_=fs_, ytm=ytm, ysq=ysq):
                    y3 = ytm.rearrange("p t (h c) -> p (t h) c", c=64)
                    q3 = ysq.rearrange("p t (h c) -> p (t h) c", c=64)
                    S.op("act", lambda: A.activation(out=Y3[:], in_=Y2[:], func=AF.Square), r=["xraw"], w=["F4"])
                    S.op("dve", lambda: V.tensor_reduce(out=st32[:, :, 0], in_=y3, axis=AX.X, op=ALU.add), r=["xraw"], w=["st32"])
                    S.op("dve", lambda: V.tensor_reduce(out=st32[:, :, 1], in_=q3, axis=AX.X, op=ALU.add), r=["F4"], w=["st32"])
                    S.op("dve", lambda: V.tensor_scalar(out=st32[:, :, 0], in0=st32[:, :, 0], scalar1=1.0 / 64.0, scalar2=None,
                                                        op0=ALU.mult), r=["st32"], w=["st32"])
                    S.op("dve", lambda: V.tensor_tensor(out=st32[:, :, 2], in0=st32[:, :, 0], in1=st32[:, :, 0], op=ALU.mult),
                         r=["st32"], w=["st32"])
                    S.op("dve", lambda: V.scalar_tensor_tensor(out=st32[:, :, 1], in0=st32[:, :, 1], scalar=1.0 / 64.0, in1=st32[:, :, 2],
                                                               op0=ALU.mult, op1=ALU.subtract), r=["st32"], w=["st32"])
                    S.op("act", lambda: A.activation(out=st32[:, :, 1], in_=st32[:, :, 1], func=AF.Sqrt, bias=self.epsc[:, 2:3], scale=1.0),
                         r=["st32", "epsc"], w=["st32"])
                    S.op("dve", lambda: V.reciprocal(out=st32[:, :, 1], in_=st32[:, :, 1]), r=["st32"], w=["st32"])
                    S.op("dve", lambda: V.tensor_tensor(out=y3, in0=y3, in1=st32[:, :, 0:1].to_broadcast([128, 2 * NT, 64]),
                                                        op=ALU.subtract), r=["xraw", "st32"], w=["xraw"])
                    S.op("dve", lambda: V.tensor_tensor(out=y3, in0=y3, in1=st32[:, :, 1:2].to_broadcast([128, 2 * NT, 64]),
                                                        op=ALU.mult), r=["xraw", "st32"], w=["xraw"])
                    S.op("pool", lambda: G.tensor_tensor(out=ytm, in0=ytm, in1=lnw[:].unsqueeze(1).to_broadcast([128, NT, 128]),
                                                         op=ALU.mult), r=["xraw", "lnw"], w=["xraw"])
                    S.op("pool", lambda: G.tensor_tensor(out=ytm, in0=ytm, in1=lnb[:].unsqueeze(1).to_broadcast([128, NT, 128]),
                                                         op=ALU.add), r=["xraw", "lnb"], w=["xraw"])
                    S.op("dve", lambda: V.tensor_tensor(out=q3, in0=Vtm[:].rearrange("p t (h c) -> p (t h) c", c=64),
                                                        in1=bon[:].rearrange("p t h -> p (t h)").unsqueeze(2).to_broadcast([128, 2 * NT, 64]),
                                                        op=ALU.mult), r=["Vtm", "bon", "F4"], w=["F4"])
                    S.op("dve", lambda: V.tensor_tensor(out=Y2[:], in0=Y2[:], in1=Y3[:], op=ALU.add), r=["xraw", "F4"], w=["xraw"])
                    for t4 in range(4):
                        pb, kp = self.psum()
                        for j in range(4):
                            tt = t4 * 4 + j
                            mm(pb[:, j * 128:(j + 1) * 128], sgT[:, tt * 128:(tt + 1) * 128], g2[:, fs_], True, True, ["sgT", "g2"], [kp])
                        S.op("dve", lambda pb=pb, t4=t4: V.tensor_tensor(
                            out=y16[:, t4 * 4:(t4 + 1) * 4, :], in0=pb[:].rearrange("p (a j) -> p a j", j=128),
                            in1=ytm[:, t4 * 4:(t4 + 1) * 4, :], op=ALU.mult), r=[kp, "xraw"], w=["Btm", kp])
                    for t4 in range(4):
                        pb, kp = self.psum()
                        pbv = pb[:].bitcast(BF16)
                        for j in range(4):
                            tt = t4 * 4 + j
                            S.op("pe", lambda j=j, tt=tt, pbv=pbv: PE.transpose(out=pbv[:, j * 128:(j + 1) * 128], in_=y16[:, tt, :],
                                                                               identity=self.identbf[:]), r=["Btm", "identbf"], w=[kp])
                        S.op("act", lambda t4=t4, pbv=pbv: A.activation(out=yTp[:, t4 * 512:(t4 + 1) * 512], in_=pbv[:, 0:512], func=AF.Copy),
                             r=[kp], w=["kT", kp])
                    S.dma("sp", ydst[fs_, :], yTp[:], r=["kT"], w=[("yT1", p)])
                output_phase()
            S.barrier()
            S.qset = set()


    def merge(self, l):
        nc, S = self.nc, self.S
        V, A, G, PE = nc.vector, nc.scalar, nc.gpsimd, nc.tensor
        Wl = self.P["w_in"][l]
        brw = [self.P["w_br_ssd"][l], self.P["w_br_rwkv"][l], self.P["w_br_hgrn"][l]]
        hTk = [("hT", t) for t in range(NT)]
        with contextlib.ExitStack() as st:
            mT = self.sb(st, "mT", [128, KT, T], F32)
            wbrs = [self.sb(st, "wbr", [128, KT, D], BF16) for _ in range(2)]
            wgts = [self.sb(st, "wgt", [128, KT, D], BF16) for _ in range(2)]
            st1 = contextlib.ExitStack()
            st1.__enter__()
            yq = [self.sb(st1, "yq", [128, KT, 512], BF16) for _ in range(2)]
            sg = [self.sb(st1, "sgm", [128, 512], BF16) for _ in range(2)]
            tmp = [self.sb(st1, "tmpm", [128, 512], F32) for _ in range(2)]
            cnt = 0
            def load_br(i):
                S.dma("pool", wbrs[i % 2][:], brw[i].rearrange("(kt p) n -> p kt n", p=128), w=[("wbr", i % 2)])
                c0 = OFF_GATES + i * 1024
                S.dma("pool", wgts[i % 2][:], Wl[:, c0:c0 + 1024].rearrange("(kt p) n -> p kt n", p=128), w=[("wgt", i % 2)])

            load_br(0)
            load_br(1)
            for i in range(3):
                wbr, wgt = wbrs[i % 2], wgts[i % 2]
                kwb, kwg = ("wbr", i % 2), ("wgt", i % 2)
                if i == 2:
                    load_br(2)
                for q in range(4):
                    qs_ = slice(q * 512, (q + 1) * 512)
                    yb = (i * 4 + q) % 2
                    S.dma("sp", yq[yb][:], self.yT_dram[i][:, qs_].rearrange("(kt p) n -> p kt n", p=128),
                          r=[("yT%d" % i, k) for k in range(8)], w=[("yq", yb)])
                    for ot in range(KT):
                        os_ = slice(ot * 128, (ot + 1) * 128)
                        pg, kg = self.psum()
                        pb, kb = self.psum()
                        for kt in range(KT):
                            S.op("pe", lambda kt=kt, pg=pg: PE.matmul(pg[:], lhsT=wgt[:, kt, os_], rhs=self.hT[:, kt, qs_],
                                                                      start=(kt == 0), stop=(kt == KT - 1)),
                                 r=[kwg] + hTk[q * 4:q * 4 + 4], w=[kg])
                        for kt in range(KT):
                            S.op("pe", lambda kt=kt, pb=pb: PE.matmul(pb[:], lhsT=wbr[:, kt, os_], rhs=yq[yb][:, kt, :],
                                                                      start=(kt == 0), stop=(kt == KT - 1)),
                                 r=[kwb, ("yq", yb)], w=[kb])
                        c2 = cnt % 2
                        cnt += 1
                        S.op("act", lambda pg=pg, c2=c2: A.activation(out=sg[c2][:], in_=pg[:], func=AF.Sigmoid),
                             r=[kg], w=[("sgm", c2), kg])
                        if i == 0:
                            S.op("dve", lambda pb=pb, c2=c2: V.tensor_tensor(out=mT[:, ot, qs_], in0=pb[:], in1=sg[c2][:], op=ALU.mult),
                                 r=[kb, ("sgm", c2)], w=[("mT", q), kb])
                        else:
                            S.op("dve", lambda pb=pb, c2=c2: V.tensor_tensor(out=tmp[c2][:], in0=pb[:], in1=sg[c2][:], op=ALU.mult),
                                 r=[kb, ("sgm", c2)], w=[("tmpm", c2), kb])
                            S.op("pool", lambda c2=c2: G.tensor_tensor(out=mT[:, ot, qs_], in0=mT[:, ot, qs_], in1=tmp[c2][:],
                                                                       op=ALU.add), r=[("tmpm", c2), ("mT", q)], w=[("mT", q)])
            self.dbg_dump("merged%d" % l, lambda o: S.dma("sp", o.rearrange("(kt p) n -> p kt n", p=128), mT[:],
                                                          r=[("mT", q) for q in range(4)]))
            S.barrier()
            st1.__exit__(None, None, None)
            wo = wbrs[1]
            S.dma("pool", wo[:], self.P["w_out"][l].rearrange("(kt p) n -> p kt n", p=128), w=[("wbr", 1)])
            gbc = self.sb(st, "gbc1", [128, D], F32)
            bbc = self.sb(st, "bbc1", [128, D], F32)
            S.dma("sp", gbc[:], self.P["ln1_g"][l].partition_broadcast(128), w=["gbc"])
            S.dma("sp", bbc[:], self.P["ln1_b"][l].partition_broadcast(128), w=["bbc"])
            lnw = self.ln_alloc(st)
            h1 = [self.sb(st, "h1m", [128, D], F32) for _ in range(2)]
            mbf1 = self.sb(st, "mbf", [128, KT, 128], BF16)
            mbf = [mbf1, mbf1]
            def m2_tile(tt):
                s2 = tt % 2
                q = tt // 4
                tsl = slice(tt * 128, (tt + 1) * 128)
                S.op("act", lambda: A.activation(out=mbf[s2][:], in_=mT[:, :, tsl], func=AF.Copy), r=[("mT", q)], w=[("mbf", 0)])
                S.dma("sp", h1[s2][:], self.h_dram[tsl, :], r=[("hd", tt)], w=[("h1m", s2)])
                xin, kx = self.ln_xin(lnw, tt)
                for half in range(2):
                    po, ko = self.psum()
                    for kt in range(KT):
                        S.op("pe", lambda kt=kt, po=po: PE.matmul(po[:], lhsT=mbf[s2][:, kt, :], rhs=wo[:, kt, half * 512:(half + 1) * 512],
                                                                  start=(kt == 0), stop=(kt == KT - 1)), r=[("mbf", 0), ("wbr", 1)], w=[ko])
                    S.op("dve", lambda po=po, half=half: V.scalar_tensor_tensor(
                        out=xin[:, half * 512:(half + 1) * 512], in0=h1[s2][:, half * 512:(half + 1) * 512], scalar=ALPHA, in1=po[:],
                        op0=ALU.mult, op1=ALU.add), r=[ko, ("h1m", s2)], w=[kx, ko])
                yield from self.ln_tile(lnw, tt, gbc, bbc, self.h_dram, router=True, extra=self.dbg_out.get("h1_%d" % l))
            self.pipeline([(lambda tt=tt: m2_tile(tt)) for tt in range(NT)], 2)
            S.barrier()

    def layer(self, l):
        S = self.S
        if "ssd" in self.stages:
            self.ssd(l)
            self.dbg_dump("ya%d" % l, lambda o: S.dma("sp", o, self.yT_dram[0], r=[("yT0", h) for h in range(8)]))
        if "rwkv" in self.stages:
            self.rwkv(l)
            self.dbg_dump("yb%d" % l, lambda o: S.dma("sp", o, self.yT_dram[1], r=[("yT1", h) for h in range(8)]))
        if "hgrn" in self.stages:
            self.hgrn(l)
            self.dbg_dump("yc%d" % l, lambda o: S.dma("sp", o, self.yT_dram[2], r=[("yT2", h) for h in range(8)]))
        if "merge" in self.stages:
            self.merge(l)
        if "moe" in self.stages:
            self.moe(l, last=(l == self.depth - 1))


_NC_CACHE = {}


def _get_nc():
    if "nc" not in _NC_CACHE:
        _NC_CACHE["nc"] = Builder().build()
    return _NC_CACHE["nc"]


def kernel(**inputs):
    nc = _get_nc()
    x = np.ascontiguousarray(inputs["x"], dtype=np.float32)
    base = {k: np.ascontiguousarray(inputs[k], dtype=np.float32) for k in PARAM_SHAPES}
    in_maps = []
    for c in range(8):
        m = dict(base)
        m["x"] = x[c]
        in_maps.append(m)
    res = run_bass_kernel_spmd(nc, in_maps, core_ids=list(range(8)))
    return np.stack([res.results[c]["out"] for c in range(8)], axis=0)
```
